# Optimizing a Trainium2 kernel written in Bass

```python
import math, functools
import jax, jax.numpy as jnp
from jax import lax
import numpy as np

D_MODEL = 1024
BATCH = 8
SEQ = 4096
DEPTH = 2

GRID_W = 64
CTX_LEN = 256
HEAD_DIM = 64
ROPE_THETA = 10000.0
BLOCK = 128
S5_WIDTH = D_MODEL // 2
S5_GROUP = 16
S5_GROUPS = S5_WIDTH // S5_GROUP
S5_STATE = 64
WIN_HEADS = (D_MODEL // 2) // HEAD_DIM
WIN_KV_HEADS = WIN_HEADS // 4
WINDOW = 128
C_HEADS = D_MODEL // HEAD_DIM
C_KV_HEADS = C_HEADS // 4
N_EXPERTS = 32
N_GROUPS = 8
EXPERTS_PER_GROUP = N_EXPERTS // N_GROUPS
TOP_K = 2
EXPERT_FF = D_MODEL // 2
N_EVEN = (DEPTH + 1) // 2
N_ODD = DEPTH // 2
ALPHA = (2 * DEPTH) ** 0.25
BETA = (8 * DEPTH) ** -0.25
LN_EPS = 1e-5
RMS_EPS = 1e-6
NEG_INF = -1e30
EVEN_IN = S5_WIDTH + (WIN_HEADS + 2 * WIN_KV_HEADS) * HEAD_DIM
EVEN_MIX = S5_WIDTH + WIN_HEADS * HEAD_DIM
ODD_IN = (C_HEADS + 2 * C_KV_HEADS) * HEAD_DIM
ODD_MIX = C_HEADS * HEAD_DIM

kernel_name = 'hybrid_s5_swa_axialgqa_groupmoe_dit'


def layer_norm(x, g, b):
    xf = x.astype(jnp.float32)
    mu = jnp.mean(xf, -1, keepdims=True)
    xc = xf - mu
    var = jnp.mean(xc * xc, -1, keepdims=True)
    return (xc * lax.rsqrt(var + LN_EPS) * g + b).astype(x.dtype)


def rms_norm(x, g):
    xf = x.astype(jnp.float32)
    return (xf * lax.rsqrt(jnp.mean(xf * xf, -1, keepdims=True) + RMS_EPS) * g).astype(x.dtype)


def axial_rope_tables(rows):
    n_freq = HEAD_DIM // 4
    inv_freq = ROPE_THETA ** (-jnp.arange(n_freq, dtype=jnp.float32) / n_freq)
    r = jnp.repeat(jnp.arange(rows, dtype=jnp.float32), GRID_W)
    col = jnp.tile(jnp.arange(GRID_W, dtype=jnp.float32), rows)
    ang = jnp.concatenate([r[:, None] * inv_freq, col[:, None] * inv_freq], -1)
    return jnp.cos(ang), jnp.sin(ang)


def apply_rope(t, cos, sin):
    t1, t2 = jnp.split(t.astype(jnp.float32), 2, axis=-1)
    return jnp.concatenate([t1 * cos - t2 * sin, t2 * cos + t1 * sin], -1).astype(t.dtype)


def to_q_heads(t, n_kv, grp):
    b, n, _ = t.shape
    return t.reshape(b, n, n_kv, grp, HEAD_DIM).transpose(0, 2, 3, 1, 4)


def to_kv_heads(t, n_kv):
    b, n, _ = t.shape
    return t.reshape(b, n, n_kv, HEAD_DIM).transpose(0, 2, 1, 3)


def merge_heads(o):
    b, h, g, n, d = o.shape
    return o.transpose(0, 3, 1, 2, 4).reshape(b, n, h * g * d)


def full_attention(q, k, v, sink=None):
    bsz, hkv, grp = q.shape[:3]
    s = jnp.einsum('bhgqd,bhkd->bhgqk', q, k, preferred_element_type=jnp.float32) * (HEAD_DIM ** -0.5)
    if sink is None:
        p = jax.nn.softmax(s, axis=-1)
    else:
        sink_b = jnp.broadcast_to(sink.astype(jnp.float32).reshape(1, hkv, grp, 1, 1), s.shape[:-1] + (1,))
        p = jax.nn.softmax(jnp.concatenate([s, sink_b], -1), axis=-1)[..., :-1]
    return jnp.einsum('bhgqk,bhkd->bhgqd', p.astype(v.dtype), v)


def blocked_attention(q, k, v):
    bsz, hkv, grp, seq, hd = q.shape
    nb = seq // BLOCK
    qb = jnp.moveaxis(q.reshape(bsz, hkv, grp, nb, BLOCK, hd), 3, 0)
    ob = lax.map(lambda qblk: full_attention(qblk, k, v), qb)
    return jnp.moveaxis(ob, 0, 3).reshape(bsz, hkv, grp, seq, hd)


def window_attention(q, k, v, kc, vc, sink):
    bsz, hkv, grp, seq, hd = q.shape
    nb = seq // BLOCK
    qb = q.reshape(bsz, hkv, grp, nb, BLOCK, hd)

    def band(t):
        tp = jnp.pad(t, ((0, 0), (0, 0), (BLOCK, BLOCK), (0, 0))).reshape(bsz, hkv, nb + 2, BLOCK, hd)
        return jnp.concatenate([tp[:, :, :-2], tp[:, :, 1:-1], tp[:, :, 2:]], axis=3)

    kb, vb = band(k), band(v)
    scale = HEAD_DIM ** -0.5
    s_loc = jnp.einsum('bhgnqd,bhnkd->bhgnqk', qb, kb, preferred_element_type=jnp.float32) * scale
    s_ctx = jnp.einsum('bhgnqd,bhkd->bhgnqk', qb, kc, preferred_element_type=jnp.float32) * scale
    q_pos = jnp.arange(seq).reshape(nb, BLOCK, 1)
    k_pos = ((jnp.arange(nb)[:, None] - 1) * BLOCK + jnp.arange(3 * BLOCK)[None, :])[:, None, :]
    valid = (jnp.abs(q_pos - k_pos) <= WINDOW) & (k_pos >= 0) & (k_pos < seq)
    s_loc = jnp.where(valid, s_loc, NEG_INF)
    sink_b = jnp.broadcast_to(sink.astype(jnp.float32).reshape(1, hkv, grp, 1, 1, 1), s_ctx.shape[:-1] + (1,))
    p = jax.nn.softmax(jnp.concatenate([s_ctx, s_loc, sink_b], -1), axis=-1).astype(v.dtype)
    n_ctx = kc.shape[2]
    out = (jnp.einsum('bhgnqk,bhkd->bhgnqd', p[..., :n_ctx], vc)
           + jnp.einsum('bhgnqk,bhnkd->bhgnqd', p[..., n_ctx:-1], vb))
    return out.reshape(bsz, hkv, grp, seq, hd)


def s5_discretize(lam_re, lam_im, log_step, b_re, b_im):
    lr, li = lam_re.astype(jnp.float32), lam_im.astype(jnp.float32)
    dt = jnp.exp(log_step.astype(jnp.float32))[:, None]
    mag = jnp.exp(lr * dt)
    ab_re, ab_im = mag * jnp.cos(li * dt), mag * jnp.sin(li * dt)
    den = lr * lr + li * li
    num_re, num_im = ab_re - 1.0, ab_im
    f_re = (num_re * lr + num_im * li) / den
    f_im = (num_im * lr - num_re * li) / den
    br, bi = b_re.astype(jnp.float32), b_im.astype(jnp.float32)
    bb_re = f_re[..., None] * br - f_im[..., None] * bi
    bb_im = f_re[..., None] * bi + f_im[..., None] * br
    return ab_re, ab_im, bb_re, bb_im


def complex_affine_combine(e1, e2):
    a1r, a1i, b1r, b1i = e1
    a2r, a2i, b2r, b2i = e2
    return (a2r * a1r - a2i * a1i, a2r * a1i + a2i * a1r,
            a2r * b1r - a2i * b1i + b2r, a2r * b1i + a2i * b1r + b2i)


def s5_scan(u, ab_re, ab_im, bb_re, bb_im, h0_re, h0_im, reverse):
    bu_re = jnp.einsum('tgc,gpc->tgp', u, bb_re)
    bu_im = jnp.einsum('tgc,gpc->tgp', u, bb_im)
    a_re = jnp.broadcast_to(ab_re, bu_re.shape)
    a_im = jnp.broadcast_to(ab_im, bu_re.shape)
    pw_re, pw_im, s_re, s_im = lax.associative_scan(complex_affine_combine, (a_re, a_im, bu_re, bu_im),
                                                    reverse=reverse, axis=0)
    s_re = s_re + pw_re * h0_re - pw_im * h0_im
    s_im = s_im + pw_re * h0_im + pw_im * h0_re
    return s_re, s_im


def s5_readout(s_re, s_im, c_re, c_im):
    return (jnp.einsum('btgp,gcp->btgc', s_re, c_re.astype(jnp.float32))
            - jnp.einsum('btgp,gcp->btgc', s_im, c_im.astype(jnp.float32)))


def s5_mixer(ul, uc, lam_re, lam_im, log_step, b_re, b_im, c_re, c_im, d_skip, w_glu, b_glu, need_ctx):
    bsz, seq, _ = ul.shape
    n_ctx = uc.shape[1]
    ulg = ul.astype(jnp.float32).reshape(bsz, seq, S5_GROUPS, S5_GROUP)
    ucg = uc.astype(jnp.float32).reshape(bsz, n_ctx, S5_GROUPS, S5_GROUP)
    zeros = jnp.zeros((bsz, S5_GROUPS, S5_STATE), jnp.float32)
    y_lat = ul.astype(jnp.float32) * d_skip
    y_ctx = uc.astype(jnp.float32) * d_skip
    for direction, reverse in ((0, False), (1, True)):
        ab_re, ab_im, bb_re, bb_im = s5_discretize(lam_re[direction], lam_im[direction], log_step[direction],
                                                   b_re[direction], b_im[direction])
        scan = jax.vmap(functools.partial(s5_scan, reverse=reverse), in_axes=(0, None, None, None, None, 0, 0))
        sc_re, sc_im = scan(ucg, ab_re, ab_im, bb_re, bb_im, zeros, zeros)
        end = 0 if reverse else -1
        sl_re, sl_im = scan(ulg, ab_re, ab_im, bb_re, bb_im, sc_re[:, end], sc_im[:, end])
        y_lat = y_lat + s5_readout(sl_re, sl_im, c_re[direction], c_im[direction]).reshape(bsz, seq, S5_WIDTH)
        if need_ctx:
            y_ctx = y_ctx + s5_readout(sc_re, sc_im, c_re[direction], c_im[direction]).reshape(bsz, n_ctx, S5_WIDTH)

    def glu(y):
        z = jax.nn.gelu(y)
        return z * jax.nn.sigmoid(jnp.dot(z, w_glu) + b_glu)

    out_ctx = glu(y_ctx).astype(uc.dtype) if need_ctx else None
    return glu(y_lat).astype(ul.dtype), out_ctx


def mixer_ab(hl, hc, cos, sin, w_in, w_out, lam_re, lam_im, log_step, b_re, b_im, c_re, c_im,
             d_skip, w_glu, b_glu, sink, need_ctx):
    grp = WIN_HEADS // WIN_KV_HEADS
    cuts = [S5_WIDTH, S5_WIDTH + WIN_HEADS * HEAD_DIM, S5_WIDTH + (WIN_HEADS + WIN_KV_HEADS) * HEAD_DIM]
    ul, ql, kl, vl = jnp.split(jnp.dot(hl, w_in), cuts, axis=-1)
    uc, qc, kc, vc = jnp.split(jnp.dot(hc, w_in), cuts, axis=-1)
    a_lat, a_ctx = s5_mixer(ul, uc, lam_re, lam_im, log_step, b_re, b_im, c_re, c_im,
                            d_skip, w_glu, b_glu, need_ctx)
    q = apply_rope(to_q_heads(ql, WIN_KV_HEADS, grp), cos, sin)
    k = apply_rope(to_kv_heads(kl, WIN_KV_HEADS), cos, sin)
    v = to_kv_heads(vl, WIN_KV_HEADS)
    kch = to_kv_heads(kc, WIN_KV_HEADS)
    vch = to_kv_heads(vc, WIN_KV_HEADS)
    o_lat = merge_heads(window_attention(q, k, v, kch, vch, sink))
    out_lat = jnp.dot(jnp.concatenate([a_lat, o_lat], -1), w_out)
    out_ctx = None
    if need_ctx:
        o_ctx = merge_heads(full_attention(to_q_heads(qc, WIN_KV_HEADS, grp), kch, vch, sink))
        out_ctx = jnp.dot(jnp.concatenate([a_ctx, o_ctx], -1), w_out)
    return out_lat, out_ctx


def mixer_c(hl, hc, cos, sin, w_in, w_out, q_norm, k_norm, need_ctx):
    grp = C_HEADS // C_KV_HEADS
    cuts = [C_HEADS * HEAD_DIM, (C_HEADS + C_KV_HEADS) * HEAD_DIM]

    def project(h):
        q, k, v = jnp.split(jnp.dot(h, w_in), cuts, axis=-1)
        return (rms_norm(to_q_heads(q, C_KV_HEADS, grp), q_norm),
                rms_norm(to_kv_heads(k, C_KV_HEADS), k_norm),
                to_kv_heads(v, C_KV_HEADS))

    ql, kl, vl = project(hl)
    qc, kc, vc = project(hc)
    ql = apply_rope(ql, cos, sin)
    kl = apply_rope(kl, cos, sin)
    k_all = jnp.concatenate([kc, kl], axis=2)
    v_all = jnp.concatenate([vc, vl], axis=2)
    out_lat = jnp.dot(merge_heads(blocked_attention(ql, k_all, v_all)), w_out)
    out_ctx = jnp.dot(merge_heads(full_attention(qc, kc, vc)), w_out) if need_ctx else None
    return out_lat, out_ctx


def moe_ffn(t, router_w, router_bias, w_gate, w_up, w_down):
    n_tok = t.shape[0]
    scores = jax.nn.sigmoid(jnp.dot(t, router_w, preferred_element_type=jnp.float32))
    sel = (scores + router_bias.astype(jnp.float32)).reshape(n_tok, N_GROUPS, EXPERTS_PER_GROUP)
    grp_score = lax.top_k(sel, TOP_K)[0].sum(-1)
    g_idx = jnp.argmax(grp_score, axis=-1)
    in_grp = sel[jnp.arange(n_tok), g_idx]
    _, e_local = lax.top_k(in_grp, TOP_K)
    e_idx = g_idx[:, None] * EXPERTS_PER_GROUP + e_local
    w = jnp.take_along_axis(scores, e_idx, axis=1)
    w = w / jnp.sum(w, -1, keepdims=True)
    combine = jnp.einsum('nk,nke->ne', w, jax.nn.one_hot(e_idx, N_EXPERTS, dtype=jnp.float32))
    out = jnp.zeros(t.shape, jnp.float32)
    for e in range(N_EXPERTS):
        hid = jax.nn.silu(jnp.dot(t, w_gate[e])) * jnp.dot(t, w_up[e])
        out = out + combine[:, e:e + 1] * jnp.dot(hid, w_down[e], preferred_element_type=jnp.float32)
    return out.astype(t.dtype)


def setup_inputs(seed: int = 0) -> dict:
    key = jax.random.key(seed)
    ks = jax.random.split(key, 32)
    f32 = jnp.float32
    D = D_MODEL

    def nrm(k, shape, s):
        return jax.random.normal(k, shape, f32) * s

    s5_shape = (N_EVEN, 2, S5_GROUPS, S5_STATE)
    n_idx = jnp.arange(S5_STATE, dtype=f32)
    return {
        'x': nrm(ks[0], (BATCH, SEQ, D), 1.0),
        'c': nrm(ks[1], (BATCH, D), 1.0),
        'ctx': nrm(ks[2], (BATCH, CTX_LEN, D), 1.0),
        'c_ctx': nrm(ks[3], (D,), 1.0),
        'ada_w': nrm(ks[4], (DEPTH, D, 6 * D), 0.5 * D ** -0.5),
        'ada_b': nrm(ks[5], (DEPTH, 6 * D), 0.02),
        'ln_g': 1.0 + nrm(ks[6], (DEPTH, 2, D), 0.02),
        'ln_b': nrm(ks[7], (DEPTH, 2, D), 0.02),
        'even_w_in': nrm(ks[8], (N_EVEN, D, EVEN_IN), D ** -0.5),
        'even_w_out': nrm(ks[9], (N_EVEN, EVEN_MIX, D), BETA * EVEN_MIX ** -0.5),
        's5_lam_re': -0.5 + nrm(ks[10], s5_shape, 0.01),
        's5_lam_im': math.pi * n_idx + nrm(ks[11], s5_shape, 0.01),
        's5_log_step': jax.random.uniform(ks[12], (N_EVEN, 2, S5_GROUPS), f32, math.log(1e-3), math.log(1e-1)),
        's5_b_re': nrm(ks[13], (N_EVEN, 2, S5_GROUPS, S5_STATE, S5_GROUP), (2 * S5_GROUP) ** -0.5),
        's5_b_im': nrm(ks[14], (N_EVEN, 2, S5_GROUPS, S5_STATE, S5_GROUP), (2 * S5_GROUP) ** -0.5),
        's5_c_re': nrm(ks[15], (N_EVEN, 2, S5_GROUPS, S5_GROUP, S5_STATE), 0.5),
        's5_c_im': nrm(ks[16], (N_EVEN, 2, S5_GROUPS, S5_GROUP, S5_STATE), 0.5),
        's5_d': nrm(ks[17], (N_EVEN, S5_WIDTH), 1.0),
        's5_w_glu': nrm(ks[18], (N_EVEN, S5_WIDTH, S5_WIDTH), S5_WIDTH ** -0.5),
        's5_b_glu': nrm(ks[19], (N_EVEN, S5_WIDTH), 0.02),
        'win_sink': nrm(ks[20], (N_EVEN, WIN_HEADS), 0.5),
        'odd_w_in': nrm(ks[21], (N_ODD, D, ODD_IN), D ** -0.5),
        'odd_w_out': nrm(ks[22], (N_ODD, ODD_MIX, D), BETA * ODD_MIX ** -0.5),
        'odd_q_norm': 1.0 + nrm(ks[23], (N_ODD, HEAD_DIM), 0.02),
        'odd_k_norm': 1.0 + nrm(ks[24], (N_ODD, HEAD_DIM), 0.02),
        'router_w': nrm(ks[25], (D, N_EXPERTS), D ** -0.5),
        'router_bias': nrm(ks[26], (N_EXPERTS,), 0.01),
        'moe_w_gate': nrm(ks[27], (DEPTH, N_EXPERTS, D, EXPERT_FF), D ** -0.5),
        'moe_w_up': nrm(ks[28], (DEPTH, N_EXPERTS, D, EXPERT_FF), D ** -0.5),
        'moe_w_down': nrm(ks[29], (DEPTH, N_EXPERTS, EXPERT_FF, D), BETA * EXPERT_FF ** -0.5),
    }


def reference(x, c, ctx, c_ctx, ada_w, ada_b, ln_g, ln_b, even_w_in, even_w_out,
              s5_lam_re, s5_lam_im, s5_log_step, s5_b_re, s5_b_im, s5_c_re, s5_c_im, s5_d,
              s5_w_glu, s5_b_glu, win_sink, odd_w_in, odd_w_out, odd_q_norm, odd_k_norm,
              router_w, router_bias, moe_w_gate, moe_w_up, moe_w_down):
    bsz, seq, dm = x.shape
    rows = seq // GRID_W
    cos, sin = axial_rope_tables(rows)
    silu_c = jax.nn.silu(c)
    silu_cc = jax.nn.silu(c_ctx)
    n_lat = bsz * seq
    xl, xc = x, ctx
    for i in range(DEPTH):
        need_ctx = i < DEPTH - 1
        j = i // 2
        mod_l = jnp.split((jnp.dot(silu_c, ada_w[i]) + ada_b[i])[:, None, :], 6, axis=-1)
        mod_c = jnp.split(jnp.dot(silu_cc, ada_w[i]) + ada_b[i], 6, axis=-1)
        hl = xl * (1.0 + mod_l[1]) + mod_l[0]
        hc = xc * (1.0 + mod_c[1]) + mod_c[0]
        if i % 2 == 0:
            ol, oc = mixer_ab(hl, hc, cos, sin, even_w_in[j], even_w_out[j], s5_lam_re[j], s5_lam_im[j],
                              s5_log_step[j], s5_b_re[j], s5_b_im[j], s5_c_re[j], s5_c_im[j], s5_d[j],
                              s5_w_glu[j], s5_b_glu[j], win_sink[j], need_ctx)
        else:
            ol, oc = mixer_c(hl, hc, cos, sin, odd_w_in[j], odd_w_out[j], odd_q_norm[j], odd_k_norm[j],
                             need_ctx)
        xl = layer_norm(ALPHA * xl + mod_l[2] * ol, ln_g[i, 0], ln_b[i, 0])
        hl = xl * (1.0 + mod_l[4]) + mod_l[3]
        if need_ctx:
            xc = layer_norm(ALPHA * xc + mod_c[2] * oc, ln_g[i, 0], ln_b[i, 0])
            hc = xc * (1.0 + mod_c[4]) + mod_c[3]
            tokens = jnp.concatenate([hl.reshape(-1, dm), hc.reshape(-1, dm)], axis=0)
        else:
            tokens = hl.reshape(-1, dm)
        f = moe_ffn(tokens, router_w, router_bias, moe_w_gate[i], moe_w_up[i], moe_w_down[i])
        xl = layer_norm(ALPHA * xl + mod_l[5] * f[:n_lat].reshape(bsz, seq, dm), ln_g[i, 1], ln_b[i, 1])
        if need_ctx:
            xc = layer_norm(ALPHA * xc + mod_c[5] * f[n_lat:].reshape(xc.shape), ln_g[i, 1], ln_b[i, 1])
    return xl
```

```python
import math
import os
DBG_SKIP = os.environ.get('DBG_SKIP', '').split(',')
DBG_NT = int(os.environ.get('DBG_NT', '34'))
from contextlib import ExitStack
import numpy as np
import ml_dtypes
import concourse.bass as bass
import concourse.mybir as mybir
from concourse.bass_utils import run_bass_kernel_spmd

F32 = mybir.dt.float32
BF16 = mybir.dt.bfloat16
I32 = mybir.dt.int32
ALU = mybir.AluOpType
AF = mybir.ActivationFunctionType
AX = mybir.AxisListType

SEM_LIMIT = 30000
NT = 34
NTOK = 4352
D = 1024
ALPHA = 4.0 ** 0.25
LN_EPS = 1e-5
RMS_EPS = 1e-6
TWO_PI = 2.0 * math.pi
CW1 = 6.28125
CW2 = TWO_PI - CW1


class Res:
    __slots__ = ("name", "w", "r", "x")

    def __init__(self, name="", x=False):
        self.name = name
        self.w = None
        self.r = []
        self.x = x


def RL(n, name="r"):
    return [Res("%s%d" % (name, i)) for i in range(n)]


class EngState:
    def __init__(self, fw, name, eng):
        self.fw = fw
        self.name = name
        self.eng = eng
        self.count = 0
        self.epoch = 0
        self.known = {}
        self._new_sem()

    def _new_sem(self):
        self.sem_key = "%s_e%d" % (self.name, self.epoch)
        self.sem = self.fw.new_sem(self.sem_key)
        self.count = 0
        self.epoch += 1


class FW:
    def __init__(self, nc, n_dma_sems=10):
        self.nc = nc
        self.es = ExitStack()
        self.sems = {}
        self.engs = {}
        for name, eng in (("pe", nc.tensor), ("act", nc.scalar), ("dve", nc.vector),
                          ("pool", nc.gpsimd), ("sp", nc.sync)):
            self.engs[name] = EngState(self, name, eng)
        self.dma_pool = {}
        for q in ("sp", "act", "pool"):
            lst = []
            for i in range(n_dma_sems):
                key = "dma_%s_%d" % (q, i)
                lst.append([key, self.new_sem(key), 0])
            self.dma_pool[q] = [lst, 0]
        self.n_instr = 0
        self.n_waits = 0

    def new_sem(self, key):
        s = self.es.enter_context(self.nc.semaphore(key))
        self.sems[key] = s
        return s

    def _wait(self, E, ev):
        if ev is None:
            return
        key, val = ev
        if E.known.get(key, 0) >= val:
            return
        E.eng.wait_ge(self.sems[key], val)
        E.known[key] = val
        self.n_waits += 1

    def _deps(self, E, reads, writes, acc=False):
        for r in reads:
            self._wait(E, r.w)
            if r.x:
                for ev in r.r:
                    if ev[0] != E.sem_key:
                        self._wait(E, ev)
        for w in writes:
            if not ((acc or E.name == "pe") and w.w is not None and w.w[0] == E.sem_key):
                self._wait(E, w.w)
            for ev in w.r:
                self._wait(E, ev)

    def _commit(self, ev, reads, writes):
        for r in reads:
            r.r.append(ev)
            if len(r.r) > 16:
                d = {}
                for k, v in r.r:
                    if d.get(k, 0) < v:
                        d[k] = v
                r.r = list(d.items())
        for w in writes:
            w.w = ev
            w.r = []

    def op(self, ename, fn, reads=(), writes=(), acc=False):
        E = self.engs[ename]
        if E.count >= SEM_LIMIT:
            E._new_sem()
        self._deps(E, reads, writes, acc=acc)
        ins = fn(E.eng)
        E.count += 1
        ins.then_inc(E.sem, 1)
        self._commit((E.sem_key, E.count), reads, writes)
        self.n_instr += 1
        return ins

    def _dma_common(self, qname, issue, reads, writes):
        E = self.engs[qname]
        pool, idx = self.dma_pool[qname]
        ent = pool[idx % len(pool)]
        self.dma_pool[qname][1] = idx + 1
        key, sem, val = ent
        if val > 0:
            self._wait(E, (key, val))
        if val + 16 > SEM_LIMIT:
            key = key + "n"
            sem = self.new_sem(key)
            val = 0
            ent[0], ent[1] = key, sem
        self._deps(E, reads, writes)
        ins = issue(E.eng)
        val += 16
        ent[2] = val
        ins.then_inc(sem, 16)
        ev = (key, val)
        self._commit(ev, reads, writes)
        self.n_instr += 1
        return ev

    def dma(self, qname, out, in_, reads=(), writes=(), **kw):
        return self._dma_common(qname, lambda e: e.dma_start(out=out, in_=in_, **kw), reads, writes)

    def barrier(self):
        evs = []
        for q in self.dma_pool:
            for key, sem, val in self.dma_pool[q][0]:
                if val > 0:
                    evs.append((key, val))
        for n, e in self.engs.items():
            if e.count > 0:
                evs.append((e.sem_key, e.count))
        for n, E in self.engs.items():
            for ev in evs:
                if ev[0] != E.sem_key:
                    self._wait(E, ev)

    def finish(self):
        E = self.engs["sp"]
        for q in self.dma_pool:
            for key, sem, val in self.dma_pool[q][0]:
                if val > 0:
                    self._wait(E, (key, val))
        for n, e in self.engs.items():
            if e.count > 0:
                self._wait(E, (e.sem_key, e.count))

    def close(self):
        self.es.close()


class Scope:
    FWREF = None

    def __init__(self, nc):
        self.nc = nc
        self.es = ExitStack()

    CNT = [0]

    def sb(self, name, shape, dtype=F32):
        Scope.CNT[0] += 1
        return self.es.enter_context(self.nc.sbuf_tensor("%s_%d" % (name, Scope.CNT[0]), list(shape), dtype))

    def ps(self, name, shape, dtype=F32):
        Scope.CNT[0] += 1
        return self.es.enter_context(self.nc.psum_tensor("%s_%d" % (name, Scope.CNT[0]), list(shape), dtype))

    def close(self):
        if Scope.FWREF is not None:
            Scope.FWREF.barrier()
        self.es.close()


def rev_ap(ap2d, n):
    last = ap2d[:, n - 1:n]
    return bass.AP(tensor=ap2d.tensor, offset=last.offset, ap=[list(ap2d.ap[0]), [-1, n]])


def build_program(stop_after=None, dbg_shape=None):
    nc = bass.Bass("TRN2", target_bir_lowering=False)

    def din(name, shape, dt=F32):
        return nc.dram_tensor(name, list(shape), dt, kind="ExternalInput").ap()

    x_d = din("x", [4096, D]); ctx_d = din("ctx", [256, D])
    c_d = din("c", [1, D]); cctx_d = din("c_ctx", [1, D])
    ada_w = din("ada_w", [2, D, 6 * D]); ada_b = din("ada_b", [2, 6 * D])
    ln_g = din("ln_g", [2, 2, D]); ln_b = din("ln_b", [2, 2, D])
    even_w_in = din("even_w_in", [D, 1280]); even_w_out = din("even_w_out", [D, D])
    lam_re = din("s5_lam_re", [2, 32, 64]); lam_im = din("s5_lam_im", [2, 32, 64])
    log_step = din("s5_log_step", [2, 32])
    b_re = din("s5_b_re", [2, 32, 64, 16]); b_im = din("s5_b_im", [2, 32, 64, 16])
    c_re = din("s5_c_re", [2, 32, 16, 64]); c_im = din("s5_c_im", [2, 32, 16, 64])
    s5_d = din("s5_d", [512]); w_glu = din("s5_w_glu", [512, 512]); b_glu = din("s5_b_glu", [512])
    win_sink = din("win_sink", [8])
    odd_w_in = din("odd_w_in", [D, 1536]); odd_w_out = din("odd_w_out", [D, D])
    q_norm = din("odd_q_norm", [64]); k_norm = din("odd_k_norm", [64])
    router_w = din("router_w", [D, 32]); router_b = din("router_bias", [32])
    w_gate = din("moe_w_gate", [2, 32, D, 512]); w_up = din("moe_w_up", [2, 32, D, 512])
    w_down = din("moe_w_down", [2, 32, 512, D])
    k_ident = din("k_ident", [128, 128]); k_rope = din("k_rope", [128, 2, 32, 32])
    k_mask = din("k_mask", [128, 3, 128]); k_jidx = din("k_jidx", [128, 128]); k_pc = din("k_pc", [128, 4])
    out_d = nc.dram_tensor("out", [4096, D], F32, kind="ExternalOutput").ap()
    XR = nc.dram_tensor("xr", [NTOK, D], F32, kind="Internal").ap()
    QT = nc.dram_tensor("qt_scr", [8, 128, 4096], BF16, kind="Internal").ap()
    OT = nc.dram_tensor("ot_scr", [8, 128, 4096], BF16, kind="Internal").ap()
    ATD = nc.dram_tensor("at_scr", [4, 128, NTOK], BF16, kind="Internal").ap()
    NS = 49
    XS = nc.dram_tensor("xs_scr", [NS * 512, D], BF16, kind="Internal").ap()
    YS = nc.dram_tensor("ys_scr", [NS * 512, D], F32, kind="Internal").ap()
    wg_all = w_gate.rearrange("l e (kk two) n -> (l e kk) (two n)", two=2)
    wu_all = w_up.rearrange("l e (kk two) n -> (l e kk) (two n)", two=2)
    wd_all = w_down.rearrange("l e f n -> (l e f) n")
    wg_rows = [wg_all, wg_all]; wu_rows = [wu_all, wu_all]; wd_rows = [wd_all, wd_all]
    dbg = None
    if dbg_shape is not None:
        dbg = nc.dram_tensor("dbg", list(dbg_shape), F32, kind="ExternalOutput").ap()

    f = FW(nc)
    Scope.FWREF = f
    G = Scope(nc)
    R_XR = RL(NT, "xr")
    R_out = Res("out")
    R_dbg = Res("dbg")

    ident = G.sb("ident", [128, 128]); R_ident = Res()
    identb = G.sb("identb", [128, 128], BF16); R_identb = Res()
    f.dma("sp", ident[:], k_ident, writes=[R_ident])
    f.op("dve", lambda e: e.tensor_copy(identb[:], ident[:]), reads=[R_ident], writes=[R_identb])
    rope = G.sb("rope", [128, 2, 32, 32]); R_rope = Res()
    f.dma("sp", rope[:], k_rope, writes=[R_rope])
    maskf = G.sb("maskf", [128, 3, 128]); maskb = G.sb("maskb", [128, 3, 128], BF16); R_mask = Res()
    f.dma("sp", maskf[:], k_mask, writes=[R_mask])
    f.op("dve", lambda e: e.tensor_copy(maskb[:], maskf[:]), reads=[R_mask], writes=[R_mask])
    R_crep = Res()
    ctmp = G.sb("ctmp", [128, 2, 8]); R_ctmp = Res()
    f.dma("sp", ctmp[:, 0, :], c_d.rearrange("o (kc p) -> p (o kc)", p=128), writes=[R_ctmp], allow_slow_non_contiguous=True)
    f.dma("sp", ctmp[:, 1, :], cctx_d.rearrange("o (kc p) -> p (o kc)", p=128), writes=[R_ctmp], allow_slow_non_contiguous=True)
    f.op("act", lambda e: e.activation(out=ctmp[:], in_=ctmp[:], func=AF.Silu), reads=[R_ctmp], writes=[R_ctmp])
    lng = G.sb("lng", [128, D]); lnb = G.sb("lnb", [128, D]); R_ln = Res()

    def load_ln(li):
        f.dma("sp", lng[:], ln_g[li // 2, li % 2].partition_broadcast(128), writes=[R_ln])
        f.dma("sp", lnb[:], ln_b[li // 2, li % 2].partition_broadcast(128), writes=[R_ln])
    epsc = G.sb("epsc", [128, 1]); R_eps = Res()
    f.op("dve", lambda e: e.memset(epsc[:], LN_EPS), writes=[R_eps])

    R_XsZ = RL(28, "xsz")
    ZS = Scope(nc)
    zt = ZS.sb("zt", [128, 7, D], BF16); R_zt = Res()
    f.op("pool", lambda e: e.memset(zt[:], 0.0), writes=[R_zt])
    for k in range(28):
        f.dma(("sp", "act")[k % 2], XS[k * 896:(k + 1) * 896, :].rearrange("(a p) d -> p a d", p=128), zt[:], reads=[R_zt], writes=[R_XsZ[k]])
    ZS.close()

    mod = G.sb("mod", [128, 2, 3, D]); R_mod = Res("mod")

    def dump(ap_sb, rows, cols, reads, r0=0, c0=0):
        f.dma("sp", dbg[r0:r0 + rows, c0:c0 + cols], ap_sb, reads=reads, writes=[R_dbg])

    def phase_mod(i, s):
        S = Scope(nc)
        crep = S.sb("crep", [128, 2, 8, 128])
        f.op("dve", lambda e: e.tensor_copy(crep[:], ctmp[:].unsqueeze(3).broadcast_to([128, 2, 8, 128])), reads=[R_ctmp], writes=[R_crep])
        slab = [S.sb("slab%d" % k, [128, 8, 512]) for k in range(2)]; R_slab = RL(2)
        adb = [S.sb("adb%d" % k, [128, 512]) for k in range(2)]; R_adb = RL(2)
        psm = [S.ps("psm%d" % k, [128, 512]) for k in range(2)]; R_psm = RL(2)
        n = 0
        for blk in range(6):
            c0 = s * 3072 + blk * 512
            bi = blk % 2
            f.dma("sp", slab[bi][:], ada_w[i, :, c0:c0 + 512].rearrange("(kc p) n -> p kc n", p=128), writes=[R_slab[bi]])
            f.dma("act", adb[bi][:], ada_b[i, c0:c0 + 512].partition_broadcast(128), writes=[R_adb[bi]])
            k, half = blk // 2, blk % 2
            for which in range(2):
                pi = n % 2; n += 1
                for kc in range(8):
                    f.op("pe", lambda e, kc=kc: e.matmul(psm[pi][:], crep[:, which, kc, :], slab[bi][:, kc, :], start=(kc == 0), stop=(kc == 7)),
                         reads=[R_crep, R_slab[bi]], writes=[R_psm[pi]], acc=(kc > 0))
                dst = mod[:, which, k, half * 512:(half + 1) * 512]
                f.op("dve", lambda e: e.scalar_tensor_tensor(out=dst, in0=psm[pi][:], scalar=(1.0 if k == 1 else 0.0), in1=adb[bi][:], op0=ALU.add, op1=ALU.add),
                     reads=[R_psm[pi], R_adb[bi]], writes=[R_mod])
        S.close()

    def resid_ln(S, xt, R_xt, o_ps, R_ops, which, li, out_t, R_outt, tmp, R_tmp, small, R_small):
        gate = mod[:, which, 2, :]
        f.op("dve", lambda e: e.tensor_tensor(tmp[:], o_ps[:], gate, ALU.mult), reads=[R_ops, R_mod], writes=[R_tmp])
        f.op("dve", lambda e: e.scalar_tensor_tensor(out=tmp[:], in0=xt[:], scalar=ALPHA, in1=tmp[:], op0=ALU.mult, op1=ALU.add),
             reads=[R_xt, R_tmp], writes=[R_tmp])
        f.op("dve", lambda e: e.bn_stats(small[:, 0:6], tmp[:, 0:512]), reads=[R_tmp], writes=[R_small])
        f.op("dve", lambda e: e.bn_stats(small[:, 6:12], tmp[:, 512:1024]), reads=[R_tmp], writes=[R_small])
        f.op("dve", lambda e: e.bn_aggr(small[:, 12:14], small[:, 0:12]), reads=[R_small], writes=[R_small])
        f.op("act", lambda e: e.activation(out=small[:, 14:15], in_=small[:, 13:14], func=AF.Sqrt, bias=epsc[:], scale=1.0), reads=[R_small, R_eps], writes=[R_small])
        f.op("dve", lambda e: e.reciprocal(small[:, 15:16], small[:, 14:15]), reads=[R_small], writes=[R_small])
        f.op("dve", lambda e: e.tensor_scalar(out=tmp[:], in0=tmp[:], scalar1=small[:, 12:13], scalar2=small[:, 15:16], op0=ALU.subtract, op1=ALU.mult),
             reads=[R_tmp, R_small], writes=[R_tmp])
        f.op("dve", lambda e: e.tensor_tensor(tmp[:], tmp[:], lng[:], ALU.mult), reads=[R_tmp, R_ln], writes=[R_tmp])
        f.op("dve", lambda e: e.tensor_tensor(out_t[:], tmp[:], lnb[:], ALU.add), reads=[R_tmp, R_ln], writes=[R_outt])

    def mod_transpose(xt, R_xt, which, h32, R_h32, ps_tp, R_pstp, hT_dst, R_hT, h32T=None, R_h32T=None):
        f.op("dve", lambda e: e.tensor_tensor(h32[:], xt[:], mod[:, which, 1, :], ALU.mult), reads=[R_xt, R_mod], writes=[R_h32])
        f.op("dve", lambda e: e.tensor_tensor(h32[:], h32[:], mod[:, which, 0, :], ALU.add), reads=[R_h32, R_mod], writes=[R_h32])
        for kc in range(8):
            f.op("pe", lambda e, kc=kc: e.transpose(ps_tp[:, kc, :], h32[:, kc * 128:(kc + 1) * 128], ident[:]),
                 reads=[R_h32, R_ident], writes=[R_pstp], acc=(kc > 0))
        f.op("act", lambda e: e.activation(out=hT_dst, in_=ps_tp[:], func=AF.Identity), reads=[R_pstp], writes=[R_hT])
        if h32T is not None:
            f.op("dve", lambda e: e.tensor_copy(h32T[:], ps_tp[:]), reads=[R_pstp], writes=[R_h32T])

    def src_tile(layer, t):
        if layer == 0:
            return (ctx_d[t * 128:(t + 1) * 128, :] if t < 2 else x_d[(t - 2) * 128:(t - 1) * 128, :]), []
        return XR[t * 128:(t + 1) * 128, :], [R_XR[t]]

    def layer0_mixer():
        L = Scope(nc)
        U = Scope(nc)
        uT = U.sb("uT", [128, 4, NTOK], BF16); R_uT = RL(NT, "uT")
        aT, R_aT = uT, R_uT

        def inproj(do_u, qT=None, R_qT=None, kT2=None, R_kT=None, vaug=None, R_v=None):
            S = Scope(nc)
            wc0, wc1 = (0, 512) if do_u else (512, 1280)
            win = S.sb("win", [128, 8, wc1 - wc0], BF16); R_win = Res()
            f.dma("pool", win[:], even_w_in[:, wc0:wc1].rearrange("(kc p) n -> p kc n", p=128), writes=[R_win])
            xt1 = S.sb("xt1", [128, D]); xt = [xt1, xt1]; R1_ = Res(); R_xt = [R1_, R1_]
            h32 = S.sb("h32", [128, D]); R_h32 = Res()
            hT = [S.sb("hT%d" % k, [128, 8, 128], BF16) for k in range(2)]; R_hT = RL(2)
            ps_tp = S.ps("ps_tp", [128, 8, 128]); R_pstp = Res(x=True)
            ps_u = S.ps("ps_u", [128, 4, 128]); R_psu = Res()
            ps_q = S.ps("ps_q", [128, 1024]); R_psq = Res(x=True)
            ps_t = S.ps("ps_t", [128, 8, 128], BF16); R_pst = Res(x=True)
            ra = S.sb("ra", [128, 10, 32]); rb = S.sb("rb", [128, 10, 32]); R_ra = Res(); R_rb = Res()
            tqk = S.sb("tqk", [128, 640], BF16); R_tqk = Res()
            kd = S.sb("kd", [128, 2, 2, 64], BF16); R_kd = Res()
            for t in range(NT if do_u else min(NT, DBG_NT)):
                b = t % 2
                src, rs = src_tile(0, t)
                f.dma("sp", xt[b][:], src, reads=rs, writes=[R_xt[b]])
                which = 1 if t < 2 else 0
                mod_transpose(xt[b], R_xt[b], which, h32, R_h32, ps_tp, R_pstp, hT[b][:], R_hT[b])
                cols = slice(t * 128, (t + 1) * 128)
                if stop_after == "h0" and t == 0:
                    f.dma("sp", dbg[0:128, :], h32[:], reads=[R_h32], writes=[R_dbg])
                    hf = S.sb("hf", [128, 1024]); R_hf = Res()
                    f.op("dve", lambda e: e.tensor_copy(hf[:], hT[b][:].rearrange("p a b -> p (a b)")), reads=[R_hT[b]], writes=[R_hf])
                    f.dma("sp", dbg[128:256, :], hf[:], reads=[R_hf], writes=[R_dbg])
                    f.op("dve", lambda e: e.tensor_copy(hf[:], win[:, 0, 0:1024]), reads=[R_win], writes=[R_hf])
                    f.dma("sp", dbg[256:384, :], hf[:], reads=[R_hf], writes=[R_dbg])
                    S.close(); return
                if do_u:
                    for ct in range(4):
                        for kc in range(8):
                            f.op("pe", lambda e, ct=ct, kc=kc: e.matmul(ps_u[:, ct, :], win[:, kc, ct * 128:(ct + 1) * 128], hT[b][:, kc, :], start=(kc == 0), stop=(kc == 7)),
                                 reads=[R_win, R_hT[b]], writes=[R_psu], acc=(ct + kc > 0))
                    f.op("act", lambda e: e.activation(out=uT[:, :, cols], in_=ps_u[:], func=AF.Identity), reads=[R_psu], writes=[R_uT[t]])
                    continue
                for (n0, n1) in ((0, 512), (512, 768)):
                    for kc in range(8):
                        f.op("pe", lambda e, kc=kc, n0=n0, n1=n1: e.matmul(ps_q[:, n0:n1], hT[b][:, kc, :], win[:, kc, n0:n1], start=(kc == 0), stop=(kc == 7)),
                             reads=[R_win, R_hT[b]], writes=[R_psq], acc=(n0 + kc > 0))
                if 'rope' in DBG_SKIP:
                    continue
                if t >= 2:
                    pv = ps_q[:, 0:640].rearrange("p (h two f) -> p h two f", two=2, f=32)
                    ov = tqk[:].rearrange("p (h two f) -> p h two f", two=2, f=32)
                    cosb = rope[:, 0, t - 2, :].unsqueeze(1).broadcast_to([128, 10, 32])
                    sinb = rope[:, 1, t - 2, :].unsqueeze(1).broadcast_to([128, 10, 32])
                    f.op("dve", lambda e: e.tensor_tensor(ra[:], pv[:, :, 0, :], cosb, ALU.mult), reads=[R_psq, R_rope], writes=[R_ra])
                    f.op("dve", lambda e: e.tensor_tensor(rb[:], pv[:, :, 1, :], sinb, ALU.mult), reads=[R_psq, R_rope], writes=[R_rb])
                    f.op("pool", lambda e: e.tensor_tensor(ov[:, :, 0, :], ra[:], rb[:], ALU.subtract), reads=[R_ra, R_rb], writes=[R_tqk])
                    f.op("dve", lambda e: e.tensor_tensor(ra[:], pv[:, :, 1, :], cosb, ALU.mult), reads=[R_psq, R_rope, R_tqk], writes=[R_ra])
                    f.op("dve", lambda e: e.tensor_tensor(rb[:], pv[:, :, 0, :], sinb, ALU.mult), reads=[R_psq, R_rope, R_tqk], writes=[R_rb])
                    f.op("pool", lambda e: e.tensor_tensor(ov[:, :, 1, :], ra[:], rb[:], ALU.add), reads=[R_ra, R_rb], writes=[R_tqk])
                else:
                    f.op("act", lambda e: e.activation(out=tqk[:], in_=ps_q[:, 0:640], func=AF.Identity), reads=[R_psq], writes=[R_tqk])
                if 'vaug' in DBG_SKIP:
                    continue
                for a in range(2):
                    if 'novaug' in DBG_SKIP:
                        break
                    f.op("dve", lambda e, a=a: e.tensor_copy(vaug[:, t, 64 + 128 * a:128 + 128 * a], ps_q[:, 640 + 64 * a:704 + 64 * a]),
                         reads=[R_psq], writes=[R_v[t]])
                if 'nokd' in DBG_SKIP:
                    continue
                kv = tqk[:, 512:640].rearrange("p (a d) -> p a d", a=2)
                f.op("dve", lambda e: e.tensor_copy(kd[:, :, 0, :], kv), reads=[R_tqk], writes=[R_kd])
                f.op("dve", lambda e: e.tensor_copy(kd[:, :, 1, :], kv), reads=[R_tqk], writes=[R_kd])
                if 'tr' in DBG_SKIP:
                    continue
                for pr in range(4):
                    f.op("pe", lambda e, pr=pr: e.transpose(ps_t[:, pr, :], tqk[:, pr * 128:(pr + 1) * 128], identb[:]),
                         reads=[R_tqk, R_identb], writes=[R_pst], acc=(pr > 0))
                for a in range(2):
                    f.op("pe", lambda e, a=a: e.transpose(ps_t[:, 4 + a, :], kd[:, a, :, :].rearrange("p a d -> p (a d)"), identb[:]),
                         reads=[R_kd, R_identb], writes=[R_pst], acc=True)
                f.op("dve", lambda e: e.tensor_copy(qT[:, :, cols], ps_t[:, 0:4, :]), reads=[R_pst], writes=[R_qT[t]])
                f.op("act", lambda e: e.activation(out=kT2[:, :, cols], in_=ps_t[:, 4:6, :], func=AF.Identity), reads=[R_pst], writes=[R_kT[t]])
            S.close()

        inproj(True)
        if stop_after == "h0":
            U.close(); L.close(); return
        if stop_after == "in0":
            S = Scope(nc)
            t32 = S.sb("t32", [128, 512]); R_t = Res()
            for ct in range(4):
                for blk in range(2):
                    f.op("dve", lambda e: e.tensor_copy(t32[:], uT[:, ct, blk * 512:(blk + 1) * 512]), reads=R_uT, writes=[R_t])
                    dump(t32[:], 128, 512, [R_t], r0=ct * 128, c0=blk * 512)
            S.close(); U.close(); L.close()
            return
        if 's5' not in DBG_SKIP:
            s5_phase(L, uT, R_uT, aT, R_aT)
        if stop_after == "s5":
            S = Scope(nc)
            t32 = S.sb("t32", [128, 512]); R_t = Res()
            for ct in range(4):
                for blk in range(9):
                    c0 = blk * 512; n = min(512, NTOK - c0)
                    f.op("dve", lambda e: e.tensor_copy(t32[:, 0:n], aT[:, ct, c0:c0 + n]), reads=R_aT, writes=[R_t])
                    dump(t32[:, 0:n], 128, n, [R_t], r0=ct * 128, c0=c0)
            S.close(); U.close(); L.close()
            return
        R_ATD = Res("atd")
        for k in range(4):
            f.dma(("sp", "act")[k % 2], ATD[k], aT[:, k, :], reads=R_aT, writes=[R_ATD])
        U.close()
        oT = L.sb("oT", [128, 4, NTOK], BF16); R_oT = RL(NT, "oT")
        W = Scope(nc)
        qT = W.sb("qT", [128, 4, NTOK], BF16); R_qT = RL(NT, "qT")
        kT2 = W.sb("kT2", [128, 2, NTOK], BF16); R_kT = RL(NT, "kT")
        vaug = W.sb("vaug", [128, NT, 320], BF16); R_v = RL(NT, "v")
        f.op("pool", lambda e: e.memset(vaug[:], 1.0), writes=R_v)
        inproj(False, qT, R_qT, kT2, R_kT, vaug, R_v)
        if stop_after == "qkv":
            W.close(); L.close(); return
        win_phase(qT, R_qT, kT2, R_kT, vaug, R_v, oT, R_oT)
        W.close()
        if stop_after == "win":
            L.close(); return

        M = Scope(nc)
        mixt = [M.sb("mixa%d" % k, [128, 4, 128], BF16) for k in range(2)]; R_mixt = RL(2)

        def mix_loader(t, b):
            c0 = t * 128
            f.dma("sp", mixt[b][:], ATD[:, :, c0:c0 + 128].rearrange("a p n -> p a n"), reads=[R_ATD], writes=[R_mixt[b]])
            return [mixt[b][:, k, :] for k in range(4)] + [oT[:, k, c0:c0 + 128] for k in range(4)], [R_mixt[b], R_oT[t]]
        out_phase(0, even_w_out, mix_loader, range(NT))
        M.close()
        L.close()

    def sincos(S, ang, n, out_s, out_c, R, tag):
        ki = S.sb("ki_" + tag, [128, n], I32); kf = S.sb("kf_" + tag, [128, n]); rd = S.sb("rd_" + tag, [128, n])
        f.op("dve", lambda e: e.tensor_scalar(out=ki[:], in0=ang, scalar1=1.0 / TWO_PI, scalar2=None, op0=ALU.mult), reads=[R], writes=[R])
        f.op("dve", lambda e: e.tensor_copy(kf[:], ki[:]), reads=[R], writes=[R])
        f.op("dve", lambda e: e.scalar_tensor_tensor(out=rd[:], in0=kf[:], scalar=-CW1, in1=ang, op0=ALU.mult, op1=ALU.add), reads=[R], writes=[R])
        f.op("dve", lambda e: e.scalar_tensor_tensor(out=rd[:], in0=kf[:], scalar=-CW2, in1=rd[:], op0=ALU.mult, op1=ALU.add), reads=[R], writes=[R])
        f.op("dve", lambda e: e.tensor_scalar(out=rd[:], in0=rd[:], scalar1=3.1415925, scalar2=-3.1415925, op0=ALU.min, op1=ALU.max), reads=[R], writes=[R])
        f.op("act", lambda e: e.activation(out=out_s, in_=rd[:], func=AF.Sin), reads=[R], writes=[R])
        f.op("dve", lambda e: e.scalar_tensor_tensor(out=rd[:], in0=rd[:], scalar=-1.0, in1=rd[:], op0=ALU.mult, op1=ALU.max), reads=[R], writes=[R])
        f.op("dve", lambda e: e.tensor_scalar(out=rd[:], in0=rd[:], scalar1=-1.0, scalar2=math.pi / 2, op0=ALU.mult, op1=ALU.add), reads=[R], writes=[R])
        f.op("act", lambda e: e.activation(out=out_c, in_=rd[:], func=AF.Sin), reads=[R], writes=[R])

    def s5_phase(L, uT, R_uT, aT, R_aT):
        P = Scope(nc)
        R = Res("s5setup")
        prm = P.sb("prm", [128, 16, 32])
        dsk = P.sb("dsk", [128, 4]); bgl = P.sb("bgl", [128, 4])
        cs2 = P.sb("cs2", [128, 32, 2]); ncs2 = P.sb("ncs2", [128, 32, 2])
        jt = P.sb("jt", [128, 128]); f.dma("sp", jt[:], k_jidx, writes=[R])
        f.dma("sp", dsk[:], s5_d.rearrange("(c p) -> p c", p=128), writes=[R], allow_slow_non_contiguous=True)
        f.dma("sp", bgl[:], b_glu.rearrange("(c p) -> p c", p=128), writes=[R], allow_slow_non_contiguous=True)
        S = Scope(nc)
        st32 = S.sb("st32", [32, 3, 128]); lsr = S.sb("lsr", [32, 2])
        f.dma("sp", st32[:, 0, :], lam_re.rearrange("d (q g) n -> (d q) (g n)", g=2), writes=[R])
        f.dma("sp", st32[:, 1, :], lam_im.rearrange("d (q g) n -> (d q) (g n)", g=2), writes=[R])
        f.dma("sp", lsr[:], log_step.rearrange("d (q g) -> (d q) g", g=2), writes=[R])
        f.op("dve", lambda e: e.tensor_copy(st32[:, 2, :].rearrange("p (g n) -> p g n", g=2), lsr[:].unsqueeze(2).broadcast_to([32, 2, 64])), reads=[R], writes=[R])
        pst = S.ps("pst", [128, 4, 128])
        for k in range(3):
            f.op("pe", lambda e, k=k: e.transpose(pst[:, k, 0:32], st32[:, k, :], ident[0:32, 0:32]), reads=[R, R_ident], writes=[R], acc=(k > 0))
        f.op("dve", lambda e: e.tensor_copy(prm[:, 0:3, :], pst[:, 0:3, 0:32]), reads=[R], writes=[R])
        lr, li = prm[:, 0, :], prm[:, 1, :]
        dt, th, rr = prm[:, 3, :], prm[:, 4, :], prm[:, 5, :]
        f.op("act", lambda e: e.activation(out=dt, in_=prm[:, 2, :], func=AF.Exp), reads=[R], writes=[R])
        f.op("dve", lambda e: e.tensor_tensor(th, li, dt, ALU.mult), reads=[R], writes=[R])
        f.op("dve", lambda e: e.tensor_tensor(prm[:, 10, :], lr, dt, ALU.mult), reads=[R], writes=[R])
        f.op("act", lambda e: e.activation(out=rr, in_=prm[:, 10, :], func=AF.Exp), reads=[R], writes=[R])
        f.op("dve", lambda e: e.tensor_scalar(out=prm[:, 10, :], in0=th, scalar1=128.0, scalar2=None, op0=ALU.mult), reads=[R], writes=[R])
        sincos(S, prm[:, 10, :], 32, prm[:, 7, :], prm[:, 6, :], R, "a")
        sincos(S, th, 32, prm[:, 12, :], prm[:, 11, :], R, "b")
        abre, abim, den, t1, t2 = prm[:, 13, :], prm[:, 14, :], prm[:, 15, :], prm[:, 10, :], prm[:, 2, :]
        f.op("dve", lambda e: e.tensor_tensor(abre, rr, prm[:, 11, :], ALU.mult), reads=[R], writes=[R])
        f.op("dve", lambda e: e.tensor_scalar(out=abre, in0=abre, scalar1=-1.0, scalar2=None, op0=ALU.add), reads=[R], writes=[R])
        f.op("dve", lambda e: e.tensor_tensor(abim, rr, prm[:, 12, :], ALU.mult), reads=[R], writes=[R])
        f.op("dve", lambda e: e.tensor_tensor(den, lr, lr, ALU.mult), reads=[R], writes=[R])
        f.op("dve", lambda e: e.tensor_tensor(t1, li, li, ALU.mult), reads=[R], writes=[R])
        f.op("dve", lambda e: e.tensor_tensor(den, den, t1, ALU.add), reads=[R], writes=[R])
        f.op("dve", lambda e: e.reciprocal(den, den), reads=[R], writes=[R])
        f.op("dve", lambda e: e.tensor_tensor(t1, abre, lr, ALU.mult), reads=[R], writes=[R])
        f.op("dve", lambda e: e.tensor_tensor(t2, abim, li, ALU.mult), reads=[R], writes=[R])
        f.op("dve", lambda e: e.tensor_tensor(t1, t1, t2, ALU.add), reads=[R], writes=[R])
        f.op("dve", lambda e: e.tensor_tensor(prm[:, 8, :], t1, den, ALU.mult), reads=[R], writes=[R])
        f.op("dve", lambda e: e.tensor_tensor(t1, abim, lr, ALU.mult), reads=[R], writes=[R])
        f.op("dve", lambda e: e.tensor_tensor(t2, abre, li, ALU.mult), reads=[R], writes=[R])
        f.op("dve", lambda e: e.tensor_tensor(t1, t1, t2, ALU.subtract), reads=[R], writes=[R])
        f.op("dve", lambda e: e.tensor_tensor(prm[:, 9, :], t1, den, ALU.mult), reads=[R], writes=[R])
        f.op("dve", lambda e: e.tensor_copy(cs2[:, :, 0], prm[:, 6, :]), reads=[R], writes=[R])
        f.op("dve", lambda e: e.tensor_copy(cs2[:, :, 1], prm[:, 7, :]), reads=[R], writes=[R])
        f.op("dve", lambda e: e.tensor_scalar(out=ncs2[:, :, 0], in0=prm[:, 7, :], scalar1=-1.0, scalar2=None, op0=ALU.mult), reads=[R], writes=[R])
        f.op("dve", lambda e: e.tensor_copy(ncs2[:, :, 1], prm[:, 6, :]), reads=[R], writes=[R])
        S.close()
        cosJ = P.sb("cosJ", [128, 8, 128]); sinJ = P.sb("sinJ", [128, 8, 128]); rtab = P.sb("rtab", [128, 8, 128])
        lB = P.sb("lB", [128, 8, 2, 128], BF16); lC = P.sb("lC", [128, 8, 2, 128], BF16)
        RT = Res("s5tab")
        S = Scope(nc)
        yacc = S.sb("yacc", [128, NTOK]); R_y = Res("yacc")
        NB = 2
        psb = [S.ps("psb%d" % k, [128, 2, 512]) for k in range(NB)]; R_psb = RL(NB)
        psy = [S.ps("psy%d" % k, [128, 512]) for k in range(NB)]; R_psy = RL(NB)
        pstr = [S.ps("pstr%d" % k, [128, 4, 128]) for k in range(2)]; R_pstr = RL(2)
        m = [S.sb("m%d" % k, [128, 2, 512]) for k in range(NB)]; R_m = RL(NB)
        ta = [S.sb("ta%d" % k, [128, 2, 512]) for k in range(NB)]; R_ta = RL(NB)
        g = [S.sb("g%d" % k, [128, 2, 512]) for k in range(NB)]; R_g = RL(NB)
        hb = [S.sb("hb%d" % k, [128, 2, 512], BF16) for k in range(NB)]; R_hb = RL(NB)
        ini = S.sb("ini", [128, 4]); R_ini = Res()
        gq1 = S.sb("gq1", [128, 512]); gq2 = S.sb("gq2", [128, 512]); R_gq1 = Res(); R_gq2 = Res()
        wgl = S.sb("wgl", [128, 4, 512], BF16); R_wgl = Res()
        f.dma("pool", wgl[:], w_glu.rearrange("(kc p) n -> p kc n", p=128), writes=[R_wgl])
        blocks = [(0, 256)] + [(256 + 512 * k, 512) for k in range(8)]
        it = 0
        for ct in range(4):
            T = Scope(nc)
            ang = T.sb("ang", [128, 8, 128])
            WB = T.sb("WB", [128, 2, 8, 128]); SC = T.sb("SC", [128, 2, 8, 128]); WB2 = T.sb("WB2", [128, 2, 8, 128])
            fre8 = T.sb("fre8", [128, 8]); fim8 = T.sb("fim8", [128, 8])
            for d in range(2):
                gsl = slice(d * 16 + ct * 4, d * 16 + ct * 4 + 4); lsl = slice(d * 4, d * 4 + 4)
                f.op("dve", lambda e: e.tensor_tensor(ang[:, lsl, :], jt[:].unsqueeze(1).broadcast_to([128, 4, 128]), th[:, gsl].unsqueeze(2).broadcast_to([128, 4, 128]), ALU.mult), reads=[R, RT], writes=[RT])
                f.op("dve", lambda e: e.tensor_copy(rtab[:, lsl, :], rr[:, gsl].unsqueeze(2).broadcast_to([128, 4, 128])), reads=[R, RT], writes=[RT])
                f.op("dve", lambda e: e.tensor_copy(fre8[:, lsl], prm[:, 8, gsl]), reads=[R, RT], writes=[RT])
                f.op("dve", lambda e: e.tensor_copy(fim8[:, lsl], prm[:, 9, gsl]), reads=[R, RT], writes=[RT])
            sincos(T, ang[:].rearrange("p a b -> p (a b)"), 1024, sinJ[:].rearrange("p a b -> p (a b)"), cosJ[:].rearrange("p a b -> p (a b)"), RT, "c%d" % ct)
            f.op("pool", lambda e: e.memset(WB[:], 0.0), reads=[RT], writes=[RT])
            f.op("pool", lambda e: e.memset(SC[:], 0.0), reads=[RT], writes=[RT])
            qn = 0
            for d in range(2):
                for gi in range(8):
                    g_ = ct * 8 + gi
                    l = d * 4 + gi // 2
                    gl = gi % 2
                    for ri, (bsrc, csrc) in enumerate(((b_re, c_re), (b_im, c_im))):
                        q1 = ("sp", "act")[qn % 2]; qn += 1
                        f.dma(q1, WB[64 * gl:64 * gl + 64, ri, l, 16 * gi:16 * gi + 16], bsrc[d, g_], writes=[RT])
                        f.dma(q1, SC[16 * gi:16 * gi + 16, ri, l, 64 * gl:64 * gl + 64], csrc[d, g_], writes=[RT])
            fre = fre8[:].unsqueeze(2).broadcast_to([128, 8, 128]); fim = fim8[:].unsqueeze(2).broadcast_to([128, 8, 128])
            f.op("dve", lambda e: e.tensor_tensor(WB2[:, 0], WB[:, 0], fre, ALU.mult), reads=[RT], writes=[RT])
            f.op("pool", lambda e: e.tensor_tensor(WB2[:, 1], WB[:, 1], fim, ALU.mult), reads=[RT], writes=[RT])
            f.op("dve", lambda e: e.tensor_tensor(WB2[:, 0], WB2[:, 0], WB2[:, 1], ALU.subtract), reads=[RT], writes=[RT])
            f.op("pool", lambda e: e.tensor_tensor(WB2[:, 1], WB[:, 1], fre, ALU.mult), reads=[RT], writes=[RT])
            f.op("dve", lambda e: e.tensor_tensor(WB[:, 0], WB[:, 0], fim, ALU.mult), reads=[RT], writes=[RT])
            f.op("dve", lambda e: e.tensor_tensor(WB2[:, 1], WB2[:, 1], WB[:, 0], ALU.add), reads=[RT], writes=[RT])
            n_ = 0
            for srct, dst, neg in ((WB2, lB, False), (SC, lC, True)):
                for ri in range(2):
                    for d4 in range(2):
                        pb = n_ % 2; n_ += 1
                        for k in range(4):
                            l = d4 * 4 + k
                            f.op("pe", lambda e, k=k, l=l: e.transpose(pstr[pb][:, k, :], srct[:, ri, l, :], ident[:]), reads=[RT, R_ident], writes=[R_pstr[pb]], acc=(k > 0))
                        scl = -1.0 if (neg and ri == 1) else 1.0
                        f.op("act", lambda e: e.activation(out=dst[:, d4 * 4:d4 * 4 + 4, ri, :], in_=pstr[pb][:], func=AF.Identity, scale=scl), reads=[R_pstr[pb], RT], writes=[RT])
            T.close()
            f.op("act", lambda e: e.activation(out=yacc[:], in_=uT[:, ct, :], func=AF.Copy, scale=dsk[:, ct:ct + 1]), reads=R_uT + [R], writes=[R_y])
            items = []
            for pi in range(4):
                for d in range(2):
                    for bidx, (s0, n) in enumerate(blocks):
                        items.append((pi, d, bidx, s0, n))
            NI = len(items)

            def v3(ap):
                return ap.rearrange("p (c j) -> p c j", j=128)

            def geom(k):
                pi, d, bidx, s0, n = items[k]
                bi = k % NB
                dq = d * 16 + ct * 4 + pi
                l = d * 4 + pi
                nch = n // 128
                if d == 0:
                    c0 = s0
                    ucols = uT[:, ct, c0:c0 + n]
                    ycols = yacc[:, c0:c0 + n]
                else:
                    c0 = (256 - s0 - n) if s0 < 256 else (4608 - s0 - n)
                    ucols = rev_ap(uT[:, ct, c0:c0 + n], n)
                    ycols = rev_ap(yacc[:, c0:c0 + n], n)
                tl = [R_uT[kk] for kk in range(c0 // 128, (c0 + n) // 128)]
                cb = cosJ[:, l, :].unsqueeze(1).broadcast_to([128, nch, 128])
                sb_ = sinJ[:, l, :].unsqueeze(1).broadcast_to([128, nch, 128])
                return pi, d, bidx, n, bi, dq, l, nch, ucols, ycols, tl, cb, sb_

            def stA(k):
                pi, d, bidx, n, bi, dq, l, nch, ucols, ycols, tl, cb, sb_ = geom(k)
                for ri in range(2):
                    f.op("pe", lambda e, ri=ri: e.matmul(psb[bi][:, ri, 0:n], lB[:, l, ri, :], ucols, start=True, stop=True),
                         reads=[RT] + tl, writes=[R_psb[bi]], acc=(ri > 0))
                bre, bim = v3(psb[bi][:, 0, 0:n]), v3(psb[bi][:, 1, 0:n])
                mre, mim = v3(m[bi][:, 0, 0:n]), v3(m[bi][:, 1, 0:n])
                t_a, t_b = v3(ta[bi][:, 0, 0:n]), v3(ta[bi][:, 1, 0:n])
                f.op("dve", lambda e: e.tensor_tensor(mre, bre, cb, ALU.mult), reads=[R_psb[bi], RT], writes=[R_m[bi]])
                f.op("dve", lambda e: e.tensor_tensor(t_a, bim, sb_, ALU.mult), reads=[R_psb[bi], RT], writes=[R_ta[bi]])
                f.op("dve", lambda e: e.tensor_tensor(mre, mre, t_a, ALU.add), reads=[R_m[bi], R_ta[bi]], writes=[R_m[bi]])
                f.op("dve", lambda e: e.tensor_tensor(mim, bim, cb, ALU.mult), reads=[R_psb[bi], RT], writes=[R_m[bi]])
                f.op("dve", lambda e: e.tensor_tensor(t_b, bre, sb_, ALU.mult), reads=[R_psb[bi], RT], writes=[R_ta[bi]])
                f.op("dve", lambda e: e.tensor_tensor(mim, mim, t_b, ALU.subtract), reads=[R_m[bi], R_ta[bi]], writes=[R_m[bi]])

            def stB(k):
                pi, d, bidx, n, bi, dq, l, nch, ucols, ycols, tl, cb, sb_ = geom(k)
                prev = None
                if bidx > 0:
                    pbi = (k - 1) % NB
                    pn = items[k - 1][4]
                    prev = (g[pbi], pbi, pn // 128 - 1)
                for c in range(nch):
                    cs = slice(c * 128, (c + 1) * 128)
                    if prev is None:
                        i_re = i_im = 0.0
                        rd_extra = []
                    else:
                        pg, pbi, pc_ = prev
                        gre_l = pg[:, 0, pc_ * 128 + 127:pc_ * 128 + 128]
                        gim_l = pg[:, 1, pc_ * 128 + 127:pc_ * 128 + 128]
                        c128 = prm[:, 6, dq:dq + 1]; s128 = prm[:, 7, dq:dq + 1]
                        f.op("dve", lambda e: e.tensor_scalar(out=ini[:, 0:2], in0=cs2[:, dq, :], scalar1=gre_l, scalar2=None, op0=ALU.mult), reads=[R_g[pbi], R], writes=[R_ini])
                        f.op("dve", lambda e: e.scalar_tensor_tensor(out=ini[:, 0:2], in0=ncs2[:, dq, :], scalar=gim_l, in1=ini[:, 0:2], op0=ALU.mult, op1=ALU.add), reads=[R_g[pbi], R, R_ini], writes=[R_ini])
                        i_re, i_im = ini[:, 0:1], ini[:, 1:2]
                        rd_extra = [R_ini]
                    f.op("dve", lambda e: e.tensor_tensor_scan(g[bi][:, 0, cs], rtab[:, l, :], m[bi][:, 0, cs], i_re, ALU.mult, ALU.add),
                         reads=[R_m[bi], RT] + rd_extra, writes=[R_g[bi]])
                    f.op("dve", lambda e: e.tensor_tensor_scan(g[bi][:, 1, cs], rtab[:, l, :], m[bi][:, 1, cs], i_im, ALU.mult, ALU.add),
                         reads=[R_m[bi], RT] + rd_extra, writes=[R_g[bi]])
                    prev = (g[bi], bi, c)

            def stC(k):
                pi, d, bidx, n, bi, dq, l, nch, ucols, ycols, tl, cb, sb_ = geom(k)
                mre, mim = v3(m[bi][:, 0, 0:n]), v3(m[bi][:, 1, 0:n])
                t_a, t_b = v3(ta[bi][:, 0, 0:n]), v3(ta[bi][:, 1, 0:n])
                gre, gim = v3(g[bi][:, 0, 0:n]), v3(g[bi][:, 1, 0:n])
                hre, him = v3(hb[bi][:, 0, 0:n]), v3(hb[bi][:, 1, 0:n])
                f.op("dve", lambda e: e.tensor_tensor(t_a, gre, cb, ALU.mult), reads=[R_g[bi], RT], writes=[R_ta[bi]])
                f.op("dve", lambda e: e.tensor_tensor(mre, gim, sb_, ALU.mult), reads=[R_g[bi], RT], writes=[R_m[bi]])
                f.op("dve", lambda e: e.tensor_tensor(hre, t_a, mre, ALU.subtract), reads=[R_ta[bi], R_m[bi]], writes=[R_hb[bi]])
                f.op("dve", lambda e: e.tensor_tensor(t_b, gre, sb_, ALU.mult), reads=[R_g[bi], RT], writes=[R_ta[bi]])
                f.op("dve", lambda e: e.tensor_tensor(mim, gim, cb, ALU.mult), reads=[R_g[bi], RT], writes=[R_m[bi]])
                f.op("dve", lambda e: e.tensor_tensor(him, t_b, mim, ALU.add), reads=[R_ta[bi], R_m[bi]], writes=[R_hb[bi]])
                for ri in range(2):
                    f.op("pe", lambda e, ri=ri: e.matmul(psy[bi][:, 0:n], lC[:, l, ri, :], hb[bi][:, ri, 0:n], start=(ri == 0), stop=(ri == 1)),
                         reads=[RT, R_hb[bi]], writes=[R_psy[bi]], acc=(ri > 0))

            def stY(k):
                pi, d, bidx, n, bi, dq, l, nch, ucols, ycols, tl, cb, sb_ = geom(k)
                f.op("dve", lambda e: e.tensor_tensor(ycols, psy[bi][:, 0:n], ycols, ALU.add), reads=[R_psy[bi], R_y], writes=[R_y])

            stA(0)
            for k in range(NI):
                if k + 1 < NI:
                    stA(k + 1)
                stB(k)
                stC(k)
                if k >= 1:
                    stY(k - 1)
            stY(NI - 1)
            for (s0, n) in blocks:
                yb = yacc[:, s0:s0 + n]
                f.op("pool", lambda e: e.tensor_tensor(gq1[:, 0:n], yb, yb, ALU.mult), reads=[R_y], writes=[R_gq1])
                f.op("dve", lambda e: e.tensor_scalar(out=gq1[:, 0:n], in0=gq1[:, 0:n], scalar1=0.044715, scalar2=1.0, op0=ALU.mult, op1=ALU.add), reads=[R_gq1], writes=[R_gq1])
                f.op("pool", lambda e: e.tensor_tensor(gq1[:, 0:n], gq1[:, 0:n], yb, ALU.mult), reads=[R_gq1, R_y], writes=[R_gq1])
                f.op("act", lambda e: e.activation(out=gq2[:, 0:n], in_=gq1[:, 0:n], func=AF.Sigmoid, scale=1.5957691216057308), reads=[R_gq1], writes=[R_gq2])
                f.op("dve", lambda e: e.tensor_tensor(aT[:, ct, s0:s0 + n], yb, gq2[:, 0:n], ALU.mult), reads=[R_gq2, R_y], writes=R_aT[s0 // 128:(s0 + n) // 128])
        sg = [S.sb("sg%d" % k, [128, 512], BF16) for k in range(2)]; R_sg = RL(2)
        anew = S.sb("anew", [128, 4, 512], BF16); R_anew = Res()
        nn = 0
        for (s0, n) in blocks:
            tl = R_aT[s0 // 128:(s0 + n) // 128]
            for cto in range(4):
                bi = nn % 2; nn += 1
                for cti in range(4):
                    f.op("pe", lambda e, cti=cti: e.matmul(psy[bi][:, 0:n], wgl[:, cti, cto * 128:(cto + 1) * 128], aT[:, cti, s0:s0 + n], start=(cti == 0), stop=(cti == 3)),
                         reads=[R_wgl] + tl, writes=[R_psy[bi]], acc=(cti > 0))
                f.op("act", lambda e: e.activation(out=sg[bi][:, 0:n], in_=psy[bi][:, 0:n], func=AF.Sigmoid, bias=bgl[:, cto:cto + 1], scale=1.0), reads=[R_psy[bi], R], writes=[R_sg[bi]])
                f.op("dve", lambda e: e.tensor_tensor(anew[:, cto, 0:n], aT[:, cto, s0:s0 + n], sg[bi][:, 0:n], ALU.mult), reads=[R_sg[bi]] + tl, writes=[R_anew])
            f.op("pool", lambda e: e.tensor_copy(aT[:, :, s0:s0 + n], anew[:, :, 0:n]), reads=[R_anew], writes=tl)
        S.close()
        P.close()

    def win_phase(qT, R_qT, kT2, R_kT, vaug, R_v, oT, R_oT):
        S = Scope(nc)
        esink = S.sb("esink", [128, 8]); R_es = Res()
        f.dma("sp", esink[:], win_sink.partition_broadcast(128), writes=[R_es])
        f.op("act", lambda e: e.activation(out=esink[:], in_=esink[:], func=AF.Exp), reads=[R_es], writes=[R_es])
        NBS = 3
        ps_s = [S.ps("ps_s%d" % k, [128, 8, 128]) for k in range(NBS)]; R_pss = RL(NBS)
        ps_o = [S.ps("ps_o%d" % k, [128, 512]) for k in range(2)]; R_pso = RL(2)
        pT = [S.sb("pT%d" % k, [128, 5, 128], BF16) for k in range(NBS)]; R_pT = RL(NBS)
        dtmp = [S.sb("dtmp%d" % k, [128, 128]) for k in range(2)]; R_dt = RL(2)
        items = []
        for t in range(NT):
            kts = [(0, None), (1, None)]
            if t >= 2:
                for kt in (t - 1, t, t + 1):
                    if 2 <= kt < NT:
                        kts.append((kt, (0 if kt == t - 1 else (1 if kt == t + 1 else None))))
            for h in range(8):
                items.append((t, h, kts))

        def front(i_):
            t, h, kts = items[i_]
            cols = slice(t * 128, (t + 1) * 128)
            nk = len(kts)
            bs = i_ % NBS
            pr, base, kvh = h // 2, 64 * (h % 2), h // 4
            for i, (kt, mk) in enumerate(kts):
                f.op("pe", lambda e, i=i, kt=kt: e.matmul(ps_s[bs][:, i, :], kT2[base:base + 64, kvh, kt * 128:(kt + 1) * 128], qT[base:base + 64, pr, cols], start=True, stop=True),
                     reads=[R_kT[kt], R_qT[t]], writes=[R_pss[bs]], acc=(i > 0))
            f.op("act", lambda e: e.activation(out=pT[bs][:, 0:nk, :], in_=ps_s[bs][:, 0:nk, :], func=AF.Exp, scale=0.125), reads=[R_pss[bs]], writes=[R_pT[bs]])
            for i, (kt, mk) in enumerate(kts):
                if mk is not None:
                    f.op("dve", lambda e, i=i, mk=mk: e.tensor_tensor(pT[bs][:, i, :], pT[bs][:, i, :], maskb[:, mk, :], ALU.mult), reads=[R_pT[bs], R_mask], writes=[R_pT[bs]])

        def back(i_):
            t, h, kts = items[i_]
            cols = slice(t * 128, (t + 1) * 128)
            nk = len(kts)
            bs = i_ % NBS
            bi = i_ % 2
            pr, base, kvh = h // 2, 64 * (h % 2), h // 4
            voff = (64 if h % 2 == 0 else 0) + 128 * kvh
            for i, (kt, mk) in enumerate(kts):
                f.op("pe", lambda e, i=i, kt=kt: e.matmul(ps_o[bi][:, 0:128], vaug[:, kt, voff:voff + 128], pT[bs][:, i, :], start=(i == 0), stop=(i == nk - 1)),
                     reads=[R_v[kt], R_pT[bs]], writes=[R_pso[bi]], acc=(i > 0))
            nb, db = (0, 64) if h % 2 == 0 else (64, 0)
            f.op("dve", lambda e: e.tensor_scalar(out=dtmp[bi][nb:nb + 64, :], in0=ps_o[bi][db:db + 64, 0:128], scalar1=esink[db:db + 64, h:h + 1], scalar2=None, op0=ALU.add),
                 reads=[R_pso[bi], R_es], writes=[R_dt[bi]])
            f.op("dve", lambda e: e.reciprocal(dtmp[bi][nb:nb + 64, :], dtmp[bi][nb:nb + 64, :]), reads=[R_dt[bi]], writes=[R_dt[bi]])
            f.op("dve", lambda e: e.tensor_tensor(oT[nb:nb + 64, pr, cols], ps_o[bi][nb:nb + 64, 0:128], dtmp[bi][nb:nb + 64, :], ALU.mult), reads=[R_pso[bi], R_dt[bi]], writes=[R_oT[t]])
        front(0)
        for i_ in range(len(items)):
            if i_ + 1 < len(items):
                front(i_ + 1)
            back(i_)
        S.close()

    def out_phase(layer, w_out_d, mix_loader, tiles):
        S = Scope(nc)
        load_ln(layer * 2 + 0)
        wo = S.sb("wo", [128, 8, D], BF16); R_wo = Res()
        f.dma("pool", wo[:], w_out_d.rearrange("(kc p) n -> p kc n", p=128), writes=[R_wo])
        xt = [S.sb("xt%d" % k, [128, D]) for k in range(2)]; R_xt = RL(2)
        ot = [S.sb("ot%d" % k, [128, D]) for k in range(2)]; R_ot = RL(2)
        tmp = S.sb("tmp", [128, D]); R_tmp = Res()
        small = S.sb("small", [128, 16]); R_small = Res()
        ps_o2 = [S.ps("ps_o2%d" % k, [128, D]) for k in range(2)]; R_ps = RL(2)
        for n, t in enumerate(tiles):
            b = n % 2
            src, rs = src_tile(layer, t)
            f.dma("sp", xt[b][:], src, reads=rs, writes=[R_xt[b]])
            mixT, R_mix = mix_loader(t, b)
            for half in range(2):
                for kc in range(8):
                    f.op("pe", lambda e, kc=kc, half=half: e.matmul(ps_o2[b][:, half * 512:(half + 1) * 512], mixT[kc], wo[:, kc, half * 512:(half + 1) * 512], start=(kc == 0), stop=(kc == 7)),
                         reads=[R_wo] + R_mix, writes=[R_ps[b]], acc=(half + kc > 0))
            resid_ln(S, xt[b], R_xt[b], ps_o2[b], R_ps[b], (1 if t < 2 else 0), layer * 2 + 0, ot[b], R_ot[b], tmp, R_tmp, small, R_small)
            f.dma("act", XR[t * 128:(t + 1) * 128, :], ot[b][:], reads=[R_ot[b]], writes=[R_XR[t]])
        S.close()

    def ffn_phase(layer, tiles_all, final):
        P = Scope(nc)
        rw = P.sb("rw", [128, 8, 32]); R_rw = Res()
        f.dma("sp", rw[:], router_w.rearrange("(kc p) n -> p kc n", p=128), writes=[R_rw])
        rbias = P.sb("rbias", [128, 32]); f.dma("sp", rbias[:], router_b.partition_broadcast(128), writes=[R_rw])
        GT = 9
        load_ln(layer * 2 + 1)
        groups = [tiles_all[i:i + GT] for i in range(0, len(tiles_all), GT)]
        for grp in groups:
            S = Scope(nc)
            ng = len(grp)
            hT = S.sb("hTg", [128, 8, GT * 128], BF16); R_hT = RL(ng, "hTg")
            comb = S.sb("comb", [128, GT, 32]); R_comb = RL(ng, "comb")
            yacc = S.sb("yaccg", [128, GT, D]); R_y = RL(ng, "yg")
            A = Scope(nc)
            xt = [A.sb("xt%d" % k, [128, D]) for k in range(2)]; R_xt = RL(2)
            h32 = A.sb("h32", [128, D]); R_h32 = Res()
            h32T = A.sb("h32T", [128, 8, 128]); R_h32T = Res()
            ps_tp = A.ps("ps_tp", [128, 8, 128]); R_pstp = Res(x=True)
            ps_r = A.ps("ps_r", [128, 512]); R_psr = Res()
            sc = A.sb("sc", [128, 32]); sel = A.sb("sel", [128, 32]); R_sc = Res()
            pa = A.sb("pa", [128, 8, 6]); pm = A.sb("pm", [128, 8, 6]); gs = A.sb("gs", [128, 8]); thr = A.sb("thr", [128, 8])
            gm = A.sb("gm", [128, 2]); mg = A.sb("mg", [128, 8]); sm = A.sb("sm", [128, 8, 4])
            for j, t in enumerate(grp):
                b = j % 2
                f.dma("sp", xt[b][:], XR[t * 128:(t + 1) * 128, :], reads=[R_XR[t]], writes=[R_xt[b]])
                which = 1 if t < 2 else 0
                mod_transpose(xt[b], R_xt[b], which, h32, R_h32, ps_tp, R_pstp, hT[:, :, j * 128:(j + 1) * 128], R_hT[j], h32T, R_h32T)
                for kc in range(8):
                    f.op("pe", lambda e, kc=kc: e.matmul(ps_r[:, 0:32], h32T[:, kc, :], rw[:, kc, :], start=(kc == 0), stop=(kc == 7)), reads=[R_h32T, R_rw], writes=[R_psr], acc=(kc > 0))
                R1 = R_sc
                f.op("act", lambda e: e.activation(out=sc[:], in_=ps_r[:, 0:32], func=AF.Sigmoid), reads=[R_psr], writes=[R1])
                f.op("dve", lambda e: e.tensor_tensor(sel[:], sc[:], rbias[:], ALU.add), reads=[R1, R_rw], writes=[R1])
                s3 = sel[:].rearrange("p (g e) -> p g e", e=4)
                pairs = [(0, 1), (0, 2), (0, 3), (1, 2), (1, 3), (2, 3)]
                for k, (a_, b_) in enumerate(pairs):
                    f.op("dve", lambda e, k=k, a_=a_, b_=b_: e.tensor_tensor(pa[:, :, k], s3[:, :, a_], s3[:, :, b_], ALU.add), reads=[R1], writes=[R1])
                    f.op("dve", lambda e, k=k, a_=a_, b_=b_: e.tensor_tensor(pm[:, :, k], s3[:, :, a_], s3[:, :, b_], ALU.min), reads=[R1], writes=[R1])
                f.op("dve", lambda e: e.tensor_reduce(out=gs[:], in_=pa[:], axis=AX.X, op=ALU.max), reads=[R1], writes=[R1])
                f.op("dve", lambda e: e.tensor_reduce(out=thr[:], in_=pm[:], axis=AX.X, op=ALU.max), reads=[R1], writes=[R1])
                f.op("dve", lambda e: e.tensor_reduce(out=gm[:, 0:1], in_=gs[:], axis=AX.X, op=ALU.max), reads=[R1], writes=[R1])
                f.op("dve", lambda e: e.tensor_scalar(out=mg[:], in0=gs[:], scalar1=gm[:, 0:1], scalar2=None, op0=ALU.is_ge), reads=[R1], writes=[R1])
                f.op("dve", lambda e: e.tensor_tensor(sm[:], s3, thr[:].unsqueeze(2).broadcast_to([128, 8, 4]), ALU.is_ge), reads=[R1], writes=[R1])
                f.op("dve", lambda e: e.tensor_tensor(sm[:], sm[:], mg[:].unsqueeze(2).broadcast_to([128, 8, 4]), ALU.mult), reads=[R1], writes=[R1])
                cj = comb[:, j, :]
                f.op("dve", lambda e: e.tensor_tensor(cj, sm[:].rearrange("p g e -> p (g e)"), sc[:], ALU.mult), reads=[R1], writes=[R_comb[j]])
                f.op("dve", lambda e: e.tensor_reduce(out=gm[:, 1:2], in_=cj, axis=AX.X, op=ALU.add), reads=[R_comb[j], R1], writes=[R1])
                f.op("dve", lambda e: e.reciprocal(gm[:, 1:2], gm[:, 1:2]), reads=[R1], writes=[R1])
                f.op("dve", lambda e: e.tensor_scalar(out=cj, in0=cj, scalar1=gm[:, 1:2], scalar2=None, op0=ALU.mult), reads=[R1, R_comb[j]], writes=[R_comb[j]])
            A.close()
            B = Scope(nc)
            wg = [B.sb("wg%d" % k, [128, 8, 512], BF16) for k in range(2)]
            wu = [B.sb("wu%d" % k, [128, 8, 512], BF16) for k in range(2)]
            wd = [B.sb("wd%d" % k, [128, 4, D], BF16) for k in range(2)]
            R_w = RL(2, "w")
            psg = [B.ps("psg%d" % k, [128, 512]) for k in range(2)]; R_psg = RL(2)
            psu = [B.ps("psu%d" % k, [128, 512]) for k in range(2)]; R_psu = RL(2)
            psd = [B.ps("psd%d" % k, [128, D]) for k in range(2)]; R_psd = RL(2)
            sg = [B.sb("sg%d" % k, [128, 512]) for k in range(2)]; R_sg = RL(2)
            hid = [B.sb("hid%d" % k, [128, 4, 512], BF16) for k in range(2)]; R_hid = RL(2)
            ntok = ng * 128
            blocks = [(c0, min(512, ntok - c0)) for c0 in range(0, ntok, 512)]
            nfc = 0; nblk = 0; nd = 0
            for ex in range(32):
                wb = ex % 2
                f.dma("pool", wg[wb][:], w_gate[layer, ex].rearrange("(kc p) n -> p kc n", p=128), writes=[R_w[wb]])
                f.dma("pool", wu[wb][:], w_up[layer, ex].rearrange("(kc p) n -> p kc n", p=128), writes=[R_w[wb]])
                f.dma("pool", wd[wb][:], w_down[layer, ex].rearrange("(kc p) n -> p kc n", p=128), writes=[R_w[wb]])
                for (c0, n) in blocks:
                    hb_ = nblk % 2; nblk += 1
                    tl = R_hT[c0 // 128:(c0 + n) // 128]
                    for fc in range(4):
                        pb = nfc % 2; nfc += 1
                        for kc in range(8):
                            f.op("pe", lambda e, kc=kc, fc=fc: e.matmul(psg[pb][:, 0:n], wg[wb][:, kc, fc * 128:(fc + 1) * 128], hT[:, kc, c0:c0 + n], start=(kc == 0), stop=(kc == 7)),
                                 reads=[R_w[wb]] + tl, writes=[R_psg[pb]], acc=(kc > 0))
                        for kc in range(8):
                            f.op("pe", lambda e, kc=kc, fc=fc: e.matmul(psu[pb][:, 0:n], wu[wb][:, kc, fc * 128:(fc + 1) * 128], hT[:, kc, c0:c0 + n], start=(kc == 0), stop=(kc == 7)),
                                 reads=[R_w[wb]] + tl, writes=[R_psu[pb]], acc=(kc > 0))
                        f.op("act", lambda e: e.activation(out=sg[pb][:, 0:n], in_=psg[pb][:, 0:n], func=AF.Silu), reads=[R_psg[pb]], writes=[R_sg[pb]])
                        f.op("dve", lambda e, fc=fc: e.tensor_tensor(hid[hb_][:, fc, 0:n], sg[pb][:, 0:n], psu[pb][:, 0:n], ALU.mult), reads=[R_sg[pb], R_psu[pb]], writes=[R_hid[hb_]])
                    for tt in range(n // 128):
                        j = c0 // 128 + tt
                        db = nd % 2; nd += 1
                        for half in range(2):
                            for fc in range(4):
                                f.op("pe", lambda e, fc=fc, half=half: e.matmul(psd[db][:, half * 512:(half + 1) * 512], hid[hb_][:, fc, tt * 128:(tt + 1) * 128], wd[wb][:, fc, half * 512:(half + 1) * 512], start=(fc == 0), stop=(fc == 3)),
                                     reads=[R_w[wb], R_hid[hb_]], writes=[R_psd[db]], acc=(half + fc > 0))
                        cw = comb[:, j, ex:ex + 1]
                        if ex == 0:
                            f.op("dve", lambda e: e.tensor_scalar(out=yacc[:, j, :], in0=psd[db][:], scalar1=cw, scalar2=None, op0=ALU.mult), reads=[R_psd[db], R_comb[j]], writes=[R_y[j]])
                        else:
                            f.op("dve", lambda e: e.scalar_tensor_tensor(out=yacc[:, j, :], in0=psd[db][:], scalar=cw, in1=yacc[:, j, :], op0=ALU.mult, op1=ALU.add), reads=[R_psd[db], R_comb[j], R_y[j]], writes=[R_y[j]])
            B.close()
            C = Scope(nc)
            xt = [C.sb("xt%d" % k, [128, D]) for k in range(2)]; R_xt = RL(2)
            ot = [C.sb("ot%d" % k, [128, D]) for k in range(2)]; R_ot = RL(2)
            tmp = C.sb("tmp", [128, D]); R_tmp = Res()
            small = C.sb("small", [128, 16]); R_small = Res()
            for j, t in enumerate(grp):
                b = j % 2
                f.dma("sp", xt[b][:], XR[t * 128:(t + 1) * 128, :], reads=[R_XR[t]], writes=[R_xt[b]])
                yj = yacc[:, j, :]

                class _V:
                    def __init__(self, ap): self.ap = ap
                    def __getitem__(self, k): return self.ap
                resid_ln(C, xt[b], R_xt[b], _V(yj), R_y[j], (1 if t < 2 else 0), layer * 2 + 1, ot[b], R_ot[b], tmp, R_tmp, small, R_small)
                if final:
                    f.dma("act", out_d[(t - 2) * 128:(t - 1) * 128, :], ot[b][:], reads=[R_ot[b]], writes=[R_out])
                else:
                    f.dma("act", XR[t * 128:(t + 1) * 128, :], ot[b][:], reads=[R_ot[b]], writes=[R_XR[t]])
            C.close()
            S.close()
        P.close()


    def ffn_sparse(layer, tiles_all, final):
        IOA = bass.IndirectOffsetOnAxis
        ng = len(tiles_all)
        M = ng * 32
        P = Scope(nc)
        rw = P.sb("rw", [128, 8, 32]); R_rw = Res()
        f.dma("sp", rw[:], router_w.rearrange("(kc p) n -> p kc n", p=128), writes=[R_rw])
        rbias = P.sb("rbias", [128, 32]); f.dma("sp", rbias[:], router_b.partition_broadcast(128), writes=[R_rw])
        jt = P.sb("jt2", [128, 128]); f.dma("sp", jt[:], k_jidx, writes=[R_rw])
        pc = P.sb("pc", [128, 4]); f.dma("sp", pc[:], k_pc, writes=[R_rw])
        load_ln(layer * 2 + 1)
        comb = P.sb("comb", [128, ng, 32]); R_comb = RL(ng, "comb")
        posA_i = P.sb("posA_i", [128, ng], I32); posB_i = P.sb("posB_i", [128, ng], I32)
        wA = P.sb("wA", [128, ng]); wB = P.sb("wB", [128, ng])
        NSO = NS - 32
        idxw = P.sb("idxw", [128, NSO, 4], I32)
        R_rt = Res("route")
        R_XsW = RL(ng, "xsw")
        R_Ys = RL(NS, "ys")
        HB = Scope(nc)
        hb_all = HB.sb("hb_all", [128, ng, D], BF16); R_hb = RL(ng, "hb")
        A = Scope(nc)
        xt = [A.sb("xt%d" % k, [128, D]) for k in range(2)]; R_xt = RL(2)
        h32 = [A.sb("h32%d" % k, [128, D]) for k in range(2)]; R_h32 = RL(2)
        h32T = A.sb("h32T", [128, 8, 128]); R_h32T = Res()
        ps_tp = [A.ps("ps_tp%d" % k, [128, 8, 128]) for k in range(2)]; R_pstp = RL(2)
        ps_r = A.ps("ps_r", [128, 512]); R_psr = Res()
        sc = A.sb("sc", [128, 32]); sel = A.sb("sel", [128, 32]); R_sc = Res()
        pa = A.sb("pa", [128, 8, 6]); pm = A.sb("pm", [128, 8, 6]); gs = A.sb("gs", [128, 8]); thr = A.sb("thr", [128, 8])
        gm = A.sb("gm", [128, 2]); mg = A.sb("mg", [128, 8]); sm = A.sb("sm", [128, 8, 4])
        for j, t in enumerate(tiles_all):
            b = j % 2
            f.dma("sp", xt[b][:], XR[t * 128:(t + 1) * 128, :], reads=[R_XR[t]], writes=[R_xt[b]])
            which = 1 if t < 2 else 0
            f.op("dve", lambda e: e.tensor_tensor(h32[b][:], xt[b][:], mod[:, which, 1, :], ALU.mult), reads=[R_xt[b], R_mod], writes=[R_h32[b]])
            f.op("dve", lambda e: e.tensor_tensor(h32[b][:], h32[b][:], mod[:, which, 0, :], ALU.add), reads=[R_h32[b], R_mod], writes=[R_h32[b]])
            for kc in range(8):
                f.op("pe", lambda e, kc=kc: e.transpose(ps_tp[b][:, kc, :], h32[b][:, kc * 128:(kc + 1) * 128], ident[:]),
                     reads=[R_h32[b], R_ident], writes=[R_pstp[b]], acc=(kc > 0))
            f.op("dve", lambda e: e.tensor_copy(h32T[:], ps_tp[b][:]), reads=[R_pstp[b]], writes=[R_h32T])
            f.op("act", lambda e: e.activation(out=hb_all[:, j, :].rearrange("p (c j q) -> p c j q", c=4, j=2),
                                               in_=h32[b][:].rearrange("p (c q j) -> p c j q", c=4, j=2), func=AF.Identity),
                 reads=[R_h32[b]], writes=[R_hb[j]])
            for kc in range(8):
                f.op("pe", lambda e, kc=kc: e.matmul(ps_r[:, 0:32], h32T[:, kc, :], rw[:, kc, :], start=(kc == 0), stop=(kc == 7)), reads=[R_h32T, R_rw], writes=[R_psr], acc=(kc > 0))
            R1 = R_sc
            f.op("act", lambda e: e.activation(out=sc[:], in_=ps_r[:, 0:32], func=AF.Sigmoid), reads=[R_psr], writes=[R1])
            f.op("dve", lambda e: e.tensor_tensor(sel[:], sc[:], rbias[:], ALU.add), reads=[R1, R_rw], writes=[R1])
            s3 = sel[:].rearrange("p (g e) -> p g e", e=4)
            pairs = [(0, 1), (0, 2), (0, 3), (1, 2), (1, 3), (2, 3)]
            for k, (a_, b_) in enumerate(pairs):
                f.op("dve", lambda e, k=k, a_=a_, b_=b_: e.tensor_tensor(pa[:, :, k], s3[:, :, a_], s3[:, :, b_], ALU.add), reads=[R1], writes=[R1])
                f.op("dve", lambda e, k=k, a_=a_, b_=b_: e.tensor_tensor(pm[:, :, k], s3[:, :, a_], s3[:, :, b_], ALU.min), reads=[R1], writes=[R1])
            f.op("dve", lambda e: e.tensor_reduce(out=gs[:], in_=pa[:], axis=AX.X, op=ALU.max), reads=[R1], writes=[R1])
            f.op("dve", lambda e: e.tensor_reduce(out=thr[:], in_=pm[:], axis=AX.X, op=ALU.max), reads=[R1], writes=[R1])
            f.op("dve", lambda e: e.tensor_reduce(out=gm[:, 0:1], in_=gs[:], axis=AX.X, op=ALU.max), reads=[R1], writes=[R1])
            f.op("dve", lambda e: e.tensor_scalar(out=mg[:], in0=gs[:], scalar1=gm[:, 0:1], scalar2=None, op0=ALU.is_ge), reads=[R1], writes=[R1])
            f.op("dve", lambda e: e.tensor_tensor(sm[:], s3, thr[:].unsqueeze(2).broadcast_to([128, 8, 4]), ALU.is_ge), reads=[R1], writes=[R1])
            f.op("dve", lambda e: e.tensor_tensor(sm[:], sm[:], mg[:].unsqueeze(2).broadcast_to([128, 8, 4]), ALU.mult), reads=[R1], writes=[R1])
            cj = comb[:, j, :]
            f.op("dve", lambda e: e.tensor_tensor(cj, sm[:].rearrange("p g e -> p (g e)"), sc[:], ALU.mult), reads=[R1], writes=[R_comb[j]])
            f.op("dve", lambda e: e.tensor_reduce(out=gm[:, 1:2], in_=cj, axis=AX.X, op=ALU.add), reads=[R_comb[j], R1], writes=[R1])
            f.op("dve", lambda e: e.reciprocal(gm[:, 1:2], gm[:, 1:2]), reads=[R1], writes=[R1])
            f.op("dve", lambda e: e.tensor_scalar(out=cj, in0=cj, scalar1=gm[:, 1:2], scalar2=None, op0=ALU.mult), reads=[R1, R_comb[j]], writes=[R_comb[j]])
        A.close()
        Bq = Scope(nc)
        m_ = Bq.sb("m_", [128, ng, 32]); mb16 = Bq.sb("mb16", [128, ng, 32], BF16)
        rank = Bq.sb("rank", [128, ng, 32]); tot = Bq.sb("tot", [128, ng, 32]); base = Bq.sb("base", [128, ng, 32])
        me = Bq.sb("me", [128, ng, 32]); Bm = Bq.sb("Bm", [128, ng, 32]); Am = Bq.sb("Am", [128, ng, 32]); tmpq = Bq.sb("tmpq", [128, ng, 32])
        ones16 = Bq.sb("ones16", [128, 128], BF16)
        cnt = Bq.sb("cnt", [128, 32]); cmp17 = Bq.sb("cmp17", [128, 32, 18]); thr18 = Bq.sb("thr18", [128, 18])
        tlf = Bq.sb("tlf", [128, 32]); sinc = Bq.sb("sinc", [128, 32]); so512 = Bq.sb("so512", [128, 32]); c1e = Bq.sb("c1e", [128, 32])
        mx = Bq.sb("mx", [128, ng]); pAf = Bq.sb("pAf", [128, ng]); pBf = Bq.sb("pBf", [128, ng])
        cmpj = Bq.sb("cmpj", [128, NSO, 32]); eidf = Bq.sb("eidf", [128, NSO]); idxf = Bq.sb("idxf", [128, NSO, 4])
        ps_rk = Bq.ps("ps_rk", [128, 3, 512]); ps_tt = Bq.ps("ps_tt", [128, 3, 512])
        RB = [R_rt]

        def fl(ap3):
            return ap3.rearrange("p a b -> p (a b)")
        f.op("dve", lambda e: e.tensor_scalar(out=fl(m_[:]), in0=fl(comb[:]), scalar1=0.0, scalar2=None, op0=ALU.is_gt), reads=R_comb, writes=RB)
        f.op("dve", lambda e: e.tensor_copy(fl(mb16[:]), fl(m_[:])), reads=RB, writes=RB)
        f.op("pool", lambda e: e.memset(ones16[:], 1.0), reads=RB, writes=RB)
        chunks = [(n0, min(M, n0 + 512)) for n0 in range(0, M, 512)]
        for ch, (n0, n1) in enumerate(chunks):
            f.op("pe", lambda e, ch=ch, n0=n0, n1=n1: e.matmul(ps_rk[:, ch, 0:n1 - n0], maskb[:, 2, :], fl(mb16[:])[:, n0:n1], start=True, stop=True), reads=RB + [R_mask], writes=RB)
            f.op("pe", lambda e, ch=ch, n0=n0, n1=n1: e.matmul(ps_tt[:, ch, 0:n1 - n0], ones16[:], fl(mb16[:])[:, n0:n1], start=True, stop=True), reads=RB, writes=RB)
        for ch, (n0, n1) in enumerate(chunks):
            f.op("dve", lambda e, ch=ch, n0=n0, n1=n1: e.tensor_copy(fl(rank[:])[:, n0:n1], ps_rk[:, ch, 0:n1 - n0]), reads=RB, writes=RB)
            f.op("dve", lambda e, ch=ch, n0=n0, n1=n1: e.tensor_copy(fl(tot[:])[:, n0:n1], ps_tt[:, ch, 0:n1 - n0]), reads=RB, writes=RB)
        f.op("dve", lambda e: e.memset(base[:, 0, :], 0.0), reads=RB, writes=RB)
        for t_ in range(1, ng):
            f.op("dve", lambda e, t_=t_: e.tensor_tensor(base[:, t_, :], base[:, t_ - 1, :], tot[:, t_ - 1, :], ALU.add), reads=RB, writes=RB)
        f.op("dve", lambda e: e.tensor_tensor(cnt[:], base[:, ng - 1, :], tot[:, ng - 1, :], ALU.add), reads=RB, writes=RB)
        f.op("dve", lambda e: e.tensor_scalar(out=thr18[:], in0=jt[:, 0:18], scalar1=512.0, scalar2=None, op0=ALU.mult), reads=RB + [R_rw], writes=RB)
        f.op("dve", lambda e: e.tensor_tensor(cmp17[:], cnt[:].unsqueeze(2).broadcast_to([128, 32, 18]), thr18[:].unsqueeze(1).broadcast_to([128, 32, 18]), ALU.is_gt), reads=RB, writes=RB)
        f.op("dve", lambda e: e.tensor_reduce(out=tlf[:], in_=cmp17[:], axis=AX.X, op=ALU.add), reads=RB, writes=RB)
        f.op("dve", lambda e: e.tensor_scalar(out=tlf[:], in0=tlf[:], scalar1=-1.0, scalar2=0.0, op0=ALU.add, op1=ALU.max), reads=RB, writes=RB)
        f.op("dve", lambda e: e.tensor_copy(sinc[:], tlf[:]), reads=RB, writes=RB)
        for e_ in range(1, 32):
            f.op("dve", lambda e, e_=e_: e.tensor_tensor(sinc[:, e_:e_ + 1], sinc[:, e_ - 1:e_], tlf[:, e_:e_ + 1], ALU.add), reads=RB, writes=RB)
        f.op("dve", lambda e: e.tensor_tensor(so512[:], sinc[:], tlf[:], ALU.subtract), reads=RB, writes=RB)
        f.op("dve", lambda e: e.tensor_scalar(out=so512[:], in0=so512[:], scalar1=512.0, scalar2=15872.0, op0=ALU.mult, op1=ALU.add), reads=RB, writes=RB)
        f.op("dve", lambda e: e.tensor_scalar(out=c1e[:], in0=jt[:, 0:32], scalar1=512.0, scalar2=None, op0=ALU.mult), reads=RB + [R_rw], writes=RB)
        f.op("dve", lambda e: e.tensor_tensor(so512[:], so512[:], c1e[:], ALU.subtract), reads=RB, writes=RB)
        f.op("dve", lambda e: e.tensor_tensor(fl(rank[:]), fl(rank[:]), fl(base[:]), ALU.add), reads=RB, writes=RB)
        f.op("dve", lambda e: e.tensor_scalar(out=fl(tmpq[:]), in0=fl(rank[:]), scalar1=512.0, scalar2=None, op0=ALU.is_ge), reads=RB, writes=RB)
        f.op("dve", lambda e: e.tensor_tensor(tmpq[:], tmpq[:], so512[:].unsqueeze(1).broadcast_to([128, ng, 32]), ALU.mult), reads=RB, writes=RB)
        f.op("dve", lambda e: e.tensor_tensor(rank[:], rank[:], c1e[:].unsqueeze(1).broadcast_to([128, ng, 32]), ALU.add), reads=RB, writes=RB)
        f.op("dve", lambda e: e.tensor_tensor(fl(rank[:]), fl(rank[:]), fl(tmpq[:]), ALU.add), reads=RB, writes=RB)
        f.op("dve", lambda e: e.tensor_tensor(me[:], m_[:], jt[:, 1:33].unsqueeze(1).broadcast_to([128, ng, 32]), ALU.mult), reads=RB, writes=RB)
        f.op("dve", lambda e: e.tensor_reduce(out=mx[:], in_=me[:], axis=AX.X, op=ALU.max), reads=RB, writes=RB)
        f.op("dve", lambda e: e.tensor_tensor(Bm[:], me[:], mx[:].unsqueeze(2).broadcast_to([128, ng, 32]), ALU.is_equal), reads=RB, writes=RB)
        f.op("dve", lambda e: e.tensor_tensor(fl(Am[:]), fl(m_[:]), fl(Bm[:]), ALU.subtract), reads=RB, writes=RB)
        for (msk, pf, wf) in ((Am, pAf, wA), (Bm, pBf, wB)):
            f.op("dve", lambda e, msk=msk: e.tensor_tensor(fl(tmpq[:]), fl(msk[:]), fl(rank[:]), ALU.mult), reads=RB, writes=RB)
            f.op("dve", lambda e, pf=pf: e.tensor_reduce(out=pf[:], in_=tmpq[:], axis=AX.X, op=ALU.add), reads=RB, writes=RB)
            f.op("dve", lambda e, msk=msk: e.tensor_tensor(fl(tmpq[:]), fl(msk[:]), fl(comb[:]), ALU.mult), reads=RB + R_comb, writes=RB)
            f.op("dve", lambda e, wf=wf: e.tensor_reduce(out=wf[:], in_=tmpq[:], axis=AX.X, op=ALU.add), reads=RB, writes=RB)
        f.op("dve", lambda e: e.tensor_copy(posA_i[:], pAf[:]), reads=RB, writes=RB)
        f.op("dve", lambda e: e.tensor_copy(posB_i[:], pBf[:]), reads=RB, writes=RB)
        f.op("dve", lambda e: e.tensor_tensor(cmpj[:], sinc[:].unsqueeze(1).broadcast_to([128, NSO, 32]), jt[:, 0:NSO].unsqueeze(2).broadcast_to([128, NSO, 32]), ALU.is_le), reads=RB, writes=RB)
        f.op("dve", lambda e: e.tensor_reduce(out=eidf[:], in_=cmpj[:], axis=AX.X, op=ALU.add), reads=RB, writes=RB)
        f.op("dve", lambda e: e.tensor_scalar(out=eidf[:], in0=eidf[:], scalar1=32.0, scalar2=512.0, op0=ALU.min, op1=ALU.mult), reads=RB, writes=RB)
        f.op("dve", lambda e: e.tensor_scalar(out=eidf[:], in0=eidf[:], scalar1=float(layer * 16384), scalar2=None, op0=ALU.add), reads=RB, writes=RB)
        f.op("dve", lambda e: e.tensor_tensor(idxf[:], eidf[:].unsqueeze(2).broadcast_to([128, NSO, 4]), pc[:].unsqueeze(1).broadcast_to([128, NSO, 4]), ALU.add), reads=RB, writes=RB)
        f.op("dve", lambda e: e.tensor_copy(idxw[:], idxf[:]), reads=RB, writes=RB)
        for j in range(ng):
            for pi_ in (posA_i, posB_i):
                f._dma_common("pool", lambda e, j=j, pi_=pi_: e.indirect_dma_start(out=XS, out_offset=IOA(ap=pi_[:, j:j + 1], axis=0), in_=hb_all[:, j, :], in_offset=None),
                              [R_hb[j]] + RB + R_XsZ, [R_XsW[j]])
        Bq.close()
        HB.close()
        Sd = Scope(nc)
        wg = [Sd.sb("wg%d" % k, [128, 4, 2, 512], BF16) for k in range(2)]
        wu = [Sd.sb("wu%d" % k, [128, 4, 2, 512], BF16) for k in range(2)]
        wd = [Sd.sb("wd%d" % k, [128, 4, D], BF16) for k in range(2)]
        R_w = RL(2, "w")
        xs = [[Sd.sb("xs%d_%d" % (a_, tt), [128, D], BF16) for tt in range(4)] for a_ in range(2)]
        R_xs = [RL(4, "xs%d" % a_) for a_ in range(2)]

        def xs_load(jn):
            for tt in range(4):
                r0 = (jn * 4 + tt) * 128
                f.dma("sp", xs[jn % 2][tt][:], XS[r0:r0 + 128, :], reads=R_XsW, writes=[R_xs[jn % 2][tt]])
        xT = [Sd.sb("xT%d" % k, [128, 8, 512], BF16) for k in range(2)]; R_xT = RL(2)
        ps_t = Sd.ps("ps_t", [128, 8, 128], BF16); R_pst = Res()
        psg = [Sd.ps("psg%d" % k, [128, 512]) for k in range(2)]; R_psg = RL(2)
        psu = [Sd.ps("psu%d" % k, [128, 512]) for k in range(2)]; R_psu = RL(2)
        psd = Sd.ps("psd", [128, D]); R_psd = Res()
        sg = [Sd.sb("sg%d" % k, [128, 512]) for k in range(2)]; R_sg = RL(2)
        hid = [Sd.sb("hid%d" % k, [128, 4, 512], BF16) for k in range(2)]; R_hid = RL(2)
        ysb = [Sd.sb("ysb%d" % k, [128, D]) for k in range(2)]; R_ysb = RL(2)
        nfc = 0; nx = 0; ny = 0
        bc_reg = nc.gpsimd.alloc_register("bc%d" % layer)
        nc.gpsimd.reg_mov(bc_reg, 16383 + layer * 16384)
        stg_g = Sd.sb("stg_g", [128, 4, 2, 512]); stg_u = Sd.sb("stg_u", [128, 4, 2, 512]); stg_d = Sd.sb("stg_d", [128, 4, D])
        R_sg_ = Res("stg_g"); R_su_ = Res("stg_u"); R_sd_ = Res("stg_d")

        def w_load(ex):
            f.dma("sp", stg_g[:], w_gate[layer, ex].rearrange("(c q j) n -> q c j n", c=4, j=2), writes=[R_sg_])
            f.dma("sp", stg_u[:], w_up[layer, ex].rearrange("(c q j) n -> q c j n", c=4, j=2), writes=[R_su_])
            f.dma("sp", stg_d[:], w_down[layer, ex].rearrange("(c p) n -> p c n", p=128), writes=[R_sd_])

        def w_cast(ex):
            wb_ = ex % 2
            f.op("act", lambda e: e.activation(out=wg[wb_][:], in_=stg_g[:], func=AF.Identity), reads=[R_sg_], writes=[R_w[wb_]])
            f.op("dve", lambda e: e.tensor_copy(wu[wb_][:], stg_u[:]), reads=[R_su_], writes=[R_w[wb_]])
            f.op("act", lambda e: e.activation(out=wd[wb_][:], in_=stg_d[:], func=AF.Identity), reads=[R_sd_], writes=[R_w[wb_]])
        xs_load(0)
        w_load(0)
        w_cast(0)
        for j in range(NS):
            wb = j % 2
            if j + 1 < NS:
                xs_load(j + 1)
            if j + 1 < 32:
                w_load(j + 1)
            if j >= 32:
                jo = j - 32
                for c in range(4):
                    for (wt_, rows_) in ((wg, wg_rows), (wu, wu_rows)):
                        f._dma_common("pool", lambda e, c=c, wt_=wt_, rows_=rows_: e.indirect_dma_start(out=wt_[wb][:, c, :, :].rearrange("p a n -> p (a n)"), out_offset=None, in_=rows_[layer],
                                                                                                   in_offset=IOA(ap=idxw[:, jo, c:c + 1], axis=0), bounds_check=bc_reg, oob_is_err=False),
                                      RB, [R_w[wb]])
                for c in range(4):
                    f._dma_common("pool", lambda e, c=c: e.indirect_dma_start(out=wd[wb][:, c, :], out_offset=None, in_=wd_rows[layer], in_offset=IOA(ap=idxw[:, jo, c:c + 1], axis=0), bounds_check=bc_reg, oob_is_err=False),
                                  RB, [R_w[wb]])
            for tt in range(4):
                for kc in range(8):
                    f.op("pe", lambda e, kc=kc, tt=tt: e.transpose(ps_t[:, kc, :], xs[j % 2][tt][:, kc * 128:(kc + 1) * 128], identb[:]), reads=[R_xs[j % 2][tt], R_identb], writes=[R_pst], acc=(kc > 0))
                f.op("act", lambda e, tt=tt: e.activation(out=xT[wb][:, :, tt * 128:(tt + 1) * 128], in_=ps_t[:], func=AF.Identity), reads=[R_pst], writes=[R_xT[wb]])
            hb_ = j % 2
            for fc in range(4):
                pb = nfc % 2; nfc += 1
                for kc in range(8):
                    f.op("pe", lambda e, kc=kc, fc=fc: e.matmul(psg[pb][:], wg[wb][:, kc // 2, kc % 2, fc * 128:(fc + 1) * 128], xT[wb][:, kc, :], start=(kc == 0), stop=(kc == 7)),
                         reads=[R_w[wb], R_xT[wb]], writes=[R_psg[pb]], acc=(kc > 0))
                for kc in range(8):
                    f.op("pe", lambda e, kc=kc, fc=fc: e.matmul(psu[pb][:], wu[wb][:, kc // 2, kc % 2, fc * 128:(fc + 1) * 128], xT[wb][:, kc, :], start=(kc == 0), stop=(kc == 7)),
                         reads=[R_w[wb], R_xT[wb]], writes=[R_psu[pb]], acc=(kc > 0))
                f.op("act", lambda e: e.activation(out=sg[pb][:], in_=psg[pb][:], func=AF.Silu), reads=[R_psg[pb]], writes=[R_sg[pb]])
                f.op("dve", lambda e, fc=fc: e.tensor_tensor(hid[hb_][:, fc, :], sg[pb][:], psu[pb][:], ALU.mult), reads=[R_sg[pb], R_psu[pb]], writes=[R_hid[hb_]])
            for tt in range(4):
                yb_ = ny % 2; ny += 1
                for half in range(2):
                    for fc in range(4):
                        f.op("pe", lambda e, fc=fc, half=half, tt=tt: e.matmul(psd[:, half * 512:(half + 1) * 512], hid[hb_][:, fc, tt * 128:(tt + 1) * 128], wd[wb][:, fc, half * 512:(half + 1) * 512], start=(fc == 0), stop=(fc == 3)),
                             reads=[R_w[wb], R_hid[hb_]], writes=[R_psd], acc=(half + fc > 0))
                f.op("dve", lambda e: e.tensor_copy(ysb[yb_][:], psd[:]), reads=[R_psd], writes=[R_ysb[yb_]])
                r0 = (j * 4 + tt) * 128
                f.dma("act", YS[r0:r0 + 128, :], ysb[yb_][:], reads=[R_ysb[yb_]], writes=[R_Ys[j]])
            if j + 1 < 32:
                w_cast(j + 1)
        Sd.close()
        nc.gpsimd.free_register(bc_reg)
        C = Scope(nc)
        xt = [C.sb("xt%d" % k, [128, D]) for k in range(2)]; R_xt = RL(2)
        ot = [C.sb("ot%d" % k, [128, D]) for k in range(2)]; R_ot = RL(2)
        ya = [C.sb("ya%d" % k, [128, D]) for k in range(2)]; R_ya = RL(2)
        yb2 = [C.sb("yb%d" % k, [128, D]) for k in range(2)]; R_yb = RL(2)
        tmp = C.sb("tmp", [128, D]); R_tmp = Res()
        small = C.sb("small", [128, 16]); R_small = Res()

        class _V:
            def __init__(self, ap): self.ap = ap
            def __getitem__(self, k): return self.ap
        for j, t in enumerate(tiles_all):
            b = j % 2
            f.dma("sp", xt[b][:], XR[t * 128:(t + 1) * 128, :], reads=[R_XR[t]], writes=[R_xt[b]])
            f._dma_common("pool", lambda e: e.indirect_dma_start(out=ya[b][:], out_offset=None, in_=YS, in_offset=IOA(ap=posA_i[:, j:j + 1], axis=0)), R_Ys + RB, [R_ya[b]])
            f._dma_common("pool", lambda e: e.indirect_dma_start(out=yb2[b][:], out_offset=None, in_=YS, in_offset=IOA(ap=posB_i[:, j:j + 1], axis=0)), R_Ys + RB, [R_yb[b]])
            f.op("dve", lambda e: e.tensor_scalar(out=ya[b][:], in0=ya[b][:], scalar1=wA[:, j:j + 1], scalar2=None, op0=ALU.mult), reads=[R_ya[b]] + RB, writes=[R_ya[b]])
            f.op("dve", lambda e: e.scalar_tensor_tensor(out=ya[b][:], in0=yb2[b][:], scalar=wB[:, j:j + 1], in1=ya[b][:], op0=ALU.mult, op1=ALU.add), reads=[R_yb[b], R_ya[b]] + RB, writes=[R_ya[b]])
            resid_ln(C, xt[b], R_xt[b], _V(ya[b][:]), R_ya[b], (1 if t < 2 else 0), layer * 2 + 1, ot[b], R_ot[b], tmp, R_tmp, small, R_small)
            if final:
                f.dma("act", out_d[(t - 2) * 128:(t - 1) * 128, :], ot[b][:], reads=[R_ot[b]], writes=[R_out])
            else:
                f.dma("act", XR[t * 128:(t + 1) * 128, :], ot[b][:], reads=[R_ot[b]], writes=[R_XR[t]])
        C.close()
        P.close()

    def layer1_mixer():
        L = Scope(nc)
        kT2 = L.sb("kT2b", [128, 4, NTOK], BF16); R_kT = RL(NT, "kT")
        vaug = L.sb("vaugb", [128, NT, 576], BF16); R_v = RL(NT, "v")
        f.op("pool", lambda e: e.memset(vaug[:], 1.0), writes=R_v)
        R_oT = RL(NT, "oT")
        R_QT = RL(NT, "QT")
        S = Scope(nc)
        win = S.sb("win1", [128, 8, 1536], BF16); R_win = Res()
        f.dma("pool", win[:], odd_w_in.rearrange("(kc p) n -> p kc n", p=128), writes=[R_win])
        gq = S.sb("gq", [128, 2, 64]); R_gq = Res()
        f.dma("sp", gq[:, 0, :], q_norm.partition_broadcast(128), writes=[R_gq])
        f.dma("sp", gq[:, 1, :], k_norm.partition_broadcast(128), writes=[R_gq])
        xt = [S.sb("xt%d" % k, [128, D]) for k in range(2)]; R_xt = RL(2)
        h32 = S.sb("h32", [128, D]); R_h32 = Res()
        hT = [S.sb("hT%d" % k, [128, 8, 128], BF16) for k in range(2)]; R_hT = RL(2)
        ps_tp = S.ps("ps_tp", [128, 8, 128]); R_pstp = Res(x=True)
        ps_q = S.ps("ps_q", [128, 1536]); R_psq = Res(x=True)
        ps_t = S.ps("ps_t", [128, 16, 128], BF16); R_pst = Res(x=True)
        qk = S.sb("qk", [128, 20, 64]); R_qk = Res()
        sq = S.sb("sq", [128, 20, 64]); ss = S.sb("ss", [128, 20]); R_ss = Res()
        ra = S.sb("ra", [128, 20, 32]); rb = S.sb("rb", [128, 20, 32]); R_ra = Res(); R_rb = Res()
        tqk = S.sb("tqk", [128, 20, 64], BF16); R_tqk = Res()
        kd = S.sb("kd", [128, 4, 2, 64], BF16); R_kd = Res()
        qts = [S.sb("qts%d" % k, [128, 8, 128], BF16) for k in range(2)]; R_qts = RL(2)
        for t in range(NT):
            b = t % 2
            f.dma("sp", xt[b][:], XR[t * 128:(t + 1) * 128, :], reads=[R_XR[t]], writes=[R_xt[b]])
            which = 1 if t < 2 else 0
            mod_transpose(xt[b], R_xt[b], which, h32, R_h32, ps_tp, R_pstp, hT[b][:], R_hT[b])
            cols = slice(t * 128, (t + 1) * 128)
            lat = t >= 2
            ranges = ((0, 512), (512, 1024), (1024, 1536)) if lat else ((1024, 1536),)
            first = True
            for (n0, n1) in ranges:
                for kc in range(8):
                    f.op("pe", lambda e, kc=kc, n0=n0, n1=n1: e.matmul(ps_q[:, n0:n1], hT[b][:, kc, :], win[:, kc, n0:n1], start=(kc == 0), stop=(kc == 7)),
                         reads=[R_win, R_hT[b]], writes=[R_psq], acc=(not first))
                    first = False
            h0 = 0 if lat else 16
            nh = 20 - h0
            pv = ps_q[:, h0 * 64:1280].rearrange("p (h d) -> p h d", d=64)
            qkv_ = qk[:, h0:20, :]
            f.op("act", lambda e: e.activation(out=sq[:, h0:20, :], in_=pv, func=AF.Square), reads=[R_psq], writes=[R_ss])
            f.op("dve", lambda e: e.tensor_reduce(out=ss[:, h0:20], in_=sq[:, h0:20, :], axis=AX.X, op=ALU.add), reads=[R_ss], writes=[R_ss])
            f.op("dve", lambda e: e.tensor_scalar(out=ss[:, h0:20], in0=ss[:, h0:20], scalar1=1.0 / 64.0, scalar2=RMS_EPS, op0=ALU.mult, op1=ALU.add), reads=[R_ss], writes=[R_ss])
            f.op("act", lambda e: e.activation(out=ss[:, h0:20], in_=ss[:, h0:20], func=AF.Sqrt), reads=[R_ss], writes=[R_ss])
            f.op("dve", lambda e: e.reciprocal(ss[:, h0:20], ss[:, h0:20]), reads=[R_ss], writes=[R_ss])
            f.op("dve", lambda e: e.tensor_tensor(qkv_, pv, ss[:, h0:20].unsqueeze(2).broadcast_to([128, nh, 64]), ALU.mult), reads=[R_psq, R_ss], writes=[R_qk])
            if lat:
                f.op("pool", lambda e: e.tensor_tensor(qk[:, 0:16, :], qk[:, 0:16, :], gq[:, 0, :].unsqueeze(1).broadcast_to([128, 16, 64]), ALU.mult), reads=[R_qk, R_gq], writes=[R_qk])
            f.op("pool", lambda e: e.tensor_tensor(qk[:, 16:20, :], qk[:, 16:20, :], gq[:, 1, :].unsqueeze(1).broadcast_to([128, 4, 64]), ALU.mult), reads=[R_qk, R_gq], writes=[R_qk])
            if lat:
                q4 = qk[:].rearrange("p h (two f) -> p h two f", two=2)
                o4 = tqk[:].rearrange("p h (two f) -> p h two f", two=2)
                cosb = rope[:, 0, t - 2, :].unsqueeze(1).broadcast_to([128, 20, 32])
                sinb = rope[:, 1, t - 2, :].unsqueeze(1).broadcast_to([128, 20, 32])
                f.op("dve", lambda e: e.tensor_tensor(ra[:], q4[:, :, 0, :], cosb, ALU.mult), reads=[R_qk, R_rope], writes=[R_ra])
                f.op("pool", lambda e: e.tensor_tensor(rb[:], q4[:, :, 1, :], sinb, ALU.mult), reads=[R_qk, R_rope], writes=[R_rb])
                f.op("dve", lambda e: e.tensor_tensor(o4[:, :, 0, :], ra[:], rb[:], ALU.subtract), reads=[R_ra, R_rb], writes=[R_tqk])
                f.op("dve", lambda e: e.tensor_tensor(ra[:], q4[:, :, 1, :], cosb, ALU.mult), reads=[R_qk, R_rope, R_tqk], writes=[R_ra])
                f.op("pool", lambda e: e.tensor_tensor(rb[:], q4[:, :, 0, :], sinb, ALU.mult), reads=[R_qk, R_rope, R_tqk], writes=[R_rb])
                f.op("dve", lambda e: e.tensor_tensor(o4[:, :, 1, :], ra[:], rb[:], ALU.add), reads=[R_ra, R_rb], writes=[R_tqk])
            else:
                f.op("dve", lambda e: e.tensor_copy(tqk[:, 16:20, :], qk[:, 16:20, :]), reads=[R_qk], writes=[R_tqk])
            for a in range(4):
                f.op("dve", lambda e, a=a: e.tensor_copy(vaug[:, t, 64 + 128 * a:128 + 128 * a], ps_q[:, 1280 + 64 * a:1344 + 64 * a]), reads=[R_psq], writes=[R_v[t]])
            f.op("dve", lambda e: e.tensor_copy(kd[:, :, 0, :], tqk[:, 16:20, :]), reads=[R_tqk], writes=[R_kd])
            f.op("dve", lambda e: e.tensor_copy(kd[:, :, 1, :], tqk[:, 16:20, :]), reads=[R_tqk], writes=[R_kd])
            firstt = True
            if lat:
                for pr in range(8):
                    f.op("pe", lambda e, pr=pr: e.transpose(ps_t[:, pr, :], tqk[:, 2 * pr:2 * pr + 2, :].rearrange("p a d -> p (a d)"), identb[:]),
                         reads=[R_tqk, R_identb], writes=[R_pst], acc=(not firstt))
                    firstt = False
            for a in range(4):
                f.op("pe", lambda e, a=a: e.transpose(ps_t[:, 8 + a, :], kd[:, a, :, :].rearrange("p a d -> p (a d)"), identb[:]),
                     reads=[R_kd, R_identb], writes=[R_pst], acc=(not firstt))
                firstt = False
            f.op("act", lambda e: e.activation(out=kT2[:, :, cols], in_=ps_t[:, 8:12, :], func=AF.Identity), reads=[R_pst], writes=[R_kT[t]])
            if lat:
                f.op("dve", lambda e: e.tensor_copy(qts[b][:], ps_t[:, 0:8, :]), reads=[R_pst], writes=[R_qts[b]])
                f.dma("act", QT[:, :, (t - 2) * 128:(t - 1) * 128].rearrange("a p n -> p a n"), qts[b][:], reads=[R_qts[b]], writes=[R_QT[t]])
        S.close()
        S = Scope(nc)
        qb = [S.sb("qb%d" % k, [128, 2, 512], BF16) for k in range(2)]; R_qb = RL(2)
        ps_s = [S.ps("ps_s%d" % k, [128, 1024]) for k in range(2)]; R_pss = RL(2)
        ps_o = [S.ps("ps_o%d" % k, [128, 512]) for k in range(4)]; R_pso = RL(4)
        pT = [S.sb("pT%d" % k, [128, 1024], BF16) for k in range(3)]; R_pT = RL(3)
        dtmp = S.sb("dtmp", [128, 512]); R_dt = Res()
        ost = [S.sb("ost%d" % k, [128, 2, 512], BF16) for k in range(2)]; R_ost = RL(2)
        it = 0
        nq = 0
        for kvh in range(4):
            for qblk in range(8):
                qbi = nq % 2; nq += 1
                tq = [R_QT[2 + qblk * 4 + k] for k in range(4)]
                f.dma("sp", qb[qbi][:], QT[2 * kvh:2 * kvh + 2, :, qblk * 512:(qblk + 1) * 512].rearrange("a p n -> p a n"), reads=tq, writes=[R_qb[qbi]])
                items = [(kt, p_) for kt in range(NT) for p_ in range(2)]
                it0 = it; it += len(items)

                def front(idx):
                    kt, p_ = items[idx]
                    si = (it0 + idx) % 2
                    pi_ = (it0 + idx) % 3
                    for half in range(2):
                        base = 64 * half
                        f.op("pe", lambda e, half=half, base=base: e.matmul(ps_s[si][:, half * 512:(half + 1) * 512], kT2[base:base + 64, kvh, kt * 128:(kt + 1) * 128], qb[qbi][base:base + 64, p_, :], start=True, stop=True),
                             reads=[R_kT[kt], R_qb[qbi]], writes=[R_pss[si]], acc=(half > 0))
                    f.op("act", lambda e: e.activation(out=pT[pi_][:], in_=ps_s[si][:], func=AF.Exp, scale=0.125), reads=[R_pss[si]], writes=[R_pT[pi_]])

                def back(idx):
                    kt, p_ = items[idx]
                    pi_ = (it0 + idx) % 3
                    for half in range(2):
                        hh = 2 * p_ + half
                        voff = (64 if half == 0 else 0) + 128 * kvh
                        f.op("pe", lambda e, half=half, hh=hh, voff=voff: e.matmul(ps_o[hh][:], vaug[:, kt, voff:voff + 128], pT[pi_][:, half * 512:(half + 1) * 512], start=(kt == 0), stop=(kt == NT - 1)),
                             reads=[R_v[kt], R_pT[pi_]], writes=[R_pso[hh]], acc=(kt > 0))
                LA = 1
                for i_ in range(len(items) + LA):
                    if i_ < len(items):
                        front(i_)
                    if i_ >= LA:
                        back(i_ - LA)
                for hh in range(4):
                    h = kvh * 4 + hh
                    nb, db = (0, 64) if h % 2 == 0 else (64, 0)
                    f.op("dve", lambda e: e.reciprocal(dtmp[nb:nb + 64, :], ps_o[hh][db:db + 64, :]), reads=[R_pso[hh]], writes=[R_dt])
                    f.op("dve", lambda e: e.tensor_tensor(ost[qbi][nb:nb + 64, hh // 2, :], ps_o[hh][nb:nb + 64, :], dtmp[nb:nb + 64, :], ALU.mult),
                         reads=[R_pso[hh], R_dt], writes=[R_ost[qbi]])
                f.dma("act", OT[2 * kvh:2 * kvh + 2, :, qblk * 512:(qblk + 1) * 512].rearrange("a p n -> p a n"), ost[qbi][:], reads=[R_ost[qbi]],
                      writes=[R_oT[2 + qblk * 4 + k] for k in range(4)])
        S.close()
        L.close()
        M = Scope(nc)
        mixt = [M.sb("mixt%d" % k, [128, 8, 128], BF16) for k in range(2)]; R_mixt = RL(2)

        def mix_loader(t, b):
            f.dma("sp", mixt[b][:], OT[:, :, (t - 2) * 128:(t - 1) * 128].rearrange("a p n -> p a n"), reads=[R_oT[t]], writes=[R_mixt[b]])
            return [mixt[b][:, k, :] for k in range(8)], [R_mixt[b]]
        out_phase(1, odd_w_out, mix_loader, range(2, NT))
        M.close()

    phase_mod(0, 0)
    if stop_after == "mod":
        f.dma("sp", dbg[0:128, :], mod[:, 0].rearrange("p a d -> p (a d)"), reads=[R_mod], writes=[R_dbg])
        f.dma("sp", dbg[128:256, :], mod[:, 1].rearrange("p a d -> p (a d)"), reads=[R_mod], writes=[R_dbg])
    else:
        layer0_mixer()
    if stop_after in ("in0", "s5", "mod", "h0", "qkv", "win"):
        pass
    else:
        if stop_after == "mix0":
            pass
        else:
            phase_mod(0, 1)
            (ffn_phase if 'dense' in DBG_SKIP else ffn_sparse)(0, list(range(NT)), final=False)
            if stop_after != "l0":
                phase_mod(1, 0)
                layer1_mixer()
                if stop_after != "mix1":
                    phase_mod(1, 1)
                    (ffn_phase if 'dense' in DBG_SKIP else ffn_sparse)(1, list(range(2, NT)), final=True)
    if stop_after is not None and stop_after not in ("in0", "s5", "mod", "h0", "qkv", "win"):
        S = Scope(nc)
        tt = S.sb("dumpt", [128, D]); R_t = Res()
        for t in range(NT):
            f.dma("sp", tt[:], XR[t * 128:(t + 1) * 128, :], reads=[R_XR[t]], writes=[R_t])
            f.dma("sp", dbg[t * 128:(t + 1) * 128, :], tt[:], reads=[R_t], writes=[R_dbg])
        S.close()
    f.finish()
    Scope.FWREF = None
    G.close()
    f.close()
    return nc


_CONST = None


def _consts():
    global _CONST
    if _CONST is None:
        ident = np.eye(128, dtype=np.float32)
        n_freq = 16
        inv_freq = (10000.0 ** (-np.arange(n_freq, dtype=np.float32) / n_freq)).astype(np.float32)
        pos = np.arange(4096)
        r = (pos // 64).astype(np.float32); cc = (pos % 64).astype(np.float32)
        ang = np.concatenate([r[:, None] * inv_freq, cc[:, None] * inv_freq], -1).astype(np.float32)
        cos = np.cos(ang).astype(np.float32).reshape(32, 128, 32).transpose(1, 0, 2)
        sin = np.sin(ang).astype(np.float32).reshape(32, 128, 32).transpose(1, 0, 2)
        rope = np.ascontiguousarray(np.stack([cos, sin], axis=1))
        k = np.arange(128)[:, None]; q = np.arange(128)[None, :]
        mask = np.stack([(q <= k), (k <= q), (k < q)], axis=1).astype(np.float32)
        pc = (np.arange(128, dtype=np.float32)[:, None] + 128.0 * np.arange(4, dtype=np.float32)[None, :]).astype(np.float32)
        jidx = np.broadcast_to(np.arange(128, dtype=np.float32)[None, :], (128, 128)).copy()
        _CONST = {"k_ident": ident, "k_rope": rope, "k_mask": np.ascontiguousarray(mask), "k_jidx": jidx, "k_pc": np.ascontiguousarray(pc)}
    return _CONST


def make_in_map(inputs, b):
    f32 = lambda a: np.ascontiguousarray(np.asarray(a, dtype=np.float32))
    m = {
        "x": f32(inputs["x"][b]), "ctx": f32(inputs["ctx"][b]), "c": f32(inputs["c"][b:b + 1]),
        "c_ctx": f32(inputs["c_ctx"]).reshape(1, D),
        "ada_w": f32(inputs["ada_w"]), "ada_b": f32(inputs["ada_b"]), "ln_g": f32(inputs["ln_g"]), "ln_b": f32(inputs["ln_b"]),
        "even_w_in": f32(inputs["even_w_in"][0]), "even_w_out": f32(inputs["even_w_out"][0]),
        "s5_lam_re": f32(inputs["s5_lam_re"][0]), "s5_lam_im": f32(inputs["s5_lam_im"][0]), "s5_log_step": f32(inputs["s5_log_step"][0]),
        "s5_b_re": f32(inputs["s5_b_re"][0]), "s5_b_im": f32(inputs["s5_b_im"][0]),
        "s5_c_re": f32(inputs["s5_c_re"][0]), "s5_c_im": f32(inputs["s5_c_im"][0]),
        "s5_d": f32(inputs["s5_d"][0]), "s5_w_glu": f32(inputs["s5_w_glu"][0]), "s5_b_glu": f32(inputs["s5_b_glu"][0]),
        "win_sink": f32(inputs["win_sink"][0]),
        "odd_w_in": f32(inputs["odd_w_in"][0]), "odd_w_out": f32(inputs["odd_w_out"][0]),
        "odd_q_norm": f32(inputs["odd_q_norm"][0]), "odd_k_norm": f32(inputs["odd_k_norm"][0]),
        "router_w": f32(inputs["router_w"]), "router_bias": f32(inputs["router_bias"]),
        "moe_w_gate": f32(inputs["moe_w_gate"]), "moe_w_up": f32(inputs["moe_w_up"]), "moe_w_down": f32(inputs["moe_w_down"]),
    }
    m.update(_consts())
    return m


def kernel(**inputs):
    nc = build_program()
    shared = make_in_map(inputs, 0)
    in_maps = []
    for b in range(8):
        m = dict(shared)
        m["x"] = np.ascontiguousarray(np.asarray(inputs["x"][b], dtype=np.float32))
        m["ctx"] = np.ascontiguousarray(np.asarray(inputs["ctx"][b], dtype=np.float32))
        m["c"] = np.ascontiguousarray(np.asarray(inputs["c"][b:b + 1], dtype=np.float32))
        in_maps.append(m)
    res = run_bass_kernel_spmd(nc, in_maps, core_ids=list(range(8)))
    return np.stack([np.asarray(r["out"], dtype=np.float32) for r in res.results], axis=0)
```

```python
import math
import os
DBG_SKIP = os.environ.get('DBG_SKIP', '').split(',')
DBG_NT = int(os.environ.get('DBG_NT', '34'))
from contextlib import ExitStack
import numpy as np
import ml_dtypes
import concourse.bass as bass
import concourse.mybir as mybir
from concourse.bass_utils import run_bass_kernel_spmd

F32 = mybir.dt.float32
BF16 = mybir.dt.bfloat16
I32 = mybir.dt.int32
ALU = mybir.AluOpType
AF = mybir.ActivationFunctionType
AX = mybir.AxisListType

SEM_LIMIT = 30000
NT = 34
NTOK = 4352
D = 1024
ALPHA = 4.0 ** 0.25
LN_EPS = 1e-5
RMS_EPS = 1e-6
TWO_PI = 2.0 * math.pi
CW1 = 6.28125
CW2 = TWO_PI - CW1


class Res:
    __slots__ = ("name", "w", "r", "x")

    def __init__(self, name="", x=False):
        self.name = name
        self.w = None
        self.r = []
        self.x = x


def RL(n, name="r"):
    return [Res("%s%d" % (name, i)) for i in range(n)]


class EngState:
    def __init__(self, fw, name, eng):
        self.fw = fw
        self.name = name
        self.eng = eng
        self.count = 0
        self.epoch = 0
        self.known = {}
        self._new_sem()

    def _new_sem(self):
        self.sem_key = "%s_e%d" % (self.name, self.epoch)
        self.sem = self.fw.new_sem(self.sem_key)
        self.count = 0
        self.epoch += 1


class FW:
    def __init__(self, nc, n_dma_sems=10):
        self.nc = nc
        self.es = ExitStack()
        self.sems = {}
        self.engs = {}
        for name, eng in (("pe", nc.tensor), ("act", nc.scalar), ("dve", nc.vector),
                          ("pool", nc.gpsimd), ("sp", nc.sync)):
            self.engs[name] = EngState(self, name, eng)
        self.dma_pool = {}
        for q in ("sp", "act", "pool"):
            lst = []
            for i in range(n_dma_sems):
                key = "dma_%s_%d" % (q, i)
                lst.append([key, self.new_sem(key), 0])
            self.dma_pool[q] = [lst, 0]
        self.n_instr = 0
        self.n_waits = 0

    def new_sem(self, key):
        s = self.es.enter_context(self.nc.semaphore(key))
        self.sems[key] = s
        return s

    def _wait(self, E, ev):
        if ev is None:
            return
        key, val = ev
        if E.known.get(key, 0) >= val:
            return
        E.eng.wait_ge(self.sems[key], val)
        E.known[key] = val
        self.n_waits += 1

    def _deps(self, E, reads, writes, acc=False):
        for r in reads:
            self._wait(E, r.w)
            if r.x:
                for ev in r.r:
                    if ev[0] != E.sem_key:
                        self._wait(E, ev)
        for w in writes:
            if not ((acc or E.name == "pe") and w.w is not None and w.w[0] == E.sem_key):
                self._wait(E, w.w)
            for ev in w.r:
                self._wait(E, ev)

    def _commit(self, ev, reads, writes):
        for r in reads:
            r.r.append(ev)
            if len(r.r) > 16:
                d = {}
                for k, v in r.r:
                    if d.get(k, 0) < v:
                        d[k] = v
                r.r = list(d.items())
        for w in writes:
            w.w = ev
            w.r = []

    def op(self, ename, fn, reads=(), writes=(), acc=False):
        E = self.engs[ename]
        if E.count >= SEM_LIMIT:
            E._new_sem()
        self._deps(E, reads, writes, acc=acc)
        ins = fn(E.eng)
        E.count += 1
        ins.then_inc(E.sem, 1)
        self._commit((E.sem_key, E.count), reads, writes)
        self.n_instr += 1
        return ins

    def _dma_common(self, qname, issue, reads, writes):
        E = self.engs[qname]
        pool, idx = self.dma_pool[qname]
        ent = pool[idx % len(pool)]
        self.dma_pool[qname][1] = idx + 1
        key, sem, val = ent
        if val > 0:
            self._wait(E, (key, val))
        if val + 16 > SEM_LIMIT:
            key = key + "n"
            sem = self.new_sem(key)
            val = 0
            ent[0], ent[1] = key, sem
        self._deps(E, reads, writes)
        ins = issue(E.eng)
        val += 16
        ent[2] = val
        ins.then_inc(sem, 16)
        ev = (key, val)
        self._commit(ev, reads, writes)
        self.n_instr += 1
        return ev

    def dma(self, qname, out, in_, reads=(), writes=(), **kw):
        return self._dma_common(qname, lambda e: e.dma_start(out=out, in_=in_, **kw), reads, writes)

    def barrier(self):
        evs = []
        for q in self.dma_pool:
            for key, sem, val in self.dma_pool[q][0]:
                if val > 0:
                    evs.append((key, val))
        for n, e in self.engs.items():
            if e.count > 0:
                evs.append((e.sem_key, e.count))
        for n, E in self.engs.items():
            for ev in evs:
                if ev[0] != E.sem_key:
                    self._wait(E, ev)

    def finish(self):
        E = self.engs["sp"]
        for q in self.dma_pool:
            for key, sem, val in self.dma_pool[q][0]:
                if val > 0:
                    self._wait(E, (key, val))
        for n, e in self.engs.items():
            if e.count > 0:
                self._wait(E, (e.sem_key, e.count))

    def close(self):
        self.es.close()


class Scope:
    FWREF = None

    def __init__(self, nc):
        self.nc = nc
        self.es = ExitStack()

    CNT = [0]

    def sb(self, name, shape, dtype=F32):
        Scope.CNT[0] += 1
        return self.es.enter_context(self.nc.sbuf_tensor("%s_%d" % (name, Scope.CNT[0]), list(shape), dtype))

    def ps(self, name, shape, dtype=F32):
        Scope.CNT[0] += 1
        return self.es.enter_context(self.nc.psum_tensor("%s_%d" % (name, Scope.CNT[0]), list(shape), dtype))

    def close(self):
        if Scope.FWREF is not None:
            Scope.FWREF.barrier()
        self.es.close()


def rev_ap(ap2d, n):
    last = ap2d[:, n - 1:n]
    return bass.AP(tensor=ap2d.tensor, offset=last.offset, ap=[list(ap2d.ap[0]), [-1, n]])


def build_program(stop_after=None, dbg_shape=None):
    nc = bass.Bass("TRN2", target_bir_lowering=False)

    def din(name, shape, dt=F32):
        return nc.dram_tensor(name, list(shape), dt, kind="ExternalInput").ap()

    x_d = din("x", [4096, D]); ctx_d = din("ctx", [256, D])
    c_d = din("c", [1, D]); cctx_d = din("c_ctx", [1, D])
    ada_w = din("ada_w", [2, D, 6 * D]); ada_b = din("ada_b", [2, 6 * D])
    ln_g = din("ln_g", [2, 2, D]); ln_b = din("ln_b", [2, 2, D])
    even_w_in = din("even_w_in", [D, 1280]); even_w_out = din("even_w_out", [D, D])
    lam_re = din("s5_lam_re", [2, 32, 64]); lam_im = din("s5_lam_im", [2, 32, 64])
    log_step = din("s5_log_step", [2, 32])
    b_re = din("s5_b_re", [2, 32, 64, 16]); b_im = din("s5_b_im", [2, 32, 64, 16])
    c_re = din("s5_c_re", [2, 32, 16, 64]); c_im = din("s5_c_im", [2, 32, 16, 64])
    s5_d = din("s5_d", [512]); w_glu = din("s5_w_glu", [512, 512]); b_glu = din("s5_b_glu", [512])
    win_sink = din("win_sink", [8])
    odd_w_in = din("odd_w_in", [D, 1536]); odd_w_out = din("odd_w_out", [D, D])
    q_norm = din("odd_q_norm", [64]); k_norm = din("odd_k_norm", [64])
    router_w = din("router_w", [D, 32]); router_b = din("router_bias", [32])
    w_gate = din("moe_w_gate", [2, 32, D, 512]); w_up = din("moe_w_up", [2, 32, D, 512])
    w_down = din("moe_w_down", [2, 32, 512, D])
    k_ident = din("k_ident", [128, 128]); k_rope = din("k_rope", [128, 2, 32, 32])
    k_mask = din("k_mask", [128, 3, 128]); k_jidx = din("k_jidx", [128, 128]); k_pc = din("k_pc", [128, 4])
    out_d = nc.dram_tensor("out", [4096, D], F32, kind="ExternalOutput").ap()
    XR = nc.dram_tensor("xr", [NTOK, D], F32, kind="Internal").ap()
    QT = nc.dram_tensor("qt_scr", [8, 128, 4096], BF16, kind="Internal").ap()
    OT = nc.dram_tensor("ot_scr", [8, 128, 4096], BF16, kind="Internal").ap()
    ATD = nc.dram_tensor("at_scr", [4, 128, NTOK], BF16, kind="Internal").ap()
    NS = 49
    XS = nc.dram_tensor("xs_scr", [NS * 512, D], BF16, kind="Internal").ap()
    YS = nc.dram_tensor("ys_scr", [NS * 512, D], F32, kind="Internal").ap()
    wg_all = w_gate.rearrange("l e (kk two) n -> (l e kk) (two n)", two=2)
    wu_all = w_up.rearrange("l e (kk two) n -> (l e kk) (two n)", two=2)
    wd_all = w_down.rearrange("l e f n -> (l e f) n")
    wg_rows = [wg_all, wg_all]; wu_rows = [wu_all, wu_all]; wd_rows = [wd_all, wd_all]
    dbg = None
    if dbg_shape is not None:
        dbg = nc.dram_tensor("dbg", list(dbg_shape), F32, kind="ExternalOutput").ap()

    f = FW(nc)
    Scope.FWREF = f
    G = Scope(nc)
    R_XR = RL(NT, "xr")
    R_out = Res("out")
    R_dbg = Res("dbg")

    ident = G.sb("ident", [128, 128]); R_ident = Res()
    identb = G.sb("identb", [128, 128], BF16); R_identb = Res()
    f.dma("sp", ident[:], k_ident, writes=[R_ident])
    f.op("dve", lambda e: e.tensor_copy(identb[:], ident[:]), reads=[R_ident], writes=[R_identb])
    rope = G.sb("rope", [128, 2, 32, 32]); R_rope = Res()
    f.dma("sp", rope[:], k_rope, writes=[R_rope])
    maskf = G.sb("maskf", [128, 3, 128]); maskb = G.sb("maskb", [128, 3, 128], BF16); R_mask = Res()
    f.dma("sp", maskf[:], k_mask, writes=[R_mask])
    f.op("dve", lambda e: e.tensor_copy(maskb[:], maskf[:]), reads=[R_mask], writes=[R_mask])
    R_crep = Res()
    ctmp = G.sb("ctmp", [128, 2, 8]); R_ctmp = Res()
    f.dma("sp", ctmp[:, 0, :], c_d.rearrange("o (kc p) -> p (o kc)", p=128), writes=[R_ctmp], allow_slow_non_contiguous=True)
    f.dma("sp", ctmp[:, 1, :], cctx_d.rearrange("o (kc p) -> p (o kc)", p=128), writes=[R_ctmp], allow_slow_non_contiguous=True)
    f.op("act", lambda e: e.activation(out=ctmp[:], in_=ctmp[:], func=AF.Silu), reads=[R_ctmp], writes=[R_ctmp])
    lng = G.sb("lng", [128, D]); lnb = G.sb("lnb", [128, D]); R_ln = Res()

    def load_ln(li):
        f.dma("sp", lng[:], ln_g[li // 2, li % 2].partition_broadcast(128), writes=[R_ln])
        f.dma("sp", lnb[:], ln_b[li // 2, li % 2].partition_broadcast(128), writes=[R_ln])
    epsc = G.sb("epsc", [128, 1]); R_eps = Res()
    f.op("dve", lambda e: e.memset(epsc[:], LN_EPS), writes=[R_eps])

    R_XsZ = RL(28, "xsz")
    ZS = Scope(nc)
    zt = ZS.sb("zt", [128, 7, D], BF16); R_zt = Res()
    f.op("pool", lambda e: e.memset(zt[:], 0.0), writes=[R_zt])
    for k in range(28):
        f.dma(("sp", "act")[k % 2], XS[k * 896:(k + 1) * 896, :].rearrange("(a p) d -> p a d", p=128), zt[:], reads=[R_zt], writes=[R_XsZ[k]])
    ZS.close()

    mod = G.sb("mod", [128, 2, 3, D]); R_mod = Res("mod")

    def dump(ap_sb, rows, cols, reads, r0=0, c0=0):
        f.dma("sp", dbg[r0:r0 + rows, c0:c0 + cols], ap_sb, reads=reads, writes=[R_dbg])

    def phase_mod(i, s):
        S = Scope(nc)
        crep = S.sb("crep", [128, 2, 8, 128])
        f.op("dve", lambda e: e.tensor_copy(crep[:], ctmp[:].unsqueeze(3).broadcast_to([128, 2, 8, 128])), reads=[R_ctmp], writes=[R_crep])
        slab = [S.sb("slab%d" % k, [128, 8, 512]) for k in range(2)]; R_slab = RL(2)
        adb = [S.sb("adb%d" % k, [128, 512]) for k in range(2)]; R_adb = RL(2)
        psm = [S.ps("psm%d" % k, [128, 512]) for k in range(2)]; R_psm = RL(2)
        n = 0
        for blk in range(6):
            c0 = s * 3072 + blk * 512
            bi = blk % 2
            f.dma("sp", slab[bi][:], ada_w[i, :, c0:c0 + 512].rearrange("(kc p) n -> p kc n", p=128), writes=[R_slab[bi]])
            f.dma("act", adb[bi][:], ada_b[i, c0:c0 + 512].partition_broadcast(128), writes=[R_adb[bi]])
            k, half = blk // 2, blk % 2
            for which in range(2):
                pi = n % 2; n += 1
                for kc in range(8):
                    f.op("pe", lambda e, kc=kc: e.matmul(psm[pi][:], crep[:, which, kc, :], slab[bi][:, kc, :], start=(kc == 0), stop=(kc == 7)),
                         reads=[R_crep, R_slab[bi]], writes=[R_psm[pi]], acc=(kc > 0))
                dst = mod[:, which, k, half * 512:(half + 1) * 512]
                f.op("dve", lambda e: e.scalar_tensor_tensor(out=dst, in0=psm[pi][:], scalar=(1.0 if k == 1 else 0.0), in1=adb[bi][:], op0=ALU.add, op1=ALU.add),
                     reads=[R_psm[pi], R_adb[bi]], writes=[R_mod])
        S.close()

    def resid_ln(S, xt, R_xt, o_ps, R_ops, which, li, out_t, R_outt, tmp, R_tmp, small, R_small):
        gate = mod[:, which, 2, :]
        f.op("dve", lambda e: e.tensor_tensor(tmp[:], o_ps[:], gate, ALU.mult), reads=[R_ops, R_mod], writes=[R_tmp])
        f.op("dve", lambda e: e.scalar_tensor_tensor(out=tmp[:], in0=xt[:], scalar=ALPHA, in1=tmp[:], op0=ALU.mult, op1=ALU.add),
             reads=[R_xt, R_tmp], writes=[R_tmp])
        f.op("dve", lambda e: e.bn_stats(small[:, 0:6], tmp[:, 0:512]), reads=[R_tmp], writes=[R_small])
        f.op("dve", lambda e: e.bn_stats(small[:, 6:12], tmp[:, 512:1024]), reads=[R_tmp], writes=[R_small])
        f.op("dve", lambda e: e.bn_aggr(small[:, 12:14], small[:, 0:12]), reads=[R_small], writes=[R_small])
        f.op("act", lambda e: e.activation(out=small[:, 14:15], in_=small[:, 13:14], func=AF.Sqrt, bias=epsc[:], scale=1.0), reads=[R_small, R_eps], writes=[R_small])
        f.op("dve", lambda e: e.reciprocal(small[:, 15:16], small[:, 14:15]), reads=[R_small], writes=[R_small])
        f.op("dve", lambda e: e.tensor_scalar(out=tmp[:], in0=tmp[:], scalar1=small[:, 12:13], scalar2=small[:, 15:16], op0=ALU.subtract, op1=ALU.mult),
             reads=[R_tmp, R_small], writes=[R_tmp])
        f.op("dve", lambda e: e.tensor_tensor(tmp[:], tmp[:], lng[:], ALU.mult), reads=[R_tmp, R_ln], writes=[R_tmp])
        f.op("dve", lambda e: e.tensor_tensor(out_t[:], tmp[:], lnb[:], ALU.add), reads=[R_tmp, R_ln], writes=[R_outt])

    def mod_transpose(xt, R_xt, which, h32, R_h32, ps_tp, R_pstp, hT_dst, R_hT, h32T=None, R_h32T=None):
        f.op("dve", lambda e: e.tensor_tensor(h32[:], xt[:], mod[:, which, 1, :], ALU.mult), reads=[R_xt, R_mod], writes=[R_h32])
        f.op("dve", lambda e: e.tensor_tensor(h32[:], h32[:], mod[:, which, 0, :], ALU.add), reads=[R_h32, R_mod], writes=[R_h32])
        for kc in range(8):
            f.op("pe", lambda e, kc=kc: e.transpose(ps_tp[:, kc, :], h32[:, kc * 128:(kc + 1) * 128], ident[:]),
                 reads=[R_h32, R_ident], writes=[R_pstp], acc=(kc > 0))
        f.op("act", lambda e: e.activation(out=hT_dst, in_=ps_tp[:], func=AF.Identity), reads=[R_pstp], writes=[R_hT])
        if h32T is not None:
            f.op("dve", lambda e: e.tensor_copy(h32T[:], ps_tp[:]), reads=[R_pstp], writes=[R_h32T])

    def src_tile(layer, t):
        if layer == 0:
            return (ctx_d[t * 128:(t + 1) * 128, :] if t < 2 else x_d[(t - 2) * 128:(t - 1) * 128, :]), []
        return XR[t * 128:(t + 1) * 128, :], [R_XR[t]]

    def layer0_mixer():
        L = Scope(nc)
        U = Scope(nc)
        uT = U.sb("uT", [128, 4, NTOK], BF16); R_uT = RL(NT, "uT")
        aT, R_aT = uT, R_uT

        def inproj(do_u, qT=None, R_qT=None, kT2=None, R_kT=None, vaug=None, R_v=None):
            S = Scope(nc)
            wc0, wc1 = (0, 512) if do_u else (512, 1280)
            win = S.sb("win", [128, 8, wc1 - wc0], BF16); R_win = Res()
            f.dma("pool", win[:], even_w_in[:, wc0:wc1].rearrange("(kc p) n -> p kc n", p=128), writes=[R_win])
            xt1 = S.sb("xt1", [128, D]); xt = [xt1, xt1]; R1_ = Res(); R_xt = [R1_, R1_]
            h32 = S.sb("h32", [128, D]); R_h32 = Res()
            hT = [S.sb("hT%d" % k, [128, 8, 128], BF16) for k in range(2)]; R_hT = RL(2)
            ps_tp = S.ps("ps_tp", [128, 8, 128]); R_pstp = Res(x=True)
            ps_u = S.ps("ps_u", [128, 4, 128]); R_psu = Res()
            ps_q = S.ps("ps_q", [128, 1024]); R_psq = Res(x=True)
            ps_t = S.ps("ps_t", [128, 8, 128], BF16); R_pst = Res(x=True)
            ra = S.sb("ra", [128, 10, 32]); rb = S.sb("rb", [128, 10, 32]); R_ra = Res(); R_rb = Res()
            tqk = S.sb("tqk", [128, 640], BF16); R_tqk = Res()
            kd = S.sb("kd", [128, 2, 2, 64], BF16); R_kd = Res()
            for t in range(NT if do_u else min(NT, DBG_NT)):
                b = t % 2
                src, rs = src_tile(0, t)
                f.dma("sp", xt[b][:], src, reads=rs, writes=[R_xt[b]])
                which = 1 if t < 2 else 0
                mod_transpose(xt[b], R_xt[b], which, h32, R_h32, ps_tp, R_pstp, hT[b][:], R_hT[b])
                cols = slice(t * 128, (t + 1) * 128)
                if stop_after == "h0" and t == 0:
                    f.dma("sp", dbg[0:128, :], h32[:], reads=[R_h32], writes=[R_dbg])
                    hf = S.sb("hf", [128, 1024]); R_hf = Res()
                    f.op("dve", lambda e: e.tensor_copy(hf[:], hT[b][:].rearrange("p a b -> p (a b)")), reads=[R_hT[b]], writes=[R_hf])
                    f.dma("sp", dbg[128:256, :], hf[:], reads=[R_hf], writes=[R_dbg])
                    f.op("dve", lambda e: e.tensor_copy(hf[:], win[:, 0, 0:1024]), reads=[R_win], writes=[R_hf])
                    f.dma("sp", dbg[256:384, :], hf[:], reads=[R_hf], writes=[R_dbg])
                    S.close(); return
                if do_u:
                    for ct in range(4):
                        for kc in range(8):
                            f.op("pe", lambda e, ct=ct, kc=kc: e.matmul(ps_u[:, ct, :], win[:, kc, ct * 128:(ct + 1) * 128], hT[b][:, kc, :], start=(kc == 0), stop=(kc == 7)),
                                 reads=[R_win, R_hT[b]], writes=[R_psu], acc=(ct + kc > 0))
                    f.op("act", lambda e: e.activation(out=uT[:, :, cols], in_=ps_u[:], func=AF.Identity), reads=[R_psu], writes=[R_uT[t]])
                    continue
                for (n0, n1) in ((0, 512), (512, 768)):
                    for kc in range(8):
                        f.op("pe", lambda e, kc=kc, n0=n0, n1=n1: e.matmul(ps_q[:, n0:n1], hT[b][:, kc, :], win[:, kc, n0:n1], start=(kc == 0), stop=(kc == 7)),
                             reads=[R_win, R_hT[b]], writes=[R_psq], acc=(n0 + kc > 0))
                if 'rope' in DBG_SKIP:
                    continue
                if t >= 2:
                    pv = ps_q[:, 0:640].rearrange("p (h two f) -> p h two f", two=2, f=32)
                    ov = tqk[:].rearrange("p (h two f) -> p h two f", two=2, f=32)
                    cosb = rope[:, 0, t - 2, :].unsqueeze(1).broadcast_to([128, 10, 32])
                    sinb = rope[:, 1, t - 2, :].unsqueeze(1).broadcast_to([128, 10, 32])
                    f.op("dve", lambda e: e.tensor_tensor(ra[:], pv[:, :, 0, :], cosb, ALU.mult), reads=[R_psq, R_rope], writes=[R_ra])
                    f.op("dve", lambda e: e.tensor_tensor(rb[:], pv[:, :, 1, :], sinb, ALU.mult), reads=[R_psq, R_rope], writes=[R_rb])
                    f.op("pool", lambda e: e.tensor_tensor(ov[:, :, 0, :], ra[:], rb[:], ALU.subtract), reads=[R_ra, R_rb], writes=[R_tqk])
                    f.op("dve", lambda e: e.tensor_tensor(ra[:], pv[:, :, 1, :], cosb, ALU.mult), reads=[R_psq, R_rope, R_tqk], writes=[R_ra])
                    f.op("dve", lambda e: e.tensor_tensor(rb[:], pv[:, :, 0, :], sinb, ALU.mult), reads=[R_psq, R_rope, R_tqk], writes=[R_rb])
                    f.op("pool", lambda e: e.tensor_tensor(ov[:, :, 1, :], ra[:], rb[:], ALU.add), reads=[R_ra, R_rb], writes=[R_tqk])
                else:
                    f.op("act", lambda e: e.activation(out=tqk[:], in_=ps_q[:, 0:640], func=AF.Identity), reads=[R_psq], writes=[R_tqk])
                if 'vaug' in DBG_SKIP:
                    continue
                for a in range(2):
                    if 'novaug' in DBG_SKIP:
                        break
                    f.op("dve", lambda e, a=a: e.tensor_copy(vaug[:, t, 64 + 128 * a:128 + 128 * a], ps_q[:, 640 + 64 * a:704 + 64 * a]),
                         reads=[R_psq], writes=[R_v[t]])
                if 'nokd' in DBG_SKIP:
                    continue
                kv = tqk[:, 512:640].rearrange("p (a d) -> p a d", a=2)
                f.op("dve", lambda e: e.tensor_copy(kd[:, :, 0, :], kv), reads=[R_tqk], writes=[R_kd])
                f.op("dve", lambda e: e.tensor_copy(kd[:, :, 1, :], kv), reads=[R_tqk], writes=[R_kd])
                if 'tr' in DBG_SKIP:
                    continue
                for pr in range(4):
                    f.op("pe", lambda e, pr=pr: e.transpose(ps_t[:, pr, :], tqk[:, pr * 128:(pr + 1) * 128], identb[:]),
                         reads=[R_tqk, R_identb], writes=[R_pst], acc=(pr > 0))
                for a in range(2):
                    f.op("pe", lambda e, a=a: e.transpose(ps_t[:, 4 + a, :], kd[:, a, :, :].rearrange("p a d -> p (a d)"), identb[:]),
                         reads=[R_kd, R_identb], writes=[R_pst], acc=True)
                f.op("dve", lambda e: e.tensor_copy(qT[:, :, cols], ps_t[:, 0:4, :]), reads=[R_pst], writes=[R_qT[t]])
                f.op("act", lambda e: e.activation(out=kT2[:, :, cols], in_=ps_t[:, 4:6, :], func=AF.Identity), reads=[R_pst], writes=[R_kT[t]])
            S.close()

        inproj(True)
        if stop_after == "h0":
            U.close(); L.close(); return
        if stop_after == "in0":
            S = Scope(nc)
            t32 = S.sb("t32", [128, 512]); R_t = Res()
            for ct in range(4):
                for blk in range(2):
                    f.op("dve", lambda e: e.tensor_copy(t32[:], uT[:, ct, blk * 512:(blk + 1) * 512]), reads=R_uT, writes=[R_t])
                    dump(t32[:], 128, 512, [R_t], r0=ct * 128, c0=blk * 512)
            S.close(); U.close(); L.close()
            return
        if 's5' not in DBG_SKIP:
            s5_phase(L, uT, R_uT, aT, R_aT)
        if stop_after == "s5":
            S = Scope(nc)
            t32 = S.sb("t32", [128, 512]); R_t = Res()
            for ct in range(4):
                for blk in range(9):
                    c0 = blk * 512; n = min(512, NTOK - c0)
                    f.op("dve", lambda e: e.tensor_copy(t32[:, 0:n], aT[:, ct, c0:c0 + n]), reads=R_aT, writes=[R_t])
                    dump(t32[:, 0:n], 128, n, [R_t], r0=ct * 128, c0=c0)
            S.close(); U.close(); L.close()
            return
        R_ATD = Res("atd")
        for k in range(4):
            f.dma(("sp", "act")[k % 2], ATD[k], aT[:, k, :], reads=R_aT, writes=[R_ATD])
        U.close()
        oT = L.sb("oT", [128, 4, NTOK], BF16); R_oT = RL(NT, "oT")
        W = Scope(nc)
        qT = W.sb("qT", [128, 4, NTOK], BF16); R_qT = RL(NT, "qT")
        kT2 = W.sb("kT2", [128, 2, NTOK], BF16); R_kT = RL(NT, "kT")
        vaug = W.sb("vaug", [128, NT, 320], BF16); R_v = RL(NT, "v")
        f.op("pool", lambda e: e.memset(vaug[:], 1.0), writes=R_v)
        inproj(False, qT, R_qT, kT2, R_kT, vaug, R_v)
        if stop_after == "qkv":
            W.close(); L.close(); return
        win_phase(qT, R_qT, kT2, R_kT, vaug, R_v, oT, R_oT)
        W.close()
        if stop_after == "win":
            L.close(); return

        M = Scope(nc)
        mixt = [M.sb("mixa%d" % k, [128, 4, 128], BF16) for k in range(2)]; R_mixt = RL(2)

        def mix_loader(t, b):
            c0 = t * 128
            f.dma("sp", mixt[b][:], ATD[:, :, c0:c0 + 128].rearrange("a p n -> p a n"), reads=[R_ATD], writes=[R_mixt[b]])
            return [mixt[b][:, k, :] for k in range(4)] + [oT[:, k, c0:c0 + 128] for k in range(4)], [R_mixt[b], R_oT[t]]
        out_phase(0, even_w_out, mix_loader, range(NT))
        M.close()
        L.close()

    def sincos(S, ang, n, out_s, out_c, R, tag):
        ki = S.sb("ki_" + tag, [128, n], I32); kf = S.sb("kf_" + tag, [128, n]); rd = S.sb("rd_" + tag, [128, n])
        f.op("dve", lambda e: e.tensor_scalar(out=ki[:], in0=ang, scalar1=1.0 / TWO_PI, scalar2=None, op0=ALU.mult), reads=[R], writes=[R])
        f.op("dve", lambda e: e.tensor_copy(kf[:], ki[:]), reads=[R], writes=[R])
        f.op("dve", lambda e: e.scalar_tensor_tensor(out=rd[:], in0=kf[:], scalar=-CW1, in1=ang, op0=ALU.mult, op1=ALU.add), reads=[R], writes=[R])
        f.op("dve", lambda e: e.scalar_tensor_tensor(out=rd[:], in0=kf[:], scalar=-CW2, in1=rd[:], op0=ALU.mult, op1=ALU.add), reads=[R], writes=[R])
        f.op("dve", lambda e: e.tensor_scalar(out=rd[:], in0=rd[:], scalar1=3.1415925, scalar2=-3.1415925, op0=ALU.min, op1=ALU.max), reads=[R], writes=[R])
        f.op("act", lambda e: e.activation(out=out_s, in_=rd[:], func=AF.Sin), reads=[R], writes=[R])
        f.op("dve", lambda e: e.scalar_tensor_tensor(out=rd[:], in0=rd[:], scalar=-1.0, in1=rd[:], op0=ALU.mult, op1=ALU.max), reads=[R], writes=[R])
        f.op("dve", lambda e: e.tensor_scalar(out=rd[:], in0=rd[:], scalar1=-1.0, scalar2=math.pi / 2, op0=ALU.mult, op1=ALU.add), reads=[R], writes=[R])
        f.op("act", lambda e: e.activation(out=out_c, in_=rd[:], func=AF.Sin), reads=[R], writes=[R])

    def s5_phase(L, uT, R_uT, aT, R_aT):
        P = Scope(nc)
        R = Res("s5setup")
        prm = P.sb("prm", [128, 16, 32])
        dsk = P.sb("dsk", [128, 4]); bgl = P.sb("bgl", [128, 4])
        cs2 = P.sb("cs2", [128, 32, 2]); ncs2 = P.sb("ncs2", [128, 32, 2])
        jt = P.sb("jt", [128, 128]); f.dma("sp", jt[:], k_jidx, writes=[R])
        f.dma("sp", dsk[:], s5_d.rearrange("(c p) -> p c", p=128), writes=[R], allow_slow_non_contiguous=True)
        f.dma("sp", bgl[:], b_glu.rearrange("(c p) -> p c", p=128), writes=[R], allow_slow_non_contiguous=True)
        S = Scope(nc)
        st32 = S.sb("st32", [32, 3, 128]); lsr = S.sb("lsr", [32, 2])
        f.dma("sp", st32[:, 0, :], lam_re.rearrange("d (q g) n -> (d q) (g n)", g=2), writes=[R])
        f.dma("sp", st32[:, 1, :], lam_im.rearrange("d (q g) n -> (d q) (g n)", g=2), writes=[R])
        f.dma("sp", lsr[:], log_step.rearrange("d (q g) -> (d q) g", g=2), writes=[R])
        f.op("dve", lambda e: e.tensor_copy(st32[:, 2, :].rearrange("p (g n) -> p g n", g=2), lsr[:].unsqueeze(2).broadcast_to([32, 2, 64])), reads=[R], writes=[R])
        pst = S.ps("pst", [128, 4, 128])
        for k in range(3):
            f.op("pe", lambda e, k=k: e.transpose(pst[:, k, 0:32], st32[:, k, :], ident[0:32, 0:32]), reads=[R, R_ident], writes=[R], acc=(k > 0))
        f.op("dve", lambda e: e.tensor_copy(prm[:, 0:3, :], pst[:, 0:3, 0:32]), reads=[R], writes=[R])
        lr, li = prm[:, 0, :], prm[:, 1, :]
        dt, th, rr = prm[:, 3, :], prm[:, 4, :], prm[:, 5, :]
        f.op("act", lambda e: e.activation(out=dt, in_=prm[:, 2, :], func=AF.Exp), reads=[R], writes=[R])
        f.op("dve", lambda e: e.tensor_tensor(th, li, dt, ALU.mult), reads=[R], writes=[R])
        f.op("dve", lambda e: e.tensor_tensor(prm[:, 10, :], lr, dt, ALU.mult), reads=[R], writes=[R])
        f.op("act", lambda e: e.activation(out=rr, in_=prm[:, 10, :], func=AF.Exp), reads=[R], writes=[R])
        f.op("dve", lambda e: e.tensor_scalar(out=prm[:, 10, :], in0=th, scalar1=128.0, scalar2=None, op0=ALU.mult), reads=[R], writes=[R])
        sincos(S, prm[:, 10, :], 32, prm[:, 7, :], prm[:, 6, :], R, "a")
        sincos(S, th, 32, prm[:, 12, :], prm[:, 11, :], R, "b")
        abre, abim, den, t1, t2 = prm[:, 13, :], prm[:, 14, :], prm[:, 15, :], prm[:, 10, :], prm[:, 2, :]
        f.op("dve", lambda e: e.tensor_tensor(abre, rr, prm[:, 11, :], ALU.mult), reads=[R], writes=[R])
        f.op("dve", lambda e: e.tensor_scalar(out=abre, in0=abre, scalar1=-1.0, scalar2=None, op0=ALU.add), reads=[R], writes=[R])
        f.op("dve", lambda e: e.tensor_tensor(abim, rr, prm[:, 12, :], ALU.mult), reads=[R], writes=[R])
        f.op("dve", lambda e: e.tensor_tensor(den, lr, lr, ALU.mult), reads=[R], writes=[R])
        f.op("dve", lambda e: e.tensor_tensor(t1, li, li, ALU.mult), reads=[R], writes=[R])
        f.op("dve", lambda e: e.tensor_tensor(den, den, t1, ALU.add), reads=[R], writes=[R])
        f.op("dve", lambda e: e.reciprocal(den, den), reads=[R], writes=[R])
        f.op("dve", lambda e: e.tensor_tensor(t1, abre, lr, ALU.mult), reads=[R], writes=[R])
        f.op("dve", lambda e: e.tensor_tensor(t2, abim, li, ALU.mult), reads=[R], writes=[R])
        f.op("dve", lambda e: e.tensor_tensor(t1, t1, t2, ALU.add), reads=[R], writes=[R])
        f.op("dve", lambda e: e.tensor_tensor(prm[:, 8, :], t1, den, ALU.mult), reads=[R], writes=[R])
        f.op("dve", lambda e: e.tensor_tensor(t1, abim, lr, ALU.mult), reads=[R], writes=[R])
        f.op("dve", lambda e: e.tensor_tensor(t2, abre, li, ALU.mult), reads=[R], writes=[R])
        f.op("dve", lambda e: e.tensor_tensor(t1, t1, t2, ALU.subtract), reads=[R], writes=[R])
        f.op("dve", lambda e: e.tensor_tensor(prm[:, 9, :], t1, den, ALU.mult), reads=[R], writes=[R])
        f.op("dve", lambda e: e.tensor_copy(cs2[:, :, 0], prm[:, 6, :]), reads=[R], writes=[R])
        f.op("dve", lambda e: e.tensor_copy(cs2[:, :, 1], prm[:, 7, :]), reads=[R], writes=[R])
        f.op("dve", lambda e: e.tensor_scalar(out=ncs2[:, :, 0], in0=prm[:, 7, :], scalar1=-1.0, scalar2=None, op0=ALU.mult), reads=[R], writes=[R])
        f.op("dve", lambda e: e.tensor_copy(ncs2[:, :, 1], prm[:, 6, :]), reads=[R], writes=[R])
        S.close()
        cosJ = P.sb("cosJ", [128, 8, 128]); sinJ = P.sb("sinJ", [128, 8, 128]); rtab = P.sb("rtab", [128, 8, 128])
        lB = P.sb("lB", [128, 8, 2, 128], BF16); lC = P.sb("lC", [128, 8, 2, 128], BF16)
        RT = Res("s5tab")
        S = Scope(nc)
        yacc = S.sb("yacc", [128, NTOK]); R_y = Res("yacc")
        NB = 2
        psb = [S.ps("psb%d" % k, [128, 2, 512]) for k in range(NB)]; R_psb = RL(NB)
        psy = [S.ps("psy%d" % k, [128, 512]) for k in range(NB)]; R_psy = RL(NB)
        pstr = [S.ps("pstr%d" % k, [128, 4, 128]) for k in range(2)]; R_pstr = RL(2)
        m = [S.sb("m%d" % k, [128, 2, 512]) for k in range(NB)]; R_m = RL(NB)
        ta = [S.sb("ta%d" % k, [128, 2, 512]) for k in range(NB)]; R_ta = RL(NB)
        g = [S.sb("g%d" % k, [128, 2, 512]) for k in range(NB)]; R_g = RL(NB)
        hb = [S.sb("hb%d" % k, [128, 2, 512], BF16) for k in range(NB)]; R_hb = RL(NB)
        ini = S.sb("ini", [128, 4]); R_ini = Res()
        gq1 = S.sb("gq1", [128, 512]); gq2 = S.sb("gq2", [128, 512]); R_gq1 = Res(); R_gq2 = Res()
        wgl = S.sb("wgl", [128, 4, 512], BF16); R_wgl = Res()
        f.dma("pool", wgl[:], w_glu.rearrange("(kc p) n -> p kc n", p=128), writes=[R_wgl])
        blocks = [(0, 256)] + [(256 + 512 * k, 512) for k in range(8)]
        it = 0
        for ct in range(4):
            T = Scope(nc)
            ang = T.sb("ang", [128, 8, 128])
            WB = T.sb("WB", [128, 2, 8, 128]); SC = T.sb("SC", [128, 2, 8, 128]); WB2 = T.sb("WB2", [128, 2, 8, 128])
            fre8 = T.sb("fre8", [128, 8]); fim8 = T.sb("fim8", [128, 8])
            for d in range(2):
                gsl = slice(d * 16 + ct * 4, d * 16 + ct * 4 + 4); lsl = slice(d * 4, d * 4 + 4)
                f.op("dve", lambda e: e.tensor_tensor(ang[:, lsl, :], jt[:].unsqueeze(1).broadcast_to([128, 4, 128]), th[:, gsl].unsqueeze(2).broadcast_to([128, 4, 128]), ALU.mult), reads=[R, RT], writes=[RT])
                f.op("dve", lambda e: e.tensor_copy(rtab[:, lsl, :], rr[:, gsl].unsqueeze(2).broadcast_to([128, 4, 128])), reads=[R, RT], writes=[RT])
                f.op("dve", lambda e: e.tensor_copy(fre8[:, lsl], prm[:, 8, gsl]), reads=[R, RT], writes=[RT])
                f.op("dve", lambda e: e.tensor_copy(fim8[:, lsl], prm[:, 9, gsl]), reads=[R, RT], writes=[RT])
            sincos(T, ang[:].rearrange("p a b -> p (a b)"), 1024, sinJ[:].rearrange("p a b -> p (a b)"), cosJ[:].rearrange("p a b -> p (a b)"), RT, "c%d" % ct)
            f.op("pool", lambda e: e.memset(WB[:], 0.0), reads=[RT], writes=[RT])
            f.op("pool", lambda e: e.memset(SC[:], 0.0), reads=[RT], writes=[RT])
            qn = 0
            for d in range(2):
                for gi in range(8):
                    g_ = ct * 8 + gi
                    l = d * 4 + gi // 2
                    gl = gi % 2
                    for ri, (bsrc, csrc) in enumerate(((b_re, c_re), (b_im, c_im))):
                        q1 = ("sp", "act")[qn % 2]; qn += 1
                        f.dma(q1, WB[64 * gl:64 * gl + 64, ri, l, 16 * gi:16 * gi + 16], bsrc[d, g_], writes=[RT])
                        f.dma(q1, SC[16 * gi:16 * gi + 16, ri, l, 64 * gl:64 * gl + 64], csrc[d, g_], writes=[RT])
            fre = fre8[:].unsqueeze(2).broadcast_to([128, 8, 128]); fim = fim8[:].unsqueeze(2).broadcast_to([128, 8, 128])
            f.op("dve", lambda e: e.tensor_tensor(WB2[:, 0], WB[:, 0], fre, ALU.mult), reads=[RT], writes=[RT])
            f.op("pool", lambda e: e.tensor_tensor(WB2[:, 1], WB[:, 1], fim, ALU.mult), reads=[RT], writes=[RT])
            f.op("dve", lambda e: e.tensor_tensor(WB2[:, 0], WB2[:, 0], WB2[:, 1], ALU.subtract), reads=[RT], writes=[RT])
            f.op("pool", lambda e: e.tensor_tensor(WB2[:, 1], WB[:, 1], fre, ALU.mult), reads=[RT], writes=[RT])
            f.op("dve", lambda e: e.tensor_tensor(WB[:, 0], WB[:, 0], fim, ALU.mult), reads=[RT], writes=[RT])
            f.op("dve", lambda e: e.tensor_tensor(WB2[:, 1], WB2[:, 1], WB[:, 0], ALU.add), reads=[RT], writes=[RT])
            n_ = 0
            for srct, dst, neg in ((WB2, lB, False), (SC, lC, True)):
                for ri in range(2):
                    for d4 in range(2):
                        pb = n_ % 2; n_ += 1
                        for k in range(4):
                            l = d4 * 4 + k
                            f.op("pe", lambda e, k=k, l=l: e.transpose(pstr[pb][:, k, :], srct[:, ri, l, :], ident[:]), reads=[RT, R_ident], writes=[R_pstr[pb]], acc=(k > 0))
                        scl = -1.0 if (neg and ri == 1) else 1.0
                        f.op("act", lambda e: e.activation(out=dst[:, d4 * 4:d4 * 4 + 4, ri, :], in_=pstr[pb][:], func=AF.Identity, scale=scl), reads=[R_pstr[pb], RT], writes=[RT])
            T.close()
            f.op("act", lambda e: e.activation(out=yacc[:], in_=uT[:, ct, :], func=AF.Copy, scale=dsk[:, ct:ct + 1]), reads=R_uT + [R], writes=[R_y])
            items = []
            for pi in range(4):
                for d in range(2):
                    for bidx, (s0, n) in enumerate(blocks):
                        items.append((pi, d, bidx, s0, n))
            NI = len(items)

            def v3(ap):
                return ap.rearrange("p (c j) -> p c j", j=128)

            def geom(k):
                pi, d, bidx, s0, n = items[k]
                bi = k % NB
                dq = d * 16 + ct * 4 + pi
                l = d * 4 + pi
                nch = n // 128
                if d == 0:
                    c0 = s0
                    ucols = uT[:, ct, c0:c0 + n]
                    ycols = yacc[:, c0:c0 + n]
                else:
                    c0 = (256 - s0 - n) if s0 < 256 else (4608 - s0 - n)
                    ucols = rev_ap(uT[:, ct, c0:c0 + n], n)
                    ycols = rev_ap(yacc[:, c0:c0 + n], n)
                tl = [R_uT[kk] for kk in range(c0 // 128, (c0 + n) // 128)]
                cb = cosJ[:, l, :].unsqueeze(1).broadcast_to([128, nch, 128])
                sb_ = sinJ[:, l, :].unsqueeze(1).broadcast_to([128, nch, 128])
                return pi, d, bidx, n, bi, dq, l, nch, ucols, ycols, tl, cb, sb_

            def stA(k):
                pi, d, bidx, n, bi, dq, l, nch, ucols, ycols, tl, cb, sb_ = geom(k)
                for ri in range(2):
                    f.op("pe", lambda e, ri=ri: e.matmul(psb[bi][:, ri, 0:n], lB[:, l, ri, :], ucols, start=True, stop=True),
                         reads=[RT] + tl, writes=[R_psb[bi]], acc=(ri > 0))
                bre, bim = v3(psb[bi][:, 0, 0:n]), v3(psb[bi][:, 1, 0:n])
                mre, mim = v3(m[bi][:, 0, 0:n]), v3(m[bi][:, 1, 0:n])
                t_a, t_b = v3(ta[bi][:, 0, 0:n]), v3(ta[bi][:, 1, 0:n])
                f.op("dve", lambda e: e.tensor_tensor(mre, bre, cb, ALU.mult), reads=[R_psb[bi], RT], writes=[R_m[bi]])
                f.op("dve", lambda e: e.tensor_tensor(t_a, bim, sb_, ALU.mult), reads=[R_psb[bi], RT], writes=[R_ta[bi]])
                f.op("dve", lambda e: e.tensor_tensor(mre, mre, t_a, ALU.add), reads=[R_m[bi], R_ta[bi]], writes=[R_m[bi]])
                f.op("dve", lambda e: e.tensor_tensor(mim, bim, cb, ALU.mult), reads=[R_psb[bi], RT], writes=[R_m[bi]])
                f.op("dve", lambda e: e.tensor_tensor(t_b, bre, sb_, ALU.mult), reads=[R_psb[bi], RT], writes=[R_ta[bi]])
                f.op("dve", lambda e: e.tensor_tensor(mim, mim, t_b, ALU.subtract), reads=[R_m[bi], R_ta[bi]], writes=[R_m[bi]])

            def stB(k):
                pi, d, bidx, n, bi, dq, l, nch, ucols, ycols, tl, cb, sb_ = geom(k)
                prev = None
                if bidx > 0:
                    pbi = (k - 1) % NB
                    pn = items[k - 1][4]
                    prev = (g[pbi], pbi, pn // 128 - 1)
                for c in range(nch):
                    cs = slice(c * 128, (c + 1) * 128)
                    if prev is None:
                        i_re = i_im = 0.0
                        rd_extra = []
                    else:
                        pg, pbi, pc_ = prev
                        gre_l = pg[:, 0, pc_ * 128 + 127:pc_ * 128 + 128]
                        gim_l = pg[:, 1, pc_ * 128 + 127:pc_ * 128 + 128]
                        c128 = prm[:, 6, dq:dq + 1]; s128 = prm[:, 7, dq:dq + 1]
                        f.op("dve", lambda e: e.tensor_scalar(out=ini[:, 0:2], in0=cs2[:, dq, :], scalar1=gre_l, scalar2=None, op0=ALU.mult), reads=[R_g[pbi], R], writes=[R_ini])
                        f.op("dve", lambda e: e.scalar_tensor_tensor(out=ini[:, 0:2], in0=ncs2[:, dq, :], scalar=gim_l, in1=ini[:, 0:2], op0=ALU.mult, op1=ALU.add), reads=[R_g[pbi], R, R_ini], writes=[R_ini])
                        i_re, i_im = ini[:, 0:1], ini[:, 1:2]
                        rd_extra = [R_ini]
                    f.op("dve", lambda e: e.tensor_tensor_scan(g[bi][:, 0, cs], rtab[:, l, :], m[bi][:, 0, cs], i_re, ALU.mult, ALU.add),
                         reads=[R_m[bi], RT] + rd_extra, writes=[R_g[bi]])
                    f.op("dve", lambda e: e.tensor_tensor_scan(g[bi][:, 1, cs], rtab[:, l, :], m[bi][:, 1, cs], i_im, ALU.mult, ALU.add),
                         reads=[R_m[bi], RT] + rd_extra, writes=[R_g[bi]])
                    prev = (g[bi], bi, c)

            def stC(k):
                pi, d, bidx, n, bi, dq, l, nch, ucols, ycols, tl, cb, sb_ = geom(k)
                mre, mim = v3(m[bi][:, 0, 0:n]), v3(m[bi][:, 1, 0:n])
                t_a, t_b = v3(ta[bi][:, 0, 0:n]), v3(ta[bi][:, 1, 0:n])
                gre, gim = v3(g[bi][:, 0, 0:n]), v3(g[bi][:, 1, 0:n])
                hre, him = v3(hb[bi][:, 0, 0:n]), v3(hb[bi][:, 1, 0:n])
                f.op("dve", lambda e: e.tensor_tensor(t_a, gre, cb, ALU.mult), reads=[R_g[bi], RT], writes=[R_ta[bi]])
                f.op("dve", lambda e: e.tensor_tensor(mre, gim, sb_, ALU.mult), reads=[R_g[bi], RT], writes=[R_m[bi]])
                f.op("dve", lambda e: e.tensor_tensor(hre, t_a, mre, ALU.subtract), reads=[R_ta[bi], R_m[bi]], writes=[R_hb[bi]])
                f.op("dve", lambda e: e.tensor_tensor(t_b, gre, sb_, ALU.mult), reads=[R_g[bi], RT], writes=[R_ta[bi]])
                f.op("dve", lambda e: e.tensor_tensor(mim, gim, cb, ALU.mult), reads=[R_g[bi], RT], writes=[R_m[bi]])
                f.op("dve", lambda e: e.tensor_tensor(him, t_b, mim, ALU.add), reads=[R_ta[bi], R_m[bi]], writes=[R_hb[bi]])
                for ri in range(2):
                    f.op("pe", lambda e, ri=ri: e.matmul(psy[bi][:, 0:n], lC[:, l, ri, :], hb[bi][:, ri, 0:n], start=(ri == 0), stop=(ri == 1)),
                         reads=[RT, R_hb[bi]], writes=[R_psy[bi]], acc=(ri > 0))

            def stY(k):
                pi, d, bidx, n, bi, dq, l, nch, ucols, ycols, tl, cb, sb_ = geom(k)
                f.op("dve", lambda e: e.tensor_tensor(ycols, psy[bi][:, 0:n], ycols, ALU.add), reads=[R_psy[bi], R_y], writes=[R_y])

            stA(0)
            for k in range(NI):
                if k + 1 < NI:
                    stA(k + 1)
                stB(k)
                stC(k)
                if k >= 1:
                    stY(k - 1)
            stY(NI - 1)
            for (s0, n) in blocks:
                yb = yacc[:, s0:s0 + n]
                f.op("pool", lambda e: e.tensor_tensor(gq1[:, 0:n], yb, yb, ALU.mult), reads=[R_y], writes=[R_gq1])
                f.op("dve", lambda e: e.tensor_scalar(out=gq1[:, 0:n], in0=gq1[:, 0:n], scalar1=0.044715, scalar2=1.0, op0=ALU.mult, op1=ALU.add), reads=[R_gq1], writes=[R_gq1])
                f.op("pool", lambda e: e.tensor_tensor(gq1[:, 0:n], gq1[:, 0:n], yb, ALU.mult), reads=[R_gq1, R_y], writes=[R_gq1])
                f.op("act", lambda e: e.activation(out=gq2[:, 0:n], in_=gq1[:, 0:n], func=AF.Sigmoid, scale=1.5957691216057308), reads=[R_gq1], writes=[R_gq2])
                f.op("dve", lambda e: e.tensor_tensor(aT[:, ct, s0:s0 + n], yb, gq2[:, 0:n], ALU.mult), reads=[R_gq2, R_y], writes=R_aT[s0 // 128:(s0 + n) // 128])
        sg = [S.sb("sg%d" % k, [128, 512], BF16) for k in range(2)]; R_sg = RL(2)
        anew = S.sb("anew", [128, 4, 512], BF16); R_anew = Res()
        nn = 0
        for (s0, n) in blocks:
            tl = R_aT[s0 // 128:(s0 + n) // 128]
            for cto in range(4):
                bi = nn % 2; nn += 1
                for cti in range(4):
                    f.op("pe", lambda e, cti=cti: e.matmul(psy[bi][:, 0:n], wgl[:, cti, cto * 128:(cto + 1) * 128], aT[:, cti, s0:s0 + n], start=(cti == 0), stop=(cti == 3)),
                         reads=[R_wgl] + tl, writes=[R_psy[bi]], acc=(cti > 0))
                f.op("act", lambda e: e.activation(out=sg[bi][:, 0:n], in_=psy[bi][:, 0:n], func=AF.Sigmoid, bias=bgl[:, cto:cto + 1], scale=1.0), reads=[R_psy[bi], R], writes=[R_sg[bi]])
                f.op("dve", lambda e: e.tensor_tensor(anew[:, cto, 0:n], aT[:, cto, s0:s0 + n], sg[bi][:, 0:n], ALU.mult), reads=[R_sg[bi]] + tl, writes=[R_anew])
            f.op("pool", lambda e: e.tensor_copy(aT[:, :, s0:s0 + n], anew[:, :, 0:n]), reads=[R_anew], writes=tl)
        S.close()
        P.close()

    def win_phase(qT, R_qT, kT2, R_kT, vaug, R_v, oT, R_oT):
        S = Scope(nc)
        esink = S.sb("esink", [128, 8]); R_es = Res()
        f.dma("sp", esink[:], win_sink.partition_broadcast(128), writes=[R_es])
        f.op("act", lambda e: e.activation(out=esink[:], in_=esink[:], func=AF.Exp), reads=[R_es], writes=[R_es])
        NBS = 3
        ps_s = [S.ps("ps_s%d" % k, [128, 8, 128]) for k in range(NBS)]; R_pss = RL(NBS)
        ps_o = [S.ps("ps_o%d" % k, [128, 512]) for k in range(2)]; R_pso = RL(2)
        pT = [S.sb("pT%d" % k, [128, 5, 128], BF16) for k in range(NBS)]; R_pT = RL(NBS)
        dtmp = [S.sb("dtmp%d" % k, [128, 128]) for k in range(2)]; R_dt = RL(2)
        items = []
        for t in range(NT):
            kts = [(0, None), (1, None)]
            if t >= 2:
                for kt in (t - 1, t, t + 1):
                    if 2 <= kt < NT:
                        kts.append((kt, (0 if kt == t - 1 else (1 if kt == t + 1 else None))))
            for h in range(8):
                items.append((t, h, kts))

        def front(i_):
            t, h, kts = items[i_]
            cols = slice(t * 128, (t + 1) * 128)
            nk = len(kts)
            bs = i_ % NBS
            pr, base, kvh = h // 2, 64 * (h % 2), h // 4
            for i, (kt, mk) in enumerate(kts):
                f.op("pe", lambda e, i=i, kt=kt: e.matmul(ps_s[bs][:, i, :], kT2[base:base + 64, kvh, kt * 128:(kt + 1) * 128], qT[base:base + 64, pr, cols], start=True, stop=True),
                     reads=[R_kT[kt], R_qT[t]], writes=[R_pss[bs]], acc=(i > 0))
            f.op("act", lambda e: e.activation(out=pT[bs][:, 0:nk, :], in_=ps_s[bs][:, 0:nk, :], func=AF.Exp, scale=0.125), reads=[R_pss[bs]], writes=[R_pT[bs]])
            for i, (kt, mk) in enumerate(kts):
                if mk is not None:
                    f.op("dve", lambda e, i=i, mk=mk: e.tensor_tensor(pT[bs][:, i, :], pT[bs][:, i, :], maskb[:, mk, :], ALU.mult), reads=[R_pT[bs], R_mask], writes=[R_pT[bs]])

        def back(i_):
            t, h, kts = items[i_]
            cols = slice(t * 128, (t + 1) * 128)
            nk = len(kts)
            bs = i_ % NBS
            bi = i_ % 2
            pr, base, kvh = h // 2, 64 * (h % 2), h // 4
            voff = (64 if h % 2 == 0 else 0) + 128 * kvh
            for i, (kt, mk) in enumerate(kts):
                f.op("pe", lambda e, i=i, kt=kt: e.matmul(ps_o[bi][:, 0:128], vaug[:, kt, voff:voff + 128], pT[bs][:, i, :], start=(i == 0), stop=(i == nk - 1)),
                     reads=[R_v[kt], R_pT[bs]], writes=[R_pso[bi]], acc=(i > 0))
            nb, db = (0, 64) if h % 2 == 0 else (64, 0)
            f.op("dve", lambda e: e.tensor_scalar(out=dtmp[bi][nb:nb + 64, :], in0=ps_o[bi][db:db + 64, 0:128], scalar1=esink[db:db + 64, h:h + 1], scalar2=None, op0=ALU.add),
                 reads=[R_pso[bi], R_es], writes=[R_dt[bi]])
            f.op("dve", lambda e: e.reciprocal(dtmp[bi][nb:nb + 64, :], dtmp[bi][nb:nb + 64, :]), reads=[R_dt[bi]], writes=[R_dt[bi]])
            f.op("dve", lambda e: e.tensor_tensor(oT[nb:nb + 64, pr, cols], ps_o[bi][nb:nb + 64, 0:128], dtmp[bi][nb:nb + 64, :], ALU.mult), reads=[R_pso[bi], R_dt[bi]], writes=[R_oT[t]])
        front(0)
        for i_ in range(len(items)):
            if i_ + 1 < len(items):
                front(i_ + 1)
            back(i_)
        S.close()

    def out_phase(layer, w_out_d, mix_loader, tiles):
        S = Scope(nc)
        load_ln(layer * 2 + 0)
        wo = S.sb("wo", [128, 8, D], BF16); R_wo = Res()
        f.dma("pool", wo[:], w_out_d.rearrange("(kc p) n -> p kc n", p=128), writes=[R_wo])
        xt = [S.sb("xt%d" % k, [128, D]) for k in range(2)]; R_xt = RL(2)
        ot = [S.sb("ot%d" % k, [128, D]) for k in range(2)]; R_ot = RL(2)
        tmp = S.sb("tmp", [128, D]); R_tmp = Res()
        small = S.sb("small", [128, 16]); R_small = Res()
        ps_o2 = [S.ps("ps_o2%d" % k, [128, D]) for k in range(2)]; R_ps = RL(2)
        for n, t in enumerate(tiles):
            b = n % 2
            src, rs = src_tile(layer, t)
            f.dma("sp", xt[b][:], src, reads=rs, writes=[R_xt[b]])
            mixT, R_mix = mix_loader(t, b)
            for half in range(2):
                for kc in range(8):
                    f.op("pe", lambda e, kc=kc, half=half: e.matmul(ps_o2[b][:, half * 512:(half + 1) * 512], mixT[kc], wo[:, kc, half * 512:(half + 1) * 512], start=(kc == 0), stop=(kc == 7)),
                         reads=[R_wo] + R_mix, writes=[R_ps[b]], acc=(half + kc > 0))
            resid_ln(S, xt[b], R_xt[b], ps_o2[b], R_ps[b], (1 if t < 2 else 0), layer * 2 + 0, ot[b], R_ot[b], tmp, R_tmp, small, R_small)
            f.dma("act", XR[t * 128:(t + 1) * 128, :], ot[b][:], reads=[R_ot[b]], writes=[R_XR[t]])
        S.close()

    def ffn_phase(layer, tiles_all, final):
        P = Scope(nc)
        rw = P.sb("rw", [128, 8, 32]); R_rw = Res()
        f.dma("sp", rw[:], router_w.rearrange("(kc p) n -> p kc n", p=128), writes=[R_rw])
        rbias = P.sb("rbias", [128, 32]); f.dma("sp", rbias[:], router_b.partition_broadcast(128), writes=[R_rw])
        GT = 9
        load_ln(layer * 2 + 1)
        groups = [tiles_all[i:i + GT] for i in range(0, len(tiles_all), GT)]
        for grp in groups:
            S = Scope(nc)
            ng = len(grp)
            hT = S.sb("hTg", [128, 8, GT * 128], BF16); R_hT = RL(ng, "hTg")
            comb = S.sb("comb", [128, GT, 32]); R_comb = RL(ng, "comb")
            yacc = S.sb("yaccg", [128, GT, D]); R_y = RL(ng, "yg")
            A = Scope(nc)
            xt = [A.sb("xt%d" % k, [128, D]) for k in range(2)]; R_xt = RL(2)
            h32 = A.sb("h32", [128, D]); R_h32 = Res()
            h32T = A.sb("h32T", [128, 8, 128]); R_h32T = Res()
            ps_tp = A.ps("ps_tp", [128, 8, 128]); R_pstp = Res(x=True)
            ps_r = A.ps("ps_r", [128, 512]); R_psr = Res()
            sc = A.sb("sc", [128, 32]); sel = A.sb("sel", [128, 32]); R_sc = Res()
            pa = A.sb("pa", [128, 8, 6]); pm = A.sb("pm", [128, 8, 6]); gs = A.sb("gs", [128, 8]); thr = A.sb("thr", [128, 8])
            gm = A.sb("gm", [128, 2]); mg = A.sb("mg", [128, 8]); sm = A.sb("sm", [128, 8, 4])
            for j, t in enumerate(grp):
                b = j % 2
                f.dma("sp", xt[b][:], XR[t * 128:(t + 1) * 128, :], reads=[R_XR[t]], writes=[R_xt[b]])
                which = 1 if t < 2 else 0
                mod_transpose(xt[b], R_xt[b], which, h32, R_h32, ps_tp, R_pstp, hT[:, :, j * 128:(j + 1) * 128], R_hT[j], h32T, R_h32T)
                for kc in range(8):
                    f.op("pe", lambda e, kc=kc: e.matmul(ps_r[:, 0:32], h32T[:, kc, :], rw[:, kc, :], start=(kc == 0), stop=(kc == 7)), reads=[R_h32T, R_rw], writes=[R_psr], acc=(kc > 0))
                R1 = R_sc
                f.op("act", lambda e: e.activation(out=sc[:], in_=ps_r[:, 0:32], func=AF.Sigmoid), reads=[R_psr], writes=[R1])
                f.op("dve", lambda e: e.tensor_tensor(sel[:], sc[:], rbias[:], ALU.add), reads=[R1, R_rw], writes=[R1])
                s3 = sel[:].rearrange("p (g e) -> p g e", e=4)
                pairs = [(0, 1), (0, 2), (0, 3), (1, 2), (1, 3), (2, 3)]
                for k, (a_, b_) in enumerate(pairs):
                    f.op("dve", lambda e, k=k, a_=a_, b_=b_: e.tensor_tensor(pa[:, :, k], s3[:, :, a_], s3[:, :, b_], ALU.add), reads=[R1], writes=[R1])
                    f.op("dve", lambda e, k=k, a_=a_, b_=b_: e.tensor_tensor(pm[:, :, k], s3[:, :, a_], s3[:, :, b_], ALU.min), reads=[R1], writes=[R1])
                f.op("dve", lambda e: e.tensor_reduce(out=gs[:], in_=pa[:], axis=AX.X, op=ALU.max), reads=[R1], writes=[R1])
                f.op("dve", lambda e: e.tensor_reduce(out=thr[:], in_=pm[:], axis=AX.X, op=ALU.max), reads=[R1], writes=[R1])
                f.op("dve", lambda e: e.tensor_reduce(out=gm[:, 0:1], in_=gs[:], axis=AX.X, op=ALU.max), reads=[R1], writes=[R1])
                f.op("dve", lambda e: e.tensor_scalar(out=mg[:], in0=gs[:], scalar1=gm[:, 0:1], scalar2=None, op0=ALU.is_ge), reads=[R1], writes=[R1])
                f.op("dve", lambda e: e.tensor_tensor(sm[:], s3, thr[:].unsqueeze(2).broadcast_to([128, 8, 4]), ALU.is_ge), reads=[R1], writes=[R1])
                f.op("dve", lambda e: e.tensor_tensor(sm[:], sm[:], mg[:].unsqueeze(2).broadcast_to([128, 8, 4]), ALU.mult), reads=[R1], writes=[R1])
                cj = comb[:, j, :]
                f.op("dve", lambda e: e.tensor_tensor(cj, sm[:].rearrange("p g e -> p (g e)"), sc[:], ALU.mult), reads=[R1], writes=[R_comb[j]])
                f.op("dve", lambda e: e.tensor_reduce(out=gm[:, 1:2], in_=cj, axis=AX.X, op=ALU.add), reads=[R_comb[j], R1], writes=[R1])
                f.op("dve", lambda e: e.reciprocal(gm[:, 1:2], gm[:, 1:2]), reads=[R1], writes=[R1])
                f.op("dve", lambda e: e.tensor_scalar(out=cj, in0=cj, scalar1=gm[:, 1:2], scalar2=None, op0=ALU.mult), reads=[R1, R_comb[j]], writes=[R_comb[j]])
            A.close()
            B = Scope(nc)
            wg = [B.sb("wg%d" % k, [128, 8, 512], BF16) for k in range(2)]
            wu = [B.sb("wu%d" % k, [128, 8, 512], BF16) for k in range(2)]
            wd = [B.sb("wd%d" % k, [128, 4, D], BF16) for k in range(2)]
            R_w = RL(2, "w")
            psg = [B.ps("psg%d" % k, [128, 512]) for k in range(2)]; R_psg = RL(2)
            psu = [B.ps("psu%d" % k, [128, 512]) for k in range(2)]; R_psu = RL(2)
            psd = [B.ps("psd%d" % k, [128, D]) for k in range(2)]; R_psd = RL(2)
            sg = [B.sb("sg%d" % k, [128, 512]) for k in range(2)]; R_sg = RL(2)
            hid = [B.sb("hid%d" % k, [128, 4, 512], BF16) for k in range(2)]; R_hid = RL(2)
            ntok = ng * 128
            blocks = [(c0, min(512, ntok - c0)) for c0 in range(0, ntok, 512)]
            nfc = 0; nblk = 0; nd = 0
            for ex in range(32):
                wb = ex % 2
                f.dma("pool", wg[wb][:], w_gate[layer, ex].rearrange("(kc p) n -> p kc n", p=128), writes=[R_w[wb]])
                f.dma("pool", wu[wb][:], w_up[layer, ex].rearrange("(kc p) n -> p kc n", p=128), writes=[R_w[wb]])
                f.dma("pool", wd[wb][:], w_down[layer, ex].rearrange("(kc p) n -> p kc n", p=128), writes=[R_w[wb]])
                for (c0, n) in blocks:
                    hb_ = nblk % 2; nblk += 1
                    tl = R_hT[c0 // 128:(c0 + n) // 128]
                    for fc in range(4):
                        pb = nfc % 2; nfc += 1
                        for kc in range(8):
                            f.op("pe", lambda e, kc=kc, fc=fc: e.matmul(psg[pb][:, 0:n], wg[wb][:, kc, fc * 128:(fc + 1) * 128], hT[:, kc, c0:c0 + n], start=(kc == 0), stop=(kc == 7)),
                                 reads=[R_w[wb]] + tl, writes=[R_psg[pb]], acc=(kc > 0))
                        for kc in range(8):
                            f.op("pe", lambda e, kc=kc, fc=fc: e.matmul(psu[pb][:, 0:n], wu[wb][:, kc, fc * 128:(fc + 1) * 128], hT[:, kc, c0:c0 + n], start=(kc == 0), stop=(kc == 7)),
                                 reads=[R_w[wb]] + tl, writes=[R_psu[pb]], acc=(kc > 0))
                        f.op("act", lambda e: e.activation(out=sg[pb][:, 0:n], in_=psg[pb][:, 0:n], func=AF.Silu), reads=[R_psg[pb]], writes=[R_sg[pb]])
                        f.op("dve", lambda e, fc=fc: e.tensor_tensor(hid[hb_][:, fc, 0:n], sg[pb][:, 0:n], psu[pb][:, 0:n], ALU.mult), reads=[R_sg[pb], R_psu[pb]], writes=[R_hid[hb_]])
                    for tt in range(n // 128):
                        j = c0 // 128 + tt
                        db = nd % 2; nd += 1
                        for half in range(2):
                            for fc in range(4):
                                f.op("pe", lambda e, fc=fc, half=half: e.matmul(psd[db][:, half * 512:(half + 1) * 512], hid[hb_][:, fc, tt * 128:(tt + 1) * 128], wd[wb][:, fc, half * 512:(half + 1) * 512], start=(fc == 0), stop=(fc == 3)),
                                     reads=[R_w[wb], R_hid[hb_]], writes=[R_psd[db]], acc=(half + fc > 0))
                        cw = comb[:, j, ex:ex + 1]
                        if ex == 0:
                            f.op("dve", lambda e: e.tensor_scalar(out=yacc[:, j, :], in0=psd[db][:], scalar1=cw, scalar2=None, op0=ALU.mult), reads=[R_psd[db], R_comb[j]], writes=[R_y[j]])
                        else:
                            f.op("dve", lambda e: e.scalar_tensor_tensor(out=yacc[:, j, :], in0=psd[db][:], scalar=cw, in1=yacc[:, j, :], op0=ALU.mult, op1=ALU.add), reads=[R_psd[db], R_comb[j], R_y[j]], writes=[R_y[j]])
            B.close()
            C = Scope(nc)
            xt = [C.sb("xt%d" % k, [128, D]) for k in range(2)]; R_xt = RL(2)
            ot = [C.sb("ot%d" % k, [128, D]) for k in range(2)]; R_ot = RL(2)
            tmp = C.sb("tmp", [128, D]); R_tmp = Res()
            small = C.sb("small", [128, 16]); R_small = Res()
            for j, t in enumerate(grp):
                b = j % 2
                f.dma("sp", xt[b][:], XR[t * 128:(t + 1) * 128, :], reads=[R_XR[t]], writes=[R_xt[b]])
                yj = yacc[:, j, :]

                class _V:
                    def __init__(self, ap): self.ap = ap
                    def __getitem__(self, k): return self.ap
                resid_ln(C, xt[b], R_xt[b], _V(yj), R_y[j], (1 if t < 2 else 0), layer * 2 + 1, ot[b], R_ot[b], tmp, R_tmp, small, R_small)
                if final:
                    f.dma("act", out_d[(t - 2) * 128:(t - 1) * 128, :], ot[b][:], reads=[R_ot[b]], writes=[R_out])
                else:
                    f.dma("act", XR[t * 128:(t + 1) * 128, :], ot[b][:], reads=[R_ot[b]], writes=[R_XR[t]])
            C.close()
            S.close()
        P.close()


    def ffn_sparse(layer, tiles_all, final):
        IOA = bass.IndirectOffsetOnAxis
        ng = len(tiles_all)
        M = ng * 32
        P = Scope(nc)
        rw = P.sb("rw", [128, 8, 32]); R_rw = Res()
        f.dma("sp", rw[:], router_w.rearrange("(kc p) n -> p kc n", p=128), writes=[R_rw])
        rbias = P.sb("rbias", [128, 32]); f.dma("sp", rbias[:], router_b.partition_broadcast(128), writes=[R_rw])
        jt = P.sb("jt2", [128, 128]); f.dma("sp", jt[:], k_jidx, writes=[R_rw])
        pc = P.sb("pc", [128, 4]); f.dma("sp", pc[:], k_pc, writes=[R_rw])
        load_ln(layer * 2 + 1)
        comb = P.sb("comb", [128, ng, 32]); R_comb = RL(ng, "comb")
        posA_i = P.sb("posA_i", [128, ng], I32); posB_i = P.sb("posB_i", [128, ng], I32)
        wA = P.sb("wA", [128, ng]); wB = P.sb("wB", [128, ng])
        NSO = NS - 32
        idxw = P.sb("idxw", [128, NSO, 4], I32)
        R_rt = Res("route")
        R_XsW = RL(ng, "xsw")
        R_Ys = RL(NS, "ys")
        HB = Scope(nc)
        hb_all = HB.sb("hb_all", [128, ng, D], BF16); R_hb = RL(ng, "hb")
        A = Scope(nc)
        xt = [A.sb("xt%d" % k, [128, D]) for k in range(2)]; R_xt = RL(2)
        h32 = [A.sb("h32%d" % k, [128, D]) for k in range(2)]; R_h32 = RL(2)
        h32T = A.sb("h32T", [128, 8, 128]); R_h32T = Res()
        ps_tp = [A.ps("ps_tp%d" % k, [128, 8, 128]) for k in range(2)]; R_pstp = RL(2)
        ps_r = A.ps("ps_r", [128, 512]); R_psr = Res()
        sc = A.sb("sc", [128, 32]); sel = A.sb("sel", [128, 32]); R_sc = Res()
        pa = A.sb("pa", [128, 8, 6]); pm = A.sb("pm", [128, 8, 6]); gs = A.sb("gs", [128, 8]); thr = A.sb("thr", [128, 8])
        gm = A.sb("gm", [128, 2]); mg = A.sb("mg", [128, 8]); sm = A.sb("sm", [128, 8, 4])
        for j, t in enumerate(tiles_all):
            b = j % 2
            f.dma("sp", xt[b][:], XR[t * 128:(t + 1) * 128, :], reads=[R_XR[t]], writes=[R_xt[b]])
            which = 1 if t < 2 else 0
            f.op("dve", lambda e: e.tensor_tensor(h32[b][:], xt[b][:], mod[:, which, 1, :], ALU.mult), reads=[R_xt[b], R_mod], writes=[R_h32[b]])
            f.op("dve", lambda e: e.tensor_tensor(h32[b][:], h32[b][:], mod[:, which, 0, :], ALU.add), reads=[R_h32[b], R_mod], writes=[R_h32[b]])
            for kc in range(8):
                f.op("pe", lambda e, kc=kc: e.transpose(ps_tp[b][:, kc, :], h32[b][:, kc * 128:(kc + 1) * 128], ident[:]),
                     reads=[R_h32[b], R_ident], writes=[R_pstp[b]], acc=(kc > 0))
            f.op("dve", lambda e: e.tensor_copy(h32T[:], ps_tp[b][:]), reads=[R_pstp[b]], writes=[R_h32T])
            f.op("act", lambda e: e.activation(out=hb_all[:, j, :].rearrange("p (c j q) -> p c j q", c=4, j=2),
                                               in_=h32[b][:].rearrange("p (c q j) -> p c j q", c=4, j=2), func=AF.Identity),
                 reads=[R_h32[b]], writes=[R_hb[j]])
            for kc in range(8):
                f.op("pe", lambda e, kc=kc: e.matmul(ps_r[:, 0:32], h32T[:, kc, :], rw[:, kc, :], start=(kc == 0), stop=(kc == 7)), reads=[R_h32T, R_rw], writes=[R_psr], acc=(kc > 0))
            R1 = R_sc
            f.op("act", lambda e: e.activation(out=sc[:], in_=ps_r[:, 0:32], func=AF.Sigmoid), reads=[R_psr], writes=[R1])
            f.op("dve", lambda e: e.tensor_tensor(sel[:], sc[:], rbias[:], ALU.add), reads=[R1, R_rw], writes=[R1])
            s3 = sel[:].rearrange("p (g e) -> p g e", e=4)
            pairs = [(0, 1), (0, 2), (0, 3), (1, 2), (1, 3), (2, 3)]
            for k, (a_, b_) in enumerate(pairs):
                f.op("dve", lambda e, k=k, a_=a_, b_=b_: e.tensor_tensor(pa[:, :, k], s3[:, :, a_], s3[:, :, b_], ALU.add), reads=[R1], writes=[R1])
                f.op("dve", lambda e, k=k, a_=a_, b_=b_: e.tensor_tensor(pm[:, :, k], s3[:, :, a_], s3[:, :, b_], ALU.min), reads=[R1], writes=[R1])
            f.op("dve", lambda e: e.tensor_reduce(out=gs[:], in_=pa[:], axis=AX.X, op=ALU.max), reads=[R1], writes=[R1])
            f.op("dve", lambda e: e.tensor_reduce(out=thr[:], in_=pm[:], axis=AX.X, op=ALU.max), reads=[R1], writes=[R1])
            f.op("dve", lambda e: e.tensor_reduce(out=gm[:, 0:1], in_=gs[:], axis=AX.X, op=ALU.max), reads=[R1], writes=[R1])
            f.op("dve", lambda e: e.tensor_scalar(out=mg[:], in0=gs[:], scalar1=gm[:, 0:1], scalar2=None, op0=ALU.is_ge), reads=[R1], writes=[R1])
            f.op("dve", lambda e: e.tensor_tensor(sm[:], s3, thr[:].unsqueeze(2).broadcast_to([128, 8, 4]), ALU.is_ge), reads=[R1], writes=[R1])
            f.op("dve", lambda e: e.tensor_tensor(sm[:], sm[:], mg[:].unsqueeze(2).broadcast_to([128, 8, 4]), ALU.mult), reads=[R1], writes=[R1])
            cj = comb[:, j, :]
            f.op("dve", lambda e: e.tensor_tensor(cj, sm[:].rearrange("p g e -> p (g e)"), sc[:], ALU.mult), reads=[R1], writes=[R_comb[j]])
            f.op("dve", lambda e: e.tensor_reduce(out=gm[:, 1:2], in_=cj, axis=AX.X, op=ALU.add), reads=[R_comb[j], R1], writes=[R1])
            f.op("dve", lambda e: e.reciprocal(gm[:, 1:2], gm[:, 1:2]), reads=[R1], writes=[R1])
            f.op("dve", lambda e: e.tensor_scalar(out=cj, in0=cj, scalar1=gm[:, 1:2], scalar2=None, op0=ALU.mult), reads=[R1, R_comb[j]], writes=[R_comb[j]])
        A.close()
        Bq = Scope(nc)
        m_ = Bq.sb("m_", [128, ng, 32]); mb16 = Bq.sb("mb16", [128, ng, 32], BF16)
        rank = Bq.sb("rank", [128, ng, 32]); tot = Bq.sb("tot", [128, ng, 32]); base = Bq.sb("base", [128, ng, 32])
        me = Bq.sb("me", [128, ng, 32]); Bm = Bq.sb("Bm", [128, ng, 32]); Am = Bq.sb("Am", [128, ng, 32]); tmpq = Bq.sb("tmpq", [128, ng, 32])
        ones16 = Bq.sb("ones16", [128, 128], BF16)
        cnt = Bq.sb("cnt", [128, 32]); cmp17 = Bq.sb("cmp17", [128, 32, 18]); thr18 = Bq.sb("thr18", [128, 18])
        tlf = Bq.sb("tlf", [128, 32]); sinc = Bq.sb("sinc", [128, 32]); so512 = Bq.sb("so512", [128, 32]); c1e = Bq.sb("c1e", [128, 32])
        mx = Bq.sb("mx", [128, ng]); pAf = Bq.sb("pAf", [128, ng]); pBf = Bq.sb("pBf", [128, ng])
        cmpj = Bq.sb("cmpj", [128, NSO, 32]); eidf = Bq.sb("eidf", [128, NSO]); idxf = Bq.sb("idxf", [128, NSO, 4])
        ps_rk = Bq.ps("ps_rk", [128, 3, 512]); ps_tt = Bq.ps("ps_tt", [128, 3, 512])
        RB = [R_rt]

        def fl(ap3):
            return ap3.rearrange("p a b -> p (a b)")
        f.op("dve", lambda e: e.tensor_scalar(out=fl(m_[:]), in0=fl(comb[:]), scalar1=0.0, scalar2=None, op0=ALU.is_gt), reads=R_comb, writes=RB)
        f.op("dve", lambda e: e.tensor_copy(fl(mb16[:]), fl(m_[:])), reads=RB, writes=RB)
        f.op("pool", lambda e: e.memset(ones16[:], 1.0), reads=RB, writes=RB)
        chunks = [(n0, min(M, n0 + 512)) for n0 in range(0, M, 512)]
        for ch, (n0, n1) in enumerate(chunks):
            f.op("pe", lambda e, ch=ch, n0=n0, n1=n1: e.matmul(ps_rk[:, ch, 0:n1 - n0], maskb[:, 2, :], fl(mb16[:])[:, n0:n1], start=True, stop=True), reads=RB + [R_mask], writes=RB)
            f.op("pe", lambda e, ch=ch, n0=n0, n1=n1: e.matmul(ps_tt[:, ch, 0:n1 - n0], ones16[:], fl(mb16[:])[:, n0:n1], start=True, stop=True), reads=RB, writes=RB)
        for ch, (n0, n1) in enumerate(chunks):
            f.op("dve", lambda e, ch=ch, n0=n0, n1=n1: e.tensor_copy(fl(rank[:])[:, n0:n1], ps_rk[:, ch, 0:n1 - n0]), reads=RB, writes=RB)
            f.op("dve", lambda e, ch=ch, n0=n0, n1=n1: e.tensor_copy(fl(tot[:])[:, n0:n1], ps_tt[:, ch, 0:n1 - n0]), reads=RB, writes=RB)
        f.op("dve", lambda e: e.memset(base[:, 0, :], 0.0), reads=RB, writes=RB)
        for t_ in range(1, ng):
            f.op("dve", lambda e, t_=t_: e.tensor_tensor(base[:, t_, :], base[:, t_ - 1, :], tot[:, t_ - 1, :], ALU.add), reads=RB, writes=RB)
        f.op("dve", lambda e: e.tensor_tensor(cnt[:], base[:, ng - 1, :], tot[:, ng - 1, :], ALU.add), reads=RB, writes=RB)
        f.op("dve", lambda e: e.tensor_scalar(out=thr18[:], in0=jt[:, 0:18], scalar1=512.0, scalar2=None, op0=ALU.mult), reads=RB + [R_rw], writes=RB)
        f.op("dve", lambda e: e.tensor_tensor(cmp17[:], cnt[:].unsqueeze(2).broadcast_to([128, 32, 18]), thr18[:].unsqueeze(1).broadcast_to([128, 32, 18]), ALU.is_gt), reads=RB, writes=RB)
        f.op("dve", lambda e: e.tensor_reduce(out=tlf[:], in_=cmp17[:], axis=AX.X, op=ALU.add), reads=RB, writes=RB)
        f.op("dve", lambda e: e.tensor_scalar(out=tlf[:], in0=tlf[:], scalar1=-1.0, scalar2=0.0, op0=ALU.add, op1=ALU.max), reads=RB, writes=RB)
        f.op("dve", lambda e: e.tensor_copy(sinc[:], tlf[:]), reads=RB, writes=RB)
        for e_ in range(1, 32):
            f.op("dve", lambda e, e_=e_: e.tensor_tensor(sinc[:, e_:e_ + 1], sinc[:, e_ - 1:e_], tlf[:, e_:e_ + 1], ALU.add), reads=RB, writes=RB)
        f.op("dve", lambda e: e.tensor_tensor(so512[:], sinc[:], tlf[:], ALU.subtract), reads=RB, writes=RB)
        f.op("dve", lambda e: e.tensor_scalar(out=so512[:], in0=so512[:], scalar1=512.0, scalar2=15872.0, op0=ALU.mult, op1=ALU.add), reads=RB, writes=RB)
        f.op("dve", lambda e: e.tensor_scalar(out=c1e[:], in0=jt[:, 0:32], scalar1=512.0, scalar2=None, op0=ALU.mult), reads=RB + [R_rw], writes=RB)
        f.op("dve", lambda e: e.tensor_tensor(so512[:], so512[:], c1e[:], ALU.subtract), reads=RB, writes=RB)
        f.op("dve", lambda e: e.tensor_tensor(fl(rank[:]), fl(rank[:]), fl(base[:]), ALU.add), reads=RB, writes=RB)
        f.op("dve", lambda e: e.tensor_scalar(out=fl(tmpq[:]), in0=fl(rank[:]), scalar1=512.0, scalar2=None, op0=ALU.is_ge), reads=RB, writes=RB)
        f.op("dve", lambda e: e.tensor_tensor(tmpq[:], tmpq[:], so512[:].unsqueeze(1).broadcast_to([128, ng, 32]), ALU.mult), reads=RB, writes=RB)
        f.op("dve", lambda e: e.tensor_tensor(rank[:], rank[:], c1e[:].unsqueeze(1).broadcast_to([128, ng, 32]), ALU.add), reads=RB, writes=RB)
        f.op("dve", lambda e: e.tensor_tensor(fl(rank[:]), fl(rank[:]), fl(tmpq[:]), ALU.add), reads=RB, writes=RB)
        f.op("dve", lambda e: e.tensor_tensor(me[:], m_[:], jt[:, 1:33].unsqueeze(1).broadcast_to([128, ng, 32]), ALU.mult), reads=RB, writes=RB)
        f.op("dve", lambda e: e.tensor_reduce(out=mx[:], in_=me[:], axis=AX.X, op=ALU.max), reads=RB, writes=RB)
        f.op("dve", lambda e: e.tensor_tensor(Bm[:], me[:], mx[:].unsqueeze(2).broadcast_to([128, ng, 32]), ALU.is_equal), reads=RB, writes=RB)
        f.op("dve", lambda e: e.tensor_tensor(fl(Am[:]), fl(m_[:]), fl(Bm[:]), ALU.subtract), reads=RB, writes=RB)
        for (msk, pf, wf) in ((Am, pAf, wA), (Bm, pBf, wB)):
            f.op("dve", lambda e, msk=msk: e.tensor_tensor(fl(tmpq[:]), fl(msk[:]), fl(rank[:]), ALU.mult), reads=RB, writes=RB)
            f.op("dve", lambda e, pf=pf: e.tensor_reduce(out=pf[:], in_=tmpq[:], axis=AX.X, op=ALU.add), reads=RB, writes=RB)
            f.op("dve", lambda e, msk=msk: e.tensor_tensor(fl(tmpq[:]), fl(msk[:]), fl(comb[:]), ALU.mult), reads=RB + R_comb, writes=RB)
            f.op("dve", lambda e, wf=wf: e.tensor_reduce(out=wf[:], in_=tmpq[:], axis=AX.X, op=ALU.add), reads=RB, writes=RB)
        f.op("dve", lambda e: e.tensor_copy(posA_i[:], pAf[:]), reads=RB, writes=RB)
        f.op("dve", lambda e: e.tensor_copy(posB_i[:], pBf[:]), reads=RB, writes=RB)
        f.op("dve", lambda e: e.tensor_tensor(cmpj[:], sinc[:].unsqueeze(1).broadcast_to([128, NSO, 32]), jt[:, 0:NSO].unsqueeze(2).broadcast_to([128, NSO, 32]), ALU.is_le), reads=RB, writes=RB)
        f.op("dve", lambda e: e.tensor_reduce(out=eidf[:], in_=cmpj[:], axis=AX.X, op=ALU.add), reads=RB, writes=RB)
        f.op("dve", lambda e: e.tensor_scalar(out=eidf[:], in0=eidf[:], scalar1=32.0, scalar2=512.0, op0=ALU.min, op1=ALU.mult), reads=RB, writes=RB)
        f.op("dve", lambda e: e.tensor_scalar(out=eidf[:], in0=eidf[:], scalar1=float(layer * 16384), scalar2=None, op0=ALU.add), reads=RB, writes=RB)
        f.op("dve", lambda e: e.tensor_tensor(idxf[:], eidf[:].unsqueeze(2).broadcast_to([128, NSO, 4]), pc[:].unsqueeze(1).broadcast_to([128, NSO, 4]), ALU.add), reads=RB, writes=RB)
        f.op("dve", lambda e: e.tensor_copy(idxw[:], idxf[:]), reads=RB, writes=RB)
        for j in range(ng):
            for pi_ in (posA_i, posB_i):
                f._dma_common("pool", lambda e, j=j, pi_=pi_: e.indirect_dma_start(out=XS, out_offset=IOA(ap=pi_[:, j:j + 1], axis=0), in_=hb_all[:, j, :], in_offset=None),
                              [R_hb[j]] + RB + R_XsZ, [R_XsW[j]])
        Bq.close()
        HB.close()
        Sd = Scope(nc)
        wg = [Sd.sb("wg%d" % k, [128, 4, 2, 512], BF16) for k in range(2)]
        wu = [Sd.sb("wu%d" % k, [128, 4, 2, 512], BF16) for k in range(2)]
        wd = [Sd.sb("wd%d" % k, [128, 4, D], BF16) for k in range(2)]
        R_w = RL(2, "w"); R_wd = RL(2, "wd")
        xs = [[Sd.sb("xs%d_%d" % (a_, tt), [128, D], BF16) for tt in range(4)] for a_ in range(2)]
        R_xs = [RL(4, "xs%d" % a_) for a_ in range(2)]

        def xs_load(jn):
            for tt in range(4):
                r0 = (jn * 4 + tt) * 128
                f.dma("sp", xs[jn % 2][tt][:], XS[r0:r0 + 128, :], reads=R_XsW, writes=[R_xs[jn % 2][tt]])
        xT = [Sd.sb("xT%d" % k, [128, 8, 512], BF16) for k in range(2)]; R_xT = RL(2)
        ps_t = [Sd.ps("ps_t%d" % k, [128, 8, 128], BF16) for k in range(2)]; R_pst = RL(2)
        psg = [Sd.ps("psg%d" % k, [128, 512]) for k in range(2)]; R_psg = RL(2)
        psu = [Sd.ps("psu%d" % k, [128, 512]) for k in range(2)]; R_psu = RL(2)
        psd = Sd.ps("psd", [128, D]); R_psd = Res()
        sg = [Sd.sb("sg%d" % k, [128, 512]) for k in range(2)]; R_sg = RL(2)
        hid = [Sd.sb("hid%d" % k, [128, 4, 512], BF16) for k in range(2)]; R_hid = RL(2)
        ysb = [Sd.sb("ysb%d" % k, [128, D]) for k in range(2)]; R_ysb = RL(2)
        nfc = 0; nx = 0; ny = 0
        bc_reg = nc.gpsimd.alloc_register("bc%d" % layer)
        nc.gpsimd.reg_mov(bc_reg, 16383 + layer * 16384)
        stg_g = Sd.sb("stg_g", [128, 4, 2, 512]); stg_u = Sd.sb("stg_u", [128, 4, 2, 512])
        R_sg_ = Res("stg_g"); R_su_ = Res("stg_u")

        def w_load(ex):
            f.dma("sp", stg_g[:], w_gate[layer, ex].rearrange("(c q j) n -> q c j n", c=4, j=2), writes=[R_sg_])
            f.dma("sp", stg_u[:], w_up[layer, ex].rearrange("(c q j) n -> q c j n", c=4, j=2), writes=[R_su_])
            f.dma("pool", wd[ex % 2][:], w_down[layer, ex].rearrange("(c p) n -> p c n", p=128), writes=[R_wd[ex % 2]])

        def w_cast(ex):
            wb_ = ex % 2
            f.op("act", lambda e: e.activation(out=wg[wb_][:], in_=stg_g[:], func=AF.Identity), reads=[R_sg_], writes=[R_w[wb_]])
            f.op("dve", lambda e: e.tensor_copy(wu[wb_][:], stg_u[:]), reads=[R_su_], writes=[R_w[wb_]])
        xs_load(0)
        w_load(0)
        w_cast(0)
        for j in range(NS):
            wb = j % 2
            if j + 1 < NS:
                xs_load(j + 1)
            if j + 1 < 32:
                w_load(j + 1)
            if j >= 32:
                jo = j - 32
                for c in range(4):
                    for (wt_, rows_) in ((wg, wg_rows), (wu, wu_rows)):
                        f._dma_common("pool", lambda e, c=c, wt_=wt_, rows_=rows_: e.indirect_dma_start(out=wt_[wb][:, c, :, :].rearrange("p a n -> p (a n)"), out_offset=None, in_=rows_[layer],
                                                                                                   in_offset=IOA(ap=idxw[:, jo, c:c + 1], axis=0), bounds_check=bc_reg, oob_is_err=False),
                                      RB, [R_w[wb]])
                for c in range(4):
                    f._dma_common("pool", lambda e, c=c: e.indirect_dma_start(out=wd[wb][:, c, :], out_offset=None, in_=wd_rows[layer], in_offset=IOA(ap=idxw[:, jo, c:c + 1], axis=0), bounds_check=bc_reg, oob_is_err=False),
                                  RB, [R_wd[wb]])
            for tt in range(4):
                for kc in range(8):
                    f.op("pe", lambda e, kc=kc, tt=tt: e.transpose(ps_t[tt % 2][:, kc, :], xs[j % 2][tt][:, kc * 128:(kc + 1) * 128], identb[:]), reads=[R_xs[j % 2][tt], R_identb], writes=[R_pst[tt % 2]], acc=(kc > 0))
                f.op("dve", lambda e, tt=tt: e.tensor_copy(xT[wb][:, :, tt * 128:(tt + 1) * 128], ps_t[tt % 2][:]), reads=[R_pst[tt % 2]], writes=[R_xT[wb]])
            hb_ = j % 2
            for fc in range(4):
                pb = nfc % 2; nfc += 1
                for kc in range(8):
                    f.op("pe", lambda e, kc=kc, fc=fc: e.matmul(psg[pb][:], wg[wb][:, kc // 2, kc % 2, fc * 128:(fc + 1) * 128], xT[wb][:, kc, :], start=(kc == 0), stop=(kc == 7)),
                         reads=[R_w[wb], R_xT[wb]], writes=[R_psg[pb]], acc=(kc > 0))
                for kc in range(8):
                    f.op("pe", lambda e, kc=kc, fc=fc: e.matmul(psu[pb][:], wu[wb][:, kc // 2, kc % 2, fc * 128:(fc + 1) * 128], xT[wb][:, kc, :], start=(kc == 0), stop=(kc == 7)),
                         reads=[R_w[wb], R_xT[wb]], writes=[R_psu[pb]], acc=(kc > 0))
                f.op("act", lambda e: e.activation(out=sg[pb][:], in_=psg[pb][:], func=AF.Silu), reads=[R_psg[pb]], writes=[R_sg[pb]])
                f.op("dve", lambda e, fc=fc: e.tensor_tensor(hid[hb_][:, fc, :], sg[pb][:], psu[pb][:], ALU.mult), reads=[R_sg[pb], R_psu[pb]], writes=[R_hid[hb_]])
            for tt in range(4):
                yb_ = ny % 2; ny += 1
                for half in range(2):
                    for fc in range(4):
                        f.op("pe", lambda e, fc=fc, half=half, tt=tt: e.matmul(psd[:, half * 512:(half + 1) * 512], hid[hb_][:, fc, tt * 128:(tt + 1) * 128], wd[wb][:, fc, half * 512:(half + 1) * 512], start=(fc == 0), stop=(fc == 3)),
                             reads=[R_wd[wb], R_hid[hb_]], writes=[R_psd], acc=(half + fc > 0))
                f.op("dve", lambda e: e.tensor_copy(ysb[yb_][:], psd[:]), reads=[R_psd], writes=[R_ysb[yb_]])
                r0 = (j * 4 + tt) * 128
                f.dma("act", YS[r0:r0 + 128, :], ysb[yb_][:], reads=[R_ysb[yb_]], writes=[R_Ys[j]])
            if j + 1 < 32:
                w_cast(j + 1)
        Sd.close()
        nc.gpsimd.free_register(bc_reg)
        C = Scope(nc)
        xt = [C.sb("xt%d" % k, [128, D]) for k in range(2)]; R_xt = RL(2)
        ot = [C.sb("ot%d" % k, [128, D]) for k in range(2)]; R_ot = RL(2)
        ya = [C.sb("ya%d" % k, [128, D]) for k in range(2)]; R_ya = RL(2)
        yb2 = [C.sb("yb%d" % k, [128, D]) for k in range(2)]; R_yb = RL(2)
        tmp = C.sb("tmp", [128, D]); R_tmp = Res()
        small = C.sb("small", [128, 16]); R_small = Res()

        class _V:
            def __init__(self, ap): self.ap = ap
            def __getitem__(self, k): return self.ap
        for j, t in enumerate(tiles_all):
            b = j % 2
            f.dma("sp", xt[b][:], XR[t * 128:(t + 1) * 128, :], reads=[R_XR[t]], writes=[R_xt[b]])
            f._dma_common("pool", lambda e: e.indirect_dma_start(out=ya[b][:], out_offset=None, in_=YS, in_offset=IOA(ap=posA_i[:, j:j + 1], axis=0)), R_Ys + RB, [R_ya[b]])
            f._dma_common("pool", lambda e: e.indirect_dma_start(out=yb2[b][:], out_offset=None, in_=YS, in_offset=IOA(ap=posB_i[:, j:j + 1], axis=0)), R_Ys + RB, [R_yb[b]])
            f.op("dve", lambda e: e.tensor_scalar(out=ya[b][:], in0=ya[b][:], scalar1=wA[:, j:j + 1], scalar2=None, op0=ALU.mult), reads=[R_ya[b]] + RB, writes=[R_ya[b]])
            f.op("dve", lambda e: e.scalar_tensor_tensor(out=ya[b][:], in0=yb2[b][:], scalar=wB[:, j:j + 1], in1=ya[b][:], op0=ALU.mult, op1=ALU.add), reads=[R_yb[b], R_ya[b]] + RB, writes=[R_ya[b]])
            resid_ln(C, xt[b], R_xt[b], _V(ya[b][:]), R_ya[b], (1 if t < 2 else 0), layer * 2 + 1, ot[b], R_ot[b], tmp, R_tmp, small, R_small)
            if final:
                f.dma("act", out_d[(t - 2) * 128:(t - 1) * 128, :], ot[b][:], reads=[R_ot[b]], writes=[R_out])
            else:
                f.dma("act", XR[t * 128:(t + 1) * 128, :], ot[b][:], reads=[R_ot[b]], writes=[R_XR[t]])
        C.close()
        P.close()

    def layer1_mixer():
        L = Scope(nc)
        kT2 = L.sb("kT2b", [128, 4, NTOK], BF16); R_kT = RL(NT, "kT")
        vaug = L.sb("vaugb", [128, NT, 576], BF16); R_v = RL(NT, "v")
        f.op("pool", lambda e: e.memset(vaug[:], 1.0), writes=R_v)
        R_oT = RL(NT, "oT")
        R_QT = RL(NT, "QT")
        S = Scope(nc)
        win = S.sb("win1", [128, 8, 1536], BF16); R_win = Res()
        f.dma("pool", win[:], odd_w_in.rearrange("(kc p) n -> p kc n", p=128), writes=[R_win])
        gq = S.sb("gq", [128, 2, 64]); R_gq = Res()
        f.dma("sp", gq[:, 0, :], q_norm.partition_broadcast(128), writes=[R_gq])
        f.dma("sp", gq[:, 1, :], k_norm.partition_broadcast(128), writes=[R_gq])
        xt = [S.sb("xt%d" % k, [128, D]) for k in range(2)]; R_xt = RL(2)
        h32 = S.sb("h32", [128, D]); R_h32 = Res()
        hT = [S.sb("hT%d" % k, [128, 8, 128], BF16) for k in range(2)]; R_hT = RL(2)
        ps_tp = S.ps("ps_tp", [128, 8, 128]); R_pstp = Res(x=True)
        ps_q = S.ps("ps_q", [128, 1536]); R_psq = Res(x=True)
        ps_t = S.ps("ps_t", [128, 16, 128], BF16); R_pst = Res(x=True)
        qk = S.sb("qk", [128, 20, 64]); R_qk = Res()
        sq = S.sb("sq", [128, 20, 64]); ss = S.sb("ss", [128, 20]); R_ss = Res()
        ra = S.sb("ra", [128, 20, 32]); rb = S.sb("rb", [128, 20, 32]); R_ra = Res(); R_rb = Res()
        tqk = S.sb("tqk", [128, 20, 64], BF16); R_tqk = Res()
        kd = S.sb("kd", [128, 4, 2, 64], BF16); R_kd = Res()
        qts = [S.sb("qts%d" % k, [128, 8, 128], BF16) for k in range(2)]; R_qts = RL(2)
        for t in range(NT):
            b = t % 2
            f.dma("sp", xt[b][:], XR[t * 128:(t + 1) * 128, :], reads=[R_XR[t]], writes=[R_xt[b]])
            which = 1 if t < 2 else 0
            mod_transpose(xt[b], R_xt[b], which, h32, R_h32, ps_tp, R_pstp, hT[b][:], R_hT[b])
            cols = slice(t * 128, (t + 1) * 128)
            lat = t >= 2
            ranges = ((0, 512), (512, 1024), (1024, 1536)) if lat else ((1024, 1536),)
            first = True
            for (n0, n1) in ranges:
                for kc in range(8):
                    f.op("pe", lambda e, kc=kc, n0=n0, n1=n1: e.matmul(ps_q[:, n0:n1], hT[b][:, kc, :], win[:, kc, n0:n1], start=(kc == 0), stop=(kc == 7)),
                         reads=[R_win, R_hT[b]], writes=[R_psq], acc=(not first))
                    first = False
            h0 = 0 if lat else 16
            nh = 20 - h0
            pv = ps_q[:, h0 * 64:1280].rearrange("p (h d) -> p h d", d=64)
            qkv_ = qk[:, h0:20, :]
            f.op("act", lambda e: e.activation(out=sq[:, h0:20, :], in_=pv, func=AF.Square), reads=[R_psq], writes=[R_ss])
            f.op("dve", lambda e: e.tensor_reduce(out=ss[:, h0:20], in_=sq[:, h0:20, :], axis=AX.X, op=ALU.add), reads=[R_ss], writes=[R_ss])
            f.op("dve", lambda e: e.tensor_scalar(out=ss[:, h0:20], in0=ss[:, h0:20], scalar1=1.0 / 64.0, scalar2=RMS_EPS, op0=ALU.mult, op1=ALU.add), reads=[R_ss], writes=[R_ss])
            f.op("act", lambda e: e.activation(out=ss[:, h0:20], in_=ss[:, h0:20], func=AF.Sqrt), reads=[R_ss], writes=[R_ss])
            f.op("dve", lambda e: e.reciprocal(ss[:, h0:20], ss[:, h0:20]), reads=[R_ss], writes=[R_ss])
            f.op("dve", lambda e: e.tensor_tensor(qkv_, pv, ss[:, h0:20].unsqueeze(2).broadcast_to([128, nh, 64]), ALU.mult), reads=[R_psq, R_ss], writes=[R_qk])
            if lat:
                f.op("pool", lambda e: e.tensor_tensor(qk[:, 0:16, :], qk[:, 0:16, :], gq[:, 0, :].unsqueeze(1).broadcast_to([128, 16, 64]), ALU.mult), reads=[R_qk, R_gq], writes=[R_qk])
            f.op("pool", lambda e: e.tensor_tensor(qk[:, 16:20, :], qk[:, 16:20, :], gq[:, 1, :].unsqueeze(1).broadcast_to([128, 4, 64]), ALU.mult), reads=[R_qk, R_gq], writes=[R_qk])
            if lat:
                q4 = qk[:].rearrange("p h (two f) -> p h two f", two=2)
                o4 = tqk[:].rearrange("p h (two f) -> p h two f", two=2)
                cosb = rope[:, 0, t - 2, :].unsqueeze(1).broadcast_to([128, 20, 32])
                sinb = rope[:, 1, t - 2, :].unsqueeze(1).broadcast_to([128, 20, 32])
                f.op("dve", lambda e: e.tensor_tensor(ra[:], q4[:, :, 0, :], cosb, ALU.mult), reads=[R_qk, R_rope], writes=[R_ra])
                f.op("pool", lambda e: e.tensor_tensor(rb[:], q4[:, :, 1, :], sinb, ALU.mult), reads=[R_qk, R_rope], writes=[R_rb])
                f.op("dve", lambda e: e.tensor_tensor(o4[:, :, 0, :], ra[:], rb[:], ALU.subtract), reads=[R_ra, R_rb], writes=[R_tqk])
                f.op("dve", lambda e: e.tensor_tensor(ra[:], q4[:, :, 1, :], cosb, ALU.mult), reads=[R_qk, R_rope, R_tqk], writes=[R_ra])
                f.op("pool", lambda e: e.tensor_tensor(rb[:], q4[:, :, 0, :], sinb, ALU.mult), reads=[R_qk, R_rope, R_tqk], writes=[R_rb])
                f.op("dve", lambda e: e.tensor_tensor(o4[:, :, 1, :], ra[:], rb[:], ALU.add), reads=[R_ra, R_rb], writes=[R_tqk])
            else:
                f.op("dve", lambda e: e.tensor_copy(tqk[:, 16:20, :], qk[:, 16:20, :]), reads=[R_qk], writes=[R_tqk])
            for a in range(4):
                f.op("dve", lambda e, a=a: e.tensor_copy(vaug[:, t, 64 + 128 * a:128 + 128 * a], ps_q[:, 1280 + 64 * a:1344 + 64 * a]), reads=[R_psq], writes=[R_v[t]])
            f.op("dve", lambda e: e.tensor_copy(kd[:, :, 0, :], tqk[:, 16:20, :]), reads=[R_tqk], writes=[R_kd])
            f.op("dve", lambda e: e.tensor_copy(kd[:, :, 1, :], tqk[:, 16:20, :]), reads=[R_tqk], writes=[R_kd])
            firstt = True
            if lat:
                for pr in range(8):
                    f.op("pe", lambda e, pr=pr: e.transpose(ps_t[:, pr, :], tqk[:, 2 * pr:2 * pr + 2, :].rearrange("p a d -> p (a d)"), identb[:]),
                         reads=[R_tqk, R_identb], writes=[R_pst], acc=(not firstt))
                    firstt = False
            for a in range(4):
                f.op("pe", lambda e, a=a: e.transpose(ps_t[:, 8 + a, :], kd[:, a, :, :].rearrange("p a d -> p (a d)"), identb[:]),
                     reads=[R_kd, R_identb], writes=[R_pst], acc=(not firstt))
                firstt = False
            f.op("act", lambda e: e.activation(out=kT2[:, :, cols], in_=ps_t[:, 8:12, :], func=AF.Identity), reads=[R_pst], writes=[R_kT[t]])
            if lat:
                f.op("dve", lambda e: e.tensor_copy(qts[b][:], ps_t[:, 0:8, :]), reads=[R_pst], writes=[R_qts[b]])
                f.dma("act", QT[:, :, (t - 2) * 128:(t - 1) * 128].rearrange("a p n -> p a n"), qts[b][:], reads=[R_qts[b]], writes=[R_QT[t]])
        S.close()
        S = Scope(nc)
        qb = [S.sb("qb%d" % k, [128, 2, 512], BF16) for k in range(2)]; R_qb = RL(2)
        ps_s = [S.ps("ps_s%d" % k, [128, 1024]) for k in range(2)]; R_pss = RL(2)
        ps_o = [S.ps("ps_o%d" % k, [128, 512]) for k in range(4)]; R_pso = RL(4)
        pT = [S.sb("pT%d" % k, [128, 1024], BF16) for k in range(3)]; R_pT = RL(3)
        dtmp = S.sb("dtmp", [128, 512]); R_dt = Res()
        ost = [S.sb("ost%d" % k, [128, 2, 512], BF16) for k in range(2)]; R_ost = RL(2)
        it = 0
        nq = 0
        for kvh in range(4):
            for qblk in range(8):
                qbi = nq % 2; nq += 1
                tq = [R_QT[2 + qblk * 4 + k] for k in range(4)]
                f.dma("sp", qb[qbi][:], QT[2 * kvh:2 * kvh + 2, :, qblk * 512:(qblk + 1) * 512].rearrange("a p n -> p a n"), reads=tq, writes=[R_qb[qbi]])
                items = [(kt, p_) for kt in range(NT) for p_ in range(2)]
                it0 = it; it += len(items)

                def front(idx):
                    kt, p_ = items[idx]
                    si = (it0 + idx) % 2
                    pi_ = (it0 + idx) % 3
                    for half in range(2):
                        base = 64 * half
                        f.op("pe", lambda e, half=half, base=base: e.matmul(ps_s[si][:, half * 512:(half + 1) * 512], kT2[base:base + 64, kvh, kt * 128:(kt + 1) * 128], qb[qbi][base:base + 64, p_, :], start=True, stop=True),
                             reads=[R_kT[kt], R_qb[qbi]], writes=[R_pss[si]], acc=(half > 0))
                    f.op("act", lambda e: e.activation(out=pT[pi_][:], in_=ps_s[si][:], func=AF.Exp, scale=0.125), reads=[R_pss[si]], writes=[R_pT[pi_]])

                def back(idx):
                    kt, p_ = items[idx]
                    pi_ = (it0 + idx) % 3
                    for half in range(2):
                        hh = 2 * p_ + half
                        voff = (64 if half == 0 else 0) + 128 * kvh
                        f.op("pe", lambda e, half=half, hh=hh, voff=voff: e.matmul(ps_o[hh][:], vaug[:, kt, voff:voff + 128], pT[pi_][:, half * 512:(half + 1) * 512], start=(kt == 0), stop=(kt == NT - 1)),
                             reads=[R_v[kt], R_pT[pi_]], writes=[R_pso[hh]], acc=(kt > 0))
                LA = 1
                for i_ in range(len(items) + LA):
                    if i_ < len(items):
                        front(i_)
                    if i_ >= LA:
                        back(i_ - LA)
                for hh in range(4):
                    h = kvh * 4 + hh
                    nb, db = (0, 64) if h % 2 == 0 else (64, 0)
                    f.op("dve", lambda e: e.reciprocal(dtmp[nb:nb + 64, :], ps_o[hh][db:db + 64, :]), reads=[R_pso[hh]], writes=[R_dt])
                    f.op("dve", lambda e: e.tensor_tensor(ost[qbi][nb:nb + 64, hh // 2, :], ps_o[hh][nb:nb + 64, :], dtmp[nb:nb + 64, :], ALU.mult),
                         reads=[R_pso[hh], R_dt], writes=[R_ost[qbi]])
                f.dma("act", OT[2 * kvh:2 * kvh + 2, :, qblk * 512:(qblk + 1) * 512].rearrange("a p n -> p a n"), ost[qbi][:], reads=[R_ost[qbi]],
                      writes=[R_oT[2 + qblk * 4 + k] for k in range(4)])
        S.close()
        L.close()
        M = Scope(nc)
        mixt = [M.sb("mixt%d" % k, [128, 8, 128], BF16) for k in range(2)]; R_mixt = RL(2)

        def mix_loader(t, b):
            f.dma("sp", mixt[b][:], OT[:, :, (t - 2) * 128:(t - 1) * 128].rearrange("a p n -> p a n"), reads=[R_oT[t]], writes=[R_mixt[b]])
            return [mixt[b][:, k, :] for k in range(8)], [R_mixt[b]]
        out_phase(1, odd_w_out, mix_loader, range(2, NT))
        M.close()

    phase_mod(0, 0)
    if stop_after == "mod":
        f.dma("sp", dbg[0:128, :], mod[:, 0].rearrange("p a d -> p (a d)"), reads=[R_mod], writes=[R_dbg])
        f.dma("sp", dbg[128:256, :], mod[:, 1].rearrange("p a d -> p (a d)"), reads=[R_mod], writes=[R_dbg])
    else:
        layer0_mixer()
    if stop_after in ("in0", "s5", "mod", "h0", "qkv", "win"):
        pass
    else:
        if stop_after == "mix0":
            pass
        else:
            phase_mod(0, 1)
            (ffn_phase if 'dense' in DBG_SKIP else ffn_sparse)(0, list(range(NT)), final=False)
            if stop_after != "l0":
                phase_mod(1, 0)
                layer1_mixer()
                if stop_after != "mix1":
                    phase_mod(1, 1)
                    (ffn_phase if 'dense' in DBG_SKIP else ffn_sparse)(1, list(range(2, NT)), final=True)
    if stop_after is not None and stop_after not in ("in0", "s5", "mod", "h0", "qkv", "win"):
        S = Scope(nc)
        tt = S.sb("dumpt", [128, D]); R_t = Res()
        for t in range(NT):
            f.dma("sp", tt[:], XR[t * 128:(t + 1) * 128, :], reads=[R_XR[t]], writes=[R_t])
            f.dma("sp", dbg[t * 128:(t + 1) * 128, :], tt[:], reads=[R_t], writes=[R_dbg])
        S.close()
    f.finish()
    Scope.FWREF = None
    G.close()
    f.close()
    return nc


_CONST = None


def _consts():
    global _CONST
    if _CONST is None:
        ident = np.eye(128, dtype=np.float32)
        n_freq = 16
        inv_freq = (10000.0 ** (-np.arange(n_freq, dtype=np.float32) / n_freq)).astype(np.float32)
        pos = np.arange(4096)
        r = (pos // 64).astype(np.float32); cc = (pos % 64).astype(np.float32)
        ang = np.concatenate([r[:, None] * inv_freq, cc[:, None] * inv_freq], -1).astype(np.float32)
        cos = np.cos(ang).astype(np.float32).reshape(32, 128, 32).transpose(1, 0, 2)
        sin = np.sin(ang).astype(np.float32).reshape(32, 128, 32).transpose(1, 0, 2)
        rope = np.ascontiguousarray(np.stack([cos, sin], axis=1))
        k = np.arange(128)[:, None]; q = np.arange(128)[None, :]
        mask = np.stack([(q <= k), (k <= q), (k < q)], axis=1).astype(np.float32)
        pc = (np.arange(128, dtype=np.float32)[:, None] + 128.0 * np.arange(4, dtype=np.float32)[None, :]).astype(np.float32)
        jidx = np.broadcast_to(np.arange(128, dtype=np.float32)[None, :], (128, 128)).copy()
        _CONST = {"k_ident": ident, "k_rope": rope, "k_mask": np.ascontiguousarray(mask), "k_jidx": jidx, "k_pc": np.ascontiguousarray(pc)}
    return _CONST


def make_in_map(inputs, b):
    f32 = lambda a: np.ascontiguousarray(np.asarray(a, dtype=np.float32))
    m = {
        "x": f32(inputs["x"][b]), "ctx": f32(inputs["ctx"][b]), "c": f32(inputs["c"][b:b + 1]),
        "c_ctx": f32(inputs["c_ctx"]).reshape(1, D),
        "ada_w": f32(inputs["ada_w"]), "ada_b": f32(inputs["ada_b"]), "ln_g": f32(inputs["ln_g"]), "ln_b": f32(inputs["ln_b"]),
        "even_w_in": f32(inputs["even_w_in"][0]), "even_w_out": f32(inputs["even_w_out"][0]),
        "s5_lam_re": f32(inputs["s5_lam_re"][0]), "s5_lam_im": f32(inputs["s5_lam_im"][0]), "s5_log_step": f32(inputs["s5_log_step"][0]),
        "s5_b_re": f32(inputs["s5_b_re"][0]), "s5_b_im": f32(inputs["s5_b_im"][0]),
        "s5_c_re": f32(inputs["s5_c_re"][0]), "s5_c_im": f32(inputs["s5_c_im"][0]),
        "s5_d": f32(inputs["s5_d"][0]), "s5_w_glu": f32(inputs["s5_w_glu"][0]), "s5_b_glu": f32(inputs["s5_b_glu"][0]),
        "win_sink": f32(inputs["win_sink"][0]),
        "odd_w_in": f32(inputs["odd_w_in"][0]), "odd_w_out": f32(inputs["odd_w_out"][0]),
        "odd_q_norm": f32(inputs["odd_q_norm"][0]), "odd_k_norm": f32(inputs["odd_k_norm"][0]),
        "router_w": f32(inputs["router_w"]), "router_bias": f32(inputs["router_bias"]),
        "moe_w_gate": f32(inputs["moe_w_gate"]), "moe_w_up": f32(inputs["moe_w_up"]), "moe_w_down": f32(inputs["moe_w_down"]),
    }
    m.update(_consts())
    return m


def kernel(**inputs):
    nc = build_program()
    shared = make_in_map(inputs, 0)
    in_maps = []
    for b in range(8):
        m = dict(shared)
        m["x"] = np.ascontiguousarray(np.asarray(inputs["x"][b], dtype=np.float32))
        m["ctx"] = np.ascontiguousarray(np.asarray(inputs["ctx"][b], dtype=np.float32))
        m["c"] = np.ascontiguousarray(np.asarray(inputs["c"][b:b + 1], dtype=np.float32))
        in_maps.append(m)
    res = run_bass_kernel_spmd(nc, in_maps, core_ids=list(range(8)))
    return np.stack([np.asarray(r["out"], dtype=np.float32) for r in res.results], axis=0)
```

```python
import math
import os
DBG_SKIP = os.environ.get('DBG_SKIP', '').split(',')
DBG_NT = int(os.environ.get('DBG_NT', '34'))
from contextlib import ExitStack
import numpy as np
import ml_dtypes
import concourse.bass as bass
import concourse.mybir as mybir
from concourse.bass_utils import run_bass_kernel_spmd

F32 = mybir.dt.float32
BF16 = mybir.dt.bfloat16
I32 = mybir.dt.int32
ALU = mybir.AluOpType
AF = mybir.ActivationFunctionType
AX = mybir.AxisListType

SEM_LIMIT = 30000
NT = 34
NTOK = 4352
D = 1024
ALPHA = 4.0 ** 0.25
LN_EPS = 1e-5
RMS_EPS = 1e-6
TWO_PI = 2.0 * math.pi
CW1 = 6.28125
CW2 = TWO_PI - CW1


class Res:
    __slots__ = ("name", "w", "r", "x")

    def __init__(self, name="", x=False):
        self.name = name
        self.w = None
        self.r = []
        self.x = x


def RL(n, name="r"):
    return [Res("%s%d" % (name, i)) for i in range(n)]


class EngState:
    def __init__(self, fw, name, eng):
        self.fw = fw
        self.name = name
        self.eng = eng
        self.count = 0
        self.epoch = 0
        self.known = {}
        self._new_sem()

    def _new_sem(self):
        self.sem_key = "%s_e%d" % (self.name, self.epoch)
        self.sem = self.fw.new_sem(self.sem_key)
        self.count = 0
        self.epoch += 1


class FW:
    def __init__(self, nc, n_dma_sems=10):
        self.nc = nc
        self.es = ExitStack()
        self.sems = {}
        self.engs = {}
        for name, eng in (("pe", nc.tensor), ("act", nc.scalar), ("dve", nc.vector),
                          ("pool", nc.gpsimd), ("sp", nc.sync)):
            self.engs[name] = EngState(self, name, eng)
        self.dma_pool = {}
        for q in ("sp", "act", "pool"):
            lst = []
            for i in range(n_dma_sems):
                key = "dma_%s_%d" % (q, i)
                lst.append([key, self.new_sem(key), 0])
            self.dma_pool[q] = [lst, 0]
        self.n_instr = 0
        self.n_waits = 0

    def new_sem(self, key):
        s = self.es.enter_context(self.nc.semaphore(key))
        self.sems[key] = s
        return s

    def _wait(self, E, ev):
        if ev is None:
            return
        key, val = ev
        if E.known.get(key, 0) >= val:
            return
        E.eng.wait_ge(self.sems[key], val)
        E.known[key] = val
        self.n_waits += 1

    def _deps(self, E, reads, writes, acc=False):
        for r in reads:
            self._wait(E, r.w)
            if r.x:
                for ev in r.r:
                    if ev[0] != E.sem_key:
                        self._wait(E, ev)
        for w in writes:
            if not ((acc or E.name == "pe") and w.w is not None and w.w[0] == E.sem_key):
                self._wait(E, w.w)
            for ev in w.r:
                self._wait(E, ev)

    def _commit(self, ev, reads, writes):
        for r in reads:
            r.r.append(ev)
            if len(r.r) > 16:
                d = {}
                for k, v in r.r:
                    if d.get(k, 0) < v:
                        d[k] = v
                r.r = list(d.items())
        for w in writes:
            w.w = ev
            w.r = []

    def op(self, ename, fn, reads=(), writes=(), acc=False):
        E = self.engs[ename]
        if E.count >= SEM_LIMIT:
            E._new_sem()
        self._deps(E, reads, writes, acc=acc)
        ins = fn(E.eng)
        E.count += 1
        ins.then_inc(E.sem, 1)
        self._commit((E.sem_key, E.count), reads, writes)
        self.n_instr += 1
        return ins

    def _dma_common(self, qname, issue, reads, writes):
        E = self.engs[qname]
        pool, idx = self.dma_pool[qname]
        ent = pool[idx % len(pool)]
        self.dma_pool[qname][1] = idx + 1
        key, sem, val = ent
        if val > 0:
            self._wait(E, (key, val))
        if val + 16 > SEM_LIMIT:
            key = key + "n"
            sem = self.new_sem(key)
            val = 0
            ent[0], ent[1] = key, sem
        self._deps(E, reads, writes)
        ins = issue(E.eng)
        val += 16
        ent[2] = val
        ins.then_inc(sem, 16)
        ev = (key, val)
        self._commit(ev, reads, writes)
        self.n_instr += 1
        return ev

    def dma(self, qname, out, in_, reads=(), writes=(), **kw):
        return self._dma_common(qname, lambda e: e.dma_start(out=out, in_=in_, **kw), reads, writes)

    def barrier(self):
        evs = []
        for q in self.dma_pool:
            for key, sem, val in self.dma_pool[q][0]:
                if val > 0:
                    evs.append((key, val))
        for n, e in self.engs.items():
            if e.count > 0:
                evs.append((e.sem_key, e.count))
        for n, E in self.engs.items():
            for ev in evs:
                if ev[0] != E.sem_key:
                    self._wait(E, ev)

    def finish(self):
        E = self.engs["sp"]
        for q in self.dma_pool:
            for key, sem, val in self.dma_pool[q][0]:
                if val > 0:
                    self._wait(E, (key, val))
        for n, e in self.engs.items():
            if e.count > 0:
                self._wait(E, (e.sem_key, e.count))

    def close(self):
        self.es.close()


class Scope:
    FWREF = None

    def __init__(self, nc):
        self.nc = nc
        self.es = ExitStack()

    CNT = [0]

    def sb(self, name, shape, dtype=F32):
        Scope.CNT[0] += 1
        return self.es.enter_context(self.nc.sbuf_tensor("%s_%d" % (name, Scope.CNT[0]), list(shape), dtype))

    def ps(self, name, shape, dtype=F32):
        Scope.CNT[0] += 1
        return self.es.enter_context(self.nc.psum_tensor("%s_%d" % (name, Scope.CNT[0]), list(shape), dtype))

    def close(self):
        if Scope.FWREF is not None:
            Scope.FWREF.barrier()
        self.es.close()


def rev_ap(ap2d, n):
    last = ap2d[:, n - 1:n]
    return bass.AP(tensor=ap2d.tensor, offset=last.offset, ap=[list(ap2d.ap[0]), [-1, n]])


def build_program(stop_after=None, dbg_shape=None):
    nc = bass.Bass("TRN2", target_bir_lowering=False)

    def din(name, shape, dt=F32):
        return nc.dram_tensor(name, list(shape), dt, kind="ExternalInput").ap()

    x_d = din("x", [4096, D]); ctx_d = din("ctx", [256, D])
    c_d = din("c", [1, D]); cctx_d = din("c_ctx", [1, D])
    ada_w = din("ada_w", [2, D, 6 * D]); ada_b = din("ada_b", [2, 6 * D])
    ln_g = din("ln_g", [2, 2, D]); ln_b = din("ln_b", [2, 2, D])
    even_w_in = din("even_w_in", [D, 1280]); even_w_out = din("even_w_out", [D, D])
    lam_re = din("s5_lam_re", [2, 32, 64]); lam_im = din("s5_lam_im", [2, 32, 64])
    log_step = din("s5_log_step", [2, 32])
    b_re = din("s5_b_re", [2, 32, 64, 16]); b_im = din("s5_b_im", [2, 32, 64, 16])
    c_re = din("s5_c_re", [2, 32, 16, 64]); c_im = din("s5_c_im", [2, 32, 16, 64])
    s5_d = din("s5_d", [512]); w_glu = din("s5_w_glu", [512, 512]); b_glu = din("s5_b_glu", [512])
    win_sink = din("win_sink", [8])
    odd_w_in = din("odd_w_in", [D, 1536]); odd_w_out = din("odd_w_out", [D, D])
    q_norm = din("odd_q_norm", [64]); k_norm = din("odd_k_norm", [64])
    router_w = din("router_w", [D, 32]); router_b = din("router_bias", [32])
    w_gate = din("moe_w_gate", [2, 32, D, 512]); w_up = din("moe_w_up", [2, 32, D, 512])
    w_down = din("moe_w_down", [2, 32, 512, D])
    k_ident = din("k_ident", [128, 128]); k_rope = din("k_rope", [128, 2, 32, 32])
    k_mask = din("k_mask", [128, 3, 128]); k_jidx = din("k_jidx", [128, 128]); k_pc = din("k_pc", [128, 4])
    out_d = nc.dram_tensor("out", [4096, D], F32, kind="ExternalOutput").ap()
    XR = nc.dram_tensor("xr", [NTOK, D], F32, kind="Internal").ap()
    QT = nc.dram_tensor("qt_scr", [8, 128, 4096], BF16, kind="Internal").ap()
    OT = nc.dram_tensor("ot_scr", [8, 128, 4096], BF16, kind="Internal").ap()
    ATD = nc.dram_tensor("at_scr", [4, 128, NTOK], BF16, kind="Internal").ap()
    NS = 49
    XS = nc.dram_tensor("xs_scr", [NS * 512, D], BF16, kind="Internal").ap()
    YS = nc.dram_tensor("ys_scr", [NS * 512, D], F32, kind="Internal").ap()
    wg_all = w_gate.rearrange("l e (kk two) n -> (l e kk) (two n)", two=2)
    wu_all = w_up.rearrange("l e (kk two) n -> (l e kk) (two n)", two=2)
    wd_all = w_down.rearrange("l e f n -> (l e f) n")
    wg_rows = [wg_all, wg_all]; wu_rows = [wu_all, wu_all]; wd_rows = [wd_all, wd_all]
    dbg = None
    if dbg_shape is not None:
        dbg = nc.dram_tensor("dbg", list(dbg_shape), F32, kind="ExternalOutput").ap()

    f = FW(nc)
    Scope.FWREF = f
    G = Scope(nc)
    R_XR = RL(NT, "xr")
    R_out = Res("out")
    R_dbg = Res("dbg")

    ident = G.sb("ident", [128, 128]); R_ident = Res()
    identb = G.sb("identb", [128, 128], BF16); R_identb = Res()
    f.dma("sp", ident[:], k_ident, writes=[R_ident])
    f.op("dve", lambda e: e.tensor_copy(identb[:], ident[:]), reads=[R_ident], writes=[R_identb])
    rope = G.sb("rope", [128, 2, 32, 32]); R_rope = Res()
    f.dma("sp", rope[:], k_rope, writes=[R_rope])
    maskf = G.sb("maskf", [128, 3, 128]); maskb = G.sb("maskb", [128, 3, 128], BF16); R_mask = Res()
    f.dma("sp", maskf[:], k_mask, writes=[R_mask])
    f.op("dve", lambda e: e.tensor_copy(maskb[:], maskf[:]), reads=[R_mask], writes=[R_mask])
    R_crep = Res()
    ctmp = G.sb("ctmp", [128, 2, 8]); R_ctmp = Res()
    f.dma("sp", ctmp[:, 0, :], c_d.rearrange("o (kc p) -> p (o kc)", p=128), writes=[R_ctmp], allow_slow_non_contiguous=True)
    f.dma("sp", ctmp[:, 1, :], cctx_d.rearrange("o (kc p) -> p (o kc)", p=128), writes=[R_ctmp], allow_slow_non_contiguous=True)
    f.op("act", lambda e: e.activation(out=ctmp[:], in_=ctmp[:], func=AF.Silu), reads=[R_ctmp], writes=[R_ctmp])
    lng = G.sb("lng", [128, D]); lnb = G.sb("lnb", [128, D]); R_ln = Res()

    def load_ln(li):
        f.dma("sp", lng[:], ln_g[li // 2, li % 2].partition_broadcast(128), writes=[R_ln])
        f.dma("sp", lnb[:], ln_b[li // 2, li % 2].partition_broadcast(128), writes=[R_ln])
    epsc = G.sb("epsc", [128, 1]); R_eps = Res()
    f.op("dve", lambda e: e.memset(epsc[:], LN_EPS), writes=[R_eps])

    R_XsZ = RL(28, "xsz")
    ZS = Scope(nc)
    zt = ZS.sb("zt", [128, 7, D], BF16); R_zt = Res()
    f.op("pool", lambda e: e.memset(zt[:], 0.0), writes=[R_zt])
    for k in range(28):
        f.dma(("sp", "act")[k % 2], XS[k * 896:(k + 1) * 896, :].rearrange("(a p) d -> p a d", p=128), zt[:], reads=[R_zt], writes=[R_XsZ[k]])
    ZS.close()

    mod = G.sb("mod", [128, 2, 3, D]); R_mod = Res("mod")

    def dump(ap_sb, rows, cols, reads, r0=0, c0=0):
        f.dma("sp", dbg[r0:r0 + rows, c0:c0 + cols], ap_sb, reads=reads, writes=[R_dbg])

    def phase_mod(i, s):
        S = Scope(nc)
        crep = S.sb("crep", [128, 2, 8, 128])
        f.op("dve", lambda e: e.tensor_copy(crep[:], ctmp[:].unsqueeze(3).broadcast_to([128, 2, 8, 128])), reads=[R_ctmp], writes=[R_crep])
        slab = [S.sb("slab%d" % k, [128, 8, 512]) for k in range(2)]; R_slab = RL(2)
        adb = [S.sb("adb%d" % k, [128, 512]) for k in range(2)]; R_adb = RL(2)
        psm = [S.ps("psm%d" % k, [128, 512]) for k in range(2)]; R_psm = RL(2)
        n = 0
        for blk in range(6):
            c0 = s * 3072 + blk * 512
            bi = blk % 2
            f.dma("sp", slab[bi][:], ada_w[i, :, c0:c0 + 512].rearrange("(kc p) n -> p kc n", p=128), writes=[R_slab[bi]])
            f.dma("act", adb[bi][:], ada_b[i, c0:c0 + 512].partition_broadcast(128), writes=[R_adb[bi]])
            k, half = blk // 2, blk % 2
            for which in range(2):
                pi = n % 2; n += 1
                for kc in range(8):
                    f.op("pe", lambda e, kc=kc: e.matmul(psm[pi][:], crep[:, which, kc, :], slab[bi][:, kc, :], start=(kc == 0), stop=(kc == 7)),
                         reads=[R_crep, R_slab[bi]], writes=[R_psm[pi]], acc=(kc > 0))
                dst = mod[:, which, k, half * 512:(half + 1) * 512]
                f.op("dve", lambda e: e.scalar_tensor_tensor(out=dst, in0=psm[pi][:], scalar=(1.0 if k == 1 else 0.0), in1=adb[bi][:], op0=ALU.add, op1=ALU.add),
                     reads=[R_psm[pi], R_adb[bi]], writes=[R_mod])
        S.close()

    def resid_ln(S, xt, R_xt, o_ps, R_ops, which, li, out_t, R_outt, tmp, R_tmp, small, R_small):
        gate = mod[:, which, 2, :]
        f.op("dve", lambda e: e.tensor_tensor(tmp[:], o_ps[:], gate, ALU.mult), reads=[R_ops, R_mod], writes=[R_tmp])
        f.op("dve", lambda e: e.scalar_tensor_tensor(out=tmp[:], in0=xt[:], scalar=ALPHA, in1=tmp[:], op0=ALU.mult, op1=ALU.add),
             reads=[R_xt, R_tmp], writes=[R_tmp])
        f.op("dve", lambda e: e.bn_stats(small[:, 0:6], tmp[:, 0:512]), reads=[R_tmp], writes=[R_small])
        f.op("dve", lambda e: e.bn_stats(small[:, 6:12], tmp[:, 512:1024]), reads=[R_tmp], writes=[R_small])
        f.op("dve", lambda e: e.bn_aggr(small[:, 12:14], small[:, 0:12]), reads=[R_small], writes=[R_small])
        f.op("act", lambda e: e.activation(out=small[:, 14:15], in_=small[:, 13:14], func=AF.Sqrt, bias=epsc[:], scale=1.0), reads=[R_small, R_eps], writes=[R_small])
        f.op("dve", lambda e: e.reciprocal(small[:, 15:16], small[:, 14:15]), reads=[R_small], writes=[R_small])
        f.op("dve", lambda e: e.tensor_scalar(out=tmp[:], in0=tmp[:], scalar1=small[:, 12:13], scalar2=small[:, 15:16], op0=ALU.subtract, op1=ALU.mult),
             reads=[R_tmp, R_small], writes=[R_tmp])
        f.op("dve", lambda e: e.tensor_tensor(tmp[:], tmp[:], lng[:], ALU.mult), reads=[R_tmp, R_ln], writes=[R_tmp])
        f.op("dve", lambda e: e.tensor_tensor(out_t[:], tmp[:], lnb[:], ALU.add), reads=[R_tmp, R_ln], writes=[R_outt])

    def mod_transpose(xt, R_xt, which, h32, R_h32, ps_tp, R_pstp, hT_dst, R_hT, h32T=None, R_h32T=None):
        f.op("dve", lambda e: e.tensor_tensor(h32[:], xt[:], mod[:, which, 1, :], ALU.mult), reads=[R_xt, R_mod], writes=[R_h32])
        f.op("dve", lambda e: e.tensor_tensor(h32[:], h32[:], mod[:, which, 0, :], ALU.add), reads=[R_h32, R_mod], writes=[R_h32])
        for kc in range(8):
            f.op("pe", lambda e, kc=kc: e.transpose(ps_tp[:, kc, :], h32[:, kc * 128:(kc + 1) * 128], ident[:]),
                 reads=[R_h32, R_ident], writes=[R_pstp], acc=(kc > 0))
        f.op("act", lambda e: e.activation(out=hT_dst, in_=ps_tp[:], func=AF.Identity), reads=[R_pstp], writes=[R_hT])
        if h32T is not None:
            f.op("dve", lambda e: e.tensor_copy(h32T[:], ps_tp[:]), reads=[R_pstp], writes=[R_h32T])

    def src_tile(layer, t):
        if layer == 0:
            return (ctx_d[t * 128:(t + 1) * 128, :] if t < 2 else x_d[(t - 2) * 128:(t - 1) * 128, :]), []
        return XR[t * 128:(t + 1) * 128, :], [R_XR[t]]

    def layer0_mixer():
        L = Scope(nc)
        U = Scope(nc)
        uT = U.sb("uT", [128, 4, NTOK], BF16); R_uT = RL(NT, "uT")
        aT, R_aT = uT, R_uT

        def inproj(do_u, qT=None, R_qT=None, kT2=None, R_kT=None, vaug=None, R_v=None):
            S = Scope(nc)
            wc0, wc1 = (0, 512) if do_u else (512, 1280)
            win = S.sb("win", [128, 8, wc1 - wc0], BF16); R_win = Res()
            f.dma("pool", win[:], even_w_in[:, wc0:wc1].rearrange("(kc p) n -> p kc n", p=128), writes=[R_win])
            xt1 = S.sb("xt1", [128, D]); xt = [xt1, xt1]; R1_ = Res(); R_xt = [R1_, R1_]
            h32 = S.sb("h32", [128, D]); R_h32 = Res()
            hT = [S.sb("hT%d" % k, [128, 8, 128], BF16) for k in range(2)]; R_hT = RL(2)
            ps_tp = S.ps("ps_tp", [128, 8, 128]); R_pstp = Res(x=True)
            ps_u = S.ps("ps_u", [128, 4, 128]); R_psu = Res()
            ps_q = S.ps("ps_q", [128, 1024]); R_psq = Res(x=True)
            ps_t = S.ps("ps_t", [128, 8, 128], BF16); R_pst = Res(x=True)
            ra = S.sb("ra", [128, 10, 32]); rb = S.sb("rb", [128, 10, 32]); R_ra = Res(); R_rb = Res()
            tqk = S.sb("tqk", [128, 640], BF16); R_tqk = Res()
            kd = S.sb("kd", [128, 2, 2, 64], BF16); R_kd = Res()
            for t in range(NT if do_u else min(NT, DBG_NT)):
                b = t % 2
                src, rs = src_tile(0, t)
                f.dma("sp", xt[b][:], src, reads=rs, writes=[R_xt[b]])
                which = 1 if t < 2 else 0
                mod_transpose(xt[b], R_xt[b], which, h32, R_h32, ps_tp, R_pstp, hT[b][:], R_hT[b])
                cols = slice(t * 128, (t + 1) * 128)
                if stop_after == "h0" and t == 0:
                    f.dma("sp", dbg[0:128, :], h32[:], reads=[R_h32], writes=[R_dbg])
                    hf = S.sb("hf", [128, 1024]); R_hf = Res()
                    f.op("dve", lambda e: e.tensor_copy(hf[:], hT[b][:].rearrange("p a b -> p (a b)")), reads=[R_hT[b]], writes=[R_hf])
                    f.dma("sp", dbg[128:256, :], hf[:], reads=[R_hf], writes=[R_dbg])
                    f.op("dve", lambda e: e.tensor_copy(hf[:], win[:, 0, 0:1024]), reads=[R_win], writes=[R_hf])
                    f.dma("sp", dbg[256:384, :], hf[:], reads=[R_hf], writes=[R_dbg])
                    S.close(); return
                if do_u:
                    for ct in range(4):
                        for kc in range(8):
                            f.op("pe", lambda e, ct=ct, kc=kc: e.matmul(ps_u[:, ct, :], win[:, kc, ct * 128:(ct + 1) * 128], hT[b][:, kc, :], start=(kc == 0), stop=(kc == 7)),
                                 reads=[R_win, R_hT[b]], writes=[R_psu], acc=(ct + kc > 0))
                    f.op("act", lambda e: e.activation(out=uT[:, :, cols], in_=ps_u[:], func=AF.Identity), reads=[R_psu], writes=[R_uT[t]])
                    continue
                for (n0, n1) in ((0, 512), (512, 768)):
                    for kc in range(8):
                        f.op("pe", lambda e, kc=kc, n0=n0, n1=n1: e.matmul(ps_q[:, n0:n1], hT[b][:, kc, :], win[:, kc, n0:n1], start=(kc == 0), stop=(kc == 7)),
                             reads=[R_win, R_hT[b]], writes=[R_psq], acc=(n0 + kc > 0))
                if 'rope' in DBG_SKIP:
                    continue
                if t >= 2:
                    pv = ps_q[:, 0:640].rearrange("p (h two f) -> p h two f", two=2, f=32)
                    ov = tqk[:].rearrange("p (h two f) -> p h two f", two=2, f=32)
                    cosb = rope[:, 0, t - 2, :].unsqueeze(1).broadcast_to([128, 10, 32])
                    sinb = rope[:, 1, t - 2, :].unsqueeze(1).broadcast_to([128, 10, 32])
                    f.op("dve", lambda e: e.tensor_tensor(ra[:], pv[:, :, 0, :], cosb, ALU.mult), reads=[R_psq, R_rope], writes=[R_ra])
                    f.op("dve", lambda e: e.tensor_tensor(rb[:], pv[:, :, 1, :], sinb, ALU.mult), reads=[R_psq, R_rope], writes=[R_rb])
                    f.op("pool", lambda e: e.tensor_tensor(ov[:, :, 0, :], ra[:], rb[:], ALU.subtract), reads=[R_ra, R_rb], writes=[R_tqk])
                    f.op("dve", lambda e: e.tensor_tensor(ra[:], pv[:, :, 1, :], cosb, ALU.mult), reads=[R_psq, R_rope, R_tqk], writes=[R_ra])
                    f.op("dve", lambda e: e.tensor_tensor(rb[:], pv[:, :, 0, :], sinb, ALU.mult), reads=[R_psq, R_rope, R_tqk], writes=[R_rb])
                    f.op("pool", lambda e: e.tensor_tensor(ov[:, :, 1, :], ra[:], rb[:], ALU.add), reads=[R_ra, R_rb], writes=[R_tqk])
                else:
                    f.op("act", lambda e: e.activation(out=tqk[:], in_=ps_q[:, 0:640], func=AF.Identity), reads=[R_psq], writes=[R_tqk])
                if 'vaug' in DBG_SKIP:
                    continue
                for a in range(2):
                    if 'novaug' in DBG_SKIP:
                        break
                    f.op("dve", lambda e, a=a: e.tensor_copy(vaug[:, t, 64 + 128 * a:128 + 128 * a], ps_q[:, 640 + 64 * a:704 + 64 * a]),
                         reads=[R_psq], writes=[R_v[t]])
                if 'nokd' in DBG_SKIP:
                    continue
                kv = tqk[:, 512:640].rearrange("p (a d) -> p a d", a=2)
                f.op("dve", lambda e: e.tensor_copy(kd[:, :, 0, :], kv), reads=[R_tqk], writes=[R_kd])
                f.op("dve", lambda e: e.tensor_copy(kd[:, :, 1, :], kv), reads=[R_tqk], writes=[R_kd])
                if 'tr' in DBG_SKIP:
                    continue
                for pr in range(4):
                    f.op("pe", lambda e, pr=pr: e.transpose(ps_t[:, pr, :], tqk[:, pr * 128:(pr + 1) * 128], identb[:]),
                         reads=[R_tqk, R_identb], writes=[R_pst], acc=(pr > 0))
                for a in range(2):
                    f.op("pe", lambda e, a=a: e.transpose(ps_t[:, 4 + a, :], kd[:, a, :, :].rearrange("p a d -> p (a d)"), identb[:]),
                         reads=[R_kd, R_identb], writes=[R_pst], acc=True)
                f.op("dve", lambda e: e.tensor_copy(qT[:, :, cols], ps_t[:, 0:4, :]), reads=[R_pst], writes=[R_qT[t]])
                f.op("act", lambda e: e.activation(out=kT2[:, :, cols], in_=ps_t[:, 4:6, :], func=AF.Identity), reads=[R_pst], writes=[R_kT[t]])
            S.close()

        inproj(True)
        if stop_after == "h0":
            U.close(); L.close(); return
        if stop_after == "in0":
            S = Scope(nc)
            t32 = S.sb("t32", [128, 512]); R_t = Res()
            for ct in range(4):
                for blk in range(2):
                    f.op("dve", lambda e: e.tensor_copy(t32[:], uT[:, ct, blk * 512:(blk + 1) * 512]), reads=R_uT, writes=[R_t])
                    dump(t32[:], 128, 512, [R_t], r0=ct * 128, c0=blk * 512)
            S.close(); U.close(); L.close()
            return
        if 's5' not in DBG_SKIP:
            s5_phase(L, uT, R_uT, aT, R_aT)
        if stop_after == "s5":
            S = Scope(nc)
            t32 = S.sb("t32", [128, 512]); R_t = Res()
            for ct in range(4):
                for blk in range(9):
                    c0 = blk * 512; n = min(512, NTOK - c0)
                    f.op("dve", lambda e: e.tensor_copy(t32[:, 0:n], aT[:, ct, c0:c0 + n]), reads=R_aT, writes=[R_t])
                    dump(t32[:, 0:n], 128, n, [R_t], r0=ct * 128, c0=c0)
            S.close(); U.close(); L.close()
            return
        R_ATD = Res("atd")
        for k in range(4):
            f.dma(("sp", "act")[k % 2], ATD[k], aT[:, k, :], reads=R_aT, writes=[R_ATD])
        U.close()
        oT = L.sb("oT", [128, 4, NTOK], BF16); R_oT = RL(NT, "oT")
        W = Scope(nc)
        qT = W.sb("qT", [128, 4, NTOK], BF16); R_qT = RL(NT, "qT")
        kT2 = W.sb("kT2", [128, 2, NTOK], BF16); R_kT = RL(NT, "kT")
        vaug = W.sb("vaug", [128, NT, 320], BF16); R_v = RL(NT, "v")
        f.op("pool", lambda e: e.memset(vaug[:], 1.0), writes=R_v)
        inproj(False, qT, R_qT, kT2, R_kT, vaug, R_v)
        if stop_after == "qkv":
            W.close(); L.close(); return
        win_phase(qT, R_qT, kT2, R_kT, vaug, R_v, oT, R_oT)
        W.close()
        if stop_after == "win":
            L.close(); return

        M = Scope(nc)
        mixt = [M.sb("mixa%d" % k, [128, 4, 128], BF16) for k in range(2)]; R_mixt = RL(2)

        def mix_loader(t, b):
            c0 = t * 128
            f.dma("sp", mixt[b][:], ATD[:, :, c0:c0 + 128].rearrange("a p n -> p a n"), reads=[R_ATD], writes=[R_mixt[b]])
            return [mixt[b][:, k, :] for k in range(4)] + [oT[:, k, c0:c0 + 128] for k in range(4)], [R_mixt[b], R_oT[t]]
        out_phase(0, even_w_out, mix_loader, range(NT))
        M.close()
        L.close()

    def sincos(S, ang, n, out_s, out_c, R, tag):
        ki = S.sb("ki_" + tag, [128, n], I32); kf = S.sb("kf_" + tag, [128, n]); rd = S.sb("rd_" + tag, [128, n])
        f.op("dve", lambda e: e.tensor_scalar(out=ki[:], in0=ang, scalar1=1.0 / TWO_PI, scalar2=None, op0=ALU.mult), reads=[R], writes=[R])
        f.op("dve", lambda e: e.tensor_copy(kf[:], ki[:]), reads=[R], writes=[R])
        f.op("dve", lambda e: e.scalar_tensor_tensor(out=rd[:], in0=kf[:], scalar=-CW1, in1=ang, op0=ALU.mult, op1=ALU.add), reads=[R], writes=[R])
        f.op("dve", lambda e: e.scalar_tensor_tensor(out=rd[:], in0=kf[:], scalar=-CW2, in1=rd[:], op0=ALU.mult, op1=ALU.add), reads=[R], writes=[R])
        f.op("dve", lambda e: e.tensor_scalar(out=rd[:], in0=rd[:], scalar1=3.1415925, scalar2=-3.1415925, op0=ALU.min, op1=ALU.max), reads=[R], writes=[R])
        f.op("act", lambda e: e.activation(out=out_s, in_=rd[:], func=AF.Sin), reads=[R], writes=[R])
        f.op("dve", lambda e: e.scalar_tensor_tensor(out=rd[:], in0=rd[:], scalar=-1.0, in1=rd[:], op0=ALU.mult, op1=ALU.max), reads=[R], writes=[R])
        f.op("dve", lambda e: e.tensor_scalar(out=rd[:], in0=rd[:], scalar1=-1.0, scalar2=math.pi / 2, op0=ALU.mult, op1=ALU.add), reads=[R], writes=[R])
        f.op("act", lambda e: e.activation(out=out_c, in_=rd[:], func=AF.Sin), reads=[R], writes=[R])

    def s5_phase(L, uT, R_uT, aT, R_aT):
        P = Scope(nc)
        R = Res("s5setup")
        prm = P.sb("prm", [128, 16, 32])
        dsk = P.sb("dsk", [128, 4]); bgl = P.sb("bgl", [128, 4])
        cs2 = P.sb("cs2", [128, 32, 2]); ncs2 = P.sb("ncs2", [128, 32, 2])
        jt = P.sb("jt", [128, 128]); f.dma("sp", jt[:], k_jidx, writes=[R])
        f.dma("sp", dsk[:], s5_d.rearrange("(c p) -> p c", p=128), writes=[R], allow_slow_non_contiguous=True)
        f.dma("sp", bgl[:], b_glu.rearrange("(c p) -> p c", p=128), writes=[R], allow_slow_non_contiguous=True)
        S = Scope(nc)
        st32 = S.sb("st32", [32, 3, 128]); lsr = S.sb("lsr", [32, 2])
        f.dma("sp", st32[:, 0, :], lam_re.rearrange("d (q g) n -> (d q) (g n)", g=2), writes=[R])
        f.dma("sp", st32[:, 1, :], lam_im.rearrange("d (q g) n -> (d q) (g n)", g=2), writes=[R])
        f.dma("sp", lsr[:], log_step.rearrange("d (q g) -> (d q) g", g=2), writes=[R])
        f.op("dve", lambda e: e.tensor_copy(st32[:, 2, :].rearrange("p (g n) -> p g n", g=2), lsr[:].unsqueeze(2).broadcast_to([32, 2, 64])), reads=[R], writes=[R])
        pst = S.ps("pst", [128, 4, 128])
        for k in range(3):
            f.op("pe", lambda e, k=k: e.transpose(pst[:, k, 0:32], st32[:, k, :], ident[0:32, 0:32]), reads=[R, R_ident], writes=[R], acc=(k > 0))
        f.op("dve", lambda e: e.tensor_copy(prm[:, 0:3, :], pst[:, 0:3, 0:32]), reads=[R], writes=[R])
        lr, li = prm[:, 0, :], prm[:, 1, :]
        dt, th, rr = prm[:, 3, :], prm[:, 4, :], prm[:, 5, :]
        f.op("act", lambda e: e.activation(out=dt, in_=prm[:, 2, :], func=AF.Exp), reads=[R], writes=[R])
        f.op("dve", lambda e: e.tensor_tensor(th, li, dt, ALU.mult), reads=[R], writes=[R])
        f.op("dve", lambda e: e.tensor_tensor(prm[:, 10, :], lr, dt, ALU.mult), reads=[R], writes=[R])
        f.op("act", lambda e: e.activation(out=rr, in_=prm[:, 10, :], func=AF.Exp), reads=[R], writes=[R])
        f.op("dve", lambda e: e.tensor_scalar(out=prm[:, 10, :], in0=th, scalar1=128.0, scalar2=None, op0=ALU.mult), reads=[R], writes=[R])
        sincos(S, prm[:, 10, :], 32, prm[:, 7, :], prm[:, 6, :], R, "a")
        sincos(S, th, 32, prm[:, 12, :], prm[:, 11, :], R, "b")
        abre, abim, den, t1, t2 = prm[:, 13, :], prm[:, 14, :], prm[:, 15, :], prm[:, 10, :], prm[:, 2, :]
        f.op("dve", lambda e: e.tensor_tensor(abre, rr, prm[:, 11, :], ALU.mult), reads=[R], writes=[R])
        f.op("dve", lambda e: e.tensor_scalar(out=abre, in0=abre, scalar1=-1.0, scalar2=None, op0=ALU.add), reads=[R], writes=[R])
        f.op("dve", lambda e: e.tensor_tensor(abim, rr, prm[:, 12, :], ALU.mult), reads=[R], writes=[R])
        f.op("dve", lambda e: e.tensor_tensor(den, lr, lr, ALU.mult), reads=[R], writes=[R])
        f.op("dve", lambda e: e.tensor_tensor(t1, li, li, ALU.mult), reads=[R], writes=[R])
        f.op("dve", lambda e: e.tensor_tensor(den, den, t1, ALU.add), reads=[R], writes=[R])
        f.op("dve", lambda e: e.reciprocal(den, den), reads=[R], writes=[R])
        f.op("dve", lambda e: e.tensor_tensor(t1, abre, lr, ALU.mult), reads=[R], writes=[R])
        f.op("dve", lambda e: e.tensor_tensor(t2, abim, li, ALU.mult), reads=[R], writes=[R])
        f.op("dve", lambda e: e.tensor_tensor(t1, t1, t2, ALU.add), reads=[R], writes=[R])
        f.op("dve", lambda e: e.tensor_tensor(prm[:, 8, :], t1, den, ALU.mult), reads=[R], writes=[R])
        f.op("dve", lambda e: e.tensor_tensor(t1, abim, lr, ALU.mult), reads=[R], writes=[R])
        f.op("dve", lambda e: e.tensor_tensor(t2, abre, li, ALU.mult), reads=[R], writes=[R])
        f.op("dve", lambda e: e.tensor_tensor(t1, t1, t2, ALU.subtract), reads=[R], writes=[R])
        f.op("dve", lambda e: e.tensor_tensor(prm[:, 9, :], t1, den, ALU.mult), reads=[R], writes=[R])
        f.op("dve", lambda e: e.tensor_copy(cs2[:, :, 0], prm[:, 6, :]), reads=[R], writes=[R])
        f.op("dve", lambda e: e.tensor_copy(cs2[:, :, 1], prm[:, 7, :]), reads=[R], writes=[R])
        f.op("dve", lambda e: e.tensor_scalar(out=ncs2[:, :, 0], in0=prm[:, 7, :], scalar1=-1.0, scalar2=None, op0=ALU.mult), reads=[R], writes=[R])
        f.op("dve", lambda e: e.tensor_copy(ncs2[:, :, 1], prm[:, 6, :]), reads=[R], writes=[R])
        S.close()
        cosJ = P.sb("cosJ", [128, 8, 128]); sinJ = P.sb("sinJ", [128, 8, 128]); rtab = P.sb("rtab", [128, 8, 128])
        lB = P.sb("lB", [128, 8, 2, 128], BF16); lC = P.sb("lC", [128, 8, 2, 128], BF16)
        RT = Res("s5tab")
        S = Scope(nc)
        yacc = S.sb("yacc", [128, NTOK]); R_y = Res("yacc")
        NB = 2
        psb = [S.ps("psb%d" % k, [128, 2, 512]) for k in range(NB)]; R_psb = RL(NB)
        psy = [S.ps("psy%d" % k, [128, 512]) for k in range(NB)]; R_psy = RL(NB)
        pstr = [S.ps("pstr%d" % k, [128, 4, 128]) for k in range(2)]; R_pstr = RL(2)
        m = [S.sb("m%d" % k, [128, 2, 512]) for k in range(NB)]; R_m = RL(NB)
        ta = [S.sb("ta%d" % k, [128, 2, 512]) for k in range(NB)]; R_ta = RL(NB)
        g = [S.sb("g%d" % k, [128, 2, 512]) for k in range(NB)]; R_g = RL(NB)
        hb = [S.sb("hb%d" % k, [128, 2, 512], BF16) for k in range(NB)]; R_hb = RL(NB)
        ini = S.sb("ini", [128, 4]); R_ini = Res()
        gq1 = S.sb("gq1", [128, 512]); gq2 = S.sb("gq2", [128, 512]); R_gq1 = Res(); R_gq2 = Res()
        wgl = S.sb("wgl", [128, 4, 512], BF16); R_wgl = Res()
        f.dma("pool", wgl[:], w_glu.rearrange("(kc p) n -> p kc n", p=128), writes=[R_wgl])
        blocks = [(0, 256)] + [(256 + 512 * k, 512) for k in range(8)]
        it = 0
        for ct in range(4):
            T = Scope(nc)
            ang = T.sb("ang", [128, 8, 128])
            WB = T.sb("WB", [128, 2, 8, 128]); SC = T.sb("SC", [128, 2, 8, 128]); WB2 = T.sb("WB2", [128, 2, 8, 128])
            fre8 = T.sb("fre8", [128, 8]); fim8 = T.sb("fim8", [128, 8])
            for d in range(2):
                gsl = slice(d * 16 + ct * 4, d * 16 + ct * 4 + 4); lsl = slice(d * 4, d * 4 + 4)
                f.op("dve", lambda e: e.tensor_tensor(ang[:, lsl, :], jt[:].unsqueeze(1).broadcast_to([128, 4, 128]), th[:, gsl].unsqueeze(2).broadcast_to([128, 4, 128]), ALU.mult), reads=[R, RT], writes=[RT])
                f.op("dve", lambda e: e.tensor_copy(rtab[:, lsl, :], rr[:, gsl].unsqueeze(2).broadcast_to([128, 4, 128])), reads=[R, RT], writes=[RT])
                f.op("dve", lambda e: e.tensor_copy(fre8[:, lsl], prm[:, 8, gsl]), reads=[R, RT], writes=[RT])
                f.op("dve", lambda e: e.tensor_copy(fim8[:, lsl], prm[:, 9, gsl]), reads=[R, RT], writes=[RT])
            sincos(T, ang[:].rearrange("p a b -> p (a b)"), 1024, sinJ[:].rearrange("p a b -> p (a b)"), cosJ[:].rearrange("p a b -> p (a b)"), RT, "c%d" % ct)
            f.op("pool", lambda e: e.memset(WB[:], 0.0), reads=[RT], writes=[RT])
            f.op("pool", lambda e: e.memset(SC[:], 0.0), reads=[RT], writes=[RT])
            qn = 0
            for d in range(2):
                for gi in range(8):
                    g_ = ct * 8 + gi
                    l = d * 4 + gi // 2
                    gl = gi % 2
                    for ri, (bsrc, csrc) in enumerate(((b_re, c_re), (b_im, c_im))):
                        q1 = ("sp", "act")[qn % 2]; qn += 1
                        f.dma(q1, WB[64 * gl:64 * gl + 64, ri, l, 16 * gi:16 * gi + 16], bsrc[d, g_], writes=[RT])
                        f.dma(q1, SC[16 * gi:16 * gi + 16, ri, l, 64 * gl:64 * gl + 64], csrc[d, g_], writes=[RT])
            fre = fre8[:].unsqueeze(2).broadcast_to([128, 8, 128]); fim = fim8[:].unsqueeze(2).broadcast_to([128, 8, 128])
            f.op("dve", lambda e: e.tensor_tensor(WB2[:, 0], WB[:, 0], fre, ALU.mult), reads=[RT], writes=[RT])
            f.op("pool", lambda e: e.tensor_tensor(WB2[:, 1], WB[:, 1], fim, ALU.mult), reads=[RT], writes=[RT])
            f.op("dve", lambda e: e.tensor_tensor(WB2[:, 0], WB2[:, 0], WB2[:, 1], ALU.subtract), reads=[RT], writes=[RT])
            f.op("pool", lambda e: e.tensor_tensor(WB2[:, 1], WB[:, 1], fre, ALU.mult), reads=[RT], writes=[RT])
            f.op("dve", lambda e: e.tensor_tensor(WB[:, 0], WB[:, 0], fim, ALU.mult), reads=[RT], writes=[RT])
            f.op("dve", lambda e: e.tensor_tensor(WB2[:, 1], WB2[:, 1], WB[:, 0], ALU.add), reads=[RT], writes=[RT])
            n_ = 0
            for srct, dst, neg in ((WB2, lB, False), (SC, lC, True)):
                for ri in range(2):
                    for d4 in range(2):
                        pb = n_ % 2; n_ += 1
                        for k in range(4):
                            l = d4 * 4 + k
                            f.op("pe", lambda e, k=k, l=l: e.transpose(pstr[pb][:, k, :], srct[:, ri, l, :], ident[:]), reads=[RT, R_ident], writes=[R_pstr[pb]], acc=(k > 0))
                        scl = -1.0 if (neg and ri == 1) else 1.0
                        f.op("act", lambda e: e.activation(out=dst[:, d4 * 4:d4 * 4 + 4, ri, :], in_=pstr[pb][:], func=AF.Identity, scale=scl), reads=[R_pstr[pb], RT], writes=[RT])
            T.close()
            f.op("act", lambda e: e.activation(out=yacc[:], in_=uT[:, ct, :], func=AF.Copy, scale=dsk[:, ct:ct + 1]), reads=R_uT + [R], writes=[R_y])
            items = []
            for pi in range(4):
                for d in range(2):
                    for bidx, (s0, n) in enumerate(blocks):
                        items.append((pi, d, bidx, s0, n))
            NI = len(items)

            def v3(ap):
                return ap.rearrange("p (c j) -> p c j", j=128)

            def geom(k):
                pi, d, bidx, s0, n = items[k]
                bi = k % NB
                dq = d * 16 + ct * 4 + pi
                l = d * 4 + pi
                nch = n // 128
                if d == 0:
                    c0 = s0
                    ucols = uT[:, ct, c0:c0 + n]
                    ycols = yacc[:, c0:c0 + n]
                else:
                    c0 = (256 - s0 - n) if s0 < 256 else (4608 - s0 - n)
                    ucols = rev_ap(uT[:, ct, c0:c0 + n], n)
                    ycols = rev_ap(yacc[:, c0:c0 + n], n)
                tl = [R_uT[kk] for kk in range(c0 // 128, (c0 + n) // 128)]
                cb = cosJ[:, l, :].unsqueeze(1).broadcast_to([128, nch, 128])
                sb_ = sinJ[:, l, :].unsqueeze(1).broadcast_to([128, nch, 128])
                return pi, d, bidx, n, bi, dq, l, nch, ucols, ycols, tl, cb, sb_

            def stA(k):
                pi, d, bidx, n, bi, dq, l, nch, ucols, ycols, tl, cb, sb_ = geom(k)
                for ri in range(2):
                    f.op("pe", lambda e, ri=ri: e.matmul(psb[bi][:, ri, 0:n], lB[:, l, ri, :], ucols, start=True, stop=True),
                         reads=[RT] + tl, writes=[R_psb[bi]], acc=(ri > 0))
                bre, bim = v3(psb[bi][:, 0, 0:n]), v3(psb[bi][:, 1, 0:n])
                mre, mim = v3(m[bi][:, 0, 0:n]), v3(m[bi][:, 1, 0:n])
                t_a, t_b = v3(ta[bi][:, 0, 0:n]), v3(ta[bi][:, 1, 0:n])
                f.op("dve", lambda e: e.tensor_tensor(mre, bre, cb, ALU.mult), reads=[R_psb[bi], RT], writes=[R_m[bi]])
                f.op("dve", lambda e: e.tensor_tensor(t_a, bim, sb_, ALU.mult), reads=[R_psb[bi], RT], writes=[R_ta[bi]])
                f.op("dve", lambda e: e.tensor_tensor(mre, mre, t_a, ALU.add), reads=[R_m[bi], R_ta[bi]], writes=[R_m[bi]])
                f.op("dve", lambda e: e.tensor_tensor(mim, bim, cb, ALU.mult), reads=[R_psb[bi], RT], writes=[R_m[bi]])
                f.op("dve", lambda e: e.tensor_tensor(t_b, bre, sb_, ALU.mult), reads=[R_psb[bi], RT], writes=[R_ta[bi]])
                f.op("dve", lambda e: e.tensor_tensor(mim, mim, t_b, ALU.subtract), reads=[R_m[bi], R_ta[bi]], writes=[R_m[bi]])

            def stB(k):
                pi, d, bidx, n, bi, dq, l, nch, ucols, ycols, tl, cb, sb_ = geom(k)
                prev = None
                if bidx > 0:
                    pbi = (k - 1) % NB
                    pn = items[k - 1][4]
                    prev = (g[pbi], pbi, pn // 128 - 1)
                for c in range(nch):
                    cs = slice(c * 128, (c + 1) * 128)
                    if prev is None:
                        i_re = i_im = 0.0
                        rd_extra = []
                    else:
                        pg, pbi, pc_ = prev
                        gre_l = pg[:, 0, pc_ * 128 + 127:pc_ * 128 + 128]
                        gim_l = pg[:, 1, pc_ * 128 + 127:pc_ * 128 + 128]
                        c128 = prm[:, 6, dq:dq + 1]; s128 = prm[:, 7, dq:dq + 1]
                        f.op("dve", lambda e: e.tensor_scalar(out=ini[:, 0:2], in0=cs2[:, dq, :], scalar1=gre_l, scalar2=None, op0=ALU.mult), reads=[R_g[pbi], R], writes=[R_ini])
                        f.op("dve", lambda e: e.scalar_tensor_tensor(out=ini[:, 0:2], in0=ncs2[:, dq, :], scalar=gim_l, in1=ini[:, 0:2], op0=ALU.mult, op1=ALU.add), reads=[R_g[pbi], R, R_ini], writes=[R_ini])
                        i_re, i_im = ini[:, 0:1], ini[:, 1:2]
                        rd_extra = [R_ini]
                    f.op("dve", lambda e: e.tensor_tensor_scan(g[bi][:, 0, cs], rtab[:, l, :], m[bi][:, 0, cs], i_re, ALU.mult, ALU.add),
                         reads=[R_m[bi], RT] + rd_extra, writes=[R_g[bi]])
                    f.op("dve", lambda e: e.tensor_tensor_scan(g[bi][:, 1, cs], rtab[:, l, :], m[bi][:, 1, cs], i_im, ALU.mult, ALU.add),
                         reads=[R_m[bi], RT] + rd_extra, writes=[R_g[bi]])
                    prev = (g[bi], bi, c)

            def stC(k):
                pi, d, bidx, n, bi, dq, l, nch, ucols, ycols, tl, cb, sb_ = geom(k)
                mre, mim = v3(m[bi][:, 0, 0:n]), v3(m[bi][:, 1, 0:n])
                t_a, t_b = v3(ta[bi][:, 0, 0:n]), v3(ta[bi][:, 1, 0:n])
                gre, gim = v3(g[bi][:, 0, 0:n]), v3(g[bi][:, 1, 0:n])
                hre, him = v3(hb[bi][:, 0, 0:n]), v3(hb[bi][:, 1, 0:n])
                f.op("dve", lambda e: e.tensor_tensor(t_a, gre, cb, ALU.mult), reads=[R_g[bi], RT], writes=[R_ta[bi]])
                f.op("dve", lambda e: e.tensor_tensor(mre, gim, sb_, ALU.mult), reads=[R_g[bi], RT], writes=[R_m[bi]])
                f.op("dve", lambda e: e.tensor_tensor(hre, t_a, mre, ALU.subtract), reads=[R_ta[bi], R_m[bi]], writes=[R_hb[bi]])
                f.op("dve", lambda e: e.tensor_tensor(t_b, gre, sb_, ALU.mult), reads=[R_g[bi], RT], writes=[R_ta[bi]])
                f.op("dve", lambda e: e.tensor_tensor(mim, gim, cb, ALU.mult), reads=[R_g[bi], RT], writes=[R_m[bi]])
                f.op("dve", lambda e: e.tensor_tensor(him, t_b, mim, ALU.add), reads=[R_ta[bi], R_m[bi]], writes=[R_hb[bi]])
                for ri in range(2):
                    f.op("pe", lambda e, ri=ri: e.matmul(psy[bi][:, 0:n], lC[:, l, ri, :], hb[bi][:, ri, 0:n], start=(ri == 0), stop=(ri == 1)),
                         reads=[RT, R_hb[bi]], writes=[R_psy[bi]], acc=(ri > 0))

            def stY(k):
                pi, d, bidx, n, bi, dq, l, nch, ucols, ycols, tl, cb, sb_ = geom(k)
                f.op("dve", lambda e: e.tensor_tensor(ycols, psy[bi][:, 0:n], ycols, ALU.add), reads=[R_psy[bi], R_y], writes=[R_y])

            stA(0)
            for k in range(NI):
                if k + 1 < NI:
                    stA(k + 1)
                stB(k)
                stC(k)
                if k >= 1:
                    stY(k - 1)
            stY(NI - 1)
            for (s0, n) in blocks:
                yb = yacc[:, s0:s0 + n]
                f.op("pool", lambda e: e.tensor_tensor(gq1[:, 0:n], yb, yb, ALU.mult), reads=[R_y], writes=[R_gq1])
                f.op("dve", lambda e: e.tensor_scalar(out=gq1[:, 0:n], in0=gq1[:, 0:n], scalar1=0.044715, scalar2=1.0, op0=ALU.mult, op1=ALU.add), reads=[R_gq1], writes=[R_gq1])
                f.op("pool", lambda e: e.tensor_tensor(gq1[:, 0:n], gq1[:, 0:n], yb, ALU.mult), reads=[R_gq1, R_y], writes=[R_gq1])
                f.op("act", lambda e: e.activation(out=gq2[:, 0:n], in_=gq1[:, 0:n], func=AF.Sigmoid, scale=1.5957691216057308), reads=[R_gq1], writes=[R_gq2])
                f.op("dve", lambda e: e.tensor_tensor(aT[:, ct, s0:s0 + n], yb, gq2[:, 0:n], ALU.mult), reads=[R_gq2, R_y], writes=R_aT[s0 // 128:(s0 + n) // 128])
        sg = [S.sb("sg%d" % k, [128, 512], BF16) for k in range(2)]; R_sg = RL(2)
        anew = S.sb("anew", [128, 4, 512], BF16); R_anew = Res()
        nn = 0
        for (s0, n) in blocks:
            tl = R_aT[s0 // 128:(s0 + n) // 128]
            for cto in range(4):
                bi = nn % 2; nn += 1
                for cti in range(4):
                    f.op("pe", lambda e, cti=cti: e.matmul(psy[bi][:, 0:n], wgl[:, cti, cto * 128:(cto + 1) * 128], aT[:, cti, s0:s0 + n], start=(cti == 0), stop=(cti == 3)),
                         reads=[R_wgl] + tl, writes=[R_psy[bi]], acc=(cti > 0))
                f.op("act", lambda e: e.activation(out=sg[bi][:, 0:n], in_=psy[bi][:, 0:n], func=AF.Sigmoid, bias=bgl[:, cto:cto + 1], scale=1.0), reads=[R_psy[bi], R], writes=[R_sg[bi]])
                f.op("dve", lambda e: e.tensor_tensor(anew[:, cto, 0:n], aT[:, cto, s0:s0 + n], sg[bi][:, 0:n], ALU.mult), reads=[R_sg[bi]] + tl, writes=[R_anew])
            f.op("pool", lambda e: e.tensor_copy(aT[:, :, s0:s0 + n], anew[:, :, 0:n]), reads=[R_anew], writes=tl)
        S.close()
        P.close()

    def win_phase(qT, R_qT, kT2, R_kT, vaug, R_v, oT, R_oT):
        S = Scope(nc)
        esink = S.sb("esink", [128, 8]); R_es = Res()
        f.dma("sp", esink[:], win_sink.partition_broadcast(128), writes=[R_es])
        f.op("act", lambda e: e.activation(out=esink[:], in_=esink[:], func=AF.Exp), reads=[R_es], writes=[R_es])
        NBS = 3
        ps_s = [S.ps("ps_s%d" % k, [128, 8, 128]) for k in range(NBS)]; R_pss = RL(NBS)
        ps_o = [S.ps("ps_o%d" % k, [128, 512]) for k in range(2)]; R_pso = RL(2)
        pT = [S.sb("pT%d" % k, [128, 5, 128], BF16) for k in range(NBS)]; R_pT = RL(NBS)
        dtmp = [S.sb("dtmp%d" % k, [128, 128]) for k in range(2)]; R_dt = RL(2)
        items = []
        for t in range(NT):
            kts = [(0, None), (1, None)]
            if t >= 2:
                for kt in (t - 1, t, t + 1):
                    if 2 <= kt < NT:
                        kts.append((kt, (0 if kt == t - 1 else (1 if kt == t + 1 else None))))
            for h in range(8):
                items.append((t, h, kts))

        def front(i_):
            t, h, kts = items[i_]
            cols = slice(t * 128, (t + 1) * 128)
            nk = len(kts)
            bs = i_ % NBS
            pr, base, kvh = h // 2, 64 * (h % 2), h // 4
            for i, (kt, mk) in enumerate(kts):
                f.op("pe", lambda e, i=i, kt=kt: e.matmul(ps_s[bs][:, i, :], kT2[base:base + 64, kvh, kt * 128:(kt + 1) * 128], qT[base:base + 64, pr, cols], start=True, stop=True),
                     reads=[R_kT[kt], R_qT[t]], writes=[R_pss[bs]], acc=(i > 0))
            f.op("act", lambda e: e.activation(out=pT[bs][:, 0:nk, :], in_=ps_s[bs][:, 0:nk, :], func=AF.Exp, scale=0.125), reads=[R_pss[bs]], writes=[R_pT[bs]])
            for i, (kt, mk) in enumerate(kts):
                if mk is not None:
                    f.op("dve", lambda e, i=i, mk=mk: e.tensor_tensor(pT[bs][:, i, :], pT[bs][:, i, :], maskb[:, mk, :], ALU.mult), reads=[R_pT[bs], R_mask], writes=[R_pT[bs]])

        def back(i_):
            t, h, kts = items[i_]
            cols = slice(t * 128, (t + 1) * 128)
            nk = len(kts)
            bs = i_ % NBS
            bi = i_ % 2
            pr, base, kvh = h // 2, 64 * (h % 2), h // 4
            voff = (64 if h % 2 == 0 else 0) + 128 * kvh
            for i, (kt, mk) in enumerate(kts):
                f.op("pe", lambda e, i=i, kt=kt: e.matmul(ps_o[bi][:, 0:128], vaug[:, kt, voff:voff + 128], pT[bs][:, i, :], start=(i == 0), stop=(i == nk - 1)),
                     reads=[R_v[kt], R_pT[bs]], writes=[R_pso[bi]], acc=(i > 0))
            nb, db = (0, 64) if h % 2 == 0 else (64, 0)
            f.op("dve", lambda e: e.tensor_scalar(out=dtmp[bi][nb:nb + 64, :], in0=ps_o[bi][db:db + 64, 0:128], scalar1=esink[db:db + 64, h:h + 1], scalar2=None, op0=ALU.add),
                 reads=[R_pso[bi], R_es], writes=[R_dt[bi]])
            f.op("dve", lambda e: e.reciprocal(dtmp[bi][nb:nb + 64, :], dtmp[bi][nb:nb + 64, :]), reads=[R_dt[bi]], writes=[R_dt[bi]])
            f.op("dve", lambda e: e.tensor_tensor(oT[nb:nb + 64, pr, cols], ps_o[bi][nb:nb + 64, 0:128], dtmp[bi][nb:nb + 64, :], ALU.mult), reads=[R_pso[bi], R_dt[bi]], writes=[R_oT[t]])
        front(0)
        for i_ in range(len(items)):
            if i_ + 1 < len(items):
                front(i_ + 1)
            back(i_)
        S.close()

    def out_phase(layer, w_out_d, mix_loader, tiles):
        S = Scope(nc)
        load_ln(layer * 2 + 0)
        wo = S.sb("wo", [128, 8, D], BF16); R_wo = Res()
        f.dma("pool", wo[:], w_out_d.rearrange("(kc p) n -> p kc n", p=128), writes=[R_wo])
        xt = [S.sb("xt%d" % k, [128, D]) for k in range(2)]; R_xt = RL(2)
        ot = [S.sb("ot%d" % k, [128, D]) for k in range(2)]; R_ot = RL(2)
        tmp = S.sb("tmp", [128, D]); R_tmp = Res()
        small = S.sb("small", [128, 16]); R_small = Res()
        ps_o2 = [S.ps("ps_o2%d" % k, [128, D]) for k in range(2)]; R_ps = RL(2)
        for n, t in enumerate(tiles):
            b = n % 2
            src, rs = src_tile(layer, t)
            f.dma("sp", xt[b][:], src, reads=rs, writes=[R_xt[b]])
            mixT, R_mix = mix_loader(t, b)
            for half in range(2):
                for kc in range(8):
                    f.op("pe", lambda e, kc=kc, half=half: e.matmul(ps_o2[b][:, half * 512:(half + 1) * 512], mixT[kc], wo[:, kc, half * 512:(half + 1) * 512], start=(kc == 0), stop=(kc == 7)),
                         reads=[R_wo] + R_mix, writes=[R_ps[b]], acc=(half + kc > 0))
            resid_ln(S, xt[b], R_xt[b], ps_o2[b], R_ps[b], (1 if t < 2 else 0), layer * 2 + 0, ot[b], R_ot[b], tmp, R_tmp, small, R_small)
            f.dma("act", XR[t * 128:(t + 1) * 128, :], ot[b][:], reads=[R_ot[b]], writes=[R_XR[t]])
        S.close()

    def ffn_phase(layer, tiles_all, final):
        P = Scope(nc)
        rw = P.sb("rw", [128, 8, 32]); R_rw = Res()
        f.dma("sp", rw[:], router_w.rearrange("(kc p) n -> p kc n", p=128), writes=[R_rw])
        rbias = P.sb("rbias", [128, 32]); f.dma("sp", rbias[:], router_b.partition_broadcast(128), writes=[R_rw])
        GT = 9
        load_ln(layer * 2 + 1)
        groups = [tiles_all[i:i + GT] for i in range(0, len(tiles_all), GT)]
        for grp in groups:
            S = Scope(nc)
            ng = len(grp)
            hT = S.sb("hTg", [128, 8, GT * 128], BF16); R_hT = RL(ng, "hTg")
            comb = S.sb("comb", [128, GT, 32]); R_comb = RL(ng, "comb")
            yacc = S.sb("yaccg", [128, GT, D]); R_y = RL(ng, "yg")
            A = Scope(nc)
            xt = [A.sb("xt%d" % k, [128, D]) for k in range(2)]; R_xt = RL(2)
            h32 = A.sb("h32", [128, D]); R_h32 = Res()
            h32T = A.sb("h32T", [128, 8, 128]); R_h32T = Res()
            ps_tp = A.ps("ps_tp", [128, 8, 128]); R_pstp = Res(x=True)
            ps_r = A.ps("ps_r", [128, 512]); R_psr = Res()
            sc = A.sb("sc", [128, 32]); sel = A.sb("sel", [128, 32]); R_sc = Res()
            pa = A.sb("pa", [128, 8, 6]); pm = A.sb("pm", [128, 8, 6]); gs = A.sb("gs", [128, 8]); thr = A.sb("thr", [128, 8])
            gm = A.sb("gm", [128, 2]); mg = A.sb("mg", [128, 8]); sm = A.sb("sm", [128, 8, 4])
            for j, t in enumerate(grp):
                b = j % 2
                f.dma("sp", xt[b][:], XR[t * 128:(t + 1) * 128, :], reads=[R_XR[t]], writes=[R_xt[b]])
                which = 1 if t < 2 else 0
                mod_transpose(xt[b], R_xt[b], which, h32, R_h32, ps_tp, R_pstp, hT[:, :, j * 128:(j + 1) * 128], R_hT[j], h32T, R_h32T)
                for kc in range(8):
                    f.op("pe", lambda e, kc=kc: e.matmul(ps_r[:, 0:32], h32T[:, kc, :], rw[:, kc, :], start=(kc == 0), stop=(kc == 7)), reads=[R_h32T, R_rw], writes=[R_psr], acc=(kc > 0))
                R1 = R_sc
                f.op("act", lambda e: e.activation(out=sc[:], in_=ps_r[:, 0:32], func=AF.Sigmoid), reads=[R_psr], writes=[R1])
                f.op("dve", lambda e: e.tensor_tensor(sel[:], sc[:], rbias[:], ALU.add), reads=[R1, R_rw], writes=[R1])
                s3 = sel[:].rearrange("p (g e) -> p g e", e=4)
                pairs = [(0, 1), (0, 2), (0, 3), (1, 2), (1, 3), (2, 3)]
                for k, (a_, b_) in enumerate(pairs):
                    f.op("dve", lambda e, k=k, a_=a_, b_=b_: e.tensor_tensor(pa[:, :, k], s3[:, :, a_], s3[:, :, b_], ALU.add), reads=[R1], writes=[R1])
                    f.op("dve", lambda e, k=k, a_=a_, b_=b_: e.tensor_tensor(pm[:, :, k], s3[:, :, a_], s3[:, :, b_], ALU.min), reads=[R1], writes=[R1])
                f.op("dve", lambda e: e.tensor_reduce(out=gs[:], in_=pa[:], axis=AX.X, op=ALU.max), reads=[R1], writes=[R1])
                f.op("dve", lambda e: e.tensor_reduce(out=thr[:], in_=pm[:], axis=AX.X, op=ALU.max), reads=[R1], writes=[R1])
                f.op("dve", lambda e: e.tensor_reduce(out=gm[:, 0:1], in_=gs[:], axis=AX.X, op=ALU.max), reads=[R1], writes=[R1])
                f.op("dve", lambda e: e.tensor_scalar(out=mg[:], in0=gs[:], scalar1=gm[:, 0:1], scalar2=None, op0=ALU.is_ge), reads=[R1], writes=[R1])
                f.op("dve", lambda e: e.tensor_tensor(sm[:], s3, thr[:].unsqueeze(2).broadcast_to([128, 8, 4]), ALU.is_ge), reads=[R1], writes=[R1])
                f.op("dve", lambda e: e.tensor_tensor(sm[:], sm[:], mg[:].unsqueeze(2).broadcast_to([128, 8, 4]), ALU.mult), reads=[R1], writes=[R1])
                cj = comb[:, j, :]
                f.op("dve", lambda e: e.tensor_tensor(cj, sm[:].rearrange("p g e -> p (g e)"), sc[:], ALU.mult), reads=[R1], writes=[R_comb[j]])
                f.op("dve", lambda e: e.tensor_reduce(out=gm[:, 1:2], in_=cj, axis=AX.X, op=ALU.add), reads=[R_comb[j], R1], writes=[R1])
                f.op("dve", lambda e: e.reciprocal(gm[:, 1:2], gm[:, 1:2]), reads=[R1], writes=[R1])
                f.op("dve", lambda e: e.tensor_scalar(out=cj, in0=cj, scalar1=gm[:, 1:2], scalar2=None, op0=ALU.mult), reads=[R1, R_comb[j]], writes=[R_comb[j]])
            A.close()
            B = Scope(nc)
            wg = [B.sb("wg%d" % k, [128, 8, 512], BF16) for k in range(2)]
            wu = [B.sb("wu%d" % k, [128, 8, 512], BF16) for k in range(2)]
            wd = [B.sb("wd%d" % k, [128, 4, D], BF16) for k in range(2)]
            R_w = RL(2, "w")
            psg = [B.ps("psg%d" % k, [128, 512]) for k in range(2)]; R_psg = RL(2)
            psu = [B.ps("psu%d" % k, [128, 512]) for k in range(2)]; R_psu = RL(2)
            psd = [B.ps("psd%d" % k, [128, D]) for k in range(2)]; R_psd = RL(2)
            sg = [B.sb("sg%d" % k, [128, 512]) for k in range(2)]; R_sg = RL(2)
            hid = [B.sb("hid%d" % k, [128, 4, 512], BF16) for k in range(2)]; R_hid = RL(2)
            ntok = ng * 128
            blocks = [(c0, min(512, ntok - c0)) for c0 in range(0, ntok, 512)]
            nfc = 0; nblk = 0; nd = 0
            for ex in range(32):
                wb = ex % 2
                f.dma("pool", wg[wb][:], w_gate[layer, ex].rearrange("(kc p) n -> p kc n", p=128), writes=[R_w[wb]])
                f.dma("pool", wu[wb][:], w_up[layer, ex].rearrange("(kc p) n -> p kc n", p=128), writes=[R_w[wb]])
                f.dma("pool", wd[wb][:], w_down[layer, ex].rearrange("(kc p) n -> p kc n", p=128), writes=[R_w[wb]])
                for (c0, n) in blocks:
                    hb_ = nblk % 2; nblk += 1
                    tl = R_hT[c0 // 128:(c0 + n) // 128]
                    for fc in range(4):
                        pb = nfc % 2; nfc += 1
                        for kc in range(8):
                            f.op("pe", lambda e, kc=kc, fc=fc: e.matmul(psg[pb][:, 0:n], wg[wb][:, kc, fc * 128:(fc + 1) * 128], hT[:, kc, c0:c0 + n], start=(kc == 0), stop=(kc == 7)),
                                 reads=[R_w[wb]] + tl, writes=[R_psg[pb]], acc=(kc > 0))
                        for kc in range(8):
                            f.op("pe", lambda e, kc=kc, fc=fc: e.matmul(psu[pb][:, 0:n], wu[wb][:, kc, fc * 128:(fc + 1) * 128], hT[:, kc, c0:c0 + n], start=(kc == 0), stop=(kc == 7)),
                                 reads=[R_w[wb]] + tl, writes=[R_psu[pb]], acc=(kc > 0))
                        f.op("act", lambda e: e.activation(out=sg[pb][:, 0:n], in_=psg[pb][:, 0:n], func=AF.Silu), reads=[R_psg[pb]], writes=[R_sg[pb]])
                        f.op("dve", lambda e, fc=fc: e.tensor_tensor(hid[hb_][:, fc, 0:n], sg[pb][:, 0:n], psu[pb][:, 0:n], ALU.mult), reads=[R_sg[pb], R_psu[pb]], writes=[R_hid[hb_]])
                    for tt in range(n // 128):
                        j = c0 // 128 + tt
                        db = nd % 2; nd += 1
                        for half in range(2):
                            for fc in range(4):
                                f.op("pe", lambda e, fc=fc, half=half: e.matmul(psd[db][:, half * 512:(half + 1) * 512], hid[hb_][:, fc, tt * 128:(tt + 1) * 128], wd[wb][:, fc, half * 512:(half + 1) * 512], start=(fc == 0), stop=(fc == 3)),
                                     reads=[R_w[wb], R_hid[hb_]], writes=[R_psd[db]], acc=(half + fc > 0))
                        cw = comb[:, j, ex:ex + 1]
                        if ex == 0:
                            f.op("dve", lambda e: e.tensor_scalar(out=yacc[:, j, :], in0=psd[db][:], scalar1=cw, scalar2=None, op0=ALU.mult), reads=[R_psd[db], R_comb[j]], writes=[R_y[j]])
                        else:
                            f.op("dve", lambda e: e.scalar_tensor_tensor(out=yacc[:, j, :], in0=psd[db][:], scalar=cw, in1=yacc[:, j, :], op0=ALU.mult, op1=ALU.add), reads=[R_psd[db], R_comb[j], R_y[j]], writes=[R_y[j]])
            B.close()
            C = Scope(nc)
            xt = [C.sb("xt%d" % k, [128, D]) for k in range(2)]; R_xt = RL(2)
            ot = [C.sb("ot%d" % k, [128, D]) for k in range(2)]; R_ot = RL(2)
            tmp = C.sb("tmp", [128, D]); R_tmp = Res()
            small = C.sb("small", [128, 16]); R_small = Res()
            for j, t in enumerate(grp):
                b = j % 2
                f.dma("sp", xt[b][:], XR[t * 128:(t + 1) * 128, :], reads=[R_XR[t]], writes=[R_xt[b]])
                yj = yacc[:, j, :]

                class _V:
                    def __init__(self, ap): self.ap = ap
                    def __getitem__(self, k): return self.ap
                resid_ln(C, xt[b], R_xt[b], _V(yj), R_y[j], (1 if t < 2 else 0), layer * 2 + 1, ot[b], R_ot[b], tmp, R_tmp, small, R_small)
                if final:
                    f.dma("act", out_d[(t - 2) * 128:(t - 1) * 128, :], ot[b][:], reads=[R_ot[b]], writes=[R_out])
                else:
                    f.dma("act", XR[t * 128:(t + 1) * 128, :], ot[b][:], reads=[R_ot[b]], writes=[R_XR[t]])
            C.close()
            S.close()
        P.close()


    def ffn_sparse(layer, tiles_all, final):
        IOA = bass.IndirectOffsetOnAxis
        ng = len(tiles_all)
        M = ng * 32
        P = Scope(nc)
        rw = P.sb("rw", [128, 8, 32]); R_rw = Res()
        f.dma("sp", rw[:], router_w.rearrange("(kc p) n -> p kc n", p=128), writes=[R_rw])
        rbias = P.sb("rbias", [128, 32]); f.dma("sp", rbias[:], router_b.partition_broadcast(128), writes=[R_rw])
        jt = P.sb("jt2", [128, 128]); f.dma("sp", jt[:], k_jidx, writes=[R_rw])
        pc = P.sb("pc", [128, 4]); f.dma("sp", pc[:], k_pc, writes=[R_rw])
        load_ln(layer * 2 + 1)
        comb = P.sb("comb", [128, ng, 32]); R_comb = RL(ng, "comb")
        posA_i = P.sb("posA_i", [128, ng], I32); posB_i = P.sb("posB_i", [128, ng], I32)
        wA = P.sb("wA", [128, ng]); wB = P.sb("wB", [128, ng])
        NSO = NS - 32
        idxw = P.sb("idxw", [128, NSO, 4], I32)
        R_rt = Res("route")
        R_XsW = RL(ng, "xsw")
        R_Ys = RL(NS, "ys")
        HB = Scope(nc)
        hb_all = HB.sb("hb_all", [128, ng, D], BF16); R_hb = RL(ng, "hb")
        A = Scope(nc)
        xt = [A.sb("xt%d" % k, [128, D]) for k in range(2)]; R_xt = RL(2)
        h32 = [A.sb("h32%d" % k, [128, D]) for k in range(2)]; R_h32 = RL(2)
        h32T = A.sb("h32T", [128, 8, 128]); R_h32T = Res()
        ps_tp = [A.ps("ps_tp%d" % k, [128, 8, 128]) for k in range(2)]; R_pstp = RL(2)
        ps_r = A.ps("ps_r", [128, 512]); R_psr = Res()
        sc = A.sb("sc", [128, 32]); sel = A.sb("sel", [128, 32]); R_sc = Res()
        pa = A.sb("pa", [128, 8, 6]); pm = A.sb("pm", [128, 8, 6]); gs = A.sb("gs", [128, 8]); thr = A.sb("thr", [128, 8])
        gm = A.sb("gm", [128, 2]); mg = A.sb("mg", [128, 8]); sm = A.sb("sm", [128, 8, 4])
        for j, t in enumerate(tiles_all):
            b = j % 2
            f.dma("sp", xt[b][:], XR[t * 128:(t + 1) * 128, :], reads=[R_XR[t]], writes=[R_xt[b]])
            which = 1 if t < 2 else 0
            f.op("dve", lambda e: e.tensor_tensor(h32[b][:], xt[b][:], mod[:, which, 1, :], ALU.mult), reads=[R_xt[b], R_mod], writes=[R_h32[b]])
            f.op("dve", lambda e: e.tensor_tensor(h32[b][:], h32[b][:], mod[:, which, 0, :], ALU.add), reads=[R_h32[b], R_mod], writes=[R_h32[b]])
            for kc in range(8):
                f.op("pe", lambda e, kc=kc: e.transpose(ps_tp[b][:, kc, :], h32[b][:, kc * 128:(kc + 1) * 128], ident[:]),
                     reads=[R_h32[b], R_ident], writes=[R_pstp[b]], acc=(kc > 0))
            f.op("dve", lambda e: e.tensor_copy(h32T[:], ps_tp[b][:]), reads=[R_pstp[b]], writes=[R_h32T])
            f.op("act", lambda e: e.activation(out=hb_all[:, j, :].rearrange("p (c j q) -> p c j q", c=4, j=2),
                                               in_=h32[b][:].rearrange("p (c q j) -> p c j q", c=4, j=2), func=AF.Identity),
                 reads=[R_h32[b]], writes=[R_hb[j]])
            for kc in range(8):
                f.op("pe", lambda e, kc=kc: e.matmul(ps_r[:, 0:32], h32T[:, kc, :], rw[:, kc, :], start=(kc == 0), stop=(kc == 7)), reads=[R_h32T, R_rw], writes=[R_psr], acc=(kc > 0))
            R1 = R_sc
            f.op("act", lambda e: e.activation(out=sc[:], in_=ps_r[:, 0:32], func=AF.Sigmoid), reads=[R_psr], writes=[R1])
            f.op("dve", lambda e: e.tensor_tensor(sel[:], sc[:], rbias[:], ALU.add), reads=[R1, R_rw], writes=[R1])
            s3 = sel[:].rearrange("p (g e) -> p g e", e=4)
            pairs = [(0, 1), (0, 2), (0, 3), (1, 2), (1, 3), (2, 3)]
            for k, (a_, b_) in enumerate(pairs):
                f.op("dve", lambda e, k=k, a_=a_, b_=b_: e.tensor_tensor(pa[:, :, k], s3[:, :, a_], s3[:, :, b_], ALU.add), reads=[R1], writes=[R1])
                f.op("dve", lambda e, k=k, a_=a_, b_=b_: e.tensor_tensor(pm[:, :, k], s3[:, :, a_], s3[:, :, b_], ALU.min), reads=[R1], writes=[R1])
            f.op("dve", lambda e: e.tensor_reduce(out=gs[:], in_=pa[:], axis=AX.X, op=ALU.max), reads=[R1], writes=[R1])
            f.op("dve", lambda e: e.tensor_reduce(out=thr[:], in_=pm[:], axis=AX.X, op=ALU.max), reads=[R1], writes=[R1])
            f.op("dve", lambda e: e.tensor_reduce(out=gm[:, 0:1], in_=gs[:], axis=AX.X, op=ALU.max), reads=[R1], writes=[R1])
            f.op("dve", lambda e: e.tensor_scalar(out=mg[:], in0=gs[:], scalar1=gm[:, 0:1], scalar2=None, op0=ALU.is_ge), reads=[R1], writes=[R1])
            f.op("dve", lambda e: e.tensor_tensor(sm[:], s3, thr[:].unsqueeze(2).broadcast_to([128, 8, 4]), ALU.is_ge), reads=[R1], writes=[R1])
            f.op("dve", lambda e: e.tensor_tensor(sm[:], sm[:], mg[:].unsqueeze(2).broadcast_to([128, 8, 4]), ALU.mult), reads=[R1], writes=[R1])
            cj = comb[:, j, :]
            f.op("dve", lambda e: e.tensor_tensor(cj, sm[:].rearrange("p g e -> p (g e)"), sc[:], ALU.mult), reads=[R1], writes=[R_comb[j]])
            f.op("dve", lambda e: e.tensor_reduce(out=gm[:, 1:2], in_=cj, axis=AX.X, op=ALU.add), reads=[R_comb[j], R1], writes=[R1])
            f.op("dve", lambda e: e.reciprocal(gm[:, 1:2], gm[:, 1:2]), reads=[R1], writes=[R1])
            f.op("dve", lambda e: e.tensor_scalar(out=cj, in0=cj, scalar1=gm[:, 1:2], scalar2=None, op0=ALU.mult), reads=[R1, R_comb[j]], writes=[R_comb[j]])
        A.close()
        Bq = Scope(nc)
        m_ = Bq.sb("m_", [128, ng, 32]); mb16 = Bq.sb("mb16", [128, ng, 32], BF16)
        rank = Bq.sb("rank", [128, ng, 32]); tot = Bq.sb("tot", [128, ng, 32]); base = Bq.sb("base", [128, ng, 32])
        me = Bq.sb("me", [128, ng, 32]); Bm = Bq.sb("Bm", [128, ng, 32]); Am = Bq.sb("Am", [128, ng, 32]); tmpq = Bq.sb("tmpq", [128, ng, 32])
        ones16 = Bq.sb("ones16", [128, 128], BF16)
        cnt = Bq.sb("cnt", [128, 32]); cmp17 = Bq.sb("cmp17", [128, 32, 18]); thr18 = Bq.sb("thr18", [128, 18])
        tlf = Bq.sb("tlf", [128, 32]); sinc = Bq.sb("sinc", [128, 32]); so512 = Bq.sb("so512", [128, 32]); c1e = Bq.sb("c1e", [128, 32])
        mx = Bq.sb("mx", [128, ng]); pAf = Bq.sb("pAf", [128, ng]); pBf = Bq.sb("pBf", [128, ng])
        cmpj = Bq.sb("cmpj", [128, NSO, 32]); eidf = Bq.sb("eidf", [128, NSO]); idxf = Bq.sb("idxf", [128, NSO, 4])
        ps_rk = Bq.ps("ps_rk", [128, 3, 512]); ps_tt = Bq.ps("ps_tt", [128, 3, 512])
        RB = [R_rt]

        def fl(ap3):
            return ap3.rearrange("p a b -> p (a b)")
        f.op("dve", lambda e: e.tensor_scalar(out=fl(m_[:]), in0=fl(comb[:]), scalar1=0.0, scalar2=None, op0=ALU.is_gt), reads=R_comb, writes=RB)
        f.op("dve", lambda e: e.tensor_copy(fl(mb16[:]), fl(m_[:])), reads=RB, writes=RB)
        f.op("pool", lambda e: e.memset(ones16[:], 1.0), reads=RB, writes=RB)
        chunks = [(n0, min(M, n0 + 512)) for n0 in range(0, M, 512)]
        for ch, (n0, n1) in enumerate(chunks):
            f.op("pe", lambda e, ch=ch, n0=n0, n1=n1: e.matmul(ps_rk[:, ch, 0:n1 - n0], maskb[:, 2, :], fl(mb16[:])[:, n0:n1], start=True, stop=True), reads=RB + [R_mask], writes=RB)
            f.op("pe", lambda e, ch=ch, n0=n0, n1=n1: e.matmul(ps_tt[:, ch, 0:n1 - n0], ones16[:], fl(mb16[:])[:, n0:n1], start=True, stop=True), reads=RB, writes=RB)
        for ch, (n0, n1) in enumerate(chunks):
            f.op("dve", lambda e, ch=ch, n0=n0, n1=n1: e.tensor_copy(fl(rank[:])[:, n0:n1], ps_rk[:, ch, 0:n1 - n0]), reads=RB, writes=RB)
            f.op("dve", lambda e, ch=ch, n0=n0, n1=n1: e.tensor_copy(fl(tot[:])[:, n0:n1], ps_tt[:, ch, 0:n1 - n0]), reads=RB, writes=RB)
        f.op("dve", lambda e: e.memset(base[:, 0, :], 0.0), reads=RB, writes=RB)
        for t_ in range(1, ng):
            f.op("dve", lambda e, t_=t_: e.tensor_tensor(base[:, t_, :], base[:, t_ - 1, :], tot[:, t_ - 1, :], ALU.add), reads=RB, writes=RB)
        f.op("dve", lambda e: e.tensor_tensor(cnt[:], base[:, ng - 1, :], tot[:, ng - 1, :], ALU.add), reads=RB, writes=RB)
        f.op("dve", lambda e: e.tensor_scalar(out=thr18[:], in0=jt[:, 0:18], scalar1=512.0, scalar2=None, op0=ALU.mult), reads=RB + [R_rw], writes=RB)
        f.op("dve", lambda e: e.tensor_tensor(cmp17[:], cnt[:].unsqueeze(2).broadcast_to([128, 32, 18]), thr18[:].unsqueeze(1).broadcast_to([128, 32, 18]), ALU.is_gt), reads=RB, writes=RB)
        f.op("dve", lambda e: e.tensor_reduce(out=tlf[:], in_=cmp17[:], axis=AX.X, op=ALU.add), reads=RB, writes=RB)
        f.op("dve", lambda e: e.tensor_scalar(out=tlf[:], in0=tlf[:], scalar1=-1.0, scalar2=0.0, op0=ALU.add, op1=ALU.max), reads=RB, writes=RB)
        f.op("dve", lambda e: e.tensor_copy(sinc[:], tlf[:]), reads=RB, writes=RB)
        for e_ in range(1, 32):
            f.op("dve", lambda e, e_=e_: e.tensor_tensor(sinc[:, e_:e_ + 1], sinc[:, e_ - 1:e_], tlf[:, e_:e_ + 1], ALU.add), reads=RB, writes=RB)
        f.op("dve", lambda e: e.tensor_tensor(so512[:], sinc[:], tlf[:], ALU.subtract), reads=RB, writes=RB)
        f.op("dve", lambda e: e.tensor_scalar(out=so512[:], in0=so512[:], scalar1=512.0, scalar2=15872.0, op0=ALU.mult, op1=ALU.add), reads=RB, writes=RB)
        f.op("dve", lambda e: e.tensor_scalar(out=c1e[:], in0=jt[:, 0:32], scalar1=512.0, scalar2=None, op0=ALU.mult), reads=RB + [R_rw], writes=RB)
        f.op("dve", lambda e: e.tensor_tensor(so512[:], so512[:], c1e[:], ALU.subtract), reads=RB, writes=RB)
        f.op("dve", lambda e: e.tensor_tensor(fl(rank[:]), fl(rank[:]), fl(base[:]), ALU.add), reads=RB, writes=RB)
        f.op("dve", lambda e: e.tensor_scalar(out=fl(tmpq[:]), in0=fl(rank[:]), scalar1=512.0, scalar2=None, op0=ALU.is_ge), reads=RB, writes=RB)
        f.op("dve", lambda e: e.tensor_tensor(tmpq[:], tmpq[:], so512[:].unsqueeze(1).broadcast_to([128, ng, 32]), ALU.mult), reads=RB, writes=RB)
        f.op("dve", lambda e: e.tensor_tensor(rank[:], rank[:], c1e[:].unsqueeze(1).broadcast_to([128, ng, 32]), ALU.add), reads=RB, writes=RB)
        f.op("dve", lambda e: e.tensor_tensor(fl(rank[:]), fl(rank[:]), fl(tmpq[:]), ALU.add), reads=RB, writes=RB)
        f.op("dve", lambda e: e.tensor_tensor(me[:], m_[:], jt[:, 1:33].unsqueeze(1).broadcast_to([128, ng, 32]), ALU.mult), reads=RB, writes=RB)
        f.op("dve", lambda e: e.tensor_reduce(out=mx[:], in_=me[:], axis=AX.X, op=ALU.max), reads=RB, writes=RB)
        f.op("dve", lambda e: e.tensor_tensor(Bm[:], me[:], mx[:].unsqueeze(2).broadcast_to([128, ng, 32]), ALU.is_equal), reads=RB, writes=RB)
        f.op("dve", lambda e: e.tensor_tensor(fl(Am[:]), fl(m_[:]), fl(Bm[:]), ALU.subtract), reads=RB, writes=RB)
        for (msk, pf, wf) in ((Am, pAf, wA), (Bm, pBf, wB)):
            f.op("dve", lambda e, msk=msk: e.tensor_tensor(fl(tmpq[:]), fl(msk[:]), fl(rank[:]), ALU.mult), reads=RB, writes=RB)
            f.op("dve", lambda e, pf=pf: e.tensor_reduce(out=pf[:], in_=tmpq[:], axis=AX.X, op=ALU.add), reads=RB, writes=RB)
            f.op("dve", lambda e, msk=msk: e.tensor_tensor(fl(tmpq[:]), fl(msk[:]), fl(comb[:]), ALU.mult), reads=RB + R_comb, writes=RB)
            f.op("dve", lambda e, wf=wf: e.tensor_reduce(out=wf[:], in_=tmpq[:], axis=AX.X, op=ALU.add), reads=RB, writes=RB)
        f.op("dve", lambda e: e.tensor_copy(posA_i[:], pAf[:]), reads=RB, writes=RB)
        f.op("dve", lambda e: e.tensor_copy(posB_i[:], pBf[:]), reads=RB, writes=RB)
        f.op("dve", lambda e: e.tensor_tensor(cmpj[:], sinc[:].unsqueeze(1).broadcast_to([128, NSO, 32]), jt[:, 0:NSO].unsqueeze(2).broadcast_to([128, NSO, 32]), ALU.is_le), reads=RB, writes=RB)
        f.op("dve", lambda e: e.tensor_reduce(out=eidf[:], in_=cmpj[:], axis=AX.X, op=ALU.add), reads=RB, writes=RB)
        f.op("dve", lambda e: e.tensor_scalar(out=eidf[:], in0=eidf[:], scalar1=32.0, scalar2=512.0, op0=ALU.min, op1=ALU.mult), reads=RB, writes=RB)
        f.op("dve", lambda e: e.tensor_scalar(out=eidf[:], in0=eidf[:], scalar1=float(layer * 16384), scalar2=None, op0=ALU.add), reads=RB, writes=RB)
        f.op("dve", lambda e: e.tensor_tensor(idxf[:], eidf[:].unsqueeze(2).broadcast_to([128, NSO, 4]), pc[:].unsqueeze(1).broadcast_to([128, NSO, 4]), ALU.add), reads=RB, writes=RB)
        f.op("dve", lambda e: e.tensor_copy(idxw[:], idxf[:]), reads=RB, writes=RB)
        for j in range(ng):
            for pi_ in (posA_i, posB_i):
                f._dma_common("pool", lambda e, j=j, pi_=pi_: e.indirect_dma_start(out=XS, out_offset=IOA(ap=pi_[:, j:j + 1], axis=0), in_=hb_all[:, j, :], in_offset=None),
                              [R_hb[j]] + RB + R_XsZ, [R_XsW[j]])
        Bq.close()
        HB.close()
        Sd = Scope(nc)
        wg = [Sd.sb("wg%d" % k, [128, 4, 2, 512], BF16) for k in range(2)]
        wu = [Sd.sb("wu%d" % k, [128, 4, 2, 512], BF16) for k in range(2)]
        wd = [Sd.sb("wd%d" % k, [128, 4, D], BF16) for k in range(2)]
        R_w = RL(2, "w"); R_wd = RL(2, "wd")
        xs = [[Sd.sb("xs%d_%d" % (a_, tt), [128, D], BF16) for tt in range(4)] for a_ in range(2)]
        R_xs = [RL(4, "xs%d" % a_) for a_ in range(2)]

        def xs_load(jn):
            for tt in range(4):
                r0 = (jn * 4 + tt) * 128
                f.dma("sp", xs[jn % 2][tt][:], XS[r0:r0 + 128, :], reads=R_XsW, writes=[R_xs[jn % 2][tt]])
        xT = [Sd.sb("xT%d" % k, [128, 8, 512], BF16) for k in range(2)]; R_xT = RL(2)
        ps_t = [Sd.ps("ps_t%d" % k, [128, 8, 128], BF16) for k in range(2)]; R_pst = RL(2)
        psg = [Sd.ps("psg%d" % k, [128, 512]) for k in range(2)]; R_psg = RL(2)
        psu = [Sd.ps("psu%d" % k, [128, 512]) for k in range(2)]; R_psu = RL(2)
        psd = [Sd.ps("psd%d" % k, [128, 512]) for k in range(2)]; R_psd = RL(2)
        sg = [Sd.sb("sg%d" % k, [128, 512]) for k in range(2)]; R_sg = RL(2)
        hid = [Sd.sb("hid%d" % k, [128, 4, 512], BF16) for k in range(2)]; R_hid = RL(2)
        ysb = [Sd.sb("ysb%d" % k, [128, D]) for k in range(2)]; R_ysb = [RL(2, "ysb%d" % k) for k in range(2)]
        nfc = 0; nx = 0; ny = 0
        bc_reg = nc.gpsimd.alloc_register("bc%d" % layer)
        nc.gpsimd.reg_mov(bc_reg, 16383 + layer * 16384)
        stg_g = Sd.sb("stg_g", [128, 4, 2, 512]); stg_u = Sd.sb("stg_u", [128, 4, 2, 512])
        R_sg_ = Res("stg_g"); R_su_ = Res("stg_u")

        def w_load(ex):
            f.dma("sp", stg_g[:], w_gate[layer, ex].rearrange("(c q j) n -> q c j n", c=4, j=2), writes=[R_sg_])
            f.dma("sp", stg_u[:], w_up[layer, ex].rearrange("(c q j) n -> q c j n", c=4, j=2), writes=[R_su_])
            f.dma("pool", wd[ex % 2][:], w_down[layer, ex].rearrange("(c p) n -> p c n", p=128), writes=[R_wd[ex % 2]])

        def w_cast(ex):
            wb_ = ex % 2
            f.op("act", lambda e: e.activation(out=wg[wb_][:], in_=stg_g[:], func=AF.Identity), reads=[R_sg_], writes=[R_w[wb_]])
            f.op("dve", lambda e: e.tensor_copy(wu[wb_][:], stg_u[:]), reads=[R_su_], writes=[R_w[wb_]])
        xs_load(0)
        w_load(0)
        w_cast(0)
        for j in range(NS):
            wb = j % 2
            if j + 1 < NS:
                xs_load(j + 1)
            if j + 1 < 32:
                w_load(j + 1)
            if j >= 32:
                jo = j - 32
                for c in range(4):
                    for (wt_, rows_) in ((wg, wg_rows), (wu, wu_rows)):
                        f._dma_common("pool", lambda e, c=c, wt_=wt_, rows_=rows_: e.indirect_dma_start(out=wt_[wb][:, c, :, :].rearrange("p a n -> p (a n)"), out_offset=None, in_=rows_[layer],
                                                                                                   in_offset=IOA(ap=idxw[:, jo, c:c + 1], axis=0), bounds_check=bc_reg, oob_is_err=False),
                                      RB, [R_w[wb]])
                for c in range(4):
                    f._dma_common("pool", lambda e, c=c: e.indirect_dma_start(out=wd[wb][:, c, :], out_offset=None, in_=wd_rows[layer], in_offset=IOA(ap=idxw[:, jo, c:c + 1], axis=0), bounds_check=bc_reg, oob_is_err=False),
                                  RB, [R_wd[wb]])
            for tt in range(4):
                for kc in range(8):
                    f.op("pe", lambda e, kc=kc, tt=tt: e.transpose(ps_t[tt % 2][:, kc, :], xs[j % 2][tt][:, kc * 128:(kc + 1) * 128], identb[:]), reads=[R_xs[j % 2][tt], R_identb], writes=[R_pst[tt % 2]], acc=(kc > 0))
                f.op("dve", lambda e, tt=tt: e.tensor_copy(xT[wb][:, :, tt * 128:(tt + 1) * 128], ps_t[tt % 2][:]), reads=[R_pst[tt % 2]], writes=[R_xT[wb]])
            hb_ = j % 2
            for fc in range(4):
                pb = nfc % 2; nfc += 1
                for kc in range(8):
                    f.op("pe", lambda e, kc=kc, fc=fc: e.matmul(psg[pb][:], wg[wb][:, kc // 2, kc % 2, fc * 128:(fc + 1) * 128], xT[wb][:, kc, :], start=(kc == 0), stop=(kc == 7)),
                         reads=[R_w[wb], R_xT[wb]], writes=[R_psg[pb]], acc=(kc > 0))
                for kc in range(8):
                    f.op("pe", lambda e, kc=kc, fc=fc: e.matmul(psu[pb][:], wu[wb][:, kc // 2, kc % 2, fc * 128:(fc + 1) * 128], xT[wb][:, kc, :], start=(kc == 0), stop=(kc == 7)),
                         reads=[R_w[wb], R_xT[wb]], writes=[R_psu[pb]], acc=(kc > 0))
                f.op("act", lambda e: e.activation(out=sg[pb][:], in_=psg[pb][:], func=AF.Silu), reads=[R_psg[pb]], writes=[R_sg[pb]])
                f.op("dve", lambda e, fc=fc: e.tensor_tensor(hid[hb_][:, fc, :], sg[pb][:], psu[pb][:], ALU.mult), reads=[R_sg[pb], R_psu[pb]], writes=[R_hid[hb_]])
            for tt in range(4):
                yb_ = ny % 2; ny += 1
                for half in range(2):
                    for fc in range(4):
                        f.op("pe", lambda e, fc=fc, half=half, tt=tt: e.matmul(psd[half][:], hid[hb_][:, fc, tt * 128:(tt + 1) * 128], wd[wb][:, fc, half * 512:(half + 1) * 512], start=(fc == 0), stop=(fc == 3)),
                             reads=[R_wd[wb], R_hid[hb_]], writes=[R_psd[half]], acc=(fc > 0))
                    if half == 0:
                        f.op("dve", lambda e: e.tensor_copy(ysb[yb_][:, 0:512], psd[0][:]), reads=[R_psd[0]], writes=[R_ysb[yb_][0]])
                    else:
                        f.op("act", lambda e: e.activation(out=ysb[yb_][:, 512:1024], in_=psd[1][:], func=AF.Identity), reads=[R_psd[1]], writes=[R_ysb[yb_][1]])
                r0 = (j * 4 + tt) * 128
                f.dma("act", YS[r0:r0 + 128, :], ysb[yb_][:], reads=R_ysb[yb_], writes=[R_Ys[j]])
            if j + 1 < 32:
                w_cast(j + 1)
        Sd.close()
        nc.gpsimd.free_register(bc_reg)
        C = Scope(nc)
        xt = [C.sb("xt%d" % k, [128, D]) for k in range(2)]; R_xt = RL(2)
        ot = [C.sb("ot%d" % k, [128, D]) for k in range(2)]; R_ot = RL(2)
        ya = [C.sb("ya%d" % k, [128, D]) for k in range(2)]; R_ya = RL(2)
        yb2 = [C.sb("yb%d" % k, [128, D]) for k in range(2)]; R_yb = RL(2)
        tmp = C.sb("tmp", [128, D]); R_tmp = Res()
        small = C.sb("small", [128, 16]); R_small = Res()

        class _V:
            def __init__(self, ap): self.ap = ap
            def __getitem__(self, k): return self.ap
        for j, t in enumerate(tiles_all):
            b = j % 2
            f.dma("sp", xt[b][:], XR[t * 128:(t + 1) * 128, :], reads=[R_XR[t]], writes=[R_xt[b]])
            f._dma_common("pool", lambda e: e.indirect_dma_start(out=ya[b][:], out_offset=None, in_=YS, in_offset=IOA(ap=posA_i[:, j:j + 1], axis=0)), R_Ys + RB, [R_ya[b]])
            f._dma_common("pool", lambda e: e.indirect_dma_start(out=yb2[b][:], out_offset=None, in_=YS, in_offset=IOA(ap=posB_i[:, j:j + 1], axis=0)), R_Ys + RB, [R_yb[b]])
            f.op("dve", lambda e: e.tensor_scalar(out=ya[b][:], in0=ya[b][:], scalar1=wA[:, j:j + 1], scalar2=None, op0=ALU.mult), reads=[R_ya[b]] + RB, writes=[R_ya[b]])
            f.op("dve", lambda e: e.scalar_tensor_tensor(out=ya[b][:], in0=yb2[b][:], scalar=wB[:, j:j + 1], in1=ya[b][:], op0=ALU.mult, op1=ALU.add), reads=[R_yb[b], R_ya[b]] + RB, writes=[R_ya[b]])
            resid_ln(C, xt[b], R_xt[b], _V(ya[b][:]), R_ya[b], (1 if t < 2 else 0), layer * 2 + 1, ot[b], R_ot[b], tmp, R_tmp, small, R_small)
            if final:
                f.dma("act", out_d[(t - 2) * 128:(t - 1) * 128, :], ot[b][:], reads=[R_ot[b]], writes=[R_out])
            else:
                f.dma("act", XR[t * 128:(t + 1) * 128, :], ot[b][:], reads=[R_ot[b]], writes=[R_XR[t]])
        C.close()
        P.close()

    def layer1_mixer():
        L = Scope(nc)
        kT2 = L.sb("kT2b", [128, 4, NTOK], BF16); R_kT = RL(NT, "kT")
        vaug = L.sb("vaugb", [128, NT, 576], BF16); R_v = RL(NT, "v")
        f.op("pool", lambda e: e.memset(vaug[:], 1.0), writes=R_v)
        R_oT = RL(NT, "oT")
        R_QT = RL(NT, "QT")
        S = Scope(nc)
        win = S.sb("win1", [128, 8, 1536], BF16); R_win = Res()
        f.dma("pool", win[:], odd_w_in.rearrange("(kc p) n -> p kc n", p=128), writes=[R_win])
        gq = S.sb("gq", [128, 2, 64]); R_gq = Res()
        f.dma("sp", gq[:, 0, :], q_norm.partition_broadcast(128), writes=[R_gq])
        f.dma("sp", gq[:, 1, :], k_norm.partition_broadcast(128), writes=[R_gq])
        xt = [S.sb("xt%d" % k, [128, D]) for k in range(2)]; R_xt = RL(2)
        h32 = S.sb("h32", [128, D]); R_h32 = Res()
        hT = [S.sb("hT%d" % k, [128, 8, 128], BF16) for k in range(2)]; R_hT = RL(2)
        ps_tp = S.ps("ps_tp", [128, 8, 128]); R_pstp = Res(x=True)
        ps_q = S.ps("ps_q", [128, 1536]); R_psq = Res(x=True)
        ps_t = S.ps("ps_t", [128, 16, 128], BF16); R_pst = Res(x=True)
        qk = S.sb("qk", [128, 20, 64]); R_qk = Res()
        sq = S.sb("sq", [128, 20, 64]); ss = S.sb("ss", [128, 20]); R_ss = Res()
        ra = S.sb("ra", [128, 20, 32]); rb = S.sb("rb", [128, 20, 32]); R_ra = Res(); R_rb = Res()
        tqk = S.sb("tqk", [128, 20, 64], BF16); R_tqk = Res()
        kd = S.sb("kd", [128, 4, 2, 64], BF16); R_kd = Res()
        qts = [S.sb("qts%d" % k, [128, 8, 128], BF16) for k in range(2)]; R_qts = RL(2)
        for t in range(NT):
            b = t % 2
            f.dma("sp", xt[b][:], XR[t * 128:(t + 1) * 128, :], reads=[R_XR[t]], writes=[R_xt[b]])
            which = 1 if t < 2 else 0
            mod_transpose(xt[b], R_xt[b], which, h32, R_h32, ps_tp, R_pstp, hT[b][:], R_hT[b])
            cols = slice(t * 128, (t + 1) * 128)
            lat = t >= 2
            ranges = ((0, 512), (512, 1024), (1024, 1536)) if lat else ((1024, 1536),)
            first = True
            for (n0, n1) in ranges:
                for kc in range(8):
                    f.op("pe", lambda e, kc=kc, n0=n0, n1=n1: e.matmul(ps_q[:, n0:n1], hT[b][:, kc, :], win[:, kc, n0:n1], start=(kc == 0), stop=(kc == 7)),
                         reads=[R_win, R_hT[b]], writes=[R_psq], acc=(not first))
                    first = False
            h0 = 0 if lat else 16
            nh = 20 - h0
            pv = ps_q[:, h0 * 64:1280].rearrange("p (h d) -> p h d", d=64)
            qkv_ = qk[:, h0:20, :]
            f.op("act", lambda e: e.activation(out=sq[:, h0:20, :], in_=pv, func=AF.Square), reads=[R_psq], writes=[R_ss])
            f.op("dve", lambda e: e.tensor_reduce(out=ss[:, h0:20], in_=sq[:, h0:20, :], axis=AX.X, op=ALU.add), reads=[R_ss], writes=[R_ss])
            f.op("dve", lambda e: e.tensor_scalar(out=ss[:, h0:20], in0=ss[:, h0:20], scalar1=1.0 / 64.0, scalar2=RMS_EPS, op0=ALU.mult, op1=ALU.add), reads=[R_ss], writes=[R_ss])
            f.op("act", lambda e: e.activation(out=ss[:, h0:20], in_=ss[:, h0:20], func=AF.Sqrt), reads=[R_ss], writes=[R_ss])
            f.op("dve", lambda e: e.reciprocal(ss[:, h0:20], ss[:, h0:20]), reads=[R_ss], writes=[R_ss])
            f.op("dve", lambda e: e.tensor_tensor(qkv_, pv, ss[:, h0:20].unsqueeze(2).broadcast_to([128, nh, 64]), ALU.mult), reads=[R_psq, R_ss], writes=[R_qk])
            if lat:
                f.op("pool", lambda e: e.tensor_tensor(qk[:, 0:16, :], qk[:, 0:16, :], gq[:, 0, :].unsqueeze(1).broadcast_to([128, 16, 64]), ALU.mult), reads=[R_qk, R_gq], writes=[R_qk])
            f.op("pool", lambda e: e.tensor_tensor(qk[:, 16:20, :], qk[:, 16:20, :], gq[:, 1, :].unsqueeze(1).broadcast_to([128, 4, 64]), ALU.mult), reads=[R_qk, R_gq], writes=[R_qk])
            if lat:
                q4 = qk[:].rearrange("p h (two f) -> p h two f", two=2)
                o4 = tqk[:].rearrange("p h (two f) -> p h two f", two=2)
                cosb = rope[:, 0, t - 2, :].unsqueeze(1).broadcast_to([128, 20, 32])
                sinb = rope[:, 1, t - 2, :].unsqueeze(1).broadcast_to([128, 20, 32])
                f.op("dve", lambda e: e.tensor_tensor(ra[:], q4[:, :, 0, :], cosb, ALU.mult), reads=[R_qk, R_rope], writes=[R_ra])
                f.op("pool", lambda e: e.tensor_tensor(rb[:], q4[:, :, 1, :], sinb, ALU.mult), reads=[R_qk, R_rope], writes=[R_rb])
                f.op("dve", lambda e: e.tensor_tensor(o4[:, :, 0, :], ra[:], rb[:], ALU.subtract), reads=[R_ra, R_rb], writes=[R_tqk])
                f.op("dve", lambda e: e.tensor_tensor(ra[:], q4[:, :, 1, :], cosb, ALU.mult), reads=[R_qk, R_rope, R_tqk], writes=[R_ra])
                f.op("pool", lambda e: e.tensor_tensor(rb[:], q4[:, :, 0, :], sinb, ALU.mult), reads=[R_qk, R_rope, R_tqk], writes=[R_rb])
                f.op("dve", lambda e: e.tensor_tensor(o4[:, :, 1, :], ra[:], rb[:], ALU.add), reads=[R_ra, R_rb], writes=[R_tqk])
            else:
                f.op("dve", lambda e: e.tensor_copy(tqk[:, 16:20, :], qk[:, 16:20, :]), reads=[R_qk], writes=[R_tqk])
            for a in range(4):
                f.op("dve", lambda e, a=a: e.tensor_copy(vaug[:, t, 64 + 128 * a:128 + 128 * a], ps_q[:, 1280 + 64 * a:1344 + 64 * a]), reads=[R_psq], writes=[R_v[t]])
            f.op("dve", lambda e: e.tensor_copy(kd[:, :, 0, :], tqk[:, 16:20, :]), reads=[R_tqk], writes=[R_kd])
            f.op("dve", lambda e: e.tensor_copy(kd[:, :, 1, :], tqk[:, 16:20, :]), reads=[R_tqk], writes=[R_kd])
            firstt = True
            if lat:
                for pr in range(8):
                    f.op("pe", lambda e, pr=pr: e.transpose(ps_t[:, pr, :], tqk[:, 2 * pr:2 * pr + 2, :].rearrange("p a d -> p (a d)"), identb[:]),
                         reads=[R_tqk, R_identb], writes=[R_pst], acc=(not firstt))
                    firstt = False
            for a in range(4):
                f.op("pe", lambda e, a=a: e.transpose(ps_t[:, 8 + a, :], kd[:, a, :, :].rearrange("p a d -> p (a d)"), identb[:]),
                     reads=[R_kd, R_identb], writes=[R_pst], acc=(not firstt))
                firstt = False
            f.op("act", lambda e: e.activation(out=kT2[:, :, cols], in_=ps_t[:, 8:12, :], func=AF.Identity), reads=[R_pst], writes=[R_kT[t]])
            if lat:
                f.op("dve", lambda e: e.tensor_copy(qts[b][:], ps_t[:, 0:8, :]), reads=[R_pst], writes=[R_qts[b]])
                f.dma("act", QT[:, :, (t - 2) * 128:(t - 1) * 128].rearrange("a p n -> p a n"), qts[b][:], reads=[R_qts[b]], writes=[R_QT[t]])
        S.close()
        S = Scope(nc)
        qb = [S.sb("qb%d" % k, [128, 2, 512], BF16) for k in range(2)]; R_qb = RL(2)
        ps_s = [S.ps("ps_s%d" % k, [128, 1024]) for k in range(2)]; R_pss = RL(2)
        ps_o = [S.ps("ps_o%d" % k, [128, 512]) for k in range(4)]; R_pso = RL(4)
        pT = [S.sb("pT%d" % k, [128, 1024], BF16) for k in range(3)]; R_pT = RL(3)
        dtmp = S.sb("dtmp", [128, 512]); R_dt = Res()
        ost = [S.sb("ost%d" % k, [128, 2, 512], BF16) for k in range(2)]; R_ost = RL(2)
        it = 0
        nq = 0
        for kvh in range(4):
            for qblk in range(8):
                qbi = nq % 2; nq += 1
                tq = [R_QT[2 + qblk * 4 + k] for k in range(4)]
                f.dma("sp", qb[qbi][:], QT[2 * kvh:2 * kvh + 2, :, qblk * 512:(qblk + 1) * 512].rearrange("a p n -> p a n"), reads=tq, writes=[R_qb[qbi]])
                items = [(kt, p_) for kt in range(NT) for p_ in range(2)]
                it0 = it; it += len(items)

                def front(idx):
                    kt, p_ = items[idx]
                    si = (it0 + idx) % 2
                    pi_ = (it0 + idx) % 3
                    for half in range(2):
                        base = 64 * half
                        f.op("pe", lambda e, half=half, base=base: e.matmul(ps_s[si][:, half * 512:(half + 1) * 512], kT2[base:base + 64, kvh, kt * 128:(kt + 1) * 128], qb[qbi][base:base + 64, p_, :], start=True, stop=True),
                             reads=[R_kT[kt], R_qb[qbi]], writes=[R_pss[si]], acc=(half > 0))
                    f.op("act", lambda e: e.activation(out=pT[pi_][:], in_=ps_s[si][:], func=AF.Exp, scale=0.125), reads=[R_pss[si]], writes=[R_pT[pi_]])

                def back(idx):
                    kt, p_ = items[idx]
                    pi_ = (it0 + idx) % 3
                    for half in range(2):
                        hh = 2 * p_ + half
                        voff = (64 if half == 0 else 0) + 128 * kvh
                        f.op("pe", lambda e, half=half, hh=hh, voff=voff: e.matmul(ps_o[hh][:], vaug[:, kt, voff:voff + 128], pT[pi_][:, half * 512:(half + 1) * 512], start=(kt == 0), stop=(kt == NT - 1)),
                             reads=[R_v[kt], R_pT[pi_]], writes=[R_pso[hh]], acc=(kt > 0))
                LA = 1
                for i_ in range(len(items) + LA):
                    if i_ < len(items):
                        front(i_)
                    if i_ >= LA:
                        back(i_ - LA)
                for hh in range(4):
                    h = kvh * 4 + hh
                    nb, db = (0, 64) if h % 2 == 0 else (64, 0)
                    f.op("dve", lambda e: e.reciprocal(dtmp[nb:nb + 64, :], ps_o[hh][db:db + 64, :]), reads=[R_pso[hh]], writes=[R_dt])
                    f.op("dve", lambda e: e.tensor_tensor(ost[qbi][nb:nb + 64, hh // 2, :], ps_o[hh][nb:nb + 64, :], dtmp[nb:nb + 64, :], ALU.mult),
                         reads=[R_pso[hh], R_dt], writes=[R_ost[qbi]])
                f.dma("act", OT[2 * kvh:2 * kvh + 2, :, qblk * 512:(qblk + 1) * 512].rearrange("a p n -> p a n"), ost[qbi][:], reads=[R_ost[qbi]],
                      writes=[R_oT[2 + qblk * 4 + k] for k in range(4)])
        S.close()
        L.close()
        M = Scope(nc)
        mixt = [M.sb("mixt%d" % k, [128, 8, 128], BF16) for k in range(2)]; R_mixt = RL(2)

        def mix_loader(t, b):
            f.dma("sp", mixt[b][:], OT[:, :, (t - 2) * 128:(t - 1) * 128].rearrange("a p n -> p a n"), reads=[R_oT[t]], writes=[R_mixt[b]])
            return [mixt[b][:, k, :] for k in range(8)], [R_mixt[b]]
        out_phase(1, odd_w_out, mix_loader, range(2, NT))
        M.close()

    phase_mod(0, 0)
    if stop_after == "mod":
        f.dma("sp", dbg[0:128, :], mod[:, 0].rearrange("p a d -> p (a d)"), reads=[R_mod], writes=[R_dbg])
        f.dma("sp", dbg[128:256, :], mod[:, 1].rearrange("p a d -> p (a d)"), reads=[R_mod], writes=[R_dbg])
    else:
        layer0_mixer()
    if stop_after in ("in0", "s5", "mod", "h0", "qkv", "win"):
        pass
    else:
        if stop_after == "mix0":
            pass
        else:
            phase_mod(0, 1)
            (ffn_phase if 'dense' in DBG_SKIP else ffn_sparse)(0, list(range(NT)), final=False)
            if stop_after != "l0":
                phase_mod(1, 0)
                layer1_mixer()
                if stop_after != "mix1":
                    phase_mod(1, 1)
                    (ffn_phase if 'dense' in DBG_SKIP else ffn_sparse)(1, list(range(2, NT)), final=True)
    if stop_after is not None and stop_after not in ("in0", "s5", "mod", "h0", "qkv", "win"):
        S = Scope(nc)
        tt = S.sb("dumpt", [128, D]); R_t = Res()
        for t in range(NT):
            f.dma("sp", tt[:], XR[t * 128:(t + 1) * 128, :], reads=[R_XR[t]], writes=[R_t])
            f.dma("sp", dbg[t * 128:(t + 1) * 128, :], tt[:], reads=[R_t], writes=[R_dbg])
        S.close()
    f.finish()
    Scope.FWREF = None
    G.close()
    f.close()
    return nc


_CONST = None


def _consts():
    global _CONST
    if _CONST is None:
        ident = np.eye(128, dtype=np.float32)
        n_freq = 16
        inv_freq = (10000.0 ** (-np.arange(n_freq, dtype=np.float32) / n_freq)).astype(np.float32)
        pos = np.arange(4096)
        r = (pos // 64).astype(np.float32); cc = (pos % 64).astype(np.float32)
        ang = np.concatenate([r[:, None] * inv_freq, cc[:, None] * inv_freq], -1).astype(np.float32)
        cos = np.cos(ang).astype(np.float32).reshape(32, 128, 32).transpose(1, 0, 2)
        sin = np.sin(ang).astype(np.float32).reshape(32, 128, 32).transpose(1, 0, 2)
        rope = np.ascontiguousarray(np.stack([cos, sin], axis=1))
        k = np.arange(128)[:, None]; q = np.arange(128)[None, :]
        mask = np.stack([(q <= k), (k <= q), (k < q)], axis=1).astype(np.float32)
        pc = (np.arange(128, dtype=np.float32)[:, None] + 128.0 * np.arange(4, dtype=np.float32)[None, :]).astype(np.float32)
        jidx = np.broadcast_to(np.arange(128, dtype=np.float32)[None, :], (128, 128)).copy()
        _CONST = {"k_ident": ident, "k_rope": rope, "k_mask": np.ascontiguousarray(mask), "k_jidx": jidx, "k_pc": np.ascontiguousarray(pc)}
    return _CONST


def make_in_map(inputs, b):
    f32 = lambda a: np.ascontiguousarray(np.asarray(a, dtype=np.float32))
    m = {
        "x": f32(inputs["x"][b]), "ctx": f32(inputs["ctx"][b]), "c": f32(inputs["c"][b:b + 1]),
        "c_ctx": f32(inputs["c_ctx"]).reshape(1, D),
        "ada_w": f32(inputs["ada_w"]), "ada_b": f32(inputs["ada_b"]), "ln_g": f32(inputs["ln_g"]), "ln_b": f32(inputs["ln_b"]),
        "even_w_in": f32(inputs["even_w_in"][0]), "even_w_out": f32(inputs["even_w_out"][0]),
        "s5_lam_re": f32(inputs["s5_lam_re"][0]), "s5_lam_im": f32(inputs["s5_lam_im"][0]), "s5_log_step": f32(inputs["s5_log_step"][0]),
        "s5_b_re": f32(inputs["s5_b_re"][0]), "s5_b_im": f32(inputs["s5_b_im"][0]),
        "s5_c_re": f32(inputs["s5_c_re"][0]), "s5_c_im": f32(inputs["s5_c_im"][0]),
        "s5_d": f32(inputs["s5_d"][0]), "s5_w_glu": f32(inputs["s5_w_glu"][0]), "s5_b_glu": f32(inputs["s5_b_glu"][0]),
        "win_sink": f32(inputs["win_sink"][0]),
        "odd_w_in": f32(inputs["odd_w_in"][0]), "odd_w_out": f32(inputs["odd_w_out"][0]),
        "odd_q_norm": f32(inputs["odd_q_norm"][0]), "odd_k_norm": f32(inputs["odd_k_norm"][0]),
        "router_w": f32(inputs["router_w"]), "router_bias": f32(inputs["router_bias"]),
        "moe_w_gate": f32(inputs["moe_w_gate"]), "moe_w_up": f32(inputs["moe_w_up"]), "moe_w_down": f32(inputs["moe_w_down"]),
    }
    m.update(_consts())
    return m


def kernel(**inputs):
    nc = build_program()
    shared = make_in_map(inputs, 0)
    in_maps = []
    for b in range(8):
        m = dict(shared)
        m["x"] = np.ascontiguousarray(np.asarray(inputs["x"][b], dtype=np.float32))
        m["ctx"] = np.ascontiguousarray(np.asarray(inputs["ctx"][b], dtype=np.float32))
        m["c"] = np.ascontiguousarray(np.asarray(inputs["c"][b:b + 1], dtype=np.float32))
        in_maps.append(m)
    res = run_bass_kernel_spmd(nc, in_maps, core_ids=list(range(8)))
    return np.stack([np.asarray(r["out"], dtype=np.float32) for r in res.results], axis=0)
```

```python
import math
import os
DBG_SKIP = os.environ.get('DBG_SKIP', '').split(',')
DBG_NT = int(os.environ.get('DBG_NT', '34'))
from contextlib import ExitStack
import numpy as np
import ml_dtypes
import concourse.bass as bass
import concourse.mybir as mybir
from concourse.bass_utils import run_bass_kernel_spmd

F32 = mybir.dt.float32
BF16 = mybir.dt.bfloat16
I32 = mybir.dt.int32
ALU = mybir.AluOpType
AF = mybir.ActivationFunctionType
AX = mybir.AxisListType

SEM_LIMIT = 30000
NT = 34
NTOK = 4352
D = 1024
ALPHA = 4.0 ** 0.25
LN_EPS = 1e-5
RMS_EPS = 1e-6
TWO_PI = 2.0 * math.pi
CW1 = 6.28125
CW2 = TWO_PI - CW1


class Res:
    __slots__ = ("name", "w", "r", "x")

    def __init__(self, name="", x=False):
        self.name = name
        self.w = None
        self.r = []
        self.x = x


def RL(n, name="r"):
    return [Res("%s%d" % (name, i)) for i in range(n)]


class EngState:
    def __init__(self, fw, name, eng):
        self.fw = fw
        self.name = name
        self.eng = eng
        self.count = 0
        self.epoch = 0
        self.known = {}
        self._new_sem()

    def _new_sem(self):
        self.sem_key = "%s_e%d" % (self.name, self.epoch)
        self.sem = self.fw.new_sem(self.sem_key)
        self.count = 0
        self.epoch += 1


class FW:
    def __init__(self, nc, n_dma_sems=10):
        self.nc = nc
        self.es = ExitStack()
        self.sems = {}
        self.engs = {}
        for name, eng in (("pe", nc.tensor), ("act", nc.scalar), ("dve", nc.vector),
                          ("pool", nc.gpsimd), ("sp", nc.sync)):
            self.engs[name] = EngState(self, name, eng)
        self.dma_pool = {}
        for q in ("sp", "act", "pool"):
            lst = []
            for i in range(n_dma_sems):
                key = "dma_%s_%d" % (q, i)
                lst.append([key, self.new_sem(key), 0])
            self.dma_pool[q] = [lst, 0]
        self.n_instr = 0
        self.n_waits = 0

    def new_sem(self, key):
        s = self.es.enter_context(self.nc.semaphore(key))
        self.sems[key] = s
        return s

    def _wait(self, E, ev):
        if ev is None:
            return
        key, val = ev
        if E.known.get(key, 0) >= val:
            return
        E.eng.wait_ge(self.sems[key], val)
        E.known[key] = val
        self.n_waits += 1

    def _deps(self, E, reads, writes, acc=False):
        for r in reads:
            self._wait(E, r.w)
            if r.x:
                for ev in r.r:
                    if ev[0] != E.sem_key:
                        self._wait(E, ev)
        for w in writes:
            if not ((acc or E.name == "pe") and w.w is not None and w.w[0] == E.sem_key):
                self._wait(E, w.w)
            for ev in w.r:
                self._wait(E, ev)

    def _commit(self, ev, reads, writes):
        for r in reads:
            r.r.append(ev)
            if len(r.r) > 16:
                d = {}
                for k, v in r.r:
                    if d.get(k, 0) < v:
                        d[k] = v
                r.r = list(d.items())
        for w in writes:
            w.w = ev
            w.r = []

    def op(self, ename, fn, reads=(), writes=(), acc=False):
        E = self.engs[ename]
        if E.count >= SEM_LIMIT:
            E._new_sem()
        self._deps(E, reads, writes, acc=acc)
        ins = fn(E.eng)
        E.count += 1
        ins.then_inc(E.sem, 1)
        self._commit((E.sem_key, E.count), reads, writes)
        self.n_instr += 1
        return ins

    def _dma_common(self, qname, issue, reads, writes):
        E = self.engs[qname]
        pool, idx = self.dma_pool[qname]
        ent = pool[idx % len(pool)]
        self.dma_pool[qname][1] = idx + 1
        key, sem, val = ent
        if val > 0:
            self._wait(E, (key, val))
        if val + 16 > SEM_LIMIT:
            key = key + "n"
            sem = self.new_sem(key)
            val = 0
            ent[0], ent[1] = key, sem
        self._deps(E, reads, writes)
        ins = issue(E.eng)
        val += 16
        ent[2] = val
        ins.then_inc(sem, 16)
        ev = (key, val)
        self._commit(ev, reads, writes)
        self.n_instr += 1
        return ev

    def dma(self, qname, out, in_, reads=(), writes=(), **kw):
        return self._dma_common(qname, lambda e: e.dma_start(out=out, in_=in_, **kw), reads, writes)

    def barrier(self):
        evs = []
        for q in self.dma_pool:
            for key, sem, val in self.dma_pool[q][0]:
                if val > 0:
                    evs.append((key, val))
        for n, e in self.engs.items():
            if e.count > 0:
                evs.append((e.sem_key, e.count))
        for n, E in self.engs.items():
            for ev in evs:
                if ev[0] != E.sem_key:
                    self._wait(E, ev)

    def finish(self):
        E = self.engs["sp"]
        for q in self.dma_pool:
            for key, sem, val in self.dma_pool[q][0]:
                if val > 0:
                    self._wait(E, (key, val))
        for n, e in self.engs.items():
            if e.count > 0:
                self._wait(E, (e.sem_key, e.count))

    def close(self):
        self.es.close()


class Scope:
    FWREF = None

    def __init__(self, nc):
        self.nc = nc
        self.es = ExitStack()

    CNT = [0]

    def sb(self, name, shape, dtype=F32):
        Scope.CNT[0] += 1
        return self.es.enter_context(self.nc.sbuf_tensor("%s_%d" % (name, Scope.CNT[0]), list(shape), dtype))

    def ps(self, name, shape, dtype=F32):
        Scope.CNT[0] += 1
        return self.es.enter_context(self.nc.psum_tensor("%s_%d" % (name, Scope.CNT[0]), list(shape), dtype))

    def close(self):
        if Scope.FWREF is not None:
            Scope.FWREF.barrier()
        self.es.close()


def rev_ap(ap2d, n):
    last = ap2d[:, n - 1:n]
    return bass.AP(tensor=ap2d.tensor, offset=last.offset, ap=[list(ap2d.ap[0]), [-1, n]])


def build_program(stop_after=None, dbg_shape=None):
    nc = bass.Bass("TRN2", target_bir_lowering=False)

    def din(name, shape, dt=F32):
        return nc.dram_tensor(name, list(shape), dt, kind="ExternalInput").ap()

    x_d = din("x", [4096, D]); ctx_d = din("ctx", [256, D])
    c_d = din("c", [1, D]); cctx_d = din("c_ctx", [1, D])
    ada_w = din("ada_w", [2, D, 6 * D]); ada_b = din("ada_b", [2, 6 * D])
    ln_g = din("ln_g", [2, 2, D]); ln_b = din("ln_b", [2, 2, D])
    even_w_in = din("even_w_in", [D, 1280]); even_w_out = din("even_w_out", [D, D])
    lam_re = din("s5_lam_re", [2, 32, 64]); lam_im = din("s5_lam_im", [2, 32, 64])
    log_step = din("s5_log_step", [2, 32])
    b_re = din("s5_b_re", [2, 32, 64, 16]); b_im = din("s5_b_im", [2, 32, 64, 16])
    c_re = din("s5_c_re", [2, 32, 16, 64]); c_im = din("s5_c_im", [2, 32, 16, 64])
    s5_d = din("s5_d", [512]); w_glu = din("s5_w_glu", [512, 512]); b_glu = din("s5_b_glu", [512])
    win_sink = din("win_sink", [8])
    odd_w_in = din("odd_w_in", [D, 1536]); odd_w_out = din("odd_w_out", [D, D])
    q_norm = din("odd_q_norm", [64]); k_norm = din("odd_k_norm", [64])
    router_w = din("router_w", [D, 32]); router_b = din("router_bias", [32])
    w_gate = din("moe_w_gate", [2, 32, D, 512]); w_up = din("moe_w_up", [2, 32, D, 512])
    w_down = din("moe_w_down", [2, 32, 512, D])
    k_ident = din("k_ident", [128, 128]); k_rope = din("k_rope", [128, 2, 32, 32])
    k_mask = din("k_mask", [128, 3, 128]); k_jidx = din("k_jidx", [128, 128]); k_pc = din("k_pc", [128, 4])
    out_d = nc.dram_tensor("out", [4096, D], F32, kind="ExternalOutput").ap()
    XR = nc.dram_tensor("xr", [NTOK, D], F32, kind="Internal").ap()
    QT = nc.dram_tensor("qt_scr", [8, 128, 4096], BF16, kind="Internal").ap()
    OT = nc.dram_tensor("ot_scr", [8, 128, 4096], BF16, kind="Internal").ap()
    ATD = nc.dram_tensor("at_scr", [4, 128, NTOK], BF16, kind="Internal").ap()
    NS = 49
    XS = nc.dram_tensor("xs_scr", [NS * 512, D], BF16, kind="Internal").ap()
    YS = nc.dram_tensor("ys_scr", [NS * 512, D], F32, kind="Internal").ap()
    wg_all = w_gate.rearrange("l e (kk two) n -> (l e kk) (two n)", two=2)
    wu_all = w_up.rearrange("l e (kk two) n -> (l e kk) (two n)", two=2)
    wd_all = w_down.rearrange("l e f n -> (l e f) n")
    wg_rows = [wg_all, wg_all]; wu_rows = [wu_all, wu_all]; wd_rows = [wd_all, wd_all]
    dbg = None
    if dbg_shape is not None:
        dbg = nc.dram_tensor("dbg", list(dbg_shape), F32, kind="ExternalOutput").ap()

    f = FW(nc)
    Scope.FWREF = f
    G = Scope(nc)
    R_XR = RL(NT, "xr")
    R_out = Res("out")
    R_dbg = Res("dbg")

    ident = G.sb("ident", [128, 128]); R_ident = Res()
    identb = G.sb("identb", [128, 128], BF16); R_identb = Res()
    f.dma("sp", ident[:], k_ident, writes=[R_ident])
    f.op("dve", lambda e: e.tensor_copy(identb[:], ident[:]), reads=[R_ident], writes=[R_identb])
    rope = G.sb("rope", [128, 2, 32, 32]); R_rope = Res()
    f.dma("sp", rope[:], k_rope, writes=[R_rope])
    maskf = G.sb("maskf", [128, 3, 128]); maskb = G.sb("maskb", [128, 3, 128], BF16); R_mask = Res()
    f.dma("sp", maskf[:], k_mask, writes=[R_mask])
    f.op("dve", lambda e: e.tensor_copy(maskb[:], maskf[:]), reads=[R_mask], writes=[R_mask])
    R_crep = Res()
    ctmp = G.sb("ctmp", [128, 2, 8]); R_ctmp = Res()
    f.dma("sp", ctmp[:, 0, :], c_d.rearrange("o (kc p) -> p (o kc)", p=128), writes=[R_ctmp], allow_slow_non_contiguous=True)
    f.dma("sp", ctmp[:, 1, :], cctx_d.rearrange("o (kc p) -> p (o kc)", p=128), writes=[R_ctmp], allow_slow_non_contiguous=True)
    f.op("act", lambda e: e.activation(out=ctmp[:], in_=ctmp[:], func=AF.Silu), reads=[R_ctmp], writes=[R_ctmp])
    lng = G.sb("lng", [128, D]); lnb = G.sb("lnb", [128, D]); R_ln = Res()

    def load_ln(li):
        f.dma("sp", lng[:], ln_g[li // 2, li % 2].partition_broadcast(128), writes=[R_ln])
        f.dma("sp", lnb[:], ln_b[li // 2, li % 2].partition_broadcast(128), writes=[R_ln])
    epsc = G.sb("epsc", [128, 1]); R_eps = Res()
    f.op("dve", lambda e: e.memset(epsc[:], LN_EPS), writes=[R_eps])

    R_XsZ = RL(28, "xsz")
    ZS = Scope(nc)
    zt = ZS.sb("zt", [128, 7, D], BF16); R_zt = Res()
    f.op("pool", lambda e: e.memset(zt[:], 0.0), writes=[R_zt])
    for k in range(28):
        f.dma(("sp", "act")[k % 2], XS[k * 896:(k + 1) * 896, :].rearrange("(a p) d -> p a d", p=128), zt[:], reads=[R_zt], writes=[R_XsZ[k]])
    ZS.close()

    mod = G.sb("mod", [128, 2, 3, D]); R_mod = Res("mod")

    def dump(ap_sb, rows, cols, reads, r0=0, c0=0):
        f.dma("sp", dbg[r0:r0 + rows, c0:c0 + cols], ap_sb, reads=reads, writes=[R_dbg])

    def phase_mod(i, s):
        S = Scope(nc)
        crep = S.sb("crep", [128, 2, 8, 128])
        f.op("dve", lambda e: e.tensor_copy(crep[:], ctmp[:].unsqueeze(3).broadcast_to([128, 2, 8, 128])), reads=[R_ctmp], writes=[R_crep])
        slab = [S.sb("slab%d" % k, [128, 8, 512]) for k in range(2)]; R_slab = RL(2)
        adb = [S.sb("adb%d" % k, [128, 512]) for k in range(2)]; R_adb = RL(2)
        psm = [S.ps("psm%d" % k, [128, 512]) for k in range(2)]; R_psm = RL(2)
        n = 0
        for blk in range(6):
            c0 = s * 3072 + blk * 512
            bi = blk % 2
            f.dma("sp", slab[bi][:], ada_w[i, :, c0:c0 + 512].rearrange("(kc p) n -> p kc n", p=128), writes=[R_slab[bi]])
            f.dma("act", adb[bi][:], ada_b[i, c0:c0 + 512].partition_broadcast(128), writes=[R_adb[bi]])
            k, half = blk // 2, blk % 2
            for which in range(2):
                pi = n % 2; n += 1
                for kc in range(8):
                    f.op("pe", lambda e, kc=kc: e.matmul(psm[pi][:], crep[:, which, kc, :], slab[bi][:, kc, :], start=(kc == 0), stop=(kc == 7)),
                         reads=[R_crep, R_slab[bi]], writes=[R_psm[pi]], acc=(kc > 0))
                dst = mod[:, which, k, half * 512:(half + 1) * 512]
                f.op("dve", lambda e: e.scalar_tensor_tensor(out=dst, in0=psm[pi][:], scalar=(1.0 if k == 1 else 0.0), in1=adb[bi][:], op0=ALU.add, op1=ALU.add),
                     reads=[R_psm[pi], R_adb[bi]], writes=[R_mod])
        S.close()

    def resid_ln(S, xt, R_xt, o_ps, R_ops, which, li, out_t, R_outt, tmp, R_tmp, small, R_small):
        gate = mod[:, which, 2, :]
        f.op("dve", lambda e: e.tensor_tensor(tmp[:], o_ps[:], gate, ALU.mult), reads=[R_ops, R_mod], writes=[R_tmp])
        f.op("dve", lambda e: e.scalar_tensor_tensor(out=tmp[:], in0=xt[:], scalar=ALPHA, in1=tmp[:], op0=ALU.mult, op1=ALU.add),
             reads=[R_xt, R_tmp], writes=[R_tmp])
        f.op("dve", lambda e: e.bn_stats(small[:, 0:6], tmp[:, 0:512]), reads=[R_tmp], writes=[R_small])
        f.op("dve", lambda e: e.bn_stats(small[:, 6:12], tmp[:, 512:1024]), reads=[R_tmp], writes=[R_small])
        f.op("dve", lambda e: e.bn_aggr(small[:, 12:14], small[:, 0:12]), reads=[R_small], writes=[R_small])
        f.op("act", lambda e: e.activation(out=small[:, 14:15], in_=small[:, 13:14], func=AF.Sqrt, bias=epsc[:], scale=1.0), reads=[R_small, R_eps], writes=[R_small])
        f.op("dve", lambda e: e.reciprocal(small[:, 15:16], small[:, 14:15]), reads=[R_small], writes=[R_small])
        f.op("dve", lambda e: e.tensor_scalar(out=tmp[:], in0=tmp[:], scalar1=small[:, 12:13], scalar2=small[:, 15:16], op0=ALU.subtract, op1=ALU.mult),
             reads=[R_tmp, R_small], writes=[R_tmp])
        f.op("dve", lambda e: e.tensor_tensor(tmp[:], tmp[:], lng[:], ALU.mult), reads=[R_tmp, R_ln], writes=[R_tmp])
        f.op("dve", lambda e: e.tensor_tensor(out_t[:], tmp[:], lnb[:], ALU.add), reads=[R_tmp, R_ln], writes=[R_outt])

    def mod_transpose(xt, R_xt, which, h32, R_h32, ps_tp, R_pstp, hT_dst, R_hT, h32T=None, R_h32T=None):
        f.op("dve", lambda e: e.tensor_tensor(h32[:], xt[:], mod[:, which, 1, :], ALU.mult), reads=[R_xt, R_mod], writes=[R_h32])
        f.op("dve", lambda e: e.tensor_tensor(h32[:], h32[:], mod[:, which, 0, :], ALU.add), reads=[R_h32, R_mod], writes=[R_h32])
        for kc in range(8):
            f.op("pe", lambda e, kc=kc: e.transpose(ps_tp[:, kc, :], h32[:, kc * 128:(kc + 1) * 128], ident[:]),
                 reads=[R_h32, R_ident], writes=[R_pstp], acc=(kc > 0))
        f.op("act", lambda e: e.activation(out=hT_dst, in_=ps_tp[:], func=AF.Identity), reads=[R_pstp], writes=[R_hT])
        if h32T is not None:
            f.op("dve", lambda e: e.tensor_copy(h32T[:], ps_tp[:]), reads=[R_pstp], writes=[R_h32T])

    def src_tile(layer, t):
        if layer == 0:
            return (ctx_d[t * 128:(t + 1) * 128, :] if t < 2 else x_d[(t - 2) * 128:(t - 1) * 128, :]), []
        return XR[t * 128:(t + 1) * 128, :], [R_XR[t]]

    def layer0_mixer():
        L = Scope(nc)
        U = Scope(nc)
        uT = U.sb("uT", [128, 4, NTOK], BF16); R_uT = RL(NT, "uT")
        aT, R_aT = uT, R_uT

        def inproj(do_u, qT=None, R_qT=None, kT2=None, R_kT=None, vaug=None, R_v=None):
            S = Scope(nc)
            wc0, wc1 = (0, 512) if do_u else (512, 1280)
            win = S.sb("win", [128, 8, wc1 - wc0], BF16); R_win = Res()
            f.dma("pool", win[:], even_w_in[:, wc0:wc1].rearrange("(kc p) n -> p kc n", p=128), writes=[R_win])
            xt1 = S.sb("xt1", [128, D]); xt = [xt1, xt1]; R1_ = Res(); R_xt = [R1_, R1_]
            h32 = S.sb("h32", [128, D]); R_h32 = Res()
            hT = [S.sb("hT%d" % k, [128, 8, 128], BF16) for k in range(2)]; R_hT = RL(2)
            ps_tp = S.ps("ps_tp", [128, 8, 128]); R_pstp = Res(x=True)
            ps_u = S.ps("ps_u", [128, 4, 128]); R_psu = Res()
            ps_q = S.ps("ps_q", [128, 1024]); R_psq = Res(x=True)
            ps_t = S.ps("ps_t", [128, 8, 128], BF16); R_pst = Res(x=True)
            ra = S.sb("ra", [128, 10, 32]); rb = S.sb("rb", [128, 10, 32]); R_ra = Res(); R_rb = Res()
            tqk = S.sb("tqk", [128, 640], BF16); R_tqk = Res()
            kd = S.sb("kd", [128, 2, 2, 64], BF16); R_kd = Res()
            for t in range(NT if do_u else min(NT, DBG_NT)):
                b = t % 2
                src, rs = src_tile(0, t)
                f.dma("sp", xt[b][:], src, reads=rs, writes=[R_xt[b]])
                which = 1 if t < 2 else 0
                mod_transpose(xt[b], R_xt[b], which, h32, R_h32, ps_tp, R_pstp, hT[b][:], R_hT[b])
                cols = slice(t * 128, (t + 1) * 128)
                if stop_after == "h0" and t == 0:
                    f.dma("sp", dbg[0:128, :], h32[:], reads=[R_h32], writes=[R_dbg])
                    hf = S.sb("hf", [128, 1024]); R_hf = Res()
                    f.op("dve", lambda e: e.tensor_copy(hf[:], hT[b][:].rearrange("p a b -> p (a b)")), reads=[R_hT[b]], writes=[R_hf])
                    f.dma("sp", dbg[128:256, :], hf[:], reads=[R_hf], writes=[R_dbg])
                    f.op("dve", lambda e: e.tensor_copy(hf[:], win[:, 0, 0:1024]), reads=[R_win], writes=[R_hf])
                    f.dma("sp", dbg[256:384, :], hf[:], reads=[R_hf], writes=[R_dbg])
                    S.close(); return
                if do_u:
                    for ct in range(4):
                        for kc in range(8):
                            f.op("pe", lambda e, ct=ct, kc=kc: e.matmul(ps_u[:, ct, :], win[:, kc, ct * 128:(ct + 1) * 128], hT[b][:, kc, :], start=(kc == 0), stop=(kc == 7)),
                                 reads=[R_win, R_hT[b]], writes=[R_psu], acc=(ct + kc > 0))
                    f.op("act", lambda e: e.activation(out=uT[:, :, cols], in_=ps_u[:], func=AF.Identity), reads=[R_psu], writes=[R_uT[t]])
                    continue
                for (n0, n1) in ((0, 512), (512, 768)):
                    for kc in range(8):
                        f.op("pe", lambda e, kc=kc, n0=n0, n1=n1: e.matmul(ps_q[:, n0:n1], hT[b][:, kc, :], win[:, kc, n0:n1], start=(kc == 0), stop=(kc == 7)),
                             reads=[R_win, R_hT[b]], writes=[R_psq], acc=(n0 + kc > 0))
                if 'rope' in DBG_SKIP:
                    continue
                if t >= 2:
                    pv = ps_q[:, 0:640].rearrange("p (h two f) -> p h two f", two=2, f=32)
                    ov = tqk[:].rearrange("p (h two f) -> p h two f", two=2, f=32)
                    cosb = rope[:, 0, t - 2, :].unsqueeze(1).broadcast_to([128, 10, 32])
                    sinb = rope[:, 1, t - 2, :].unsqueeze(1).broadcast_to([128, 10, 32])
                    f.op("dve", lambda e: e.tensor_tensor(ra[:], pv[:, :, 0, :], cosb, ALU.mult), reads=[R_psq, R_rope], writes=[R_ra])
                    f.op("dve", lambda e: e.tensor_tensor(rb[:], pv[:, :, 1, :], sinb, ALU.mult), reads=[R_psq, R_rope], writes=[R_rb])
                    f.op("pool", lambda e: e.tensor_tensor(ov[:, :, 0, :], ra[:], rb[:], ALU.subtract), reads=[R_ra, R_rb], writes=[R_tqk])
                    f.op("dve", lambda e: e.tensor_tensor(ra[:], pv[:, :, 1, :], cosb, ALU.mult), reads=[R_psq, R_rope, R_tqk], writes=[R_ra])
                    f.op("dve", lambda e: e.tensor_tensor(rb[:], pv[:, :, 0, :], sinb, ALU.mult), reads=[R_psq, R_rope, R_tqk], writes=[R_rb])
                    f.op("pool", lambda e: e.tensor_tensor(ov[:, :, 1, :], ra[:], rb[:], ALU.add), reads=[R_ra, R_rb], writes=[R_tqk])
                else:
                    f.op("act", lambda e: e.activation(out=tqk[:], in_=ps_q[:, 0:640], func=AF.Identity), reads=[R_psq], writes=[R_tqk])
                if 'vaug' in DBG_SKIP:
                    continue
                for a in range(2):
                    if 'novaug' in DBG_SKIP:
                        break
                    f.op("dve", lambda e, a=a: e.tensor_copy(vaug[:, t, 64 + 128 * a:128 + 128 * a], ps_q[:, 640 + 64 * a:704 + 64 * a]),
                         reads=[R_psq], writes=[R_v[t]])
                if 'nokd' in DBG_SKIP:
                    continue
                kv = tqk[:, 512:640].rearrange("p (a d) -> p a d", a=2)
                f.op("dve", lambda e: e.tensor_copy(kd[:, :, 0, :], kv), reads=[R_tqk], writes=[R_kd])
                f.op("dve", lambda e: e.tensor_copy(kd[:, :, 1, :], kv), reads=[R_tqk], writes=[R_kd])
                if 'tr' in DBG_SKIP:
                    continue
                for pr in range(4):
                    f.op("pe", lambda e, pr=pr: e.transpose(ps_t[:, pr, :], tqk[:, pr * 128:(pr + 1) * 128], identb[:]),
                         reads=[R_tqk, R_identb], writes=[R_pst], acc=(pr > 0))
                for a in range(2):
                    f.op("pe", lambda e, a=a: e.transpose(ps_t[:, 4 + a, :], kd[:, a, :, :].rearrange("p a d -> p (a d)"), identb[:]),
                         reads=[R_kd, R_identb], writes=[R_pst], acc=True)
                f.op("dve", lambda e: e.tensor_copy(qT[:, :, cols], ps_t[:, 0:4, :]), reads=[R_pst], writes=[R_qT[t]])
                f.op("act", lambda e: e.activation(out=kT2[:, :, cols], in_=ps_t[:, 4:6, :], func=AF.Identity), reads=[R_pst], writes=[R_kT[t]])
            S.close()

        inproj(True)
        if stop_after == "h0":
            U.close(); L.close(); return
        if stop_after == "in0":
            S = Scope(nc)
            t32 = S.sb("t32", [128, 512]); R_t = Res()
            for ct in range(4):
                for blk in range(2):
                    f.op("dve", lambda e: e.tensor_copy(t32[:], uT[:, ct, blk * 512:(blk + 1) * 512]), reads=R_uT, writes=[R_t])
                    dump(t32[:], 128, 512, [R_t], r0=ct * 128, c0=blk * 512)
            S.close(); U.close(); L.close()
            return
        if 's5' not in DBG_SKIP:
            s5_phase(L, uT, R_uT, aT, R_aT)
        if stop_after == "s5":
            S = Scope(nc)
            t32 = S.sb("t32", [128, 512]); R_t = Res()
            for ct in range(4):
                for blk in range(9):
                    c0 = blk * 512; n = min(512, NTOK - c0)
                    f.op("dve", lambda e: e.tensor_copy(t32[:, 0:n], aT[:, ct, c0:c0 + n]), reads=R_aT, writes=[R_t])
                    dump(t32[:, 0:n], 128, n, [R_t], r0=ct * 128, c0=c0)
            S.close(); U.close(); L.close()
            return
        R_ATD = Res("atd")
        for k in range(4):
            f.dma(("sp", "act")[k % 2], ATD[k], aT[:, k, :], reads=R_aT, writes=[R_ATD])
        U.close()
        oT = L.sb("oT", [128, 4, NTOK], BF16); R_oT = RL(NT, "oT")
        W = Scope(nc)
        qT = W.sb("qT", [128, 4, NTOK], BF16); R_qT = RL(NT, "qT")
        kT2 = W.sb("kT2", [128, 2, NTOK], BF16); R_kT = RL(NT, "kT")
        vaug = W.sb("vaug", [128, NT, 320], BF16); R_v = RL(NT, "v")
        f.op("pool", lambda e: e.memset(vaug[:], 1.0), writes=R_v)
        inproj(False, qT, R_qT, kT2, R_kT, vaug, R_v)
        if stop_after == "qkv":
            W.close(); L.close(); return
        win_phase(qT, R_qT, kT2, R_kT, vaug, R_v, oT, R_oT)
        W.close()
        if stop_after == "win":
            L.close(); return

        M = Scope(nc)
        mixt = [M.sb("mixa%d" % k, [128, 4, 128], BF16) for k in range(2)]; R_mixt = RL(2)

        def mix_loader(t, b):
            c0 = t * 128
            f.dma("sp", mixt[b][:], ATD[:, :, c0:c0 + 128].rearrange("a p n -> p a n"), reads=[R_ATD], writes=[R_mixt[b]])
            return [mixt[b][:, k, :] for k in range(4)] + [oT[:, k, c0:c0 + 128] for k in range(4)], [R_mixt[b], R_oT[t]]
        out_phase(0, even_w_out, mix_loader, range(NT))
        M.close()
        L.close()

    def sincos(S, ang, n, out_s, out_c, R, tag):
        ki = S.sb("ki_" + tag, [128, n], I32); kf = S.sb("kf_" + tag, [128, n]); rd = S.sb("rd_" + tag, [128, n])
        f.op("dve", lambda e: e.tensor_scalar(out=ki[:], in0=ang, scalar1=1.0 / TWO_PI, scalar2=None, op0=ALU.mult), reads=[R], writes=[R])
        f.op("dve", lambda e: e.tensor_copy(kf[:], ki[:]), reads=[R], writes=[R])
        f.op("dve", lambda e: e.scalar_tensor_tensor(out=rd[:], in0=kf[:], scalar=-CW1, in1=ang, op0=ALU.mult, op1=ALU.add), reads=[R], writes=[R])
        f.op("dve", lambda e: e.scalar_tensor_tensor(out=rd[:], in0=kf[:], scalar=-CW2, in1=rd[:], op0=ALU.mult, op1=ALU.add), reads=[R], writes=[R])
        f.op("dve", lambda e: e.tensor_scalar(out=rd[:], in0=rd[:], scalar1=3.1415925, scalar2=-3.1415925, op0=ALU.min, op1=ALU.max), reads=[R], writes=[R])
        f.op("act", lambda e: e.activation(out=out_s, in_=rd[:], func=AF.Sin), reads=[R], writes=[R])
        f.op("dve", lambda e: e.scalar_tensor_tensor(out=rd[:], in0=rd[:], scalar=-1.0, in1=rd[:], op0=ALU.mult, op1=ALU.max), reads=[R], writes=[R])
        f.op("dve", lambda e: e.tensor_scalar(out=rd[:], in0=rd[:], scalar1=-1.0, scalar2=math.pi / 2, op0=ALU.mult, op1=ALU.add), reads=[R], writes=[R])
        f.op("act", lambda e: e.activation(out=out_c, in_=rd[:], func=AF.Sin), reads=[R], writes=[R])

    def s5_phase(L, uT, R_uT, aT, R_aT):
        P = Scope(nc)
        R = Res("s5setup")
        prm = P.sb("prm", [128, 16, 32])
        dsk = P.sb("dsk", [128, 4]); bgl = P.sb("bgl", [128, 4])
        cs2 = P.sb("cs2", [128, 32, 2]); ncs2 = P.sb("ncs2", [128, 32, 2])
        jt = P.sb("jt", [128, 128]); f.dma("sp", jt[:], k_jidx, writes=[R])
        f.dma("sp", dsk[:], s5_d.rearrange("(c p) -> p c", p=128), writes=[R], allow_slow_non_contiguous=True)
        f.dma("sp", bgl[:], b_glu.rearrange("(c p) -> p c", p=128), writes=[R], allow_slow_non_contiguous=True)
        S = Scope(nc)
        st32 = S.sb("st32", [32, 3, 128]); lsr = S.sb("lsr", [32, 2])
        f.dma("sp", st32[:, 0, :], lam_re.rearrange("d (q g) n -> (d q) (g n)", g=2), writes=[R])
        f.dma("sp", st32[:, 1, :], lam_im.rearrange("d (q g) n -> (d q) (g n)", g=2), writes=[R])
        f.dma("sp", lsr[:], log_step.rearrange("d (q g) -> (d q) g", g=2), writes=[R])
        f.op("dve", lambda e: e.tensor_copy(st32[:, 2, :].rearrange("p (g n) -> p g n", g=2), lsr[:].unsqueeze(2).broadcast_to([32, 2, 64])), reads=[R], writes=[R])
        pst = S.ps("pst", [128, 4, 128])
        for k in range(3):
            f.op("pe", lambda e, k=k: e.transpose(pst[:, k, 0:32], st32[:, k, :], ident[0:32, 0:32]), reads=[R, R_ident], writes=[R], acc=(k > 0))
        f.op("dve", lambda e: e.tensor_copy(prm[:, 0:3, :], pst[:, 0:3, 0:32]), reads=[R], writes=[R])
        lr, li = prm[:, 0, :], prm[:, 1, :]
        dt, th, rr = prm[:, 3, :], prm[:, 4, :], prm[:, 5, :]
        f.op("act", lambda e: e.activation(out=dt, in_=prm[:, 2, :], func=AF.Exp), reads=[R], writes=[R])
        f.op("dve", lambda e: e.tensor_tensor(th, li, dt, ALU.mult), reads=[R], writes=[R])
        f.op("dve", lambda e: e.tensor_tensor(prm[:, 10, :], lr, dt, ALU.mult), reads=[R], writes=[R])
        f.op("act", lambda e: e.activation(out=rr, in_=prm[:, 10, :], func=AF.Exp), reads=[R], writes=[R])
        f.op("dve", lambda e: e.tensor_scalar(out=prm[:, 10, :], in0=th, scalar1=128.0, scalar2=None, op0=ALU.mult), reads=[R], writes=[R])
        sincos(S, prm[:, 10, :], 32, prm[:, 7, :], prm[:, 6, :], R, "a")
        sincos(S, th, 32, prm[:, 12, :], prm[:, 11, :], R, "b")
        abre, abim, den, t1, t2 = prm[:, 13, :], prm[:, 14, :], prm[:, 15, :], prm[:, 10, :], prm[:, 2, :]
        f.op("dve", lambda e: e.tensor_tensor(abre, rr, prm[:, 11, :], ALU.mult), reads=[R], writes=[R])
        f.op("dve", lambda e: e.tensor_scalar(out=abre, in0=abre, scalar1=-1.0, scalar2=None, op0=ALU.add), reads=[R], writes=[R])
        f.op("dve", lambda e: e.tensor_tensor(abim, rr, prm[:, 12, :], ALU.mult), reads=[R], writes=[R])
        f.op("dve", lambda e: e.tensor_tensor(den, lr, lr, ALU.mult), reads=[R], writes=[R])
        f.op("dve", lambda e: e.tensor_tensor(t1, li, li, ALU.mult), reads=[R], writes=[R])
        f.op("dve", lambda e: e.tensor_tensor(den, den, t1, ALU.add), reads=[R], writes=[R])
        f.op("dve", lambda e: e.reciprocal(den, den), reads=[R], writes=[R])
        f.op("dve", lambda e: e.tensor_tensor(t1, abre, lr, ALU.mult), reads=[R], writes=[R])
        f.op("dve", lambda e: e.tensor_tensor(t2, abim, li, ALU.mult), reads=[R], writes=[R])
        f.op("dve", lambda e: e.tensor_tensor(t1, t1, t2, ALU.add), reads=[R], writes=[R])
        f.op("dve", lambda e: e.tensor_tensor(prm[:, 8, :], t1, den, ALU.mult), reads=[R], writes=[R])
        f.op("dve", lambda e: e.tensor_tensor(t1, abim, lr, ALU.mult), reads=[R], writes=[R])
        f.op("dve", lambda e: e.tensor_tensor(t2, abre, li, ALU.mult), reads=[R], writes=[R])
        f.op("dve", lambda e: e.tensor_tensor(t1, t1, t2, ALU.subtract), reads=[R], writes=[R])
        f.op("dve", lambda e: e.tensor_tensor(prm[:, 9, :], t1, den, ALU.mult), reads=[R], writes=[R])
        f.op("dve", lambda e: e.tensor_copy(cs2[:, :, 0], prm[:, 6, :]), reads=[R], writes=[R])
        f.op("dve", lambda e: e.tensor_copy(cs2[:, :, 1], prm[:, 7, :]), reads=[R], writes=[R])
        f.op("dve", lambda e: e.tensor_scalar(out=ncs2[:, :, 0], in0=prm[:, 7, :], scalar1=-1.0, scalar2=None, op0=ALU.mult), reads=[R], writes=[R])
        f.op("dve", lambda e: e.tensor_copy(ncs2[:, :, 1], prm[:, 6, :]), reads=[R], writes=[R])
        S.close()
        cosJ = P.sb("cosJ", [128, 8, 128]); sinJ = P.sb("sinJ", [128, 8, 128]); rtab = P.sb("rtab", [128, 8, 128])
        lB = P.sb("lB", [128, 8, 2, 128], BF16); lC = P.sb("lC", [128, 8, 2, 128], BF16)
        RT = Res("s5tab")
        S = Scope(nc)
        yacc = S.sb("yacc", [128, NTOK]); R_y = Res("yacc")
        NB = 2
        psb = [S.ps("psb%d" % k, [128, 2, 512]) for k in range(NB)]; R_psb = RL(NB)
        psy = [S.ps("psy%d" % k, [128, 512]) for k in range(NB)]; R_psy = RL(NB)
        pstr = [S.ps("pstr%d" % k, [128, 4, 128]) for k in range(2)]; R_pstr = RL(2)
        m = [S.sb("m%d" % k, [128, 2, 512]) for k in range(NB)]; R_m = RL(NB)
        ta = [S.sb("ta%d" % k, [128, 2, 512]) for k in range(NB)]; R_ta = RL(NB)
        g = [S.sb("g%d" % k, [128, 2, 512]) for k in range(NB)]; R_g = RL(NB)
        hb = [S.sb("hb%d" % k, [128, 2, 512], BF16) for k in range(NB)]; R_hb = RL(NB)
        ini = S.sb("ini", [128, 4]); R_ini = Res()
        gq1 = S.sb("gq1", [128, 512]); gq2 = S.sb("gq2", [128, 512]); R_gq1 = Res(); R_gq2 = Res()
        wgl = S.sb("wgl", [128, 4, 512], BF16); R_wgl = Res()
        f.dma("pool", wgl[:], w_glu.rearrange("(kc p) n -> p kc n", p=128), writes=[R_wgl])
        blocks = [(0, 256)] + [(256 + 512 * k, 512) for k in range(8)]
        it = 0
        for ct in range(4):
            T = Scope(nc)
            ang = T.sb("ang", [128, 8, 128])
            WB = T.sb("WB", [128, 2, 8, 128]); SC = T.sb("SC", [128, 2, 8, 128]); WB2 = T.sb("WB2", [128, 2, 8, 128])
            fre8 = T.sb("fre8", [128, 8]); fim8 = T.sb("fim8", [128, 8])
            for d in range(2):
                gsl = slice(d * 16 + ct * 4, d * 16 + ct * 4 + 4); lsl = slice(d * 4, d * 4 + 4)
                f.op("dve", lambda e: e.tensor_tensor(ang[:, lsl, :], jt[:].unsqueeze(1).broadcast_to([128, 4, 128]), th[:, gsl].unsqueeze(2).broadcast_to([128, 4, 128]), ALU.mult), reads=[R, RT], writes=[RT])
                f.op("dve", lambda e: e.tensor_copy(rtab[:, lsl, :], rr[:, gsl].unsqueeze(2).broadcast_to([128, 4, 128])), reads=[R, RT], writes=[RT])
                f.op("dve", lambda e: e.tensor_copy(fre8[:, lsl], prm[:, 8, gsl]), reads=[R, RT], writes=[RT])
                f.op("dve", lambda e: e.tensor_copy(fim8[:, lsl], prm[:, 9, gsl]), reads=[R, RT], writes=[RT])
            sincos(T, ang[:].rearrange("p a b -> p (a b)"), 1024, sinJ[:].rearrange("p a b -> p (a b)"), cosJ[:].rearrange("p a b -> p (a b)"), RT, "c%d" % ct)
            f.op("pool", lambda e: e.memset(WB[:], 0.0), reads=[RT], writes=[RT])
            f.op("pool", lambda e: e.memset(SC[:], 0.0), reads=[RT], writes=[RT])
            qn = 0
            for d in range(2):
                for gi in range(8):
                    g_ = ct * 8 + gi
                    l = d * 4 + gi // 2
                    gl = gi % 2
                    for ri, (bsrc, csrc) in enumerate(((b_re, c_re), (b_im, c_im))):
                        q1 = ("sp", "act")[qn % 2]; qn += 1
                        f.dma(q1, WB[64 * gl:64 * gl + 64, ri, l, 16 * gi:16 * gi + 16], bsrc[d, g_], writes=[RT])
                        f.dma(q1, SC[16 * gi:16 * gi + 16, ri, l, 64 * gl:64 * gl + 64], csrc[d, g_], writes=[RT])
            fre = fre8[:].unsqueeze(2).broadcast_to([128, 8, 128]); fim = fim8[:].unsqueeze(2).broadcast_to([128, 8, 128])
            f.op("dve", lambda e: e.tensor_tensor(WB2[:, 0], WB[:, 0], fre, ALU.mult), reads=[RT], writes=[RT])
            f.op("pool", lambda e: e.tensor_tensor(WB2[:, 1], WB[:, 1], fim, ALU.mult), reads=[RT], writes=[RT])
            f.op("dve", lambda e: e.tensor_tensor(WB2[:, 0], WB2[:, 0], WB2[:, 1], ALU.subtract), reads=[RT], writes=[RT])
            f.op("pool", lambda e: e.tensor_tensor(WB2[:, 1], WB[:, 1], fre, ALU.mult), reads=[RT], writes=[RT])
            f.op("dve", lambda e: e.tensor_tensor(WB[:, 0], WB[:, 0], fim, ALU.mult), reads=[RT], writes=[RT])
            f.op("dve", lambda e: e.tensor_tensor(WB2[:, 1], WB2[:, 1], WB[:, 0], ALU.add), reads=[RT], writes=[RT])
            n_ = 0
            for srct, dst, neg in ((WB2, lB, False), (SC, lC, True)):
                for ri in range(2):
                    for d4 in range(2):
                        pb = n_ % 2; n_ += 1
                        for k in range(4):
                            l = d4 * 4 + k
                            f.op("pe", lambda e, k=k, l=l: e.transpose(pstr[pb][:, k, :], srct[:, ri, l, :], ident[:]), reads=[RT, R_ident], writes=[R_pstr[pb]], acc=(k > 0))
                        scl = -1.0 if (neg and ri == 1) else 1.0
                        f.op("act", lambda e: e.activation(out=dst[:, d4 * 4:d4 * 4 + 4, ri, :], in_=pstr[pb][:], func=AF.Identity, scale=scl), reads=[R_pstr[pb], RT], writes=[RT])
            T.close()
            f.op("act", lambda e: e.activation(out=yacc[:], in_=uT[:, ct, :], func=AF.Copy, scale=dsk[:, ct:ct + 1]), reads=R_uT + [R], writes=[R_y])
            items = []
            for pi in range(4):
                for d in range(2):
                    for bidx, (s0, n) in enumerate(blocks):
                        items.append((pi, d, bidx, s0, n))
            NI = len(items)

            def v3(ap):
                return ap.rearrange("p (c j) -> p c j", j=128)

            def geom(k):
                pi, d, bidx, s0, n = items[k]
                bi = k % NB
                dq = d * 16 + ct * 4 + pi
                l = d * 4 + pi
                nch = n // 128
                if d == 0:
                    c0 = s0
                    ucols = uT[:, ct, c0:c0 + n]
                    ycols = yacc[:, c0:c0 + n]
                else:
                    c0 = (256 - s0 - n) if s0 < 256 else (4608 - s0 - n)
                    ucols = rev_ap(uT[:, ct, c0:c0 + n], n)
                    ycols = rev_ap(yacc[:, c0:c0 + n], n)
                tl = [R_uT[kk] for kk in range(c0 // 128, (c0 + n) // 128)]
                cb = cosJ[:, l, :].unsqueeze(1).broadcast_to([128, nch, 128])
                sb_ = sinJ[:, l, :].unsqueeze(1).broadcast_to([128, nch, 128])
                return pi, d, bidx, n, bi, dq, l, nch, ucols, ycols, tl, cb, sb_

            def stA(k):
                pi, d, bidx, n, bi, dq, l, nch, ucols, ycols, tl, cb, sb_ = geom(k)
                for ri in range(2):
                    f.op("pe", lambda e, ri=ri: e.matmul(psb[bi][:, ri, 0:n], lB[:, l, ri, :], ucols, start=True, stop=True),
                         reads=[RT] + tl, writes=[R_psb[bi]], acc=(ri > 0))
                bre, bim = v3(psb[bi][:, 0, 0:n]), v3(psb[bi][:, 1, 0:n])
                mre, mim = v3(m[bi][:, 0, 0:n]), v3(m[bi][:, 1, 0:n])
                t_a, t_b = v3(ta[bi][:, 0, 0:n]), v3(ta[bi][:, 1, 0:n])
                f.op("dve", lambda e: e.tensor_tensor(mre, bre, cb, ALU.mult), reads=[R_psb[bi], RT], writes=[R_m[bi]])
                f.op("dve", lambda e: e.tensor_tensor(t_a, bim, sb_, ALU.mult), reads=[R_psb[bi], RT], writes=[R_ta[bi]])
                f.op("dve", lambda e: e.tensor_tensor(mre, mre, t_a, ALU.add), reads=[R_m[bi], R_ta[bi]], writes=[R_m[bi]])
                f.op("dve", lambda e: e.tensor_tensor(mim, bim, cb, ALU.mult), reads=[R_psb[bi], RT], writes=[R_m[bi]])
                f.op("dve", lambda e: e.tensor_tensor(t_b, bre, sb_, ALU.mult), reads=[R_psb[bi], RT], writes=[R_ta[bi]])
                f.op("dve", lambda e: e.tensor_tensor(mim, mim, t_b, ALU.subtract), reads=[R_m[bi], R_ta[bi]], writes=[R_m[bi]])

            def stB(k):
                pi, d, bidx, n, bi, dq, l, nch, ucols, ycols, tl, cb, sb_ = geom(k)
                prev = None
                if bidx > 0:
                    pbi = (k - 1) % NB
                    pn = items[k - 1][4]
                    prev = (g[pbi], pbi, pn // 128 - 1)
                for c in range(nch):
                    cs = slice(c * 128, (c + 1) * 128)
                    if prev is None:
                        i_re = i_im = 0.0
                        rd_extra = []
                    else:
                        pg, pbi, pc_ = prev
                        gre_l = pg[:, 0, pc_ * 128 + 127:pc_ * 128 + 128]
                        gim_l = pg[:, 1, pc_ * 128 + 127:pc_ * 128 + 128]
                        c128 = prm[:, 6, dq:dq + 1]; s128 = prm[:, 7, dq:dq + 1]
                        f.op("dve", lambda e: e.tensor_scalar(out=ini[:, 0:2], in0=cs2[:, dq, :], scalar1=gre_l, scalar2=None, op0=ALU.mult), reads=[R_g[pbi], R], writes=[R_ini])
                        f.op("dve", lambda e: e.scalar_tensor_tensor(out=ini[:, 0:2], in0=ncs2[:, dq, :], scalar=gim_l, in1=ini[:, 0:2], op0=ALU.mult, op1=ALU.add), reads=[R_g[pbi], R, R_ini], writes=[R_ini])
                        i_re, i_im = ini[:, 0:1], ini[:, 1:2]
                        rd_extra = [R_ini]
                    f.op("dve", lambda e: e.tensor_tensor_scan(g[bi][:, 0, cs], rtab[:, l, :], m[bi][:, 0, cs], i_re, ALU.mult, ALU.add),
                         reads=[R_m[bi], RT] + rd_extra, writes=[R_g[bi]])
                    f.op("dve", lambda e: e.tensor_tensor_scan(g[bi][:, 1, cs], rtab[:, l, :], m[bi][:, 1, cs], i_im, ALU.mult, ALU.add),
                         reads=[R_m[bi], RT] + rd_extra, writes=[R_g[bi]])
                    prev = (g[bi], bi, c)

            def stC(k):
                pi, d, bidx, n, bi, dq, l, nch, ucols, ycols, tl, cb, sb_ = geom(k)
                mre, mim = v3(m[bi][:, 0, 0:n]), v3(m[bi][:, 1, 0:n])
                t_a, t_b = v3(ta[bi][:, 0, 0:n]), v3(ta[bi][:, 1, 0:n])
                gre, gim = v3(g[bi][:, 0, 0:n]), v3(g[bi][:, 1, 0:n])
                hre, him = v3(hb[bi][:, 0, 0:n]), v3(hb[bi][:, 1, 0:n])
                f.op("dve", lambda e: e.tensor_tensor(t_a, gre, cb, ALU.mult), reads=[R_g[bi], RT], writes=[R_ta[bi]])
                f.op("dve", lambda e: e.tensor_tensor(mre, gim, sb_, ALU.mult), reads=[R_g[bi], RT], writes=[R_m[bi]])
                f.op("dve", lambda e: e.tensor_tensor(hre, t_a, mre, ALU.subtract), reads=[R_ta[bi], R_m[bi]], writes=[R_hb[bi]])
                f.op("dve", lambda e: e.tensor_tensor(t_b, gre, sb_, ALU.mult), reads=[R_g[bi], RT], writes=[R_ta[bi]])
                f.op("dve", lambda e: e.tensor_tensor(mim, gim, cb, ALU.mult), reads=[R_g[bi], RT], writes=[R_m[bi]])
                f.op("dve", lambda e: e.tensor_tensor(him, t_b, mim, ALU.add), reads=[R_ta[bi], R_m[bi]], writes=[R_hb[bi]])
                for ri in range(2):
                    f.op("pe", lambda e, ri=ri: e.matmul(psy[bi][:, 0:n], lC[:, l, ri, :], hb[bi][:, ri, 0:n], start=(ri == 0), stop=(ri == 1)),
                         reads=[RT, R_hb[bi]], writes=[R_psy[bi]], acc=(ri > 0))

            def stY(k):
                pi, d, bidx, n, bi, dq, l, nch, ucols, ycols, tl, cb, sb_ = geom(k)
                f.op("dve", lambda e: e.tensor_tensor(ycols, psy[bi][:, 0:n], ycols, ALU.add), reads=[R_psy[bi], R_y], writes=[R_y])

            stA(0)
            for k in range(NI):
                if k + 1 < NI:
                    stA(k + 1)
                stB(k)
                stC(k)
                if k >= 1:
                    stY(k - 1)
            stY(NI - 1)
            for (s0, n) in blocks:
                yb = yacc[:, s0:s0 + n]
                f.op("pool", lambda e: e.tensor_tensor(gq1[:, 0:n], yb, yb, ALU.mult), reads=[R_y], writes=[R_gq1])
                f.op("dve", lambda e: e.tensor_scalar(out=gq1[:, 0:n], in0=gq1[:, 0:n], scalar1=0.044715, scalar2=1.0, op0=ALU.mult, op1=ALU.add), reads=[R_gq1], writes=[R_gq1])
                f.op("pool", lambda e: e.tensor_tensor(gq1[:, 0:n], gq1[:, 0:n], yb, ALU.mult), reads=[R_gq1, R_y], writes=[R_gq1])
                f.op("act", lambda e: e.activation(out=gq2[:, 0:n], in_=gq1[:, 0:n], func=AF.Sigmoid, scale=1.5957691216057308), reads=[R_gq1], writes=[R_gq2])
                f.op("dve", lambda e: e.tensor_tensor(aT[:, ct, s0:s0 + n], yb, gq2[:, 0:n], ALU.mult), reads=[R_gq2, R_y], writes=R_aT[s0 // 128:(s0 + n) // 128])
        sg = [S.sb("sg%d" % k, [128, 512], BF16) for k in range(2)]; R_sg = RL(2)
        anew = S.sb("anew", [128, 4, 512], BF16); R_anew = Res()
        nn = 0
        for (s0, n) in blocks:
            tl = R_aT[s0 // 128:(s0 + n) // 128]
            for cto in range(4):
                bi = nn % 2; nn += 1
                for cti in range(4):
                    f.op("pe", lambda e, cti=cti: e.matmul(psy[bi][:, 0:n], wgl[:, cti, cto * 128:(cto + 1) * 128], aT[:, cti, s0:s0 + n], start=(cti == 0), stop=(cti == 3)),
                         reads=[R_wgl] + tl, writes=[R_psy[bi]], acc=(cti > 0))
                f.op("act", lambda e: e.activation(out=sg[bi][:, 0:n], in_=psy[bi][:, 0:n], func=AF.Sigmoid, bias=bgl[:, cto:cto + 1], scale=1.0), reads=[R_psy[bi], R], writes=[R_sg[bi]])
                f.op("dve", lambda e: e.tensor_tensor(anew[:, cto, 0:n], aT[:, cto, s0:s0 + n], sg[bi][:, 0:n], ALU.mult), reads=[R_sg[bi]] + tl, writes=[R_anew])
            f.op("pool", lambda e: e.tensor_copy(aT[:, :, s0:s0 + n], anew[:, :, 0:n]), reads=[R_anew], writes=tl)
        S.close()
        P.close()

    def win_phase(qT, R_qT, kT2, R_kT, vaug, R_v, oT, R_oT):
        S = Scope(nc)
        esink = S.sb("esink", [128, 8]); R_es = Res()
        f.dma("sp", esink[:], win_sink.partition_broadcast(128), writes=[R_es])
        f.op("act", lambda e: e.activation(out=esink[:], in_=esink[:], func=AF.Exp), reads=[R_es], writes=[R_es])
        NBS = 3
        ps_s = [S.ps("ps_s%d" % k, [128, 8, 128]) for k in range(NBS)]; R_pss = RL(NBS)
        ps_o = [S.ps("ps_o%d" % k, [128, 512]) for k in range(2)]; R_pso = RL(2)
        pT = [S.sb("pT%d" % k, [128, 5, 128], BF16) for k in range(NBS)]; R_pT = RL(NBS)
        dtmp = [S.sb("dtmp%d" % k, [128, 128]) for k in range(2)]; R_dt = RL(2)
        items = []
        for t in range(NT):
            kts = [(0, None), (1, None)]
            if t >= 2:
                for kt in (t - 1, t, t + 1):
                    if 2 <= kt < NT:
                        kts.append((kt, (0 if kt == t - 1 else (1 if kt == t + 1 else None))))
            for h in range(8):
                items.append((t, h, kts))

        def front(i_):
            t, h, kts = items[i_]
            cols = slice(t * 128, (t + 1) * 128)
            nk = len(kts)
            bs = i_ % NBS
            pr, base, kvh = h // 2, 64 * (h % 2), h // 4
            for i, (kt, mk) in enumerate(kts):
                f.op("pe", lambda e, i=i, kt=kt: e.matmul(ps_s[bs][:, i, :], kT2[base:base + 64, kvh, kt * 128:(kt + 1) * 128], qT[base:base + 64, pr, cols], start=True, stop=True),
                     reads=[R_kT[kt], R_qT[t]], writes=[R_pss[bs]], acc=(i > 0))
            f.op("act", lambda e: e.activation(out=pT[bs][:, 0:nk, :], in_=ps_s[bs][:, 0:nk, :], func=AF.Exp, scale=0.125), reads=[R_pss[bs]], writes=[R_pT[bs]])
            for i, (kt, mk) in enumerate(kts):
                if mk is not None:
                    f.op("dve", lambda e, i=i, mk=mk: e.tensor_tensor(pT[bs][:, i, :], pT[bs][:, i, :], maskb[:, mk, :], ALU.mult), reads=[R_pT[bs], R_mask], writes=[R_pT[bs]])

        def back(i_):
            t, h, kts = items[i_]
            cols = slice(t * 128, (t + 1) * 128)
            nk = len(kts)
            bs = i_ % NBS
            bi = i_ % 2
            pr, base, kvh = h // 2, 64 * (h % 2), h // 4
            voff = (64 if h % 2 == 0 else 0) + 128 * kvh
            for i, (kt, mk) in enumerate(kts):
                f.op("pe", lambda e, i=i, kt=kt: e.matmul(ps_o[bi][:, 0:128], vaug[:, kt, voff:voff + 128], pT[bs][:, i, :], start=(i == 0), stop=(i == nk - 1)),
                     reads=[R_v[kt], R_pT[bs]], writes=[R_pso[bi]], acc=(i > 0))
            nb, db = (0, 64) if h % 2 == 0 else (64, 0)
            f.op("dve", lambda e: e.tensor_scalar(out=dtmp[bi][nb:nb + 64, :], in0=ps_o[bi][db:db + 64, 0:128], scalar1=esink[db:db + 64, h:h + 1], scalar2=None, op0=ALU.add),
                 reads=[R_pso[bi], R_es], writes=[R_dt[bi]])
            f.op("dve", lambda e: e.reciprocal(dtmp[bi][nb:nb + 64, :], dtmp[bi][nb:nb + 64, :]), reads=[R_dt[bi]], writes=[R_dt[bi]])
            f.op("dve", lambda e: e.tensor_tensor(oT[nb:nb + 64, pr, cols], ps_o[bi][nb:nb + 64, 0:128], dtmp[bi][nb:nb + 64, :], ALU.mult), reads=[R_pso[bi], R_dt[bi]], writes=[R_oT[t]])
        front(0)
        for i_ in range(len(items)):
            if i_ + 1 < len(items):
                front(i_ + 1)
            back(i_)
        S.close()

    def out_phase(layer, w_out_d, mix_loader, tiles):
        S = Scope(nc)
        load_ln(layer * 2 + 0)
        wo = S.sb("wo", [128, 8, D], BF16); R_wo = Res()
        f.dma("pool", wo[:], w_out_d.rearrange("(kc p) n -> p kc n", p=128), writes=[R_wo])
        xt = [S.sb("xt%d" % k, [128, D]) for k in range(2)]; R_xt = RL(2)
        ot = [S.sb("ot%d" % k, [128, D]) for k in range(2)]; R_ot = RL(2)
        tmp = S.sb("tmp", [128, D]); R_tmp = Res()
        small = S.sb("small", [128, 16]); R_small = Res()
        ps_o2 = [S.ps("ps_o2%d" % k, [128, D]) for k in range(2)]; R_ps = RL(2)
        for n, t in enumerate(tiles):
            b = n % 2
            src, rs = src_tile(layer, t)
            f.dma("sp", xt[b][:], src, reads=rs, writes=[R_xt[b]])
            mixT, R_mix = mix_loader(t, b)
            for half in range(2):
                for kc in range(8):
                    f.op("pe", lambda e, kc=kc, half=half: e.matmul(ps_o2[b][:, half * 512:(half + 1) * 512], mixT[kc], wo[:, kc, half * 512:(half + 1) * 512], start=(kc == 0), stop=(kc == 7)),
                         reads=[R_wo] + R_mix, writes=[R_ps[b]], acc=(half + kc > 0))
            resid_ln(S, xt[b], R_xt[b], ps_o2[b], R_ps[b], (1 if t < 2 else 0), layer * 2 + 0, ot[b], R_ot[b], tmp, R_tmp, small, R_small)
            f.dma("act", XR[t * 128:(t + 1) * 128, :], ot[b][:], reads=[R_ot[b]], writes=[R_XR[t]])
        S.close()

    def ffn_phase(layer, tiles_all, final):
        P = Scope(nc)
        rw = P.sb("rw", [128, 8, 32]); R_rw = Res()
        f.dma("sp", rw[:], router_w.rearrange("(kc p) n -> p kc n", p=128), writes=[R_rw])
        rbias = P.sb("rbias", [128, 32]); f.dma("sp", rbias[:], router_b.partition_broadcast(128), writes=[R_rw])
        GT = 9
        load_ln(layer * 2 + 1)
        groups = [tiles_all[i:i + GT] for i in range(0, len(tiles_all), GT)]
        for grp in groups:
            S = Scope(nc)
            ng = len(grp)
            hT = S.sb("hTg", [128, 8, GT * 128], BF16); R_hT = RL(ng, "hTg")
            comb = S.sb("comb", [128, GT, 32]); R_comb = RL(ng, "comb")
            yacc = S.sb("yaccg", [128, GT, D]); R_y = RL(ng, "yg")
            A = Scope(nc)
            xt = [A.sb("xt%d" % k, [128, D]) for k in range(2)]; R_xt = RL(2)
            h32 = A.sb("h32", [128, D]); R_h32 = Res()
            h32T = A.sb("h32T", [128, 8, 128]); R_h32T = Res()
            ps_tp = A.ps("ps_tp", [128, 8, 128]); R_pstp = Res(x=True)
            ps_r = A.ps("ps_r", [128, 512]); R_psr = Res()
            sc = A.sb("sc", [128, 32]); sel = A.sb("sel", [128, 32]); R_sc = Res()
            pa = A.sb("pa", [128, 8, 6]); pm = A.sb("pm", [128, 8, 6]); gs = A.sb("gs", [128, 8]); thr = A.sb("thr", [128, 8])
            gm = A.sb("gm", [128, 2]); mg = A.sb("mg", [128, 8]); sm = A.sb("sm", [128, 8, 4])
            for j, t in enumerate(grp):
                b = j % 2
                f.dma("sp", xt[b][:], XR[t * 128:(t + 1) * 128, :], reads=[R_XR[t]], writes=[R_xt[b]])
                which = 1 if t < 2 else 0
                mod_transpose(xt[b], R_xt[b], which, h32, R_h32, ps_tp, R_pstp, hT[:, :, j * 128:(j + 1) * 128], R_hT[j], h32T, R_h32T)
                for kc in range(8):
                    f.op("pe", lambda e, kc=kc: e.matmul(ps_r[:, 0:32], h32T[:, kc, :], rw[:, kc, :], start=(kc == 0), stop=(kc == 7)), reads=[R_h32T, R_rw], writes=[R_psr], acc=(kc > 0))
                R1 = R_sc
                f.op("act", lambda e: e.activation(out=sc[:], in_=ps_r[:, 0:32], func=AF.Sigmoid), reads=[R_psr], writes=[R1])
                f.op("dve", lambda e: e.tensor_tensor(sel[:], sc[:], rbias[:], ALU.add), reads=[R1, R_rw], writes=[R1])
                s3 = sel[:].rearrange("p (g e) -> p g e", e=4)
                pairs = [(0, 1), (0, 2), (0, 3), (1, 2), (1, 3), (2, 3)]
                for k, (a_, b_) in enumerate(pairs):
                    f.op("dve", lambda e, k=k, a_=a_, b_=b_: e.tensor_tensor(pa[:, :, k], s3[:, :, a_], s3[:, :, b_], ALU.add), reads=[R1], writes=[R1])
                    f.op("dve", lambda e, k=k, a_=a_, b_=b_: e.tensor_tensor(pm[:, :, k], s3[:, :, a_], s3[:, :, b_], ALU.min), reads=[R1], writes=[R1])
                f.op("dve", lambda e: e.tensor_reduce(out=gs[:], in_=pa[:], axis=AX.X, op=ALU.max), reads=[R1], writes=[R1])
                f.op("dve", lambda e: e.tensor_reduce(out=thr[:], in_=pm[:], axis=AX.X, op=ALU.max), reads=[R1], writes=[R1])
                f.op("dve", lambda e: e.tensor_reduce(out=gm[:, 0:1], in_=gs[:], axis=AX.X, op=ALU.max), reads=[R1], writes=[R1])
                f.op("dve", lambda e: e.tensor_scalar(out=mg[:], in0=gs[:], scalar1=gm[:, 0:1], scalar2=None, op0=ALU.is_ge), reads=[R1], writes=[R1])
                f.op("dve", lambda e: e.tensor_tensor(sm[:], s3, thr[:].unsqueeze(2).broadcast_to([128, 8, 4]), ALU.is_ge), reads=[R1], writes=[R1])
                f.op("dve", lambda e: e.tensor_tensor(sm[:], sm[:], mg[:].unsqueeze(2).broadcast_to([128, 8, 4]), ALU.mult), reads=[R1], writes=[R1])
                cj = comb[:, j, :]
                f.op("dve", lambda e: e.tensor_tensor(cj, sm[:].rearrange("p g e -> p (g e)"), sc[:], ALU.mult), reads=[R1], writes=[R_comb[j]])
                f.op("dve", lambda e: e.tensor_reduce(out=gm[:, 1:2], in_=cj, axis=AX.X, op=ALU.add), reads=[R_comb[j], R1], writes=[R1])
                f.op("dve", lambda e: e.reciprocal(gm[:, 1:2], gm[:, 1:2]), reads=[R1], writes=[R1])
                f.op("dve", lambda e: e.tensor_scalar(out=cj, in0=cj, scalar1=gm[:, 1:2], scalar2=None, op0=ALU.mult), reads=[R1, R_comb[j]], writes=[R_comb[j]])
            A.close()
            B = Scope(nc)
            wg = [B.sb("wg%d" % k, [128, 8, 512], BF16) for k in range(2)]
            wu = [B.sb("wu%d" % k, [128, 8, 512], BF16) for k in range(2)]
            wd = [B.sb("wd%d" % k, [128, 4, D], BF16) for k in range(2)]
            R_w = RL(2, "w")
            psg = [B.ps("psg%d" % k, [128, 512]) for k in range(2)]; R_psg = RL(2)
            psu = [B.ps("psu%d" % k, [128, 512]) for k in range(2)]; R_psu = RL(2)
            psd = [B.ps("psd%d" % k, [128, D]) for k in range(2)]; R_psd = RL(2)
            sg = [B.sb("sg%d" % k, [128, 512]) for k in range(2)]; R_sg = RL(2)
            hid = [B.sb("hid%d" % k, [128, 4, 512], BF16) for k in range(2)]; R_hid = RL(2)
            ntok = ng * 128
            blocks = [(c0, min(512, ntok - c0)) for c0 in range(0, ntok, 512)]
            nfc = 0; nblk = 0; nd = 0
            for ex in range(32):
                wb = ex % 2
                f.dma("pool", wg[wb][:], w_gate[layer, ex].rearrange("(kc p) n -> p kc n", p=128), writes=[R_w[wb]])
                f.dma("pool", wu[wb][:], w_up[layer, ex].rearrange("(kc p) n -> p kc n", p=128), writes=[R_w[wb]])
                f.dma("pool", wd[wb][:], w_down[layer, ex].rearrange("(kc p) n -> p kc n", p=128), writes=[R_w[wb]])
                for (c0, n) in blocks:
                    hb_ = nblk % 2; nblk += 1
                    tl = R_hT[c0 // 128:(c0 + n) // 128]
                    for fc in range(4):
                        pb = nfc % 2; nfc += 1
                        for kc in range(8):
                            f.op("pe", lambda e, kc=kc, fc=fc: e.matmul(psg[pb][:, 0:n], wg[wb][:, kc, fc * 128:(fc + 1) * 128], hT[:, kc, c0:c0 + n], start=(kc == 0), stop=(kc == 7)),
                                 reads=[R_w[wb]] + tl, writes=[R_psg[pb]], acc=(kc > 0))
                        for kc in range(8):
                            f.op("pe", lambda e, kc=kc, fc=fc: e.matmul(psu[pb][:, 0:n], wu[wb][:, kc, fc * 128:(fc + 1) * 128], hT[:, kc, c0:c0 + n], start=(kc == 0), stop=(kc == 7)),
                                 reads=[R_w[wb]] + tl, writes=[R_psu[pb]], acc=(kc > 0))
                        f.op("act", lambda e: e.activation(out=sg[pb][:, 0:n], in_=psg[pb][:, 0:n], func=AF.Silu), reads=[R_psg[pb]], writes=[R_sg[pb]])
                        f.op("dve", lambda e, fc=fc: e.tensor_tensor(hid[hb_][:, fc, 0:n], sg[pb][:, 0:n], psu[pb][:, 0:n], ALU.mult), reads=[R_sg[pb], R_psu[pb]], writes=[R_hid[hb_]])
                    for tt in range(n // 128):
                        j = c0 // 128 + tt
                        db = nd % 2; nd += 1
                        for half in range(2):
                            for fc in range(4):
                                f.op("pe", lambda e, fc=fc, half=half: e.matmul(psd[db][:, half * 512:(half + 1) * 512], hid[hb_][:, fc, tt * 128:(tt + 1) * 128], wd[wb][:, fc, half * 512:(half + 1) * 512], start=(fc == 0), stop=(fc == 3)),
                                     reads=[R_w[wb], R_hid[hb_]], writes=[R_psd[db]], acc=(half + fc > 0))
                        cw = comb[:, j, ex:ex + 1]
                        if ex == 0:
                            f.op("dve", lambda e: e.tensor_scalar(out=yacc[:, j, :], in0=psd[db][:], scalar1=cw, scalar2=None, op0=ALU.mult), reads=[R_psd[db], R_comb[j]], writes=[R_y[j]])
                        else:
                            f.op("dve", lambda e: e.scalar_tensor_tensor(out=yacc[:, j, :], in0=psd[db][:], scalar=cw, in1=yacc[:, j, :], op0=ALU.mult, op1=ALU.add), reads=[R_psd[db], R_comb[j], R_y[j]], writes=[R_y[j]])
            B.close()
            C = Scope(nc)
            xt = [C.sb("xt%d" % k, [128, D]) for k in range(2)]; R_xt = RL(2)
            ot = [C.sb("ot%d" % k, [128, D]) for k in range(2)]; R_ot = RL(2)
            tmp = C.sb("tmp", [128, D]); R_tmp = Res()
            small = C.sb("small", [128, 16]); R_small = Res()
            for j, t in enumerate(grp):
                b = j % 2
                f.dma("sp", xt[b][:], XR[t * 128:(t + 1) * 128, :], reads=[R_XR[t]], writes=[R_xt[b]])
                yj = yacc[:, j, :]

                class _V:
                    def __init__(self, ap): self.ap = ap
                    def __getitem__(self, k): return self.ap
                resid_ln(C, xt[b], R_xt[b], _V(yj), R_y[j], (1 if t < 2 else 0), layer * 2 + 1, ot[b], R_ot[b], tmp, R_tmp, small, R_small)
                if final:
                    f.dma("act", out_d[(t - 2) * 128:(t - 1) * 128, :], ot[b][:], reads=[R_ot[b]], writes=[R_out])
                else:
                    f.dma("act", XR[t * 128:(t + 1) * 128, :], ot[b][:], reads=[R_ot[b]], writes=[R_XR[t]])
            C.close()
            S.close()
        P.close()


    def ffn_sparse(layer, tiles_all, final):
        IOA = bass.IndirectOffsetOnAxis
        ng = len(tiles_all)
        M = ng * 32
        P = Scope(nc)
        rw = P.sb("rw", [128, 8, 32]); R_rw = Res()
        f.dma("sp", rw[:], router_w.rearrange("(kc p) n -> p kc n", p=128), writes=[R_rw])
        rbias = P.sb("rbias", [128, 32]); f.dma("sp", rbias[:], router_b.partition_broadcast(128), writes=[R_rw])
        jt = P.sb("jt2", [128, 128]); f.dma("sp", jt[:], k_jidx, writes=[R_rw])
        pc = P.sb("pc", [128, 4]); f.dma("sp", pc[:], k_pc, writes=[R_rw])
        load_ln(layer * 2 + 1)
        comb = P.sb("comb", [128, ng, 32]); R_comb = RL(ng, "comb")
        posA_i = P.sb("posA_i", [128, ng], I32); posB_i = P.sb("posB_i", [128, ng], I32)
        wA = P.sb("wA", [128, ng]); wB = P.sb("wB", [128, ng])
        NSO = NS - 32
        idxw = P.sb("idxw", [128, NSO, 4], I32)
        R_rt = Res("route")
        R_XsW = RL(ng, "xsw")
        R_Ys = RL(NS, "ys")
        HB = Scope(nc)
        hb_all = HB.sb("hb_all", [128, ng, D], BF16); R_hb = RL(ng, "hb")
        A = Scope(nc)
        xt = [A.sb("xt%d" % k, [128, D]) for k in range(2)]; R_xt = RL(2)
        h32 = [A.sb("h32%d" % k, [128, D]) for k in range(2)]; R_h32 = RL(2)
        h32T = A.sb("h32T", [128, 8, 128]); R_h32T = Res()
        ps_tp = [A.ps("ps_tp%d" % k, [128, 8, 128]) for k in range(2)]; R_pstp = RL(2)
        ps_r = A.ps("ps_r", [128, 512]); R_psr = Res()
        R_sc = Res()
        sc_all = A.sb("sc_all", [128, ng, 32]); sel_all = A.sb("sel_all", [128, ng, 32])
        pa_all = A.sb("pa_all", [128, ng, 8, 6]); pm_all = A.sb("pm_all", [128, ng, 8, 6])
        gs_all = A.sb("gs_all", [128, ng, 8]); thr_all = A.sb("thr_all", [128, ng, 8]); mg_all = A.sb("mg_all", [128, ng, 8])
        gm_all = A.sb("gm_all", [128, ng]); sm_all = A.sb("sm_all", [128, ng, 8, 4])
        for j, t in enumerate(tiles_all):
            b = j % 2
            f.dma("sp", xt[b][:], XR[t * 128:(t + 1) * 128, :], reads=[R_XR[t]], writes=[R_xt[b]])
            which = 1 if t < 2 else 0
            f.op("dve", lambda e: e.tensor_tensor(h32[b][:], xt[b][:], mod[:, which, 1, :], ALU.mult), reads=[R_xt[b], R_mod], writes=[R_h32[b]])
            f.op("dve", lambda e: e.tensor_tensor(h32[b][:], h32[b][:], mod[:, which, 0, :], ALU.add), reads=[R_h32[b], R_mod], writes=[R_h32[b]])
            for kc in range(8):
                f.op("pe", lambda e, kc=kc: e.transpose(ps_tp[b][:, kc, :], h32[b][:, kc * 128:(kc + 1) * 128], ident[:]),
                     reads=[R_h32[b], R_ident], writes=[R_pstp[b]], acc=(kc > 0))
            f.op("dve", lambda e: e.tensor_copy(h32T[:], ps_tp[b][:]), reads=[R_pstp[b]], writes=[R_h32T])
            f.op("act", lambda e: e.activation(out=hb_all[:, j, :].rearrange("p (c j q) -> p c j q", c=4, j=2),
                                               in_=h32[b][:].rearrange("p (c q j) -> p c j q", c=4, j=2), func=AF.Identity),
                 reads=[R_h32[b]], writes=[R_hb[j]])
            for kc in range(8):
                f.op("pe", lambda e, kc=kc: e.matmul(ps_r[:, 0:32], h32T[:, kc, :], rw[:, kc, :], start=(kc == 0), stop=(kc == 7)), reads=[R_h32T, R_rw], writes=[R_psr], acc=(kc > 0))
            f.op("act", lambda e: e.activation(out=sc_all[:, j, :], in_=ps_r[:, 0:32], func=AF.Sigmoid), reads=[R_psr], writes=[R_sc])
        R1 = R_sc
        s4 = sel_all[:].rearrange("p t (g e) -> p t g e", e=4)
        f.op("dve", lambda e: e.tensor_tensor(sel_all[:], sc_all[:], rbias[:].unsqueeze(1).broadcast_to([128, ng, 32]), ALU.add), reads=[R1, R_rw], writes=[R1])
        pairs = [(0, 1), (0, 2), (0, 3), (1, 2), (1, 3), (2, 3)]
        for k, (a_, b_) in enumerate(pairs):
            f.op("dve", lambda e, k=k, a_=a_, b_=b_: e.tensor_tensor(pa_all[:, :, :, k], s4[:, :, :, a_], s4[:, :, :, b_], ALU.add), reads=[R1], writes=[R1])
            f.op("dve", lambda e, k=k, a_=a_, b_=b_: e.tensor_tensor(pm_all[:, :, :, k], s4[:, :, :, a_], s4[:, :, :, b_], ALU.min), reads=[R1], writes=[R1])
        f.op("dve", lambda e: e.tensor_reduce(out=gs_all[:], in_=pa_all[:], axis=AX.X, op=ALU.max), reads=[R1], writes=[R1])
        f.op("dve", lambda e: e.tensor_reduce(out=thr_all[:], in_=pm_all[:], axis=AX.X, op=ALU.max), reads=[R1], writes=[R1])
        f.op("dve", lambda e: e.tensor_reduce(out=gm_all[:], in_=gs_all[:], axis=AX.X, op=ALU.max), reads=[R1], writes=[R1])
        f.op("dve", lambda e: e.tensor_tensor(mg_all[:], gs_all[:], gm_all[:].unsqueeze(2).broadcast_to([128, ng, 8]), ALU.is_ge), reads=[R1], writes=[R1])
        f.op("dve", lambda e: e.tensor_tensor(sm_all[:], s4, thr_all[:].unsqueeze(3).broadcast_to([128, ng, 8, 4]), ALU.is_ge), reads=[R1], writes=[R1])
        f.op("dve", lambda e: e.tensor_tensor(sm_all[:], sm_all[:], mg_all[:].unsqueeze(3).broadcast_to([128, ng, 8, 4]), ALU.mult), reads=[R1], writes=[R1])
        f.op("dve", lambda e: e.tensor_tensor(comb[:], sm_all[:].rearrange("p t g e -> p t (g e)"), sc_all[:], ALU.mult), reads=[R1], writes=R_comb)
        f.op("dve", lambda e: e.tensor_reduce(out=gm_all[:], in_=comb[:], axis=AX.X, op=ALU.add), reads=R_comb + [R1], writes=[R1])
        f.op("dve", lambda e: e.reciprocal(gm_all[:], gm_all[:]), reads=[R1], writes=[R1])
        f.op("dve", lambda e: e.tensor_tensor(comb[:], comb[:], gm_all[:].unsqueeze(2).broadcast_to([128, ng, 32]), ALU.mult), reads=[R1] + R_comb, writes=R_comb)
        A.close()
        Bq = Scope(nc)
        m_ = Bq.sb("m_", [128, ng, 32]); mb16 = Bq.sb("mb16", [128, ng, 32], BF16)
        rank = Bq.sb("rank", [128, ng, 32]); tot = Bq.sb("tot", [128, ng, 32]); base = Bq.sb("base", [128, ng, 32])
        me = Bq.sb("me", [128, ng, 32]); Bm = Bq.sb("Bm", [128, ng, 32]); Am = Bq.sb("Am", [128, ng, 32]); tmpq = Bq.sb("tmpq", [128, ng, 32])
        ones16 = Bq.sb("ones16", [128, 128], BF16)
        cnt = Bq.sb("cnt", [128, 32]); cmp17 = Bq.sb("cmp17", [128, 32, 18]); thr18 = Bq.sb("thr18", [128, 18])
        tlf = Bq.sb("tlf", [128, 32]); sinc = Bq.sb("sinc", [128, 32]); so512 = Bq.sb("so512", [128, 32]); c1e = Bq.sb("c1e", [128, 32])
        mx = Bq.sb("mx", [128, ng]); pAf = Bq.sb("pAf", [128, ng]); pBf = Bq.sb("pBf", [128, ng])
        cmpj = Bq.sb("cmpj", [128, NSO, 32]); eidf = Bq.sb("eidf", [128, NSO]); idxf = Bq.sb("idxf", [128, NSO, 4])
        ps_rk = Bq.ps("ps_rk", [128, 3, 512]); ps_tt = Bq.ps("ps_tt", [128, 3, 512])
        RB = [R_rt]

        def fl(ap3):
            return ap3.rearrange("p a b -> p (a b)")
        f.op("dve", lambda e: e.tensor_scalar(out=fl(m_[:]), in0=fl(comb[:]), scalar1=0.0, scalar2=None, op0=ALU.is_gt), reads=R_comb, writes=RB)
        f.op("dve", lambda e: e.tensor_copy(fl(mb16[:]), fl(m_[:])), reads=RB, writes=RB)
        f.op("pool", lambda e: e.memset(ones16[:], 1.0), reads=RB, writes=RB)
        chunks = [(n0, min(M, n0 + 512)) for n0 in range(0, M, 512)]
        for ch, (n0, n1) in enumerate(chunks):
            f.op("pe", lambda e, ch=ch, n0=n0, n1=n1: e.matmul(ps_rk[:, ch, 0:n1 - n0], maskb[:, 2, :], fl(mb16[:])[:, n0:n1], start=True, stop=True), reads=RB + [R_mask], writes=RB)
            f.op("pe", lambda e, ch=ch, n0=n0, n1=n1: e.matmul(ps_tt[:, ch, 0:n1 - n0], ones16[:], fl(mb16[:])[:, n0:n1], start=True, stop=True), reads=RB, writes=RB)
        for ch, (n0, n1) in enumerate(chunks):
            f.op("dve", lambda e, ch=ch, n0=n0, n1=n1: e.tensor_copy(fl(rank[:])[:, n0:n1], ps_rk[:, ch, 0:n1 - n0]), reads=RB, writes=RB)
            f.op("dve", lambda e, ch=ch, n0=n0, n1=n1: e.tensor_copy(fl(tot[:])[:, n0:n1], ps_tt[:, ch, 0:n1 - n0]), reads=RB, writes=RB)
        f.op("dve", lambda e: e.memset(base[:, 0, :], 0.0), reads=RB, writes=RB)
        for t_ in range(1, ng):
            f.op("dve", lambda e, t_=t_: e.tensor_tensor(base[:, t_, :], base[:, t_ - 1, :], tot[:, t_ - 1, :], ALU.add), reads=RB, writes=RB)
        f.op("dve", lambda e: e.tensor_tensor(cnt[:], base[:, ng - 1, :], tot[:, ng - 1, :], ALU.add), reads=RB, writes=RB)
        f.op("dve", lambda e: e.tensor_scalar(out=thr18[:], in0=jt[:, 0:18], scalar1=512.0, scalar2=None, op0=ALU.mult), reads=RB + [R_rw], writes=RB)
        f.op("dve", lambda e: e.tensor_tensor(cmp17[:], cnt[:].unsqueeze(2).broadcast_to([128, 32, 18]), thr18[:].unsqueeze(1).broadcast_to([128, 32, 18]), ALU.is_gt), reads=RB, writes=RB)
        f.op("dve", lambda e: e.tensor_reduce(out=tlf[:], in_=cmp17[:], axis=AX.X, op=ALU.add), reads=RB, writes=RB)
        f.op("dve", lambda e: e.tensor_scalar(out=tlf[:], in0=tlf[:], scalar1=-1.0, scalar2=0.0, op0=ALU.add, op1=ALU.max), reads=RB, writes=RB)
        f.op("dve", lambda e: e.tensor_copy(sinc[:], tlf[:]), reads=RB, writes=RB)
        for e_ in range(1, 32):
            f.op("dve", lambda e, e_=e_: e.tensor_tensor(sinc[:, e_:e_ + 1], sinc[:, e_ - 1:e_], tlf[:, e_:e_ + 1], ALU.add), reads=RB, writes=RB)
        f.op("dve", lambda e: e.tensor_tensor(so512[:], sinc[:], tlf[:], ALU.subtract), reads=RB, writes=RB)
        f.op("dve", lambda e: e.tensor_scalar(out=so512[:], in0=so512[:], scalar1=512.0, scalar2=15872.0, op0=ALU.mult, op1=ALU.add), reads=RB, writes=RB)
        f.op("dve", lambda e: e.tensor_scalar(out=c1e[:], in0=jt[:, 0:32], scalar1=512.0, scalar2=None, op0=ALU.mult), reads=RB + [R_rw], writes=RB)
        f.op("dve", lambda e: e.tensor_tensor(so512[:], so512[:], c1e[:], ALU.subtract), reads=RB, writes=RB)
        f.op("dve", lambda e: e.tensor_tensor(fl(rank[:]), fl(rank[:]), fl(base[:]), ALU.add), reads=RB, writes=RB)
        f.op("dve", lambda e: e.tensor_scalar(out=fl(tmpq[:]), in0=fl(rank[:]), scalar1=512.0, scalar2=None, op0=ALU.is_ge), reads=RB, writes=RB)
        f.op("dve", lambda e: e.tensor_tensor(tmpq[:], tmpq[:], so512[:].unsqueeze(1).broadcast_to([128, ng, 32]), ALU.mult), reads=RB, writes=RB)
        f.op("dve", lambda e: e.tensor_tensor(rank[:], rank[:], c1e[:].unsqueeze(1).broadcast_to([128, ng, 32]), ALU.add), reads=RB, writes=RB)
        f.op("dve", lambda e: e.tensor_tensor(fl(rank[:]), fl(rank[:]), fl(tmpq[:]), ALU.add), reads=RB, writes=RB)
        f.op("dve", lambda e: e.tensor_tensor(me[:], m_[:], jt[:, 1:33].unsqueeze(1).broadcast_to([128, ng, 32]), ALU.mult), reads=RB, writes=RB)
        f.op("dve", lambda e: e.tensor_reduce(out=mx[:], in_=me[:], axis=AX.X, op=ALU.max), reads=RB, writes=RB)
        f.op("dve", lambda e: e.tensor_tensor(Bm[:], me[:], mx[:].unsqueeze(2).broadcast_to([128, ng, 32]), ALU.is_equal), reads=RB, writes=RB)
        f.op("dve", lambda e: e.tensor_tensor(fl(Am[:]), fl(m_[:]), fl(Bm[:]), ALU.subtract), reads=RB, writes=RB)
        for (msk, pf, wf) in ((Am, pAf, wA), (Bm, pBf, wB)):
            f.op("dve", lambda e, msk=msk: e.tensor_tensor(fl(tmpq[:]), fl(msk[:]), fl(rank[:]), ALU.mult), reads=RB, writes=RB)
            f.op("dve", lambda e, pf=pf: e.tensor_reduce(out=pf[:], in_=tmpq[:], axis=AX.X, op=ALU.add), reads=RB, writes=RB)
            f.op("dve", lambda e, msk=msk: e.tensor_tensor(fl(tmpq[:]), fl(msk[:]), fl(comb[:]), ALU.mult), reads=RB + R_comb, writes=RB)
            f.op("dve", lambda e, wf=wf: e.tensor_reduce(out=wf[:], in_=tmpq[:], axis=AX.X, op=ALU.add), reads=RB, writes=RB)
        f.op("dve", lambda e: e.tensor_copy(posA_i[:], pAf[:]), reads=RB, writes=RB)
        f.op("dve", lambda e: e.tensor_copy(posB_i[:], pBf[:]), reads=RB, writes=RB)
        f.op("dve", lambda e: e.tensor_tensor(cmpj[:], sinc[:].unsqueeze(1).broadcast_to([128, NSO, 32]), jt[:, 0:NSO].unsqueeze(2).broadcast_to([128, NSO, 32]), ALU.is_le), reads=RB, writes=RB)
        f.op("dve", lambda e: e.tensor_reduce(out=eidf[:], in_=cmpj[:], axis=AX.X, op=ALU.add), reads=RB, writes=RB)
        f.op("dve", lambda e: e.tensor_scalar(out=eidf[:], in0=eidf[:], scalar1=32.0, scalar2=512.0, op0=ALU.min, op1=ALU.mult), reads=RB, writes=RB)
        f.op("dve", lambda e: e.tensor_scalar(out=eidf[:], in0=eidf[:], scalar1=float(layer * 16384), scalar2=None, op0=ALU.add), reads=RB, writes=RB)
        f.op("dve", lambda e: e.tensor_tensor(idxf[:], eidf[:].unsqueeze(2).broadcast_to([128, NSO, 4]), pc[:].unsqueeze(1).broadcast_to([128, NSO, 4]), ALU.add), reads=RB, writes=RB)
        f.op("dve", lambda e: e.tensor_copy(idxw[:], idxf[:]), reads=RB, writes=RB)
        for j in range(ng):
            for pi_ in (posA_i, posB_i):
                f._dma_common("pool", lambda e, j=j, pi_=pi_: e.indirect_dma_start(out=XS, out_offset=IOA(ap=pi_[:, j:j + 1], axis=0), in_=hb_all[:, j, :], in_offset=None),
                              [R_hb[j]] + RB + R_XsZ, [R_XsW[j]])
        Bq.close()
        HB.close()
        Sd = Scope(nc)
        wg = [Sd.sb("wg%d" % k, [128, 4, 2, 512], BF16) for k in range(2)]
        wu = [Sd.sb("wu%d" % k, [128, 4, 2, 512], BF16) for k in range(2)]
        wd = [Sd.sb("wd%d" % k, [128, 4, D], BF16) for k in range(2)]
        R_w = RL(2, "w"); R_wd = RL(2, "wd")
        xs = [[Sd.sb("xs%d_%d" % (a_, tt), [128, D], BF16) for tt in range(4)] for a_ in range(2)]
        R_xs = [RL(4, "xs%d" % a_) for a_ in range(2)]

        def xs_load(jn):
            for tt in range(4):
                r0 = (jn * 4 + tt) * 128
                f.dma("sp", xs[jn % 2][tt][:], XS[r0:r0 + 128, :], reads=R_XsW, writes=[R_xs[jn % 2][tt]])
        xT = [Sd.sb("xT%d" % k, [128, 8, 512], BF16) for k in range(2)]; R_xT = RL(2)
        ps_t = [Sd.ps("ps_t%d" % k, [128, 8, 128], BF16) for k in range(2)]; R_pst = RL(2)
        psg = [Sd.ps("psg%d" % k, [128, 512]) for k in range(2)]; R_psg = RL(2)
        psu = [Sd.ps("psu%d" % k, [128, 512]) for k in range(2)]; R_psu = RL(2)
        psd = [Sd.ps("psd%d" % k, [128, 512]) for k in range(2)]; R_psd = RL(2)
        sg = [Sd.sb("sg%d" % k, [128, 512]) for k in range(2)]; R_sg = RL(2)
        hid = [Sd.sb("hid%d" % k, [128, 4, 512], BF16) for k in range(2)]; R_hid = RL(2)
        ysb = [Sd.sb("ysb%d" % k, [128, D]) for k in range(2)]; R_ysb = [RL(2, "ysb%d" % k) for k in range(2)]
        nfc = 0; nx = 0; ny = 0
        bc_reg = nc.gpsimd.alloc_register("bc%d" % layer)
        nc.gpsimd.reg_mov(bc_reg, 16383 + layer * 16384)
        stg_g = Sd.sb("stg_g", [128, 4, 2, 512]); stg_u = Sd.sb("stg_u", [128, 4, 2, 512])
        R_sg_ = Res("stg_g"); R_su_ = Res("stg_u")

        def w_load(ex):
            f.dma("sp", stg_g[:], w_gate[layer, ex].rearrange("(c q j) n -> q c j n", c=4, j=2), writes=[R_sg_])
            f.dma("sp", stg_u[:], w_up[layer, ex].rearrange("(c q j) n -> q c j n", c=4, j=2), writes=[R_su_])
            f.dma("pool", wd[ex % 2][:], w_down[layer, ex].rearrange("(c p) n -> p c n", p=128), writes=[R_wd[ex % 2]])

        def w_cast(ex):
            wb_ = ex % 2
            f.op("act", lambda e: e.activation(out=wg[wb_][:], in_=stg_g[:], func=AF.Identity), reads=[R_sg_], writes=[R_w[wb_]])
            f.op("dve", lambda e: e.tensor_copy(wu[wb_][:], stg_u[:]), reads=[R_su_], writes=[R_w[wb_]])
        xs_load(0)
        w_load(0)
        w_cast(0)
        for j in range(NS):
            wb = j % 2
            if j + 1 < NS:
                xs_load(j + 1)
            if j + 1 < 32:
                w_load(j + 1)
            if j >= 32:
                jo = j - 32
                for c in range(4):
                    for (wt_, rows_) in ((wg, wg_rows), (wu, wu_rows)):
                        f._dma_common("pool", lambda e, c=c, wt_=wt_, rows_=rows_: e.indirect_dma_start(out=wt_[wb][:, c, :, :].rearrange("p a n -> p (a n)"), out_offset=None, in_=rows_[layer],
                                                                                                   in_offset=IOA(ap=idxw[:, jo, c:c + 1], axis=0), bounds_check=bc_reg, oob_is_err=False),
                                      RB, [R_w[wb]])
                for c in range(4):
                    f._dma_common("pool", lambda e, c=c: e.indirect_dma_start(out=wd[wb][:, c, :], out_offset=None, in_=wd_rows[layer], in_offset=IOA(ap=idxw[:, jo, c:c + 1], axis=0), bounds_check=bc_reg, oob_is_err=False),
                                  RB, [R_wd[wb]])
            for tt in range(4):
                for kc in range(8):
                    f.op("pe", lambda e, kc=kc, tt=tt: e.transpose(ps_t[tt % 2][:, kc, :], xs[j % 2][tt][:, kc * 128:(kc + 1) * 128], identb[:]), reads=[R_xs[j % 2][tt], R_identb], writes=[R_pst[tt % 2]], acc=(kc > 0))
                f.op("dve", lambda e, tt=tt: e.tensor_copy(xT[wb][:, :, tt * 128:(tt + 1) * 128], ps_t[tt % 2][:]), reads=[R_pst[tt % 2]], writes=[R_xT[wb]])
            hb_ = j % 2
            for fc in range(4):
                pb = nfc % 2; nfc += 1
                for kc in range(8):
                    f.op("pe", lambda e, kc=kc, fc=fc: e.matmul(psg[pb][:], wg[wb][:, kc // 2, kc % 2, fc * 128:(fc + 1) * 128], xT[wb][:, kc, :], start=(kc == 0), stop=(kc == 7)),
                         reads=[R_w[wb], R_xT[wb]], writes=[R_psg[pb]], acc=(kc > 0))
                for kc in range(8):
                    f.op("pe", lambda e, kc=kc, fc=fc: e.matmul(psu[pb][:], wu[wb][:, kc // 2, kc % 2, fc * 128:(fc + 1) * 128], xT[wb][:, kc, :], start=(kc == 0), stop=(kc == 7)),
                         reads=[R_w[wb], R_xT[wb]], writes=[R_psu[pb]], acc=(kc > 0))
                f.op("act", lambda e: e.activation(out=sg[pb][:], in_=psg[pb][:], func=AF.Silu), reads=[R_psg[pb]], writes=[R_sg[pb]])
                f.op("dve", lambda e, fc=fc: e.tensor_tensor(hid[hb_][:, fc, :], sg[pb][:], psu[pb][:], ALU.mult), reads=[R_sg[pb], R_psu[pb]], writes=[R_hid[hb_]])
            for tt in range(4):
                yb_ = ny % 2; ny += 1
                for half in range(2):
                    for fc in range(4):
                        f.op("pe", lambda e, fc=fc, half=half, tt=tt: e.matmul(psd[half][:], hid[hb_][:, fc, tt * 128:(tt + 1) * 128], wd[wb][:, fc, half * 512:(half + 1) * 512], start=(fc == 0), stop=(fc == 3)),
                             reads=[R_wd[wb], R_hid[hb_]], writes=[R_psd[half]], acc=(fc > 0))
                    if half == 0:
                        f.op("dve", lambda e: e.tensor_copy(ysb[yb_][:, 0:512], psd[0][:]), reads=[R_psd[0]], writes=[R_ysb[yb_][0]])
                    else:
                        f.op("act", lambda e: e.activation(out=ysb[yb_][:, 512:1024], in_=psd[1][:], func=AF.Identity), reads=[R_psd[1]], writes=[R_ysb[yb_][1]])
                r0 = (j * 4 + tt) * 128
                f.dma("act", YS[r0:r0 + 128, :], ysb[yb_][:], reads=R_ysb[yb_], writes=[R_Ys[j]])
            if j + 1 < 32:
                w_cast(j + 1)
        Sd.close()
        nc.gpsimd.free_register(bc_reg)
        C = Scope(nc)
        xt = [C.sb("xt%d" % k, [128, D]) for k in range(2)]; R_xt = RL(2)
        ot = [C.sb("ot%d" % k, [128, D]) for k in range(2)]; R_ot = RL(2)
        ya = [C.sb("ya%d" % k, [128, D]) for k in range(2)]; R_ya = RL(2)
        yb2 = [C.sb("yb%d" % k, [128, D]) for k in range(2)]; R_yb = RL(2)
        tmp = C.sb("tmp", [128, D]); R_tmp = Res()
        small = C.sb("small", [128, 16]); R_small = Res()

        class _V:
            def __init__(self, ap): self.ap = ap
            def __getitem__(self, k): return self.ap
        for j, t in enumerate(tiles_all):
            b = j % 2
            f.dma("sp", xt[b][:], XR[t * 128:(t + 1) * 128, :], reads=[R_XR[t]], writes=[R_xt[b]])
            f._dma_common("pool", lambda e: e.indirect_dma_start(out=ya[b][:], out_offset=None, in_=YS, in_offset=IOA(ap=posA_i[:, j:j + 1], axis=0)), R_Ys + RB, [R_ya[b]])
            f._dma_common("pool", lambda e: e.indirect_dma_start(out=yb2[b][:], out_offset=None, in_=YS, in_offset=IOA(ap=posB_i[:, j:j + 1], axis=0)), R_Ys + RB, [R_yb[b]])
            f.op("dve", lambda e: e.tensor_scalar(out=ya[b][:], in0=ya[b][:], scalar1=wA[:, j:j + 1], scalar2=None, op0=ALU.mult), reads=[R_ya[b]] + RB, writes=[R_ya[b]])
            f.op("dve", lambda e: e.scalar_tensor_tensor(out=ya[b][:], in0=yb2[b][:], scalar=wB[:, j:j + 1], in1=ya[b][:], op0=ALU.mult, op1=ALU.add), reads=[R_yb[b], R_ya[b]] + RB, writes=[R_ya[b]])
            resid_ln(C, xt[b], R_xt[b], _V(ya[b][:]), R_ya[b], (1 if t < 2 else 0), layer * 2 + 1, ot[b], R_ot[b], tmp, R_tmp, small, R_small)
            if final:
                f.dma("act", out_d[(t - 2) * 128:(t - 1) * 128, :], ot[b][:], reads=[R_ot[b]], writes=[R_out])
            else:
                f.dma("act", XR[t * 128:(t + 1) * 128, :], ot[b][:], reads=[R_ot[b]], writes=[R_XR[t]])
        C.close()
        P.close()

    def layer1_mixer():
        L = Scope(nc)
        kT2 = L.sb("kT2b", [128, 4, NTOK], BF16); R_kT = RL(NT, "kT")
        vaug = L.sb("vaugb", [128, NT, 576], BF16); R_v = RL(NT, "v")
        f.op("pool", lambda e: e.memset(vaug[:], 1.0), writes=R_v)
        R_oT = RL(NT, "oT")
        R_QT = RL(NT, "QT")
        S = Scope(nc)
        win = S.sb("win1", [128, 8, 1536], BF16); R_win = Res()
        f.dma("pool", win[:], odd_w_in.rearrange("(kc p) n -> p kc n", p=128), writes=[R_win])
        gq = S.sb("gq", [128, 2, 64]); R_gq = Res()
        f.dma("sp", gq[:, 0, :], q_norm.partition_broadcast(128), writes=[R_gq])
        f.dma("sp", gq[:, 1, :], k_norm.partition_broadcast(128), writes=[R_gq])
        xt = [S.sb("xt%d" % k, [128, D]) for k in range(2)]; R_xt = RL(2)
        h32 = S.sb("h32", [128, D]); R_h32 = Res()
        hT = [S.sb("hT%d" % k, [128, 8, 128], BF16) for k in range(2)]; R_hT = RL(2)
        ps_tp = S.ps("ps_tp", [128, 8, 128]); R_pstp = Res(x=True)
        ps_q = S.ps("ps_q", [128, 1536]); R_psq = Res(x=True)
        ps_t = S.ps("ps_t", [128, 16, 128], BF16); R_pst = Res(x=True)
        qk = S.sb("qk", [128, 20, 64]); R_qk = Res()
        sq = S.sb("sq", [128, 20, 64]); ss = S.sb("ss", [128, 20]); R_ss = Res()
        ra = S.sb("ra", [128, 20, 32]); rb = S.sb("rb", [128, 20, 32]); R_ra = Res(); R_rb = Res()
        tqk = S.sb("tqk", [128, 20, 64], BF16); R_tqk = Res()
        kd = S.sb("kd", [128, 4, 2, 64], BF16); R_kd = Res()
        qts = [S.sb("qts%d" % k, [128, 8, 128], BF16) for k in range(2)]; R_qts = RL(2)
        for t in range(NT):
            b = t % 2
            f.dma("sp", xt[b][:], XR[t * 128:(t + 1) * 128, :], reads=[R_XR[t]], writes=[R_xt[b]])
            which = 1 if t < 2 else 0
            mod_transpose(xt[b], R_xt[b], which, h32, R_h32, ps_tp, R_pstp, hT[b][:], R_hT[b])
            cols = slice(t * 128, (t + 1) * 128)
            lat = t >= 2
            ranges = ((0, 512), (512, 1024), (1024, 1536)) if lat else ((1024, 1536),)
            first = True
            for (n0, n1) in ranges:
                for kc in range(8):
                    f.op("pe", lambda e, kc=kc, n0=n0, n1=n1: e.matmul(ps_q[:, n0:n1], hT[b][:, kc, :], win[:, kc, n0:n1], start=(kc == 0), stop=(kc == 7)),
                         reads=[R_win, R_hT[b]], writes=[R_psq], acc=(not first))
                    first = False
            h0 = 0 if lat else 16
            nh = 20 - h0
            pv = ps_q[:, h0 * 64:1280].rearrange("p (h d) -> p h d", d=64)
            qkv_ = qk[:, h0:20, :]
            f.op("act", lambda e: e.activation(out=sq[:, h0:20, :], in_=pv, func=AF.Square), reads=[R_psq], writes=[R_ss])
            f.op("dve", lambda e: e.tensor_reduce(out=ss[:, h0:20], in_=sq[:, h0:20, :], axis=AX.X, op=ALU.add), reads=[R_ss], writes=[R_ss])
            f.op("dve", lambda e: e.tensor_scalar(out=ss[:, h0:20], in0=ss[:, h0:20], scalar1=1.0 / 64.0, scalar2=RMS_EPS, op0=ALU.mult, op1=ALU.add), reads=[R_ss], writes=[R_ss])
            f.op("act", lambda e: e.activation(out=ss[:, h0:20], in_=ss[:, h0:20], func=AF.Sqrt), reads=[R_ss], writes=[R_ss])
            f.op("dve", lambda e: e.reciprocal(ss[:, h0:20], ss[:, h0:20]), reads=[R_ss], writes=[R_ss])
            f.op("dve", lambda e: e.tensor_tensor(qkv_, pv, ss[:, h0:20].unsqueeze(2).broadcast_to([128, nh, 64]), ALU.mult), reads=[R_psq, R_ss], writes=[R_qk])
            if lat:
                f.op("pool", lambda e: e.tensor_tensor(qk[:, 0:16, :], qk[:, 0:16, :], gq[:, 0, :].unsqueeze(1).broadcast_to([128, 16, 64]), ALU.mult), reads=[R_qk, R_gq], writes=[R_qk])
            f.op("pool", lambda e: e.tensor_tensor(qk[:, 16:20, :], qk[:, 16:20, :], gq[:, 1, :].unsqueeze(1).broadcast_to([128, 4, 64]), ALU.mult), reads=[R_qk, R_gq], writes=[R_qk])
            if lat:
                q4 = qk[:].rearrange("p h (two f) -> p h two f", two=2)
                o4 = tqk[:].rearrange("p h (two f) -> p h two f", two=2)
                cosb = rope[:, 0, t - 2, :].unsqueeze(1).broadcast_to([128, 20, 32])
                sinb = rope[:, 1, t - 2, :].unsqueeze(1).broadcast_to([128, 20, 32])
                f.op("dve", lambda e: e.tensor_tensor(ra[:], q4[:, :, 0, :], cosb, ALU.mult), reads=[R_qk, R_rope], writes=[R_ra])
                f.op("pool", lambda e: e.tensor_tensor(rb[:], q4[:, :, 1, :], sinb, ALU.mult), reads=[R_qk, R_rope], writes=[R_rb])
                f.op("dve", lambda e: e.tensor_tensor(o4[:, :, 0, :], ra[:], rb[:], ALU.subtract), reads=[R_ra, R_rb], writes=[R_tqk])
                f.op("dve", lambda e: e.tensor_tensor(ra[:], q4[:, :, 1, :], cosb, ALU.mult), reads=[R_qk, R_rope, R_tqk], writes=[R_ra])
                f.op("pool", lambda e: e.tensor_tensor(rb[:], q4[:, :, 0, :], sinb, ALU.mult), reads=[R_qk, R_rope, R_tqk], writes=[R_rb])
                f.op("dve", lambda e: e.tensor_tensor(o4[:, :, 1, :], ra[:], rb[:], ALU.add), reads=[R_ra, R_rb], writes=[R_tqk])
            else:
                f.op("dve", lambda e: e.tensor_copy(tqk[:, 16:20, :], qk[:, 16:20, :]), reads=[R_qk], writes=[R_tqk])
            for a in range(4):
                f.op("dve", lambda e, a=a: e.tensor_copy(vaug[:, t, 64 + 128 * a:128 + 128 * a], ps_q[:, 1280 + 64 * a:1344 + 64 * a]), reads=[R_psq], writes=[R_v[t]])
            f.op("dve", lambda e: e.tensor_copy(kd[:, :, 0, :], tqk[:, 16:20, :]), reads=[R_tqk], writes=[R_kd])
            f.op("dve", lambda e: e.tensor_copy(kd[:, :, 1, :], tqk[:, 16:20, :]), reads=[R_tqk], writes=[R_kd])
            firstt = True
            if lat:
                for pr in range(8):
                    f.op("pe", lambda e, pr=pr: e.transpose(ps_t[:, pr, :], tqk[:, 2 * pr:2 * pr + 2, :].rearrange("p a d -> p (a d)"), identb[:]),
                         reads=[R_tqk, R_identb], writes=[R_pst], acc=(not firstt))
                    firstt = False
            for a in range(4):
                f.op("pe", lambda e, a=a: e.transpose(ps_t[:, 8 + a, :], kd[:, a, :, :].rearrange("p a d -> p (a d)"), identb[:]),
                     reads=[R_kd, R_identb], writes=[R_pst], acc=(not firstt))
                firstt = False
            f.op("act", lambda e: e.activation(out=kT2[:, :, cols], in_=ps_t[:, 8:12, :], func=AF.Identity), reads=[R_pst], writes=[R_kT[t]])
            if lat:
                f.op("dve", lambda e: e.tensor_copy(qts[b][:], ps_t[:, 0:8, :]), reads=[R_pst], writes=[R_qts[b]])
                f.dma("act", QT[:, :, (t - 2) * 128:(t - 1) * 128].rearrange("a p n -> p a n"), qts[b][:], reads=[R_qts[b]], writes=[R_QT[t]])
        S.close()
        S = Scope(nc)
        qb = [S.sb("qb%d" % k, [128, 2, 512], BF16) for k in range(2)]; R_qb = RL(2)
        ps_s = [S.ps("ps_s%d" % k, [128, 1024]) for k in range(2)]; R_pss = RL(2)
        ps_o = [S.ps("ps_o%d" % k, [128, 512]) for k in range(4)]; R_pso = RL(4)
        pT = [S.sb("pT%d" % k, [128, 1024], BF16) for k in range(3)]; R_pT = RL(3)
        dtmp = S.sb("dtmp", [128, 512]); R_dt = Res()
        ost = [S.sb("ost%d" % k, [128, 2, 512], BF16) for k in range(2)]; R_ost = RL(2)
        it = 0
        nq = 0
        for kvh in range(4):
            for qblk in range(8):
                qbi = nq % 2; nq += 1
                tq = [R_QT[2 + qblk * 4 + k] for k in range(4)]
                f.dma("sp", qb[qbi][:], QT[2 * kvh:2 * kvh + 2, :, qblk * 512:(qblk + 1) * 512].rearrange("a p n -> p a n"), reads=tq, writes=[R_qb[qbi]])
                items = [(kt, p_) for kt in range(NT) for p_ in range(2)]
                it0 = it; it += len(items)

                def front(idx):
                    kt, p_ = items[idx]
                    si = (it0 + idx) % 2
                    pi_ = (it0 + idx) % 3
                    for half in range(2):
                        base = 64 * half
                        f.op("pe", lambda e, half=half, base=base: e.matmul(ps_s[si][:, half * 512:(half + 1) * 512], kT2[base:base + 64, kvh, kt * 128:(kt + 1) * 128], qb[qbi][base:base + 64, p_, :], start=True, stop=True),
                             reads=[R_kT[kt], R_qb[qbi]], writes=[R_pss[si]], acc=(half > 0))
                    f.op("act", lambda e: e.activation(out=pT[pi_][:], in_=ps_s[si][:], func=AF.Exp, scale=0.125), reads=[R_pss[si]], writes=[R_pT[pi_]])

                def back(idx):
                    kt, p_ = items[idx]
                    pi_ = (it0 + idx) % 3
                    for half in range(2):
                        hh = 2 * p_ + half
                        voff = (64 if half == 0 else 0) + 128 * kvh
                        f.op("pe", lambda e, half=half, hh=hh, voff=voff: e.matmul(ps_o[hh][:], vaug[:, kt, voff:voff + 128], pT[pi_][:, half * 512:(half + 1) * 512], start=(kt == 0), stop=(kt == NT - 1)),
                             reads=[R_v[kt], R_pT[pi_]], writes=[R_pso[hh]], acc=(kt > 0))
                LA = 1
                for i_ in range(len(items) + LA):
                    if i_ < len(items):
                        front(i_)
                    if i_ >= LA:
                        back(i_ - LA)
                for hh in range(4):
                    h = kvh * 4 + hh
                    nb, db = (0, 64) if h % 2 == 0 else (64, 0)
                    f.op("dve", lambda e: e.reciprocal(dtmp[nb:nb + 64, :], ps_o[hh][db:db + 64, :]), reads=[R_pso[hh]], writes=[R_dt])
                    f.op("dve", lambda e: e.tensor_tensor(ost[qbi][nb:nb + 64, hh // 2, :], ps_o[hh][nb:nb + 64, :], dtmp[nb:nb + 64, :], ALU.mult),
                         reads=[R_pso[hh], R_dt], writes=[R_ost[qbi]])
                f.dma("act", OT[2 * kvh:2 * kvh + 2, :, qblk * 512:(qblk + 1) * 512].rearrange("a p n -> p a n"), ost[qbi][:], reads=[R_ost[qbi]],
                      writes=[R_oT[2 + qblk * 4 + k] for k in range(4)])
        S.close()
        L.close()
        M = Scope(nc)
        mixt = [M.sb("mixt%d" % k, [128, 8, 128], BF16) for k in range(2)]; R_mixt = RL(2)

        def mix_loader(t, b):
            f.dma("sp", mixt[b][:], OT[:, :, (t - 2) * 128:(t - 1) * 128].rearrange("a p n -> p a n"), reads=[R_oT[t]], writes=[R_mixt[b]])
            return [mixt[b][:, k, :] for k in range(8)], [R_mixt[b]]
        out_phase(1, odd_w_out, mix_loader, range(2, NT))
        M.close()

    phase_mod(0, 0)
    if stop_after == "mod":
        f.dma("sp", dbg[0:128, :], mod[:, 0].rearrange("p a d -> p (a d)"), reads=[R_mod], writes=[R_dbg])
        f.dma("sp", dbg[128:256, :], mod[:, 1].rearrange("p a d -> p (a d)"), reads=[R_mod], writes=[R_dbg])
    else:
        layer0_mixer()
    if stop_after in ("in0", "s5", "mod", "h0", "qkv", "win"):
        pass
    else:
        if stop_after == "mix0":
            pass
        else:
            phase_mod(0, 1)
            (ffn_phase if 'dense' in DBG_SKIP else ffn_sparse)(0, list(range(NT)), final=False)
            if stop_after != "l0":
                phase_mod(1, 0)
                layer1_mixer()
                if stop_after != "mix1":
                    phase_mod(1, 1)
                    (ffn_phase if 'dense' in DBG_SKIP else ffn_sparse)(1, list(range(2, NT)), final=True)
    if stop_after is not None and stop_after not in ("in0", "s5", "mod", "h0", "qkv", "win"):
        S = Scope(nc)
        tt = S.sb("dumpt", [128, D]); R_t = Res()
        for t in range(NT):
            f.dma("sp", tt[:], XR[t * 128:(t + 1) * 128, :], reads=[R_XR[t]], writes=[R_t])
            f.dma("sp", dbg[t * 128:(t + 1) * 128, :], tt[:], reads=[R_t], writes=[R_dbg])
        S.close()
    f.finish()
    Scope.FWREF = None
    G.close()
    f.close()
    return nc


_CONST = None


def _consts():
    global _CONST
    if _CONST is None:
        ident = np.eye(128, dtype=np.float32)
        n_freq = 16
        inv_freq = (10000.0 ** (-np.arange(n_freq, dtype=np.float32) / n_freq)).astype(np.float32)
        pos = np.arange(4096)
        r = (pos // 64).astype(np.float32); cc = (pos % 64).astype(np.float32)
        ang = np.concatenate([r[:, None] * inv_freq, cc[:, None] * inv_freq], -1).astype(np.float32)
        cos = np.cos(ang).astype(np.float32).reshape(32, 128, 32).transpose(1, 0, 2)
        sin = np.sin(ang).astype(np.float32).reshape(32, 128, 32).transpose(1, 0, 2)
        rope = np.ascontiguousarray(np.stack([cos, sin], axis=1))
        k = np.arange(128)[:, None]; q = np.arange(128)[None, :]
        mask = np.stack([(q <= k), (k <= q), (k < q)], axis=1).astype(np.float32)
        pc = (np.arange(128, dtype=np.float32)[:, None] + 128.0 * np.arange(4, dtype=np.float32)[None, :]).astype(np.float32)
        jidx = np.broadcast_to(np.arange(128, dtype=np.float32)[None, :], (128, 128)).copy()
        _CONST = {"k_ident": ident, "k_rope": rope, "k_mask": np.ascontiguousarray(mask), "k_jidx": jidx, "k_pc": np.ascontiguousarray(pc)}
    return _CONST


def make_in_map(inputs, b):
    f32 = lambda a: np.ascontiguousarray(np.asarray(a, dtype=np.float32))
    m = {
        "x": f32(inputs["x"][b]), "ctx": f32(inputs["ctx"][b]), "c": f32(inputs["c"][b:b + 1]),
        "c_ctx": f32(inputs["c_ctx"]).reshape(1, D),
        "ada_w": f32(inputs["ada_w"]), "ada_b": f32(inputs["ada_b"]), "ln_g": f32(inputs["ln_g"]), "ln_b": f32(inputs["ln_b"]),
        "even_w_in": f32(inputs["even_w_in"][0]), "even_w_out": f32(inputs["even_w_out"][0]),
        "s5_lam_re": f32(inputs["s5_lam_re"][0]), "s5_lam_im": f32(inputs["s5_lam_im"][0]), "s5_log_step": f32(inputs["s5_log_step"][0]),
        "s5_b_re": f32(inputs["s5_b_re"][0]), "s5_b_im": f32(inputs["s5_b_im"][0]),
        "s5_c_re": f32(inputs["s5_c_re"][0]), "s5_c_im": f32(inputs["s5_c_im"][0]),
        "s5_d": f32(inputs["s5_d"][0]), "s5_w_glu": f32(inputs["s5_w_glu"][0]), "s5_b_glu": f32(inputs["s5_b_glu"][0]),
        "win_sink": f32(inputs["win_sink"][0]),
        "odd_w_in": f32(inputs["odd_w_in"][0]), "odd_w_out": f32(inputs["odd_w_out"][0]),
        "odd_q_norm": f32(inputs["odd_q_norm"][0]), "odd_k_norm": f32(inputs["odd_k_norm"][0]),
        "router_w": f32(inputs["router_w"]), "router_bias": f32(inputs["router_bias"]),
        "moe_w_gate": f32(inputs["moe_w_gate"]), "moe_w_up": f32(inputs["moe_w_up"]), "moe_w_down": f32(inputs["moe_w_down"]),
    }
    m.update(_consts())
    return m


def kernel(**inputs):
    nc = build_program()
    shared = make_in_map(inputs, 0)
    in_maps = []
    for b in range(8):
        m = dict(shared)
        m["x"] = np.ascontiguousarray(np.asarray(inputs["x"][b], dtype=np.float32))
        m["ctx"] = np.ascontiguousarray(np.asarray(inputs["ctx"][b], dtype=np.float32))
        m["c"] = np.ascontiguousarray(np.asarray(inputs["c"][b:b + 1], dtype=np.float32))
        in_maps.append(m)
    res = run_bass_kernel_spmd(nc, in_maps, core_ids=list(range(8)))
    return np.stack([np.asarray(r["out"], dtype=np.float32) for r in res.results], axis=0)
```

```python
import math
import os
DBG_SKIP = os.environ.get('DBG_SKIP', '').split(',')
DBG_NT = int(os.environ.get('DBG_NT', '34'))
from contextlib import ExitStack
import numpy as np
import ml_dtypes
import concourse.bass as bass
import concourse.mybir as mybir
from concourse.bass_utils import run_bass_kernel_spmd

F32 = mybir.dt.float32
BF16 = mybir.dt.bfloat16
I32 = mybir.dt.int32
ALU = mybir.AluOpType
AF = mybir.ActivationFunctionType
AX = mybir.AxisListType

SEM_LIMIT = 30000
NT = 34
NTOK = 4352
D = 1024
ALPHA = 4.0 ** 0.25
LN_EPS = 1e-5
RMS_EPS = 1e-6
TWO_PI = 2.0 * math.pi
CW1 = 6.28125
CW2 = TWO_PI - CW1


class Res:
    __slots__ = ("name", "w", "r", "x")

    def __init__(self, name="", x=False):
        self.name = name
        self.w = None
        self.r = []
        self.x = x


def RL(n, name="r"):
    return [Res("%s%d" % (name, i)) for i in range(n)]


class EngState:
    def __init__(self, fw, name, eng):
        self.fw = fw
        self.name = name
        self.eng = eng
        self.count = 0
        self.epoch = 0
        self.known = {}
        self._new_sem()

    def _new_sem(self):
        self.sem_key = "%s_e%d" % (self.name, self.epoch)
        self.sem = self.fw.new_sem(self.sem_key)
        self.count = 0
        self.epoch += 1


class FW:
    def __init__(self, nc, n_dma_sems=10):
        self.nc = nc
        self.es = ExitStack()
        self.sems = {}
        self.engs = {}
        for name, eng in (("pe", nc.tensor), ("act", nc.scalar), ("dve", nc.vector),
                          ("pool", nc.gpsimd), ("sp", nc.sync)):
            self.engs[name] = EngState(self, name, eng)
        self.dma_pool = {}
        for q in ("sp", "act", "pool"):
            lst = []
            for i in range(n_dma_sems):
                key = "dma_%s_%d" % (q, i)
                lst.append([key, self.new_sem(key), 0])
            self.dma_pool[q] = [lst, 0]
        self.n_instr = 0
        self.n_waits = 0

    def new_sem(self, key):
        s = self.es.enter_context(self.nc.semaphore(key))
        self.sems[key] = s
        return s

    def _wait(self, E, ev):
        if ev is None:
            return
        key, val = ev
        if E.known.get(key, 0) >= val:
            return
        E.eng.wait_ge(self.sems[key], val)
        E.known[key] = val
        self.n_waits += 1

    def _deps(self, E, reads, writes, acc=False):
        for r in reads:
            self._wait(E, r.w)
            if r.x:
                for ev in r.r:
                    if ev[0] != E.sem_key:
                        self._wait(E, ev)
        for w in writes:
            if not ((acc or E.name == "pe") and w.w is not None and w.w[0] == E.sem_key):
                self._wait(E, w.w)
            for ev in w.r:
                self._wait(E, ev)

    def _commit(self, ev, reads, writes):
        for r in reads:
            r.r.append(ev)
            if len(r.r) > 16:
                d = {}
                for k, v in r.r:
                    if d.get(k, 0) < v:
                        d[k] = v
                r.r = list(d.items())
        for w in writes:
            w.w = ev
            w.r = []

    def op(self, ename, fn, reads=(), writes=(), acc=False):
        E = self.engs[ename]
        if E.count >= SEM_LIMIT:
            E._new_sem()
        self._deps(E, reads, writes, acc=acc)
        ins = fn(E.eng)
        E.count += 1
        ins.then_inc(E.sem, 1)
        self._commit((E.sem_key, E.count), reads, writes)
        self.n_instr += 1
        return ins

    def _dma_common(self, qname, issue, reads, writes):
        E = self.engs[qname]
        pool, idx = self.dma_pool[qname]
        ent = pool[idx % len(pool)]
        self.dma_pool[qname][1] = idx + 1
        key, sem, val = ent
        if val > 0:
            self._wait(E, (key, val))
        if val + 16 > SEM_LIMIT:
            key = key + "n"
            sem = self.new_sem(key)
            val = 0
            ent[0], ent[1] = key, sem
        self._deps(E, reads, writes)
        ins = issue(E.eng)
        val += 16
        ent[2] = val
        ins.then_inc(sem, 16)
        ev = (key, val)
        self._commit(ev, reads, writes)
        self.n_instr += 1
        return ev

    def dma(self, qname, out, in_, reads=(), writes=(), **kw):
        return self._dma_common(qname, lambda e: e.dma_start(out=out, in_=in_, **kw), reads, writes)

    def barrier(self):
        evs = []
        for q in self.dma_pool:
            for key, sem, val in self.dma_pool[q][0]:
                if val > 0:
                    evs.append((key, val))
        for n, e in self.engs.items():
            if e.count > 0:
                evs.append((e.sem_key, e.count))
        for n, E in self.engs.items():
            for ev in evs:
                if ev[0] != E.sem_key:
                    self._wait(E, ev)

    def finish(self):
        E = self.engs["sp"]
        for q in self.dma_pool:
            for key, sem, val in self.dma_pool[q][0]:
                if val > 0:
                    self._wait(E, (key, val))
        for n, e in self.engs.items():
            if e.count > 0:
                self._wait(E, (e.sem_key, e.count))

    def close(self):
        self.es.close()


class Scope:
    FWREF = None

    def __init__(self, nc):
        self.nc = nc
        self.es = ExitStack()

    CNT = [0]

    def sb(self, name, shape, dtype=F32):
        Scope.CNT[0] += 1
        return self.es.enter_context(self.nc.sbuf_tensor("%s_%d" % (name, Scope.CNT[0]), list(shape), dtype))

    def ps(self, name, shape, dtype=F32):
        Scope.CNT[0] += 1
        return self.es.enter_context(self.nc.psum_tensor("%s_%d" % (name, Scope.CNT[0]), list(shape), dtype))

    def close(self):
        if Scope.FWREF is not None:
            Scope.FWREF.barrier()
        self.es.close()


def rev_ap(ap2d, n):
    last = ap2d[:, n - 1:n]
    return bass.AP(tensor=ap2d.tensor, offset=last.offset, ap=[list(ap2d.ap[0]), [-1, n]])


def build_program(stop_after=None, dbg_shape=None):
    nc = bass.Bass("TRN2", target_bir_lowering=False)

    def din(name, shape, dt=F32):
        return nc.dram_tensor(name, list(shape), dt, kind="ExternalInput").ap()

    x_d = din("x", [4096, D]); ctx_d = din("ctx", [256, D])
    c_d = din("c", [1, D]); cctx_d = din("c_ctx", [1, D])
    ada_w = din("ada_w", [2, D, 6 * D]); ada_b = din("ada_b", [2, 6 * D])
    ln_g = din("ln_g", [2, 2, D]); ln_b = din("ln_b", [2, 2, D])
    even_w_in = din("even_w_in", [D, 1280]); even_w_out = din("even_w_out", [D, D])
    lam_re = din("s5_lam_re", [2, 32, 64]); lam_im = din("s5_lam_im", [2, 32, 64])
    log_step = din("s5_log_step", [2, 32])
    b_re = din("s5_b_re", [2, 32, 64, 16]); b_im = din("s5_b_im", [2, 32, 64, 16])
    c_re = din("s5_c_re", [2, 32, 16, 64]); c_im = din("s5_c_im", [2, 32, 16, 64])
    s5_d = din("s5_d", [512]); w_glu = din("s5_w_glu", [512, 512]); b_glu = din("s5_b_glu", [512])
    win_sink = din("win_sink", [8])
    odd_w_in = din("odd_w_in", [D, 1536]); odd_w_out = din("odd_w_out", [D, D])
    q_norm = din("odd_q_norm", [64]); k_norm = din("odd_k_norm", [64])
    router_w = din("router_w", [D, 32]); router_b = din("router_bias", [32])
    w_gate = din("moe_w_gate", [2, 32, D, 512]); w_up = din("moe_w_up", [2, 32, D, 512])
    w_down = din("moe_w_down", [2, 32, 512, D])
    k_ident = din("k_ident", [128, 128]); k_rope = din("k_rope", [128, 2, 32, 32])
    k_mask = din("k_mask", [128, 3, 128]); k_jidx = din("k_jidx", [128, 128]); k_pc = din("k_pc", [128, 4])
    out_d = nc.dram_tensor("out", [4096, D], F32, kind="ExternalOutput").ap()
    XR = nc.dram_tensor("xr", [NTOK, D], F32, kind="Internal").ap()
    QT = nc.dram_tensor("qt_scr", [8, 128, 4096], BF16, kind="Internal").ap()
    OT = nc.dram_tensor("ot_scr", [8, 128, 4096], BF16, kind="Internal").ap()
    ATD = nc.dram_tensor("at_scr", [4, 128, NTOK], BF16, kind="Internal").ap()
    NS = 49
    XS = nc.dram_tensor("xs_scr", [NS * 512, D], BF16, kind="Internal").ap()
    YS = nc.dram_tensor("ys_scr", [NS * 512, D], F32, kind="Internal").ap()
    wg_all = w_gate.rearrange("l e (kk two) n -> (l e kk) (two n)", two=2)
    wu_all = w_up.rearrange("l e (kk two) n -> (l e kk) (two n)", two=2)
    wd_all = w_down.rearrange("l e f n -> (l e f) n")
    wg_rows = [wg_all, wg_all]; wu_rows = [wu_all, wu_all]; wd_rows = [wd_all, wd_all]
    dbg = None
    if dbg_shape is not None:
        dbg = nc.dram_tensor("dbg", list(dbg_shape), F32, kind="ExternalOutput").ap()

    f = FW(nc)
    Scope.FWREF = f
    G = Scope(nc)
    R_XR = RL(NT, "xr")
    R_out = Res("out")
    R_dbg = Res("dbg")

    ident = G.sb("ident", [128, 128]); R_ident = Res()
    identb = G.sb("identb", [128, 128], BF16); R_identb = Res()
    f.dma("sp", ident[:], k_ident, writes=[R_ident])
    f.op("dve", lambda e: e.tensor_copy(identb[:], ident[:]), reads=[R_ident], writes=[R_identb])
    rope = G.sb("rope", [128, 2, 32, 32]); R_rope = Res()
    f.dma("sp", rope[:], k_rope, writes=[R_rope])
    maskf = G.sb("maskf", [128, 3, 128]); maskb = G.sb("maskb", [128, 3, 128], BF16); R_mask = Res()
    f.dma("sp", maskf[:], k_mask, writes=[R_mask])
    f.op("dve", lambda e: e.tensor_copy(maskb[:], maskf[:]), reads=[R_mask], writes=[R_mask])
    R_crep = Res()
    ctmp = G.sb("ctmp", [128, 2, 8]); R_ctmp = Res()
    f.dma("sp", ctmp[:, 0, :], c_d.rearrange("o (kc p) -> p (o kc)", p=128), writes=[R_ctmp], allow_slow_non_contiguous=True)
    f.dma("sp", ctmp[:, 1, :], cctx_d.rearrange("o (kc p) -> p (o kc)", p=128), writes=[R_ctmp], allow_slow_non_contiguous=True)
    f.op("act", lambda e: e.activation(out=ctmp[:], in_=ctmp[:], func=AF.Silu), reads=[R_ctmp], writes=[R_ctmp])
    lng = G.sb("lng", [128, D]); lnb = G.sb("lnb", [128, D]); R_ln = Res()

    def load_ln(li):
        f.dma("sp", lng[:], ln_g[li // 2, li % 2].partition_broadcast(128), writes=[R_ln])
        f.dma("sp", lnb[:], ln_b[li // 2, li % 2].partition_broadcast(128), writes=[R_ln])
    epsc = G.sb("epsc", [128, 1]); R_eps = Res()
    f.op("dve", lambda e: e.memset(epsc[:], LN_EPS), writes=[R_eps])

    R_XsZ = RL(28, "xsz")
    ZS = Scope(nc)
    zt = ZS.sb("zt", [128, 7, D], BF16); R_zt = Res()
    f.op("pool", lambda e: e.memset(zt[:], 0.0), writes=[R_zt])
    for k in range(28):
        f.dma(("sp", "act")[k % 2], XS[k * 896:(k + 1) * 896, :].rearrange("(a p) d -> p a d", p=128), zt[:], reads=[R_zt], writes=[R_XsZ[k]])
    ZS.close()

    mod = G.sb("mod", [128, 2, 3, D]); R_mod = Res("mod")

    def dump(ap_sb, rows, cols, reads, r0=0, c0=0):
        f.dma("sp", dbg[r0:r0 + rows, c0:c0 + cols], ap_sb, reads=reads, writes=[R_dbg])

    def phase_mod(i, s):
        S = Scope(nc)
        crep = S.sb("crep", [128, 2, 8, 128])
        f.op("dve", lambda e: e.tensor_copy(crep[:], ctmp[:].unsqueeze(3).broadcast_to([128, 2, 8, 128])), reads=[R_ctmp], writes=[R_crep])
        slab = [S.sb("slab%d" % k, [128, 8, 512]) for k in range(2)]; R_slab = RL(2)
        adb = [S.sb("adb%d" % k, [128, 512]) for k in range(2)]; R_adb = RL(2)
        psm = [S.ps("psm%d" % k, [128, 512]) for k in range(2)]; R_psm = RL(2)
        n = 0
        for blk in range(6):
            c0 = s * 3072 + blk * 512
            bi = blk % 2
            f.dma("sp", slab[bi][:], ada_w[i, :, c0:c0 + 512].rearrange("(kc p) n -> p kc n", p=128), writes=[R_slab[bi]])
            f.dma("act", adb[bi][:], ada_b[i, c0:c0 + 512].partition_broadcast(128), writes=[R_adb[bi]])
            k, half = blk // 2, blk % 2
            for which in range(2):
                pi = n % 2; n += 1
                for kc in range(8):
                    f.op("pe", lambda e, kc=kc: e.matmul(psm[pi][:], crep[:, which, kc, :], slab[bi][:, kc, :], start=(kc == 0), stop=(kc == 7)),
                         reads=[R_crep, R_slab[bi]], writes=[R_psm[pi]], acc=(kc > 0))
                dst = mod[:, which, k, half * 512:(half + 1) * 512]
                f.op("dve", lambda e: e.scalar_tensor_tensor(out=dst, in0=psm[pi][:], scalar=(1.0 if k == 1 else 0.0), in1=adb[bi][:], op0=ALU.add, op1=ALU.add),
                     reads=[R_psm[pi], R_adb[bi]], writes=[R_mod])
        S.close()

    def resid_ln(S, xt, R_xt, o_ps, R_ops, which, li, out_t, R_outt, tmp, R_tmp, small, R_small):
        gate = mod[:, which, 2, :]
        f.op("dve", lambda e: e.tensor_tensor(tmp[:], o_ps[:], gate, ALU.mult), reads=[R_ops, R_mod], writes=[R_tmp])
        f.op("dve", lambda e: e.scalar_tensor_tensor(out=tmp[:], in0=xt[:], scalar=ALPHA, in1=tmp[:], op0=ALU.mult, op1=ALU.add),
             reads=[R_xt, R_tmp], writes=[R_tmp])
        f.op("dve", lambda e: e.bn_stats(small[:, 0:6], tmp[:, 0:512]), reads=[R_tmp], writes=[R_small])
        f.op("dve", lambda e: e.bn_stats(small[:, 6:12], tmp[:, 512:1024]), reads=[R_tmp], writes=[R_small])
        f.op("dve", lambda e: e.bn_aggr(small[:, 12:14], small[:, 0:12]), reads=[R_small], writes=[R_small])
        f.op("act", lambda e: e.activation(out=small[:, 14:15], in_=small[:, 13:14], func=AF.Sqrt, bias=epsc[:], scale=1.0), reads=[R_small, R_eps], writes=[R_small])
        f.op("dve", lambda e: e.reciprocal(small[:, 15:16], small[:, 14:15]), reads=[R_small], writes=[R_small])
        f.op("dve", lambda e: e.tensor_scalar(out=tmp[:], in0=tmp[:], scalar1=small[:, 12:13], scalar2=small[:, 15:16], op0=ALU.subtract, op1=ALU.mult),
             reads=[R_tmp, R_small], writes=[R_tmp])
        f.op("dve", lambda e: e.tensor_tensor(tmp[:], tmp[:], lng[:], ALU.mult), reads=[R_tmp, R_ln], writes=[R_tmp])
        f.op("dve", lambda e: e.tensor_tensor(out_t[:], tmp[:], lnb[:], ALU.add), reads=[R_tmp, R_ln], writes=[R_outt])

    def mod_transpose(xt, R_xt, which, h32, R_h32, ps_tp, R_pstp, hT_dst, R_hT, h32T=None, R_h32T=None):
        f.op("dve", lambda e: e.tensor_tensor(h32[:], xt[:], mod[:, which, 1, :], ALU.mult), reads=[R_xt, R_mod], writes=[R_h32])
        f.op("dve", lambda e: e.tensor_tensor(h32[:], h32[:], mod[:, which, 0, :], ALU.add), reads=[R_h32, R_mod], writes=[R_h32])
        for kc in range(8):
            f.op("pe", lambda e, kc=kc: e.transpose(ps_tp[:, kc, :], h32[:, kc * 128:(kc + 1) * 128], ident[:]),
                 reads=[R_h32, R_ident], writes=[R_pstp], acc=(kc > 0))
        f.op("act", lambda e: e.activation(out=hT_dst, in_=ps_tp[:], func=AF.Identity), reads=[R_pstp], writes=[R_hT])
        if h32T is not None:
            f.op("dve", lambda e: e.tensor_copy(h32T[:], ps_tp[:]), reads=[R_pstp], writes=[R_h32T])

    def src_tile(layer, t):
        if layer == 0:
            return (ctx_d[t * 128:(t + 1) * 128, :] if t < 2 else x_d[(t - 2) * 128:(t - 1) * 128, :]), []
        return XR[t * 128:(t + 1) * 128, :], [R_XR[t]]

    def layer0_mixer():
        L = Scope(nc)
        U = Scope(nc)
        uT = U.sb("uT", [128, 4, NTOK], BF16); R_uT = RL(NT, "uT")
        aT, R_aT = uT, R_uT

        def inproj(do_u, qT=None, R_qT=None, kT2=None, R_kT=None, vaug=None, R_v=None):
            S = Scope(nc)
            wc0, wc1 = (0, 512) if do_u else (512, 1280)
            win = S.sb("win", [128, 8, wc1 - wc0], BF16); R_win = Res()
            f.dma("pool", win[:], even_w_in[:, wc0:wc1].rearrange("(kc p) n -> p kc n", p=128), writes=[R_win])
            xt1 = S.sb("xt1", [128, D]); xt = [xt1, xt1]; R1_ = Res(); R_xt = [R1_, R1_]
            h32 = S.sb("h32", [128, D]); R_h32 = Res()
            hT = [S.sb("hT%d" % k, [128, 8, 128], BF16) for k in range(2)]; R_hT = RL(2)
            ps_tp = S.ps("ps_tp", [128, 8, 128]); R_pstp = Res(x=True)
            ps_u = S.ps("ps_u", [128, 4, 128]); R_psu = Res()
            ps_q = S.ps("ps_q", [128, 1024]); R_psq = Res(x=True)
            ps_t = S.ps("ps_t", [128, 8, 128], BF16); R_pst = Res(x=True)
            ra = S.sb("ra", [128, 10, 32]); rb = S.sb("rb", [128, 10, 32]); R_ra = Res(); R_rb = Res()
            tqk = S.sb("tqk", [128, 640], BF16); R_tqk = Res()
            kd = S.sb("kd", [128, 2, 2, 64], BF16); R_kd = Res()
            for t in range(NT if do_u else min(NT, DBG_NT)):
                b = t % 2
                src, rs = src_tile(0, t)
                f.dma("sp", xt[b][:], src, reads=rs, writes=[R_xt[b]])
                which = 1 if t < 2 else 0
                mod_transpose(xt[b], R_xt[b], which, h32, R_h32, ps_tp, R_pstp, hT[b][:], R_hT[b])
                cols = slice(t * 128, (t + 1) * 128)
                if stop_after == "h0" and t == 0:
                    f.dma("sp", dbg[0:128, :], h32[:], reads=[R_h32], writes=[R_dbg])
                    hf = S.sb("hf", [128, 1024]); R_hf = Res()
                    f.op("dve", lambda e: e.tensor_copy(hf[:], hT[b][:].rearrange("p a b -> p (a b)")), reads=[R_hT[b]], writes=[R_hf])
                    f.dma("sp", dbg[128:256, :], hf[:], reads=[R_hf], writes=[R_dbg])
                    f.op("dve", lambda e: e.tensor_copy(hf[:], win[:, 0, 0:1024]), reads=[R_win], writes=[R_hf])
                    f.dma("sp", dbg[256:384, :], hf[:], reads=[R_hf], writes=[R_dbg])
                    S.close(); return
                if do_u:
                    for ct in range(4):
                        for kc in range(8):
                            f.op("pe", lambda e, ct=ct, kc=kc: e.matmul(ps_u[:, ct, :], win[:, kc, ct * 128:(ct + 1) * 128], hT[b][:, kc, :], start=(kc == 0), stop=(kc == 7)),
                                 reads=[R_win, R_hT[b]], writes=[R_psu], acc=(ct + kc > 0))
                    f.op("act", lambda e: e.activation(out=uT[:, :, cols], in_=ps_u[:], func=AF.Identity), reads=[R_psu], writes=[R_uT[t]])
                    continue
                for (n0, n1) in ((0, 512), (512, 768)):
                    for kc in range(8):
                        f.op("pe", lambda e, kc=kc, n0=n0, n1=n1: e.matmul(ps_q[:, n0:n1], hT[b][:, kc, :], win[:, kc, n0:n1], start=(kc == 0), stop=(kc == 7)),
                             reads=[R_win, R_hT[b]], writes=[R_psq], acc=(n0 + kc > 0))
                if 'rope' in DBG_SKIP:
                    continue
                if t >= 2:
                    pv = ps_q[:, 0:640].rearrange("p (h two f) -> p h two f", two=2, f=32)
                    ov = tqk[:].rearrange("p (h two f) -> p h two f", two=2, f=32)
                    cosb = rope[:, 0, t - 2, :].unsqueeze(1).broadcast_to([128, 10, 32])
                    sinb = rope[:, 1, t - 2, :].unsqueeze(1).broadcast_to([128, 10, 32])
                    f.op("dve", lambda e: e.tensor_tensor(ra[:], pv[:, :, 0, :], cosb, ALU.mult), reads=[R_psq, R_rope], writes=[R_ra])
                    f.op("dve", lambda e: e.tensor_tensor(rb[:], pv[:, :, 1, :], sinb, ALU.mult), reads=[R_psq, R_rope], writes=[R_rb])
                    f.op("dve", lambda e: e.tensor_tensor(ov[:, :, 0, :], ra[:], rb[:], ALU.subtract), reads=[R_ra, R_rb], writes=[R_tqk])
                    f.op("dve", lambda e: e.tensor_tensor(ra[:], pv[:, :, 1, :], cosb, ALU.mult), reads=[R_psq, R_rope, R_tqk], writes=[R_ra])
                    f.op("dve", lambda e: e.tensor_tensor(rb[:], pv[:, :, 0, :], sinb, ALU.mult), reads=[R_psq, R_rope, R_tqk], writes=[R_rb])
                    f.op("dve", lambda e: e.tensor_tensor(ov[:, :, 1, :], ra[:], rb[:], ALU.add), reads=[R_ra, R_rb], writes=[R_tqk])
                else:
                    f.op("act", lambda e: e.activation(out=tqk[:], in_=ps_q[:, 0:640], func=AF.Identity), reads=[R_psq], writes=[R_tqk])
                if 'vaug' in DBG_SKIP:
                    continue
                for a in range(2):
                    if 'novaug' in DBG_SKIP:
                        break
                    f.op("dve", lambda e, a=a: e.tensor_copy(vaug[:, t, 64 + 128 * a:128 + 128 * a], ps_q[:, 640 + 64 * a:704 + 64 * a]),
                         reads=[R_psq], writes=[R_v[t]])
                if 'nokd' in DBG_SKIP:
                    continue
                kv = tqk[:, 512:640].rearrange("p (a d) -> p a d", a=2)
                f.op("dve", lambda e: e.tensor_copy(kd[:, :, 0, :], kv), reads=[R_tqk], writes=[R_kd])
                f.op("dve", lambda e: e.tensor_copy(kd[:, :, 1, :], kv), reads=[R_tqk], writes=[R_kd])
                if 'tr' in DBG_SKIP:
                    continue
                for pr in range(4):
                    f.op("pe", lambda e, pr=pr: e.transpose(ps_t[:, pr, :], tqk[:, pr * 128:(pr + 1) * 128], identb[:]),
                         reads=[R_tqk, R_identb], writes=[R_pst], acc=(pr > 0))
                for a in range(2):
                    f.op("pe", lambda e, a=a: e.transpose(ps_t[:, 4 + a, :], kd[:, a, :, :].rearrange("p a d -> p (a d)"), identb[:]),
                         reads=[R_kd, R_identb], writes=[R_pst], acc=True)
                f.op("dve", lambda e: e.tensor_copy(qT[:, :, cols], ps_t[:, 0:4, :]), reads=[R_pst], writes=[R_qT[t]])
                f.op("act", lambda e: e.activation(out=kT2[:, :, cols], in_=ps_t[:, 4:6, :], func=AF.Identity), reads=[R_pst], writes=[R_kT[t]])
            S.close()

        inproj(True)
        if stop_after == "h0":
            U.close(); L.close(); return
        if stop_after == "in0":
            S = Scope(nc)
            t32 = S.sb("t32", [128, 512]); R_t = Res()
            for ct in range(4):
                for blk in range(2):
                    f.op("dve", lambda e: e.tensor_copy(t32[:], uT[:, ct, blk * 512:(blk + 1) * 512]), reads=R_uT, writes=[R_t])
                    dump(t32[:], 128, 512, [R_t], r0=ct * 128, c0=blk * 512)
            S.close(); U.close(); L.close()
            return
        if 's5' not in DBG_SKIP:
            s5_phase(L, uT, R_uT, aT, R_aT)
        if stop_after == "s5":
            S = Scope(nc)
            t32 = S.sb("t32", [128, 512]); R_t = Res()
            for ct in range(4):
                for blk in range(9):
                    c0 = blk * 512; n = min(512, NTOK - c0)
                    f.op("dve", lambda e: e.tensor_copy(t32[:, 0:n], aT[:, ct, c0:c0 + n]), reads=R_aT, writes=[R_t])
                    dump(t32[:, 0:n], 128, n, [R_t], r0=ct * 128, c0=c0)
            S.close(); U.close(); L.close()
            return
        R_ATD = Res("atd")
        for k in range(4):
            f.dma(("sp", "act")[k % 2], ATD[k], aT[:, k, :], reads=R_aT, writes=[R_ATD])
        U.close()
        oT = L.sb("oT", [128, 4, NTOK], BF16); R_oT = RL(NT, "oT")
        W = Scope(nc)
        qT = W.sb("qT", [128, 4, NTOK], BF16); R_qT = RL(NT, "qT")
        kT2 = W.sb("kT2", [128, 2, NTOK], BF16); R_kT = RL(NT, "kT")
        vaug = W.sb("vaug", [128, NT, 320], BF16); R_v = RL(NT, "v")
        f.op("pool", lambda e: e.memset(vaug[:], 1.0), writes=R_v)
        inproj(False, qT, R_qT, kT2, R_kT, vaug, R_v)
        if stop_after == "qkv":
            W.close(); L.close(); return
        win_phase(qT, R_qT, kT2, R_kT, vaug, R_v, oT, R_oT)
        W.close()
        if stop_after == "win":
            L.close(); return

        M = Scope(nc)
        mixt = [M.sb("mixa%d" % k, [128, 4, 128], BF16) for k in range(2)]; R_mixt = RL(2)

        def mix_loader(t, b):
            c0 = t * 128
            f.dma("sp", mixt[b][:], ATD[:, :, c0:c0 + 128].rearrange("a p n -> p a n"), reads=[R_ATD], writes=[R_mixt[b]])
            return [mixt[b][:, k, :] for k in range(4)] + [oT[:, k, c0:c0 + 128] for k in range(4)], [R_mixt[b], R_oT[t]]
        out_phase(0, even_w_out, mix_loader, range(NT))
        M.close()
        L.close()

    def sincos(S, ang, n, out_s, out_c, R, tag):
        ki = S.sb("ki_" + tag, [128, n], I32); kf = S.sb("kf_" + tag, [128, n]); rd = S.sb("rd_" + tag, [128, n])
        f.op("dve", lambda e: e.tensor_scalar(out=ki[:], in0=ang, scalar1=1.0 / TWO_PI, scalar2=None, op0=ALU.mult), reads=[R], writes=[R])
        f.op("dve", lambda e: e.tensor_copy(kf[:], ki[:]), reads=[R], writes=[R])
        f.op("dve", lambda e: e.scalar_tensor_tensor(out=rd[:], in0=kf[:], scalar=-CW1, in1=ang, op0=ALU.mult, op1=ALU.add), reads=[R], writes=[R])
        f.op("dve", lambda e: e.scalar_tensor_tensor(out=rd[:], in0=kf[:], scalar=-CW2, in1=rd[:], op0=ALU.mult, op1=ALU.add), reads=[R], writes=[R])
        f.op("dve", lambda e: e.tensor_scalar(out=rd[:], in0=rd[:], scalar1=3.1415925, scalar2=-3.1415925, op0=ALU.min, op1=ALU.max), reads=[R], writes=[R])
        f.op("act", lambda e: e.activation(out=out_s, in_=rd[:], func=AF.Sin), reads=[R], writes=[R])
        f.op("dve", lambda e: e.scalar_tensor_tensor(out=rd[:], in0=rd[:], scalar=-1.0, in1=rd[:], op0=ALU.mult, op1=ALU.max), reads=[R], writes=[R])
        f.op("dve", lambda e: e.tensor_scalar(out=rd[:], in0=rd[:], scalar1=-1.0, scalar2=math.pi / 2, op0=ALU.mult, op1=ALU.add), reads=[R], writes=[R])
        f.op("act", lambda e: e.activation(out=out_c, in_=rd[:], func=AF.Sin), reads=[R], writes=[R])

    def s5_phase(L, uT, R_uT, aT, R_aT):
        P = Scope(nc)
        R = Res("s5setup")
        prm = P.sb("prm", [128, 16, 32])
        dsk = P.sb("dsk", [128, 4]); bgl = P.sb("bgl", [128, 4])
        cs2 = P.sb("cs2", [128, 32, 2]); ncs2 = P.sb("ncs2", [128, 32, 2])
        jt = P.sb("jt", [128, 128]); f.dma("sp", jt[:], k_jidx, writes=[R])
        f.dma("sp", dsk[:], s5_d.rearrange("(c p) -> p c", p=128), writes=[R], allow_slow_non_contiguous=True)
        f.dma("sp", bgl[:], b_glu.rearrange("(c p) -> p c", p=128), writes=[R], allow_slow_non_contiguous=True)
        S = Scope(nc)
        st32 = S.sb("st32", [32, 3, 128]); lsr = S.sb("lsr", [32, 2])
        f.dma("sp", st32[:, 0, :], lam_re.rearrange("d (q g) n -> (d q) (g n)", g=2), writes=[R])
        f.dma("sp", st32[:, 1, :], lam_im.rearrange("d (q g) n -> (d q) (g n)", g=2), writes=[R])
        f.dma("sp", lsr[:], log_step.rearrange("d (q g) -> (d q) g", g=2), writes=[R])
        f.op("dve", lambda e: e.tensor_copy(st32[:, 2, :].rearrange("p (g n) -> p g n", g=2), lsr[:].unsqueeze(2).broadcast_to([32, 2, 64])), reads=[R], writes=[R])
        pst = S.ps("pst", [128, 4, 128])
        for k in range(3):
            f.op("pe", lambda e, k=k: e.transpose(pst[:, k, 0:32], st32[:, k, :], ident[0:32, 0:32]), reads=[R, R_ident], writes=[R], acc=(k > 0))
        f.op("dve", lambda e: e.tensor_copy(prm[:, 0:3, :], pst[:, 0:3, 0:32]), reads=[R], writes=[R])
        lr, li = prm[:, 0, :], prm[:, 1, :]
        dt, th, rr = prm[:, 3, :], prm[:, 4, :], prm[:, 5, :]
        f.op("act", lambda e: e.activation(out=dt, in_=prm[:, 2, :], func=AF.Exp), reads=[R], writes=[R])
        f.op("dve", lambda e: e.tensor_tensor(th, li, dt, ALU.mult), reads=[R], writes=[R])
        f.op("dve", lambda e: e.tensor_tensor(prm[:, 10, :], lr, dt, ALU.mult), reads=[R], writes=[R])
        f.op("act", lambda e: e.activation(out=rr, in_=prm[:, 10, :], func=AF.Exp), reads=[R], writes=[R])
        f.op("dve", lambda e: e.tensor_scalar(out=prm[:, 10, :], in0=th, scalar1=128.0, scalar2=None, op0=ALU.mult), reads=[R], writes=[R])
        sincos(S, prm[:, 10, :], 32, prm[:, 7, :], prm[:, 6, :], R, "a")
        sincos(S, th, 32, prm[:, 12, :], prm[:, 11, :], R, "b")
        abre, abim, den, t1, t2 = prm[:, 13, :], prm[:, 14, :], prm[:, 15, :], prm[:, 10, :], prm[:, 2, :]
        f.op("dve", lambda e: e.tensor_tensor(abre, rr, prm[:, 11, :], ALU.mult), reads=[R], writes=[R])
        f.op("dve", lambda e: e.tensor_scalar(out=abre, in0=abre, scalar1=-1.0, scalar2=None, op0=ALU.add), reads=[R], writes=[R])
        f.op("dve", lambda e: e.tensor_tensor(abim, rr, prm[:, 12, :], ALU.mult), reads=[R], writes=[R])
        f.op("dve", lambda e: e.tensor_tensor(den, lr, lr, ALU.mult), reads=[R], writes=[R])
        f.op("dve", lambda e: e.tensor_tensor(t1, li, li, ALU.mult), reads=[R], writes=[R])
        f.op("dve", lambda e: e.tensor_tensor(den, den, t1, ALU.add), reads=[R], writes=[R])
        f.op("dve", lambda e: e.reciprocal(den, den), reads=[R], writes=[R])
        f.op("dve", lambda e: e.tensor_tensor(t1, abre, lr, ALU.mult), reads=[R], writes=[R])
        f.op("dve", lambda e: e.tensor_tensor(t2, abim, li, ALU.mult), reads=[R], writes=[R])
        f.op("dve", lambda e: e.tensor_tensor(t1, t1, t2, ALU.add), reads=[R], writes=[R])
        f.op("dve", lambda e: e.tensor_tensor(prm[:, 8, :], t1, den, ALU.mult), reads=[R], writes=[R])
        f.op("dve", lambda e: e.tensor_tensor(t1, abim, lr, ALU.mult), reads=[R], writes=[R])
        f.op("dve", lambda e: e.tensor_tensor(t2, abre, li, ALU.mult), reads=[R], writes=[R])
        f.op("dve", lambda e: e.tensor_tensor(t1, t1, t2, ALU.subtract), reads=[R], writes=[R])
        f.op("dve", lambda e: e.tensor_tensor(prm[:, 9, :], t1, den, ALU.mult), reads=[R], writes=[R])
        f.op("dve", lambda e: e.tensor_copy(cs2[:, :, 0], prm[:, 6, :]), reads=[R], writes=[R])
        f.op("dve", lambda e: e.tensor_copy(cs2[:, :, 1], prm[:, 7, :]), reads=[R], writes=[R])
        f.op("dve", lambda e: e.tensor_scalar(out=ncs2[:, :, 0], in0=prm[:, 7, :], scalar1=-1.0, scalar2=None, op0=ALU.mult), reads=[R], writes=[R])
        f.op("dve", lambda e: e.tensor_copy(ncs2[:, :, 1], prm[:, 6, :]), reads=[R], writes=[R])
        S.close()
        cosJ = P.sb("cosJ", [128, 8, 128]); sinJ = P.sb("sinJ", [128, 8, 128]); rtab = P.sb("rtab", [128, 8, 128])
        lB = P.sb("lB", [128, 8, 2, 128], BF16); lC = P.sb("lC", [128, 8, 2, 128], BF16)
        RT = Res("s5tab")
        S = Scope(nc)
        yacc = S.sb("yacc", [128, NTOK]); R_y = Res("yacc")
        NB = 2
        psb = [S.ps("psb%d" % k, [128, 2, 512]) for k in range(NB)]; R_psb = RL(NB)
        psy = [S.ps("psy%d" % k, [128, 512]) for k in range(NB)]; R_psy = RL(NB)
        pstr = [S.ps("pstr%d" % k, [128, 4, 128]) for k in range(2)]; R_pstr = RL(2)
        m = [S.sb("m%d" % k, [128, 2, 512]) for k in range(NB)]; R_m = RL(NB)
        ta = [S.sb("ta%d" % k, [128, 2, 512]) for k in range(NB)]; R_ta = RL(NB)
        g = [S.sb("g%d" % k, [128, 2, 512]) for k in range(NB)]; R_g = RL(NB)
        hb = [S.sb("hb%d" % k, [128, 2, 512], BF16) for k in range(NB)]; R_hb = RL(NB)
        ini = S.sb("ini", [128, 4]); R_ini = Res()
        gq1 = S.sb("gq1", [128, 512]); gq2 = S.sb("gq2", [128, 512]); R_gq1 = Res(); R_gq2 = Res()
        wgl = S.sb("wgl", [128, 4, 512], BF16); R_wgl = Res()
        f.dma("pool", wgl[:], w_glu.rearrange("(kc p) n -> p kc n", p=128), writes=[R_wgl])
        blocks = [(0, 256)] + [(256 + 512 * k, 512) for k in range(8)]
        it = 0
        for ct in range(4):
            T = Scope(nc)
            ang = T.sb("ang", [128, 8, 128])
            WB = T.sb("WB", [128, 2, 8, 128]); SC = T.sb("SC", [128, 2, 8, 128]); WB2 = T.sb("WB2", [128, 2, 8, 128])
            fre8 = T.sb("fre8", [128, 8]); fim8 = T.sb("fim8", [128, 8])
            for d in range(2):
                gsl = slice(d * 16 + ct * 4, d * 16 + ct * 4 + 4); lsl = slice(d * 4, d * 4 + 4)
                f.op("dve", lambda e: e.tensor_tensor(ang[:, lsl, :], jt[:].unsqueeze(1).broadcast_to([128, 4, 128]), th[:, gsl].unsqueeze(2).broadcast_to([128, 4, 128]), ALU.mult), reads=[R, RT], writes=[RT])
                f.op("dve", lambda e: e.tensor_copy(rtab[:, lsl, :], rr[:, gsl].unsqueeze(2).broadcast_to([128, 4, 128])), reads=[R, RT], writes=[RT])
                f.op("dve", lambda e: e.tensor_copy(fre8[:, lsl], prm[:, 8, gsl]), reads=[R, RT], writes=[RT])
                f.op("dve", lambda e: e.tensor_copy(fim8[:, lsl], prm[:, 9, gsl]), reads=[R, RT], writes=[RT])
            sincos(T, ang[:].rearrange("p a b -> p (a b)"), 1024, sinJ[:].rearrange("p a b -> p (a b)"), cosJ[:].rearrange("p a b -> p (a b)"), RT, "c%d" % ct)
            f.op("pool", lambda e: e.memset(WB[:], 0.0), reads=[RT], writes=[RT])
            f.op("pool", lambda e: e.memset(SC[:], 0.0), reads=[RT], writes=[RT])
            qn = 0
            for d in range(2):
                for gi in range(8):
                    g_ = ct * 8 + gi
                    l = d * 4 + gi // 2
                    gl = gi % 2
                    for ri, (bsrc, csrc) in enumerate(((b_re, c_re), (b_im, c_im))):
                        q1 = ("sp", "act")[qn % 2]; qn += 1
                        f.dma(q1, WB[64 * gl:64 * gl + 64, ri, l, 16 * gi:16 * gi + 16], bsrc[d, g_], writes=[RT])
                        f.dma(q1, SC[16 * gi:16 * gi + 16, ri, l, 64 * gl:64 * gl + 64], csrc[d, g_], writes=[RT])
            fre = fre8[:].unsqueeze(2).broadcast_to([128, 8, 128]); fim = fim8[:].unsqueeze(2).broadcast_to([128, 8, 128])
            f.op("dve", lambda e: e.tensor_tensor(WB2[:, 0], WB[:, 0], fre, ALU.mult), reads=[RT], writes=[RT])
            f.op("pool", lambda e: e.tensor_tensor(WB2[:, 1], WB[:, 1], fim, ALU.mult), reads=[RT], writes=[RT])
            f.op("dve", lambda e: e.tensor_tensor(WB2[:, 0], WB2[:, 0], WB2[:, 1], ALU.subtract), reads=[RT], writes=[RT])
            f.op("pool", lambda e: e.tensor_tensor(WB2[:, 1], WB[:, 1], fre, ALU.mult), reads=[RT], writes=[RT])
            f.op("dve", lambda e: e.tensor_tensor(WB[:, 0], WB[:, 0], fim, ALU.mult), reads=[RT], writes=[RT])
            f.op("dve", lambda e: e.tensor_tensor(WB2[:, 1], WB2[:, 1], WB[:, 0], ALU.add), reads=[RT], writes=[RT])
            n_ = 0
            for srct, dst, neg in ((WB2, lB, False), (SC, lC, True)):
                for ri in range(2):
                    for d4 in range(2):
                        pb = n_ % 2; n_ += 1
                        for k in range(4):
                            l = d4 * 4 + k
                            f.op("pe", lambda e, k=k, l=l: e.transpose(pstr[pb][:, k, :], srct[:, ri, l, :], ident[:]), reads=[RT, R_ident], writes=[R_pstr[pb]], acc=(k > 0))
                        scl = -1.0 if (neg and ri == 1) else 1.0
                        f.op("act", lambda e: e.activation(out=dst[:, d4 * 4:d4 * 4 + 4, ri, :], in_=pstr[pb][:], func=AF.Identity, scale=scl), reads=[R_pstr[pb], RT], writes=[RT])
            T.close()
            f.op("act", lambda e: e.activation(out=yacc[:], in_=uT[:, ct, :], func=AF.Copy, scale=dsk[:, ct:ct + 1]), reads=R_uT + [R], writes=[R_y])
            items = []
            for pi in range(4):
                for d in range(2):
                    for bidx, (s0, n) in enumerate(blocks):
                        items.append((pi, d, bidx, s0, n))
            NI = len(items)

            def v3(ap):
                return ap.rearrange("p (c j) -> p c j", j=128)

            def geom(k):
                pi, d, bidx, s0, n = items[k]
                bi = k % NB
                dq = d * 16 + ct * 4 + pi
                l = d * 4 + pi
                nch = n // 128
                if d == 0:
                    c0 = s0
                    ucols = uT[:, ct, c0:c0 + n]
                    ycols = yacc[:, c0:c0 + n]
                else:
                    c0 = (256 - s0 - n) if s0 < 256 else (4608 - s0 - n)
                    ucols = rev_ap(uT[:, ct, c0:c0 + n], n)
                    ycols = rev_ap(yacc[:, c0:c0 + n], n)
                tl = [R_uT[kk] for kk in range(c0 // 128, (c0 + n) // 128)]
                cb = cosJ[:, l, :].unsqueeze(1).broadcast_to([128, nch, 128])
                sb_ = sinJ[:, l, :].unsqueeze(1).broadcast_to([128, nch, 128])
                return pi, d, bidx, n, bi, dq, l, nch, ucols, ycols, tl, cb, sb_

            def stA(k):
                pi, d, bidx, n, bi, dq, l, nch, ucols, ycols, tl, cb, sb_ = geom(k)
                for ri in range(2):
                    f.op("pe", lambda e, ri=ri: e.matmul(psb[bi][:, ri, 0:n], lB[:, l, ri, :], ucols, start=True, stop=True),
                         reads=[RT] + tl, writes=[R_psb[bi]], acc=(ri > 0))
                bre, bim = v3(psb[bi][:, 0, 0:n]), v3(psb[bi][:, 1, 0:n])
                mre, mim = v3(m[bi][:, 0, 0:n]), v3(m[bi][:, 1, 0:n])
                t_a, t_b = v3(ta[bi][:, 0, 0:n]), v3(ta[bi][:, 1, 0:n])
                f.op("dve", lambda e: e.tensor_tensor(mre, bre, cb, ALU.mult), reads=[R_psb[bi], RT], writes=[R_m[bi]])
                f.op("dve", lambda e: e.tensor_tensor(t_a, bim, sb_, ALU.mult), reads=[R_psb[bi], RT], writes=[R_ta[bi]])
                f.op("dve", lambda e: e.tensor_tensor(mre, mre, t_a, ALU.add), reads=[R_m[bi], R_ta[bi]], writes=[R_m[bi]])
                f.op("dve", lambda e: e.tensor_tensor(mim, bim, cb, ALU.mult), reads=[R_psb[bi], RT], writes=[R_m[bi]])
                f.op("dve", lambda e: e.tensor_tensor(t_b, bre, sb_, ALU.mult), reads=[R_psb[bi], RT], writes=[R_ta[bi]])
                f.op("dve", lambda e: e.tensor_tensor(mim, mim, t_b, ALU.subtract), reads=[R_m[bi], R_ta[bi]], writes=[R_m[bi]])

            def stB(k):
                pi, d, bidx, n, bi, dq, l, nch, ucols, ycols, tl, cb, sb_ = geom(k)
                prev = None
                if bidx > 0:
                    pbi = (k - 1) % NB
                    pn = items[k - 1][4]
                    prev = (g[pbi], pbi, pn // 128 - 1)
                for c in range(nch):
                    cs = slice(c * 128, (c + 1) * 128)
                    if prev is None:
                        i_re = i_im = 0.0
                        rd_extra = []
                    else:
                        pg, pbi, pc_ = prev
                        gre_l = pg[:, 0, pc_ * 128 + 127:pc_ * 128 + 128]
                        gim_l = pg[:, 1, pc_ * 128 + 127:pc_ * 128 + 128]
                        c128 = prm[:, 6, dq:dq + 1]; s128 = prm[:, 7, dq:dq + 1]
                        f.op("dve", lambda e: e.tensor_scalar(out=ini[:, 0:2], in0=cs2[:, dq, :], scalar1=gre_l, scalar2=None, op0=ALU.mult), reads=[R_g[pbi], R], writes=[R_ini])
                        f.op("dve", lambda e: e.scalar_tensor_tensor(out=ini[:, 0:2], in0=ncs2[:, dq, :], scalar=gim_l, in1=ini[:, 0:2], op0=ALU.mult, op1=ALU.add), reads=[R_g[pbi], R, R_ini], writes=[R_ini])
                        i_re, i_im = ini[:, 0:1], ini[:, 1:2]
                        rd_extra = [R_ini]
                    f.op("dve", lambda e: e.tensor_tensor_scan(g[bi][:, 0, cs], rtab[:, l, :], m[bi][:, 0, cs], i_re, ALU.mult, ALU.add),
                         reads=[R_m[bi], RT] + rd_extra, writes=[R_g[bi]])
                    f.op("dve", lambda e: e.tensor_tensor_scan(g[bi][:, 1, cs], rtab[:, l, :], m[bi][:, 1, cs], i_im, ALU.mult, ALU.add),
                         reads=[R_m[bi], RT] + rd_extra, writes=[R_g[bi]])
                    prev = (g[bi], bi, c)

            def stC(k):
                pi, d, bidx, n, bi, dq, l, nch, ucols, ycols, tl, cb, sb_ = geom(k)
                mre, mim = v3(m[bi][:, 0, 0:n]), v3(m[bi][:, 1, 0:n])
                t_a, t_b = v3(ta[bi][:, 0, 0:n]), v3(ta[bi][:, 1, 0:n])
                gre, gim = v3(g[bi][:, 0, 0:n]), v3(g[bi][:, 1, 0:n])
                hre, him = v3(hb[bi][:, 0, 0:n]), v3(hb[bi][:, 1, 0:n])
                f.op("dve", lambda e: e.tensor_tensor(t_a, gre, cb, ALU.mult), reads=[R_g[bi], RT], writes=[R_ta[bi]])
                f.op("dve", lambda e: e.tensor_tensor(mre, gim, sb_, ALU.mult), reads=[R_g[bi], RT], writes=[R_m[bi]])
                f.op("dve", lambda e: e.tensor_tensor(hre, t_a, mre, ALU.subtract), reads=[R_ta[bi], R_m[bi]], writes=[R_hb[bi]])
                f.op("dve", lambda e: e.tensor_tensor(t_b, gre, sb_, ALU.mult), reads=[R_g[bi], RT], writes=[R_ta[bi]])
                f.op("dve", lambda e: e.tensor_tensor(mim, gim, cb, ALU.mult), reads=[R_g[bi], RT], writes=[R_m[bi]])
                f.op("dve", lambda e: e.tensor_tensor(him, t_b, mim, ALU.add), reads=[R_ta[bi], R_m[bi]], writes=[R_hb[bi]])
                for ri in range(2):
                    f.op("pe", lambda e, ri=ri: e.matmul(psy[bi][:, 0:n], lC[:, l, ri, :], hb[bi][:, ri, 0:n], start=(ri == 0), stop=(ri == 1)),
                         reads=[RT, R_hb[bi]], writes=[R_psy[bi]], acc=(ri > 0))

            def stY(k):
                pi, d, bidx, n, bi, dq, l, nch, ucols, ycols, tl, cb, sb_ = geom(k)
                f.op("dve", lambda e: e.tensor_tensor(ycols, psy[bi][:, 0:n], ycols, ALU.add), reads=[R_psy[bi], R_y], writes=[R_y])

            stA(0)
            for k in range(NI):
                if k + 1 < NI:
                    stA(k + 1)
                stB(k)
                stC(k)
                if k >= 1:
                    stY(k - 1)
            stY(NI - 1)
            for (s0, n) in blocks:
                yb = yacc[:, s0:s0 + n]
                f.op("dve", lambda e: e.tensor_tensor(gq1[:, 0:n], yb, yb, ALU.mult), reads=[R_y], writes=[R_gq1])
                f.op("dve", lambda e: e.tensor_scalar(out=gq1[:, 0:n], in0=gq1[:, 0:n], scalar1=0.044715, scalar2=1.0, op0=ALU.mult, op1=ALU.add), reads=[R_gq1], writes=[R_gq1])
                f.op("dve", lambda e: e.tensor_tensor(gq1[:, 0:n], gq1[:, 0:n], yb, ALU.mult), reads=[R_gq1, R_y], writes=[R_gq1])
                f.op("act", lambda e: e.activation(out=gq2[:, 0:n], in_=gq1[:, 0:n], func=AF.Sigmoid, scale=1.5957691216057308), reads=[R_gq1], writes=[R_gq2])
                f.op("dve", lambda e: e.tensor_tensor(aT[:, ct, s0:s0 + n], yb, gq2[:, 0:n], ALU.mult), reads=[R_gq2, R_y], writes=R_aT[s0 // 128:(s0 + n) // 128])
        sg = [S.sb("sg%d" % k, [128, 512], BF16) for k in range(2)]; R_sg = RL(2)
        anew = S.sb("anew", [128, 4, 512], BF16); R_anew = Res()
        nn = 0
        for (s0, n) in blocks:
            tl = R_aT[s0 // 128:(s0 + n) // 128]
            for cto in range(4):
                bi = nn % 2; nn += 1
                for cti in range(4):
                    f.op("pe", lambda e, cti=cti: e.matmul(psy[bi][:, 0:n], wgl[:, cti, cto * 128:(cto + 1) * 128], aT[:, cti, s0:s0 + n], start=(cti == 0), stop=(cti == 3)),
                         reads=[R_wgl] + tl, writes=[R_psy[bi]], acc=(cti > 0))
                f.op("act", lambda e: e.activation(out=sg[bi][:, 0:n], in_=psy[bi][:, 0:n], func=AF.Sigmoid, bias=bgl[:, cto:cto + 1], scale=1.0), reads=[R_psy[bi], R], writes=[R_sg[bi]])
                f.op("dve", lambda e: e.tensor_tensor(anew[:, cto, 0:n], aT[:, cto, s0:s0 + n], sg[bi][:, 0:n], ALU.mult), reads=[R_sg[bi]] + tl, writes=[R_anew])
            f.op("dve", lambda e: e.tensor_copy(aT[:, :, s0:s0 + n], anew[:, :, 0:n]), reads=[R_anew], writes=tl)
        S.close()
        P.close()

    def win_phase(qT, R_qT, kT2, R_kT, vaug, R_v, oT, R_oT):
        S = Scope(nc)
        esink = S.sb("esink", [128, 8]); R_es = Res()
        f.dma("sp", esink[:], win_sink.partition_broadcast(128), writes=[R_es])
        f.op("act", lambda e: e.activation(out=esink[:], in_=esink[:], func=AF.Exp), reads=[R_es], writes=[R_es])
        NBS = 3
        ps_s = [S.ps("ps_s%d" % k, [128, 8, 128]) for k in range(NBS)]; R_pss = RL(NBS)
        ps_o = [S.ps("ps_o%d" % k, [128, 512]) for k in range(2)]; R_pso = RL(2)
        pT = [S.sb("pT%d" % k, [128, 5, 128], BF16) for k in range(NBS)]; R_pT = RL(NBS)
        dtmp = [S.sb("dtmp%d" % k, [128, 128]) for k in range(2)]; R_dt = RL(2)
        items = []
        for t in range(NT):
            kts = [(0, None), (1, None)]
            if t >= 2:
                for kt in (t - 1, t, t + 1):
                    if 2 <= kt < NT:
                        kts.append((kt, (0 if kt == t - 1 else (1 if kt == t + 1 else None))))
            for h in range(8):
                items.append((t, h, kts))

        def front(i_):
            t, h, kts = items[i_]
            cols = slice(t * 128, (t + 1) * 128)
            nk = len(kts)
            bs = i_ % NBS
            pr, base, kvh = h // 2, 64 * (h % 2), h // 4
            for i, (kt, mk) in enumerate(kts):
                f.op("pe", lambda e, i=i, kt=kt: e.matmul(ps_s[bs][:, i, :], kT2[base:base + 64, kvh, kt * 128:(kt + 1) * 128], qT[base:base + 64, pr, cols], start=True, stop=True),
                     reads=[R_kT[kt], R_qT[t]], writes=[R_pss[bs]], acc=(i > 0))
            f.op("act", lambda e: e.activation(out=pT[bs][:, 0:nk, :], in_=ps_s[bs][:, 0:nk, :], func=AF.Exp, scale=0.125), reads=[R_pss[bs]], writes=[R_pT[bs]])
            for i, (kt, mk) in enumerate(kts):
                if mk is not None:
                    f.op("dve", lambda e, i=i, mk=mk: e.tensor_tensor(pT[bs][:, i, :], pT[bs][:, i, :], maskb[:, mk, :], ALU.mult), reads=[R_pT[bs], R_mask], writes=[R_pT[bs]])

        def back(i_):
            t, h, kts = items[i_]
            cols = slice(t * 128, (t + 1) * 128)
            nk = len(kts)
            bs = i_ % NBS
            bi = i_ % 2
            pr, base, kvh = h // 2, 64 * (h % 2), h // 4
            voff = (64 if h % 2 == 0 else 0) + 128 * kvh
            for i, (kt, mk) in enumerate(kts):
                f.op("pe", lambda e, i=i, kt=kt: e.matmul(ps_o[bi][:, 0:128], vaug[:, kt, voff:voff + 128], pT[bs][:, i, :], start=(i == 0), stop=(i == nk - 1)),
                     reads=[R_v[kt], R_pT[bs]], writes=[R_pso[bi]], acc=(i > 0))
            nb, db = (0, 64) if h % 2 == 0 else (64, 0)
            f.op("dve", lambda e: e.tensor_scalar(out=dtmp[bi][nb:nb + 64, :], in0=ps_o[bi][db:db + 64, 0:128], scalar1=esink[db:db + 64, h:h + 1], scalar2=None, op0=ALU.add),
                 reads=[R_pso[bi], R_es], writes=[R_dt[bi]])
            f.op("dve", lambda e: e.reciprocal(dtmp[bi][nb:nb + 64, :], dtmp[bi][nb:nb + 64, :]), reads=[R_dt[bi]], writes=[R_dt[bi]])
            f.op("dve", lambda e: e.tensor_tensor(oT[nb:nb + 64, pr, cols], ps_o[bi][nb:nb + 64, 0:128], dtmp[bi][nb:nb + 64, :], ALU.mult), reads=[R_pso[bi], R_dt[bi]], writes=[R_oT[t]])
        front(0)
        for i_ in range(len(items)):
            if i_ + 1 < len(items):
                front(i_ + 1)
            back(i_)
        S.close()

    def out_phase(layer, w_out_d, mix_loader, tiles):
        S = Scope(nc)
        load_ln(layer * 2 + 0)
        wo = S.sb("wo", [128, 8, D], BF16); R_wo = Res()
        f.dma("pool", wo[:], w_out_d.rearrange("(kc p) n -> p kc n", p=128), writes=[R_wo])
        xt = [S.sb("xt%d" % k, [128, D]) for k in range(2)]; R_xt = RL(2)
        ot = [S.sb("ot%d" % k, [128, D]) for k in range(2)]; R_ot = RL(2)
        tmp = S.sb("tmp", [128, D]); R_tmp = Res()
        small = S.sb("small", [128, 16]); R_small = Res()
        ps_o2 = [S.ps("ps_o2%d" % k, [128, D]) for k in range(2)]; R_ps = RL(2)
        for n, t in enumerate(tiles):
            b = n % 2
            src, rs = src_tile(layer, t)
            f.dma("sp", xt[b][:], src, reads=rs, writes=[R_xt[b]])
            mixT, R_mix = mix_loader(t, b)
            for half in range(2):
                for kc in range(8):
                    f.op("pe", lambda e, kc=kc, half=half: e.matmul(ps_o2[b][:, half * 512:(half + 1) * 512], mixT[kc], wo[:, kc, half * 512:(half + 1) * 512], start=(kc == 0), stop=(kc == 7)),
                         reads=[R_wo] + R_mix, writes=[R_ps[b]], acc=(half + kc > 0))
            resid_ln(S, xt[b], R_xt[b], ps_o2[b], R_ps[b], (1 if t < 2 else 0), layer * 2 + 0, ot[b], R_ot[b], tmp, R_tmp, small, R_small)
            f.dma("act", XR[t * 128:(t + 1) * 128, :], ot[b][:], reads=[R_ot[b]], writes=[R_XR[t]])
        S.close()

    def ffn_phase(layer, tiles_all, final):
        P = Scope(nc)
        rw = P.sb("rw", [128, 8, 32]); R_rw = Res()
        f.dma("sp", rw[:], router_w.rearrange("(kc p) n -> p kc n", p=128), writes=[R_rw])
        rbias = P.sb("rbias", [128, 32]); f.dma("sp", rbias[:], router_b.partition_broadcast(128), writes=[R_rw])
        GT = 9
        load_ln(layer * 2 + 1)
        groups = [tiles_all[i:i + GT] for i in range(0, len(tiles_all), GT)]
        for grp in groups:
            S = Scope(nc)
            ng = len(grp)
            hT = S.sb("hTg", [128, 8, GT * 128], BF16); R_hT = RL(ng, "hTg")
            comb = S.sb("comb", [128, GT, 32]); R_comb = RL(ng, "comb")
            yacc = S.sb("yaccg", [128, GT, D]); R_y = RL(ng, "yg")
            A = Scope(nc)
            xt = [A.sb("xt%d" % k, [128, D]) for k in range(2)]; R_xt = RL(2)
            h32 = A.sb("h32", [128, D]); R_h32 = Res()
            h32T = A.sb("h32T", [128, 8, 128]); R_h32T = Res()
            ps_tp = A.ps("ps_tp", [128, 8, 128]); R_pstp = Res(x=True)
            ps_r = A.ps("ps_r", [128, 512]); R_psr = Res()
            sc = A.sb("sc", [128, 32]); sel = A.sb("sel", [128, 32]); R_sc = Res()
            pa = A.sb("pa", [128, 8, 6]); pm = A.sb("pm", [128, 8, 6]); gs = A.sb("gs", [128, 8]); thr = A.sb("thr", [128, 8])
            gm = A.sb("gm", [128, 2]); mg = A.sb("mg", [128, 8]); sm = A.sb("sm", [128, 8, 4])
            for j, t in enumerate(grp):
                b = j % 2
                f.dma("sp", xt[b][:], XR[t * 128:(t + 1) * 128, :], reads=[R_XR[t]], writes=[R_xt[b]])
                which = 1 if t < 2 else 0
                mod_transpose(xt[b], R_xt[b], which, h32, R_h32, ps_tp, R_pstp, hT[:, :, j * 128:(j + 1) * 128], R_hT[j], h32T, R_h32T)
                for kc in range(8):
                    f.op("pe", lambda e, kc=kc: e.matmul(ps_r[:, 0:32], h32T[:, kc, :], rw[:, kc, :], start=(kc == 0), stop=(kc == 7)), reads=[R_h32T, R_rw], writes=[R_psr], acc=(kc > 0))
                R1 = R_sc
                f.op("act", lambda e: e.activation(out=sc[:], in_=ps_r[:, 0:32], func=AF.Sigmoid), reads=[R_psr], writes=[R1])
                f.op("dve", lambda e: e.tensor_tensor(sel[:], sc[:], rbias[:], ALU.add), reads=[R1, R_rw], writes=[R1])
                s3 = sel[:].rearrange("p (g e) -> p g e", e=4)
                pairs = [(0, 1), (0, 2), (0, 3), (1, 2), (1, 3), (2, 3)]
                for k, (a_, b_) in enumerate(pairs):
                    f.op("dve", lambda e, k=k, a_=a_, b_=b_: e.tensor_tensor(pa[:, :, k], s3[:, :, a_], s3[:, :, b_], ALU.add), reads=[R1], writes=[R1])
                    f.op("dve", lambda e, k=k, a_=a_, b_=b_: e.tensor_tensor(pm[:, :, k], s3[:, :, a_], s3[:, :, b_], ALU.min), reads=[R1], writes=[R1])
                f.op("dve", lambda e: e.tensor_reduce(out=gs[:], in_=pa[:], axis=AX.X, op=ALU.max), reads=[R1], writes=[R1])
                f.op("dve", lambda e: e.tensor_reduce(out=thr[:], in_=pm[:], axis=AX.X, op=ALU.max), reads=[R1], writes=[R1])
                f.op("dve", lambda e: e.tensor_reduce(out=gm[:, 0:1], in_=gs[:], axis=AX.X, op=ALU.max), reads=[R1], writes=[R1])
                f.op("dve", lambda e: e.tensor_scalar(out=mg[:], in0=gs[:], scalar1=gm[:, 0:1], scalar2=None, op0=ALU.is_ge), reads=[R1], writes=[R1])
                f.op("dve", lambda e: e.tensor_tensor(sm[:], s3, thr[:].unsqueeze(2).broadcast_to([128, 8, 4]), ALU.is_ge), reads=[R1], writes=[R1])
                f.op("dve", lambda e: e.tensor_tensor(sm[:], sm[:], mg[:].unsqueeze(2).broadcast_to([128, 8, 4]), ALU.mult), reads=[R1], writes=[R1])
                cj = comb[:, j, :]
                f.op("dve", lambda e: e.tensor_tensor(cj, sm[:].rearrange("p g e -> p (g e)"), sc[:], ALU.mult), reads=[R1], writes=[R_comb[j]])
                f.op("dve", lambda e: e.tensor_reduce(out=gm[:, 1:2], in_=cj, axis=AX.X, op=ALU.add), reads=[R_comb[j], R1], writes=[R1])
                f.op("dve", lambda e: e.reciprocal(gm[:, 1:2], gm[:, 1:2]), reads=[R1], writes=[R1])
                f.op("dve", lambda e: e.tensor_scalar(out=cj, in0=cj, scalar1=gm[:, 1:2], scalar2=None, op0=ALU.mult), reads=[R1, R_comb[j]], writes=[R_comb[j]])
            A.close()
            B = Scope(nc)
            wg = [B.sb("wg%d" % k, [128, 8, 512], BF16) for k in range(2)]
            wu = [B.sb("wu%d" % k, [128, 8, 512], BF16) for k in range(2)]
            wd = [B.sb("wd%d" % k, [128, 4, D], BF16) for k in range(2)]
            R_w = RL(2, "w")
            psg = [B.ps("psg%d" % k, [128, 512]) for k in range(2)]; R_psg = RL(2)
            psu = [B.ps("psu%d" % k, [128, 512]) for k in range(2)]; R_psu = RL(2)
            psd = [B.ps("psd%d" % k, [128, D]) for k in range(2)]; R_psd = RL(2)
            sg = [B.sb("sg%d" % k, [128, 512]) for k in range(2)]; R_sg = RL(2)
            hid = [B.sb("hid%d" % k, [128, 4, 512], BF16) for k in range(2)]; R_hid = RL(2)
            ntok = ng * 128
            blocks = [(c0, min(512, ntok - c0)) for c0 in range(0, ntok, 512)]
            nfc = 0; nblk = 0; nd = 0
            for ex in range(32):
                wb = ex % 2
                f.dma("pool", wg[wb][:], w_gate[layer, ex].rearrange("(kc p) n -> p kc n", p=128), writes=[R_w[wb]])
                f.dma("pool", wu[wb][:], w_up[layer, ex].rearrange("(kc p) n -> p kc n", p=128), writes=[R_w[wb]])
                f.dma("pool", wd[wb][:], w_down[layer, ex].rearrange("(kc p) n -> p kc n", p=128), writes=[R_w[wb]])
                for (c0, n) in blocks:
                    hb_ = nblk % 2; nblk += 1
                    tl = R_hT[c0 // 128:(c0 + n) // 128]
                    for fc in range(4):
                        pb = nfc % 2; nfc += 1
                        for kc in range(8):
                            f.op("pe", lambda e, kc=kc, fc=fc: e.matmul(psg[pb][:, 0:n], wg[wb][:, kc, fc * 128:(fc + 1) * 128], hT[:, kc, c0:c0 + n], start=(kc == 0), stop=(kc == 7)),
                                 reads=[R_w[wb]] + tl, writes=[R_psg[pb]], acc=(kc > 0))
                        for kc in range(8):
                            f.op("pe", lambda e, kc=kc, fc=fc: e.matmul(psu[pb][:, 0:n], wu[wb][:, kc, fc * 128:(fc + 1) * 128], hT[:, kc, c0:c0 + n], start=(kc == 0), stop=(kc == 7)),
                                 reads=[R_w[wb]] + tl, writes=[R_psu[pb]], acc=(kc > 0))
                        f.op("act", lambda e: e.activation(out=sg[pb][:, 0:n], in_=psg[pb][:, 0:n], func=AF.Silu), reads=[R_psg[pb]], writes=[R_sg[pb]])
                        f.op("dve", lambda e, fc=fc: e.tensor_tensor(hid[hb_][:, fc, 0:n], sg[pb][:, 0:n], psu[pb][:, 0:n], ALU.mult), reads=[R_sg[pb], R_psu[pb]], writes=[R_hid[hb_]])
                    for tt in range(n // 128):
                        j = c0 // 128 + tt
                        db = nd % 2; nd += 1
                        for half in range(2):
                            for fc in range(4):
                                f.op("pe", lambda e, fc=fc, half=half: e.matmul(psd[db][:, half * 512:(half + 1) * 512], hid[hb_][:, fc, tt * 128:(tt + 1) * 128], wd[wb][:, fc, half * 512:(half + 1) * 512], start=(fc == 0), stop=(fc == 3)),
                                     reads=[R_w[wb], R_hid[hb_]], writes=[R_psd[db]], acc=(half + fc > 0))
                        cw = comb[:, j, ex:ex + 1]
                        if ex == 0:
                            f.op("dve", lambda e: e.tensor_scalar(out=yacc[:, j, :], in0=psd[db][:], scalar1=cw, scalar2=None, op0=ALU.mult), reads=[R_psd[db], R_comb[j]], writes=[R_y[j]])
                        else:
                            f.op("dve", lambda e: e.scalar_tensor_tensor(out=yacc[:, j, :], in0=psd[db][:], scalar=cw, in1=yacc[:, j, :], op0=ALU.mult, op1=ALU.add), reads=[R_psd[db], R_comb[j], R_y[j]], writes=[R_y[j]])
            B.close()
            C = Scope(nc)
            xt = [C.sb("xt%d" % k, [128, D]) for k in range(2)]; R_xt = RL(2)
            ot = [C.sb("ot%d" % k, [128, D]) for k in range(2)]; R_ot = RL(2)
            tmp = C.sb("tmp", [128, D]); R_tmp = Res()
            small = C.sb("small", [128, 16]); R_small = Res()
            for j, t in enumerate(grp):
                b = j % 2
                f.dma("sp", xt[b][:], XR[t * 128:(t + 1) * 128, :], reads=[R_XR[t]], writes=[R_xt[b]])
                yj = yacc[:, j, :]

                class _V:
                    def __init__(self, ap): self.ap = ap
                    def __getitem__(self, k): return self.ap
                resid_ln(C, xt[b], R_xt[b], _V(yj), R_y[j], (1 if t < 2 else 0), layer * 2 + 1, ot[b], R_ot[b], tmp, R_tmp, small, R_small)
                if final:
                    f.dma("act", out_d[(t - 2) * 128:(t - 1) * 128, :], ot[b][:], reads=[R_ot[b]], writes=[R_out])
                else:
                    f.dma("act", XR[t * 128:(t + 1) * 128, :], ot[b][:], reads=[R_ot[b]], writes=[R_XR[t]])
            C.close()
            S.close()
        P.close()


    def ffn_sparse(layer, tiles_all, final):
        IOA = bass.IndirectOffsetOnAxis
        ng = len(tiles_all)
        M = ng * 32
        P = Scope(nc)
        rw = P.sb("rw", [128, 8, 32]); R_rw = Res()
        f.dma("sp", rw[:], router_w.rearrange("(kc p) n -> p kc n", p=128), writes=[R_rw])
        rbias = P.sb("rbias", [128, 32]); f.dma("sp", rbias[:], router_b.partition_broadcast(128), writes=[R_rw])
        jt = P.sb("jt2", [128, 128]); f.dma("sp", jt[:], k_jidx, writes=[R_rw])
        pc = P.sb("pc", [128, 4]); f.dma("sp", pc[:], k_pc, writes=[R_rw])
        load_ln(layer * 2 + 1)
        comb = P.sb("comb", [128, ng, 32]); R_comb = RL(ng, "comb")
        posA_i = P.sb("posA_i", [128, ng], I32); posB_i = P.sb("posB_i", [128, ng], I32)
        wA = P.sb("wA", [128, ng]); wB = P.sb("wB", [128, ng])
        NSO = NS - 32
        idxw = P.sb("idxw", [128, NSO, 4], I32)
        R_rt = Res("route")
        R_XsW = RL(ng, "xsw")
        R_Ys = RL(NS, "ys")
        HB = Scope(nc)
        hb_all = HB.sb("hb_all", [128, ng, D], BF16); R_hb = RL(ng, "hb")
        A = Scope(nc)
        xt = [A.sb("xt%d" % k, [128, D]) for k in range(2)]; R_xt = RL(2)
        h32 = [A.sb("h32%d" % k, [128, D]) for k in range(2)]; R_h32 = RL(2)
        h32T = A.sb("h32T", [128, 8, 128]); R_h32T = Res()
        ps_tp = [A.ps("ps_tp%d" % k, [128, 8, 128]) for k in range(2)]; R_pstp = RL(2)
        ps_r = A.ps("ps_r", [128, 512]); R_psr = Res()
        R_sc = Res()
        sc_all = A.sb("sc_all", [128, ng, 32]); sel_all = A.sb("sel_all", [128, ng, 32])
        pa_all = A.sb("pa_all", [128, ng, 8, 6]); pm_all = A.sb("pm_all", [128, ng, 8, 6])
        gs_all = A.sb("gs_all", [128, ng, 8]); thr_all = A.sb("thr_all", [128, ng, 8]); mg_all = A.sb("mg_all", [128, ng, 8])
        gm_all = A.sb("gm_all", [128, ng]); sm_all = A.sb("sm_all", [128, ng, 8, 4])
        for j, t in enumerate(tiles_all):
            b = j % 2
            f.dma("sp", xt[b][:], XR[t * 128:(t + 1) * 128, :], reads=[R_XR[t]], writes=[R_xt[b]])
            which = 1 if t < 2 else 0
            f.op("dve", lambda e: e.tensor_tensor(h32[b][:], xt[b][:], mod[:, which, 1, :], ALU.mult), reads=[R_xt[b], R_mod], writes=[R_h32[b]])
            f.op("dve", lambda e: e.tensor_tensor(h32[b][:], h32[b][:], mod[:, which, 0, :], ALU.add), reads=[R_h32[b], R_mod], writes=[R_h32[b]])
            for kc in range(8):
                f.op("pe", lambda e, kc=kc: e.transpose(ps_tp[b][:, kc, :], h32[b][:, kc * 128:(kc + 1) * 128], ident[:]),
                     reads=[R_h32[b], R_ident], writes=[R_pstp[b]], acc=(kc > 0))
            f.op("dve", lambda e: e.tensor_copy(h32T[:], ps_tp[b][:]), reads=[R_pstp[b]], writes=[R_h32T])
            f.op("act", lambda e: e.activation(out=hb_all[:, j, :].rearrange("p (c j q) -> p c j q", c=4, j=2),
                                               in_=h32[b][:].rearrange("p (c q j) -> p c j q", c=4, j=2), func=AF.Identity),
                 reads=[R_h32[b]], writes=[R_hb[j]])
            for kc in range(8):
                f.op("pe", lambda e, kc=kc: e.matmul(ps_r[:, 0:32], h32T[:, kc, :], rw[:, kc, :], start=(kc == 0), stop=(kc == 7)), reads=[R_h32T, R_rw], writes=[R_psr], acc=(kc > 0))
            f.op("act", lambda e: e.activation(out=sc_all[:, j, :], in_=ps_r[:, 0:32], func=AF.Sigmoid), reads=[R_psr], writes=[R_sc])
        R1 = R_sc
        s4 = sel_all[:].rearrange("p t (g e) -> p t g e", e=4)
        f.op("dve", lambda e: e.tensor_tensor(sel_all[:], sc_all[:], rbias[:].unsqueeze(1).broadcast_to([128, ng, 32]), ALU.add), reads=[R1, R_rw], writes=[R1])
        pairs = [(0, 1), (0, 2), (0, 3), (1, 2), (1, 3), (2, 3)]
        for k, (a_, b_) in enumerate(pairs):
            f.op("dve", lambda e, k=k, a_=a_, b_=b_: e.tensor_tensor(pa_all[:, :, :, k], s4[:, :, :, a_], s4[:, :, :, b_], ALU.add), reads=[R1], writes=[R1])
            f.op("dve", lambda e, k=k, a_=a_, b_=b_: e.tensor_tensor(pm_all[:, :, :, k], s4[:, :, :, a_], s4[:, :, :, b_], ALU.min), reads=[R1], writes=[R1])
        f.op("dve", lambda e: e.tensor_reduce(out=gs_all[:], in_=pa_all[:], axis=AX.X, op=ALU.max), reads=[R1], writes=[R1])
        f.op("dve", lambda e: e.tensor_reduce(out=thr_all[:], in_=pm_all[:], axis=AX.X, op=ALU.max), reads=[R1], writes=[R1])
        f.op("dve", lambda e: e.tensor_reduce(out=gm_all[:], in_=gs_all[:], axis=AX.X, op=ALU.max), reads=[R1], writes=[R1])
        f.op("dve", lambda e: e.tensor_tensor(mg_all[:], gs_all[:], gm_all[:].unsqueeze(2).broadcast_to([128, ng, 8]), ALU.is_ge), reads=[R1], writes=[R1])
        f.op("dve", lambda e: e.tensor_tensor(sm_all[:], s4, thr_all[:].unsqueeze(3).broadcast_to([128, ng, 8, 4]), ALU.is_ge), reads=[R1], writes=[R1])
        f.op("dve", lambda e: e.tensor_tensor(sm_all[:], sm_all[:], mg_all[:].unsqueeze(3).broadcast_to([128, ng, 8, 4]), ALU.mult), reads=[R1], writes=[R1])
        f.op("dve", lambda e: e.tensor_tensor(comb[:], sm_all[:].rearrange("p t g e -> p t (g e)"), sc_all[:], ALU.mult), reads=[R1], writes=R_comb)
        f.op("dve", lambda e: e.tensor_reduce(out=gm_all[:], in_=comb[:], axis=AX.X, op=ALU.add), reads=R_comb + [R1], writes=[R1])
        f.op("dve", lambda e: e.reciprocal(gm_all[:], gm_all[:]), reads=[R1], writes=[R1])
        f.op("dve", lambda e: e.tensor_tensor(comb[:], comb[:], gm_all[:].unsqueeze(2).broadcast_to([128, ng, 32]), ALU.mult), reads=[R1] + R_comb, writes=R_comb)
        A.close()
        Bq = Scope(nc)
        m_ = Bq.sb("m_", [128, ng, 32]); mb16 = Bq.sb("mb16", [128, ng, 32], BF16)
        rank = Bq.sb("rank", [128, ng, 32]); tot = Bq.sb("tot", [128, ng, 32]); base = Bq.sb("base", [128, ng, 32])
        me = Bq.sb("me", [128, ng, 32]); Bm = Bq.sb("Bm", [128, ng, 32]); Am = Bq.sb("Am", [128, ng, 32]); tmpq = Bq.sb("tmpq", [128, ng, 32])
        ones16 = Bq.sb("ones16", [128, 128], BF16)
        cnt = Bq.sb("cnt", [128, 32]); cmp17 = Bq.sb("cmp17", [128, 32, 18]); thr18 = Bq.sb("thr18", [128, 18])
        tlf = Bq.sb("tlf", [128, 32]); sinc = Bq.sb("sinc", [128, 32]); so512 = Bq.sb("so512", [128, 32]); c1e = Bq.sb("c1e", [128, 32])
        mx = Bq.sb("mx", [128, ng]); pAf = Bq.sb("pAf", [128, ng]); pBf = Bq.sb("pBf", [128, ng])
        cmpj = Bq.sb("cmpj", [128, NSO, 32]); eidf = Bq.sb("eidf", [128, NSO]); idxf = Bq.sb("idxf", [128, NSO, 4])
        ps_rk = Bq.ps("ps_rk", [128, 3, 512]); ps_tt = Bq.ps("ps_tt", [128, 3, 512])
        RB = [R_rt]

        def fl(ap3):
            return ap3.rearrange("p a b -> p (a b)")
        f.op("dve", lambda e: e.tensor_scalar(out=fl(m_[:]), in0=fl(comb[:]), scalar1=0.0, scalar2=None, op0=ALU.is_gt), reads=R_comb, writes=RB)
        f.op("dve", lambda e: e.tensor_copy(fl(mb16[:]), fl(m_[:])), reads=RB, writes=RB)
        f.op("pool", lambda e: e.memset(ones16[:], 1.0), reads=RB, writes=RB)
        chunks = [(n0, min(M, n0 + 512)) for n0 in range(0, M, 512)]
        for ch, (n0, n1) in enumerate(chunks):
            f.op("pe", lambda e, ch=ch, n0=n0, n1=n1: e.matmul(ps_rk[:, ch, 0:n1 - n0], maskb[:, 2, :], fl(mb16[:])[:, n0:n1], start=True, stop=True), reads=RB + [R_mask], writes=RB)
            f.op("pe", lambda e, ch=ch, n0=n0, n1=n1: e.matmul(ps_tt[:, ch, 0:n1 - n0], ones16[:], fl(mb16[:])[:, n0:n1], start=True, stop=True), reads=RB, writes=RB)
        for ch, (n0, n1) in enumerate(chunks):
            f.op("dve", lambda e, ch=ch, n0=n0, n1=n1: e.tensor_copy(fl(rank[:])[:, n0:n1], ps_rk[:, ch, 0:n1 - n0]), reads=RB, writes=RB)
            f.op("dve", lambda e, ch=ch, n0=n0, n1=n1: e.tensor_copy(fl(tot[:])[:, n0:n1], ps_tt[:, ch, 0:n1 - n0]), reads=RB, writes=RB)
        f.op("dve", lambda e: e.memset(base[:, 0, :], 0.0), reads=RB, writes=RB)
        for t_ in range(1, ng):
            f.op("dve", lambda e, t_=t_: e.tensor_tensor(base[:, t_, :], base[:, t_ - 1, :], tot[:, t_ - 1, :], ALU.add), reads=RB, writes=RB)
        f.op("dve", lambda e: e.tensor_tensor(cnt[:], base[:, ng - 1, :], tot[:, ng - 1, :], ALU.add), reads=RB, writes=RB)
        f.op("dve", lambda e: e.tensor_scalar(out=thr18[:], in0=jt[:, 0:18], scalar1=512.0, scalar2=None, op0=ALU.mult), reads=RB + [R_rw], writes=RB)
        f.op("dve", lambda e: e.tensor_tensor(cmp17[:], cnt[:].unsqueeze(2).broadcast_to([128, 32, 18]), thr18[:].unsqueeze(1).broadcast_to([128, 32, 18]), ALU.is_gt), reads=RB, writes=RB)
        f.op("dve", lambda e: e.tensor_reduce(out=tlf[:], in_=cmp17[:], axis=AX.X, op=ALU.add), reads=RB, writes=RB)
        f.op("dve", lambda e: e.tensor_scalar(out=tlf[:], in0=tlf[:], scalar1=-1.0, scalar2=0.0, op0=ALU.add, op1=ALU.max), reads=RB, writes=RB)
        f.op("dve", lambda e: e.tensor_copy(sinc[:], tlf[:]), reads=RB, writes=RB)
        for e_ in range(1, 32):
            f.op("dve", lambda e, e_=e_: e.tensor_tensor(sinc[:, e_:e_ + 1], sinc[:, e_ - 1:e_], tlf[:, e_:e_ + 1], ALU.add), reads=RB, writes=RB)
        f.op("dve", lambda e: e.tensor_tensor(so512[:], sinc[:], tlf[:], ALU.subtract), reads=RB, writes=RB)
        f.op("dve", lambda e: e.tensor_scalar(out=so512[:], in0=so512[:], scalar1=512.0, scalar2=15872.0, op0=ALU.mult, op1=ALU.add), reads=RB, writes=RB)
        f.op("dve", lambda e: e.tensor_scalar(out=c1e[:], in0=jt[:, 0:32], scalar1=512.0, scalar2=None, op0=ALU.mult), reads=RB + [R_rw], writes=RB)
        f.op("dve", lambda e: e.tensor_tensor(so512[:], so512[:], c1e[:], ALU.subtract), reads=RB, writes=RB)
        f.op("dve", lambda e: e.tensor_tensor(fl(rank[:]), fl(rank[:]), fl(base[:]), ALU.add), reads=RB, writes=RB)
        f.op("dve", lambda e: e.tensor_scalar(out=fl(tmpq[:]), in0=fl(rank[:]), scalar1=512.0, scalar2=None, op0=ALU.is_ge), reads=RB, writes=RB)
        f.op("dve", lambda e: e.tensor_tensor(tmpq[:], tmpq[:], so512[:].unsqueeze(1).broadcast_to([128, ng, 32]), ALU.mult), reads=RB, writes=RB)
        f.op("dve", lambda e: e.tensor_tensor(rank[:], rank[:], c1e[:].unsqueeze(1).broadcast_to([128, ng, 32]), ALU.add), reads=RB, writes=RB)
        f.op("dve", lambda e: e.tensor_tensor(fl(rank[:]), fl(rank[:]), fl(tmpq[:]), ALU.add), reads=RB, writes=RB)
        f.op("dve", lambda e: e.tensor_tensor(me[:], m_[:], jt[:, 1:33].unsqueeze(1).broadcast_to([128, ng, 32]), ALU.mult), reads=RB, writes=RB)
        f.op("dve", lambda e: e.tensor_reduce(out=mx[:], in_=me[:], axis=AX.X, op=ALU.max), reads=RB, writes=RB)
        f.op("dve", lambda e: e.tensor_tensor(Bm[:], me[:], mx[:].unsqueeze(2).broadcast_to([128, ng, 32]), ALU.is_equal), reads=RB, writes=RB)
        f.op("dve", lambda e: e.tensor_tensor(fl(Am[:]), fl(m_[:]), fl(Bm[:]), ALU.subtract), reads=RB, writes=RB)
        for (msk, pf, wf) in ((Am, pAf, wA), (Bm, pBf, wB)):
            f.op("dve", lambda e, msk=msk: e.tensor_tensor(fl(tmpq[:]), fl(msk[:]), fl(rank[:]), ALU.mult), reads=RB, writes=RB)
            f.op("dve", lambda e, pf=pf: e.tensor_reduce(out=pf[:], in_=tmpq[:], axis=AX.X, op=ALU.add), reads=RB, writes=RB)
            f.op("dve", lambda e, msk=msk: e.tensor_tensor(fl(tmpq[:]), fl(msk[:]), fl(comb[:]), ALU.mult), reads=RB + R_comb, writes=RB)
            f.op("dve", lambda e, wf=wf: e.tensor_reduce(out=wf[:], in_=tmpq[:], axis=AX.X, op=ALU.add), reads=RB, writes=RB)
        f.op("dve", lambda e: e.tensor_copy(posA_i[:], pAf[:]), reads=RB, writes=RB)
        f.op("dve", lambda e: e.tensor_copy(posB_i[:], pBf[:]), reads=RB, writes=RB)
        f.op("dve", lambda e: e.tensor_tensor(cmpj[:], sinc[:].unsqueeze(1).broadcast_to([128, NSO, 32]), jt[:, 0:NSO].unsqueeze(2).broadcast_to([128, NSO, 32]), ALU.is_le), reads=RB, writes=RB)
        f.op("dve", lambda e: e.tensor_reduce(out=eidf[:], in_=cmpj[:], axis=AX.X, op=ALU.add), reads=RB, writes=RB)
        f.op("dve", lambda e: e.tensor_scalar(out=eidf[:], in0=eidf[:], scalar1=32.0, scalar2=512.0, op0=ALU.min, op1=ALU.mult), reads=RB, writes=RB)
        f.op("dve", lambda e: e.tensor_scalar(out=eidf[:], in0=eidf[:], scalar1=float(layer * 16384), scalar2=None, op0=ALU.add), reads=RB, writes=RB)
        f.op("dve", lambda e: e.tensor_tensor(idxf[:], eidf[:].unsqueeze(2).broadcast_to([128, NSO, 4]), pc[:].unsqueeze(1).broadcast_to([128, NSO, 4]), ALU.add), reads=RB, writes=RB)
        f.op("dve", lambda e: e.tensor_copy(idxw[:], idxf[:]), reads=RB, writes=RB)
        for j in range(ng):
            for pi_ in (posA_i, posB_i):
                f._dma_common("pool", lambda e, j=j, pi_=pi_: e.indirect_dma_start(out=XS, out_offset=IOA(ap=pi_[:, j:j + 1], axis=0), in_=hb_all[:, j, :], in_offset=None),
                              [R_hb[j]] + RB + R_XsZ, [R_XsW[j]])
        Bq.close()
        HB.close()
        Sd = Scope(nc)
        wg = [Sd.sb("wg%d" % k, [128, 4, 2, 512], BF16) for k in range(2)]
        wu = [Sd.sb("wu%d" % k, [128, 4, 2, 512], BF16) for k in range(2)]
        wd = [Sd.sb("wd%d" % k, [128, 4, D], BF16) for k in range(2)]
        R_w = RL(2, "w"); R_wd = RL(2, "wd")
        xs = [[Sd.sb("xs%d_%d" % (a_, tt), [128, D], BF16) for tt in range(4)] for a_ in range(2)]
        R_xs = [RL(4, "xs%d" % a_) for a_ in range(2)]

        def xs_load(jn):
            for tt in range(4):
                r0 = (jn * 4 + tt) * 128
                f.dma("sp", xs[jn % 2][tt][:], XS[r0:r0 + 128, :], reads=R_XsW, writes=[R_xs[jn % 2][tt]])
        xT = [Sd.sb("xT%d" % k, [128, 8, 512], BF16) for k in range(2)]; R_xT = RL(2)
        ps_t = [Sd.ps("ps_t%d" % k, [128, 8, 128], BF16) for k in range(2)]; R_pst = RL(2)
        psg = [Sd.ps("psg%d" % k, [128, 512]) for k in range(2)]; R_psg = RL(2)
        psu = [Sd.ps("psu%d" % k, [128, 512]) for k in range(2)]; R_psu = RL(2)
        psd = [Sd.ps("psd%d" % k, [128, 512]) for k in range(2)]; R_psd = RL(2)
        sg = [Sd.sb("sg%d" % k, [128, 512]) for k in range(2)]; R_sg = RL(2)
        hid = [Sd.sb("hid%d" % k, [128, 4, 512], BF16) for k in range(2)]; R_hid = RL(2)
        ysb = [Sd.sb("ysb%d" % k, [128, D]) for k in range(2)]; R_ysb = [RL(2, "ysb%d" % k) for k in range(2)]
        nfc = 0; nx = 0; ny = 0
        bc_reg = nc.gpsimd.alloc_register("bc%d" % layer)
        nc.gpsimd.reg_mov(bc_reg, 16383 + layer * 16384)
        stg_g = Sd.sb("stg_g", [128, 4, 2, 512]); stg_u = Sd.sb("stg_u", [128, 4, 2, 512])
        R_sg_ = Res("stg_g"); R_su_ = Res("stg_u")

        def w_load(ex):
            f.dma("sp", stg_g[:], w_gate[layer, ex].rearrange("(c q j) n -> q c j n", c=4, j=2), writes=[R_sg_])
            f.dma("sp", stg_u[:], w_up[layer, ex].rearrange("(c q j) n -> q c j n", c=4, j=2), writes=[R_su_])
            f.dma("pool", wd[ex % 2][:], w_down[layer, ex].rearrange("(c p) n -> p c n", p=128), writes=[R_wd[ex % 2]])

        def w_cast(ex):
            wb_ = ex % 2
            f.op("act", lambda e: e.activation(out=wg[wb_][:], in_=stg_g[:], func=AF.Identity), reads=[R_sg_], writes=[R_w[wb_]])
            f.op("dve", lambda e: e.tensor_copy(wu[wb_][:], stg_u[:]), reads=[R_su_], writes=[R_w[wb_]])
        xs_load(0)
        w_load(0)
        w_cast(0)
        for j in range(NS):
            wb = j % 2
            if j + 1 < NS:
                xs_load(j + 1)
            if j + 1 < 32:
                w_load(j + 1)
            if j >= 32:
                jo = j - 32
                for c in range(4):
                    for (wt_, rows_) in ((wg, wg_rows), (wu, wu_rows)):
                        f._dma_common("pool", lambda e, c=c, wt_=wt_, rows_=rows_: e.indirect_dma_start(out=wt_[wb][:, c, :, :].rearrange("p a n -> p (a n)"), out_offset=None, in_=rows_[layer],
                                                                                                   in_offset=IOA(ap=idxw[:, jo, c:c + 1], axis=0), bounds_check=bc_reg, oob_is_err=False),
                                      RB, [R_w[wb]])
                for c in range(4):
                    f._dma_common("pool", lambda e, c=c: e.indirect_dma_start(out=wd[wb][:, c, :], out_offset=None, in_=wd_rows[layer], in_offset=IOA(ap=idxw[:, jo, c:c + 1], axis=0), bounds_check=bc_reg, oob_is_err=False),
                                  RB, [R_wd[wb]])
            for tt in range(4):
                for kc in range(8):
                    f.op("pe", lambda e, kc=kc, tt=tt: e.transpose(ps_t[tt % 2][:, kc, :], xs[j % 2][tt][:, kc * 128:(kc + 1) * 128], identb[:]), reads=[R_xs[j % 2][tt], R_identb], writes=[R_pst[tt % 2]], acc=(kc > 0))
                f.op("dve", lambda e, tt=tt: e.tensor_copy(xT[wb][:, :, tt * 128:(tt + 1) * 128], ps_t[tt % 2][:]), reads=[R_pst[tt % 2]], writes=[R_xT[wb]])
            hb_ = j % 2
            for fc in range(4):
                pb = nfc % 2; nfc += 1
                for kc in range(8):
                    f.op("pe", lambda e, kc=kc, fc=fc: e.matmul(psg[pb][:], wg[wb][:, kc // 2, kc % 2, fc * 128:(fc + 1) * 128], xT[wb][:, kc, :], start=(kc == 0), stop=(kc == 7)),
                         reads=[R_w[wb], R_xT[wb]], writes=[R_psg[pb]], acc=(kc > 0))
                for kc in range(8):
                    f.op("pe", lambda e, kc=kc, fc=fc: e.matmul(psu[pb][:], wu[wb][:, kc // 2, kc % 2, fc * 128:(fc + 1) * 128], xT[wb][:, kc, :], start=(kc == 0), stop=(kc == 7)),
                         reads=[R_w[wb], R_xT[wb]], writes=[R_psu[pb]], acc=(kc > 0))
                f.op("act", lambda e: e.activation(out=sg[pb][:], in_=psg[pb][:], func=AF.Silu), reads=[R_psg[pb]], writes=[R_sg[pb]])
                f.op("dve", lambda e, fc=fc: e.tensor_tensor(hid[hb_][:, fc, :], sg[pb][:], psu[pb][:], ALU.mult), reads=[R_sg[pb], R_psu[pb]], writes=[R_hid[hb_]])
            for tt in range(4):
                yb_ = ny % 2; ny += 1
                for half in range(2):
                    for fc in range(4):
                        f.op("pe", lambda e, fc=fc, half=half, tt=tt: e.matmul(psd[half][:], hid[hb_][:, fc, tt * 128:(tt + 1) * 128], wd[wb][:, fc, half * 512:(half + 1) * 512], start=(fc == 0), stop=(fc == 3)),
                             reads=[R_wd[wb], R_hid[hb_]], writes=[R_psd[half]], acc=(fc > 0))
                    if half == 0:
                        f.op("dve", lambda e: e.tensor_copy(ysb[yb_][:, 0:512], psd[0][:]), reads=[R_psd[0]], writes=[R_ysb[yb_][0]])
                    else:
                        f.op("act", lambda e: e.activation(out=ysb[yb_][:, 512:1024], in_=psd[1][:], func=AF.Identity), reads=[R_psd[1]], writes=[R_ysb[yb_][1]])
                r0 = (j * 4 + tt) * 128
                f.dma("act", YS[r0:r0 + 128, :], ysb[yb_][:], reads=R_ysb[yb_], writes=[R_Ys[j]])
            if j + 1 < 32:
                w_cast(j + 1)
        Sd.close()
        nc.gpsimd.free_register(bc_reg)
        C = Scope(nc)
        xt = [C.sb("xt%d" % k, [128, D]) for k in range(2)]; R_xt = RL(2)
        ot = [C.sb("ot%d" % k, [128, D]) for k in range(2)]; R_ot = RL(2)
        ya = [C.sb("ya%d" % k, [128, D]) for k in range(2)]; R_ya = RL(2)
        yb2 = [C.sb("yb%d" % k, [128, D]) for k in range(2)]; R_yb = RL(2)
        tmp = C.sb("tmp", [128, D]); R_tmp = Res()
        small = C.sb("small", [128, 16]); R_small = Res()

        class _V:
            def __init__(self, ap): self.ap = ap
            def __getitem__(self, k): return self.ap
        for j, t in enumerate(tiles_all):
            b = j % 2
            f.dma("sp", xt[b][:], XR[t * 128:(t + 1) * 128, :], reads=[R_XR[t]], writes=[R_xt[b]])
            f._dma_common("pool", lambda e: e.indirect_dma_start(out=ya[b][:], out_offset=None, in_=YS, in_offset=IOA(ap=posA_i[:, j:j + 1], axis=0)), R_Ys + RB, [R_ya[b]])
            f._dma_common("pool", lambda e: e.indirect_dma_start(out=yb2[b][:], out_offset=None, in_=YS, in_offset=IOA(ap=posB_i[:, j:j + 1], axis=0)), R_Ys + RB, [R_yb[b]])
            f.op("dve", lambda e: e.tensor_scalar(out=ya[b][:], in0=ya[b][:], scalar1=wA[:, j:j + 1], scalar2=None, op0=ALU.mult), reads=[R_ya[b]] + RB, writes=[R_ya[b]])
            f.op("dve", lambda e: e.scalar_tensor_tensor(out=ya[b][:], in0=yb2[b][:], scalar=wB[:, j:j + 1], in1=ya[b][:], op0=ALU.mult, op1=ALU.add), reads=[R_yb[b], R_ya[b]] + RB, writes=[R_ya[b]])
            resid_ln(C, xt[b], R_xt[b], _V(ya[b][:]), R_ya[b], (1 if t < 2 else 0), layer * 2 + 1, ot[b], R_ot[b], tmp, R_tmp, small, R_small)
            if final:
                f.dma("act", out_d[(t - 2) * 128:(t - 1) * 128, :], ot[b][:], reads=[R_ot[b]], writes=[R_out])
            else:
                f.dma("act", XR[t * 128:(t + 1) * 128, :], ot[b][:], reads=[R_ot[b]], writes=[R_XR[t]])
        C.close()
        P.close()

    def layer1_mixer():
        L = Scope(nc)
        kT2 = L.sb("kT2b", [128, 4, NTOK], BF16); R_kT = RL(NT, "kT")
        vaug = L.sb("vaugb", [128, NT, 576], BF16); R_v = RL(NT, "v")
        f.op("pool", lambda e: e.memset(vaug[:], 1.0), writes=R_v)
        R_oT = RL(NT, "oT")
        R_QT = RL(NT, "QT")
        S = Scope(nc)
        win = S.sb("win1", [128, 8, 1536], BF16); R_win = Res()
        f.dma("pool", win[:], odd_w_in.rearrange("(kc p) n -> p kc n", p=128), writes=[R_win])
        gq = S.sb("gq", [128, 2, 64]); R_gq = Res()
        f.dma("sp", gq[:, 0, :], q_norm.partition_broadcast(128), writes=[R_gq])
        f.dma("sp", gq[:, 1, :], k_norm.partition_broadcast(128), writes=[R_gq])
        xt = [S.sb("xt%d" % k, [128, D]) for k in range(2)]; R_xt = RL(2)
        h32 = S.sb("h32", [128, D]); R_h32 = Res()
        hT = [S.sb("hT%d" % k, [128, 8, 128], BF16) for k in range(2)]; R_hT = RL(2)
        ps_tp = S.ps("ps_tp", [128, 8, 128]); R_pstp = Res(x=True)
        ps_q = S.ps("ps_q", [128, 1536]); R_psq = Res(x=True)
        ps_t = S.ps("ps_t", [128, 16, 128], BF16); R_pst = Res(x=True)
        qk = S.sb("qk", [128, 20, 64]); R_qk = Res()
        sq = S.sb("sq", [128, 20, 64]); ss = S.sb("ss", [128, 20]); R_ss = Res()
        ra = S.sb("ra", [128, 20, 32]); rb = S.sb("rb", [128, 20, 32]); R_ra = Res(); R_rb = Res()
        tqk = S.sb("tqk", [128, 20, 64], BF16); R_tqk = Res()
        kd = S.sb("kd", [128, 4, 2, 64], BF16); R_kd = Res()
        qts = [S.sb("qts%d" % k, [128, 8, 128], BF16) for k in range(2)]; R_qts = RL(2)
        for t in range(NT):
            b = t % 2
            f.dma("sp", xt[b][:], XR[t * 128:(t + 1) * 128, :], reads=[R_XR[t]], writes=[R_xt[b]])
            which = 1 if t < 2 else 0
            mod_transpose(xt[b], R_xt[b], which, h32, R_h32, ps_tp, R_pstp, hT[b][:], R_hT[b])
            cols = slice(t * 128, (t + 1) * 128)
            lat = t >= 2
            ranges = ((0, 512), (512, 1024), (1024, 1536)) if lat else ((1024, 1536),)
            first = True
            for (n0, n1) in ranges:
                for kc in range(8):
                    f.op("pe", lambda e, kc=kc, n0=n0, n1=n1: e.matmul(ps_q[:, n0:n1], hT[b][:, kc, :], win[:, kc, n0:n1], start=(kc == 0), stop=(kc == 7)),
                         reads=[R_win, R_hT[b]], writes=[R_psq], acc=(not first))
                    first = False
            h0 = 0 if lat else 16
            nh = 20 - h0
            pv = ps_q[:, h0 * 64:1280].rearrange("p (h d) -> p h d", d=64)
            qkv_ = qk[:, h0:20, :]
            f.op("act", lambda e: e.activation(out=sq[:, h0:20, :], in_=pv, func=AF.Square), reads=[R_psq], writes=[R_ss])
            f.op("dve", lambda e: e.tensor_reduce(out=ss[:, h0:20], in_=sq[:, h0:20, :], axis=AX.X, op=ALU.add), reads=[R_ss], writes=[R_ss])
            f.op("dve", lambda e: e.tensor_scalar(out=ss[:, h0:20], in0=ss[:, h0:20], scalar1=1.0 / 64.0, scalar2=RMS_EPS, op0=ALU.mult, op1=ALU.add), reads=[R_ss], writes=[R_ss])
            f.op("act", lambda e: e.activation(out=ss[:, h0:20], in_=ss[:, h0:20], func=AF.Sqrt), reads=[R_ss], writes=[R_ss])
            f.op("dve", lambda e: e.reciprocal(ss[:, h0:20], ss[:, h0:20]), reads=[R_ss], writes=[R_ss])
            f.op("dve", lambda e: e.tensor_tensor(qkv_, pv, ss[:, h0:20].unsqueeze(2).broadcast_to([128, nh, 64]), ALU.mult), reads=[R_psq, R_ss], writes=[R_qk])
            if lat:
                f.op("dve", lambda e: e.tensor_tensor(qk[:, 0:16, :], qk[:, 0:16, :], gq[:, 0, :].unsqueeze(1).broadcast_to([128, 16, 64]), ALU.mult), reads=[R_qk, R_gq], writes=[R_qk])
            f.op("dve", lambda e: e.tensor_tensor(qk[:, 16:20, :], qk[:, 16:20, :], gq[:, 1, :].unsqueeze(1).broadcast_to([128, 4, 64]), ALU.mult), reads=[R_qk, R_gq], writes=[R_qk])
            if lat:
                q4 = qk[:].rearrange("p h (two f) -> p h two f", two=2)
                o4 = tqk[:].rearrange("p h (two f) -> p h two f", two=2)
                cosb = rope[:, 0, t - 2, :].unsqueeze(1).broadcast_to([128, 20, 32])
                sinb = rope[:, 1, t - 2, :].unsqueeze(1).broadcast_to([128, 20, 32])
                f.op("dve", lambda e: e.tensor_tensor(ra[:], q4[:, :, 0, :], cosb, ALU.mult), reads=[R_qk, R_rope], writes=[R_ra])
                f.op("dve", lambda e: e.tensor_tensor(rb[:], q4[:, :, 1, :], sinb, ALU.mult), reads=[R_qk, R_rope], writes=[R_rb])
                f.op("dve", lambda e: e.tensor_tensor(o4[:, :, 0, :], ra[:], rb[:], ALU.subtract), reads=[R_ra, R_rb], writes=[R_tqk])
                f.op("dve", lambda e: e.tensor_tensor(ra[:], q4[:, :, 1, :], cosb, ALU.mult), reads=[R_qk, R_rope, R_tqk], writes=[R_ra])
                f.op("dve", lambda e: e.tensor_tensor(rb[:], q4[:, :, 0, :], sinb, ALU.mult), reads=[R_qk, R_rope, R_tqk], writes=[R_rb])
                f.op("dve", lambda e: e.tensor_tensor(o4[:, :, 1, :], ra[:], rb[:], ALU.add), reads=[R_ra, R_rb], writes=[R_tqk])
            else:
                f.op("dve", lambda e: e.tensor_copy(tqk[:, 16:20, :], qk[:, 16:20, :]), reads=[R_qk], writes=[R_tqk])
            for a in range(4):
                f.op("dve", lambda e, a=a: e.tensor_copy(vaug[:, t, 64 + 128 * a:128 + 128 * a], ps_q[:, 1280 + 64 * a:1344 + 64 * a]), reads=[R_psq], writes=[R_v[t]])
            f.op("dve", lambda e: e.tensor_copy(kd[:, :, 0, :], tqk[:, 16:20, :]), reads=[R_tqk], writes=[R_kd])
            f.op("dve", lambda e: e.tensor_copy(kd[:, :, 1, :], tqk[:, 16:20, :]), reads=[R_tqk], writes=[R_kd])
            firstt = True
            if lat:
                for pr in range(8):
                    f.op("pe", lambda e, pr=pr: e.transpose(ps_t[:, pr, :], tqk[:, 2 * pr:2 * pr + 2, :].rearrange("p a d -> p (a d)"), identb[:]),
                         reads=[R_tqk, R_identb], writes=[R_pst], acc=(not firstt))
                    firstt = False
            for a in range(4):
                f.op("pe", lambda e, a=a: e.transpose(ps_t[:, 8 + a, :], kd[:, a, :, :].rearrange("p a d -> p (a d)"), identb[:]),
                     reads=[R_kd, R_identb], writes=[R_pst], acc=(not firstt))
                firstt = False
            f.op("act", lambda e: e.activation(out=kT2[:, :, cols], in_=ps_t[:, 8:12, :], func=AF.Identity), reads=[R_pst], writes=[R_kT[t]])
            if lat:
                f.op("dve", lambda e: e.tensor_copy(qts[b][:], ps_t[:, 0:8, :]), reads=[R_pst], writes=[R_qts[b]])
                f.dma("act", QT[:, :, (t - 2) * 128:(t - 1) * 128].rearrange("a p n -> p a n"), qts[b][:], reads=[R_qts[b]], writes=[R_QT[t]])
        S.close()
        S = Scope(nc)
        qb = [S.sb("qb%d" % k, [128, 2, 512], BF16) for k in range(2)]; R_qb = RL(2)
        ps_s = [S.ps("ps_s%d" % k, [128, 1024]) for k in range(2)]; R_pss = RL(2)
        ps_o = [S.ps("ps_o%d" % k, [128, 512]) for k in range(4)]; R_pso = RL(4)
        pT = [S.sb("pT%d" % k, [128, 1024], BF16) for k in range(3)]; R_pT = RL(3)
        dtmp = S.sb("dtmp", [128, 512]); R_dt = Res()
        ost = [S.sb("ost%d" % k, [128, 2, 512], BF16) for k in range(2)]; R_ost = RL(2)
        it = 0
        nq = 0
        for kvh in range(4):
            for qblk in range(8):
                qbi = nq % 2; nq += 1
                tq = [R_QT[2 + qblk * 4 + k] for k in range(4)]
                f.dma("sp", qb[qbi][:], QT[2 * kvh:2 * kvh + 2, :, qblk * 512:(qblk + 1) * 512].rearrange("a p n -> p a n"), reads=tq, writes=[R_qb[qbi]])
                items = [(kt, p_) for kt in range(NT) for p_ in range(2)]
                it0 = it; it += len(items)

                def front(idx):
                    kt, p_ = items[idx]
                    si = (it0 + idx) % 2
                    pi_ = (it0 + idx) % 3
                    for half in range(2):
                        base = 64 * half
                        f.op("pe", lambda e, half=half, base=base: e.matmul(ps_s[si][:, half * 512:(half + 1) * 512], kT2[base:base + 64, kvh, kt * 128:(kt + 1) * 128], qb[qbi][base:base + 64, p_, :], start=True, stop=True),
                             reads=[R_kT[kt], R_qb[qbi]], writes=[R_pss[si]], acc=(half > 0))
                    f.op("act", lambda e: e.activation(out=pT[pi_][:], in_=ps_s[si][:], func=AF.Exp, scale=0.125), reads=[R_pss[si]], writes=[R_pT[pi_]])

                def back(idx):
                    kt, p_ = items[idx]
                    pi_ = (it0 + idx) % 3
                    for half in range(2):
                        hh = 2 * p_ + half
                        voff = (64 if half == 0 else 0) + 128 * kvh
                        f.op("pe", lambda e, half=half, hh=hh, voff=voff: e.matmul(ps_o[hh][:], vaug[:, kt, voff:voff + 128], pT[pi_][:, half * 512:(half + 1) * 512], start=(kt == 0), stop=(kt == NT - 1)),
                             reads=[R_v[kt], R_pT[pi_]], writes=[R_pso[hh]], acc=(kt > 0))
                LA = 1
                for i_ in range(len(items) + LA):
                    if i_ < len(items):
                        front(i_)
                    if i_ >= LA:
                        back(i_ - LA)
                for hh in range(4):
                    h = kvh * 4 + hh
                    nb, db = (0, 64) if h % 2 == 0 else (64, 0)
                    f.op("dve", lambda e: e.reciprocal(dtmp[nb:nb + 64, :], ps_o[hh][db:db + 64, :]), reads=[R_pso[hh]], writes=[R_dt])
                    f.op("dve", lambda e: e.tensor_tensor(ost[qbi][nb:nb + 64, hh // 2, :], ps_o[hh][nb:nb + 64, :], dtmp[nb:nb + 64, :], ALU.mult),
                         reads=[R_pso[hh], R_dt], writes=[R_ost[qbi]])
                f.dma("act", OT[2 * kvh:2 * kvh + 2, :, qblk * 512:(qblk + 1) * 512].rearrange("a p n -> p a n"), ost[qbi][:], reads=[R_ost[qbi]],
                      writes=[R_oT[2 + qblk * 4 + k] for k in range(4)])
        S.close()
        L.close()
        M = Scope(nc)
        mixt = [M.sb("mixt%d" % k, [128, 8, 128], BF16) for k in range(2)]; R_mixt = RL(2)

        def mix_loader(t, b):
            f.dma("sp", mixt[b][:], OT[:, :, (t - 2) * 128:(t - 1) * 128].rearrange("a p n -> p a n"), reads=[R_oT[t]], writes=[R_mixt[b]])
            return [mixt[b][:, k, :] for k in range(8)], [R_mixt[b]]
        out_phase(1, odd_w_out, mix_loader, range(2, NT))
        M.close()

    phase_mod(0, 0)
    if stop_after == "mod":
        f.dma("sp", dbg[0:128, :], mod[:, 0].rearrange("p a d -> p (a d)"), reads=[R_mod], writes=[R_dbg])
        f.dma("sp", dbg[128:256, :], mod[:, 1].rearrange("p a d -> p (a d)"), reads=[R_mod], writes=[R_dbg])
    else:
        layer0_mixer()
    if stop_after in ("in0", "s5", "mod", "h0", "qkv", "win"):
        pass
    else:
        if stop_after == "mix0":
            pass
        else:
            phase_mod(0, 1)
            (ffn_phase if 'dense' in DBG_SKIP else ffn_sparse)(0, list(range(NT)), final=False)
            if stop_after != "l0":
                phase_mod(1, 0)
                layer1_mixer()
                if stop_after != "mix1":
                    phase_mod(1, 1)
                    (ffn_phase if 'dense' in DBG_SKIP else ffn_sparse)(1, list(range(2, NT)), final=True)
    if stop_after is not None and stop_after not in ("in0", "s5", "mod", "h0", "qkv", "win"):
        S = Scope(nc)
        tt = S.sb("dumpt", [128, D]); R_t = Res()
        for t in range(NT):
            f.dma("sp", tt[:], XR[t * 128:(t + 1) * 128, :], reads=[R_XR[t]], writes=[R_t])
            f.dma("sp", dbg[t * 128:(t + 1) * 128, :], tt[:], reads=[R_t], writes=[R_dbg])
        S.close()
    f.finish()
    Scope.FWREF = None
    G.close()
    f.close()
    return nc


_CONST = None


def _consts():
    global _CONST
    if _CONST is None:
        ident = np.eye(128, dtype=np.float32)
        n_freq = 16
        inv_freq = (10000.0 ** (-np.arange(n_freq, dtype=np.float32) / n_freq)).astype(np.float32)
        pos = np.arange(4096)
        r = (pos // 64).astype(np.float32); cc = (pos % 64).astype(np.float32)
        ang = np.concatenate([r[:, None] * inv_freq, cc[:, None] * inv_freq], -1).astype(np.float32)
        cos = np.cos(ang).astype(np.float32).reshape(32, 128, 32).transpose(1, 0, 2)
        sin = np.sin(ang).astype(np.float32).reshape(32, 128, 32).transpose(1, 0, 2)
        rope = np.ascontiguousarray(np.stack([cos, sin], axis=1))
        k = np.arange(128)[:, None]; q = np.arange(128)[None, :]
        mask = np.stack([(q <= k), (k <= q), (k < q)], axis=1).astype(np.float32)
        pc = (np.arange(128, dtype=np.float32)[:, None] + 128.0 * np.arange(4, dtype=np.float32)[None, :]).astype(np.float32)
        jidx = np.broadcast_to(np.arange(128, dtype=np.float32)[None, :], (128, 128)).copy()
        _CONST = {"k_ident": ident, "k_rope": rope, "k_mask": np.ascontiguousarray(mask), "k_jidx": jidx, "k_pc": np.ascontiguousarray(pc)}
    return _CONST


def make_in_map(inputs, b):
    f32 = lambda a: np.ascontiguousarray(np.asarray(a, dtype=np.float32))
    m = {
        "x": f32(inputs["x"][b]), "ctx": f32(inputs["ctx"][b]), "c": f32(inputs["c"][b:b + 1]),
        "c_ctx": f32(inputs["c_ctx"]).reshape(1, D),
        "ada_w": f32(inputs["ada_w"]), "ada_b": f32(inputs["ada_b"]), "ln_g": f32(inputs["ln_g"]), "ln_b": f32(inputs["ln_b"]),
        "even_w_in": f32(inputs["even_w_in"][0]), "even_w_out": f32(inputs["even_w_out"][0]),
        "s5_lam_re": f32(inputs["s5_lam_re"][0]), "s5_lam_im": f32(inputs["s5_lam_im"][0]), "s5_log_step": f32(inputs["s5_log_step"][0]),
        "s5_b_re": f32(inputs["s5_b_re"][0]), "s5_b_im": f32(inputs["s5_b_im"][0]),
        "s5_c_re": f32(inputs["s5_c_re"][0]), "s5_c_im": f32(inputs["s5_c_im"][0]),
        "s5_d": f32(inputs["s5_d"][0]), "s5_w_glu": f32(inputs["s5_w_glu"][0]), "s5_b_glu": f32(inputs["s5_b_glu"][0]),
        "win_sink": f32(inputs["win_sink"][0]),
        "odd_w_in": f32(inputs["odd_w_in"][0]), "odd_w_out": f32(inputs["odd_w_out"][0]),
        "odd_q_norm": f32(inputs["odd_q_norm"][0]), "odd_k_norm": f32(inputs["odd_k_norm"][0]),
        "router_w": f32(inputs["router_w"]), "router_bias": f32(inputs["router_bias"]),
        "moe_w_gate": f32(inputs["moe_w_gate"]), "moe_w_up": f32(inputs["moe_w_up"]), "moe_w_down": f32(inputs["moe_w_down"]),
    }
    m.update(_consts())
    return m


def kernel(**inputs):
    nc = build_program()
    shared = make_in_map(inputs, 0)
    in_maps = []
    for b in range(8):
        m = dict(shared)
        m["x"] = np.ascontiguousarray(np.asarray(inputs["x"][b], dtype=np.float32))
        m["ctx"] = np.ascontiguousarray(np.asarray(inputs["ctx"][b], dtype=np.float32))
        m["c"] = np.ascontiguousarray(np.asarray(inputs["c"][b:b + 1], dtype=np.float32))
        in_maps.append(m)
    res = run_bass_kernel_spmd(nc, in_maps, core_ids=list(range(8)))
    return np.stack([np.asarray(r["out"], dtype=np.float32) for r in res.results], axis=0)
```

```python
import math
import os
DBG_SKIP = os.environ.get('DBG_SKIP', '').split(',')
DBG_NT = int(os.environ.get('DBG_NT', '34'))
from contextlib import ExitStack
import numpy as np
import ml_dtypes
import concourse.bass as bass
import concourse.mybir as mybir
from concourse.bass_utils import run_bass_kernel_spmd

F32 = mybir.dt.float32
BF16 = mybir.dt.bfloat16
I32 = mybir.dt.int32
ALU = mybir.AluOpType
AF = mybir.ActivationFunctionType
AX = mybir.AxisListType

SEM_LIMIT = 30000
NT = 34
NTOK = 4352
D = 1024
ALPHA = 4.0 ** 0.25
LN_EPS = 1e-5
RMS_EPS = 1e-6
TWO_PI = 2.0 * math.pi
CW1 = 6.28125
CW2 = TWO_PI - CW1


class Res:
    __slots__ = ("name", "w", "r", "x")

    def __init__(self, name="", x=False):
        self.name = name
        self.w = None
        self.r = []
        self.x = x


def RL(n, name="r"):
    return [Res("%s%d" % (name, i)) for i in range(n)]


class EngState:
    def __init__(self, fw, name, eng):
        self.fw = fw
        self.name = name
        self.eng = eng
        self.count = 0
        self.epoch = 0
        self.known = {}
        self._new_sem()

    def _new_sem(self):
        self.sem_key = "%s_e%d" % (self.name, self.epoch)
        self.sem = self.fw.new_sem(self.sem_key)
        self.count = 0
        self.epoch += 1


class FW:
    def __init__(self, nc, n_dma_sems=10):
        self.nc = nc
        self.es = ExitStack()
        self.sems = {}
        self.engs = {}
        for name, eng in (("pe", nc.tensor), ("act", nc.scalar), ("dve", nc.vector),
                          ("pool", nc.gpsimd), ("sp", nc.sync)):
            self.engs[name] = EngState(self, name, eng)
        self.dma_pool = {}
        for q in ("sp", "act", "pool"):
            lst = []
            for i in range(n_dma_sems):
                key = "dma_%s_%d" % (q, i)
                lst.append([key, self.new_sem(key), 0])
            self.dma_pool[q] = [lst, 0]
        self.n_instr = 0
        self.n_waits = 0

    def new_sem(self, key):
        s = self.es.enter_context(self.nc.semaphore(key))
        self.sems[key] = s
        return s

    def _wait(self, E, ev):
        if ev is None:
            return
        key, val = ev
        if E.known.get(key, 0) >= val:
            return
        E.eng.wait_ge(self.sems[key], val)
        E.known[key] = val
        self.n_waits += 1

    def _deps(self, E, reads, writes, acc=False):
        for r in reads:
            self._wait(E, r.w)
            if r.x:
                for ev in r.r:
                    if ev[0] != E.sem_key:
                        self._wait(E, ev)
        for w in writes:
            if not ((acc or E.name == "pe") and w.w is not None and w.w[0] == E.sem_key):
                self._wait(E, w.w)
            for ev in w.r:
                self._wait(E, ev)

    def _commit(self, ev, reads, writes):
        for r in reads:
            r.r.append(ev)
            if len(r.r) > 16:
                d = {}
                for k, v in r.r:
                    if d.get(k, 0) < v:
                        d[k] = v
                r.r = list(d.items())
        for w in writes:
            w.w = ev
            w.r = []

    def op(self, ename, fn, reads=(), writes=(), acc=False):
        E = self.engs[ename]
        if E.count >= SEM_LIMIT:
            E._new_sem()
        self._deps(E, reads, writes, acc=acc)
        ins = fn(E.eng)
        E.count += 1
        ins.then_inc(E.sem, 1)
        self._commit((E.sem_key, E.count), reads, writes)
        self.n_instr += 1
        return ins

    def _dma_common(self, qname, issue, reads, writes):
        E = self.engs[qname]
        pool, idx = self.dma_pool[qname]
        ent = pool[idx % len(pool)]
        self.dma_pool[qname][1] = idx + 1
        key, sem, val = ent
        if val > 0:
            self._wait(E, (key, val))
        if val + 16 > SEM_LIMIT:
            key = key + "n"
            sem = self.new_sem(key)
            val = 0
            ent[0], ent[1] = key, sem
        self._deps(E, reads, writes)
        ins = issue(E.eng)
        val += 16
        ent[2] = val
        ins.then_inc(sem, 16)
        ev = (key, val)
        self._commit(ev, reads, writes)
        self.n_instr += 1
        return ev

    def dma(self, qname, out, in_, reads=(), writes=(), **kw):
        return self._dma_common(qname, lambda e: e.dma_start(out=out, in_=in_, **kw), reads, writes)

    def barrier(self):
        evs = []
        for q in self.dma_pool:
            for key, sem, val in self.dma_pool[q][0]:
                if val > 0:
                    evs.append((key, val))
        for n, e in self.engs.items():
            if e.count > 0:
                evs.append((e.sem_key, e.count))
        for n, E in self.engs.items():
            for ev in evs:
                if ev[0] != E.sem_key:
                    self._wait(E, ev)

    def finish(self):
        E = self.engs["sp"]
        for q in self.dma_pool:
            for key, sem, val in self.dma_pool[q][0]:
                if val > 0:
                    self._wait(E, (key, val))
        for n, e in self.engs.items():
            if e.count > 0:
                self._wait(E, (e.sem_key, e.count))

    def close(self):
        self.es.close()


class Scope:
    FWREF = None

    def __init__(self, nc):
        self.nc = nc
        self.es = ExitStack()

    CNT = [0]

    def sb(self, name, shape, dtype=F32):
        Scope.CNT[0] += 1
        return self.es.enter_context(self.nc.sbuf_tensor("%s_%d" % (name, Scope.CNT[0]), list(shape), dtype))

    def ps(self, name, shape, dtype=F32):
        Scope.CNT[0] += 1
        return self.es.enter_context(self.nc.psum_tensor("%s_%d" % (name, Scope.CNT[0]), list(shape), dtype))

    def close(self):
        if Scope.FWREF is not None:
            Scope.FWREF.barrier()
        self.es.close()


def rev_ap(ap2d, n):
    last = ap2d[:, n - 1:n]
    return bass.AP(tensor=ap2d.tensor, offset=last.offset, ap=[list(ap2d.ap[0]), [-1, n]])


def build_program(stop_after=None, dbg_shape=None):
    nc = bass.Bass("TRN2", target_bir_lowering=False)

    def din(name, shape, dt=F32):
        return nc.dram_tensor(name, list(shape), dt, kind="ExternalInput").ap()

    x_d = din("x", [4096, D]); ctx_d = din("ctx", [256, D])
    c_d = din("c", [1, D]); cctx_d = din("c_ctx", [1, D])
    ada_w = din("ada_w", [2, D, 6 * D]); ada_b = din("ada_b", [2, 6 * D])
    ln_g = din("ln_g", [2, 2, D]); ln_b = din("ln_b", [2, 2, D])
    even_w_in = din("even_w_in", [D, 1280]); even_w_out = din("even_w_out", [D, D])
    lam_re = din("s5_lam_re", [2, 32, 64]); lam_im = din("s5_lam_im", [2, 32, 64])
    log_step = din("s5_log_step", [2, 32])
    b_re = din("s5_b_re", [2, 32, 64, 16]); b_im = din("s5_b_im", [2, 32, 64, 16])
    c_re = din("s5_c_re", [2, 32, 16, 64]); c_im = din("s5_c_im", [2, 32, 16, 64])
    s5_d = din("s5_d", [512]); w_glu = din("s5_w_glu", [512, 512]); b_glu = din("s5_b_glu", [512])
    win_sink = din("win_sink", [8])
    odd_w_in = din("odd_w_in", [D, 1536]); odd_w_out = din("odd_w_out", [D, D])
    q_norm = din("odd_q_norm", [64]); k_norm = din("odd_k_norm", [64])
    router_w = din("router_w", [D, 32]); router_b = din("router_bias", [32])
    w_gate = din("moe_w_gate", [2, 32, D, 512]); w_up = din("moe_w_up", [2, 32, D, 512])
    w_down = din("moe_w_down", [2, 32, 512, D])
    k_ident = din("k_ident", [128, 128]); k_rope = din("k_rope", [128, 2, 32, 32])
    k_mask = din("k_mask", [128, 3, 128]); k_jidx = din("k_jidx", [128, 128]); k_pc = din("k_pc", [128, 4])
    out_d = nc.dram_tensor("out", [4096, D], F32, kind="ExternalOutput").ap()
    XR = nc.dram_tensor("xr", [NTOK, D], F32, kind="Internal").ap()
    QT = nc.dram_tensor("qt_scr", [8, 128, 4096], BF16, kind="Internal").ap()
    OT = nc.dram_tensor("ot_scr", [8, 128, 4096], BF16, kind="Internal").ap()
    ATD = nc.dram_tensor("at_scr", [4, 128, NTOK], BF16, kind="Internal").ap()
    NS = 49
    XS = nc.dram_tensor("xs_scr", [NS * 512, D], BF16, kind="Internal").ap()
    YS = nc.dram_tensor("ys_scr", [NS * 512, D], F32, kind="Internal").ap()
    wg_all = w_gate.rearrange("l e (kk two) n -> (l e kk) (two n)", two=2)
    wu_all = w_up.rearrange("l e (kk two) n -> (l e kk) (two n)", two=2)
    wd_all = w_down.rearrange("l e f n -> (l e f) n")
    wg_rows = [wg_all, wg_all]; wu_rows = [wu_all, wu_all]; wd_rows = [wd_all, wd_all]
    dbg = None
    if dbg_shape is not None:
        dbg = nc.dram_tensor("dbg", list(dbg_shape), F32, kind="ExternalOutput").ap()

    f = FW(nc)
    Scope.FWREF = f
    G = Scope(nc)
    R_XR = RL(NT, "xr")
    R_out = Res("out")
    R_dbg = Res("dbg")

    ident = G.sb("ident", [128, 128]); R_ident = Res()
    identb = G.sb("identb", [128, 128], BF16); R_identb = Res()
    f.dma("sp", ident[:], k_ident, writes=[R_ident])
    f.op("dve", lambda e: e.tensor_copy(identb[:], ident[:]), reads=[R_ident], writes=[R_identb])
    rope = G.sb("rope", [128, 2, 32, 32]); R_rope = Res()
    f.dma("sp", rope[:], k_rope, writes=[R_rope])
    maskf = G.sb("maskf", [128, 3, 128]); maskb = G.sb("maskb", [128, 3, 128], BF16); R_mask = Res()
    f.dma("sp", maskf[:], k_mask, writes=[R_mask])
    f.op("dve", lambda e: e.tensor_copy(maskb[:], maskf[:]), reads=[R_mask], writes=[R_mask])
    R_crep = Res()
    ctmp = G.sb("ctmp", [128, 2, 8]); R_ctmp = Res()
    f.dma("sp", ctmp[:, 0, :], c_d.rearrange("o (kc p) -> p (o kc)", p=128), writes=[R_ctmp], allow_slow_non_contiguous=True)
    f.dma("sp", ctmp[:, 1, :], cctx_d.rearrange("o (kc p) -> p (o kc)", p=128), writes=[R_ctmp], allow_slow_non_contiguous=True)
    f.op("act", lambda e: e.activation(out=ctmp[:], in_=ctmp[:], func=AF.Silu), reads=[R_ctmp], writes=[R_ctmp])
    lng = G.sb("lng", [128, D]); lnb = G.sb("lnb", [128, D]); R_ln = Res()

    def load_ln(li):
        f.dma("sp", lng[:], ln_g[li // 2, li % 2].partition_broadcast(128), writes=[R_ln])
        f.dma("sp", lnb[:], ln_b[li // 2, li % 2].partition_broadcast(128), writes=[R_ln])
    epsc = G.sb("epsc", [128, 1]); R_eps = Res()
    f.op("dve", lambda e: e.memset(epsc[:], LN_EPS), writes=[R_eps])

    R_XsZ = RL(28, "xsz")
    ZS = Scope(nc)
    zt = ZS.sb("zt", [128, 7, D], BF16); R_zt = Res()
    f.op("pool", lambda e: e.memset(zt[:], 0.0), writes=[R_zt])
    for k in range(28):
        f.dma(("sp", "act")[k % 2], XS[k * 896:(k + 1) * 896, :].rearrange("(a p) d -> p a d", p=128), zt[:], reads=[R_zt], writes=[R_XsZ[k]])
    ZS.close()

    mod = G.sb("mod", [128, 2, 3, D]); R_mod = Res("mod")

    def dump(ap_sb, rows, cols, reads, r0=0, c0=0):
        f.dma("sp", dbg[r0:r0 + rows, c0:c0 + cols], ap_sb, reads=reads, writes=[R_dbg])

    def phase_mod(i, s):
        S = Scope(nc)
        crep = S.sb("crep", [128, 2, 8, 128])
        f.op("dve", lambda e: e.tensor_copy(crep[:], ctmp[:].unsqueeze(3).broadcast_to([128, 2, 8, 128])), reads=[R_ctmp], writes=[R_crep])
        slab = [S.sb("slab%d" % k, [128, 8, 512]) for k in range(2)]; R_slab = RL(2)
        adb = [S.sb("adb%d" % k, [128, 512]) for k in range(2)]; R_adb = RL(2)
        psm = [S.ps("psm%d" % k, [128, 512]) for k in range(2)]; R_psm = RL(2)
        n = 0
        for blk in range(6):
            c0 = s * 3072 + blk * 512
            bi = blk % 2
            f.dma("sp", slab[bi][:], ada_w[i, :, c0:c0 + 512].rearrange("(kc p) n -> p kc n", p=128), writes=[R_slab[bi]])
            f.dma("act", adb[bi][:], ada_b[i, c0:c0 + 512].partition_broadcast(128), writes=[R_adb[bi]])
            k, half = blk // 2, blk % 2
            for which in range(2):
                pi = n % 2; n += 1
                for kc in range(8):
                    f.op("pe", lambda e, kc=kc: e.matmul(psm[pi][:], crep[:, which, kc, :], slab[bi][:, kc, :], start=(kc == 0), stop=(kc == 7)),
                         reads=[R_crep, R_slab[bi]], writes=[R_psm[pi]], acc=(kc > 0))
                dst = mod[:, which, k, half * 512:(half + 1) * 512]
                f.op("dve", lambda e: e.scalar_tensor_tensor(out=dst, in0=psm[pi][:], scalar=(1.0 if k == 1 else 0.0), in1=adb[bi][:], op0=ALU.add, op1=ALU.add),
                     reads=[R_psm[pi], R_adb[bi]], writes=[R_mod])
        S.close()

    def resid_ln(S, xt, R_xt, o_ps, R_ops, which, li, out_t, R_outt, tmp, R_tmp, small, R_small):
        gate = mod[:, which, 2, :]
        f.op("dve", lambda e: e.tensor_tensor(tmp[:], o_ps[:], gate, ALU.mult), reads=[R_ops, R_mod], writes=[R_tmp])
        f.op("dve", lambda e: e.scalar_tensor_tensor(out=tmp[:], in0=xt[:], scalar=ALPHA, in1=tmp[:], op0=ALU.mult, op1=ALU.add),
             reads=[R_xt, R_tmp], writes=[R_tmp])
        f.op("dve", lambda e: e.bn_stats(small[:, 0:6], tmp[:, 0:512]), reads=[R_tmp], writes=[R_small])
        f.op("dve", lambda e: e.bn_stats(small[:, 6:12], tmp[:, 512:1024]), reads=[R_tmp], writes=[R_small])
        f.op("dve", lambda e: e.bn_aggr(small[:, 12:14], small[:, 0:12]), reads=[R_small], writes=[R_small])
        f.op("act", lambda e: e.activation(out=small[:, 14:15], in_=small[:, 13:14], func=AF.Sqrt, bias=epsc[:], scale=1.0), reads=[R_small, R_eps], writes=[R_small])
        f.op("dve", lambda e: e.reciprocal(small[:, 15:16], small[:, 14:15]), reads=[R_small], writes=[R_small])
        f.op("dve", lambda e: e.tensor_scalar(out=tmp[:], in0=tmp[:], scalar1=small[:, 12:13], scalar2=small[:, 15:16], op0=ALU.subtract, op1=ALU.mult),
             reads=[R_tmp, R_small], writes=[R_tmp])
        f.op("dve", lambda e: e.tensor_tensor(tmp[:], tmp[:], lng[:], ALU.mult), reads=[R_tmp, R_ln], writes=[R_tmp])
        f.op("dve", lambda e: e.tensor_tensor(out_t[:], tmp[:], lnb[:], ALU.add), reads=[R_tmp, R_ln], writes=[R_outt])

    def mod_transpose(xt, R_xt, which, h32, R_h32, ps_tp, R_pstp, hT_dst, R_hT, h32T=None, R_h32T=None):
        f.op("dve", lambda e: e.tensor_tensor(h32[:], xt[:], mod[:, which, 1, :], ALU.mult), reads=[R_xt, R_mod], writes=[R_h32])
        f.op("dve", lambda e: e.tensor_tensor(h32[:], h32[:], mod[:, which, 0, :], ALU.add), reads=[R_h32, R_mod], writes=[R_h32])
        for kc in range(8):
            f.op("pe", lambda e, kc=kc: e.transpose(ps_tp[:, kc, :], h32[:, kc * 128:(kc + 1) * 128], ident[:]),
                 reads=[R_h32, R_ident], writes=[R_pstp], acc=(kc > 0))
        f.op("act", lambda e: e.activation(out=hT_dst, in_=ps_tp[:], func=AF.Identity), reads=[R_pstp], writes=[R_hT])
        if h32T is not None:
            f.op("dve", lambda e: e.tensor_copy(h32T[:], ps_tp[:]), reads=[R_pstp], writes=[R_h32T])

    def src_tile(layer, t):
        if layer == 0:
            return (ctx_d[t * 128:(t + 1) * 128, :] if t < 2 else x_d[(t - 2) * 128:(t - 1) * 128, :]), []
        return XR[t * 128:(t + 1) * 128, :], [R_XR[t]]

    def layer0_mixer():
        L = Scope(nc)
        U = Scope(nc)
        uT = U.sb("uT", [128, 4, NTOK], BF16); R_uT = RL(NT, "uT")
        aT, R_aT = uT, R_uT

        def inproj(do_u, qT=None, R_qT=None, kT2=None, R_kT=None, vaug=None, R_v=None):
            S = Scope(nc)
            wc0, wc1 = (0, 512) if do_u else (512, 1280)
            win = S.sb("win", [128, 8, wc1 - wc0], BF16); R_win = Res()
            f.dma("pool", win[:], even_w_in[:, wc0:wc1].rearrange("(kc p) n -> p kc n", p=128), writes=[R_win])
            xt1 = S.sb("xt1", [128, D]); xt = [xt1, xt1]; R1_ = Res(); R_xt = [R1_, R1_]
            h32 = S.sb("h32", [128, D]); R_h32 = Res()
            hT = [S.sb("hT%d" % k, [128, 8, 128], BF16) for k in range(2)]; R_hT = RL(2)
            ps_tp = S.ps("ps_tp", [128, 8, 128]); R_pstp = Res(x=True)
            ps_u = S.ps("ps_u", [128, 4, 128]); R_psu = Res()
            ps_q = S.ps("ps_q", [128, 1024]); R_psq = Res(x=True)
            ps_t = S.ps("ps_t", [128, 8, 128], BF16); R_pst = Res(x=True)
            ra = S.sb("ra", [128, 10, 32]); rb = S.sb("rb", [128, 10, 32]); R_ra = Res(); R_rb = Res()
            tqk = S.sb("tqk", [128, 640], BF16); R_tqk = Res()
            kd = S.sb("kd", [128, 2, 2, 64], BF16); R_kd = Res()
            for t in range(NT if do_u else min(NT, DBG_NT)):
                b = t % 2
                src, rs = src_tile(0, t)
                f.dma("sp", xt[b][:], src, reads=rs, writes=[R_xt[b]])
                which = 1 if t < 2 else 0
                mod_transpose(xt[b], R_xt[b], which, h32, R_h32, ps_tp, R_pstp, hT[b][:], R_hT[b])
                cols = slice(t * 128, (t + 1) * 128)
                if stop_after == "h0" and t == 0:
                    f.dma("sp", dbg[0:128, :], h32[:], reads=[R_h32], writes=[R_dbg])
                    hf = S.sb("hf", [128, 1024]); R_hf = Res()
                    f.op("dve", lambda e: e.tensor_copy(hf[:], hT[b][:].rearrange("p a b -> p (a b)")), reads=[R_hT[b]], writes=[R_hf])
                    f.dma("sp", dbg[128:256, :], hf[:], reads=[R_hf], writes=[R_dbg])
                    f.op("dve", lambda e: e.tensor_copy(hf[:], win[:, 0, 0:1024]), reads=[R_win], writes=[R_hf])
                    f.dma("sp", dbg[256:384, :], hf[:], reads=[R_hf], writes=[R_dbg])
                    S.close(); return
                if do_u:
                    for ct in range(4):
                        for kc in range(8):
                            f.op("pe", lambda e, ct=ct, kc=kc: e.matmul(ps_u[:, ct, :], win[:, kc, ct * 128:(ct + 1) * 128], hT[b][:, kc, :], start=(kc == 0), stop=(kc == 7)),
                                 reads=[R_win, R_hT[b]], writes=[R_psu], acc=(ct + kc > 0))
                    f.op("act", lambda e: e.activation(out=uT[:, :, cols], in_=ps_u[:], func=AF.Identity), reads=[R_psu], writes=[R_uT[t]])
                    continue
                for (n0, n1) in ((0, 512), (512, 768)):
                    for kc in range(8):
                        f.op("pe", lambda e, kc=kc, n0=n0, n1=n1: e.matmul(ps_q[:, n0:n1], hT[b][:, kc, :], win[:, kc, n0:n1], start=(kc == 0), stop=(kc == 7)),
                             reads=[R_win, R_hT[b]], writes=[R_psq], acc=(n0 + kc > 0))
                if 'rope' in DBG_SKIP:
                    continue
                if t >= 2:
                    pv = ps_q[:, 0:640].rearrange("p (h two f) -> p h two f", two=2, f=32)
                    ov = tqk[:].rearrange("p (h two f) -> p h two f", two=2, f=32)
                    cosb = rope[:, 0, t - 2, :].unsqueeze(1).broadcast_to([128, 10, 32])
                    sinb = rope[:, 1, t - 2, :].unsqueeze(1).broadcast_to([128, 10, 32])
                    f.op("dve", lambda e: e.tensor_tensor(ra[:], pv[:, :, 0, :], cosb, ALU.mult), reads=[R_psq, R_rope], writes=[R_ra])
                    f.op("dve", lambda e: e.tensor_tensor(rb[:], pv[:, :, 1, :], sinb, ALU.mult), reads=[R_psq, R_rope], writes=[R_rb])
                    f.op("dve", lambda e: e.tensor_tensor(ov[:, :, 0, :], ra[:], rb[:], ALU.subtract), reads=[R_ra, R_rb], writes=[R_tqk])
                    f.op("dve", lambda e: e.tensor_tensor(ra[:], pv[:, :, 1, :], cosb, ALU.mult), reads=[R_psq, R_rope, R_tqk], writes=[R_ra])
                    f.op("dve", lambda e: e.tensor_tensor(rb[:], pv[:, :, 0, :], sinb, ALU.mult), reads=[R_psq, R_rope, R_tqk], writes=[R_rb])
                    f.op("dve", lambda e: e.tensor_tensor(ov[:, :, 1, :], ra[:], rb[:], ALU.add), reads=[R_ra, R_rb], writes=[R_tqk])
                else:
                    f.op("act", lambda e: e.activation(out=tqk[:], in_=ps_q[:, 0:640], func=AF.Identity), reads=[R_psq], writes=[R_tqk])
                if 'vaug' in DBG_SKIP:
                    continue
                for a in range(2):
                    if 'novaug' in DBG_SKIP:
                        break
                    f.op("dve", lambda e, a=a: e.tensor_copy(vaug[:, t, 64 + 128 * a:128 + 128 * a], ps_q[:, 640 + 64 * a:704 + 64 * a]),
                         reads=[R_psq], writes=[R_v[t]])
                if 'nokd' in DBG_SKIP:
                    continue
                kv = tqk[:, 512:640].rearrange("p (a d) -> p a d", a=2)
                f.op("dve", lambda e: e.tensor_copy(kd[:, :, 0, :], kv), reads=[R_tqk], writes=[R_kd])
                f.op("dve", lambda e: e.tensor_copy(kd[:, :, 1, :], kv), reads=[R_tqk], writes=[R_kd])
                if 'tr' in DBG_SKIP:
                    continue
                for pr in range(4):
                    f.op("pe", lambda e, pr=pr: e.transpose(ps_t[:, pr, :], tqk[:, pr * 128:(pr + 1) * 128], identb[:]),
                         reads=[R_tqk, R_identb], writes=[R_pst], acc=(pr > 0))
                for a in range(2):
                    f.op("pe", lambda e, a=a: e.transpose(ps_t[:, 4 + a, :], kd[:, a, :, :].rearrange("p a d -> p (a d)"), identb[:]),
                         reads=[R_kd, R_identb], writes=[R_pst], acc=True)
                f.op("dve", lambda e: e.tensor_copy(qT[:, :, cols], ps_t[:, 0:4, :]), reads=[R_pst], writes=[R_qT[t]])
                f.op("act", lambda e: e.activation(out=kT2[:, :, cols], in_=ps_t[:, 4:6, :], func=AF.Identity), reads=[R_pst], writes=[R_kT[t]])
            S.close()

        inproj(True)
        if stop_after == "h0":
            U.close(); L.close(); return
        if stop_after == "in0":
            S = Scope(nc)
            t32 = S.sb("t32", [128, 512]); R_t = Res()
            for ct in range(4):
                for blk in range(2):
                    f.op("dve", lambda e: e.tensor_copy(t32[:], uT[:, ct, blk * 512:(blk + 1) * 512]), reads=R_uT, writes=[R_t])
                    dump(t32[:], 128, 512, [R_t], r0=ct * 128, c0=blk * 512)
            S.close(); U.close(); L.close()
            return
        if 's5' not in DBG_SKIP:
            s5_phase(L, uT, R_uT, aT, R_aT)
        if stop_after == "s5":
            S = Scope(nc)
            t32 = S.sb("t32", [128, 512]); R_t = Res()
            for ct in range(4):
                for blk in range(9):
                    c0 = blk * 512; n = min(512, NTOK - c0)
                    f.op("dve", lambda e: e.tensor_copy(t32[:, 0:n], aT[:, ct, c0:c0 + n]), reads=R_aT, writes=[R_t])
                    dump(t32[:, 0:n], 128, n, [R_t], r0=ct * 128, c0=c0)
            S.close(); U.close(); L.close()
            return
        R_ATD = Res("atd")
        for k in range(4):
            f.dma(("sp", "act")[k % 2], ATD[k], aT[:, k, :], reads=R_aT, writes=[R_ATD])
        U.close()
        oT = L.sb("oT", [128, 4, NTOK], BF16); R_oT = RL(NT, "oT")
        W = Scope(nc)
        qT = W.sb("qT", [128, 4, NTOK], BF16); R_qT = RL(NT, "qT")
        kT2 = W.sb("kT2", [128, 2, NTOK], BF16); R_kT = RL(NT, "kT")
        vaug = W.sb("vaug", [128, NT, 320], BF16); R_v = RL(NT, "v")
        f.op("pool", lambda e: e.memset(vaug[:], 1.0), writes=R_v)
        inproj(False, qT, R_qT, kT2, R_kT, vaug, R_v)
        if stop_after == "qkv":
            W.close(); L.close(); return
        win_phase(qT, R_qT, kT2, R_kT, vaug, R_v, oT, R_oT)
        W.close()
        if stop_after == "win":
            L.close(); return

        M = Scope(nc)
        mixt = [M.sb("mixa%d" % k, [128, 4, 128], BF16) for k in range(2)]; R_mixt = RL(2)

        def mix_loader(t, b):
            c0 = t * 128
            f.dma("sp", mixt[b][:], ATD[:, :, c0:c0 + 128].rearrange("a p n -> p a n"), reads=[R_ATD], writes=[R_mixt[b]])
            return [mixt[b][:, k, :] for k in range(4)] + [oT[:, k, c0:c0 + 128] for k in range(4)], [R_mixt[b], R_oT[t]]
        out_phase(0, even_w_out, mix_loader, range(NT))
        M.close()
        L.close()

    def sincos(S, ang, n, out_s, out_c, R, tag):
        ki = S.sb("ki_" + tag, [128, n], I32); kf = S.sb("kf_" + tag, [128, n]); rd = S.sb("rd_" + tag, [128, n])
        f.op("dve", lambda e: e.tensor_scalar(out=ki[:], in0=ang, scalar1=1.0 / TWO_PI, scalar2=None, op0=ALU.mult), reads=[R], writes=[R])
        f.op("dve", lambda e: e.tensor_copy(kf[:], ki[:]), reads=[R], writes=[R])
        f.op("dve", lambda e: e.scalar_tensor_tensor(out=rd[:], in0=kf[:], scalar=-CW1, in1=ang, op0=ALU.mult, op1=ALU.add), reads=[R], writes=[R])
        f.op("dve", lambda e: e.scalar_tensor_tensor(out=rd[:], in0=kf[:], scalar=-CW2, in1=rd[:], op0=ALU.mult, op1=ALU.add), reads=[R], writes=[R])
        f.op("dve", lambda e: e.tensor_scalar(out=rd[:], in0=rd[:], scalar1=3.1415925, scalar2=-3.1415925, op0=ALU.min, op1=ALU.max), reads=[R], writes=[R])
        f.op("act", lambda e: e.activation(out=out_s, in_=rd[:], func=AF.Sin), reads=[R], writes=[R])
        f.op("dve", lambda e: e.scalar_tensor_tensor(out=rd[:], in0=rd[:], scalar=-1.0, in1=rd[:], op0=ALU.mult, op1=ALU.max), reads=[R], writes=[R])
        f.op("dve", lambda e: e.tensor_scalar(out=rd[:], in0=rd[:], scalar1=-1.0, scalar2=math.pi / 2, op0=ALU.mult, op1=ALU.add), reads=[R], writes=[R])
        f.op("act", lambda e: e.activation(out=out_c, in_=rd[:], func=AF.Sin), reads=[R], writes=[R])

    def s5_phase(L, uT, R_uT, aT, R_aT):
        P = Scope(nc)
        R = Res("s5setup")
        prm = P.sb("prm", [128, 16, 32])
        dsk = P.sb("dsk", [128, 4]); bgl = P.sb("bgl", [128, 4])
        cs2 = P.sb("cs2", [128, 32, 2]); ncs2 = P.sb("ncs2", [128, 32, 2])
        jt = P.sb("jt", [128, 128]); f.dma("sp", jt[:], k_jidx, writes=[R])
        f.dma("sp", dsk[:], s5_d.rearrange("(c p) -> p c", p=128), writes=[R], allow_slow_non_contiguous=True)
        f.dma("sp", bgl[:], b_glu.rearrange("(c p) -> p c", p=128), writes=[R], allow_slow_non_contiguous=True)
        S = Scope(nc)
        st32 = S.sb("st32", [32, 3, 128]); lsr = S.sb("lsr", [32, 2])
        f.dma("sp", st32[:, 0, :], lam_re.rearrange("d (q g) n -> (d q) (g n)", g=2), writes=[R])
        f.dma("sp", st32[:, 1, :], lam_im.rearrange("d (q g) n -> (d q) (g n)", g=2), writes=[R])
        f.dma("sp", lsr[:], log_step.rearrange("d (q g) -> (d q) g", g=2), writes=[R])
        f.op("dve", lambda e: e.tensor_copy(st32[:, 2, :].rearrange("p (g n) -> p g n", g=2), lsr[:].unsqueeze(2).broadcast_to([32, 2, 64])), reads=[R], writes=[R])
        pst = S.ps("pst", [128, 4, 128])
        for k in range(3):
            f.op("pe", lambda e, k=k: e.transpose(pst[:, k, 0:32], st32[:, k, :], ident[0:32, 0:32]), reads=[R, R_ident], writes=[R], acc=(k > 0))
        f.op("dve", lambda e: e.tensor_copy(prm[:, 0:3, :], pst[:, 0:3, 0:32]), reads=[R], writes=[R])
        lr, li = prm[:, 0, :], prm[:, 1, :]
        dt, th, rr = prm[:, 3, :], prm[:, 4, :], prm[:, 5, :]
        f.op("act", lambda e: e.activation(out=dt, in_=prm[:, 2, :], func=AF.Exp), reads=[R], writes=[R])
        f.op("dve", lambda e: e.tensor_tensor(th, li, dt, ALU.mult), reads=[R], writes=[R])
        f.op("dve", lambda e: e.tensor_tensor(prm[:, 10, :], lr, dt, ALU.mult), reads=[R], writes=[R])
        f.op("act", lambda e: e.activation(out=rr, in_=prm[:, 10, :], func=AF.Exp), reads=[R], writes=[R])
        f.op("dve", lambda e: e.tensor_scalar(out=prm[:, 10, :], in0=th, scalar1=128.0, scalar2=None, op0=ALU.mult), reads=[R], writes=[R])
        sincos(S, prm[:, 10, :], 32, prm[:, 7, :], prm[:, 6, :], R, "a")
        sincos(S, th, 32, prm[:, 12, :], prm[:, 11, :], R, "b")
        abre, abim, den, t1, t2 = prm[:, 13, :], prm[:, 14, :], prm[:, 15, :], prm[:, 10, :], prm[:, 2, :]
        f.op("dve", lambda e: e.tensor_tensor(abre, rr, prm[:, 11, :], ALU.mult), reads=[R], writes=[R])
        f.op("dve", lambda e: e.tensor_scalar(out=abre, in0=abre, scalar1=-1.0, scalar2=None, op0=ALU.add), reads=[R], writes=[R])
        f.op("dve", lambda e: e.tensor_tensor(abim, rr, prm[:, 12, :], ALU.mult), reads=[R], writes=[R])
        f.op("dve", lambda e: e.tensor_tensor(den, lr, lr, ALU.mult), reads=[R], writes=[R])
        f.op("dve", lambda e: e.tensor_tensor(t1, li, li, ALU.mult), reads=[R], writes=[R])
        f.op("dve", lambda e: e.tensor_tensor(den, den, t1, ALU.add), reads=[R], writes=[R])
        f.op("dve", lambda e: e.reciprocal(den, den), reads=[R], writes=[R])
        f.op("dve", lambda e: e.tensor_tensor(t1, abre, lr, ALU.mult), reads=[R], writes=[R])
        f.op("dve", lambda e: e.tensor_tensor(t2, abim, li, ALU.mult), reads=[R], writes=[R])
        f.op("dve", lambda e: e.tensor_tensor(t1, t1, t2, ALU.add), reads=[R], writes=[R])
        f.op("dve", lambda e: e.tensor_tensor(prm[:, 8, :], t1, den, ALU.mult), reads=[R], writes=[R])
        f.op("dve", lambda e: e.tensor_tensor(t1, abim, lr, ALU.mult), reads=[R], writes=[R])
        f.op("dve", lambda e: e.tensor_tensor(t2, abre, li, ALU.mult), reads=[R], writes=[R])
        f.op("dve", lambda e: e.tensor_tensor(t1, t1, t2, ALU.subtract), reads=[R], writes=[R])
        f.op("dve", lambda e: e.tensor_tensor(prm[:, 9, :], t1, den, ALU.mult), reads=[R], writes=[R])
        f.op("dve", lambda e: e.tensor_copy(cs2[:, :, 0], prm[:, 6, :]), reads=[R], writes=[R])
        f.op("dve", lambda e: e.tensor_copy(cs2[:, :, 1], prm[:, 7, :]), reads=[R], writes=[R])
        f.op("dve", lambda e: e.tensor_scalar(out=ncs2[:, :, 0], in0=prm[:, 7, :], scalar1=-1.0, scalar2=None, op0=ALU.mult), reads=[R], writes=[R])
        f.op("dve", lambda e: e.tensor_copy(ncs2[:, :, 1], prm[:, 6, :]), reads=[R], writes=[R])
        S.close()
        cosJ = P.sb("cosJ", [128, 8, 128]); sinJ = P.sb("sinJ", [128, 8, 128]); rtab = P.sb("rtab", [128, 8, 128])
        lB = P.sb("lB", [128, 8, 2, 128], BF16); lC = P.sb("lC", [128, 8, 2, 128], BF16)
        RT = Res("s5tab")
        S = Scope(nc)
        yacc = S.sb("yacc", [128, NTOK]); R_y = Res("yacc")
        NB = 2
        psb = [S.ps("psb%d" % k, [128, 2, 512]) for k in range(NB)]; R_psb = RL(NB)
        psy = [S.ps("psy%d" % k, [128, 512]) for k in range(NB)]; R_psy = RL(NB)
        pstr = [S.ps("pstr%d" % k, [128, 4, 128]) for k in range(2)]; R_pstr = RL(2)
        m = [S.sb("m%d" % k, [128, 2, 512]) for k in range(NB)]; R_m = RL(NB)
        ta = [S.sb("ta%d" % k, [128, 2, 512]) for k in range(NB)]; R_ta = RL(NB)
        g = [S.sb("g%d" % k, [128, 2, 512]) for k in range(NB)]; R_g = RL(NB)
        hb = [S.sb("hb%d" % k, [128, 2, 512], BF16) for k in range(NB)]; R_hb = RL(NB)
        ini = S.sb("ini", [128, 4]); R_ini = Res()
        gq1 = S.sb("gq1", [128, 512]); gq2 = S.sb("gq2", [128, 512]); R_gq1 = Res(); R_gq2 = Res()
        wgl = S.sb("wgl", [128, 4, 512], BF16); R_wgl = Res()
        f.dma("pool", wgl[:], w_glu.rearrange("(kc p) n -> p kc n", p=128), writes=[R_wgl])
        blocks = [(0, 256)] + [(256 + 512 * k, 512) for k in range(8)]
        it = 0
        for ct in range(4):
            T = Scope(nc)
            ang = T.sb("ang", [128, 8, 128])
            WB = T.sb("WB", [128, 2, 8, 128]); SC = T.sb("SC", [128, 2, 8, 128]); WB2 = T.sb("WB2", [128, 2, 8, 128])
            fre8 = T.sb("fre8", [128, 8]); fim8 = T.sb("fim8", [128, 8])
            for d in range(2):
                gsl = slice(d * 16 + ct * 4, d * 16 + ct * 4 + 4); lsl = slice(d * 4, d * 4 + 4)
                f.op("dve", lambda e: e.tensor_tensor(ang[:, lsl, :], jt[:].unsqueeze(1).broadcast_to([128, 4, 128]), th[:, gsl].unsqueeze(2).broadcast_to([128, 4, 128]), ALU.mult), reads=[R, RT], writes=[RT])
                f.op("dve", lambda e: e.tensor_copy(rtab[:, lsl, :], rr[:, gsl].unsqueeze(2).broadcast_to([128, 4, 128])), reads=[R, RT], writes=[RT])
                f.op("dve", lambda e: e.tensor_copy(fre8[:, lsl], prm[:, 8, gsl]), reads=[R, RT], writes=[RT])
                f.op("dve", lambda e: e.tensor_copy(fim8[:, lsl], prm[:, 9, gsl]), reads=[R, RT], writes=[RT])
            sincos(T, ang[:].rearrange("p a b -> p (a b)"), 1024, sinJ[:].rearrange("p a b -> p (a b)"), cosJ[:].rearrange("p a b -> p (a b)"), RT, "c%d" % ct)
            f.op("pool", lambda e: e.memset(WB[:], 0.0), reads=[RT], writes=[RT])
            f.op("pool", lambda e: e.memset(SC[:], 0.0), reads=[RT], writes=[RT])
            qn = 0
            for d in range(2):
                for gi in range(8):
                    g_ = ct * 8 + gi
                    l = d * 4 + gi // 2
                    gl = gi % 2
                    for ri, (bsrc, csrc) in enumerate(((b_re, c_re), (b_im, c_im))):
                        q1 = ("sp", "act")[qn % 2]; qn += 1
                        f.dma(q1, WB[64 * gl:64 * gl + 64, ri, l, 16 * gi:16 * gi + 16], bsrc[d, g_], writes=[RT])
                        f.dma(q1, SC[16 * gi:16 * gi + 16, ri, l, 64 * gl:64 * gl + 64], csrc[d, g_], writes=[RT])
            fre = fre8[:].unsqueeze(2).broadcast_to([128, 8, 128]); fim = fim8[:].unsqueeze(2).broadcast_to([128, 8, 128])
            f.op("dve", lambda e: e.tensor_tensor(WB2[:, 0], WB[:, 0], fre, ALU.mult), reads=[RT], writes=[RT])
            f.op("pool", lambda e: e.tensor_tensor(WB2[:, 1], WB[:, 1], fim, ALU.mult), reads=[RT], writes=[RT])
            f.op("dve", lambda e: e.tensor_tensor(WB2[:, 0], WB2[:, 0], WB2[:, 1], ALU.subtract), reads=[RT], writes=[RT])
            f.op("pool", lambda e: e.tensor_tensor(WB2[:, 1], WB[:, 1], fre, ALU.mult), reads=[RT], writes=[RT])
            f.op("dve", lambda e: e.tensor_tensor(WB[:, 0], WB[:, 0], fim, ALU.mult), reads=[RT], writes=[RT])
            f.op("dve", lambda e: e.tensor_tensor(WB2[:, 1], WB2[:, 1], WB[:, 0], ALU.add), reads=[RT], writes=[RT])
            n_ = 0
            for srct, dst, neg in ((WB2, lB, False), (SC, lC, True)):
                for ri in range(2):
                    for d4 in range(2):
                        pb = n_ % 2; n_ += 1
                        for k in range(4):
                            l = d4 * 4 + k
                            f.op("pe", lambda e, k=k, l=l: e.transpose(pstr[pb][:, k, :], srct[:, ri, l, :], ident[:]), reads=[RT, R_ident], writes=[R_pstr[pb]], acc=(k > 0))
                        scl = -1.0 if (neg and ri == 1) else 1.0
                        f.op("act", lambda e: e.activation(out=dst[:, d4 * 4:d4 * 4 + 4, ri, :], in_=pstr[pb][:], func=AF.Identity, scale=scl), reads=[R_pstr[pb], RT], writes=[RT])
            T.close()
            f.op("act", lambda e: e.activation(out=yacc[:], in_=uT[:, ct, :], func=AF.Copy, scale=dsk[:, ct:ct + 1]), reads=R_uT + [R], writes=[R_y])
            items = []
            for pi in range(4):
                for d in range(2):
                    for bidx, (s0, n) in enumerate(blocks):
                        items.append((pi, d, bidx, s0, n))
            NI = len(items)

            def v3(ap):
                return ap.rearrange("p (c j) -> p c j", j=128)

            def geom(k):
                pi, d, bidx, s0, n = items[k]
                bi = k % NB
                dq = d * 16 + ct * 4 + pi
                l = d * 4 + pi
                nch = n // 128
                if d == 0:
                    c0 = s0
                    ucols = uT[:, ct, c0:c0 + n]
                    ycols = yacc[:, c0:c0 + n]
                else:
                    c0 = (256 - s0 - n) if s0 < 256 else (4608 - s0 - n)
                    ucols = rev_ap(uT[:, ct, c0:c0 + n], n)
                    ycols = rev_ap(yacc[:, c0:c0 + n], n)
                tl = [R_uT[kk] for kk in range(c0 // 128, (c0 + n) // 128)]
                cb = cosJ[:, l, :].unsqueeze(1).broadcast_to([128, nch, 128])
                sb_ = sinJ[:, l, :].unsqueeze(1).broadcast_to([128, nch, 128])
                return pi, d, bidx, n, bi, dq, l, nch, ucols, ycols, tl, cb, sb_

            def stA(k):
                pi, d, bidx, n, bi, dq, l, nch, ucols, ycols, tl, cb, sb_ = geom(k)
                for ri in range(2):
                    f.op("pe", lambda e, ri=ri: e.matmul(psb[bi][:, ri, 0:n], lB[:, l, ri, :], ucols, start=True, stop=True),
                         reads=[RT] + tl, writes=[R_psb[bi]], acc=(ri > 0))
                bre, bim = v3(psb[bi][:, 0, 0:n]), v3(psb[bi][:, 1, 0:n])
                mre, mim = v3(m[bi][:, 0, 0:n]), v3(m[bi][:, 1, 0:n])
                t_a, t_b = v3(ta[bi][:, 0, 0:n]), v3(ta[bi][:, 1, 0:n])
                f.op("dve", lambda e: e.tensor_tensor(mre, bre, cb, ALU.mult), reads=[R_psb[bi], RT], writes=[R_m[bi]])
                f.op("dve", lambda e: e.tensor_tensor(t_a, bim, sb_, ALU.mult), reads=[R_psb[bi], RT], writes=[R_ta[bi]])
                f.op("dve", lambda e: e.tensor_tensor(mre, mre, t_a, ALU.add), reads=[R_m[bi], R_ta[bi]], writes=[R_m[bi]])
                f.op("dve", lambda e: e.tensor_tensor(mim, bim, cb, ALU.mult), reads=[R_psb[bi], RT], writes=[R_m[bi]])
                f.op("dve", lambda e: e.tensor_tensor(t_b, bre, sb_, ALU.mult), reads=[R_psb[bi], RT], writes=[R_ta[bi]])
                f.op("dve", lambda e: e.tensor_tensor(mim, mim, t_b, ALU.subtract), reads=[R_m[bi], R_ta[bi]], writes=[R_m[bi]])

            def stB(k):
                pi, d, bidx, n, bi, dq, l, nch, ucols, ycols, tl, cb, sb_ = geom(k)
                prev = None
                if bidx > 0:
                    pbi = (k - 1) % NB
                    pn = items[k - 1][4]
                    prev = (g[pbi], pbi, pn // 128 - 1)
                for c in range(nch):
                    cs = slice(c * 128, (c + 1) * 128)
                    if prev is None:
                        i_re = i_im = 0.0
                        rd_extra = []
                    else:
                        pg, pbi, pc_ = prev
                        gre_l = pg[:, 0, pc_ * 128 + 127:pc_ * 128 + 128]
                        gim_l = pg[:, 1, pc_ * 128 + 127:pc_ * 128 + 128]
                        c128 = prm[:, 6, dq:dq + 1]; s128 = prm[:, 7, dq:dq + 1]
                        f.op("dve", lambda e: e.tensor_scalar(out=ini[:, 0:2], in0=cs2[:, dq, :], scalar1=gre_l, scalar2=None, op0=ALU.mult), reads=[R_g[pbi], R], writes=[R_ini])
                        f.op("dve", lambda e: e.scalar_tensor_tensor(out=ini[:, 0:2], in0=ncs2[:, dq, :], scalar=gim_l, in1=ini[:, 0:2], op0=ALU.mult, op1=ALU.add), reads=[R_g[pbi], R, R_ini], writes=[R_ini])
                        i_re, i_im = ini[:, 0:1], ini[:, 1:2]
                        rd_extra = [R_ini]
                    f.op("dve", lambda e: e.tensor_tensor_scan(g[bi][:, 0, cs], rtab[:, l, :], m[bi][:, 0, cs], i_re, ALU.mult, ALU.add),
                         reads=[R_m[bi], RT] + rd_extra, writes=[R_g[bi]])
                    f.op("dve", lambda e: e.tensor_tensor_scan(g[bi][:, 1, cs], rtab[:, l, :], m[bi][:, 1, cs], i_im, ALU.mult, ALU.add),
                         reads=[R_m[bi], RT] + rd_extra, writes=[R_g[bi]])
                    prev = (g[bi], bi, c)

            def stC(k):
                pi, d, bidx, n, bi, dq, l, nch, ucols, ycols, tl, cb, sb_ = geom(k)
                mre, mim = v3(m[bi][:, 0, 0:n]), v3(m[bi][:, 1, 0:n])
                t_a, t_b = v3(ta[bi][:, 0, 0:n]), v3(ta[bi][:, 1, 0:n])
                gre, gim = v3(g[bi][:, 0, 0:n]), v3(g[bi][:, 1, 0:n])
                hre, him = v3(hb[bi][:, 0, 0:n]), v3(hb[bi][:, 1, 0:n])
                f.op("dve", lambda e: e.tensor_tensor(t_a, gre, cb, ALU.mult), reads=[R_g[bi], RT], writes=[R_ta[bi]])
                f.op("dve", lambda e: e.tensor_tensor(mre, gim, sb_, ALU.mult), reads=[R_g[bi], RT], writes=[R_m[bi]])
                f.op("dve", lambda e: e.tensor_tensor(hre, t_a, mre, ALU.subtract), reads=[R_ta[bi], R_m[bi]], writes=[R_hb[bi]])
                f.op("dve", lambda e: e.tensor_tensor(t_b, gre, sb_, ALU.mult), reads=[R_g[bi], RT], writes=[R_ta[bi]])
                f.op("dve", lambda e: e.tensor_tensor(mim, gim, cb, ALU.mult), reads=[R_g[bi], RT], writes=[R_m[bi]])
                f.op("dve", lambda e: e.tensor_tensor(him, t_b, mim, ALU.add), reads=[R_ta[bi], R_m[bi]], writes=[R_hb[bi]])
                for ri in range(2):
                    f.op("pe", lambda e, ri=ri: e.matmul(psy[bi][:, 0:n], lC[:, l, ri, :], hb[bi][:, ri, 0:n], start=(ri == 0), stop=(ri == 1)),
                         reads=[RT, R_hb[bi]], writes=[R_psy[bi]], acc=(ri > 0))

            def stY(k):
                pi, d, bidx, n, bi, dq, l, nch, ucols, ycols, tl, cb, sb_ = geom(k)
                f.op("dve", lambda e: e.tensor_tensor(ycols, psy[bi][:, 0:n], ycols, ALU.add), reads=[R_psy[bi], R_y], writes=[R_y])

            stA(0)
            for k in range(NI):
                if k + 1 < NI:
                    stA(k + 1)
                stB(k)
                stC(k)
                if k >= 1:
                    stY(k - 1)
            stY(NI - 1)
            for (s0, n) in blocks:
                yb = yacc[:, s0:s0 + n]
                f.op("dve", lambda e: e.tensor_tensor(gq1[:, 0:n], yb, yb, ALU.mult), reads=[R_y], writes=[R_gq1])
                f.op("dve", lambda e: e.tensor_scalar(out=gq1[:, 0:n], in0=gq1[:, 0:n], scalar1=0.044715, scalar2=1.0, op0=ALU.mult, op1=ALU.add), reads=[R_gq1], writes=[R_gq1])
                f.op("dve", lambda e: e.tensor_tensor(gq1[:, 0:n], gq1[:, 0:n], yb, ALU.mult), reads=[R_gq1, R_y], writes=[R_gq1])
                f.op("act", lambda e: e.activation(out=gq2[:, 0:n], in_=gq1[:, 0:n], func=AF.Sigmoid, scale=1.5957691216057308), reads=[R_gq1], writes=[R_gq2])
                f.op("dve", lambda e: e.tensor_tensor(aT[:, ct, s0:s0 + n], yb, gq2[:, 0:n], ALU.mult), reads=[R_gq2, R_y], writes=R_aT[s0 // 128:(s0 + n) // 128])
        sg = [S.sb("sg%d" % k, [128, 512], BF16) for k in range(2)]; R_sg = RL(2)
        anew = S.sb("anew", [128, 4, 512], BF16); R_anew = Res()
        nn = 0
        for (s0, n) in blocks:
            tl = R_aT[s0 // 128:(s0 + n) // 128]
            for cto in range(4):
                bi = nn % 2; nn += 1
                for cti in range(4):
                    f.op("pe", lambda e, cti=cti: e.matmul(psy[bi][:, 0:n], wgl[:, cti, cto * 128:(cto + 1) * 128], aT[:, cti, s0:s0 + n], start=(cti == 0), stop=(cti == 3)),
                         reads=[R_wgl] + tl, writes=[R_psy[bi]], acc=(cti > 0))
                f.op("act", lambda e: e.activation(out=sg[bi][:, 0:n], in_=psy[bi][:, 0:n], func=AF.Sigmoid, bias=bgl[:, cto:cto + 1], scale=1.0), reads=[R_psy[bi], R], writes=[R_sg[bi]])
                f.op("dve", lambda e: e.tensor_tensor(anew[:, cto, 0:n], aT[:, cto, s0:s0 + n], sg[bi][:, 0:n], ALU.mult), reads=[R_sg[bi]] + tl, writes=[R_anew])
            f.op("dve", lambda e: e.tensor_copy(aT[:, :, s0:s0 + n], anew[:, :, 0:n]), reads=[R_anew], writes=tl)
        S.close()
        P.close()

    def win_phase(qT, R_qT, kT2, R_kT, vaug, R_v, oT, R_oT):
        S = Scope(nc)
        esink = S.sb("esink", [128, 8]); R_es = Res()
        f.dma("sp", esink[:], win_sink.partition_broadcast(128), writes=[R_es])
        f.op("act", lambda e: e.activation(out=esink[:], in_=esink[:], func=AF.Exp), reads=[R_es], writes=[R_es])
        NBS = 3
        ps_s = [S.ps("ps_s%d" % k, [128, 8, 128]) for k in range(NBS)]; R_pss = RL(NBS)
        ps_o = [S.ps("ps_o%d" % k, [128, 512]) for k in range(2)]; R_pso = RL(2)
        pT = [S.sb("pT%d" % k, [128, 5, 128], BF16) for k in range(NBS)]; R_pT = RL(NBS)
        dtmp = [S.sb("dtmp%d" % k, [128, 128]) for k in range(2)]; R_dt = RL(2)
        items = []
        for t in range(NT):
            kts = [(0, None), (1, None)]
            if t >= 2:
                for kt in (t - 1, t, t + 1):
                    if 2 <= kt < NT:
                        kts.append((kt, (0 if kt == t - 1 else (1 if kt == t + 1 else None))))
            for h in range(8):
                items.append((t, h, kts))

        def front(i_):
            t, h, kts = items[i_]
            cols = slice(t * 128, (t + 1) * 128)
            nk = len(kts)
            bs = i_ % NBS
            pr, base, kvh = h // 2, 64 * (h % 2), h // 4
            for i, (kt, mk) in enumerate(kts):
                f.op("pe", lambda e, i=i, kt=kt: e.matmul(ps_s[bs][:, i, :], kT2[base:base + 64, kvh, kt * 128:(kt + 1) * 128], qT[base:base + 64, pr, cols], start=True, stop=True),
                     reads=[R_kT[kt], R_qT[t]], writes=[R_pss[bs]], acc=(i > 0))
            f.op("act", lambda e: e.activation(out=pT[bs][:, 0:nk, :], in_=ps_s[bs][:, 0:nk, :], func=AF.Exp, scale=0.125), reads=[R_pss[bs]], writes=[R_pT[bs]])
            for i, (kt, mk) in enumerate(kts):
                if mk is not None:
                    f.op("dve", lambda e, i=i, mk=mk: e.tensor_tensor(pT[bs][:, i, :], pT[bs][:, i, :], maskb[:, mk, :], ALU.mult), reads=[R_pT[bs], R_mask], writes=[R_pT[bs]])

        def back(i_):
            t, h, kts = items[i_]
            cols = slice(t * 128, (t + 1) * 128)
            nk = len(kts)
            bs = i_ % NBS
            bi = i_ % 2
            pr, base, kvh = h // 2, 64 * (h % 2), h // 4
            voff = (64 if h % 2 == 0 else 0) + 128 * kvh
            for i, (kt, mk) in enumerate(kts):
                f.op("pe", lambda e, i=i, kt=kt: e.matmul(ps_o[bi][:, 0:128], vaug[:, kt, voff:voff + 128], pT[bs][:, i, :], start=(i == 0), stop=(i == nk - 1)),
                     reads=[R_v[kt], R_pT[bs]], writes=[R_pso[bi]], acc=(i > 0))
            nb, db = (0, 64) if h % 2 == 0 else (64, 0)
            f.op("dve", lambda e: e.tensor_scalar(out=dtmp[bi][nb:nb + 64, :], in0=ps_o[bi][db:db + 64, 0:128], scalar1=esink[db:db + 64, h:h + 1], scalar2=None, op0=ALU.add),
                 reads=[R_pso[bi], R_es], writes=[R_dt[bi]])
            f.op("dve", lambda e: e.reciprocal(dtmp[bi][nb:nb + 64, :], dtmp[bi][nb:nb + 64, :]), reads=[R_dt[bi]], writes=[R_dt[bi]])
            f.op("dve", lambda e: e.tensor_tensor(oT[nb:nb + 64, pr, cols], ps_o[bi][nb:nb + 64, 0:128], dtmp[bi][nb:nb + 64, :], ALU.mult), reads=[R_pso[bi], R_dt[bi]], writes=[R_oT[t]])
        front(0)
        for i_ in range(len(items)):
            if i_ + 1 < len(items):
                front(i_ + 1)
            back(i_)
        S.close()

    def out_phase(layer, w_out_d, mix_loader, tiles):
        S = Scope(nc)
        load_ln(layer * 2 + 0)
        wo = S.sb("wo", [128, 8, D], BF16); R_wo = Res()
        f.dma("pool", wo[:], w_out_d.rearrange("(kc p) n -> p kc n", p=128), writes=[R_wo])
        xt = [S.sb("xt%d" % k, [128, D]) for k in range(2)]; R_xt = RL(2)
        ot = [S.sb("ot%d" % k, [128, D]) for k in range(2)]; R_ot = RL(2)
        tmp = S.sb("tmp", [128, D]); R_tmp = Res()
        small = S.sb("small", [128, 16]); R_small = Res()
        ps_o2 = [S.ps("ps_o2%d" % k, [128, D]) for k in range(2)]; R_ps = RL(2)
        for n, t in enumerate(tiles):
            b = n % 2
            src, rs = src_tile(layer, t)
            f.dma("sp", xt[b][:], src, reads=rs, writes=[R_xt[b]])
            mixT, R_mix = mix_loader(t, b)
            for half in range(2):
                for kc in range(8):
                    f.op("pe", lambda e, kc=kc, half=half: e.matmul(ps_o2[b][:, half * 512:(half + 1) * 512], mixT[kc], wo[:, kc, half * 512:(half + 1) * 512], start=(kc == 0), stop=(kc == 7)),
                         reads=[R_wo] + R_mix, writes=[R_ps[b]], acc=(half + kc > 0))
            resid_ln(S, xt[b], R_xt[b], ps_o2[b], R_ps[b], (1 if t < 2 else 0), layer * 2 + 0, ot[b], R_ot[b], tmp, R_tmp, small, R_small)
            f.dma("act", XR[t * 128:(t + 1) * 128, :], ot[b][:], reads=[R_ot[b]], writes=[R_XR[t]])
        S.close()

    def ffn_phase(layer, tiles_all, final):
        P = Scope(nc)
        rw = P.sb("rw", [128, 8, 32]); R_rw = Res()
        f.dma("sp", rw[:], router_w.rearrange("(kc p) n -> p kc n", p=128), writes=[R_rw])
        rbias = P.sb("rbias", [128, 32]); f.dma("sp", rbias[:], router_b.partition_broadcast(128), writes=[R_rw])
        GT = 9
        load_ln(layer * 2 + 1)
        groups = [tiles_all[i:i + GT] for i in range(0, len(tiles_all), GT)]
        for grp in groups:
            S = Scope(nc)
            ng = len(grp)
            hT = S.sb("hTg", [128, 8, GT * 128], BF16); R_hT = RL(ng, "hTg")
            comb = S.sb("comb", [128, GT, 32]); R_comb = RL(ng, "comb")
            yacc = S.sb("yaccg", [128, GT, D]); R_y = RL(ng, "yg")
            A = Scope(nc)
            xt = [A.sb("xt%d" % k, [128, D]) for k in range(2)]; R_xt = RL(2)
            h32 = A.sb("h32", [128, D]); R_h32 = Res()
            h32T = A.sb("h32T", [128, 8, 128]); R_h32T = Res()
            ps_tp = A.ps("ps_tp", [128, 8, 128]); R_pstp = Res(x=True)
            ps_r = A.ps("ps_r", [128, 512]); R_psr = Res()
            sc = A.sb("sc", [128, 32]); sel = A.sb("sel", [128, 32]); R_sc = Res()
            pa = A.sb("pa", [128, 8, 6]); pm = A.sb("pm", [128, 8, 6]); gs = A.sb("gs", [128, 8]); thr = A.sb("thr", [128, 8])
            gm = A.sb("gm", [128, 2]); mg = A.sb("mg", [128, 8]); sm = A.sb("sm", [128, 8, 4])
            for j, t in enumerate(grp):
                b = j % 2
                f.dma("sp", xt[b][:], XR[t * 128:(t + 1) * 128, :], reads=[R_XR[t]], writes=[R_xt[b]])
                which = 1 if t < 2 else 0
                mod_transpose(xt[b], R_xt[b], which, h32, R_h32, ps_tp, R_pstp, hT[:, :, j * 128:(j + 1) * 128], R_hT[j], h32T, R_h32T)
                for kc in range(8):
                    f.op("pe", lambda e, kc=kc: e.matmul(ps_r[:, 0:32], h32T[:, kc, :], rw[:, kc, :], start=(kc == 0), stop=(kc == 7)), reads=[R_h32T, R_rw], writes=[R_psr], acc=(kc > 0))
                R1 = R_sc
                f.op("act", lambda e: e.activation(out=sc[:], in_=ps_r[:, 0:32], func=AF.Sigmoid), reads=[R_psr], writes=[R1])
                f.op("dve", lambda e: e.tensor_tensor(sel[:], sc[:], rbias[:], ALU.add), reads=[R1, R_rw], writes=[R1])
                s3 = sel[:].rearrange("p (g e) -> p g e", e=4)
                pairs = [(0, 1), (0, 2), (0, 3), (1, 2), (1, 3), (2, 3)]
                for k, (a_, b_) in enumerate(pairs):
                    f.op("dve", lambda e, k=k, a_=a_, b_=b_: e.tensor_tensor(pa[:, :, k], s3[:, :, a_], s3[:, :, b_], ALU.add), reads=[R1], writes=[R1])
                    f.op("dve", lambda e, k=k, a_=a_, b_=b_: e.tensor_tensor(pm[:, :, k], s3[:, :, a_], s3[:, :, b_], ALU.min), reads=[R1], writes=[R1])
                f.op("dve", lambda e: e.tensor_reduce(out=gs[:], in_=pa[:], axis=AX.X, op=ALU.max), reads=[R1], writes=[R1])
                f.op("dve", lambda e: e.tensor_reduce(out=thr[:], in_=pm[:], axis=AX.X, op=ALU.max), reads=[R1], writes=[R1])
                f.op("dve", lambda e: e.tensor_reduce(out=gm[:, 0:1], in_=gs[:], axis=AX.X, op=ALU.max), reads=[R1], writes=[R1])
                f.op("dve", lambda e: e.tensor_scalar(out=mg[:], in0=gs[:], scalar1=gm[:, 0:1], scalar2=None, op0=ALU.is_ge), reads=[R1], writes=[R1])
                f.op("dve", lambda e: e.tensor_tensor(sm[:], s3, thr[:].unsqueeze(2).broadcast_to([128, 8, 4]), ALU.is_ge), reads=[R1], writes=[R1])
                f.op("dve", lambda e: e.tensor_tensor(sm[:], sm[:], mg[:].unsqueeze(2).broadcast_to([128, 8, 4]), ALU.mult), reads=[R1], writes=[R1])
                cj = comb[:, j, :]
                f.op("dve", lambda e: e.tensor_tensor(cj, sm[:].rearrange("p g e -> p (g e)"), sc[:], ALU.mult), reads=[R1], writes=[R_comb[j]])
                f.op("dve", lambda e: e.tensor_reduce(out=gm[:, 1:2], in_=cj, axis=AX.X, op=ALU.add), reads=[R_comb[j], R1], writes=[R1])
                f.op("dve", lambda e: e.reciprocal(gm[:, 1:2], gm[:, 1:2]), reads=[R1], writes=[R1])
                f.op("dve", lambda e: e.tensor_scalar(out=cj, in0=cj, scalar1=gm[:, 1:2], scalar2=None, op0=ALU.mult), reads=[R1, R_comb[j]], writes=[R_comb[j]])
            A.close()
            B = Scope(nc)
            wg = [B.sb("wg%d" % k, [128, 8, 512], BF16) for k in range(2)]
            wu = [B.sb("wu%d" % k, [128, 8, 512], BF16) for k in range(2)]
            wd = [B.sb("wd%d" % k, [128, 4, D], BF16) for k in range(2)]
            R_w = RL(2, "w")
            psg = [B.ps("psg%d" % k, [128, 512]) for k in range(2)]; R_psg = RL(2)
            psu = [B.ps("psu%d" % k, [128, 512]) for k in range(2)]; R_psu = RL(2)
            psd = [B.ps("psd%d" % k, [128, D]) for k in range(2)]; R_psd = RL(2)
            sg = [B.sb("sg%d" % k, [128, 512]) for k in range(2)]; R_sg = RL(2)
            hid = [B.sb("hid%d" % k, [128, 4, 512], BF16) for k in range(2)]; R_hid = RL(2)
            ntok = ng * 128
            blocks = [(c0, min(512, ntok - c0)) for c0 in range(0, ntok, 512)]
            nfc = 0; nblk = 0; nd = 0
            for ex in range(32):
                wb = ex % 2
                f.dma("pool", wg[wb][:], w_gate[layer, ex].rearrange("(kc p) n -> p kc n", p=128), writes=[R_w[wb]])
                f.dma("pool", wu[wb][:], w_up[layer, ex].rearrange("(kc p) n -> p kc n", p=128), writes=[R_w[wb]])
                f.dma("pool", wd[wb][:], w_down[layer, ex].rearrange("(kc p) n -> p kc n", p=128), writes=[R_w[wb]])
                for (c0, n) in blocks:
                    hb_ = nblk % 2; nblk += 1
                    tl = R_hT[c0 // 128:(c0 + n) // 128]
                    for fc in range(4):
                        pb = nfc % 2; nfc += 1
                        for kc in range(8):
                            f.op("pe", lambda e, kc=kc, fc=fc: e.matmul(psg[pb][:, 0:n], wg[wb][:, kc, fc * 128:(fc + 1) * 128], hT[:, kc, c0:c0 + n], start=(kc == 0), stop=(kc == 7)),
                                 reads=[R_w[wb]] + tl, writes=[R_psg[pb]], acc=(kc > 0))
                        for kc in range(8):
                            f.op("pe", lambda e, kc=kc, fc=fc: e.matmul(psu[pb][:, 0:n], wu[wb][:, kc, fc * 128:(fc + 1) * 128], hT[:, kc, c0:c0 + n], start=(kc == 0), stop=(kc == 7)),
                                 reads=[R_w[wb]] + tl, writes=[R_psu[pb]], acc=(kc > 0))
                        f.op("act", lambda e: e.activation(out=sg[pb][:, 0:n], in_=psg[pb][:, 0:n], func=AF.Silu), reads=[R_psg[pb]], writes=[R_sg[pb]])
                        f.op("dve", lambda e, fc=fc: e.tensor_tensor(hid[hb_][:, fc, 0:n], sg[pb][:, 0:n], psu[pb][:, 0:n], ALU.mult), reads=[R_sg[pb], R_psu[pb]], writes=[R_hid[hb_]])
                    for tt in range(n // 128):
                        j = c0 // 128 + tt
                        db = nd % 2; nd += 1
                        for half in range(2):
                            for fc in range(4):
                                f.op("pe", lambda e, fc=fc, half=half: e.matmul(psd[db][:, half * 512:(half + 1) * 512], hid[hb_][:, fc, tt * 128:(tt + 1) * 128], wd[wb][:, fc, half * 512:(half + 1) * 512], start=(fc == 0), stop=(fc == 3)),
                                     reads=[R_w[wb], R_hid[hb_]], writes=[R_psd[db]], acc=(half + fc > 0))
                        cw = comb[:, j, ex:ex + 1]
                        if ex == 0:
                            f.op("dve", lambda e: e.tensor_scalar(out=yacc[:, j, :], in0=psd[db][:], scalar1=cw, scalar2=None, op0=ALU.mult), reads=[R_psd[db], R_comb[j]], writes=[R_y[j]])
                        else:
                            f.op("dve", lambda e: e.scalar_tensor_tensor(out=yacc[:, j, :], in0=psd[db][:], scalar=cw, in1=yacc[:, j, :], op0=ALU.mult, op1=ALU.add), reads=[R_psd[db], R_comb[j], R_y[j]], writes=[R_y[j]])
            B.close()
            C = Scope(nc)
            xt = [C.sb("xt%d" % k, [128, D]) for k in range(2)]; R_xt = RL(2)
            ot = [C.sb("ot%d" % k, [128, D]) for k in range(2)]; R_ot = RL(2)
            tmp = C.sb("tmp", [128, D]); R_tmp = Res()
            small = C.sb("small", [128, 16]); R_small = Res()
            for j, t in enumerate(grp):
                b = j % 2
                f.dma("sp", xt[b][:], XR[t * 128:(t + 1) * 128, :], reads=[R_XR[t]], writes=[R_xt[b]])
                yj = yacc[:, j, :]

                class _V:
                    def __init__(self, ap): self.ap = ap
                    def __getitem__(self, k): return self.ap
                resid_ln(C, xt[b], R_xt[b], _V(yj), R_y[j], (1 if t < 2 else 0), layer * 2 + 1, ot[b], R_ot[b], tmp, R_tmp, small, R_small)
                if final:
                    f.dma("act", out_d[(t - 2) * 128:(t - 1) * 128, :], ot[b][:], reads=[R_ot[b]], writes=[R_out])
                else:
                    f.dma("act", XR[t * 128:(t + 1) * 128, :], ot[b][:], reads=[R_ot[b]], writes=[R_XR[t]])
            C.close()
            S.close()
        P.close()


    def ffn_sparse(layer, tiles_all, final):
        IOA = bass.IndirectOffsetOnAxis
        ng = len(tiles_all)
        M = ng * 32
        P = Scope(nc)
        rw = P.sb("rw", [128, 8, 32]); R_rw = Res()
        f.dma("sp", rw[:], router_w.rearrange("(kc p) n -> p kc n", p=128), writes=[R_rw])
        rbias = P.sb("rbias", [128, 32]); f.dma("sp", rbias[:], router_b.partition_broadcast(128), writes=[R_rw])
        jt = P.sb("jt2", [128, 128]); f.dma("sp", jt[:], k_jidx, writes=[R_rw])
        pc = P.sb("pc", [128, 4]); f.dma("sp", pc[:], k_pc, writes=[R_rw])
        load_ln(layer * 2 + 1)
        comb = P.sb("comb", [128, ng, 32]); R_comb = RL(ng, "comb")
        posA_i = P.sb("posA_i", [128, ng], I32); posB_i = P.sb("posB_i", [128, ng], I32)
        wA = P.sb("wA", [128, ng]); wB = P.sb("wB", [128, ng])
        NSO = NS - 32
        idxw = P.sb("idxw", [128, NSO, 4], I32)
        R_rt = Res("route")
        R_XsW = RL(ng, "xsw")
        R_Ys = RL(NS, "ys")
        HB = Scope(nc)
        hb_all = HB.sb("hb_all", [128, ng, D], BF16); R_hb = RL(ng, "hb")
        A = Scope(nc)
        xt = [A.sb("xt%d" % k, [128, D]) for k in range(2)]; R_xt = RL(2)
        h32 = [A.sb("h32%d" % k, [128, D]) for k in range(2)]; R_h32 = RL(2)
        h32T = A.sb("h32T", [128, 8, 128]); R_h32T = Res()
        ps_tp = [A.ps("ps_tp%d" % k, [128, 8, 128]) for k in range(2)]; R_pstp = RL(2)
        ps_r = A.ps("ps_r", [128, 512]); R_psr = Res()
        R_sc = Res()
        sc_all = A.sb("sc_all", [128, ng, 32]); sel_all = A.sb("sel_all", [128, ng, 32])
        pa_all = A.sb("pa_all", [128, ng, 8, 6]); pm_all = A.sb("pm_all", [128, ng, 8, 6])
        gs_all = A.sb("gs_all", [128, ng, 8]); thr_all = A.sb("thr_all", [128, ng, 8]); mg_all = A.sb("mg_all", [128, ng, 8])
        gm_all = A.sb("gm_all", [128, ng]); sm_all = A.sb("sm_all", [128, ng, 8, 4])
        for j, t in enumerate(tiles_all):
            b = j % 2
            f.dma("sp", xt[b][:], XR[t * 128:(t + 1) * 128, :], reads=[R_XR[t]], writes=[R_xt[b]])
            which = 1 if t < 2 else 0
            f.op("dve", lambda e: e.tensor_tensor(h32[b][:], xt[b][:], mod[:, which, 1, :], ALU.mult), reads=[R_xt[b], R_mod], writes=[R_h32[b]])
            f.op("dve", lambda e: e.tensor_tensor(h32[b][:], h32[b][:], mod[:, which, 0, :], ALU.add), reads=[R_h32[b], R_mod], writes=[R_h32[b]])
            for kc in range(8):
                f.op("pe", lambda e, kc=kc: e.transpose(ps_tp[b][:, kc, :], h32[b][:, kc * 128:(kc + 1) * 128], ident[:]),
                     reads=[R_h32[b], R_ident], writes=[R_pstp[b]], acc=(kc > 0))
            f.op("dve", lambda e: e.tensor_copy(h32T[:], ps_tp[b][:]), reads=[R_pstp[b]], writes=[R_h32T])
            f.op("act", lambda e: e.activation(out=hb_all[:, j, :].rearrange("p (c j q) -> p c j q", c=4, j=2),
                                               in_=h32[b][:].rearrange("p (c q j) -> p c j q", c=4, j=2), func=AF.Identity),
                 reads=[R_h32[b]], writes=[R_hb[j]])
            for kc in range(8):
                f.op("pe", lambda e, kc=kc: e.matmul(ps_r[:, 0:32], h32T[:, kc, :], rw[:, kc, :], start=(kc == 0), stop=(kc == 7)), reads=[R_h32T, R_rw], writes=[R_psr], acc=(kc > 0))
            f.op("act", lambda e: e.activation(out=sc_all[:, j, :], in_=ps_r[:, 0:32], func=AF.Sigmoid), reads=[R_psr], writes=[R_sc])
        R1 = R_sc
        s4 = sel_all[:].rearrange("p t (g e) -> p t g e", e=4)
        f.op("dve", lambda e: e.tensor_tensor(sel_all[:], sc_all[:], rbias[:].unsqueeze(1).broadcast_to([128, ng, 32]), ALU.add), reads=[R1, R_rw], writes=[R1])
        pairs = [(0, 1), (0, 2), (0, 3), (1, 2), (1, 3), (2, 3)]
        for k, (a_, b_) in enumerate(pairs):
            f.op("dve", lambda e, k=k, a_=a_, b_=b_: e.tensor_tensor(pa_all[:, :, :, k], s4[:, :, :, a_], s4[:, :, :, b_], ALU.add), reads=[R1], writes=[R1])
            f.op("dve", lambda e, k=k, a_=a_, b_=b_: e.tensor_tensor(pm_all[:, :, :, k], s4[:, :, :, a_], s4[:, :, :, b_], ALU.min), reads=[R1], writes=[R1])
        f.op("dve", lambda e: e.tensor_reduce(out=gs_all[:], in_=pa_all[:], axis=AX.X, op=ALU.max), reads=[R1], writes=[R1])
        f.op("dve", lambda e: e.tensor_reduce(out=thr_all[:], in_=pm_all[:], axis=AX.X, op=ALU.max), reads=[R1], writes=[R1])
        f.op("dve", lambda e: e.tensor_reduce(out=gm_all[:], in_=gs_all[:], axis=AX.X, op=ALU.max), reads=[R1], writes=[R1])
        f.op("dve", lambda e: e.tensor_tensor(mg_all[:], gs_all[:], gm_all[:].unsqueeze(2).broadcast_to([128, ng, 8]), ALU.is_ge), reads=[R1], writes=[R1])
        f.op("dve", lambda e: e.tensor_tensor(sm_all[:], s4, thr_all[:].unsqueeze(3).broadcast_to([128, ng, 8, 4]), ALU.is_ge), reads=[R1], writes=[R1])
        f.op("dve", lambda e: e.tensor_tensor(sm_all[:], sm_all[:], mg_all[:].unsqueeze(3).broadcast_to([128, ng, 8, 4]), ALU.mult), reads=[R1], writes=[R1])
        f.op("dve", lambda e: e.tensor_tensor(comb[:], sm_all[:].rearrange("p t g e -> p t (g e)"), sc_all[:], ALU.mult), reads=[R1], writes=R_comb)
        f.op("dve", lambda e: e.tensor_reduce(out=gm_all[:], in_=comb[:], axis=AX.X, op=ALU.add), reads=R_comb + [R1], writes=[R1])
        f.op("dve", lambda e: e.reciprocal(gm_all[:], gm_all[:]), reads=[R1], writes=[R1])
        f.op("dve", lambda e: e.tensor_tensor(comb[:], comb[:], gm_all[:].unsqueeze(2).broadcast_to([128, ng, 32]), ALU.mult), reads=[R1] + R_comb, writes=R_comb)
        A.close()
        Bq = Scope(nc)
        m_ = Bq.sb("m_", [128, ng, 32]); mb16 = Bq.sb("mb16", [128, ng, 32], BF16)
        rank = Bq.sb("rank", [128, ng, 32]); tot = Bq.sb("tot", [128, ng, 32]); base = Bq.sb("base", [128, ng, 32])
        me = Bq.sb("me", [128, ng, 32]); Bm = Bq.sb("Bm", [128, ng, 32]); Am = Bq.sb("Am", [128, ng, 32]); tmpq = Bq.sb("tmpq", [128, ng, 32])
        ones16 = Bq.sb("ones16", [128, 128], BF16)
        cnt = Bq.sb("cnt", [128, 32]); cmp17 = Bq.sb("cmp17", [128, 32, 18]); thr18 = Bq.sb("thr18", [128, 18])
        tlf = Bq.sb("tlf", [128, 32]); sinc = Bq.sb("sinc", [128, 32]); so512 = Bq.sb("so512", [128, 32]); c1e = Bq.sb("c1e", [128, 32])
        mx = Bq.sb("mx", [128, ng]); pAf = Bq.sb("pAf", [128, ng]); pBf = Bq.sb("pBf", [128, ng])
        cmpj = Bq.sb("cmpj", [128, NSO, 32]); eidf = Bq.sb("eidf", [128, NSO]); idxf = Bq.sb("idxf", [128, NSO, 4])
        ps_rk = Bq.ps("ps_rk", [128, 3, 512]); ps_tt = Bq.ps("ps_tt", [128, 3, 512])
        RB = [R_rt]

        def fl(ap3):
            return ap3.rearrange("p a b -> p (a b)")
        f.op("dve", lambda e: e.tensor_scalar(out=fl(m_[:]), in0=fl(comb[:]), scalar1=0.0, scalar2=None, op0=ALU.is_gt), reads=R_comb, writes=RB)
        f.op("dve", lambda e: e.tensor_copy(fl(mb16[:]), fl(m_[:])), reads=RB, writes=RB)
        f.op("pool", lambda e: e.memset(ones16[:], 1.0), reads=RB, writes=RB)
        chunks = [(n0, min(M, n0 + 512)) for n0 in range(0, M, 512)]
        for ch, (n0, n1) in enumerate(chunks):
            f.op("pe", lambda e, ch=ch, n0=n0, n1=n1: e.matmul(ps_rk[:, ch, 0:n1 - n0], maskb[:, 2, :], fl(mb16[:])[:, n0:n1], start=True, stop=True), reads=RB + [R_mask], writes=RB)
            f.op("pe", lambda e, ch=ch, n0=n0, n1=n1: e.matmul(ps_tt[:, ch, 0:n1 - n0], ones16[:], fl(mb16[:])[:, n0:n1], start=True, stop=True), reads=RB, writes=RB)
        for ch, (n0, n1) in enumerate(chunks):
            f.op("dve", lambda e, ch=ch, n0=n0, n1=n1: e.tensor_copy(fl(rank[:])[:, n0:n1], ps_rk[:, ch, 0:n1 - n0]), reads=RB, writes=RB)
            f.op("dve", lambda e, ch=ch, n0=n0, n1=n1: e.tensor_copy(fl(tot[:])[:, n0:n1], ps_tt[:, ch, 0:n1 - n0]), reads=RB, writes=RB)
        f.op("dve", lambda e: e.memset(base[:, 0, :], 0.0), reads=RB, writes=RB)
        for t_ in range(1, ng):
            f.op("dve", lambda e, t_=t_: e.tensor_tensor(base[:, t_, :], base[:, t_ - 1, :], tot[:, t_ - 1, :], ALU.add), reads=RB, writes=RB)
        f.op("dve", lambda e: e.tensor_tensor(cnt[:], base[:, ng - 1, :], tot[:, ng - 1, :], ALU.add), reads=RB, writes=RB)
        f.op("dve", lambda e: e.tensor_scalar(out=thr18[:], in0=jt[:, 0:18], scalar1=512.0, scalar2=None, op0=ALU.mult), reads=RB + [R_rw], writes=RB)
        f.op("dve", lambda e: e.tensor_tensor(cmp17[:], cnt[:].unsqueeze(2).broadcast_to([128, 32, 18]), thr18[:].unsqueeze(1).broadcast_to([128, 32, 18]), ALU.is_gt), reads=RB, writes=RB)
        f.op("dve", lambda e: e.tensor_reduce(out=tlf[:], in_=cmp17[:], axis=AX.X, op=ALU.add), reads=RB, writes=RB)
        f.op("dve", lambda e: e.tensor_scalar(out=tlf[:], in0=tlf[:], scalar1=-1.0, scalar2=0.0, op0=ALU.add, op1=ALU.max), reads=RB, writes=RB)
        f.op("dve", lambda e: e.tensor_copy(sinc[:], tlf[:]), reads=RB, writes=RB)
        for e_ in range(1, 32):
            f.op("dve", lambda e, e_=e_: e.tensor_tensor(sinc[:, e_:e_ + 1], sinc[:, e_ - 1:e_], tlf[:, e_:e_ + 1], ALU.add), reads=RB, writes=RB)
        f.op("dve", lambda e: e.tensor_tensor(so512[:], sinc[:], tlf[:], ALU.subtract), reads=RB, writes=RB)
        f.op("dve", lambda e: e.tensor_scalar(out=so512[:], in0=so512[:], scalar1=512.0, scalar2=15872.0, op0=ALU.mult, op1=ALU.add), reads=RB, writes=RB)
        f.op("dve", lambda e: e.tensor_scalar(out=c1e[:], in0=jt[:, 0:32], scalar1=512.0, scalar2=None, op0=ALU.mult), reads=RB + [R_rw], writes=RB)
        f.op("dve", lambda e: e.tensor_tensor(so512[:], so512[:], c1e[:], ALU.subtract), reads=RB, writes=RB)
        f.op("dve", lambda e: e.tensor_tensor(fl(rank[:]), fl(rank[:]), fl(base[:]), ALU.add), reads=RB, writes=RB)
        f.op("dve", lambda e: e.tensor_scalar(out=fl(tmpq[:]), in0=fl(rank[:]), scalar1=512.0, scalar2=None, op0=ALU.is_ge), reads=RB, writes=RB)
        f.op("dve", lambda e: e.tensor_tensor(tmpq[:], tmpq[:], so512[:].unsqueeze(1).broadcast_to([128, ng, 32]), ALU.mult), reads=RB, writes=RB)
        f.op("dve", lambda e: e.tensor_tensor(rank[:], rank[:], c1e[:].unsqueeze(1).broadcast_to([128, ng, 32]), ALU.add), reads=RB, writes=RB)
        f.op("dve", lambda e: e.tensor_tensor(fl(rank[:]), fl(rank[:]), fl(tmpq[:]), ALU.add), reads=RB, writes=RB)
        f.op("dve", lambda e: e.tensor_tensor(me[:], m_[:], jt[:, 1:33].unsqueeze(1).broadcast_to([128, ng, 32]), ALU.mult), reads=RB, writes=RB)
        f.op("dve", lambda e: e.tensor_reduce(out=mx[:], in_=me[:], axis=AX.X, op=ALU.max), reads=RB, writes=RB)
        f.op("dve", lambda e: e.tensor_tensor(Bm[:], me[:], mx[:].unsqueeze(2).broadcast_to([128, ng, 32]), ALU.is_equal), reads=RB, writes=RB)
        f.op("dve", lambda e: e.tensor_tensor(fl(Am[:]), fl(m_[:]), fl(Bm[:]), ALU.subtract), reads=RB, writes=RB)
        for (msk, pf, wf) in ((Am, pAf, wA), (Bm, pBf, wB)):
            f.op("dve", lambda e, msk=msk: e.tensor_tensor(fl(tmpq[:]), fl(msk[:]), fl(rank[:]), ALU.mult), reads=RB, writes=RB)
            f.op("dve", lambda e, pf=pf: e.tensor_reduce(out=pf[:], in_=tmpq[:], axis=AX.X, op=ALU.add), reads=RB, writes=RB)
            f.op("dve", lambda e, msk=msk: e.tensor_tensor(fl(tmpq[:]), fl(msk[:]), fl(comb[:]), ALU.mult), reads=RB + R_comb, writes=RB)
            f.op("dve", lambda e, wf=wf: e.tensor_reduce(out=wf[:], in_=tmpq[:], axis=AX.X, op=ALU.add), reads=RB, writes=RB)
        f.op("dve", lambda e: e.tensor_copy(posA_i[:], pAf[:]), reads=RB, writes=RB)
        f.op("dve", lambda e: e.tensor_copy(posB_i[:], pBf[:]), reads=RB, writes=RB)
        f.op("dve", lambda e: e.tensor_tensor(cmpj[:], sinc[:].unsqueeze(1).broadcast_to([128, NSO, 32]), jt[:, 0:NSO].unsqueeze(2).broadcast_to([128, NSO, 32]), ALU.is_le), reads=RB, writes=RB)
        f.op("dve", lambda e: e.tensor_reduce(out=eidf[:], in_=cmpj[:], axis=AX.X, op=ALU.add), reads=RB, writes=RB)
        f.op("dve", lambda e: e.tensor_scalar(out=eidf[:], in0=eidf[:], scalar1=32.0, scalar2=512.0, op0=ALU.min, op1=ALU.mult), reads=RB, writes=RB)
        f.op("dve", lambda e: e.tensor_scalar(out=eidf[:], in0=eidf[:], scalar1=float(layer * 16384), scalar2=None, op0=ALU.add), reads=RB, writes=RB)
        f.op("dve", lambda e: e.tensor_tensor(idxf[:], eidf[:].unsqueeze(2).broadcast_to([128, NSO, 4]), pc[:].unsqueeze(1).broadcast_to([128, NSO, 4]), ALU.add), reads=RB, writes=RB)
        f.op("dve", lambda e: e.tensor_copy(idxw[:], idxf[:]), reads=RB, writes=RB)
        for j in range(ng):
            for pi_ in (posA_i, posB_i):
                f._dma_common("pool", lambda e, j=j, pi_=pi_: e.indirect_dma_start(out=XS, out_offset=IOA(ap=pi_[:, j:j + 1], axis=0), in_=hb_all[:, j, :], in_offset=None),
                              [R_hb[j]] + RB + R_XsZ, [R_XsW[j]])
        Bq.close()
        HB.close()
        Sd = Scope(nc)
        wg = [Sd.sb("wg%d" % k, [128, 4, 2, 512], BF16) for k in range(2)]
        wu = [Sd.sb("wu%d" % k, [128, 4, 2, 512], BF16) for k in range(2)]
        wd = [Sd.sb("wd%d" % k, [128, 4, D], BF16) for k in range(2)]
        R_w = RL(2, "w"); R_wd = RL(2, "wd")
        xs = [[Sd.sb("xs%d_%d" % (a_, tt), [128, D], BF16) for tt in range(4)] for a_ in range(2)]
        R_xs = [RL(4, "xs%d" % a_) for a_ in range(2)]

        def xs_load(jn):
            for tt in range(4):
                r0 = (jn * 4 + tt) * 128
                f.dma("sp", xs[jn % 2][tt][:], XS[r0:r0 + 128, :], reads=R_XsW, writes=[R_xs[jn % 2][tt]])
        xT = [Sd.sb("xT%d" % k, [128, 8, 512], BF16) for k in range(2)]; R_xT = RL(2)
        ps_t = [Sd.ps("ps_t%d" % k, [128, 8, 128], BF16) for k in range(2)]; R_pst = RL(2)
        psg = [Sd.ps("psg%d" % k, [128, 512]) for k in range(2)]; R_psg = RL(2)
        psu = [Sd.ps("psu%d" % k, [128, 512]) for k in range(2)]; R_psu = RL(2)
        psd = [Sd.ps("psd%d" % k, [128, 512]) for k in range(2)]; R_psd = RL(2)
        sg = [Sd.sb("sg%d" % k, [128, 512]) for k in range(2)]; R_sg = RL(2)
        hid = [Sd.sb("hid%d" % k, [128, 4, 512], BF16) for k in range(2)]; R_hid = RL(2)
        ysb = [Sd.sb("ysb%d" % k, [128, D]) for k in range(2)]; R_ysb = [RL(2, "ysb%d" % k) for k in range(2)]
        nfc = 0; nx = 0; ny = 0
        bc_reg = nc.gpsimd.alloc_register("bc%d" % layer)
        nc.gpsimd.reg_mov(bc_reg, 16383 + layer * 16384)
        stg_g = Sd.sb("stg_g", [128, 4, 2, 512]); stg_u = Sd.sb("stg_u", [128, 4, 2, 512])
        R_sg_ = Res("stg_g"); R_su_ = Res("stg_u")

        def w_load(ex):
            f.dma("sp", stg_g[:], w_gate[layer, ex].rearrange("(c q j) n -> q c j n", c=4, j=2), writes=[R_sg_])
            f.dma("sp", stg_u[:], w_up[layer, ex].rearrange("(c q j) n -> q c j n", c=4, j=2), writes=[R_su_])
            f.dma("pool", wd[ex % 2][:], w_down[layer, ex].rearrange("(c p) n -> p c n", p=128), writes=[R_wd[ex % 2]])

        def w_cast(ex):
            wb_ = ex % 2
            f.op("act", lambda e: e.activation(out=wg[wb_][:], in_=stg_g[:], func=AF.Identity), reads=[R_sg_], writes=[R_w[wb_]])
            f.op("dve", lambda e: e.tensor_copy(wu[wb_][:], stg_u[:]), reads=[R_su_], writes=[R_w[wb_]])
        xs_load(0)
        w_load(0)
        w_cast(0)
        ns_l = 32 + (ng * 128 * 2) // 512
        for j in range(ns_l):
            wb = j % 2
            if j + 1 < ns_l:
                xs_load(j + 1)
            if j + 1 < 32:
                w_load(j + 1)
            if j >= 32:
                jo = j - 32
                for c in range(4):
                    for (wt_, rows_) in ((wg, wg_rows), (wu, wu_rows)):
                        f._dma_common("pool", lambda e, c=c, wt_=wt_, rows_=rows_: e.indirect_dma_start(out=wt_[wb][:, c, :, :].rearrange("p a n -> p (a n)"), out_offset=None, in_=rows_[layer],
                                                                                                   in_offset=IOA(ap=idxw[:, jo, c:c + 1], axis=0), bounds_check=bc_reg, oob_is_err=False),
                                      RB, [R_w[wb]])
                for c in range(4):
                    f._dma_common("pool", lambda e, c=c: e.indirect_dma_start(out=wd[wb][:, c, :], out_offset=None, in_=wd_rows[layer], in_offset=IOA(ap=idxw[:, jo, c:c + 1], axis=0), bounds_check=bc_reg, oob_is_err=False),
                                  RB, [R_wd[wb]])
            for tt in range(4):
                for kc in range(8):
                    f.op("pe", lambda e, kc=kc, tt=tt: e.transpose(ps_t[tt % 2][:, kc, :], xs[j % 2][tt][:, kc * 128:(kc + 1) * 128], identb[:]), reads=[R_xs[j % 2][tt], R_identb], writes=[R_pst[tt % 2]], acc=(kc > 0))
                f.op("dve", lambda e, tt=tt: e.tensor_copy(xT[wb][:, :, tt * 128:(tt + 1) * 128], ps_t[tt % 2][:]), reads=[R_pst[tt % 2]], writes=[R_xT[wb]])
            hb_ = j % 2
            for fc in range(4):
                pb = nfc % 2; nfc += 1
                for kc in range(8):
                    f.op("pe", lambda e, kc=kc, fc=fc: e.matmul(psg[pb][:], wg[wb][:, kc // 2, kc % 2, fc * 128:(fc + 1) * 128], xT[wb][:, kc, :], start=(kc == 0), stop=(kc == 7)),
                         reads=[R_w[wb], R_xT[wb]], writes=[R_psg[pb]], acc=(kc > 0))
                for kc in range(8):
                    f.op("pe", lambda e, kc=kc, fc=fc: e.matmul(psu[pb][:], wu[wb][:, kc // 2, kc % 2, fc * 128:(fc + 1) * 128], xT[wb][:, kc, :], start=(kc == 0), stop=(kc == 7)),
                         reads=[R_w[wb], R_xT[wb]], writes=[R_psu[pb]], acc=(kc > 0))
                f.op("act", lambda e: e.activation(out=sg[pb][:], in_=psg[pb][:], func=AF.Silu), reads=[R_psg[pb]], writes=[R_sg[pb]])
                f.op("dve", lambda e, fc=fc: e.tensor_tensor(hid[hb_][:, fc, :], sg[pb][:], psu[pb][:], ALU.mult), reads=[R_sg[pb], R_psu[pb]], writes=[R_hid[hb_]])
            for tt in range(4):
                yb_ = ny % 2; ny += 1
                for half in range(2):
                    for fc in range(4):
                        f.op("pe", lambda e, fc=fc, half=half, tt=tt: e.matmul(psd[half][:], hid[hb_][:, fc, tt * 128:(tt + 1) * 128], wd[wb][:, fc, half * 512:(half + 1) * 512], start=(fc == 0), stop=(fc == 3)),
                             reads=[R_wd[wb], R_hid[hb_]], writes=[R_psd[half]], acc=(fc > 0))
                    if half == 0:
                        f.op("dve", lambda e: e.tensor_copy(ysb[yb_][:, 0:512], psd[0][:]), reads=[R_psd[0]], writes=[R_ysb[yb_][0]])
                    else:
                        f.op("act", lambda e: e.activation(out=ysb[yb_][:, 512:1024], in_=psd[1][:], func=AF.Identity), reads=[R_psd[1]], writes=[R_ysb[yb_][1]])
                r0 = (j * 4 + tt) * 128
                f.dma("act", YS[r0:r0 + 128, :], ysb[yb_][:], reads=R_ysb[yb_], writes=[R_Ys[j]])
            if j + 1 < 32:
                w_cast(j + 1)
        Sd.close()
        nc.gpsimd.free_register(bc_reg)
        C = Scope(nc)
        xt = [C.sb("xt%d" % k, [128, D]) for k in range(2)]; R_xt = RL(2)
        ot = [C.sb("ot%d" % k, [128, D]) for k in range(2)]; R_ot = RL(2)
        ya = [C.sb("ya%d" % k, [128, D]) for k in range(2)]; R_ya = RL(2)
        yb2 = [C.sb("yb%d" % k, [128, D]) for k in range(2)]; R_yb = RL(2)
        tmp = C.sb("tmp", [128, D]); R_tmp = Res()
        small = C.sb("small", [128, 16]); R_small = Res()

        class _V:
            def __init__(self, ap): self.ap = ap
            def __getitem__(self, k): return self.ap
        for j, t in enumerate(tiles_all):
            b = j % 2
            f.dma("sp", xt[b][:], XR[t * 128:(t + 1) * 128, :], reads=[R_XR[t]], writes=[R_xt[b]])
            f._dma_common("pool", lambda e: e.indirect_dma_start(out=ya[b][:], out_offset=None, in_=YS, in_offset=IOA(ap=posA_i[:, j:j + 1], axis=0)), R_Ys + RB, [R_ya[b]])
            f._dma_common("pool", lambda e: e.indirect_dma_start(out=yb2[b][:], out_offset=None, in_=YS, in_offset=IOA(ap=posB_i[:, j:j + 1], axis=0)), R_Ys + RB, [R_yb[b]])
            f.op("dve", lambda e: e.tensor_scalar(out=ya[b][:], in0=ya[b][:], scalar1=wA[:, j:j + 1], scalar2=None, op0=ALU.mult), reads=[R_ya[b]] + RB, writes=[R_ya[b]])
            f.op("dve", lambda e: e.scalar_tensor_tensor(out=ya[b][:], in0=yb2[b][:], scalar=wB[:, j:j + 1], in1=ya[b][:], op0=ALU.mult, op1=ALU.add), reads=[R_yb[b], R_ya[b]] + RB, writes=[R_ya[b]])
            resid_ln(C, xt[b], R_xt[b], _V(ya[b][:]), R_ya[b], (1 if t < 2 else 0), layer * 2 + 1, ot[b], R_ot[b], tmp, R_tmp, small, R_small)
            if final:
                f.dma("act", out_d[(t - 2) * 128:(t - 1) * 128, :], ot[b][:], reads=[R_ot[b]], writes=[R_out])
            else:
                f.dma("act", XR[t * 128:(t + 1) * 128, :], ot[b][:], reads=[R_ot[b]], writes=[R_XR[t]])
        C.close()
        P.close()

    def layer1_mixer():
        L = Scope(nc)
        kT2 = L.sb("kT2b", [128, 4, NTOK], BF16); R_kT = RL(NT, "kT")
        vaug = L.sb("vaugb", [128, NT, 576], BF16); R_v = RL(NT, "v")
        f.op("pool", lambda e: e.memset(vaug[:], 1.0), writes=R_v)
        R_oT = RL(NT, "oT")
        R_QT = RL(NT, "QT")
        S = Scope(nc)
        win = S.sb("win1", [128, 8, 1536], BF16); R_win = Res()
        f.dma("pool", win[:], odd_w_in.rearrange("(kc p) n -> p kc n", p=128), writes=[R_win])
        gq = S.sb("gq", [128, 2, 64]); R_gq = Res()
        f.dma("sp", gq[:, 0, :], q_norm.partition_broadcast(128), writes=[R_gq])
        f.dma("sp", gq[:, 1, :], k_norm.partition_broadcast(128), writes=[R_gq])
        xt = [S.sb("xt%d" % k, [128, D]) for k in range(2)]; R_xt = RL(2)
        h32 = S.sb("h32", [128, D]); R_h32 = Res()
        hT = [S.sb("hT%d" % k, [128, 8, 128], BF16) for k in range(2)]; R_hT = RL(2)
        ps_tp = S.ps("ps_tp", [128, 8, 128]); R_pstp = Res(x=True)
        ps_q = S.ps("ps_q", [128, 1536]); R_psq = Res(x=True)
        ps_t = S.ps("ps_t", [128, 16, 128], BF16); R_pst = Res(x=True)
        qk = S.sb("qk", [128, 20, 64]); R_qk = Res()
        sq = S.sb("sq", [128, 20, 64]); ss = S.sb("ss", [128, 20]); R_ss = Res()
        ra = S.sb("ra", [128, 20, 32]); rb = S.sb("rb", [128, 20, 32]); R_ra = Res(); R_rb = Res()
        tqk = S.sb("tqk", [128, 20, 64], BF16); R_tqk = Res()
        kd = S.sb("kd", [128, 4, 2, 64], BF16); R_kd = Res()
        qts = [S.sb("qts%d" % k, [128, 8, 128], BF16) for k in range(2)]; R_qts = RL(2)
        for t in range(NT):
            b = t % 2
            f.dma("sp", xt[b][:], XR[t * 128:(t + 1) * 128, :], reads=[R_XR[t]], writes=[R_xt[b]])
            which = 1 if t < 2 else 0
            mod_transpose(xt[b], R_xt[b], which, h32, R_h32, ps_tp, R_pstp, hT[b][:], R_hT[b])
            cols = slice(t * 128, (t + 1) * 128)
            lat = t >= 2
            ranges = ((0, 512), (512, 1024), (1024, 1536)) if lat else ((1024, 1536),)
            first = True
            for (n0, n1) in ranges:
                for kc in range(8):
                    f.op("pe", lambda e, kc=kc, n0=n0, n1=n1: e.matmul(ps_q[:, n0:n1], hT[b][:, kc, :], win[:, kc, n0:n1], start=(kc == 0), stop=(kc == 7)),
                         reads=[R_win, R_hT[b]], writes=[R_psq], acc=(not first))
                    first = False
            h0 = 0 if lat else 16
            nh = 20 - h0
            pv = ps_q[:, h0 * 64:1280].rearrange("p (h d) -> p h d", d=64)
            qkv_ = qk[:, h0:20, :]
            f.op("act", lambda e: e.activation(out=sq[:, h0:20, :], in_=pv, func=AF.Square), reads=[R_psq], writes=[R_ss])
            f.op("dve", lambda e: e.tensor_reduce(out=ss[:, h0:20], in_=sq[:, h0:20, :], axis=AX.X, op=ALU.add), reads=[R_ss], writes=[R_ss])
            f.op("dve", lambda e: e.tensor_scalar(out=ss[:, h0:20], in0=ss[:, h0:20], scalar1=1.0 / 64.0, scalar2=RMS_EPS, op0=ALU.mult, op1=ALU.add), reads=[R_ss], writes=[R_ss])
            f.op("act", lambda e: e.activation(out=ss[:, h0:20], in_=ss[:, h0:20], func=AF.Sqrt), reads=[R_ss], writes=[R_ss])
            f.op("dve", lambda e: e.reciprocal(ss[:, h0:20], ss[:, h0:20]), reads=[R_ss], writes=[R_ss])
            f.op("dve", lambda e: e.tensor_tensor(qkv_, pv, ss[:, h0:20].unsqueeze(2).broadcast_to([128, nh, 64]), ALU.mult), reads=[R_psq, R_ss], writes=[R_qk])
            if lat:
                f.op("dve", lambda e: e.tensor_tensor(qk[:, 0:16, :], qk[:, 0:16, :], gq[:, 0, :].unsqueeze(1).broadcast_to([128, 16, 64]), ALU.mult), reads=[R_qk, R_gq], writes=[R_qk])
            f.op("dve", lambda e: e.tensor_tensor(qk[:, 16:20, :], qk[:, 16:20, :], gq[:, 1, :].unsqueeze(1).broadcast_to([128, 4, 64]), ALU.mult), reads=[R_qk, R_gq], writes=[R_qk])
            if lat:
                q4 = qk[:].rearrange("p h (two f) -> p h two f", two=2)
                o4 = tqk[:].rearrange("p h (two f) -> p h two f", two=2)
                cosb = rope[:, 0, t - 2, :].unsqueeze(1).broadcast_to([128, 20, 32])
                sinb = rope[:, 1, t - 2, :].unsqueeze(1).broadcast_to([128, 20, 32])
                f.op("dve", lambda e: e.tensor_tensor(ra[:], q4[:, :, 0, :], cosb, ALU.mult), reads=[R_qk, R_rope], writes=[R_ra])
                f.op("dve", lambda e: e.tensor_tensor(rb[:], q4[:, :, 1, :], sinb, ALU.mult), reads=[R_qk, R_rope], writes=[R_rb])
                f.op("dve", lambda e: e.tensor_tensor(o4[:, :, 0, :], ra[:], rb[:], ALU.subtract), reads=[R_ra, R_rb], writes=[R_tqk])
                f.op("dve", lambda e: e.tensor_tensor(ra[:], q4[:, :, 1, :], cosb, ALU.mult), reads=[R_qk, R_rope, R_tqk], writes=[R_ra])
                f.op("dve", lambda e: e.tensor_tensor(rb[:], q4[:, :, 0, :], sinb, ALU.mult), reads=[R_qk, R_rope, R_tqk], writes=[R_rb])
                f.op("dve", lambda e: e.tensor_tensor(o4[:, :, 1, :], ra[:], rb[:], ALU.add), reads=[R_ra, R_rb], writes=[R_tqk])
            else:
                f.op("dve", lambda e: e.tensor_copy(tqk[:, 16:20, :], qk[:, 16:20, :]), reads=[R_qk], writes=[R_tqk])
            for a in range(4):
                f.op("dve", lambda e, a=a: e.tensor_copy(vaug[:, t, 64 + 128 * a:128 + 128 * a], ps_q[:, 1280 + 64 * a:1344 + 64 * a]), reads=[R_psq], writes=[R_v[t]])
            f.op("dve", lambda e: e.tensor_copy(kd[:, :, 0, :], tqk[:, 16:20, :]), reads=[R_tqk], writes=[R_kd])
            f.op("dve", lambda e: e.tensor_copy(kd[:, :, 1, :], tqk[:, 16:20, :]), reads=[R_tqk], writes=[R_kd])
            firstt = True
            if lat:
                for pr in range(8):
                    f.op("pe", lambda e, pr=pr: e.transpose(ps_t[:, pr, :], tqk[:, 2 * pr:2 * pr + 2, :].rearrange("p a d -> p (a d)"), identb[:]),
                         reads=[R_tqk, R_identb], writes=[R_pst], acc=(not firstt))
                    firstt = False
            for a in range(4):
                f.op("pe", lambda e, a=a: e.transpose(ps_t[:, 8 + a, :], kd[:, a, :, :].rearrange("p a d -> p (a d)"), identb[:]),
                     reads=[R_kd, R_identb], writes=[R_pst], acc=(not firstt))
                firstt = False
            f.op("act", lambda e: e.activation(out=kT2[:, :, cols], in_=ps_t[:, 8:12, :], func=AF.Identity), reads=[R_pst], writes=[R_kT[t]])
            if lat:
                f.op("dve", lambda e: e.tensor_copy(qts[b][:], ps_t[:, 0:8, :]), reads=[R_pst], writes=[R_qts[b]])
                f.dma("act", QT[:, :, (t - 2) * 128:(t - 1) * 128].rearrange("a p n -> p a n"), qts[b][:], reads=[R_qts[b]], writes=[R_QT[t]])
        S.close()
        S = Scope(nc)
        qb = [S.sb("qb%d" % k, [128, 2, 512], BF16) for k in range(2)]; R_qb = RL(2)
        ps_s = [S.ps("ps_s%d" % k, [128, 1024]) for k in range(2)]; R_pss = RL(2)
        ps_o = [S.ps("ps_o%d" % k, [128, 512]) for k in range(4)]; R_pso = RL(4)
        pT = [S.sb("pT%d" % k, [128, 1024], BF16) for k in range(3)]; R_pT = RL(3)
        dtmp = S.sb("dtmp", [128, 512]); R_dt = Res()
        ost = [S.sb("ost%d" % k, [128, 2, 512], BF16) for k in range(2)]; R_ost = RL(2)
        it = 0
        nq = 0
        for kvh in range(4):
            for qblk in range(8):
                qbi = nq % 2; nq += 1
                tq = [R_QT[2 + qblk * 4 + k] for k in range(4)]
                f.dma("sp", qb[qbi][:], QT[2 * kvh:2 * kvh + 2, :, qblk * 512:(qblk + 1) * 512].rearrange("a p n -> p a n"), reads=tq, writes=[R_qb[qbi]])
                items = [(kt, p_) for kt in range(NT) for p_ in range(2)]
                it0 = it; it += len(items)

                def front(idx):
                    kt, p_ = items[idx]
                    si = (it0 + idx) % 2
                    pi_ = (it0 + idx) % 3
                    for half in range(2):
                        base = 64 * half
                        f.op("pe", lambda e, half=half, base=base: e.matmul(ps_s[si][:, half * 512:(half + 1) * 512], kT2[base:base + 64, kvh, kt * 128:(kt + 1) * 128], qb[qbi][base:base + 64, p_, :], start=True, stop=True),
                             reads=[R_kT[kt], R_qb[qbi]], writes=[R_pss[si]], acc=(half > 0))
                    f.op("act", lambda e: e.activation(out=pT[pi_][:], in_=ps_s[si][:], func=AF.Exp, scale=0.125), reads=[R_pss[si]], writes=[R_pT[pi_]])

                def back(idx):
                    kt, p_ = items[idx]
                    pi_ = (it0 + idx) % 3
                    for half in range(2):
                        hh = 2 * p_ + half
                        voff = (64 if half == 0 else 0) + 128 * kvh
                        f.op("pe", lambda e, half=half, hh=hh, voff=voff: e.matmul(ps_o[hh][:], vaug[:, kt, voff:voff + 128], pT[pi_][:, half * 512:(half + 1) * 512], start=(kt == 0), stop=(kt == NT - 1)),
                             reads=[R_v[kt], R_pT[pi_]], writes=[R_pso[hh]], acc=(kt > 0))
                LA = 1
                for i_ in range(len(items) + LA):
                    if i_ < len(items):
                        front(i_)
                    if i_ >= LA:
                        back(i_ - LA)
                for hh in range(4):
                    h = kvh * 4 + hh
                    nb, db = (0, 64) if h % 2 == 0 else (64, 0)
                    f.op("dve", lambda e: e.reciprocal(dtmp[nb:nb + 64, :], ps_o[hh][db:db + 64, :]), reads=[R_pso[hh]], writes=[R_dt])
                    f.op("dve", lambda e: e.tensor_tensor(ost[qbi][nb:nb + 64, hh // 2, :], ps_o[hh][nb:nb + 64, :], dtmp[nb:nb + 64, :], ALU.mult),
                         reads=[R_pso[hh], R_dt], writes=[R_ost[qbi]])
                f.dma("act", OT[2 * kvh:2 * kvh + 2, :, qblk * 512:(qblk + 1) * 512].rearrange("a p n -> p a n"), ost[qbi][:], reads=[R_ost[qbi]],
                      writes=[R_oT[2 + qblk * 4 + k] for k in range(4)])
        S.close()
        L.close()
        M = Scope(nc)
        mixt = [M.sb("mixt%d" % k, [128, 8, 128], BF16) for k in range(2)]; R_mixt = RL(2)

        def mix_loader(t, b):
            f.dma("sp", mixt[b][:], OT[:, :, (t - 2) * 128:(t - 1) * 128].rearrange("a p n -> p a n"), reads=[R_oT[t]], writes=[R_mixt[b]])
            return [mixt[b][:, k, :] for k in range(8)], [R_mixt[b]]
        out_phase(1, odd_w_out, mix_loader, range(2, NT))
        M.close()

    phase_mod(0, 0)
    if stop_after == "mod":
        f.dma("sp", dbg[0:128, :], mod[:, 0].rearrange("p a d -> p (a d)"), reads=[R_mod], writes=[R_dbg])
        f.dma("sp", dbg[128:256, :], mod[:, 1].rearrange("p a d -> p (a d)"), reads=[R_mod], writes=[R_dbg])
    else:
        layer0_mixer()
    if stop_after in ("in0", "s5", "mod", "h0", "qkv", "win"):
        pass
    else:
        if stop_after == "mix0":
            pass
        else:
            phase_mod(0, 1)
            (ffn_phase if 'dense' in DBG_SKIP else ffn_sparse)(0, list(range(NT)), final=False)
            if stop_after != "l0":
                phase_mod(1, 0)
                layer1_mixer()
                if stop_after != "mix1":
                    phase_mod(1, 1)
                    (ffn_phase if 'dense' in DBG_SKIP else ffn_sparse)(1, list(range(2, NT)), final=True)
    if stop_after is not None and stop_after not in ("in0", "s5", "mod", "h0", "qkv", "win"):
        S = Scope(nc)
        tt = S.sb("dumpt", [128, D]); R_t = Res()
        for t in range(NT):
            f.dma("sp", tt[:], XR[t * 128:(t + 1) * 128, :], reads=[R_XR[t]], writes=[R_t])
            f.dma("sp", dbg[t * 128:(t + 1) * 128, :], tt[:], reads=[R_t], writes=[R_dbg])
        S.close()
    f.finish()
    Scope.FWREF = None
    G.close()
    f.close()
    return nc


_CONST = None


def _consts():
    global _CONST
    if _CONST is None:
        ident = np.eye(128, dtype=np.float32)
        n_freq = 16
        inv_freq = (10000.0 ** (-np.arange(n_freq, dtype=np.float32) / n_freq)).astype(np.float32)
        pos = np.arange(4096)
        r = (pos // 64).astype(np.float32); cc = (pos % 64).astype(np.float32)
        ang = np.concatenate([r[:, None] * inv_freq, cc[:, None] * inv_freq], -1).astype(np.float32)
        cos = np.cos(ang).astype(np.float32).reshape(32, 128, 32).transpose(1, 0, 2)
        sin = np.sin(ang).astype(np.float32).reshape(32, 128, 32).transpose(1, 0, 2)
        rope = np.ascontiguousarray(np.stack([cos, sin], axis=1))
        k = np.arange(128)[:, None]; q = np.arange(128)[None, :]
        mask = np.stack([(q <= k), (k <= q), (k < q)], axis=1).astype(np.float32)
        pc = (np.arange(128, dtype=np.float32)[:, None] + 128.0 * np.arange(4, dtype=np.float32)[None, :]).astype(np.float32)
        jidx = np.broadcast_to(np.arange(128, dtype=np.float32)[None, :], (128, 128)).copy()
        _CONST = {"k_ident": ident, "k_rope": rope, "k_mask": np.ascontiguousarray(mask), "k_jidx": jidx, "k_pc": np.ascontiguousarray(pc)}
    return _CONST


def make_in_map(inputs, b):
    f32 = lambda a: np.ascontiguousarray(np.asarray(a, dtype=np.float32))
    m = {
        "x": f32(inputs["x"][b]), "ctx": f32(inputs["ctx"][b]), "c": f32(inputs["c"][b:b + 1]),
        "c_ctx": f32(inputs["c_ctx"]).reshape(1, D),
        "ada_w": f32(inputs["ada_w"]), "ada_b": f32(inputs["ada_b"]), "ln_g": f32(inputs["ln_g"]), "ln_b": f32(inputs["ln_b"]),
        "even_w_in": f32(inputs["even_w_in"][0]), "even_w_out": f32(inputs["even_w_out"][0]),
        "s5_lam_re": f32(inputs["s5_lam_re"][0]), "s5_lam_im": f32(inputs["s5_lam_im"][0]), "s5_log_step": f32(inputs["s5_log_step"][0]),
        "s5_b_re": f32(inputs["s5_b_re"][0]), "s5_b_im": f32(inputs["s5_b_im"][0]),
        "s5_c_re": f32(inputs["s5_c_re"][0]), "s5_c_im": f32(inputs["s5_c_im"][0]),
        "s5_d": f32(inputs["s5_d"][0]), "s5_w_glu": f32(inputs["s5_w_glu"][0]), "s5_b_glu": f32(inputs["s5_b_glu"][0]),
        "win_sink": f32(inputs["win_sink"][0]),
        "odd_w_in": f32(inputs["odd_w_in"][0]), "odd_w_out": f32(inputs["odd_w_out"][0]),
        "odd_q_norm": f32(inputs["odd_q_norm"][0]), "odd_k_norm": f32(inputs["odd_k_norm"][0]),
        "router_w": f32(inputs["router_w"]), "router_bias": f32(inputs["router_bias"]),
        "moe_w_gate": f32(inputs["moe_w_gate"]), "moe_w_up": f32(inputs["moe_w_up"]), "moe_w_down": f32(inputs["moe_w_down"]),
    }
    m.update(_consts())
    return m


def kernel(**inputs):
    nc = build_program()
    shared = make_in_map(inputs, 0)
    in_maps = []
    for b in range(8):
        m = dict(shared)
        m["x"] = np.ascontiguousarray(np.asarray(inputs["x"][b], dtype=np.float32))
        m["ctx"] = np.ascontiguousarray(np.asarray(inputs["ctx"][b], dtype=np.float32))
        m["c"] = np.ascontiguousarray(np.asarray(inputs["c"][b:b + 1], dtype=np.float32))
        in_maps.append(m)
    res = run_bass_kernel_spmd(nc, in_maps, core_ids=list(range(8)))
    return np.stack([np.asarray(r["out"], dtype=np.float32) for r in res.results], axis=0)
```

```python
import math
import os
DBG_SKIP = os.environ.get('DBG_SKIP', '').split(',')
DBG_NT = int(os.environ.get('DBG_NT', '34'))
from contextlib import ExitStack
import numpy as np
import ml_dtypes
import concourse.bass as bass
import concourse.mybir as mybir
from concourse.bass_utils import run_bass_kernel_spmd

F32 = mybir.dt.float32
BF16 = mybir.dt.bfloat16
I32 = mybir.dt.int32
ALU = mybir.AluOpType
AF = mybir.ActivationFunctionType
AX = mybir.AxisListType

SEM_LIMIT = 30000
NT = 34
NTOK = 4352
D = 1024
ALPHA = 4.0 ** 0.25
LN_EPS = 1e-5
RMS_EPS = 1e-6
TWO_PI = 2.0 * math.pi
CW1 = 6.28125
CW2 = TWO_PI - CW1


class Res:
    __slots__ = ("name", "w", "r", "x")

    def __init__(self, name="", x=False):
        self.name = name
        self.w = None
        self.r = []
        self.x = x


def RL(n, name="r"):
    return [Res("%s%d" % (name, i)) for i in range(n)]


class EngState:
    def __init__(self, fw, name, eng):
        self.fw = fw
        self.name = name
        self.eng = eng
        self.count = 0
        self.epoch = 0
        self.known = {}
        self._new_sem()

    def _new_sem(self):
        self.sem_key = "%s_e%d" % (self.name, self.epoch)
        self.sem = self.fw.new_sem(self.sem_key)
        self.count = 0
        self.epoch += 1


class FW:
    def __init__(self, nc, n_dma_sems=10):
        self.nc = nc
        self.es = ExitStack()
        self.sems = {}
        self.engs = {}
        for name, eng in (("pe", nc.tensor), ("act", nc.scalar), ("dve", nc.vector),
                          ("pool", nc.gpsimd), ("sp", nc.sync)):
            self.engs[name] = EngState(self, name, eng)
        self.dma_pool = {}
        for q in ("sp", "act", "pool"):
            lst = []
            for i in range(n_dma_sems):
                key = "dma_%s_%d" % (q, i)
                lst.append([key, self.new_sem(key), 0])
            self.dma_pool[q] = [lst, 0]
        self.n_instr = 0
        self.n_waits = 0

    def new_sem(self, key):
        s = self.es.enter_context(self.nc.semaphore(key))
        self.sems[key] = s
        return s

    def _wait(self, E, ev):
        if ev is None:
            return
        key, val = ev
        if E.known.get(key, 0) >= val:
            return
        E.eng.wait_ge(self.sems[key], val)
        E.known[key] = val
        self.n_waits += 1

    def _deps(self, E, reads, writes, acc=False):
        for r in reads:
            self._wait(E, r.w)
            if r.x:
                for ev in r.r:
                    if ev[0] != E.sem_key:
                        self._wait(E, ev)
        for w in writes:
            if not ((acc or E.name == "pe") and w.w is not None and w.w[0] == E.sem_key):
                self._wait(E, w.w)
            for ev in w.r:
                self._wait(E, ev)

    def _commit(self, ev, reads, writes):
        for r in reads:
            r.r.append(ev)
            if len(r.r) > 16:
                d = {}
                for k, v in r.r:
                    if d.get(k, 0) < v:
                        d[k] = v
                r.r = list(d.items())
        for w in writes:
            w.w = ev
            w.r = []

    def op(self, ename, fn, reads=(), writes=(), acc=False):
        E = self.engs[ename]
        if E.count >= SEM_LIMIT:
            E._new_sem()
        self._deps(E, reads, writes, acc=acc)
        ins = fn(E.eng)
        E.count += 1
        ins.then_inc(E.sem, 1)
        self._commit((E.sem_key, E.count), reads, writes)
        self.n_instr += 1
        return ins

    def _dma_common(self, qname, issue, reads, writes):
        E = self.engs[qname]
        pool, idx = self.dma_pool[qname]
        ent = pool[idx % len(pool)]
        self.dma_pool[qname][1] = idx + 1
        key, sem, val = ent
        if val > 0:
            self._wait(E, (key, val))
        if val + 16 > SEM_LIMIT:
            key = key + "n"
            sem = self.new_sem(key)
            val = 0
            ent[0], ent[1] = key, sem
        self._deps(E, reads, writes)
        ins = issue(E.eng)
        val += 16
        ent[2] = val
        ins.then_inc(sem, 16)
        ev = (key, val)
        self._commit(ev, reads, writes)
        self.n_instr += 1
        return ev

    def dma(self, qname, out, in_, reads=(), writes=(), **kw):
        return self._dma_common(qname, lambda e: e.dma_start(out=out, in_=in_, **kw), reads, writes)

    def barrier(self):
        evs = []
        for q in self.dma_pool:
            for key, sem, val in self.dma_pool[q][0]:
                if val > 0:
                    evs.append((key, val))
        for n, e in self.engs.items():
            if e.count > 0:
                evs.append((e.sem_key, e.count))
        for n, E in self.engs.items():
            for ev in evs:
                if ev[0] != E.sem_key:
                    self._wait(E, ev)

    def finish(self):
        E = self.engs["sp"]
        for q in self.dma_pool:
            for key, sem, val in self.dma_pool[q][0]:
                if val > 0:
                    self._wait(E, (key, val))
        for n, e in self.engs.items():
            if e.count > 0:
                self._wait(E, (e.sem_key, e.count))

    def close(self):
        self.es.close()


class Scope:
    FWREF = None

    def __init__(self, nc):
        self.nc = nc
        self.es = ExitStack()

    CNT = [0]

    def sb(self, name, shape, dtype=F32):
        Scope.CNT[0] += 1
        return self.es.enter_context(self.nc.sbuf_tensor("%s_%d" % (name, Scope.CNT[0]), list(shape), dtype))

    def ps(self, name, shape, dtype=F32):
        Scope.CNT[0] += 1
        return self.es.enter_context(self.nc.psum_tensor("%s_%d" % (name, Scope.CNT[0]), list(shape), dtype))

    def close(self):
        if Scope.FWREF is not None:
            Scope.FWREF.barrier()
        self.es.close()


def rev_ap(ap2d, n):
    last = ap2d[:, n - 1:n]
    return bass.AP(tensor=ap2d.tensor, offset=last.offset, ap=[list(ap2d.ap[0]), [-1, n]])


def build_program(stop_after=None, dbg_shape=None):
    nc = bass.Bass("TRN2", target_bir_lowering=False)

    def din(name, shape, dt=F32):
        return nc.dram_tensor(name, list(shape), dt, kind="ExternalInput").ap()

    x_d = din("x", [4096, D]); ctx_d = din("ctx", [256, D])
    c_d = din("c", [1, D]); cctx_d = din("c_ctx", [1, D])
    ada_w = din("ada_w", [2, D, 6 * D]); ada_b = din("ada_b", [2, 6 * D])
    ln_g = din("ln_g", [2, 2, D]); ln_b = din("ln_b", [2, 2, D])
    even_w_in = din("even_w_in", [D, 1280]); even_w_out = din("even_w_out", [D, D])
    lam_re = din("s5_lam_re", [2, 32, 64]); lam_im = din("s5_lam_im", [2, 32, 64])
    log_step = din("s5_log_step", [2, 32])
    b_re = din("s5_b_re", [2, 32, 64, 16]); b_im = din("s5_b_im", [2, 32, 64, 16])
    c_re = din("s5_c_re", [2, 32, 16, 64]); c_im = din("s5_c_im", [2, 32, 16, 64])
    s5_d = din("s5_d", [512]); w_glu = din("s5_w_glu", [512, 512]); b_glu = din("s5_b_glu", [512])
    win_sink = din("win_sink", [8])
    odd_w_in = din("odd_w_in", [D, 1536]); odd_w_out = din("odd_w_out", [D, D])
    q_norm = din("odd_q_norm", [64]); k_norm = din("odd_k_norm", [64])
    router_w = din("router_w", [D, 32]); router_b = din("router_bias", [32])
    w_gate = din("moe_w_gate", [2, 32, D, 512]); w_up = din("moe_w_up", [2, 32, D, 512])
    w_down = din("moe_w_down", [2, 32, 512, D])
    k_ident = din("k_ident", [128, 128]); k_rope = din("k_rope", [128, 2, 32, 32])
    k_mask = din("k_mask", [128, 3, 128]); k_jidx = din("k_jidx", [128, 128]); k_pc = din("k_pc", [128, 4])
    out_d = nc.dram_tensor("out", [4096, D], F32, kind="ExternalOutput").ap()
    XR = nc.dram_tensor("xr", [NTOK, D], F32, kind="Internal").ap()
    QT = nc.dram_tensor("qt_scr", [8, 128, 4096], BF16, kind="Internal").ap()
    OT = nc.dram_tensor("ot_scr", [8, 128, 4096], BF16, kind="Internal").ap()
    ATD = nc.dram_tensor("at_scr", [4, 128, NTOK], BF16, kind="Internal").ap()
    NS = 49
    XS = nc.dram_tensor("xs_scr", [NS * 512, D], BF16, kind="Internal").ap()
    YS = nc.dram_tensor("ys_scr", [NS * 512, D], F32, kind="Internal").ap()
    wg_all = w_gate.rearrange("l e (kk two) n -> (l e kk) (two n)", two=2)
    wu_all = w_up.rearrange("l e (kk two) n -> (l e kk) (two n)", two=2)
    wd_all = w_down.rearrange("l e f n -> (l e f) n")
    wg_rows = [wg_all, wg_all]; wu_rows = [wu_all, wu_all]; wd_rows = [wd_all, wd_all]
    dbg = None
    if dbg_shape is not None:
        dbg = nc.dram_tensor("dbg", list(dbg_shape), F32, kind="ExternalOutput").ap()

    f = FW(nc)
    Scope.FWREF = f
    G = Scope(nc)
    R_XR = RL(NT, "xr")
    R_out = Res("out")
    R_dbg = Res("dbg")

    ident = G.sb("ident", [128, 128]); R_ident = Res()
    identb = G.sb("identb", [128, 128], BF16); R_identb = Res()
    f.dma("sp", ident[:], k_ident, writes=[R_ident])
    f.op("dve", lambda e: e.tensor_copy(identb[:], ident[:]), reads=[R_ident], writes=[R_identb])
    rope = G.sb("rope", [128, 2, 32, 32]); R_rope = Res()
    f.dma("sp", rope[:], k_rope, writes=[R_rope])
    maskf = G.sb("maskf", [128, 3, 128]); maskb = G.sb("maskb", [128, 3, 128], BF16); R_mask = Res()
    f.dma("sp", maskf[:], k_mask, writes=[R_mask])
    f.op("dve", lambda e: e.tensor_copy(maskb[:], maskf[:]), reads=[R_mask], writes=[R_mask])
    R_crep = Res()
    ctmp = G.sb("ctmp", [128, 2, 8]); R_ctmp = Res()
    f.dma("sp", ctmp[:, 0, :], c_d.rearrange("o (kc p) -> p (o kc)", p=128), writes=[R_ctmp], allow_slow_non_contiguous=True)
    f.dma("sp", ctmp[:, 1, :], cctx_d.rearrange("o (kc p) -> p (o kc)", p=128), writes=[R_ctmp], allow_slow_non_contiguous=True)
    f.op("act", lambda e: e.activation(out=ctmp[:], in_=ctmp[:], func=AF.Silu), reads=[R_ctmp], writes=[R_ctmp])
    lng = G.sb("lng", [128, D]); lnb = G.sb("lnb", [128, D]); R_ln = Res()

    def load_ln(li):
        f.dma("sp", lng[:], ln_g[li // 2, li % 2].partition_broadcast(128), writes=[R_ln])
        f.dma("sp", lnb[:], ln_b[li // 2, li % 2].partition_broadcast(128), writes=[R_ln])
    epsc = G.sb("epsc", [128, 1]); R_eps = Res()
    f.op("dve", lambda e: e.memset(epsc[:], LN_EPS), writes=[R_eps])

    R_XsZ = RL(28, "xsz")
    ZS = Scope(nc)
    zt = ZS.sb("zt", [128, 7, D], BF16); R_zt = Res()
    f.op("pool", lambda e: e.memset(zt[:], 0.0), writes=[R_zt])
    for k in range(28):
        f.dma(("sp", "act")[k % 2], XS[k * 896:(k + 1) * 896, :].rearrange("(a p) d -> p a d", p=128), zt[:], reads=[R_zt], writes=[R_XsZ[k]])
    ZS.close()

    mod = G.sb("mod", [128, 2, 3, D]); R_mod = Res("mod")

    def dump(ap_sb, rows, cols, reads, r0=0, c0=0):
        f.dma("sp", dbg[r0:r0 + rows, c0:c0 + cols], ap_sb, reads=reads, writes=[R_dbg])

    def phase_mod(i, s):
        S = Scope(nc)
        crep = S.sb("crep", [128, 2, 8, 128])
        f.op("dve", lambda e: e.tensor_copy(crep[:], ctmp[:].unsqueeze(3).broadcast_to([128, 2, 8, 128])), reads=[R_ctmp], writes=[R_crep])
        slab = [S.sb("slab%d" % k, [128, 8, 512]) for k in range(2)]; R_slab = RL(2)
        adb = [S.sb("adb%d" % k, [128, 512]) for k in range(2)]; R_adb = RL(2)
        psm = [S.ps("psm%d" % k, [128, 512]) for k in range(2)]; R_psm = RL(2)
        n = 0
        for blk in range(6):
            c0 = s * 3072 + blk * 512
            bi = blk % 2
            f.dma("sp", slab[bi][:], ada_w[i, :, c0:c0 + 512].rearrange("(kc p) n -> p kc n", p=128), writes=[R_slab[bi]])
            f.dma("act", adb[bi][:], ada_b[i, c0:c0 + 512].partition_broadcast(128), writes=[R_adb[bi]])
            k, half = blk // 2, blk % 2
            for which in range(2):
                pi = n % 2; n += 1
                for kc in range(8):
                    f.op("pe", lambda e, kc=kc: e.matmul(psm[pi][:], crep[:, which, kc, :], slab[bi][:, kc, :], start=(kc == 0), stop=(kc == 7)),
                         reads=[R_crep, R_slab[bi]], writes=[R_psm[pi]], acc=(kc > 0))
                dst = mod[:, which, k, half * 512:(half + 1) * 512]
                f.op("dve", lambda e: e.scalar_tensor_tensor(out=dst, in0=psm[pi][:], scalar=(1.0 if k == 1 else 0.0), in1=adb[bi][:], op0=ALU.add, op1=ALU.add),
                     reads=[R_psm[pi], R_adb[bi]], writes=[R_mod])
        S.close()

    def resid_ln(S, xt, R_xt, o_ps, R_ops, which, li, out_t, R_outt, tmp, R_tmp, small, R_small):
        gate = mod[:, which, 2, :]
        f.op("dve", lambda e: e.tensor_tensor(tmp[:], o_ps[:], gate, ALU.mult), reads=[R_ops, R_mod], writes=[R_tmp])
        f.op("dve", lambda e: e.scalar_tensor_tensor(out=tmp[:], in0=xt[:], scalar=ALPHA, in1=tmp[:], op0=ALU.mult, op1=ALU.add),
             reads=[R_xt, R_tmp], writes=[R_tmp])
        f.op("dve", lambda e: e.bn_stats(small[:, 0:6], tmp[:, 0:512]), reads=[R_tmp], writes=[R_small])
        f.op("dve", lambda e: e.bn_stats(small[:, 6:12], tmp[:, 512:1024]), reads=[R_tmp], writes=[R_small])
        f.op("dve", lambda e: e.bn_aggr(small[:, 12:14], small[:, 0:12]), reads=[R_small], writes=[R_small])
        f.op("act", lambda e: e.activation(out=small[:, 14:15], in_=small[:, 13:14], func=AF.Sqrt, bias=epsc[:], scale=1.0), reads=[R_small, R_eps], writes=[R_small])
        f.op("dve", lambda e: e.reciprocal(small[:, 15:16], small[:, 14:15]), reads=[R_small], writes=[R_small])
        f.op("dve", lambda e: e.tensor_scalar(out=tmp[:], in0=tmp[:], scalar1=small[:, 12:13], scalar2=small[:, 15:16], op0=ALU.subtract, op1=ALU.mult),
             reads=[R_tmp, R_small], writes=[R_tmp])
        f.op("dve", lambda e: e.tensor_tensor(tmp[:], tmp[:], lng[:], ALU.mult), reads=[R_tmp, R_ln], writes=[R_tmp])
        f.op("dve", lambda e: e.tensor_tensor(out_t[:], tmp[:], lnb[:], ALU.add), reads=[R_tmp, R_ln], writes=[R_outt])

    def mod_transpose(xt, R_xt, which, h32, R_h32, ps_tp, R_pstp, hT_dst, R_hT, h32T=None, R_h32T=None):
        f.op("dve", lambda e: e.tensor_tensor(h32[:], xt[:], mod[:, which, 1, :], ALU.mult), reads=[R_xt, R_mod], writes=[R_h32])
        f.op("dve", lambda e: e.tensor_tensor(h32[:], h32[:], mod[:, which, 0, :], ALU.add), reads=[R_h32, R_mod], writes=[R_h32])
        for kc in range(8):
            f.op("pe", lambda e, kc=kc: e.transpose(ps_tp[:, kc, :], h32[:, kc * 128:(kc + 1) * 128], ident[:]),
                 reads=[R_h32, R_ident], writes=[R_pstp], acc=(kc > 0))
        f.op("act", lambda e: e.activation(out=hT_dst, in_=ps_tp[:], func=AF.Identity), reads=[R_pstp], writes=[R_hT])
        if h32T is not None:
            f.op("dve", lambda e: e.tensor_copy(h32T[:], ps_tp[:]), reads=[R_pstp], writes=[R_h32T])

    def src_tile(layer, t):
        if layer == 0:
            return (ctx_d[t * 128:(t + 1) * 128, :] if t < 2 else x_d[(t - 2) * 128:(t - 1) * 128, :]), []
        return XR[t * 128:(t + 1) * 128, :], [R_XR[t]]

    def layer0_mixer():
        L = Scope(nc)
        U = Scope(nc)
        uT = U.sb("uT", [128, 4, NTOK], BF16); R_uT = RL(NT, "uT")
        aT, R_aT = uT, R_uT

        def inproj(do_u, qT=None, R_qT=None, kT2=None, R_kT=None, vaug=None, R_v=None):
            S = Scope(nc)
            wc0, wc1 = (0, 512) if do_u else (512, 1280)
            win = S.sb("win", [128, 8, wc1 - wc0], BF16); R_win = Res()
            f.dma("pool", win[:], even_w_in[:, wc0:wc1].rearrange("(kc p) n -> p kc n", p=128), writes=[R_win])
            xt = [S.sb("xt%d" % k, [128, D]) for k in range(2)]; R_xt = RL(2)
            h32 = S.sb("h32", [128, D]); R_h32 = Res()
            hT = [S.sb("hT%d" % k, [128, 8, 128], BF16) for k in range(2)]; R_hT = RL(2)
            ps_tp = S.ps("ps_tp", [128, 8, 128]); R_pstp = Res(x=True)
            ps_u = S.ps("ps_u", [128, 4, 128]); R_psu = Res()
            ps_q = S.ps("ps_q", [128, 1024]); R_psq = Res(x=True)
            ps_t = S.ps("ps_t", [128, 8, 128], BF16); R_pst = Res(x=True)
            ra = S.sb("ra", [128, 10, 32]); rb = S.sb("rb", [128, 10, 32]); R_ra = Res(); R_rb = Res()
            tqk = S.sb("tqk", [128, 640], BF16); R_tqk = Res()
            kd = S.sb("kd", [128, 2, 2, 64], BF16); R_kd = Res()
            for t in range(NT if do_u else min(NT, DBG_NT)):
                b = t % 2
                src, rs = src_tile(0, t)
                f.dma("sp", xt[b][:], src, reads=rs, writes=[R_xt[b]])
                which = 1 if t < 2 else 0
                mod_transpose(xt[b], R_xt[b], which, h32, R_h32, ps_tp, R_pstp, hT[b][:], R_hT[b])
                cols = slice(t * 128, (t + 1) * 128)
                if stop_after == "h0" and t == 0:
                    f.dma("sp", dbg[0:128, :], h32[:], reads=[R_h32], writes=[R_dbg])
                    hf = S.sb("hf", [128, 1024]); R_hf = Res()
                    f.op("dve", lambda e: e.tensor_copy(hf[:], hT[b][:].rearrange("p a b -> p (a b)")), reads=[R_hT[b]], writes=[R_hf])
                    f.dma("sp", dbg[128:256, :], hf[:], reads=[R_hf], writes=[R_dbg])
                    f.op("dve", lambda e: e.tensor_copy(hf[:], win[:, 0, 0:1024]), reads=[R_win], writes=[R_hf])
                    f.dma("sp", dbg[256:384, :], hf[:], reads=[R_hf], writes=[R_dbg])
                    S.close(); return
                if do_u:
                    for ct in range(4):
                        for kc in range(8):
                            f.op("pe", lambda e, ct=ct, kc=kc: e.matmul(ps_u[:, ct, :], win[:, kc, ct * 128:(ct + 1) * 128], hT[b][:, kc, :], start=(kc == 0), stop=(kc == 7)),
                                 reads=[R_win, R_hT[b]], writes=[R_psu], acc=(ct + kc > 0))
                    f.op("act", lambda e: e.activation(out=uT[:, :, cols], in_=ps_u[:], func=AF.Identity), reads=[R_psu], writes=[R_uT[t]])
                    continue
                for (n0, n1) in ((0, 512), (512, 768)):
                    for kc in range(8):
                        f.op("pe", lambda e, kc=kc, n0=n0, n1=n1: e.matmul(ps_q[:, n0:n1], hT[b][:, kc, :], win[:, kc, n0:n1], start=(kc == 0), stop=(kc == 7)),
                             reads=[R_win, R_hT[b]], writes=[R_psq], acc=(n0 + kc > 0))
                if 'rope' in DBG_SKIP:
                    continue
                if t >= 2:
                    pv = ps_q[:, 0:640].rearrange("p (h two f) -> p h two f", two=2, f=32)
                    ov = tqk[:].rearrange("p (h two f) -> p h two f", two=2, f=32)
                    cosb = rope[:, 0, t - 2, :].unsqueeze(1).broadcast_to([128, 10, 32])
                    sinb = rope[:, 1, t - 2, :].unsqueeze(1).broadcast_to([128, 10, 32])
                    f.op("dve", lambda e: e.tensor_tensor(ra[:], pv[:, :, 0, :], cosb, ALU.mult), reads=[R_psq, R_rope], writes=[R_ra])
                    f.op("dve", lambda e: e.tensor_tensor(rb[:], pv[:, :, 1, :], sinb, ALU.mult), reads=[R_psq, R_rope], writes=[R_rb])
                    f.op("dve", lambda e: e.tensor_tensor(ov[:, :, 0, :], ra[:], rb[:], ALU.subtract), reads=[R_ra, R_rb], writes=[R_tqk])
                    f.op("dve", lambda e: e.tensor_tensor(ra[:], pv[:, :, 1, :], cosb, ALU.mult), reads=[R_psq, R_rope, R_tqk], writes=[R_ra])
                    f.op("dve", lambda e: e.tensor_tensor(rb[:], pv[:, :, 0, :], sinb, ALU.mult), reads=[R_psq, R_rope, R_tqk], writes=[R_rb])
                    f.op("dve", lambda e: e.tensor_tensor(ov[:, :, 1, :], ra[:], rb[:], ALU.add), reads=[R_ra, R_rb], writes=[R_tqk])
                else:
                    f.op("act", lambda e: e.activation(out=tqk[:], in_=ps_q[:, 0:640], func=AF.Identity), reads=[R_psq], writes=[R_tqk])
                if 'vaug' in DBG_SKIP:
                    continue
                for a in range(2):
                    if 'novaug' in DBG_SKIP:
                        break
                    f.op("dve", lambda e, a=a: e.tensor_copy(vaug[:, t, 64 + 128 * a:128 + 128 * a], ps_q[:, 640 + 64 * a:704 + 64 * a]),
                         reads=[R_psq], writes=[R_v[t]])
                if 'nokd' in DBG_SKIP:
                    continue
                kv = tqk[:, 512:640].rearrange("p (a d) -> p a d", a=2)
                f.op("dve", lambda e: e.tensor_copy(kd[:, :, 0, :], kv), reads=[R_tqk], writes=[R_kd])
                f.op("dve", lambda e: e.tensor_copy(kd[:, :, 1, :], kv), reads=[R_tqk], writes=[R_kd])
                if 'tr' in DBG_SKIP:
                    continue
                for pr in range(4):
                    f.op("pe", lambda e, pr=pr: e.transpose(ps_t[:, pr, :], tqk[:, pr * 128:(pr + 1) * 128], identb[:]),
                         reads=[R_tqk, R_identb], writes=[R_pst], acc=(pr > 0))
                for a in range(2):
                    f.op("pe", lambda e, a=a: e.transpose(ps_t[:, 4 + a, :], kd[:, a, :, :].rearrange("p a d -> p (a d)"), identb[:]),
                         reads=[R_kd, R_identb], writes=[R_pst], acc=True)
                f.op("dve", lambda e: e.tensor_copy(qT[:, :, cols], ps_t[:, 0:4, :]), reads=[R_pst], writes=[R_qT[t]])
                f.op("act", lambda e: e.activation(out=kT2[:, :, cols], in_=ps_t[:, 4:6, :], func=AF.Identity), reads=[R_pst], writes=[R_kT[t]])
            S.close()

        inproj(True)
        if stop_after == "h0":
            U.close(); L.close(); return
        if stop_after == "in0":
            S = Scope(nc)
            t32 = S.sb("t32", [128, 512]); R_t = Res()
            for ct in range(4):
                for blk in range(2):
                    f.op("dve", lambda e: e.tensor_copy(t32[:], uT[:, ct, blk * 512:(blk + 1) * 512]), reads=R_uT, writes=[R_t])
                    dump(t32[:], 128, 512, [R_t], r0=ct * 128, c0=blk * 512)
            S.close(); U.close(); L.close()
            return
        if 's5' not in DBG_SKIP:
            s5_phase(L, uT, R_uT, aT, R_aT)
        if stop_after == "s5":
            S = Scope(nc)
            t32 = S.sb("t32", [128, 512]); R_t = Res()
            for ct in range(4):
                for blk in range(9):
                    c0 = blk * 512; n = min(512, NTOK - c0)
                    f.op("dve", lambda e: e.tensor_copy(t32[:, 0:n], aT[:, ct, c0:c0 + n]), reads=R_aT, writes=[R_t])
                    dump(t32[:, 0:n], 128, n, [R_t], r0=ct * 128, c0=c0)
            S.close(); U.close(); L.close()
            return
        R_ATD = Res("atd")
        for k in range(4):
            f.dma(("sp", "act")[k % 2], ATD[k], aT[:, k, :], reads=R_aT, writes=[R_ATD])
        U.close()
        oT = L.sb("oT", [128, 4, NTOK], BF16); R_oT = RL(NT, "oT")
        W = Scope(nc)
        qT = W.sb("qT", [128, 4, NTOK], BF16); R_qT = RL(NT, "qT")
        kT2 = W.sb("kT2", [128, 2, NTOK], BF16); R_kT = RL(NT, "kT")
        vaug = W.sb("vaug", [128, NT, 320], BF16); R_v = RL(NT, "v")
        f.op("pool", lambda e: e.memset(vaug[:], 1.0), writes=R_v)
        inproj(False, qT, R_qT, kT2, R_kT, vaug, R_v)
        if stop_after == "qkv":
            W.close(); L.close(); return
        win_phase(qT, R_qT, kT2, R_kT, vaug, R_v, oT, R_oT)
        W.close()
        if stop_after == "win":
            L.close(); return

        M = Scope(nc)
        mixt = [M.sb("mixa%d" % k, [128, 4, 128], BF16) for k in range(2)]; R_mixt = RL(2)

        def mix_loader(t, b):
            c0 = t * 128
            f.dma("sp", mixt[b][:], ATD[:, :, c0:c0 + 128].rearrange("a p n -> p a n"), reads=[R_ATD], writes=[R_mixt[b]])
            return [mixt[b][:, k, :] for k in range(4)] + [oT[:, k, c0:c0 + 128] for k in range(4)], [R_mixt[b], R_oT[t]]
        out_phase(0, even_w_out, mix_loader, range(NT))
        M.close()
        L.close()

    def sincos(S, ang, n, out_s, out_c, R, tag):
        ki = S.sb("ki_" + tag, [128, n], I32); kf = S.sb("kf_" + tag, [128, n]); rd = S.sb("rd_" + tag, [128, n])
        f.op("dve", lambda e: e.tensor_scalar(out=ki[:], in0=ang, scalar1=1.0 / TWO_PI, scalar2=None, op0=ALU.mult), reads=[R], writes=[R])
        f.op("dve", lambda e: e.tensor_copy(kf[:], ki[:]), reads=[R], writes=[R])
        f.op("dve", lambda e: e.scalar_tensor_tensor(out=rd[:], in0=kf[:], scalar=-CW1, in1=ang, op0=ALU.mult, op1=ALU.add), reads=[R], writes=[R])
        f.op("dve", lambda e: e.scalar_tensor_tensor(out=rd[:], in0=kf[:], scalar=-CW2, in1=rd[:], op0=ALU.mult, op1=ALU.add), reads=[R], writes=[R])
        f.op("dve", lambda e: e.tensor_scalar(out=rd[:], in0=rd[:], scalar1=3.1415925, scalar2=-3.1415925, op0=ALU.min, op1=ALU.max), reads=[R], writes=[R])
        f.op("act", lambda e: e.activation(out=out_s, in_=rd[:], func=AF.Sin), reads=[R], writes=[R])
        f.op("dve", lambda e: e.scalar_tensor_tensor(out=rd[:], in0=rd[:], scalar=-1.0, in1=rd[:], op0=ALU.mult, op1=ALU.max), reads=[R], writes=[R])
        f.op("dve", lambda e: e.tensor_scalar(out=rd[:], in0=rd[:], scalar1=-1.0, scalar2=math.pi / 2, op0=ALU.mult, op1=ALU.add), reads=[R], writes=[R])
        f.op("act", lambda e: e.activation(out=out_c, in_=rd[:], func=AF.Sin), reads=[R], writes=[R])

    def s5_phase(L, uT, R_uT, aT, R_aT):
        P = Scope(nc)
        R = Res("s5setup")
        prm = P.sb("prm", [128, 16, 32])
        dsk = P.sb("dsk", [128, 4]); bgl = P.sb("bgl", [128, 4])
        cs2 = P.sb("cs2", [128, 32, 2]); ncs2 = P.sb("ncs2", [128, 32, 2])
        jt = P.sb("jt", [128, 128]); f.dma("sp", jt[:], k_jidx, writes=[R])
        f.dma("sp", dsk[:], s5_d.rearrange("(c p) -> p c", p=128), writes=[R], allow_slow_non_contiguous=True)
        f.dma("sp", bgl[:], b_glu.rearrange("(c p) -> p c", p=128), writes=[R], allow_slow_non_contiguous=True)
        S = Scope(nc)
        st32 = S.sb("st32", [32, 3, 128]); lsr = S.sb("lsr", [32, 2])
        f.dma("sp", st32[:, 0, :], lam_re.rearrange("d (q g) n -> (d q) (g n)", g=2), writes=[R])
        f.dma("sp", st32[:, 1, :], lam_im.rearrange("d (q g) n -> (d q) (g n)", g=2), writes=[R])
        f.dma("sp", lsr[:], log_step.rearrange("d (q g) -> (d q) g", g=2), writes=[R])
        f.op("dve", lambda e: e.tensor_copy(st32[:, 2, :].rearrange("p (g n) -> p g n", g=2), lsr[:].unsqueeze(2).broadcast_to([32, 2, 64])), reads=[R], writes=[R])
        pst = S.ps("pst", [128, 4, 128])
        for k in range(3):
            f.op("pe", lambda e, k=k: e.transpose(pst[:, k, 0:32], st32[:, k, :], ident[0:32, 0:32]), reads=[R, R_ident], writes=[R], acc=(k > 0))
        f.op("dve", lambda e: e.tensor_copy(prm[:, 0:3, :], pst[:, 0:3, 0:32]), reads=[R], writes=[R])
        lr, li = prm[:, 0, :], prm[:, 1, :]
        dt, th, rr = prm[:, 3, :], prm[:, 4, :], prm[:, 5, :]
        f.op("act", lambda e: e.activation(out=dt, in_=prm[:, 2, :], func=AF.Exp), reads=[R], writes=[R])
        f.op("dve", lambda e: e.tensor_tensor(th, li, dt, ALU.mult), reads=[R], writes=[R])
        f.op("dve", lambda e: e.tensor_tensor(prm[:, 10, :], lr, dt, ALU.mult), reads=[R], writes=[R])
        f.op("act", lambda e: e.activation(out=rr, in_=prm[:, 10, :], func=AF.Exp), reads=[R], writes=[R])
        f.op("dve", lambda e: e.tensor_scalar(out=prm[:, 10, :], in0=th, scalar1=128.0, scalar2=None, op0=ALU.mult), reads=[R], writes=[R])
        sincos(S, prm[:, 10, :], 32, prm[:, 7, :], prm[:, 6, :], R, "a")
        sincos(S, th, 32, prm[:, 12, :], prm[:, 11, :], R, "b")
        abre, abim, den, t1, t2 = prm[:, 13, :], prm[:, 14, :], prm[:, 15, :], prm[:, 10, :], prm[:, 2, :]
        f.op("dve", lambda e: e.tensor_tensor(abre, rr, prm[:, 11, :], ALU.mult), reads=[R], writes=[R])
        f.op("dve", lambda e: e.tensor_scalar(out=abre, in0=abre, scalar1=-1.0, scalar2=None, op0=ALU.add), reads=[R], writes=[R])
        f.op("dve", lambda e: e.tensor_tensor(abim, rr, prm[:, 12, :], ALU.mult), reads=[R], writes=[R])
        f.op("dve", lambda e: e.tensor_tensor(den, lr, lr, ALU.mult), reads=[R], writes=[R])
        f.op("dve", lambda e: e.tensor_tensor(t1, li, li, ALU.mult), reads=[R], writes=[R])
        f.op("dve", lambda e: e.tensor_tensor(den, den, t1, ALU.add), reads=[R], writes=[R])
        f.op("dve", lambda e: e.reciprocal(den, den), reads=[R], writes=[R])
        f.op("dve", lambda e: e.tensor_tensor(t1, abre, lr, ALU.mult), reads=[R], writes=[R])
        f.op("dve", lambda e: e.tensor_tensor(t2, abim, li, ALU.mult), reads=[R], writes=[R])
        f.op("dve", lambda e: e.tensor_tensor(t1, t1, t2, ALU.add), reads=[R], writes=[R])
        f.op("dve", lambda e: e.tensor_tensor(prm[:, 8, :], t1, den, ALU.mult), reads=[R], writes=[R])
        f.op("dve", lambda e: e.tensor_tensor(t1, abim, lr, ALU.mult), reads=[R], writes=[R])
        f.op("dve", lambda e: e.tensor_tensor(t2, abre, li, ALU.mult), reads=[R], writes=[R])
        f.op("dve", lambda e: e.tensor_tensor(t1, t1, t2, ALU.subtract), reads=[R], writes=[R])
        f.op("dve", lambda e: e.tensor_tensor(prm[:, 9, :], t1, den, ALU.mult), reads=[R], writes=[R])
        f.op("dve", lambda e: e.tensor_copy(cs2[:, :, 0], prm[:, 6, :]), reads=[R], writes=[R])
        f.op("dve", lambda e: e.tensor_copy(cs2[:, :, 1], prm[:, 7, :]), reads=[R], writes=[R])
        f.op("dve", lambda e: e.tensor_scalar(out=ncs2[:, :, 0], in0=prm[:, 7, :], scalar1=-1.0, scalar2=None, op0=ALU.mult), reads=[R], writes=[R])
        f.op("dve", lambda e: e.tensor_copy(ncs2[:, :, 1], prm[:, 6, :]), reads=[R], writes=[R])
        S.close()
        cosJ = P.sb("cosJ", [128, 8, 128]); sinJ = P.sb("sinJ", [128, 8, 128]); rtab = P.sb("rtab", [128, 8, 128])
        lB = P.sb("lB", [128, 8, 2, 128], BF16); lC = P.sb("lC", [128, 8, 2, 128], BF16)
        RT = Res("s5tab")
        S = Scope(nc)
        yacc = S.sb("yacc", [128, NTOK]); R_y = Res("yacc")
        NB = 2
        psb = [S.ps("psb%d" % k, [128, 2, 512]) for k in range(NB)]; R_psb = RL(NB)
        psy = [S.ps("psy%d" % k, [128, 512]) for k in range(NB)]; R_psy = RL(NB)
        pstr = [S.ps("pstr%d" % k, [128, 4, 128]) for k in range(2)]; R_pstr = RL(2)
        m = [S.sb("m%d" % k, [128, 2, 512]) for k in range(NB)]; R_m = RL(NB)
        ta = [S.sb("ta%d" % k, [128, 2, 512]) for k in range(NB)]; R_ta = RL(NB)
        g = [S.sb("g%d" % k, [128, 2, 512]) for k in range(NB)]; R_g = RL(NB)
        hb = [S.sb("hb%d" % k, [128, 2, 512], BF16) for k in range(NB)]; R_hb = RL(NB)
        ini = S.sb("ini", [128, 4]); R_ini = Res()
        gq1 = S.sb("gq1", [128, 512]); gq2 = S.sb("gq2", [128, 512]); R_gq1 = Res(); R_gq2 = Res()
        wgl = S.sb("wgl", [128, 4, 512], BF16); R_wgl = Res()
        f.dma("pool", wgl[:], w_glu.rearrange("(kc p) n -> p kc n", p=128), writes=[R_wgl])
        blocks = [(0, 256)] + [(256 + 512 * k, 512) for k in range(8)]
        it = 0
        for ct in range(4):
            T = Scope(nc)
            ang = T.sb("ang", [128, 8, 128])
            WB = T.sb("WB", [128, 2, 8, 128]); SC = T.sb("SC", [128, 2, 8, 128]); WB2 = T.sb("WB2", [128, 2, 8, 128])
            fre8 = T.sb("fre8", [128, 8]); fim8 = T.sb("fim8", [128, 8])
            for d in range(2):
                gsl = slice(d * 16 + ct * 4, d * 16 + ct * 4 + 4); lsl = slice(d * 4, d * 4 + 4)
                f.op("dve", lambda e: e.tensor_tensor(ang[:, lsl, :], jt[:].unsqueeze(1).broadcast_to([128, 4, 128]), th[:, gsl].unsqueeze(2).broadcast_to([128, 4, 128]), ALU.mult), reads=[R, RT], writes=[RT])
                f.op("dve", lambda e: e.tensor_copy(rtab[:, lsl, :], rr[:, gsl].unsqueeze(2).broadcast_to([128, 4, 128])), reads=[R, RT], writes=[RT])
                f.op("dve", lambda e: e.tensor_copy(fre8[:, lsl], prm[:, 8, gsl]), reads=[R, RT], writes=[RT])
                f.op("dve", lambda e: e.tensor_copy(fim8[:, lsl], prm[:, 9, gsl]), reads=[R, RT], writes=[RT])
            sincos(T, ang[:].rearrange("p a b -> p (a b)"), 1024, sinJ[:].rearrange("p a b -> p (a b)"), cosJ[:].rearrange("p a b -> p (a b)"), RT, "c%d" % ct)
            f.op("pool", lambda e: e.memset(WB[:], 0.0), reads=[RT], writes=[RT])
            f.op("pool", lambda e: e.memset(SC[:], 0.0), reads=[RT], writes=[RT])
            qn = 0
            for d in range(2):
                for gi in range(8):
                    g_ = ct * 8 + gi
                    l = d * 4 + gi // 2
                    gl = gi % 2
                    for ri, (bsrc, csrc) in enumerate(((b_re, c_re), (b_im, c_im))):
                        q1 = ("sp", "act")[qn % 2]; qn += 1
                        f.dma(q1, WB[64 * gl:64 * gl + 64, ri, l, 16 * gi:16 * gi + 16], bsrc[d, g_], writes=[RT])
                        f.dma(q1, SC[16 * gi:16 * gi + 16, ri, l, 64 * gl:64 * gl + 64], csrc[d, g_], writes=[RT])
            fre = fre8[:].unsqueeze(2).broadcast_to([128, 8, 128]); fim = fim8[:].unsqueeze(2).broadcast_to([128, 8, 128])
            f.op("dve", lambda e: e.tensor_tensor(WB2[:, 0], WB[:, 0], fre, ALU.mult), reads=[RT], writes=[RT])
            f.op("pool", lambda e: e.tensor_tensor(WB2[:, 1], WB[:, 1], fim, ALU.mult), reads=[RT], writes=[RT])
            f.op("dve", lambda e: e.tensor_tensor(WB2[:, 0], WB2[:, 0], WB2[:, 1], ALU.subtract), reads=[RT], writes=[RT])
            f.op("pool", lambda e: e.tensor_tensor(WB2[:, 1], WB[:, 1], fre, ALU.mult), reads=[RT], writes=[RT])
            f.op("dve", lambda e: e.tensor_tensor(WB[:, 0], WB[:, 0], fim, ALU.mult), reads=[RT], writes=[RT])
            f.op("dve", lambda e: e.tensor_tensor(WB2[:, 1], WB2[:, 1], WB[:, 0], ALU.add), reads=[RT], writes=[RT])
            n_ = 0
            for srct, dst, neg in ((WB2, lB, False), (SC, lC, True)):
                for ri in range(2):
                    for d4 in range(2):
                        pb = n_ % 2; n_ += 1
                        for k in range(4):
                            l = d4 * 4 + k
                            f.op("pe", lambda e, k=k, l=l: e.transpose(pstr[pb][:, k, :], srct[:, ri, l, :], ident[:]), reads=[RT, R_ident], writes=[R_pstr[pb]], acc=(k > 0))
                        scl = -1.0 if (neg and ri == 1) else 1.0
                        f.op("act", lambda e: e.activation(out=dst[:, d4 * 4:d4 * 4 + 4, ri, :], in_=pstr[pb][:], func=AF.Identity, scale=scl), reads=[R_pstr[pb], RT], writes=[RT])
            T.close()
            f.op("act", lambda e: e.activation(out=yacc[:], in_=uT[:, ct, :], func=AF.Copy, scale=dsk[:, ct:ct + 1]), reads=R_uT + [R], writes=[R_y])
            items = []
            for pi in range(4):
                for d in range(2):
                    for bidx, (s0, n) in enumerate(blocks):
                        items.append((pi, d, bidx, s0, n))
            NI = len(items)

            def v3(ap):
                return ap.rearrange("p (c j) -> p c j", j=128)

            def geom(k):
                pi, d, bidx, s0, n = items[k]
                bi = k % NB
                dq = d * 16 + ct * 4 + pi
                l = d * 4 + pi
                nch = n // 128
                if d == 0:
                    c0 = s0
                    ucols = uT[:, ct, c0:c0 + n]
                    ycols = yacc[:, c0:c0 + n]
                else:
                    c0 = (256 - s0 - n) if s0 < 256 else (4608 - s0 - n)
                    ucols = rev_ap(uT[:, ct, c0:c0 + n], n)
                    ycols = rev_ap(yacc[:, c0:c0 + n], n)
                tl = [R_uT[kk] for kk in range(c0 // 128, (c0 + n) // 128)]
                cb = cosJ[:, l, :].unsqueeze(1).broadcast_to([128, nch, 128])
                sb_ = sinJ[:, l, :].unsqueeze(1).broadcast_to([128, nch, 128])
                return pi, d, bidx, n, bi, dq, l, nch, ucols, ycols, tl, cb, sb_

            def stA(k):
                pi, d, bidx, n, bi, dq, l, nch, ucols, ycols, tl, cb, sb_ = geom(k)
                for ri in range(2):
                    f.op("pe", lambda e, ri=ri: e.matmul(psb[bi][:, ri, 0:n], lB[:, l, ri, :], ucols, start=True, stop=True),
                         reads=[RT] + tl, writes=[R_psb[bi]], acc=(ri > 0))
                bre, bim = v3(psb[bi][:, 0, 0:n]), v3(psb[bi][:, 1, 0:n])
                mre, mim = v3(m[bi][:, 0, 0:n]), v3(m[bi][:, 1, 0:n])
                t_a, t_b = v3(ta[bi][:, 0, 0:n]), v3(ta[bi][:, 1, 0:n])
                f.op("dve", lambda e: e.tensor_tensor(mre, bre, cb, ALU.mult), reads=[R_psb[bi], RT], writes=[R_m[bi]])
                f.op("dve", lambda e: e.tensor_tensor(t_a, bim, sb_, ALU.mult), reads=[R_psb[bi], RT], writes=[R_ta[bi]])
                f.op("dve", lambda e: e.tensor_tensor(mre, mre, t_a, ALU.add), reads=[R_m[bi], R_ta[bi]], writes=[R_m[bi]])
                f.op("dve", lambda e: e.tensor_tensor(mim, bim, cb, ALU.mult), reads=[R_psb[bi], RT], writes=[R_m[bi]])
                f.op("dve", lambda e: e.tensor_tensor(t_b, bre, sb_, ALU.mult), reads=[R_psb[bi], RT], writes=[R_ta[bi]])
                f.op("dve", lambda e: e.tensor_tensor(mim, mim, t_b, ALU.subtract), reads=[R_m[bi], R_ta[bi]], writes=[R_m[bi]])

            def stB(k):
                pi, d, bidx, n, bi, dq, l, nch, ucols, ycols, tl, cb, sb_ = geom(k)
                prev = None
                if bidx > 0:
                    pbi = (k - 1) % NB
                    pn = items[k - 1][4]
                    prev = (g[pbi], pbi, pn // 128 - 1)
                for c in range(nch):
                    cs = slice(c * 128, (c + 1) * 128)
                    if prev is None:
                        i_re = i_im = 0.0
                        rd_extra = []
                    else:
                        pg, pbi, pc_ = prev
                        gre_l = pg[:, 0, pc_ * 128 + 127:pc_ * 128 + 128]
                        gim_l = pg[:, 1, pc_ * 128 + 127:pc_ * 128 + 128]
                        c128 = prm[:, 6, dq:dq + 1]; s128 = prm[:, 7, dq:dq + 1]
                        f.op("dve", lambda e: e.tensor_scalar(out=ini[:, 0:2], in0=cs2[:, dq, :], scalar1=gre_l, scalar2=None, op0=ALU.mult), reads=[R_g[pbi], R], writes=[R_ini])
                        f.op("dve", lambda e: e.scalar_tensor_tensor(out=ini[:, 0:2], in0=ncs2[:, dq, :], scalar=gim_l, in1=ini[:, 0:2], op0=ALU.mult, op1=ALU.add), reads=[R_g[pbi], R, R_ini], writes=[R_ini])
                        i_re, i_im = ini[:, 0:1], ini[:, 1:2]
                        rd_extra = [R_ini]
                    f.op("dve", lambda e: e.tensor_tensor_scan(g[bi][:, 0, cs], rtab[:, l, :], m[bi][:, 0, cs], i_re, ALU.mult, ALU.add),
                         reads=[R_m[bi], RT] + rd_extra, writes=[R_g[bi]])
                    f.op("dve", lambda e: e.tensor_tensor_scan(g[bi][:, 1, cs], rtab[:, l, :], m[bi][:, 1, cs], i_im, ALU.mult, ALU.add),
                         reads=[R_m[bi], RT] + rd_extra, writes=[R_g[bi]])
                    prev = (g[bi], bi, c)

            def stC(k):
                pi, d, bidx, n, bi, dq, l, nch, ucols, ycols, tl, cb, sb_ = geom(k)
                mre, mim = v3(m[bi][:, 0, 0:n]), v3(m[bi][:, 1, 0:n])
                t_a, t_b = v3(ta[bi][:, 0, 0:n]), v3(ta[bi][:, 1, 0:n])
                gre, gim = v3(g[bi][:, 0, 0:n]), v3(g[bi][:, 1, 0:n])
                hre, him = v3(hb[bi][:, 0, 0:n]), v3(hb[bi][:, 1, 0:n])
                f.op("dve", lambda e: e.tensor_tensor(t_a, gre, cb, ALU.mult), reads=[R_g[bi], RT], writes=[R_ta[bi]])
                f.op("dve", lambda e: e.tensor_tensor(mre, gim, sb_, ALU.mult), reads=[R_g[bi], RT], writes=[R_m[bi]])
                f.op("dve", lambda e: e.tensor_tensor(hre, t_a, mre, ALU.subtract), reads=[R_ta[bi], R_m[bi]], writes=[R_hb[bi]])
                f.op("dve", lambda e: e.tensor_tensor(t_b, gre, sb_, ALU.mult), reads=[R_g[bi], RT], writes=[R_ta[bi]])
                f.op("dve", lambda e: e.tensor_tensor(mim, gim, cb, ALU.mult), reads=[R_g[bi], RT], writes=[R_m[bi]])
                f.op("dve", lambda e: e.tensor_tensor(him, t_b, mim, ALU.add), reads=[R_ta[bi], R_m[bi]], writes=[R_hb[bi]])
                for ri in range(2):
                    f.op("pe", lambda e, ri=ri: e.matmul(psy[bi][:, 0:n], lC[:, l, ri, :], hb[bi][:, ri, 0:n], start=(ri == 0), stop=(ri == 1)),
                         reads=[RT, R_hb[bi]], writes=[R_psy[bi]], acc=(ri > 0))

            def stY(k):
                pi, d, bidx, n, bi, dq, l, nch, ucols, ycols, tl, cb, sb_ = geom(k)
                f.op("dve", lambda e: e.tensor_tensor(ycols, psy[bi][:, 0:n], ycols, ALU.add), reads=[R_psy[bi], R_y], writes=[R_y])

            stA(0)
            for k in range(NI):
                if k + 1 < NI:
                    stA(k + 1)
                stB(k)
                stC(k)
                if k >= 1:
                    stY(k - 1)
            stY(NI - 1)
            for (s0, n) in blocks:
                yb = yacc[:, s0:s0 + n]
                f.op("dve", lambda e: e.tensor_tensor(gq1[:, 0:n], yb, yb, ALU.mult), reads=[R_y], writes=[R_gq1])
                f.op("dve", lambda e: e.tensor_scalar(out=gq1[:, 0:n], in0=gq1[:, 0:n], scalar1=0.044715, scalar2=1.0, op0=ALU.mult, op1=ALU.add), reads=[R_gq1], writes=[R_gq1])
                f.op("dve", lambda e: e.tensor_tensor(gq1[:, 0:n], gq1[:, 0:n], yb, ALU.mult), reads=[R_gq1, R_y], writes=[R_gq1])
                f.op("act", lambda e: e.activation(out=gq2[:, 0:n], in_=gq1[:, 0:n], func=AF.Sigmoid, scale=1.5957691216057308), reads=[R_gq1], writes=[R_gq2])
                f.op("dve", lambda e: e.tensor_tensor(aT[:, ct, s0:s0 + n], yb, gq2[:, 0:n], ALU.mult), reads=[R_gq2, R_y], writes=R_aT[s0 // 128:(s0 + n) // 128])
        sg = [S.sb("sg%d" % k, [128, 512], BF16) for k in range(2)]; R_sg = RL(2)
        anew = S.sb("anew", [128, 4, 512], BF16); R_anew = Res()
        nn = 0
        for (s0, n) in blocks:
            tl = R_aT[s0 // 128:(s0 + n) // 128]
            for cto in range(4):
                bi = nn % 2; nn += 1
                for cti in range(4):
                    f.op("pe", lambda e, cti=cti: e.matmul(psy[bi][:, 0:n], wgl[:, cti, cto * 128:(cto + 1) * 128], aT[:, cti, s0:s0 + n], start=(cti == 0), stop=(cti == 3)),
                         reads=[R_wgl] + tl, writes=[R_psy[bi]], acc=(cti > 0))
                f.op("act", lambda e: e.activation(out=sg[bi][:, 0:n], in_=psy[bi][:, 0:n], func=AF.Sigmoid, bias=bgl[:, cto:cto + 1], scale=1.0), reads=[R_psy[bi], R], writes=[R_sg[bi]])
                f.op("dve", lambda e: e.tensor_tensor(anew[:, cto, 0:n], aT[:, cto, s0:s0 + n], sg[bi][:, 0:n], ALU.mult), reads=[R_sg[bi]] + tl, writes=[R_anew])
            f.op("dve", lambda e: e.tensor_copy(aT[:, :, s0:s0 + n], anew[:, :, 0:n]), reads=[R_anew], writes=tl)
        S.close()
        P.close()

    def win_phase(qT, R_qT, kT2, R_kT, vaug, R_v, oT, R_oT):
        S = Scope(nc)
        esink = S.sb("esink", [128, 8]); R_es = Res()
        f.dma("sp", esink[:], win_sink.partition_broadcast(128), writes=[R_es])
        f.op("act", lambda e: e.activation(out=esink[:], in_=esink[:], func=AF.Exp), reads=[R_es], writes=[R_es])
        NBS = 3
        ps_s = [S.ps("ps_s%d" % k, [128, 8, 128]) for k in range(NBS)]; R_pss = RL(NBS)
        ps_o = [S.ps("ps_o%d" % k, [128, 512]) for k in range(2)]; R_pso = RL(2)
        pT = [S.sb("pT%d" % k, [128, 5, 128], BF16) for k in range(NBS)]; R_pT = RL(NBS)
        dtmp = [S.sb("dtmp%d" % k, [128, 128]) for k in range(2)]; R_dt = RL(2)
        items = []
        for t in range(NT):
            kts = [(0, None), (1, None)]
            if t >= 2:
                for kt in (t - 1, t, t + 1):
                    if 2 <= kt < NT:
                        kts.append((kt, (0 if kt == t - 1 else (1 if kt == t + 1 else None))))
            for h in range(8):
                items.append((t, h, kts))

        def front(i_):
            t, h, kts = items[i_]
            cols = slice(t * 128, (t + 1) * 128)
            nk = len(kts)
            bs = i_ % NBS
            pr, base, kvh = h // 2, 64 * (h % 2), h // 4
            for i, (kt, mk) in enumerate(kts):
                f.op("pe", lambda e, i=i, kt=kt: e.matmul(ps_s[bs][:, i, :], kT2[base:base + 64, kvh, kt * 128:(kt + 1) * 128], qT[base:base + 64, pr, cols], start=True, stop=True),
                     reads=[R_kT[kt], R_qT[t]], writes=[R_pss[bs]], acc=(i > 0))
            f.op("act", lambda e: e.activation(out=pT[bs][:, 0:nk, :], in_=ps_s[bs][:, 0:nk, :], func=AF.Exp, scale=0.125), reads=[R_pss[bs]], writes=[R_pT[bs]])
            for i, (kt, mk) in enumerate(kts):
                if mk is not None:
                    f.op("dve", lambda e, i=i, mk=mk: e.tensor_tensor(pT[bs][:, i, :], pT[bs][:, i, :], maskb[:, mk, :], ALU.mult), reads=[R_pT[bs], R_mask], writes=[R_pT[bs]])

        def back(i_):
            t, h, kts = items[i_]
            cols = slice(t * 128, (t + 1) * 128)
            nk = len(kts)
            bs = i_ % NBS
            bi = i_ % 2
            pr, base, kvh = h // 2, 64 * (h % 2), h // 4
            voff = (64 if h % 2 == 0 else 0) + 128 * kvh
            for i, (kt, mk) in enumerate(kts):
                f.op("pe", lambda e, i=i, kt=kt: e.matmul(ps_o[bi][:, 0:128], vaug[:, kt, voff:voff + 128], pT[bs][:, i, :], start=(i == 0), stop=(i == nk - 1)),
                     reads=[R_v[kt], R_pT[bs]], writes=[R_pso[bi]], acc=(i > 0))
            nb, db = (0, 64) if h % 2 == 0 else (64, 0)
            f.op("dve", lambda e: e.tensor_scalar(out=dtmp[bi][nb:nb + 64, :], in0=ps_o[bi][db:db + 64, 0:128], scalar1=esink[db:db + 64, h:h + 1], scalar2=None, op0=ALU.add),
                 reads=[R_pso[bi], R_es], writes=[R_dt[bi]])
            f.op("dve", lambda e: e.reciprocal(dtmp[bi][nb:nb + 64, :], dtmp[bi][nb:nb + 64, :]), reads=[R_dt[bi]], writes=[R_dt[bi]])
            f.op("dve", lambda e: e.tensor_tensor(oT[nb:nb + 64, pr, cols], ps_o[bi][nb:nb + 64, 0:128], dtmp[bi][nb:nb + 64, :], ALU.mult), reads=[R_pso[bi], R_dt[bi]], writes=[R_oT[t]])
        front(0)
        for i_ in range(len(items)):
            if i_ + 1 < len(items):
                front(i_ + 1)
            back(i_)
        S.close()

    def out_phase(layer, w_out_d, mix_loader, tiles):
        S = Scope(nc)
        load_ln(layer * 2 + 0)
        wo = S.sb("wo", [128, 8, D], BF16); R_wo = Res()
        f.dma("pool", wo[:], w_out_d.rearrange("(kc p) n -> p kc n", p=128), writes=[R_wo])
        xt = [S.sb("xt%d" % k, [128, D]) for k in range(2)]; R_xt = RL(2)
        ot = [S.sb("ot%d" % k, [128, D]) for k in range(2)]; R_ot = RL(2)
        tmp = S.sb("tmp", [128, D]); R_tmp = Res()
        small = S.sb("small", [128, 16]); R_small = Res()
        ps_o2 = [S.ps("ps_o2%d" % k, [128, D]) for k in range(2)]; R_ps = RL(2)
        for n, t in enumerate(tiles):
            b = n % 2
            src, rs = src_tile(layer, t)
            f.dma("sp", xt[b][:], src, reads=rs, writes=[R_xt[b]])
            mixT, R_mix = mix_loader(t, b)
            for half in range(2):
                for kc in range(8):
                    f.op("pe", lambda e, kc=kc, half=half: e.matmul(ps_o2[b][:, half * 512:(half + 1) * 512], mixT[kc], wo[:, kc, half * 512:(half + 1) * 512], start=(kc == 0), stop=(kc == 7)),
                         reads=[R_wo] + R_mix, writes=[R_ps[b]], acc=(half + kc > 0))
            resid_ln(S, xt[b], R_xt[b], ps_o2[b], R_ps[b], (1 if t < 2 else 0), layer * 2 + 0, ot[b], R_ot[b], tmp, R_tmp, small, R_small)
            f.dma("act", XR[t * 128:(t + 1) * 128, :], ot[b][:], reads=[R_ot[b]], writes=[R_XR[t]])
        S.close()

    def ffn_phase(layer, tiles_all, final):
        P = Scope(nc)
        rw = P.sb("rw", [128, 8, 32]); R_rw = Res()
        f.dma("sp", rw[:], router_w.rearrange("(kc p) n -> p kc n", p=128), writes=[R_rw])
        rbias = P.sb("rbias", [128, 32]); f.dma("sp", rbias[:], router_b.partition_broadcast(128), writes=[R_rw])
        GT = 9
        load_ln(layer * 2 + 1)
        groups = [tiles_all[i:i + GT] for i in range(0, len(tiles_all), GT)]
        for grp in groups:
            S = Scope(nc)
            ng = len(grp)
            hT = S.sb("hTg", [128, 8, GT * 128], BF16); R_hT = RL(ng, "hTg")
            comb = S.sb("comb", [128, GT, 32]); R_comb = RL(ng, "comb")
            yacc = S.sb("yaccg", [128, GT, D]); R_y = RL(ng, "yg")
            A = Scope(nc)
            xt = [A.sb("xt%d" % k, [128, D]) for k in range(2)]; R_xt = RL(2)
            h32 = A.sb("h32", [128, D]); R_h32 = Res()
            h32T = A.sb("h32T", [128, 8, 128]); R_h32T = Res()
            ps_tp = A.ps("ps_tp", [128, 8, 128]); R_pstp = Res(x=True)
            ps_r = A.ps("ps_r", [128, 512]); R_psr = Res()
            sc = A.sb("sc", [128, 32]); sel = A.sb("sel", [128, 32]); R_sc = Res()
            pa = A.sb("pa", [128, 8, 6]); pm = A.sb("pm", [128, 8, 6]); gs = A.sb("gs", [128, 8]); thr = A.sb("thr", [128, 8])
            gm = A.sb("gm", [128, 2]); mg = A.sb("mg", [128, 8]); sm = A.sb("sm", [128, 8, 4])
            for j, t in enumerate(grp):
                b = j % 2
                f.dma("sp", xt[b][:], XR[t * 128:(t + 1) * 128, :], reads=[R_XR[t]], writes=[R_xt[b]])
                which = 1 if t < 2 else 0
                mod_transpose(xt[b], R_xt[b], which, h32, R_h32, ps_tp, R_pstp, hT[:, :, j * 128:(j + 1) * 128], R_hT[j], h32T, R_h32T)
                for kc in range(8):
                    f.op("pe", lambda e, kc=kc: e.matmul(ps_r[:, 0:32], h32T[:, kc, :], rw[:, kc, :], start=(kc == 0), stop=(kc == 7)), reads=[R_h32T, R_rw], writes=[R_psr], acc=(kc > 0))
                R1 = R_sc
                f.op("act", lambda e: e.activation(out=sc[:], in_=ps_r[:, 0:32], func=AF.Sigmoid), reads=[R_psr], writes=[R1])
                f.op("dve", lambda e: e.tensor_tensor(sel[:], sc[:], rbias[:], ALU.add), reads=[R1, R_rw], writes=[R1])
                s3 = sel[:].rearrange("p (g e) -> p g e", e=4)
                pairs = [(0, 1), (0, 2), (0, 3), (1, 2), (1, 3), (2, 3)]
                for k, (a_, b_) in enumerate(pairs):
                    f.op("dve", lambda e, k=k, a_=a_, b_=b_: e.tensor_tensor(pa[:, :, k], s3[:, :, a_], s3[:, :, b_], ALU.add), reads=[R1], writes=[R1])
                    f.op("dve", lambda e, k=k, a_=a_, b_=b_: e.tensor_tensor(pm[:, :, k], s3[:, :, a_], s3[:, :, b_], ALU.min), reads=[R1], writes=[R1])
                f.op("dve", lambda e: e.tensor_reduce(out=gs[:], in_=pa[:], axis=AX.X, op=ALU.max), reads=[R1], writes=[R1])
                f.op("dve", lambda e: e.tensor_reduce(out=thr[:], in_=pm[:], axis=AX.X, op=ALU.max), reads=[R1], writes=[R1])
                f.op("dve", lambda e: e.tensor_reduce(out=gm[:, 0:1], in_=gs[:], axis=AX.X, op=ALU.max), reads=[R1], writes=[R1])
                f.op("dve", lambda e: e.tensor_scalar(out=mg[:], in0=gs[:], scalar1=gm[:, 0:1], scalar2=None, op0=ALU.is_ge), reads=[R1], writes=[R1])
                f.op("dve", lambda e: e.tensor_tensor(sm[:], s3, thr[:].unsqueeze(2).broadcast_to([128, 8, 4]), ALU.is_ge), reads=[R1], writes=[R1])
                f.op("dve", lambda e: e.tensor_tensor(sm[:], sm[:], mg[:].unsqueeze(2).broadcast_to([128, 8, 4]), ALU.mult), reads=[R1], writes=[R1])
                cj = comb[:, j, :]
                f.op("dve", lambda e: e.tensor_tensor(cj, sm[:].rearrange("p g e -> p (g e)"), sc[:], ALU.mult), reads=[R1], writes=[R_comb[j]])
                f.op("dve", lambda e: e.tensor_reduce(out=gm[:, 1:2], in_=cj, axis=AX.X, op=ALU.add), reads=[R_comb[j], R1], writes=[R1])
                f.op("dve", lambda e: e.reciprocal(gm[:, 1:2], gm[:, 1:2]), reads=[R1], writes=[R1])
                f.op("dve", lambda e: e.tensor_scalar(out=cj, in0=cj, scalar1=gm[:, 1:2], scalar2=None, op0=ALU.mult), reads=[R1, R_comb[j]], writes=[R_comb[j]])
            A.close()
            B = Scope(nc)
            wg = [B.sb("wg%d" % k, [128, 8, 512], BF16) for k in range(2)]
            wu = [B.sb("wu%d" % k, [128, 8, 512], BF16) for k in range(2)]
            wd = [B.sb("wd%d" % k, [128, 4, D], BF16) for k in range(2)]
            R_w = RL(2, "w")
            psg = [B.ps("psg%d" % k, [128, 512]) for k in range(2)]; R_psg = RL(2)
            psu = [B.ps("psu%d" % k, [128, 512]) for k in range(2)]; R_psu = RL(2)
            psd = [B.ps("psd%d" % k, [128, D]) for k in range(2)]; R_psd = RL(2)
            sg = [B.sb("sg%d" % k, [128, 512]) for k in range(2)]; R_sg = RL(2)
            hid = [B.sb("hid%d" % k, [128, 4, 512], BF16) for k in range(2)]; R_hid = RL(2)
            ntok = ng * 128
            blocks = [(c0, min(512, ntok - c0)) for c0 in range(0, ntok, 512)]
            nfc = 0; nblk = 0; nd = 0
            for ex in range(32):
                wb = ex % 2
                f.dma("pool", wg[wb][:], w_gate[layer, ex].rearrange("(kc p) n -> p kc n", p=128), writes=[R_w[wb]])
                f.dma("pool", wu[wb][:], w_up[layer, ex].rearrange("(kc p) n -> p kc n", p=128), writes=[R_w[wb]])
                f.dma("pool", wd[wb][:], w_down[layer, ex].rearrange("(kc p) n -> p kc n", p=128), writes=[R_w[wb]])
                for (c0, n) in blocks:
                    hb_ = nblk % 2; nblk += 1
                    tl = R_hT[c0 // 128:(c0 + n) // 128]
                    for fc in range(4):
                        pb = nfc % 2; nfc += 1
                        for kc in range(8):
                            f.op("pe", lambda e, kc=kc, fc=fc: e.matmul(psg[pb][:, 0:n], wg[wb][:, kc, fc * 128:(fc + 1) * 128], hT[:, kc, c0:c0 + n], start=(kc == 0), stop=(kc == 7)),
                                 reads=[R_w[wb]] + tl, writes=[R_psg[pb]], acc=(kc > 0))
                        for kc in range(8):
                            f.op("pe", lambda e, kc=kc, fc=fc: e.matmul(psu[pb][:, 0:n], wu[wb][:, kc, fc * 128:(fc + 1) * 128], hT[:, kc, c0:c0 + n], start=(kc == 0), stop=(kc == 7)),
                                 reads=[R_w[wb]] + tl, writes=[R_psu[pb]], acc=(kc > 0))
                        f.op("act", lambda e: e.activation(out=sg[pb][:, 0:n], in_=psg[pb][:, 0:n], func=AF.Silu), reads=[R_psg[pb]], writes=[R_sg[pb]])
                        f.op("dve", lambda e, fc=fc: e.tensor_tensor(hid[hb_][:, fc, 0:n], sg[pb][:, 0:n], psu[pb][:, 0:n], ALU.mult), reads=[R_sg[pb], R_psu[pb]], writes=[R_hid[hb_]])
                    for tt in range(n // 128):
                        j = c0 // 128 + tt
                        db = nd % 2; nd += 1
                        for half in range(2):
                            for fc in range(4):
                                f.op("pe", lambda e, fc=fc, half=half: e.matmul(psd[db][:, half * 512:(half + 1) * 512], hid[hb_][:, fc, tt * 128:(tt + 1) * 128], wd[wb][:, fc, half * 512:(half + 1) * 512], start=(fc == 0), stop=(fc == 3)),
                                     reads=[R_w[wb], R_hid[hb_]], writes=[R_psd[db]], acc=(half + fc > 0))
                        cw = comb[:, j, ex:ex + 1]
                        if ex == 0:
                            f.op("dve", lambda e: e.tensor_scalar(out=yacc[:, j, :], in0=psd[db][:], scalar1=cw, scalar2=None, op0=ALU.mult), reads=[R_psd[db], R_comb[j]], writes=[R_y[j]])
                        else:
                            f.op("dve", lambda e: e.scalar_tensor_tensor(out=yacc[:, j, :], in0=psd[db][:], scalar=cw, in1=yacc[:, j, :], op0=ALU.mult, op1=ALU.add), reads=[R_psd[db], R_comb[j], R_y[j]], writes=[R_y[j]])
            B.close()
            C = Scope(nc)
            xt = [C.sb("xt%d" % k, [128, D]) for k in range(2)]; R_xt = RL(2)
            ot = [C.sb("ot%d" % k, [128, D]) for k in range(2)]; R_ot = RL(2)
            tmp = C.sb("tmp", [128, D]); R_tmp = Res()
            small = C.sb("small", [128, 16]); R_small = Res()
            for j, t in enumerate(grp):
                b = j % 2
                f.dma("sp", xt[b][:], XR[t * 128:(t + 1) * 128, :], reads=[R_XR[t]], writes=[R_xt[b]])
                yj = yacc[:, j, :]

                class _V:
                    def __init__(self, ap): self.ap = ap
                    def __getitem__(self, k): return self.ap
                resid_ln(C, xt[b], R_xt[b], _V(yj), R_y[j], (1 if t < 2 else 0), layer * 2 + 1, ot[b], R_ot[b], tmp, R_tmp, small, R_small)
                if final:
                    f.dma("act", out_d[(t - 2) * 128:(t - 1) * 128, :], ot[b][:], reads=[R_ot[b]], writes=[R_out])
                else:
                    f.dma("act", XR[t * 128:(t + 1) * 128, :], ot[b][:], reads=[R_ot[b]], writes=[R_XR[t]])
            C.close()
            S.close()
        P.close()


    def ffn_sparse(layer, tiles_all, final):
        IOA = bass.IndirectOffsetOnAxis
        ng = len(tiles_all)
        M = ng * 32
        P = Scope(nc)
        rw = P.sb("rw", [128, 8, 32]); R_rw = Res()
        f.dma("sp", rw[:], router_w.rearrange("(kc p) n -> p kc n", p=128), writes=[R_rw])
        rbias = P.sb("rbias", [128, 32]); f.dma("sp", rbias[:], router_b.partition_broadcast(128), writes=[R_rw])
        jt = P.sb("jt2", [128, 128]); f.dma("sp", jt[:], k_jidx, writes=[R_rw])
        pc = P.sb("pc", [128, 4]); f.dma("sp", pc[:], k_pc, writes=[R_rw])
        load_ln(layer * 2 + 1)
        comb = P.sb("comb", [128, ng, 32]); R_comb = RL(ng, "comb")
        posA_i = P.sb("posA_i", [128, ng], I32); posB_i = P.sb("posB_i", [128, ng], I32)
        wA = P.sb("wA", [128, ng]); wB = P.sb("wB", [128, ng])
        NSO = NS - 32
        idxw = P.sb("idxw", [128, NSO, 4], I32)
        R_rt = Res("route")
        R_XsW = RL(ng, "xsw")
        R_Ys = RL(NS, "ys")
        HB = Scope(nc)
        hb_all = HB.sb("hb_all", [128, ng, D], BF16); R_hb = RL(ng, "hb")
        A = Scope(nc)
        xt = [A.sb("xt%d" % k, [128, D]) for k in range(2)]; R_xt = RL(2)
        h32 = [A.sb("h32%d" % k, [128, D]) for k in range(2)]; R_h32 = RL(2)
        h32T = A.sb("h32T", [128, 8, 128]); R_h32T = Res()
        ps_tp = [A.ps("ps_tp%d" % k, [128, 8, 128]) for k in range(2)]; R_pstp = RL(2)
        ps_r = A.ps("ps_r", [128, 512]); R_psr = Res()
        R_sc = Res()
        sc_all = A.sb("sc_all", [128, ng, 32]); sel_all = A.sb("sel_all", [128, ng, 32])
        pa_all = A.sb("pa_all", [128, ng, 8, 6]); pm_all = A.sb("pm_all", [128, ng, 8, 6])
        gs_all = A.sb("gs_all", [128, ng, 8]); thr_all = A.sb("thr_all", [128, ng, 8]); mg_all = A.sb("mg_all", [128, ng, 8])
        gm_all = A.sb("gm_all", [128, ng]); sm_all = A.sb("sm_all", [128, ng, 8, 4])
        for j, t in enumerate(tiles_all):
            b = j % 2
            f.dma("sp", xt[b][:], XR[t * 128:(t + 1) * 128, :], reads=[R_XR[t]], writes=[R_xt[b]])
            which = 1 if t < 2 else 0
            f.op("dve", lambda e: e.tensor_tensor(h32[b][:], xt[b][:], mod[:, which, 1, :], ALU.mult), reads=[R_xt[b], R_mod], writes=[R_h32[b]])
            f.op("dve", lambda e: e.tensor_tensor(h32[b][:], h32[b][:], mod[:, which, 0, :], ALU.add), reads=[R_h32[b], R_mod], writes=[R_h32[b]])
            for kc in range(8):
                f.op("pe", lambda e, kc=kc: e.transpose(ps_tp[b][:, kc, :], h32[b][:, kc * 128:(kc + 1) * 128], ident[:]),
                     reads=[R_h32[b], R_ident], writes=[R_pstp[b]], acc=(kc > 0))
            f.op("dve", lambda e: e.tensor_copy(h32T[:], ps_tp[b][:]), reads=[R_pstp[b]], writes=[R_h32T])
            f.op("act", lambda e: e.activation(out=hb_all[:, j, :].rearrange("p (c j q) -> p c j q", c=4, j=2),
                                               in_=h32[b][:].rearrange("p (c q j) -> p c j q", c=4, j=2), func=AF.Identity),
                 reads=[R_h32[b]], writes=[R_hb[j]])
            for kc in range(8):
                f.op("pe", lambda e, kc=kc: e.matmul(ps_r[:, 0:32], h32T[:, kc, :], rw[:, kc, :], start=(kc == 0), stop=(kc == 7)), reads=[R_h32T, R_rw], writes=[R_psr], acc=(kc > 0))
            f.op("act", lambda e: e.activation(out=sc_all[:, j, :], in_=ps_r[:, 0:32], func=AF.Sigmoid), reads=[R_psr], writes=[R_sc])
        R1 = R_sc
        s4 = sel_all[:].rearrange("p t (g e) -> p t g e", e=4)
        f.op("dve", lambda e: e.tensor_tensor(sel_all[:], sc_all[:], rbias[:].unsqueeze(1).broadcast_to([128, ng, 32]), ALU.add), reads=[R1, R_rw], writes=[R1])
        pairs = [(0, 1), (0, 2), (0, 3), (1, 2), (1, 3), (2, 3)]
        for k, (a_, b_) in enumerate(pairs):
            f.op("dve", lambda e, k=k, a_=a_, b_=b_: e.tensor_tensor(pa_all[:, :, :, k], s4[:, :, :, a_], s4[:, :, :, b_], ALU.add), reads=[R1], writes=[R1])
            f.op("dve", lambda e, k=k, a_=a_, b_=b_: e.tensor_tensor(pm_all[:, :, :, k], s4[:, :, :, a_], s4[:, :, :, b_], ALU.min), reads=[R1], writes=[R1])
        f.op("dve", lambda e: e.tensor_reduce(out=gs_all[:], in_=pa_all[:], axis=AX.X, op=ALU.max), reads=[R1], writes=[R1])
        f.op("dve", lambda e: e.tensor_reduce(out=thr_all[:], in_=pm_all[:], axis=AX.X, op=ALU.max), reads=[R1], writes=[R1])
        f.op("dve", lambda e: e.tensor_reduce(out=gm_all[:], in_=gs_all[:], axis=AX.X, op=ALU.max), reads=[R1], writes=[R1])
        f.op("dve", lambda e: e.tensor_tensor(mg_all[:], gs_all[:], gm_all[:].unsqueeze(2).broadcast_to([128, ng, 8]), ALU.is_ge), reads=[R1], writes=[R1])
        f.op("dve", lambda e: e.tensor_tensor(sm_all[:], s4, thr_all[:].unsqueeze(3).broadcast_to([128, ng, 8, 4]), ALU.is_ge), reads=[R1], writes=[R1])
        f.op("dve", lambda e: e.tensor_tensor(sm_all[:], sm_all[:], mg_all[:].unsqueeze(3).broadcast_to([128, ng, 8, 4]), ALU.mult), reads=[R1], writes=[R1])
        f.op("dve", lambda e: e.tensor_tensor(comb[:], sm_all[:].rearrange("p t g e -> p t (g e)"), sc_all[:], ALU.mult), reads=[R1], writes=R_comb)
        f.op("dve", lambda e: e.tensor_reduce(out=gm_all[:], in_=comb[:], axis=AX.X, op=ALU.add), reads=R_comb + [R1], writes=[R1])
        f.op("dve", lambda e: e.reciprocal(gm_all[:], gm_all[:]), reads=[R1], writes=[R1])
        f.op("dve", lambda e: e.tensor_tensor(comb[:], comb[:], gm_all[:].unsqueeze(2).broadcast_to([128, ng, 32]), ALU.mult), reads=[R1] + R_comb, writes=R_comb)
        A.close()
        Bq = Scope(nc)
        m_ = Bq.sb("m_", [128, ng, 32]); mb16 = Bq.sb("mb16", [128, ng, 32], BF16)
        rank = Bq.sb("rank", [128, ng, 32]); tot = Bq.sb("tot", [128, ng, 32]); base = Bq.sb("base", [128, ng, 32])
        me = Bq.sb("me", [128, ng, 32]); Bm = Bq.sb("Bm", [128, ng, 32]); Am = Bq.sb("Am", [128, ng, 32]); tmpq = Bq.sb("tmpq", [128, ng, 32])
        ones16 = Bq.sb("ones16", [128, 128], BF16)
        cnt = Bq.sb("cnt", [128, 32]); cmp17 = Bq.sb("cmp17", [128, 32, 18]); thr18 = Bq.sb("thr18", [128, 18])
        tlf = Bq.sb("tlf", [128, 32]); sinc = Bq.sb("sinc", [128, 32]); so512 = Bq.sb("so512", [128, 32]); c1e = Bq.sb("c1e", [128, 32])
        mx = Bq.sb("mx", [128, ng]); pAf = Bq.sb("pAf", [128, ng]); pBf = Bq.sb("pBf", [128, ng])
        cmpj = Bq.sb("cmpj", [128, NSO, 32]); eidf = Bq.sb("eidf", [128, NSO]); idxf = Bq.sb("idxf", [128, NSO, 4])
        ps_rk = Bq.ps("ps_rk", [128, 3, 512]); ps_tt = Bq.ps("ps_tt", [128, 3, 512])
        RB = [R_rt]

        def fl(ap3):
            return ap3.rearrange("p a b -> p (a b)")
        f.op("dve", lambda e: e.tensor_scalar(out=fl(m_[:]), in0=fl(comb[:]), scalar1=0.0, scalar2=None, op0=ALU.is_gt), reads=R_comb, writes=RB)
        f.op("dve", lambda e: e.tensor_copy(fl(mb16[:]), fl(m_[:])), reads=RB, writes=RB)
        f.op("pool", lambda e: e.memset(ones16[:], 1.0), reads=RB, writes=RB)
        chunks = [(n0, min(M, n0 + 512)) for n0 in range(0, M, 512)]
        for ch, (n0, n1) in enumerate(chunks):
            f.op("pe", lambda e, ch=ch, n0=n0, n1=n1: e.matmul(ps_rk[:, ch, 0:n1 - n0], maskb[:, 2, :], fl(mb16[:])[:, n0:n1], start=True, stop=True), reads=RB + [R_mask], writes=RB)
            f.op("pe", lambda e, ch=ch, n0=n0, n1=n1: e.matmul(ps_tt[:, ch, 0:n1 - n0], ones16[:], fl(mb16[:])[:, n0:n1], start=True, stop=True), reads=RB, writes=RB)
        for ch, (n0, n1) in enumerate(chunks):
            f.op("dve", lambda e, ch=ch, n0=n0, n1=n1: e.tensor_copy(fl(rank[:])[:, n0:n1], ps_rk[:, ch, 0:n1 - n0]), reads=RB, writes=RB)
            f.op("dve", lambda e, ch=ch, n0=n0, n1=n1: e.tensor_copy(fl(tot[:])[:, n0:n1], ps_tt[:, ch, 0:n1 - n0]), reads=RB, writes=RB)
        f.op("dve", lambda e: e.memset(base[:, 0, :], 0.0), reads=RB, writes=RB)
        for t_ in range(1, ng):
            f.op("dve", lambda e, t_=t_: e.tensor_tensor(base[:, t_, :], base[:, t_ - 1, :], tot[:, t_ - 1, :], ALU.add), reads=RB, writes=RB)
        f.op("dve", lambda e: e.tensor_tensor(cnt[:], base[:, ng - 1, :], tot[:, ng - 1, :], ALU.add), reads=RB, writes=RB)
        f.op("dve", lambda e: e.tensor_scalar(out=thr18[:], in0=jt[:, 0:18], scalar1=512.0, scalar2=None, op0=ALU.mult), reads=RB + [R_rw], writes=RB)
        f.op("dve", lambda e: e.tensor_tensor(cmp17[:], cnt[:].unsqueeze(2).broadcast_to([128, 32, 18]), thr18[:].unsqueeze(1).broadcast_to([128, 32, 18]), ALU.is_gt), reads=RB, writes=RB)
        f.op("dve", lambda e: e.tensor_reduce(out=tlf[:], in_=cmp17[:], axis=AX.X, op=ALU.add), reads=RB, writes=RB)
        f.op("dve", lambda e: e.tensor_scalar(out=tlf[:], in0=tlf[:], scalar1=-1.0, scalar2=0.0, op0=ALU.add, op1=ALU.max), reads=RB, writes=RB)
        f.op("dve", lambda e: e.tensor_copy(sinc[:], tlf[:]), reads=RB, writes=RB)
        for e_ in range(1, 32):
            f.op("dve", lambda e, e_=e_: e.tensor_tensor(sinc[:, e_:e_ + 1], sinc[:, e_ - 1:e_], tlf[:, e_:e_ + 1], ALU.add), reads=RB, writes=RB)
        f.op("dve", lambda e: e.tensor_tensor(so512[:], sinc[:], tlf[:], ALU.subtract), reads=RB, writes=RB)
        f.op("dve", lambda e: e.tensor_scalar(out=so512[:], in0=so512[:], scalar1=512.0, scalar2=15872.0, op0=ALU.mult, op1=ALU.add), reads=RB, writes=RB)
        f.op("dve", lambda e: e.tensor_scalar(out=c1e[:], in0=jt[:, 0:32], scalar1=512.0, scalar2=None, op0=ALU.mult), reads=RB + [R_rw], writes=RB)
        f.op("dve", lambda e: e.tensor_tensor(so512[:], so512[:], c1e[:], ALU.subtract), reads=RB, writes=RB)
        f.op("dve", lambda e: e.tensor_tensor(fl(rank[:]), fl(rank[:]), fl(base[:]), ALU.add), reads=RB, writes=RB)
        f.op("dve", lambda e: e.tensor_scalar(out=fl(tmpq[:]), in0=fl(rank[:]), scalar1=512.0, scalar2=None, op0=ALU.is_ge), reads=RB, writes=RB)
        f.op("dve", lambda e: e.tensor_tensor(tmpq[:], tmpq[:], so512[:].unsqueeze(1).broadcast_to([128, ng, 32]), ALU.mult), reads=RB, writes=RB)
        f.op("dve", lambda e: e.tensor_tensor(rank[:], rank[:], c1e[:].unsqueeze(1).broadcast_to([128, ng, 32]), ALU.add), reads=RB, writes=RB)
        f.op("dve", lambda e: e.tensor_tensor(fl(rank[:]), fl(rank[:]), fl(tmpq[:]), ALU.add), reads=RB, writes=RB)
        f.op("dve", lambda e: e.tensor_tensor(me[:], m_[:], jt[:, 1:33].unsqueeze(1).broadcast_to([128, ng, 32]), ALU.mult), reads=RB, writes=RB)
        f.op("dve", lambda e: e.tensor_reduce(out=mx[:], in_=me[:], axis=AX.X, op=ALU.max), reads=RB, writes=RB)
        f.op("dve", lambda e: e.tensor_tensor(Bm[:], me[:], mx[:].unsqueeze(2).broadcast_to([128, ng, 32]), ALU.is_equal), reads=RB, writes=RB)
        f.op("dve", lambda e: e.tensor_tensor(fl(Am[:]), fl(m_[:]), fl(Bm[:]), ALU.subtract), reads=RB, writes=RB)
        for (msk, pf, wf) in ((Am, pAf, wA), (Bm, pBf, wB)):
            f.op("dve", lambda e, msk=msk: e.tensor_tensor(fl(tmpq[:]), fl(msk[:]), fl(rank[:]), ALU.mult), reads=RB, writes=RB)
            f.op("dve", lambda e, pf=pf: e.tensor_reduce(out=pf[:], in_=tmpq[:], axis=AX.X, op=ALU.add), reads=RB, writes=RB)
            f.op("dve", lambda e, msk=msk: e.tensor_tensor(fl(tmpq[:]), fl(msk[:]), fl(comb[:]), ALU.mult), reads=RB + R_comb, writes=RB)
            f.op("dve", lambda e, wf=wf: e.tensor_reduce(out=wf[:], in_=tmpq[:], axis=AX.X, op=ALU.add), reads=RB, writes=RB)
        f.op("dve", lambda e: e.tensor_copy(posA_i[:], pAf[:]), reads=RB, writes=RB)
        f.op("dve", lambda e: e.tensor_copy(posB_i[:], pBf[:]), reads=RB, writes=RB)
        f.op("dve", lambda e: e.tensor_tensor(cmpj[:], sinc[:].unsqueeze(1).broadcast_to([128, NSO, 32]), jt[:, 0:NSO].unsqueeze(2).broadcast_to([128, NSO, 32]), ALU.is_le), reads=RB, writes=RB)
        f.op("dve", lambda e: e.tensor_reduce(out=eidf[:], in_=cmpj[:], axis=AX.X, op=ALU.add), reads=RB, writes=RB)
        f.op("dve", lambda e: e.tensor_scalar(out=eidf[:], in0=eidf[:], scalar1=32.0, scalar2=512.0, op0=ALU.min, op1=ALU.mult), reads=RB, writes=RB)
        f.op("dve", lambda e: e.tensor_scalar(out=eidf[:], in0=eidf[:], scalar1=float(layer * 16384), scalar2=None, op0=ALU.add), reads=RB, writes=RB)
        f.op("dve", lambda e: e.tensor_tensor(idxf[:], eidf[:].unsqueeze(2).broadcast_to([128, NSO, 4]), pc[:].unsqueeze(1).broadcast_to([128, NSO, 4]), ALU.add), reads=RB, writes=RB)
        f.op("dve", lambda e: e.tensor_copy(idxw[:], idxf[:]), reads=RB, writes=RB)
        for j in range(ng):
            for pi_ in (posA_i, posB_i):
                f._dma_common("pool", lambda e, j=j, pi_=pi_: e.indirect_dma_start(out=XS, out_offset=IOA(ap=pi_[:, j:j + 1], axis=0), in_=hb_all[:, j, :], in_offset=None),
                              [R_hb[j]] + RB + R_XsZ, [R_XsW[j]])
        Bq.close()
        HB.close()
        Sd = Scope(nc)
        wg = [Sd.sb("wg%d" % k, [128, 4, 2, 512], BF16) for k in range(2)]
        wu = [Sd.sb("wu%d" % k, [128, 4, 2, 512], BF16) for k in range(2)]
        wd = [Sd.sb("wd%d" % k, [128, 4, D], BF16) for k in range(2)]
        R_w = RL(2, "w"); R_wd = RL(2, "wd")
        xs = [[Sd.sb("xs%d_%d" % (a_, tt), [128, D], BF16) for tt in range(4)] for a_ in range(2)]
        R_xs = [RL(4, "xs%d" % a_) for a_ in range(2)]

        def xs_load(jn):
            for tt in range(4):
                r0 = (jn * 4 + tt) * 128
                f.dma("sp", xs[jn % 2][tt][:], XS[r0:r0 + 128, :], reads=R_XsW, writes=[R_xs[jn % 2][tt]])
        xT = [Sd.sb("xT%d" % k, [128, 8, 512], BF16) for k in range(2)]; R_xT = RL(2)
        ps_t = [Sd.ps("ps_t%d" % k, [128, 8, 128], BF16) for k in range(2)]; R_pst = RL(2)
        psg = [Sd.ps("psg%d" % k, [128, 512]) for k in range(2)]; R_psg = RL(2)
        psu = [Sd.ps("psu%d" % k, [128, 512]) for k in range(2)]; R_psu = RL(2)
        psd = [Sd.ps("psd%d" % k, [128, 512]) for k in range(2)]; R_psd = RL(2)
        sg = [Sd.sb("sg%d" % k, [128, 512]) for k in range(2)]; R_sg = RL(2)
        hid = [Sd.sb("hid%d" % k, [128, 4, 512], BF16) for k in range(2)]; R_hid = RL(2)
        ysb = [Sd.sb("ysb%d" % k, [128, D]) for k in range(2)]; R_ysb = [RL(2, "ysb%d" % k) for k in range(2)]
        nfc = 0; nx = 0; ny = 0
        bc_reg = nc.gpsimd.alloc_register("bc%d" % layer)
        nc.gpsimd.reg_mov(bc_reg, 16383 + layer * 16384)
        stg_g = Sd.sb("stg_g", [128, 4, 2, 512]); stg_u = Sd.sb("stg_u", [128, 4, 2, 512])
        R_sg_ = Res("stg_g"); R_su_ = Res("stg_u")

        def w_load(ex):
            f.dma("sp", stg_g[:], w_gate[layer, ex].rearrange("(c q j) n -> q c j n", c=4, j=2), writes=[R_sg_])
            f.dma("sp", stg_u[:], w_up[layer, ex].rearrange("(c q j) n -> q c j n", c=4, j=2), writes=[R_su_])
            f.dma("pool", wd[ex % 2][:], w_down[layer, ex].rearrange("(c p) n -> p c n", p=128), writes=[R_wd[ex % 2]])

        def w_cast(ex):
            wb_ = ex % 2
            f.op("act", lambda e: e.activation(out=wg[wb_][:], in_=stg_g[:], func=AF.Identity), reads=[R_sg_], writes=[R_w[wb_]])
            f.op("dve", lambda e: e.tensor_copy(wu[wb_][:], stg_u[:]), reads=[R_su_], writes=[R_w[wb_]])
        xs_load(0)
        w_load(0)
        w_cast(0)
        ns_l = 32 + (ng * 128 * 2) // 512
        for j in range(ns_l):
            wb = j % 2
            if j + 1 < ns_l:
                xs_load(j + 1)
            if j + 1 < 32:
                w_load(j + 1)
            if j >= 32:
                jo = j - 32
                for c in range(4):
                    for (wt_, rows_) in ((wg, wg_rows), (wu, wu_rows)):
                        f._dma_common("pool", lambda e, c=c, wt_=wt_, rows_=rows_: e.indirect_dma_start(out=wt_[wb][:, c, :, :].rearrange("p a n -> p (a n)"), out_offset=None, in_=rows_[layer],
                                                                                                   in_offset=IOA(ap=idxw[:, jo, c:c + 1], axis=0), bounds_check=bc_reg, oob_is_err=False),
                                      RB, [R_w[wb]])
                for c in range(4):
                    f._dma_common("pool", lambda e, c=c: e.indirect_dma_start(out=wd[wb][:, c, :], out_offset=None, in_=wd_rows[layer], in_offset=IOA(ap=idxw[:, jo, c:c + 1], axis=0), bounds_check=bc_reg, oob_is_err=False),
                                  RB, [R_wd[wb]])
            for tt in range(4):
                for kc in range(8):
                    f.op("pe", lambda e, kc=kc, tt=tt: e.transpose(ps_t[tt % 2][:, kc, :], xs[j % 2][tt][:, kc * 128:(kc + 1) * 128], identb[:]), reads=[R_xs[j % 2][tt], R_identb], writes=[R_pst[tt % 2]], acc=(kc > 0))
                f.op("dve", lambda e, tt=tt: e.tensor_copy(xT[wb][:, :, tt * 128:(tt + 1) * 128], ps_t[tt % 2][:]), reads=[R_pst[tt % 2]], writes=[R_xT[wb]])
            hb_ = j % 2
            for fc in range(4):
                pb = nfc % 2; nfc += 1
                for kc in range(8):
                    f.op("pe", lambda e, kc=kc, fc=fc: e.matmul(psg[pb][:], wg[wb][:, kc // 2, kc % 2, fc * 128:(fc + 1) * 128], xT[wb][:, kc, :], start=(kc == 0), stop=(kc == 7)),
                         reads=[R_w[wb], R_xT[wb]], writes=[R_psg[pb]], acc=(kc > 0))
                for kc in range(8):
                    f.op("pe", lambda e, kc=kc, fc=fc: e.matmul(psu[pb][:], wu[wb][:, kc // 2, kc % 2, fc * 128:(fc + 1) * 128], xT[wb][:, kc, :], start=(kc == 0), stop=(kc == 7)),
                         reads=[R_w[wb], R_xT[wb]], writes=[R_psu[pb]], acc=(kc > 0))
                f.op("act", lambda e: e.activation(out=sg[pb][:], in_=psg[pb][:], func=AF.Silu), reads=[R_psg[pb]], writes=[R_sg[pb]])
                f.op("dve", lambda e, fc=fc: e.tensor_tensor(hid[hb_][:, fc, :], sg[pb][:], psu[pb][:], ALU.mult), reads=[R_sg[pb], R_psu[pb]], writes=[R_hid[hb_]])
            for tt in range(4):
                yb_ = ny % 2; ny += 1
                for half in range(2):
                    for fc in range(4):
                        f.op("pe", lambda e, fc=fc, half=half, tt=tt: e.matmul(psd[half][:], hid[hb_][:, fc, tt * 128:(tt + 1) * 128], wd[wb][:, fc, half * 512:(half + 1) * 512], start=(fc == 0), stop=(fc == 3)),
                             reads=[R_wd[wb], R_hid[hb_]], writes=[R_psd[half]], acc=(fc > 0))
                    if half == 0:
                        f.op("dve", lambda e: e.tensor_copy(ysb[yb_][:, 0:512], psd[0][:]), reads=[R_psd[0]], writes=[R_ysb[yb_][0]])
                    else:
                        f.op("act", lambda e: e.activation(out=ysb[yb_][:, 512:1024], in_=psd[1][:], func=AF.Identity), reads=[R_psd[1]], writes=[R_ysb[yb_][1]])
                r0 = (j * 4 + tt) * 128
                f.dma("act", YS[r0:r0 + 128, :], ysb[yb_][:], reads=R_ysb[yb_], writes=[R_Ys[j]])
            if j + 1 < 32:
                w_cast(j + 1)
        Sd.close()
        nc.gpsimd.free_register(bc_reg)
        C = Scope(nc)
        xt = [C.sb("xt%d" % k, [128, D]) for k in range(2)]; R_xt = RL(2)
        ot = [C.sb("ot%d" % k, [128, D]) for k in range(2)]; R_ot = RL(2)
        ya = [C.sb("ya%d" % k, [128, D]) for k in range(2)]; R_ya = RL(2)
        yb2 = [C.sb("yb%d" % k, [128, D]) for k in range(2)]; R_yb = RL(2)
        tmp = C.sb("tmp", [128, D]); R_tmp = Res()
        small = C.sb("small", [128, 16]); R_small = Res()

        class _V:
            def __init__(self, ap): self.ap = ap
            def __getitem__(self, k): return self.ap
        for j, t in enumerate(tiles_all):
            b = j % 2
            f.dma("sp", xt[b][:], XR[t * 128:(t + 1) * 128, :], reads=[R_XR[t]], writes=[R_xt[b]])
            f._dma_common("pool", lambda e: e.indirect_dma_start(out=ya[b][:], out_offset=None, in_=YS, in_offset=IOA(ap=posA_i[:, j:j + 1], axis=0)), R_Ys + RB, [R_ya[b]])
            f._dma_common("pool", lambda e: e.indirect_dma_start(out=yb2[b][:], out_offset=None, in_=YS, in_offset=IOA(ap=posB_i[:, j:j + 1], axis=0)), R_Ys + RB, [R_yb[b]])
            f.op("dve", lambda e: e.tensor_scalar(out=ya[b][:], in0=ya[b][:], scalar1=wA[:, j:j + 1], scalar2=None, op0=ALU.mult), reads=[R_ya[b]] + RB, writes=[R_ya[b]])
            f.op("dve", lambda e: e.scalar_tensor_tensor(out=ya[b][:], in0=yb2[b][:], scalar=wB[:, j:j + 1], in1=ya[b][:], op0=ALU.mult, op1=ALU.add), reads=[R_yb[b], R_ya[b]] + RB, writes=[R_ya[b]])
            resid_ln(C, xt[b], R_xt[b], _V(ya[b][:]), R_ya[b], (1 if t < 2 else 0), layer * 2 + 1, ot[b], R_ot[b], tmp, R_tmp, small, R_small)
            if final:
                f.dma("act", out_d[(t - 2) * 128:(t - 1) * 128, :], ot[b][:], reads=[R_ot[b]], writes=[R_out])
            else:
                f.dma("act", XR[t * 128:(t + 1) * 128, :], ot[b][:], reads=[R_ot[b]], writes=[R_XR[t]])
        C.close()
        P.close()

    def layer1_mixer():
        L = Scope(nc)
        kT2 = L.sb("kT2b", [128, 4, NTOK], BF16); R_kT = RL(NT, "kT")
        vaug = L.sb("vaugb", [128, NT, 576], BF16); R_v = RL(NT, "v")
        f.op("pool", lambda e: e.memset(vaug[:], 1.0), writes=R_v)
        R_oT = RL(NT, "oT")
        R_QT = RL(NT, "QT")
        S = Scope(nc)
        win = S.sb("win1", [128, 8, 1536], BF16); R_win = Res()
        f.dma("pool", win[:], odd_w_in.rearrange("(kc p) n -> p kc n", p=128), writes=[R_win])
        gq = S.sb("gq", [128, 2, 64]); R_gq = Res()
        f.dma("sp", gq[:, 0, :], q_norm.partition_broadcast(128), writes=[R_gq])
        f.dma("sp", gq[:, 1, :], k_norm.partition_broadcast(128), writes=[R_gq])
        xt = [S.sb("xt%d" % k, [128, D]) for k in range(2)]; R_xt = RL(2)
        h32 = S.sb("h32", [128, D]); R_h32 = Res()
        hT = [S.sb("hT%d" % k, [128, 8, 128], BF16) for k in range(2)]; R_hT = RL(2)
        ps_tp = S.ps("ps_tp", [128, 8, 128]); R_pstp = Res(x=True)
        ps_q = S.ps("ps_q", [128, 1536]); R_psq = Res(x=True)
        ps_t = S.ps("ps_t", [128, 16, 128], BF16); R_pst = Res(x=True)
        qk = S.sb("qk", [128, 20, 64]); R_qk = Res()
        sq = S.sb("sq", [128, 20, 64]); ss = S.sb("ss", [128, 20]); R_ss = Res()
        ra = S.sb("ra", [128, 20, 32]); rb = S.sb("rb", [128, 20, 32]); R_ra = Res(); R_rb = Res()
        tqk = S.sb("tqk", [128, 20, 64], BF16); R_tqk = Res()
        kd = S.sb("kd", [128, 4, 2, 64], BF16); R_kd = Res()
        qts = [S.sb("qts%d" % k, [128, 8, 128], BF16) for k in range(2)]; R_qts = RL(2)
        for t in range(NT):
            b = t % 2
            f.dma("sp", xt[b][:], XR[t * 128:(t + 1) * 128, :], reads=[R_XR[t]], writes=[R_xt[b]])
            which = 1 if t < 2 else 0
            mod_transpose(xt[b], R_xt[b], which, h32, R_h32, ps_tp, R_pstp, hT[b][:], R_hT[b])
            cols = slice(t * 128, (t + 1) * 128)
            lat = t >= 2
            ranges = ((0, 512), (512, 1024), (1024, 1536)) if lat else ((1024, 1536),)
            first = True
            for (n0, n1) in ranges:
                for kc in range(8):
                    f.op("pe", lambda e, kc=kc, n0=n0, n1=n1: e.matmul(ps_q[:, n0:n1], hT[b][:, kc, :], win[:, kc, n0:n1], start=(kc == 0), stop=(kc == 7)),
                         reads=[R_win, R_hT[b]], writes=[R_psq], acc=(not first))
                    first = False
            h0 = 0 if lat else 16
            nh = 20 - h0
            pv = ps_q[:, h0 * 64:1280].rearrange("p (h d) -> p h d", d=64)
            qkv_ = qk[:, h0:20, :]
            f.op("act", lambda e: e.activation(out=sq[:, h0:20, :], in_=pv, func=AF.Square), reads=[R_psq], writes=[R_ss])
            f.op("dve", lambda e: e.tensor_reduce(out=ss[:, h0:20], in_=sq[:, h0:20, :], axis=AX.X, op=ALU.add), reads=[R_ss], writes=[R_ss])
            f.op("dve", lambda e: e.tensor_scalar(out=ss[:, h0:20], in0=ss[:, h0:20], scalar1=1.0 / 64.0, scalar2=RMS_EPS, op0=ALU.mult, op1=ALU.add), reads=[R_ss], writes=[R_ss])
            f.op("act", lambda e: e.activation(out=ss[:, h0:20], in_=ss[:, h0:20], func=AF.Sqrt), reads=[R_ss], writes=[R_ss])
            f.op("dve", lambda e: e.reciprocal(ss[:, h0:20], ss[:, h0:20]), reads=[R_ss], writes=[R_ss])
            f.op("dve", lambda e: e.tensor_tensor(qkv_, pv, ss[:, h0:20].unsqueeze(2).broadcast_to([128, nh, 64]), ALU.mult), reads=[R_psq, R_ss], writes=[R_qk])
            if lat:
                f.op("dve", lambda e: e.tensor_tensor(qk[:, 0:16, :], qk[:, 0:16, :], gq[:, 0, :].unsqueeze(1).broadcast_to([128, 16, 64]), ALU.mult), reads=[R_qk, R_gq], writes=[R_qk])
            f.op("dve", lambda e: e.tensor_tensor(qk[:, 16:20, :], qk[:, 16:20, :], gq[:, 1, :].unsqueeze(1).broadcast_to([128, 4, 64]), ALU.mult), reads=[R_qk, R_gq], writes=[R_qk])
            if lat:
                q4 = qk[:].rearrange("p h (two f) -> p h two f", two=2)
                o4 = tqk[:].rearrange("p h (two f) -> p h two f", two=2)
                cosb = rope[:, 0, t - 2, :].unsqueeze(1).broadcast_to([128, 20, 32])
                sinb = rope[:, 1, t - 2, :].unsqueeze(1).broadcast_to([128, 20, 32])
                f.op("dve", lambda e: e.tensor_tensor(ra[:], q4[:, :, 0, :], cosb, ALU.mult), reads=[R_qk, R_rope], writes=[R_ra])
                f.op("dve", lambda e: e.tensor_tensor(rb[:], q4[:, :, 1, :], sinb, ALU.mult), reads=[R_qk, R_rope], writes=[R_rb])
                f.op("dve", lambda e: e.tensor_tensor(o4[:, :, 0, :], ra[:], rb[:], ALU.subtract), reads=[R_ra, R_rb], writes=[R_tqk])
                f.op("dve", lambda e: e.tensor_tensor(ra[:], q4[:, :, 1, :], cosb, ALU.mult), reads=[R_qk, R_rope, R_tqk], writes=[R_ra])
                f.op("dve", lambda e: e.tensor_tensor(rb[:], q4[:, :, 0, :], sinb, ALU.mult), reads=[R_qk, R_rope, R_tqk], writes=[R_rb])
                f.op("dve", lambda e: e.tensor_tensor(o4[:, :, 1, :], ra[:], rb[:], ALU.add), reads=[R_ra, R_rb], writes=[R_tqk])
            else:
                f.op("dve", lambda e: e.tensor_copy(tqk[:, 16:20, :], qk[:, 16:20, :]), reads=[R_qk], writes=[R_tqk])
            for a in range(4):
                f.op("dve", lambda e, a=a: e.tensor_copy(vaug[:, t, 64 + 128 * a:128 + 128 * a], ps_q[:, 1280 + 64 * a:1344 + 64 * a]), reads=[R_psq], writes=[R_v[t]])
            f.op("dve", lambda e: e.tensor_copy(kd[:, :, 0, :], tqk[:, 16:20, :]), reads=[R_tqk], writes=[R_kd])
            f.op("dve", lambda e: e.tensor_copy(kd[:, :, 1, :], tqk[:, 16:20, :]), reads=[R_tqk], writes=[R_kd])
            firstt = True
            if lat:
                for pr in range(8):
                    f.op("pe", lambda e, pr=pr: e.transpose(ps_t[:, pr, :], tqk[:, 2 * pr:2 * pr + 2, :].rearrange("p a d -> p (a d)"), identb[:]),
                         reads=[R_tqk, R_identb], writes=[R_pst], acc=(not firstt))
                    firstt = False
            for a in range(4):
                f.op("pe", lambda e, a=a: e.transpose(ps_t[:, 8 + a, :], kd[:, a, :, :].rearrange("p a d -> p (a d)"), identb[:]),
                     reads=[R_kd, R_identb], writes=[R_pst], acc=(not firstt))
                firstt = False
            f.op("act", lambda e: e.activation(out=kT2[:, :, cols], in_=ps_t[:, 8:12, :], func=AF.Identity), reads=[R_pst], writes=[R_kT[t]])
            if lat:
                f.op("dve", lambda e: e.tensor_copy(qts[b][:], ps_t[:, 0:8, :]), reads=[R_pst], writes=[R_qts[b]])
                f.dma("act", QT[:, :, (t - 2) * 128:(t - 1) * 128].rearrange("a p n -> p a n"), qts[b][:], reads=[R_qts[b]], writes=[R_QT[t]])
        S.close()
        S = Scope(nc)
        qb = [S.sb("qb%d" % k, [128, 2, 512], BF16) for k in range(2)]; R_qb = RL(2)
        ps_s = [S.ps("ps_s%d" % k, [128, 1024]) for k in range(2)]; R_pss = RL(2)
        ps_o = [S.ps("ps_o%d" % k, [128, 512]) for k in range(4)]; R_pso = RL(4)
        pT = [S.sb("pT%d" % k, [128, 1024], BF16) for k in range(3)]; R_pT = RL(3)
        dtmp = S.sb("dtmp", [128, 512]); R_dt = Res()
        ost = [S.sb("ost%d" % k, [128, 2, 512], BF16) for k in range(2)]; R_ost = RL(2)
        it = 0
        nq = 0
        for kvh in range(4):
            for qblk in range(8):
                qbi = nq % 2; nq += 1
                tq = [R_QT[2 + qblk * 4 + k] for k in range(4)]
                f.dma("sp", qb[qbi][:], QT[2 * kvh:2 * kvh + 2, :, qblk * 512:(qblk + 1) * 512].rearrange("a p n -> p a n"), reads=tq, writes=[R_qb[qbi]])
                items = [(kt, p_) for kt in range(NT) for p_ in range(2)]
                it0 = it; it += len(items)

                def front(idx):
                    kt, p_ = items[idx]
                    si = (it0 + idx) % 2
                    pi_ = (it0 + idx) % 3
                    for half in range(2):
                        base = 64 * half
                        f.op("pe", lambda e, half=half, base=base: e.matmul(ps_s[si][:, half * 512:(half + 1) * 512], kT2[base:base + 64, kvh, kt * 128:(kt + 1) * 128], qb[qbi][base:base + 64, p_, :], start=True, stop=True),
                             reads=[R_kT[kt], R_qb[qbi]], writes=[R_pss[si]], acc=(half > 0))
                    f.op("act", lambda e: e.activation(out=pT[pi_][:], in_=ps_s[si][:], func=AF.Exp, scale=0.125), reads=[R_pss[si]], writes=[R_pT[pi_]])

                def back(idx):
                    kt, p_ = items[idx]
                    pi_ = (it0 + idx) % 3
                    for half in range(2):
                        hh = 2 * p_ + half
                        voff = (64 if half == 0 else 0) + 128 * kvh
                        f.op("pe", lambda e, half=half, hh=hh, voff=voff: e.matmul(ps_o[hh][:], vaug[:, kt, voff:voff + 128], pT[pi_][:, half * 512:(half + 1) * 512], start=(kt == 0), stop=(kt == NT - 1)),
                             reads=[R_v[kt], R_pT[pi_]], writes=[R_pso[hh]], acc=(kt > 0))
                LA = 1
                for i_ in range(len(items) + LA):
                    if i_ < len(items):
                        front(i_)
                    if i_ >= LA:
                        back(i_ - LA)
                for hh in range(4):
                    h = kvh * 4 + hh
                    nb, db = (0, 64) if h % 2 == 0 else (64, 0)
                    f.op("dve", lambda e: e.reciprocal(dtmp[nb:nb + 64, :], ps_o[hh][db:db + 64, :]), reads=[R_pso[hh]], writes=[R_dt])
                    f.op("dve", lambda e: e.tensor_tensor(ost[qbi][nb:nb + 64, hh // 2, :], ps_o[hh][nb:nb + 64, :], dtmp[nb:nb + 64, :], ALU.mult),
                         reads=[R_pso[hh], R_dt], writes=[R_ost[qbi]])
                f.dma("act", OT[2 * kvh:2 * kvh + 2, :, qblk * 512:(qblk + 1) * 512].rearrange("a p n -> p a n"), ost[qbi][:], reads=[R_ost[qbi]],
                      writes=[R_oT[2 + qblk * 4 + k] for k in range(4)])
        S.close()
        L.close()
        M = Scope(nc)
        mixt = [M.sb("mixt%d" % k, [128, 8, 128], BF16) for k in range(2)]; R_mixt = RL(2)

        def mix_loader(t, b):
            f.dma("sp", mixt[b][:], OT[:, :, (t - 2) * 128:(t - 1) * 128].rearrange("a p n -> p a n"), reads=[R_oT[t]], writes=[R_mixt[b]])
            return [mixt[b][:, k, :] for k in range(8)], [R_mixt[b]]
        out_phase(1, odd_w_out, mix_loader, range(2, NT))
        M.close()

    phase_mod(0, 0)
    if stop_after == "mod":
        f.dma("sp", dbg[0:128, :], mod[:, 0].rearrange("p a d -> p (a d)"), reads=[R_mod], writes=[R_dbg])
        f.dma("sp", dbg[128:256, :], mod[:, 1].rearrange("p a d -> p (a d)"), reads=[R_mod], writes=[R_dbg])
    else:
        layer0_mixer()
    if stop_after in ("in0", "s5", "mod", "h0", "qkv", "win"):
        pass
    else:
        if stop_after == "mix0":
            pass
        else:
            phase_mod(0, 1)
            (ffn_phase if 'dense' in DBG_SKIP else ffn_sparse)(0, list(range(NT)), final=False)
            if stop_after != "l0":
                phase_mod(1, 0)
                layer1_mixer()
                if stop_after != "mix1":
                    phase_mod(1, 1)
                    (ffn_phase if 'dense' in DBG_SKIP else ffn_sparse)(1, list(range(2, NT)), final=True)
    if stop_after is not None and stop_after not in ("in0", "s5", "mod", "h0", "qkv", "win"):
        S = Scope(nc)
        tt = S.sb("dumpt", [128, D]); R_t = Res()
        for t in range(NT):
            f.dma("sp", tt[:], XR[t * 128:(t + 1) * 128, :], reads=[R_XR[t]], writes=[R_t])
            f.dma("sp", dbg[t * 128:(t + 1) * 128, :], tt[:], reads=[R_t], writes=[R_dbg])
        S.close()
    f.finish()
    Scope.FWREF = None
    G.close()
    f.close()
    return nc


_CONST = None


def _consts():
    global _CONST
    if _CONST is None:
        ident = np.eye(128, dtype=np.float32)
        n_freq = 16
        inv_freq = (10000.0 ** (-np.arange(n_freq, dtype=np.float32) / n_freq)).astype(np.float32)
        pos = np.arange(4096)
        r = (pos // 64).astype(np.float32); cc = (pos % 64).astype(np.float32)
        ang = np.concatenate([r[:, None] * inv_freq, cc[:, None] * inv_freq], -1).astype(np.float32)
        cos = np.cos(ang).astype(np.float32).reshape(32, 128, 32).transpose(1, 0, 2)
        sin = np.sin(ang).astype(np.float32).reshape(32, 128, 32).transpose(1, 0, 2)
        rope = np.ascontiguousarray(np.stack([cos, sin], axis=1))
        k = np.arange(128)[:, None]; q = np.arange(128)[None, :]
        mask = np.stack([(q <= k), (k <= q), (k < q)], axis=1).astype(np.float32)
        pc = (np.arange(128, dtype=np.float32)[:, None] + 128.0 * np.arange(4, dtype=np.float32)[None, :]).astype(np.float32)
        jidx = np.broadcast_to(np.arange(128, dtype=np.float32)[None, :], (128, 128)).copy()
        _CONST = {"k_ident": ident, "k_rope": rope, "k_mask": np.ascontiguousarray(mask), "k_jidx": jidx, "k_pc": np.ascontiguousarray(pc)}
    return _CONST


def make_in_map(inputs, b):
    f32 = lambda a: np.ascontiguousarray(np.asarray(a, dtype=np.float32))
    m = {
        "x": f32(inputs["x"][b]), "ctx": f32(inputs["ctx"][b]), "c": f32(inputs["c"][b:b + 1]),
        "c_ctx": f32(inputs["c_ctx"]).reshape(1, D),
        "ada_w": f32(inputs["ada_w"]), "ada_b": f32(inputs["ada_b"]), "ln_g": f32(inputs["ln_g"]), "ln_b": f32(inputs["ln_b"]),
        "even_w_in": f32(inputs["even_w_in"][0]), "even_w_out": f32(inputs["even_w_out"][0]),
        "s5_lam_re": f32(inputs["s5_lam_re"][0]), "s5_lam_im": f32(inputs["s5_lam_im"][0]), "s5_log_step": f32(inputs["s5_log_step"][0]),
        "s5_b_re": f32(inputs["s5_b_re"][0]), "s5_b_im": f32(inputs["s5_b_im"][0]),
        "s5_c_re": f32(inputs["s5_c_re"][0]), "s5_c_im": f32(inputs["s5_c_im"][0]),
        "s5_d": f32(inputs["s5_d"][0]), "s5_w_glu": f32(inputs["s5_w_glu"][0]), "s5_b_glu": f32(inputs["s5_b_glu"][0]),
        "win_sink": f32(inputs["win_sink"][0]),
        "odd_w_in": f32(inputs["odd_w_in"][0]), "odd_w_out": f32(inputs["odd_w_out"][0]),
        "odd_q_norm": f32(inputs["odd_q_norm"][0]), "odd_k_norm": f32(inputs["odd_k_norm"][0]),
        "router_w": f32(inputs["router_w"]), "router_bias": f32(inputs["router_bias"]),
        "moe_w_gate": f32(inputs["moe_w_gate"]), "moe_w_up": f32(inputs["moe_w_up"]), "moe_w_down": f32(inputs["moe_w_down"]),
    }
    m.update(_consts())
    return m


def kernel(**inputs):
    nc = build_program()
    shared = make_in_map(inputs, 0)
    in_maps = []
    for b in range(8):
        m = dict(shared)
        m["x"] = np.ascontiguousarray(np.asarray(inputs["x"][b], dtype=np.float32))
        m["ctx"] = np.ascontiguousarray(np.asarray(inputs["ctx"][b], dtype=np.float32))
        m["c"] = np.ascontiguousarray(np.asarray(inputs["c"][b:b + 1], dtype=np.float32))
        in_maps.append(m)
    res = run_bass_kernel_spmd(nc, in_maps, core_ids=list(range(8)))
    return np.stack([np.asarray(r["out"], dtype=np.float32) for r in res.results], axis=0)
```
